# Optimizing a Trainium2 kernel written in Bass

```python
import math
import jax
import jax.numpy as jnp
from jax import lax
import numpy as np

D_MODEL = 1024
BATCH = 2
SEQ = 16384
DEPTH = 2

GRID_W = 64
CTX_LEN = 256
N_BRANCH = 3
BRANCH_WIDTH = D_MODEL // 2
POOL_WINDOWS = (2, 4, 8, 16)
POOL_GROUPS = len(POOL_WINDOWS)
POOL_GROUP_DIM = BRANCH_WIDTH // POOL_GROUPS
DIFF_HEAD_DIM = 64
DIFF_HEADS = BRANCH_WIDTH // (2 * DIFF_HEAD_DIM)
DIFF_V_DIM = 2 * DIFF_HEAD_DIM
DIFF_QK_WIDTH = DIFF_HEADS * 2 * DIFF_HEAD_DIM
DIFF_V_WIDTH = DIFF_HEADS * DIFF_V_DIM
ROPE_AXIS_FREQS = DIFF_HEAD_DIM // 4
ROPE_THETA = 10000.0
Q_BLOCK = 128
RWKV_HEAD = 64
RWKV_HEADS = BRANCH_WIDTH // RWKV_HEAD
RWKV_WIDTH = RWKV_HEADS * RWKV_HEAD
DECAY_LORA = 64
AAA_LORA = 64
GATE_LORA = 128
RWKV_SPLITS = (RWKV_WIDTH, RWKV_WIDTH, RWKV_WIDTH, 2 * DECAY_LORA, 2 * AAA_LORA, GATE_LORA)
RWKV_IN = 3 * RWKV_WIDTH + 2 * DECAY_LORA + 2 * AAA_LORA + GATE_LORA
IN_SPLITS = (BRANCH_WIDTH, DIFF_QK_WIDTH, DIFF_QK_WIDTH, DIFF_V_WIDTH, RWKV_IN, N_BRANCH * D_MODEL)
IN_WIDTH = BRANCH_WIDTH + 2 * DIFF_QK_WIDTH + DIFF_V_WIDTH + RWKV_IN + N_BRANCH * D_MODEL
N_GROUPS = 4
EXPERTS_PER_GROUP = 8
N_EXPERTS = N_GROUPS * EXPERTS_PER_GROUP
TOP_K = 2
EXPERT_FF = D_MODEL // 2
MOE_BLOCK = 128
N_MOD = 6
NORM_EPS = 1e-6
GN_EPS = 64e-5

kernel_name = 'hybrid_pool_diffattn_rwkv7_hmoe_dit'


def split_cols(p, sizes):
    return jnp.split(p, [int(s) for s in np.cumsum(sizes)[:-1]], axis=-1)


def rms_norm(x, g):
    xf = x.astype(jnp.float32)
    y = xf * lax.rsqrt(jnp.mean(xf * xf, axis=-1, keepdims=True) + NORM_EPS)
    return (y * g.astype(jnp.float32)).astype(x.dtype)


def modulate(x, g, shift, scale):
    return rms_norm(x, g) * (1 + scale) + shift


def centred_window_mean(u, win):
    L = u.shape[1]
    cs = jnp.pad(jnp.cumsum(u.astype(jnp.float32), axis=1), ((0, 0), (1, 0), (0, 0)))
    t = jnp.arange(L)
    lo = jnp.clip(t - win // 2, 0, L - 1)
    hi = jnp.clip(t + win // 2 - 1, 0, L - 1)
    cnt = (hi - lo + 1).astype(jnp.float32)
    return (cs[:, hi + 1] - cs[:, lo]) / cnt[None, :, None]


def pool_mixer(u, w_pool, pool_scale):
    B, L, _ = u.shape
    ug = u.reshape(B, L, POOL_GROUPS, POOL_GROUP_DIM)
    pooled = jnp.stack([centred_window_mean(ug[:, :, i], w) for i, w in enumerate(POOL_WINDOWS)], axis=2)
    diff = pooled.astype(u.dtype) - ug
    y = jnp.einsum('blgc,gcd->blgd', diff, w_pool)
    return y.reshape(B, L, BRANCH_WIDTH) * pool_scale


def rope_rotate(u, ang):
    u1, u2 = jnp.split(u, 2, axis=-1)
    cs = jnp.cos(ang).astype(u.dtype)
    sn = jnp.sin(ang).astype(u.dtype)
    return jnp.concatenate([u1 * cs - u2 * sn, u2 * cs + u1 * sn], axis=-1)


def axial_rope(x, ang_r, ang_c):
    xr, xc = jnp.split(x, 2, axis=-1)
    ar = ang_r[None, :, None, None, :]
    ac = ang_c[None, :, None, None, :]
    return jnp.concatenate([rope_rotate(xr, ar), rope_rotate(xc, ac)], axis=-1)


def diff_qkv(pq, pk, pv, qn_g, kn_g):
    B, L, _ = pq.shape
    q = rms_norm(pq.reshape(B, L, DIFF_HEADS, 2, DIFF_HEAD_DIM), qn_g)
    k = rms_norm(pk.reshape(B, L, DIFF_HEADS, 2, DIFF_HEAD_DIM), kn_g)
    v = pv.reshape(B, L, DIFF_HEADS, DIFF_V_DIM)
    return q, k, v


def diff_attn_block(qb, keys, vals, lam):
    s = jnp.einsum('bqhmd,bkhmd->bhmqk', qb, keys).astype(jnp.float32) * (DIFF_HEAD_DIM ** -0.5)
    p = jax.nn.softmax(s, axis=-1)
    a = p[:, :, 0] - lam * p[:, :, 1]
    return jnp.einsum('bhqk,bkhv->bqhv', a.astype(vals.dtype), vals)


def diff_attn_latent(q, keys, vals, lam):
    B, L = q.shape[:2]
    nb = L // Q_BLOCK
    qb = jnp.moveaxis(q.reshape(B, nb, Q_BLOCK, DIFF_HEADS, 2, DIFF_HEAD_DIM), 1, 0)
    out = lax.map(lambda blk: diff_attn_block(blk, keys, vals, lam), qb)
    return jnp.moveaxis(out, 0, 1).reshape(B, L, DIFF_HEADS, DIFF_V_DIM)


def diff_post(o, subln_g, lam_init):
    B, L = o.shape[:2]
    return (rms_norm(o, subln_g) * (1 - lam_init)).reshape(B, L, DIFF_V_WIDTH)


def token_shift_centred(u, mu):
    prev = jnp.pad(u, ((0, 0), (1, 0), (0, 0)))[:, :-1]
    nxt = jnp.pad(u, ((0, 0), (0, 1), (0, 0)))[:, 1:]
    return u + (prev - u) * mu[0] + (nxt - u) * mu[1]


def rwkv_prepare(pr, shift_mu, w0, w2, a0, a2, g2, k_k, k_a):
    B, L, _ = pr.shape
    u = token_shift_centred(pr, shift_mu)
    r, k, v, wl, al, gl = split_cols(u, RWKV_SPLITS)
    wl = wl.reshape(B, L, 2, DECAY_LORA)
    al = al.reshape(B, L, 2, AAA_LORA)
    w_log = -jax.nn.softplus(-(w0 + jnp.einsum('bldr,drc->bldc', jnp.tanh(wl), w2))) - 0.5
    decay = jnp.exp(-jnp.exp(w_log.astype(jnp.float32)))
    a = jax.nn.sigmoid(a0 + jnp.einsum('bldr,drc->bldc', al, a2))
    g = jax.nn.sigmoid(gl) @ g2
    kk = (k * k_k).reshape(B, L, RWKV_HEADS, RWKV_HEAD).astype(jnp.float32)
    kk = kk / jnp.maximum(jnp.sqrt(jnp.sum(kk * kk, axis=-1, keepdims=True)), 1e-12)
    kk = kk.reshape(B, L, RWKV_WIDTH).astype(k.dtype)
    k_mod = k[:, :, None] * (1 + (a - 1) * k_a)
    b_vec = kk[:, :, None] * a
    return r, k_mod, v, decay, -kk, b_vec, g


def to_scan(u):
    B, L = u.shape[:2]
    u = jnp.stack([u[:, :, 0], jnp.flip(u[:, :, 1], axis=1)], axis=0)
    return jnp.moveaxis(u.reshape(2, B, L, RWKV_HEADS, RWKV_HEAD), 2, 0).astype(jnp.float32)


def wkv_step(S, inp):
    r, w, k, v, a, b = inp
    sa = jnp.einsum('dbhvk,dbhk->dbhv', S, a)
    S = S * w[..., None, :] + sa[..., None] * b[..., None, :] + v[..., None] * k[..., None, :]
    return S, jnp.einsum('dbhvk,dbhk->dbhv', S, r)


def wkv_bidir(S0, r, k_mod, v, decay, a_vec, b_vec):
    B, L, C = r.shape
    both = lambda u: jnp.broadcast_to(u[:, :, None], (B, L, 2, C))
    xs = tuple(to_scan(u) for u in (both(r), decay, k_mod, both(v), both(a_vec), b_vec))
    S, ys = lax.scan(wkv_step, S0, xs)
    y = ys[:, 0] + jnp.flip(ys[:, 1], axis=0)
    return S, jnp.moveaxis(y, 0, 1)


def rwkv_output(y, prep, r_k, gn_w, gn_b):
    r, k_mod, v, g = prep[0], prep[1], prep[2], prep[6]
    B, L, _ = r.shape
    mu = jnp.mean(y, axis=-1, keepdims=True)
    var = jnp.mean(jnp.square(y - mu), axis=-1, keepdims=True)
    yn = ((y - mu) * lax.rsqrt(var + GN_EPS)).reshape(B, L, RWKV_WIDTH) * gn_w + gn_b
    coef = jnp.sum((r[:, :, None] * k_mod * r_k.reshape(-1)).reshape(B, L, 2, RWKV_HEADS, RWKV_HEAD), axis=(2, 4))
    bonus = (coef[..., None] * v.reshape(B, L, RWKV_HEADS, RWKV_HEAD)).reshape(B, L, RWKV_WIDTH)
    return (yn.astype(r.dtype) + bonus) * g


def merge_branches(branches, gates, w_br, w_out):
    B, L, _ = gates.shape
    g = jax.nn.sigmoid(gates.reshape(B, L, N_BRANCH, D_MODEL))
    merged = g[:, :, 0] * (branches[0] @ w_br[0])
    for n in range(1, N_BRANCH):
        merged = merged + g[:, :, n] * (branches[n] @ w_br[n])
    return merged @ w_out


def expert_dispatch(h, eid, gates, w_g, w_u, w_d):
    T, D = h.shape
    A = T * TOP_K
    flat_e = eid.reshape(A)
    flat_tok = jnp.repeat(jnp.arange(T, dtype=jnp.int32), TOP_K)
    order = jnp.argsort(flat_e)
    e_sorted = flat_e[order]
    counts = jnp.bincount(flat_e, length=N_EXPERTS)
    padded = (counts + MOE_BLOCK - 1) // MOE_BLOCK * MOE_BLOCK
    start = jnp.cumsum(counts) - counts
    pend = jnp.cumsum(padded)
    pstart = pend - padded
    slot = pstart[e_sorted] + jnp.arange(A, dtype=jnp.int32) - start[e_sorted]
    P = -(-A // MOE_BLOCK) * MOE_BLOCK + N_EXPERTS * MOE_BLOCK
    n_blk = P // MOE_BLOCK
    slot_tok = jnp.full((P,), T, jnp.int32).at[slot].set(flat_tok[order])
    slot_gate = jnp.zeros((P,), h.dtype).at[slot].set(gates.reshape(A)[order])
    blk_e = jnp.minimum(jnp.searchsorted(pend, jnp.arange(n_blk, dtype=jnp.int32) * MOE_BLOCK, side='right'), N_EXPERTS - 1)
    xb = jnp.concatenate([h, jnp.zeros((1, D), h.dtype)], axis=0)[slot_tok].reshape(n_blk, MOE_BLOCK, D)

    def block_ffn(args):
        xblk, e = args
        return (jax.nn.silu(xblk @ w_g[e]) * (xblk @ w_u[e])) @ w_d[e]

    yb = lax.map(block_ffn, (xb, blk_e)).reshape(P, D)
    return jax.ops.segment_sum(yb * slot_gate[:, None], slot_tok, num_segments=T + 1)[:T]


def hier_moe(h, w_rg, w_re, w_g, w_u, w_d):
    T = h.shape[0]
    lg = (h @ w_rg).astype(jnp.float32)
    grp = jnp.argmax(lg, axis=-1)
    p_grp = jnp.take_along_axis(jax.nn.softmax(lg, axis=-1), grp[:, None], axis=-1)
    le = (h @ w_re).astype(jnp.float32).reshape(T, N_GROUPS, EXPERTS_PER_GROUP)
    le = jnp.take_along_axis(le, grp[:, None, None], axis=1)[:, 0]
    top_v, top_i = lax.top_k(le, TOP_K)
    gates = p_grp * jax.nn.softmax(top_v, axis=-1)
    eid = (grp[:, None] * EXPERTS_PER_GROUP + top_i).astype(jnp.int32)
    return expert_dispatch(h, eid, gates.astype(h.dtype), w_g, w_u, w_d)


def setup_inputs(seed: int = 0) -> dict:
    key = jax.random.key(seed)
    keys = jax.random.split(key, 40)
    cnt = [0]

    def nk():
        cnt[0] += 1
        return keys[cnt[0] - 1]

    def nrm(shape, scale):
        return scale * jax.random.normal(nk(), shape, jnp.float32)

    def uni(shape, lo, hi):
        return jax.random.uniform(nk(), shape, jnp.float32, lo, hi)

    D = D_MODEL
    return {
        'x': nrm((BATCH, SEQ, D), 1.0),
        'c': nrm((BATCH, D), 1.0),
        'ctx': nrm((BATCH, CTX_LEN, D), 1.0),
        'c_ctx': nrm((D,), 1.0),
        'w_ada': nrm((DEPTH, D, N_MOD * D), 0.5 * D ** -0.5),
        'b_ada': nrm((DEPTH, N_MOD * D), 0.02),
        'norm1_g': 1.0 + nrm((DEPTH, D), 0.05),
        'norm2_g': 1.0 + nrm((DEPTH, D), 0.05),
        'w_in': nrm((DEPTH, D, IN_WIDTH), D ** -0.5),
        'pool_w': nrm((DEPTH, POOL_GROUPS, POOL_GROUP_DIM, POOL_GROUP_DIM), POOL_GROUP_DIM ** -0.5),
        'pool_scale': 1.0 + nrm((DEPTH, BRANCH_WIDTH), 0.1),
        'q_norm_g': 1.0 + nrm((DEPTH, DIFF_HEAD_DIM), 0.05),
        'k_norm_g': 1.0 + nrm((DEPTH, DIFF_HEAD_DIM), 0.05),
        'lambda_qk': nrm((DEPTH, 4, DIFF_HEAD_DIM), 0.1),
        'subln_g': 1.0 + nrm((DEPTH, DIFF_V_DIM), 0.05),
        'shift_mu': uni((DEPTH, 2, RWKV_IN), 0.0, 0.5),
        'decay_w0': uni((DEPTH, 2, RWKV_WIDTH), -4.0, 1.0),
        'decay_w2': nrm((DEPTH, 2, DECAY_LORA, RWKV_WIDTH), 0.5 * DECAY_LORA ** -0.5),
        'aaa_a0': nrm((DEPTH, 2, RWKV_WIDTH), 0.5),
        'aaa_a2': nrm((DEPTH, 2, AAA_LORA, RWKV_WIDTH), 0.5 * AAA_LORA ** -0.5),
        'gate_w2': nrm((DEPTH, GATE_LORA, RWKV_WIDTH), GATE_LORA ** -0.5),
        'k_k': 0.85 + nrm((DEPTH, RWKV_WIDTH), 0.1),
        'k_a': 1.0 + nrm((DEPTH, RWKV_WIDTH), 0.1),
        'r_k': nrm((DEPTH, RWKV_HEADS, RWKV_HEAD), 0.1),
        'gn_w': 1.0 + nrm((DEPTH, RWKV_WIDTH), 0.05),
        'gn_b': nrm((DEPTH, RWKV_WIDTH), 0.02),
        'w_br': nrm((DEPTH, N_BRANCH, BRANCH_WIDTH, D), BRANCH_WIDTH ** -0.5),
        'w_out': nrm((DEPTH, D, D), D ** -0.5),
        'w_router_group': nrm((DEPTH, D, N_GROUPS), D ** -0.5),
        'w_router_expert': nrm((DEPTH, D, N_EXPERTS), D ** -0.5),
        'w_exp_gate': nrm((DEPTH, N_EXPERTS, D, EXPERT_FF), D ** -0.5),
        'w_exp_up': nrm((DEPTH, N_EXPERTS, D, EXPERT_FF), D ** -0.5),
        'w_exp_down': nrm((DEPTH, N_EXPERTS, EXPERT_FF, D), EXPERT_FF ** -0.5),
    }


def reference(x, c, ctx, c_ctx, w_ada, b_ada, norm1_g, norm2_g, w_in, pool_w, pool_scale, q_norm_g, k_norm_g,
              lambda_qk, subln_g, shift_mu, decay_w0, decay_w2, aaa_a0, aaa_a2, gate_w2, k_k, k_a, r_k, gn_w, gn_b,
              w_br, w_out, w_router_group, w_router_expert, w_exp_gate, w_exp_up, w_exp_down):
    B, L, D = x.shape
    C = ctx.shape[1]
    rows = L // GRID_W
    row_idx = jnp.repeat(jnp.arange(rows), GRID_W).astype(jnp.float32)
    col_idx = jnp.tile(jnp.arange(GRID_W), rows).astype(jnp.float32)
    inv_freq = ROPE_THETA ** (-jnp.arange(ROPE_AXIS_FREQS, dtype=jnp.float32) / ROPE_AXIS_FREQS)
    ang_r = row_idx[:, None] * inv_freq
    ang_c = col_idx[:, None] * inv_freq
    s_lat = jax.nn.silu(c)
    s_ctx = jax.nn.silu(c_ctx)
    S0 = jnp.zeros((2, B, RWKV_HEADS, RWKV_HEAD, RWKV_HEAD), jnp.float32)

    for l in range(DEPTH):
        last = l == DEPTH - 1
        mod = (s_lat @ w_ada[l] + b_ada[l]).reshape(B, N_MOD, 1, D)
        mod_c = (s_ctx @ w_ada[l] + b_ada[l]).reshape(N_MOD, 1, 1, D)

        p_l = modulate(x, norm1_g[l], mod[:, 0], mod[:, 1]) @ w_in[l]
        p_c = modulate(ctx, norm1_g[l], mod_c[0], mod_c[1]) @ w_in[l]
        pool_l, q_l, k_l, v_l, rw_l, gate_l = split_cols(p_l, IN_SPLITS)
        pool_c, q_c, k_c, v_c, rw_c, gate_c = split_cols(p_c, IN_SPLITS)

        lam_init = 0.8 - 0.6 * math.exp(-0.3 * l)
        lq = lambda_qk[l].astype(jnp.float32)
        lam = jnp.exp(jnp.sum(lq[0] * lq[1])) - jnp.exp(jnp.sum(lq[2] * lq[3])) + lam_init
        qh_l, kh_l, vh_l = diff_qkv(q_l, k_l, v_l, q_norm_g[l], k_norm_g[l])
        qh_l = axial_rope(qh_l, ang_r, ang_c)
        kh_l = axial_rope(kh_l, ang_r, ang_c)
        qh_c, kh_c, vh_c = diff_qkv(q_c, k_c, v_c, q_norm_g[l], k_norm_g[l])
        keys = jnp.concatenate([kh_c, kh_l], axis=1)
        vals = jnp.concatenate([vh_c, vh_l], axis=1)
        att_l = diff_post(diff_attn_latent(qh_l, keys, vals, lam), subln_g[l], lam_init)

        rw_par = (shift_mu[l], decay_w0[l], decay_w2[l], aaa_a0[l], aaa_a2[l], gate_w2[l], k_k[l], k_a[l])
        prep_c = rwkv_prepare(rw_c, *rw_par)
        S_c, y_c = wkv_bidir(S0, *prep_c[:6])
        prep_l = rwkv_prepare(rw_l, *rw_par)
        _, y_l = wkv_bidir(S_c, *prep_l[:6])
        rwo_l = rwkv_output(y_l, prep_l, r_k[l], gn_w[l], gn_b[l])

        mix_l = merge_branches((pool_mixer(pool_l, pool_w[l], pool_scale[l]), att_l, rwo_l), gate_l, w_br[l], w_out[l])
        x = x + mod[:, 2] * mix_l
        if not last:
            att_c = diff_post(diff_attn_block(qh_c, kh_c, vh_c, lam), subln_g[l], lam_init)
            rwo_c = rwkv_output(y_c, prep_c, r_k[l], gn_w[l], gn_b[l])
            mix_c = merge_branches((pool_mixer(pool_c, pool_w[l], pool_scale[l]), att_c, rwo_c), gate_c, w_br[l], w_out[l])
            ctx = ctx + mod_c[2] * mix_c

        moe_par = (w_router_group[l], w_router_expert[l], w_exp_gate[l], w_exp_up[l], w_exp_down[l])
        f_l = modulate(x, norm2_g[l], mod[:, 3], mod[:, 4]).reshape(B * L, D)
        if last:
            x = x + mod[:, 5] * hier_moe(f_l, *moe_par).reshape(B, L, D)
        else:
            f_c = modulate(ctx, norm2_g[l], mod_c[3], mod_c[4]).reshape(B * C, D)
            y = hier_moe(jnp.concatenate([f_c, f_l], axis=0), *moe_par)
            ctx = ctx + mod_c[5] * y[:B * C].reshape(B, C, D)
            x = x + mod[:, 5] * y[B * C:].reshape(B, L, D)
    return x
```

```python
import math
import contextlib


import numpy as np
import concourse.bass as bass
import concourse.mybir as mybir
from concourse.bass_utils import run_bass_kernel_spmd

F32 = mybir.dt.float32
BF16 = mybir.dt.bfloat16
I32 = mybir.dt.int32
AF = mybir.ActivationFunctionType
ALU = mybir.AluOpType
AX = mybir.AxisListType

SEM_LIMIT = 8000
DMA_POOL = 40


class Prog:
    def __init__(self, nc):
        self.nc = nc
        self.ops = []

    def op(self, eng, fn, r=(), w=()):
        self.ops.append(dict(eng=eng, fn=fn, r=tuple(r), w=tuple(w), dma=False, final=False))

    def dma(self, eng, out, in_, r=(), w=(), final=False, **kw):
        def fn(e, out=out, in_=in_, kw=kw):
            return e.dma_start(out=out, in_=in_, **kw)
        self.ops.append(dict(eng=eng, fn=fn, r=tuple(r), w=tuple(w), dma=True, final=final))

    def cc(self, ins_ap, out_ap, groups, r=(), w=()):
        def fn(e, ins_ap=ins_ap, out_ap=out_ap, groups=groups):
            return e.collective_compute("AllGather", ALU.bypass, replica_groups=groups, ins=[ins_ap], outs=[out_ap])
        self.ops.append(dict(eng='pool', fn=fn, r=tuple(r), w=tuple(w), dma=True, final=False, cc=True))

    def fence(self):
        self.ops.append(dict(eng=None, fn=None, r=(), w=(), dma=False, final=False, fence=True))

    def emit(self):
        nc = self.nc
        raw_ops = self.ops
        ops = []
        fence_after = {}
        fence_pos = []
        for o in raw_ops:
            if o.get('fence'):
                fence_pos.append(len(ops))
            else:
                ops.append(o)
        self.ops = ops
        n = len(ops)
        fence_deps_at = {}
        prev = 0
        for fp in fence_pos:
            last = {}
            dm = set()
            for i in range(prev, fp):
                o = ops[i]
                if o['dma']:
                    dm.add(i)
                else:
                    last[o['eng']] = i
            fence_deps_at[fp] = set(last.values()) | dm
            prev = fp
        last_w = {}
        readers = {}
        deps = [None] * n
        cur_fence = set()
        first_after = {}
        for i, o in enumerate(ops):
            if i in fence_deps_at:
                cur_fence = fence_deps_at[i]
                first_after = {}
            d = {}

            def add(j, raw):
                d[j] = d.get(j, False) or raw
            for k in o['r']:
                if k in last_w:
                    add(last_w[k], True)
            for k in o['w']:
                if k in last_w:
                    add(last_w[k], False)
                for tok, j in readers.get(k, {}).items():
                    add(j, False)
            keep = set()
            for j, raw in d.items():
                if j == i:
                    continue
                oj = ops[j]
                if (not oj['dma']) and (not o['dma']) and oj['eng'] == o['eng']:
                    if o['eng'] == 'pe':
                        continue
                    if not raw:
                        continue
                keep.add(j)
            tok_e = o['eng']
            if cur_fence and tok_e not in first_after:
                first_after[tok_e] = i
                for j in cur_fence:
                    if ops[j]['dma'] or ops[j]['eng'] != tok_e:
                        keep.add(j)
            deps[i] = keep
            for k in o['r']:
                tok = ('d', i) if o['dma'] else o['eng']
                readers.setdefault(k, {})[tok] = i
            for k in o['w']:
                last_w[k] = i
                readers[k] = {}
        needed = [False] * n
        for i in range(n):
            for j in deps[i]:
                needed[j] = True
        engs = ['pe', 'act', 'dve', 'pool', 'sp']
        cnt = {e: 0 for e in engs}
        sig = [None] * n
        dma_uses = [0] * DMA_POOL
        dma_last = [None] * DMA_POOL
        ndma = 0
        semkeys = set()
        for i, o in enumerate(ops):
            if o.get('cc'):
                ncc_ = getattr(self, '_ncc', 0) + 1
                self._ncc = ncc_
                sig[i] = (('cc', 0), ncc_)
                semkeys.add(('cc', 0))
            elif o['dma']:
                j = ndma % DMA_POOL
                ndma += 1
                dma_uses[j] += 1
                if dma_last[j] is not None:
                    deps[i].add(dma_last[j])
                dma_last[j] = i
                sig[i] = (('dma', j), 16 * dma_uses[j])
                semkeys.add(('dma', j))
            elif needed[i]:
                e = o['eng']
                c = cnt[e]
                cnt[e] += 1
                sk = (e, c // SEM_LIMIT)
                sig[i] = (sk, c % SEM_LIMIT + 1)
                semkeys.add(sk)
        finals = [i for i, o in enumerate(ops) if o['final']]
        seen = {e: {} for e in engs}
        streams = {e: [] for e in engs}
        for i, o in enumerate(ops):
            e = o['eng']
            waits = {}
            for j in deps[i]:
                sk, v = sig[j]
                if seen[e].get(sk, 0) >= v:
                    continue
                waits[sk] = max(waits.get(sk, 0), v)
            for sk, v in waits.items():
                seen[e][sk] = v
            streams[e].append((list(waits.items()), o['fn'], sig[i]))
        fw = {}
        for i in finals:
            sk, v = sig[i]
            if seen['sp'].get(sk, 0) >= v:
                continue
            fw[sk] = max(fw.get(sk, 0), v)
        streams['sp'].append((list(fw.items()), None, None))
        self.stats = dict(n_ops=n, cnt=dict(cnt), ndma=ndma,
                          nwaits={e: sum(len(s[0]) for s in streams[e]) for e in engs})
        semkeys = sorted(semkeys, key=str)
        import contextlib
        with contextlib.ExitStack() as st:
            sems = {}
            for sk in semkeys:
                sems[sk] = st.enter_context(nc.semaphore("s_%s_%s" % (sk[0], sk[1])))
            block = st.enter_context(nc.Block())

            def run(engine, items):
                for waits, fn, sg in items:
                    for sk, v in waits:
                        engine.wait_ge(sems[sk], v)
                    if fn is None:
                        continue
                    ins = fn(engine)
                    if sg is not None:
                        inc = 16 if sg[0][0] == 'dma' else 1
                        ins.then_inc(sems[sg[0]], inc)

            @block.tensor
            def _(e):
                run(e, streams['pe'])

            @block.scalar
            def _(e):
                run(e, streams['act'])

            @block.vector
            def _(e):
                run(e, streams['dve'])

            @block.gpsimd
            def _(e):
                run(e, streams['pool'])

            @block.sync
            def _(e):
                run(e, streams['sp'])
        return self.stats


class Arena:
    LO = 16512
    HI = 229376

    def __init__(self, nc):
        self.nc = nc
        self.top = self.LO
        self.n = 0
        self.peak = self.LO

    def alloc(self, name, shape, dt=F32):
        esz = {F32: 4, BF16: 2, I32: 4}[dt]
        nb = esz
        for s_ in shape[1:]:
            nb *= s_
        off = (self.top + 63) // 64 * 64
        assert off + nb <= self.HI, ("SBUF overflow", name, off + nb)
        self.top = off + nb
        self.peak = max(self.peak, self.top)
        self.n += 1
        return self.nc.alloc_sbuf_tensor_at("%s_%d" % (name, self.n), list(shape), dt, offset=off)

    def mark(self):
        return self.top

    def release(self, m):
        self.top = m


GN_EPS = 64e-5
FINAL_OUT = False
KAPPA = 0.6065306597126334
CTXN = 256


def rwkv_phase(nc, P, A, bank, pT, L, ctx_out, out_ap, pfx=''):
    T = CTXN + L
    di = lambda name, shape: nc.dram_tensor(pfx + name, list(shape), F32, kind="ExternalInput").ap()
    cmu = di("rw_cmu", [128, 6, 2])
    g2s = di("rw_g2", [128, 128])
    w2s = di("rw_w2", [64, 256])
    a2s = di("rw_a2", [64, 256])
    w0b = di("rw_w0b", [64, 256])
    a0f = di("rw_a0f", [64, 4])
    prm = di("rw_prm", [64, 2, 5])
    cst = di("rw_cst", [64, 448])
    uT = nc.dram_tensor(pfx + "uT_scr", [6, 128, T], F32, kind="Internal").ap()
    Yd = nc.dram_tensor(pfx + "Yd_scr", [2, 128, T], F32, kind="Internal").ap()
    Bd = nc.dram_tensor(pfx + "Bd_scr", [2, 128, T], F32, kind="Internal").ap()

    def TT(eng, out, a, b, op, r, w):
        P.op(eng, lambda e: e.tensor_tensor(out=out, in0=a, in1=b, op=op), r=r, w=w)

    def TS(eng, out, a, s1, s2, op0, op1, r, w):
        if op1 is None:
            P.op(eng, lambda e: e.tensor_scalar(out=out, in0=a, scalar1=s1, scalar2=None, op0=op0), r=r, w=w)
        else:
            P.op(eng, lambda e: e.tensor_scalar(out=out, in0=a, scalar1=s1, scalar2=s2, op0=op0, op1=op1), r=r, w=w)

    def STT(out, a, s, b, op0, op1, r, w):
        P.op('dve', lambda e: e.scalar_tensor_tensor(out=out, in0=a, scalar=s, in1=b, op0=op0, op1=op1), r=r, w=w)

    def ACT(out, a, func, r, w, scale=1.0, bias=None):
        if bias is None:
            P.op('act', lambda e: e.activation(out=out, in_=a, func=func, scale=scale), r=r, w=w)
        else:
            P.op('act', lambda e: e.activation(out=out, in_=a, func=func, scale=scale, bias=bias), r=r, w=w)

    def MM(out, lhsT, rhs, r, w, start=True, stop=True):
        P.op('pe', lambda e: e.matmul(out, lhsT=lhsT, rhs=rhs, start=start, stop=stop), r=r, w=w)

    m0 = A.mark()
    cmu_sb = A.alloc("cmu", [128, 6, 2])
    c0_sb = A.alloc("c0", [128, 6])
    g2_sb = A.alloc("g2", [128, 128])
    raw = [A.alloc("raw%d" % i, [128, 6, 514]) for i in range(2)]
    ush = A.alloc("ush", [128, 6, 512])
    sg = A.alloc("sg", [128, 512])
    P.dma('sp', cmu_sb[:], cmu, w=['cmu'])
    P.dma('sp', g2_sb[:], g2s, w=['g2'])
    TT('dve', c0_sb[:], cmu_sb[:, :, 0], cmu_sb[:, :, 1], ALU.add, ['cmu'], ['c0'])
    TS('dve', c0_sb[:], c0_sb[:], -1.0, 1.0, ALU.mult, ALU.add, ['c0'], ['c0'])
    seqs = [(0, CTXN), (CTXN, L)]
    ti = 0
    for (s0, slen) in seqs:
        for b0 in range(0, slen, 512):
            n = min(512, slen - b0)
            rb = raw[ti % 2]
            rk = ('raw', ti % 2)
            ti += 1
            lo = max(0, b0 - 1)
            hi = min(slen, b0 + n + 1)
            if b0 == 0 or b0 + n == slen:
                P.op('pool', lambda e, rb=rb: e.memset(rb[:], 0.0), w=[rk])
            P.dma('sp', rb[:, :, 1 + lo - b0:1 + hi - b0], pT[1:7, :, s0 + lo:s0 + hi].rearrange("g p t -> p g t"),
                  r=['pTall'], w=[rk])
            for g in range(6):
                eng = 'dve'
                TS('pool', ush[:, g, :n], rb[:, g, 1:1 + n], c0_sb[:, g:g + 1], None, ALU.mult, None, [rk, 'c0'], ['ush'])
                STT(ush[:, g, :n], rb[:, g, 0:n], cmu_sb[:, g, 0:1], ush[:, g, :n], ALU.mult, ALU.add, [rk, 'cmu', 'ush'], ['ush'])
                STT(ush[:, g, :n], rb[:, g, 2:2 + n], cmu_sb[:, g, 1:2], ush[:, g, :n], ALU.mult, ALU.add, [rk, 'cmu', 'ush'], ['ush'])
            ACT(sg[:, :n], ush[:, 5, :n], AF.Sigmoid, ['ush'], ['sg'])
            MM(bank[0][:, :n], g2_sb[:], sg[:, :n], ['g2', 'sg'], [('ps', 0)])
            P.op('act', lambda e, n=n: e.copy(out=ush[:, 5, :n], in_=bank[0][:, :n]), r=[('ps', 0), 'ush'], w=['ush'])
            P.dma('pool', uT[:, :, s0 + b0:s0 + b0 + n].rearrange("g p t -> p g t"), ush[:, :, :n], r=['ush'], w=['uTall'])
    P.fence()
    A.release(m0)
    w2_sb = A.alloc("w2", [64, 256])
    a2_sb = A.alloc("a2", [64, 256])
    w0b_sb = A.alloc("w0b", [64, 256])
    a0f_sb = A.alloc("a0f", [64, 4])
    prm_sb = A.alloc("prm", [64, 2, 5])
    cst_sb = A.alloc("cst", [64, 448])
    ones64 = A.alloc("ones64", [64, 64])
    for nm, dst, src in [('w2', w2_sb, w2s), ('a2', a2_sb, a2s), ('w0b', w0b_sb, w0b), ('a0f', a0f_sb, a0f),
                         ('prm', prm_sb, prm), ('cst', cst_sb, cst)]:
        P.dma('sp', dst[:], src, w=[nm])
    P.op('pool', lambda e: e.memset(ones64[:], 1.0), w=['ones64'])
    Tri3 = cst_sb[:, 0:192]
    Mk = cst_sb[:, 192:320]
    MkN = cst_sb[:, 320:384]
    I64 = cst_sb[:, 384:448]
    ST = A.alloc("ST", [64, 4, 64])
    Stmp = A.alloc("Stmp", [64, 4, 64])
    P.op('pool', lambda e: e.memset(ST[:], 0.0), w=['ST'])
    NB = 2
    ld = [dict(r=A.alloc("ld_r%d" % i, [64, 2, 2, 64]), k=A.alloc("ld_k%d" % i, [64, 2, 2, 64]),
               v=A.alloc("ld_v%d" % i, [64, 2, 2, 64]), wl=A.alloc("ld_wl%d" % i, [64, 2, 64]),
               al=A.alloc("ld_al%d" % i, [64, 2, 64])) for i in range(NB)]
    f4 = lambda name: A.alloc(name, [64, 4, 64])
    ur, uk, uv = f4("ur"), f4("uk"), f4("uv")
    uwl = A.alloc("uwl", [64, 2, 64])
    ual = A.alloc("ual", [64, 2, 64])
    tw = A.alloc("tw", [64, 2, 64])
    swt = A.alloc("swt", [64, 256])
    alr = f4("alr")
    eI, eE, eN, eT = f4("eI"), f4("eE"), f4("eN"), f4("eT")
    gC = A.alloc("gC", [64, 4])
    kk, kk2, rn, kkn, bb, km, t1 = f4("kk"), f4("kk2"), f4("rn"), f4("kkn"), f4("bb"), f4("km"), f4("t1")
    AR = A.alloc("AR", [64, 4, 128])
    BT, KTt, BhT, KhT = f4("BT"), f4("KTt"), f4("BhT"), f4("KhT")
    bon = f4("bon")
    TM = A.alloc("TM", [64, 2, 4, 64])
    Vt = f4("Vt")
    Gb = A.alloc("Gb", [64, 4, 128])
    Gk = A.alloc("Gk", [64, 4, 128])
    Nn = f4("Nn")
    Pk = [A.alloc("Pk%d" % i, [64, 2, 4, 64]) for i in range(2)]
    Qk = [f4("Q0"), f4("Q1")]
    Xsb, Usb = f4("Xsb"), f4("Usb")
    Ysb = [f4("Ysb0"), f4("Ysb1")]
    prmb = lambda j: prm_sb[:, :, j:j + 1].unsqueeze(1).broadcast_to([64, 2, 2, 64])
    v4 = lambda t: t[:].rearrange("p (d h) t -> p d h t", d=2)

    nlat = L // 64
    steps = [(s, 3 - s) for s in range(4)] + [(4 + s, 4 + nlat - 1 - s) for s in range(nlat)]

    def issue_loads(si):
        cf, cb = steps[si]
        L_ = ld[si % NB]
        for d, cidx in enumerate((cf, cb)):
            t0 = cidx * 64
            for nm, row in (('r', 0), ('k', 1), ('v', 2)):
                P.dma('sp', L_[nm][:, d, :, :], uT[row, :, t0:t0 + 64].rearrange("(h c) t -> c h t", h=2),
                      r=['uTall'], w=[('ld', si % NB)])
            P.dma('sp', L_['wl'][:, d, :], uT[3, d * 64:(d + 1) * 64, t0:t0 + 64], r=['uTall'], w=[('ld', si % NB)])
            P.dma('sp', L_['al'][:, d, :], uT[4, d * 64:(d + 1) * 64, t0:t0 + 64], r=['uTall'], w=[('ld', si % NB)])

    issue_loads(0)
    for si in range(len(steps)):
        cf, cb = steps[si]
        if si + 1 < len(steps):
            issue_loads(si + 1)
        L_ = ld[si % NB]
        lk = ('ld', si % NB)
        for nm, dst in (('r', ur), ('k', uk), ('v', uv)):
            d4 = v4(dst)
            P.op('pool', lambda e, d4=d4, src=L_[nm]: e.tensor_copy(out=d4[:, 0], in_=src[:, 0]), r=[lk], w=[nm])
            P.op('pool', lambda e, d4=d4, src=L_[nm]: e.tensor_copy(out=d4[:, 1], in_=src[:, 1, :, ::-1]), r=[lk], w=[nm])
        for nm, dst in (('wl', uwl), ('al', ual)):
            P.op('pool', lambda e, dst=dst, src=L_[nm]: e.tensor_copy(out=dst[:, 0, :], in_=src[:, 0, :]), r=[lk], w=[nm])
            P.op('pool', lambda e, dst=dst, src=L_[nm]: e.tensor_copy(out=dst[:, 1, :], in_=src[:, 1, ::-1]), r=[lk], w=[nm])
        ACT(tw[:], uwl[:], AF.Tanh, ['wl'], ['tw'])
        for d in range(2):
            for h in range(2):
                dh = d * 2 + h
                MM(bank[0][0:64, dh * 64:(dh + 1) * 64], tw[:, d, :], w2_sb[:, dh * 64:(dh + 1) * 64], ['tw', 'w2'], [('ps', 0)])
                MM(bank[0][0:64, 256 + dh * 64:256 + (dh + 1) * 64], a2_sb[:, dh * 64:(dh + 1) * 64], ual[:, d, :],
                   ['a2', 'al'], [('ps', 0)])
        TT('dve', swt[:], bank[0][0:64, 0:256], w0b_sb[:], ALU.add, [('ps', 0), 'w0b'], ['swt'])
        ACT(swt[:], swt[:], AF.Sigmoid, ['swt'], ['swt'])
        TT('dve', alr[:], bank[0][0:64, 256:512].rearrange("p (a t) -> p a t", t=64),
           a0f_sb[:].unsqueeze(2).broadcast_to([64, 4, 64]), ALU.add, [('ps', 0), 'a0f'], ['alr'])
        ACT(alr[:], alr[:], AF.Sigmoid, ['alr'], ['alr'])
        for dh in range(4):
            bk = 1 + dh // 2
            MM(bank[bk][0:64, (dh % 2) * 192:(dh % 2) * 192 + 192], swt[:, dh * 64:(dh + 1) * 64], Tri3, ['swt', 'cst'], [('ps', bk)])
        for half in range(2):
            bk = 1 + half
            cv = bank[bk][0:64, 0:384].rearrange("p (a x) -> p a x", x=192)
            sl = slice(half * 2, half * 2 + 2)
            ACT(eI[:, sl, :], cv[:, :, 0:64], AF.Exp, [('ps', bk)], ['eI'], scale=-KAPPA)
            ACT(eE[:, sl, :], cv[:, :, 64:128], AF.Exp, [('ps', bk)], ['eE'], scale=-KAPPA)
            ACT(eN[:, sl, :], cv[:, :, 0:64], AF.Exp, [('ps', bk)], ['eN'], scale=KAPPA)
            ACT(gC[:, sl], cv[:, :, 128], AF.Exp, [('ps', bk)], ['gC'], scale=-KAPPA)
        TT('dve', eT[:], eN[:], gC[:].unsqueeze(2).broadcast_to([64, 4, 64]), ALU.mult, ['eN', 'gC'], ['eT'])
        TT('dve', v4(kk), v4(uk), prmb(0), ALU.mult, ['k', 'prm'], ['kk'])
        TT('pool', kk2[:], kk[:], kk[:], ALU.mult, ['kk'], ['kk2'])
        MM(bank[3][0:64, 0:256], ones64[:], kk2[:].rearrange("p a t -> p (a t)"), ['ones64', 'kk2'], [('ps', 3)])
        ACT(rn[:].rearrange("p a t -> p (a t)"), bank[3][0:64, 0:256], AF.Sqrt, [('ps', 3)], ['rn'])
        TS('dve', rn[:], rn[:], 1e-12, None, ALU.max, None, ['rn'], ['rn'])
        P.op('dve', lambda e: e.reciprocal(out=rn[:], in_=rn[:]), r=['rn'], w=['rn'])
        TT('dve', kkn[:], kk[:], rn[:], ALU.mult, ['kk', 'rn'], ['kkn'])
        TT('pool', bb[:], kkn[:], alr[:], ALU.mult, ['kkn', 'alr'], ['bb'])
        TS('pool', t1[:], alr[:], -1.0, None, ALU.add, None, ['alr'], ['t1'])
        TT('pool', v4(t1), v4(t1), prmb(1), ALU.mult, ['t1', 'prm'], ['t1'])
        STT(km[:], t1[:], 1.0, uk[:], ALU.add, ALU.mult, ['t1', 'k'], ['km'])
        STT(AR[:, :, 0:64], kkn[:], -1.0, eE[:], ALU.mult, ALU.mult, ['kkn', 'eE'], ['AR'])
        TT('pool', AR[:, :, 64:128], ur[:], eI[:], ALU.mult, ['r', 'eI'], ['AR'])
        TT('dve', BT[:], bb[:], eN[:], ALU.mult, ['bb', 'eN'], ['BT'])
        TT('pool', KTt[:], km[:], eN[:], ALU.mult, ['km', 'eN'], ['KTt'])
        TT('dve', BhT[:], bb[:], eT[:], ALU.mult, ['bb', 'eT'], ['BhT'])
        TT('pool', KhT[:], km[:], eT[:], ALU.mult, ['km', 'eT'], ['KhT'])
        TT('pool', t1[:], ur[:], km[:], ALU.mult, ['r', 'km', 't1'], ['t1'])
        TT('pool', v4(t1), v4(t1), prmb(2), ALU.mult, ['t1', 'prm'], ['t1'])
        MM(bank[3][0:64, 256:512], ones64[:], t1[:].rearrange("p a t -> p (a t)"), ['ones64', 't1'], [('ps', 3)])
        TT('dve', bon[:], bank[3][0:64, 256:512].rearrange("p (a t) -> p a t", t=64), uv[:], ALU.mult, [('ps', 3), 'v'], ['bon'])
        P.op('pool', lambda e: e.tensor_copy(out=kk2[:, 2:4, :], in_=bon[:, 2:4, ::-1]), r=['bon', 'kk2'], w=['kk2'])
        P.dma('pool', Bd[0, :, cf * 64:cf * 64 + 64].rearrange("(h c) t -> c h t", h=2), bon[:, 0:2, :], r=['bon'], w=['Bdall'])
        P.dma('pool', Bd[1, :, cb * 64:cb * 64 + 64].rearrange("(h c) t -> c h t", h=2), kk2[:, 2:4, :], r=['kk2'], w=['Bdall'])
        for dh in range(4):
            MM(bank[4][0:64, dh * 64:(dh + 1) * 64], BhT[:, dh, :], I64, ['BhT', 'cst'], [('ps', 4)])
            MM(bank[4][0:64, 256 + dh * 64:256 + (dh + 1) * 64], KhT[:, dh, :], I64, ['KhT', 'cst'], [('ps', 4)])
            MM(bank[5][0:64, dh * 64:(dh + 1) * 64], uv[:, dh, :], I64, ['v', 'cst'], [('ps', 5)])
        P.op('act', lambda e: e.copy(out=TM[:].rearrange("p a b t -> p (a b t)"), in_=bank[4][0:64, :]), r=[('ps', 4)], w=['TM'])
        P.op('dve', lambda e: e.tensor_copy(out=Vt[:].rearrange("p a t -> p (a t)"), in_=bank[5][0:64, 0:256]), r=[('ps', 5)], w=['Vt'])
        for dh in range(4):
            MM(bank[6][0:64, dh * 128:(dh + 1) * 128], BT[:, dh, :], AR[:, dh, :], ['BT', 'AR'], [('ps', 6)])
            MM(bank[7][0:64, dh * 128:(dh + 1) * 128], KTt[:, dh, :], AR[:, dh, :], ['KTt', 'AR'], [('ps', 7)])
            MM(bank[5][0:64, 256 + dh * 64:256 + (dh + 1) * 64], AR[:, dh, 0:64], BT[:, dh, :], ['AR', 'BT'], [('ps', 5)])
        mk4 = Mk.unsqueeze(1).broadcast_to([64, 4, 128])
        TT('dve', Gb[:], bank[6][0:64, :].rearrange("p (a x) -> p a x", x=128), mk4, ALU.mult, [('ps', 6), 'cst'], ['Gb'])
        TT('dve', Gk[:], bank[7][0:64, :].rearrange("p (a x) -> p a x", x=128), mk4, ALU.mult, [('ps', 7), 'cst'], ['Gk'])
        TT('dve', Nn[:], bank[5][0:64, 256:512].rearrange("p (a x) -> p a x", x=64),
           MkN.unsqueeze(1).broadcast_to([64, 4, 64]), ALU.mult, [('ps', 5), 'cst'], ['Nn'])
        TT('pool', Qk[0][:], Gb[:, :, 0:64], I64.unsqueeze(1).broadcast_to([64, 4, 64]), ALU.add, ['Gb', 'cst'], [('Q', 0)])
        pk_prev = (lambda dh: Gb[:, dh, 0:64], lambda dh: Nn[:, dh, :], ['Gb', 'Nn'])
        qi = 0
        for lv in range(1, 6):
            pb = lv % 2
            Pn = Pk[pb]
            pkey = ('Pk', pb)
            bkp = pb
            for dh in range(4):
                if lv < 5:
                    MM(bank[bkp][0:64, dh * 64:(dh + 1) * 64], pk_prev[1](dh), pk_prev[0](dh), pk_prev[2], [('ps', bkp)])
                MM(bank[bkp][0:64, 256 + dh * 64:256 + (dh + 1) * 64], pk_prev[0](dh), pk_prev[1](dh), pk_prev[2], [('ps', bkp)])
            if lv % 2:
                P.op('act', lambda e, Pn=Pn, bkp=bkp: e.copy(out=Pn[:].rearrange("p a b t -> p (a b t)"), in_=bank[bkp][0:64, :]),
                     r=[('ps', bkp)], w=[pkey])
            else:
                P.op('dve', lambda e, Pn=Pn, bkp=bkp: e.tensor_copy(out=Pn[:].rearrange("p a b t -> p (a b t)"), in_=bank[bkp][0:64, :]),
                     r=[('ps', bkp)], w=[pkey])
            pk_prev = (lambda dh, Pn=Pn: Pn[:, 0, dh, :], lambda dh, Pn=Pn: Pn[:, 1, dh, :], [pkey])
            for dh in range(4):
                MM(bank[2][0:64, dh * 64:(dh + 1) * 64], Pn[:, 1, dh, :], Qk[qi][:, dh, :], [pkey, ('Q', qi)], [('ps', 2)])
            TT('dve', Qk[1 - qi][:], bank[2][0:64, 0:256].rearrange("p (a t) -> p a t", t=64), Qk[qi][:], ALU.add,
               [('ps', 2), ('Q', qi)], [('Q', 1 - qi)])
            qi = 1 - qi
        TTm = Qk[qi]
        tkey = ('Q', qi)
        for dh in range(4):
            MM(bank[3][0:64, dh * 64:(dh + 1) * 64], AR[:, dh, 0:64], ST[:, dh, :], ['AR', 'ST'], [('ps', 3)], start=True, stop=False)
            MM(bank[3][0:64, dh * 64:(dh + 1) * 64], Gk[:, dh, 0:64], Vt[:, dh, :], ['Gk', 'Vt'], [('ps', 3)], start=False, stop=True)
        P.op('act', lambda e: e.copy(out=Xsb[:].rearrange("p a t -> p (a t)"), in_=bank[3][0:64, 0:256]), r=[('ps', 3)], w=['Xsb'])
        for dh in range(4):
            MM(bank[3][0:64, 256 + dh * 64:256 + (dh + 1) * 64], TTm[:, dh, :], Xsb[:, dh, :], [tkey, 'Xsb'], [('ps', 3)])
        P.op('act', lambda e: e.copy(out=Usb[:].rearrange("p a t -> p (a t)"), in_=bank[3][0:64, 256:512]), r=[('ps', 3)], w=['Usb'])
        for dh in range(4):
            o = bank[6][0:64, dh * 64:(dh + 1) * 64]
            MM(o, ST[:, dh, :], AR[:, dh, 64:128], ['ST', 'AR'], [('ps', 6)], start=True, stop=False)
            MM(o, Usb[:, dh, :], Gb[:, dh, 64:128], ['Usb', 'Gb'], [('ps', 6)], start=False, stop=False)
            MM(o, Vt[:, dh, :], Gk[:, dh, 64:128], ['Vt', 'Gk'], [('ps', 6)], start=False, stop=True)
        Yb = Ysb[si % 2]
        yk = ('Ysb', si % 2)
        yv = bank[6][0:64, 0:256].rearrange("p (a t) -> p a t", t=64)
        P.op('act', lambda e, Yb=Yb, yv=yv: e.copy(out=Yb[:, 0:2, :], in_=yv[:, 0:2, :]), r=[('ps', 6)], w=[yk])
        P.op('act', lambda e, Yb=Yb, yv=yv: e.copy(out=Yb[:, 2:4, ::-1], in_=yv[:, 2:4, :]), r=[('ps', 6)], w=[yk])
        P.dma('pool', Yd[0, :, cf * 64:cf * 64 + 64].rearrange("(h c) t -> c h t", h=2), Yb[:, 0:2, :], r=[yk], w=['Ydall'])
        P.dma('pool', Yd[1, :, cb * 64:cb * 64 + 64].rearrange("(h c) t -> c h t", h=2), Yb[:, 2:4, :], r=[yk], w=['Ydall'])
        TT('pool', Stmp[:], ST[:], gC[:].unsqueeze(2).broadcast_to([64, 4, 64]), ALU.mult, ['ST', 'gC'], ['Stmp'])
        for dh in range(4):
            o = bank[7][0:64, dh * 64:(dh + 1) * 64]
            MM(o, TM[:, 0, dh, :], Usb[:, dh, :], ['TM', 'Usb'], [('ps', 7)], start=True, stop=False)
            MM(o, TM[:, 1, dh, :], Vt[:, dh, :], ['TM', 'Vt'], [('ps', 7)], start=False, stop=True)
        TT('dve', ST[:], bank[7][0:64, 0:256].rearrange("p (a t) -> p a t", t=64), Stmp[:], ALU.add, [('ps', 7), 'Stmp'], ['ST'])
    P.fence()
    A.release(m0)
    prm2 = A.alloc("prm2", [64, 2, 5])
    on64 = A.alloc("on64", [64, 64])
    eps2 = A.alloc("eps2", [64, 1])
    P.dma('sp', prm2[:], prm, w=['prm2'])
    P.op('pool', lambda e: e.memset(on64[:], 1.0 / 64), w=['on64'])
    P.op('pool', lambda e: e.memset(eps2[:], GN_EPS), w=['eps2'])
    yb = [A.alloc("yb%d" % i, [64, 2, 2, 512]) for i in range(2)]
    bd = [A.alloc("bd%d" % i, [64, 2, 2, 512]) for i in range(2)]
    gg = [A.alloc("gg%d" % i, [64, 2, 512]) for i in range(2)]
    y = A.alloc("y", [64, 2, 512])
    yc = A.alloc("yc", [64, 2, 512])
    y2 = A.alloc("y2", [64, 2, 512])
    rs = A.alloc("rs", [64, 2, 512])
    jobs = []
    if ctx_out:
        jobs.append((0, CTXN, 0))
    o0 = CTXN if ctx_out else 0
    for b0 in range(0, L, 512):
        jobs.append((CTXN + b0, 512, o0 + b0))
    for ji, (t0, n, orow) in enumerate(jobs):
        b = ji % 2
        P.dma('sp', yb[b][:, :, :, :n], Yd[:, :, t0:t0 + n].rearrange("d (h c) t -> c d h t", h=2), r=['Ydall'], w=[('yb', b)])
        P.dma('sp', bd[b][:, :, :, :n], Bd[:, :, t0:t0 + n].rearrange("d (h c) t -> c d h t", h=2), r=['Bdall'], w=[('bd', b)])
        P.dma('sp', gg[b][:, :, :n], uT[5, :, t0:t0 + n].rearrange("(h c) t -> c h t", h=2), r=['uTall'], w=[('gg', b)])
        TT('dve', y[:, :, :n], yb[b][:, 0, :, :n], yb[b][:, 1, :, :n], ALU.add, [('yb', b)], ['y'])
        for h in range(2):
            MM(bank[h][0:64, :n], on64[:], y[:, h, :n], ['on64', 'y'], [('ps', h)])
            TT('dve', yc[:, h, :n], y[:, h, :n], bank[h][0:64, :n], ALU.subtract, ['y', ('ps', h)], ['yc'])
        TT('pool', y2[:, :, :n], yc[:, :, :n], yc[:, :, :n], ALU.mult, ['yc'], ['y2'])
        for h in range(2):
            MM(bank[2 + h][0:64, :n], on64[:], y2[:, h, :n], ['on64', 'y2'], [('ps', 2 + h)])
            ACT(rs[:, h, :n], bank[2 + h][0:64, :n], AF.Sqrt, [('ps', 2 + h), 'eps2'], ['rs'], bias=eps2[:, 0:1])
        P.op('dve', lambda e, n=n: e.reciprocal(out=rs[:, :, :n], in_=rs[:, :, :n]), r=['rs'], w=['rs'])
        TT('dve', yc[:, :, :n], yc[:, :, :n], rs[:, :, :n], ALU.mult, ['yc', 'rs'], ['yc'])
        for h in range(2):
            TS('pool', yc[:, h, :n], yc[:, h, :n], prm2[:, h, 3:4], prm2[:, h, 4:5], ALU.mult, ALU.add, ['yc', 'prm2'], ['yc'])
        TT('dve', yc[:, :, :n], yc[:, :, :n], bd[b][:, 0, :, :n], ALU.add, ['yc', ('bd', b)], ['yc'])
        TT('dve', yc[:, :, :n], yc[:, :, :n], bd[b][:, 1, :, :n], ALU.add, ['yc', ('bd', b)], ['yc'])
        TT('pool', y2[:, :, :n], yc[:, :, :n], gg[b][:, :, :n], ALU.mult, ['yc', ('gg', b), 'y2'], ['y2'])
        P.dma('pool', out_ap(orow, n).rearrange("(h c) t -> c h t", h=2), y2[:, :, :n], r=['y2'], w=['brb'], final=FINAL_OUT)
    P.fence()


NORM_EPS = 1e-6
CTXN = 256


def phase_A(nc, P, A, bank, pfx, L, ctx_out, lam_init, x_loader, brb, do_attn=True, do_pool=True, do_rwkv=True):
    T = CTXN + L
    NQ = T if ctx_out else L
    di = lambda name, shape: nc.dram_tensor(pfx + name, list(shape), F32, kind="ExternalInput").ap()
    if x_loader is None:
        xT = di("xT", [128, 8, T])

        def x_loader(dst, t0, sz, key):
            P.dma('sp', dst[:, :, :sz], xT[:, :, t0:t0 + sz], w=[key])
    identA = di("identA", [128, 128])
    cT = di("cT", [128, 8, 2])
    wada = di("wada", [128, 8, 2048])
    bada = di("bada", [128, 16])
    g1 = di("g1", [128, 8])
    win = di("win", [128, 8, 1280])
    qkg = di("qkg", [128, 2])
    ropeR = di("ropeR", [128, 2, L // 64])
    ropeC = di("ropeC", [128, 2, 64])
    perm = di("perm", [128, 128])
    lamqk = di("lamqk", [128, 256])
    subg = di("subg", [128, 128])
    wpool = di("wpool", [128, 128])
    pscale = di("pscale", [128, 1])
    selw = di("selw", [128, 4])
    efix = di("efix", [128, 16])
    pT = nc.dram_tensor(pfx + "pT_scr", [7, 128, T], F32, kind="Internal").ap()
    base_mark = A.mark()
    if True:
        ones = A.alloc("ones", [128, 128])
        blk = A.alloc("blk", [128, 128])
        eps_sb = A.alloc("eps", [128, 1])
        mod_sb = A.alloc("mod", [128, 16, 2])
        A_sb = A.alloc("A", [128, 8, 2])
        NKT = T // 128
        QT = A.alloc("QT", [128, T], BF16)
        KT = A.alloc("KT", [128, T], BF16)
        V = A.alloc("V", [128, NKT, 130], BF16)
        lam_sb = A.alloc("lam", [128, 1])
        subg_sb = A.alloc("subg", [128, 128])
        ident_sb = A.alloc("identA", [128, 128])
        P.dma('sp', ident_sb[:], identA, w=['identA'])
        P.op('pool', lambda e: e.memset(ones[:], 1.0), w=['ones'])
        P.op('pool', lambda e: e.memset(blk[:], 0.0), w=['blk'])
        P.op('pool', lambda e: e.memset(blk[0:64, 0:64], 1.0), w=['blk'])
        P.op('pool', lambda e: e.memset(blk[64:128, 64:128], 1.0), w=['blk'])
        P.op('pool', lambda e: e.memset(eps_sb[:], NORM_EPS), w=['eps'])
        P.op('pool', lambda e: e.memset(V[:, :, 128:130], 1.0), w=['Vones'])
        m_a1 = A.mark()
        c_sb = A.alloc("c", [128, 8, 2])
        s_sb = A.alloc("s", [128, 8, 2])
        bada_sb = A.alloc("bada", [128, 16])
        g1_sb = A.alloc("g1", [128, 8])
        qkg_sb = A.alloc("qkg", [128, 2])
        perm_sb = A.alloc("perm", [128, 128])
        ropeR_sb = A.alloc("ropeR", [128, 2, L // 64])
        ropeC_sb = A.alloc("ropeC", [128, 2, 64])
        lamqk_sb = A.alloc("lamqk", [128, 256])
        lamt = A.alloc("lamt", [128, 4])
        wbf = A.alloc("wbf", [128, 8, 1280], BF16)
        xt = [A.alloc("xt%d" % i, [128, 8, 512]) for i in range(2)]
        sq = A.alloc("sq", [128, 8, 512])
        rstd = A.alloc("rstd", [128, 512])
        xn = A.alloc("xn", [128, 8, 512], BF16)
        ob = [A.alloc("ob%d" % i, [128, 512]) for i in range(2)]
        qk32 = A.alloc("qk32", [128, 512])
        qksq = A.alloc("qksq", [128, 512])
        qkr = A.alloc("qkr", [128, 512])
        qkn = A.alloc("qkn", [128, 512])
        cs_t = A.alloc("cs_t", [128, 2, 512])
        rtmp = A.alloc("rtmp", [128, 2, 512])

        for nm, dst, src in [('c_sb', c_sb, cT), ('bada', bada_sb, bada), ('g1', g1_sb, g1), ('qkg', qkg_sb, qkg),
                             ('perm', perm_sb, perm), ('ropeR', ropeR_sb, ropeR), ('ropeC', ropeC_sb, ropeC),
                             ('lamqk', lamqk_sb, lamqk), ('subg', subg_sb, subg)]:
            P.dma('sp', dst[:], src, w=[nm])
        lq = lamqk_sb[:].rearrange("p (a b) -> p a b", b=64)
        P.op('dve', lambda e: e.tensor_tensor(out=sq[:, 0, 0:64], in0=lq[:, 0, :], in1=lq[:, 1, :], op=ALU.mult),
             r=['lamqk'], w=['sq'])
        P.op('dve', lambda e: e.tensor_tensor(out=sq[:, 0, 64:128], in0=lq[:, 2, :], in1=lq[:, 3, :], op=ALU.mult),
             r=['lamqk'], w=['sq'])
        P.op('dve', lambda e: e.tensor_reduce(out=lamt[:, 0:2], in_=sq[:, 0, 0:128].rearrange("p (a b) -> p a b", b=64),
                                              axis=AX.X, op=ALU.add), r=['sq'], w=['lamt'])
        P.op('act', lambda e: e.activation(out=lamt[:, 2:4], in_=lamt[:, 0:2], func=AF.Exp), r=['lamt'], w=['lamt2'])
        P.op('dve', lambda e: e.tensor_tensor(out=lam_sb[:], in0=lamt[:, 2:3], in1=lamt[:, 3:4], op=ALU.subtract),
             r=['lamt2'], w=['lam'])
        P.op('dve', lambda e: e.tensor_scalar(out=lam_sb[:], in0=lam_sb[:], scalar1=lam_init, scalar2=-1.0,
                                              op0=ALU.add, op1=ALU.mult), r=['lam'], w=['lam'])
        P.op('act', lambda e: e.activation(out=s_sb[:], in_=c_sb[:], func=AF.Silu), r=['c_sb'], w=['s_sb'])
        ps_mod = bank[0][:, 0:32]
        for piece in range(4):
            b = piece % 2
            P.dma('sp', xt[b][:], wada[:, :, piece * 512:(piece + 1) * 512], w=[('xt', b)])
            for occ in range(4):
                oc = piece * 4 + occ
                for k in range(8):
                    P.op('pe', lambda e, b=b, occ=occ, oc=oc, k=k: e.matmul(
                        ps_mod[:, oc * 2:oc * 2 + 2], lhsT=xt[b][:, k, occ * 128:(occ + 1) * 128],
                        rhs=s_sb[:, k, :], start=(k == 0), stop=(k == 7)),
                        r=[('xt', b), 's_sb'], w=[('ps', 0)])
        P.op('dve', lambda e: e.tensor_tensor(
            out=mod_sb[:], in0=ps_mod.rearrange("p (a b) -> p a b", b=2),
            in1=bada_sb[:].unsqueeze(2).broadcast_to([128, 16, 2]), op=ALU.add),
            r=[('ps', 0), 'bada'], w=['mod'])
        P.op('dve', lambda e: e.tensor_scalar(out=A_sb[:], in0=mod_sb[:, 8:16, :], scalar1=1.0, scalar2=None,
                                              op0=ALU.add), r=['mod'], w=['A'])
        P.op('dve', lambda e: e.tensor_tensor(out=A_sb[:], in0=A_sb[:],
                                              in1=g1_sb[:].unsqueeze(2).broadcast_to([128, 8, 2]), op=ALU.mult),
             r=['A', 'g1'], w=['A'])
        for piece, (c0, csz) in enumerate([(0, 512), (512, 512), (1024, 256)]):
            b = piece % 2
            P.dma('sp', xt[b][:, :, :csz], win[:, :, c0:c0 + csz], w=[('xt', b)])
            P.op('pool', lambda e, b=b, c0=c0, csz=csz: e.tensor_copy(out=wbf[:, :, c0:c0 + csz], in_=xt[b][:, :, :csz]),
                 r=[('xt', b)], w=['wbf'])
        tiles = [(0, CTXN, 1)] + [(CTXN + i * 512, 512, 0) for i in range(L // 512)]
        SCR = {0: 0, 4: 1, 5: 2, 6: 3, 7: 4, 8: 5, 9: 6}
        oi = 0
        for ti, (t0, sz, j) in enumerate(tiles):
            b = ti % 2
            x_loader(xt[b], t0, sz, ('xt', b))
            P.op('act', lambda e, b=b, sz=sz: e.activation(out=sq[:, :, :sz], in_=xt[b][:, :, :sz], func=AF.Square),
                 r=[('xt', b)], w=['sq'])
            for k in range(8):
                P.op('pe', lambda e, k=k, sz=sz: e.matmul(bank[0][:, :sz], lhsT=ones[:], rhs=sq[:, k, :sz],
                                                          start=(k == 0), stop=(k == 7)),
                     r=['ones', 'sq'], w=[('ps', 0)])
            P.op('act', lambda e, sz=sz: e.activation(out=rstd[:, :sz], in_=bank[0][:, :sz], func=AF.Sqrt,
                                                      scale=1.0 / 1024, bias=eps_sb[:, 0:1]),
                 r=[('ps', 0), 'eps'], w=['rstd'])
            P.op('dve', lambda e, sz=sz: e.reciprocal(out=rstd[:, :sz], in_=rstd[:, :sz]), r=['rstd'], w=['rstd'])
            P.op('dve', lambda e, b=b, sz=sz: e.tensor_tensor(
                out=sq[:, :, :sz], in0=xt[b][:, :, :sz],
                in1=rstd[:, :sz].unsqueeze(1).broadcast_to([128, 8, sz]), op=ALU.mult),
                r=[('xt', b), 'rstd', 'sq'], w=['sq'])
            for k in range(8):
                P.op('dve' if k % 2 else 'pool', lambda e, k=k, sz=sz, j=j: e.tensor_scalar(
                    out=xn[:, k, :sz], in0=sq[:, k, :sz], scalar1=A_sb[:, k, j:j + 1],
                    scalar2=mod_sb[:, k, j:j + 1], op0=ALU.mult, op1=ALU.add),
                    r=['sq', 'A', 'mod'], w=['xn'])
            if j == 0:
                r0 = (t0 - CTXN) // 64
                for cs in range(2):
                    P.op('pool', lambda e, cs=cs, r0=r0: e.tensor_tensor(
                        out=cs_t[:, cs, :].rearrange("p (r c) -> p r c", c=64),
                        in0=ropeR_sb[:, cs, r0:r0 + 8].unsqueeze(2).broadcast_to([128, 8, 64]),
                        in1=ropeC_sb[:, cs, :].unsqueeze(1).broadcast_to([128, 8, 64]), op=ALU.mult),
                        r=['ropeR', 'ropeC'], w=['cs_t'])
            for c in range(10):
                if c == 3:
                    for s in range(sz // 128):
                        kt = t0 // 128 + s
                        for k in range(8):
                            P.op('pe', lambda e, k=k, s=s: e.matmul(
                                bank[3][:, 0:128], lhsT=xn[:, k, s * 128:(s + 1) * 128], rhs=wbf[:, k, 384:512],
                                start=(k == 0), stop=(k == 7)), r=['xn', 'wbf'], w=[('ps', 3)])
                        P.op('act', lambda e, kt=kt: e.copy(out=V[:, kt, 0:128], in_=bank[3][:, 0:128]),
                             r=[('ps', 3)], w=['V'])
                    continue
                pb = 1 + (oi % 2)
                oi += 1
                for k in range(8):
                    P.op('pe', lambda e, c=c, k=k, sz=sz, pb=pb: e.matmul(
                        bank[pb][:, :sz], lhsT=wbf[:, k, c * 128:(c + 1) * 128], rhs=xn[:, k, :sz],
                        start=(k == 0), stop=(k == 7)), r=['wbf', 'xn'], w=[('ps', pb)])
                if c in SCR:
                    o = ob[oi % 2]
                    ok = ('ob', oi % 2)
                    if oi % 2:
                        P.op('act', lambda e, o=o, pb=pb, sz=sz: e.copy(out=o[:, :sz], in_=bank[pb][:, :sz]),
                             r=[('ps', pb)], w=[ok])
                    else:
                        P.op('dve', lambda e, o=o, pb=pb, sz=sz: e.tensor_copy(out=o[:, :sz], in_=bank[pb][:, :sz]),
                             r=[('ps', pb)], w=[ok])
                    P.dma('pool', pT[SCR[c], :, t0:t0 + sz], o[:, :sz], r=[ok], w=[('pT', SCR[c], ti)])
                else:
                    dst = QT if c == 1 else KT
                    gi = c - 1
                    P.op('act', lambda e, pb=pb, sz=sz: e.copy(out=qk32[:, :sz], in_=bank[pb][:, :sz]),
                         r=[('ps', pb)], w=['qk32'])
                    P.op('act', lambda e, sz=sz: e.activation(out=qksq[:, :sz], in_=qk32[:, :sz], func=AF.Square),
                         r=['qk32'], w=['qksq'])
                    P.op('pe', lambda e, sz=sz: e.matmul(bank[4][:, :sz], lhsT=blk[:], rhs=qksq[:, :sz],
                                                         start=True, stop=True), r=['blk', 'qksq'], w=[('ps', 4)])
                    P.op('act', lambda e, sz=sz: e.activation(out=qkr[:, :sz], in_=bank[4][:, :sz], func=AF.Sqrt,
                                                              scale=1.0 / 64, bias=eps_sb[:, 0:1]),
                         r=[('ps', 4), 'eps'], w=['qkr'])
                    P.op('dve', lambda e, sz=sz: e.reciprocal(out=qkr[:, :sz], in_=qkr[:, :sz]), r=['qkr'], w=['qkr'])
                    if j == 1:
                        P.op('dve', lambda e, sz=sz, gi=gi, dst=dst, t0=t0: e.scalar_tensor_tensor(
                            out=dst[:, t0:t0 + sz], in0=qk32[:, :sz], scalar=qkg_sb[:, gi:gi + 1], in1=qkr[:, :sz],
                            op0=ALU.mult, op1=ALU.mult), r=['qk32', 'qkg', 'qkr'], w=['QK'])
                    else:
                        P.op('dve', lambda e, sz=sz, gi=gi: e.scalar_tensor_tensor(
                            out=qkn[:, :sz], in0=qk32[:, :sz], scalar=qkg_sb[:, gi:gi + 1], in1=qkr[:, :sz],
                            op0=ALU.mult, op1=ALU.mult), r=['qk32', 'qkg', 'qkr'], w=['qkn'])
                        P.op('pe', lambda e, sz=sz: e.matmul(bank[5][:, :sz], lhsT=perm_sb[:], rhs=qkn[:, :sz],
                                                             start=True, stop=True), r=['perm', 'qkn'], w=[('ps', 5)])
                        P.op('pool', lambda e, sz=sz: e.tensor_tensor(out=rtmp[:, 0, :sz], in0=qkn[:, :sz],
                                                                      in1=cs_t[:, 0, :sz], op=ALU.mult),
                             r=['qkn', 'cs_t'], w=['rtmp0'])
                        P.op('dve', lambda e, sz=sz: e.tensor_tensor(out=rtmp[:, 1, :sz], in0=bank[5][:, :sz],
                                                                     in1=cs_t[:, 1, :sz], op=ALU.mult),
                             r=[('ps', 5), 'cs_t'], w=['rtmp1'])
                        P.op('dve', lambda e, sz=sz, dst=dst, t0=t0: e.tensor_tensor(
                            out=dst[:, t0:t0 + sz], in0=rtmp[:, 0, :sz], in1=rtmp[:, 1, :sz], op=ALU.add),
                            r=['rtmp0', 'rtmp1'], w=['QK'])
        P.fence()
        A.release(m_a1)
        m_ph = A.mark()
        if do_pool:
            wpool_f = A.alloc("wpool_f", [128, 128])
            wpool_b = A.alloc("wpool_b", [128, 128], BF16)
            pscale_sb = A.alloc("pscale", [128, 1])
            selw_sb = A.alloc("selw", [128, 4])
            efix_sb = A.alloc("efix", [128, 16])
            NB = 2048
            U = A.alloc("U", [128, NB + 32])
            W = [A.alloc("W%d" % i, [128, NB + 32]) for i in range(2)]
            comb = A.alloc("comb", [128, NB])
            diffb = A.alloc("diffb", [128, NB], BF16)
            pob = [A.alloc("pob%d" % i, [128, 512]) for i in range(2)]
            P.dma('sp', wpool_f[:], wpool, w=['wpool_f'])
            P.dma('sp', pscale_sb[:], pscale, w=['pscale'])
            P.dma('sp', selw_sb[:], selw, w=['selw'])
            P.dma('sp', efix_sb[:], efix, w=['efix'])
            P.op('dve', lambda e: e.tensor_copy(out=wpool_b[:], in_=wpool_f[:]), r=['wpool_f'], w=['wpool_b'])
            seqs = [(CTXN, L, (0 if not ctx_out else CTXN))]
            if ctx_out:
                seqs.append((0, CTXN, 0))
            pi = 0
            for (s0, slen, o0) in seqs:
                for b0 in range(0, slen, NB):
                    n = min(NB, slen - b0)
                    lo = max(0, b0 - 16)
                    hi = min(slen, b0 + n + 16)
                    P.op('pool', lambda e: e.memset(U[:], 0.0), w=['U'])
                    P.dma('sp', U[:, 16 + lo - b0:16 + hi - b0], pT[0, :, s0 + lo:s0 + hi], r=[('pT', 0, t) for t in range(len(tiles))], w=['U'])
                    NP = n + 32
                    src = U
                    for lv, sh in enumerate([1, 2, 4, 8]):
                        dstw = W[lv % 2]
                        P.op('dve', lambda e, src=src, dstw=dstw, sh=sh, NP=NP: e.tensor_tensor(
                            out=dstw[:, sh:NP], in0=src[:, sh:NP], in1=src[:, 0:NP - sh], op=ALU.add),
                            r=['U', ('W', 0), ('W', 1)], w=[('W', lv % 2)])
                        w_ = 2 * sh
                        off = 16 + w_ // 2 - 1
                        if lv == 0:
                            P.op('pool', lambda e, dstw=dstw, off=off, n=n, lv=lv: e.tensor_scalar(
                                out=comb[:, :n], in0=dstw[:, off:off + n], scalar1=selw_sb[:, lv:lv + 1], scalar2=None,
                                op0=ALU.mult), r=[('W', lv % 2), 'selw'], w=['comb'])
                        else:
                            P.op('dve', lambda e, dstw=dstw, off=off, n=n, lv=lv: e.scalar_tensor_tensor(
                                out=comb[:, :n], in0=dstw[:, off:off + n], scalar=selw_sb[:, lv:lv + 1], in1=comb[:, :n],
                                op0=ALU.mult, op1=ALU.add), r=[('W', lv % 2), 'selw', 'comb'], w=['comb'])
                        src = dstw
                    if b0 == 0:
                        P.op('pool', lambda e: e.tensor_tensor(out=comb[:, 0:8], in0=comb[:, 0:8], in1=efix_sb[:, 0:8],
                                                               op=ALU.mult), r=['comb', 'efix'], w=['comb'])
                    if b0 + n == slen:
                        P.op('pool', lambda e, n=n: e.tensor_tensor(out=comb[:, n - 8:n], in0=comb[:, n - 8:n],
                                                                    in1=efix_sb[:, 8:16], op=ALU.mult),
                             r=['comb', 'efix'], w=['comb'])
                    P.op('dve', lambda e, n=n: e.tensor_tensor(out=diffb[:, :n], in0=comb[:, :n], in1=U[:, 16:16 + n],
                                                               op=ALU.subtract), r=['comb', 'U'], w=['diffb'])
                    for c0 in range(0, n, 512):
                        cs = min(512, n - c0)
                        pb = 6 + pi % 2
                        o = pob[pi % 2]
                        ok = ('pob', pi % 2)
                        pi += 1
                        P.op('pe', lambda e, c0=c0, cs=cs, pb=pb: e.matmul(bank[pb][:, :cs], lhsT=wpool_b[:],
                                                                            rhs=diffb[:, c0:c0 + cs], start=True, stop=True),
                             r=['wpool_b', 'diffb'], w=[('ps', pb)])
                        P.op('act', lambda e, o=o, pb=pb, cs=cs: e.activation(out=o[:, :cs], in_=bank[pb][:, :cs],
                                                                               func=AF.Copy, scale=pscale_sb[:, 0:1]),
                             r=[('ps', pb), 'pscale'], w=[ok])
                        P.dma('pool', brb.dst(0, o0 + b0 + c0, cs), o[:, :cs], r=[ok], w=['brb'])
            P.fence()
            A.release(m_ph)
        if do_attn:
            PT = [[A.alloc("PT%d_%d" % (m, i), [128, 512], BF16) for i in range(2)] for m in range(2)]
            osb = A.alloc("osb", [128, 2, 4, 130])
            rec = A.alloc("rec", [128, 2, 4])
            o1 = A.alloc("o1", [128, 4, 128])
            o2 = A.alloc("o2", [128, 4, 128])
            ssq = A.alloc("ssq", [128, 4])
            oT = A.alloc("oT", [128, 512])
            def oacc(m, qs):
                i = m * 4 + qs
                return bank[4 + i // 3][:, (i % 3) * 130:(i % 3) * 130 + 130], ('ps', 4 + i // 3)
            qjobs = []
            if ctx_out:
                qjobs.append((0, CTXN, (0, 2), 0))
            for i in range(L // 512):
                qjobs.append((CTXN + i * 512, 512, (0, NKT), (CTXN if ctx_out else 0) + i * 512))
            si = 0
            for (q0, nq, (k0, k1), orow) in qjobs:
                nqs = nq // 128
                started = set()
                for kt in range(k0, k1):
                    for m in range(2):
                        sb_ = si % 2
                        pb = m * 2 + sb_
                        P.op('pe', lambda e, m=m, kt=kt, q0=q0, nq=nq, pb=pb: e.matmul(
                            bank[pb][:, :nq], lhsT=KT[m * 64:(m + 1) * 64, kt * 128:(kt + 1) * 128],
                            rhs=QT[m * 64:(m + 1) * 64, q0:q0 + nq], start=True, stop=True),
                            r=['QK'], w=[('ps', pb)])
                        P.op('act', lambda e, m=m, sb_=sb_, pb=pb, nq=nq: e.activation(
                            out=PT[m][sb_][:, :nq], in_=bank[pb][:, :nq], func=AF.Exp, scale=0.125),
                            r=[('ps', pb)], w=[('PT', m, sb_)])
                        for qs in range(nqs):
                            oap, okey = oacc(m, qs)
                            first_in_bank = (kt == k0) and (okey not in started)
                            started.add(okey)
                            P.op('pe', lambda e, m=m, sb_=sb_, qs=qs, kt=kt, oap=oap, fib=first_in_bank, k1=k1: e.matmul(
                                oap, lhsT=PT[m][sb_][:, qs * 128:(qs + 1) * 128], rhs=V[:, kt, :],
                                start=fib, stop=(kt == k1 - 1)),
                                r=[('PT', m, sb_), 'V', 'Vones'], w=[okey])
                    si += 1
                for m in range(2):
                    for qs in range(nqs):
                        oap, okey = oacc(m, qs)
                        P.op('dve' if (m + qs) % 2 else 'act',
                             (lambda e, m=m, qs=qs, oap=oap: e.tensor_copy(out=osb[:, m, qs, :], in_=oap)) if (m + qs) % 2
                             else (lambda e, m=m, qs=qs, oap=oap: e.copy(out=osb[:, m, qs, :], in_=oap)),
                             r=[okey], w=['osb'])
                P.op('dve', lambda e, nqs=nqs: e.reciprocal(out=rec[:, :, :nqs], in_=osb[:, :, :nqs, 128]),
                     r=['osb'], w=['rec'])
                P.op('dve', lambda e, nqs=nqs: e.tensor_scalar(out=rec[:, 1, :nqs], in0=rec[:, 1, :nqs],
                                                               scalar1=lam_sb[:, 0:1], scalar2=None, op0=ALU.mult),
                     r=['rec', 'lam'], w=['rec'])
                P.op('dve', lambda e, nqs=nqs: e.tensor_tensor(
                    out=o1[:, :nqs, :], in0=osb[:, 0, :nqs, 0:128],
                    in1=rec[:, 0, :nqs].unsqueeze(2).broadcast_to([128, nqs, 128]), op=ALU.mult),
                    r=['osb', 'rec'], w=['o1'])
                P.op('pool', lambda e, nqs=nqs: e.tensor_tensor(
                    out=o2[:, :nqs, :], in0=osb[:, 1, :nqs, 0:128],
                    in1=rec[:, 1, :nqs].unsqueeze(2).broadcast_to([128, nqs, 128]), op=ALU.mult),
                    r=['osb', 'rec'], w=['o2'])
                P.op('dve', lambda e, nqs=nqs: e.tensor_tensor(out=o1[:, :nqs, :], in0=o1[:, :nqs, :], in1=o2[:, :nqs, :],
                                                               op=ALU.add), r=['o1', 'o2'], w=['o1'])
                P.op('pool', lambda e, nqs=nqs: e.tensor_tensor(out=o2[:, :nqs, :], in0=o1[:, :nqs, :], in1=o1[:, :nqs, :],
                                                                op=ALU.mult), r=['o1', 'o2'], w=['o2'])
                P.op('dve', lambda e, nqs=nqs: e.tensor_reduce(out=ssq[:, :nqs], in_=o2[:, :nqs, :], axis=AX.X, op=ALU.add),
                     r=['o2'], w=['ssq'])
                P.op('act', lambda e, nqs=nqs: e.activation(out=ssq[:, :nqs], in_=ssq[:, :nqs], func=AF.Sqrt,
                                                            scale=1.0 / 128, bias=eps_sb[:, 0:1]),
                     r=['ssq', 'eps'], w=['ssq'])
                P.op('dve', lambda e, nqs=nqs: e.reciprocal(out=ssq[:, :nqs], in_=ssq[:, :nqs]), r=['ssq'], w=['ssq'])
                P.op('dve', lambda e, nqs=nqs: e.tensor_tensor(
                    out=o1[:, :nqs, :], in0=o1[:, :nqs, :],
                    in1=ssq[:, :nqs].unsqueeze(2).broadcast_to([128, nqs, 128]), op=ALU.mult),
                    r=['o1', 'ssq'], w=['o1'])
                P.op('pool', lambda e, nqs=nqs: e.tensor_tensor(
                    out=o2[:, :nqs, :], in0=o1[:, :nqs, :],
                    in1=subg_sb[:].unsqueeze(1).broadcast_to([128, nqs, 128]), op=ALU.mult),
                    r=['o1', 'subg', 'o2'], w=['o2'])
                for qs in range(nqs):
                    P.op('pe', lambda e, qs=qs: e.matmul(bank[7][:, qs * 128:(qs + 1) * 128], lhsT=o2[:, qs, :], rhs=ident_sb[:],
                                                         start=True, stop=True), r=['o2', 'identA'], w=[('ps', 7)])
                P.op('act', lambda e, nq=nq: e.copy(out=oT[:, :nq], in_=bank[7][:, :nq]), r=[('ps', 7)], w=['oT'])
                P.dma('sp', brb.dst(1, orow, nq), oT[:, :nq], r=['oT'], w=['brb'])
            P.fence()
        if do_rwkv:
            A.release(m_a1)
            rwkv_phase(nc, P, A, bank, pT, L, ctx_out, (lambda c0, n: brb.dst(2, c0, n)), pfx)
        A.release(base_mark)


NORM_EPS = 1e-6


def phase_B(nc, P, A, bank, pfx, NT_LAT, NT_CTX, x_load, gath, off_lat, x_store, NEXP=32):
    NT = NT_LAT + NT_CTX
    di = lambda name, shape: nc.dram_tensor(pfx + name, list(shape), F32, kind="ExternalInput").ap()
    selq = di("selq", [128, 4])
    cT = di("cT", [128, 8, 2])
    wada = di("wada", [128, 8, 6144])
    bada = di("bada", [128, 48])
    g12 = di("g12", [128, 2, 8])
    wgate = di("wgate", [128, 8, 3072])
    wbr = di("wbr", [128, 12, 1024])
    wout = di("wout", [128, 8, 1024])
    wrt = di("wrt", [128, 8, 36])
    wg = di("wg", [NEXP, 128, 8, 512])
    wu = di("wu", [NEXP, 128, 8, 512])
    wd = di("wd", [NEXP, 128, 4, 1024])
    selE = di("selE", [32, 32 * 128])
    ident = di("ident", [128, 128])
    x2s = nc.dram_tensor(pfx + "x2_scr", [128, 8, NT], F32, kind="Internal").ap()
    xn2s = nc.dram_tensor(pfx + "xn2_scr", [128, 8, NT], BF16, kind="Internal").ap()
    gTs = nc.dram_tensor(pfx + "gT_scr", [32, NT], F32, kind="Internal").ap()
    base_mark = A.mark()

    def TT(eng, out, a, b, op, r, w):
        P.op(eng, lambda e: e.tensor_tensor(out=out, in0=a, in1=b, op=op), r=r, w=w)

    def TS(eng, out, a, s1, s2, op0, op1, r, w):
        if op1 is None:
            P.op(eng, lambda e: e.tensor_scalar(out=out, in0=a, scalar1=s1, scalar2=None, op0=op0), r=r, w=w)
        else:
            P.op(eng, lambda e: e.tensor_scalar(out=out, in0=a, scalar1=s1, scalar2=s2, op0=op0, op1=op1), r=r, w=w)

    def STT(out, a, s, b, op0, op1, r, w):
        P.op('dve', lambda e: e.scalar_tensor_tensor(out=out, in0=a, scalar=s, in1=b, op0=op0, op1=op1), r=r, w=w)

    def ACT(out, a, func, r, w, scale=1.0, bias=None):
        if bias is None:
            P.op('act', lambda e: e.activation(out=out, in_=a, func=func, scale=scale), r=r, w=w)
        else:
            P.op('act', lambda e: e.activation(out=out, in_=a, func=func, scale=scale, bias=bias), r=r, w=w)

    def MM(out, lhsT, rhs, r, w, start=True, stop=True):
        P.op('pe', lambda e: e.matmul(out, lhsT=lhsT, rhs=rhs, start=start, stop=stop), r=r, w=w)

    def RED(out, a, op, r, w):
        P.op('dve', lambda e: e.tensor_reduce(out=out, in_=a, axis=AX.X, op=op), r=r, w=w)

    if True:
        ones = A.alloc("ones", [128, 128])
        selq_sb = A.alloc("selq", [128, 4])
        P.dma('sp', selq_sb[:], selq, w=['selq'])
        eps_sb = A.alloc("eps", [128, 1])
        mod_sb = A.alloc("mod", [128, 48, 2])
        A1 = A.alloc("A1", [128, 8, 2])
        A2 = A.alloc("A2", [128, 8, 2])
        g12_sb = A.alloc("g12", [128, 2, 8])
        ident_sb = A.alloc("ident", [128, 128])
        wrt_sb = A.alloc("wrt", [128, 8, 36])
        P.op('pool', lambda e: e.memset(ones[:], 1.0), w=['ones'])
        P.op('pool', lambda e: e.memset(eps_sb[:], NORM_EPS), w=['eps'])
        P.dma('sp', g12_sb[:], g12, w=['g12'])
        P.dma('sp', ident_sb[:], ident, w=['ident'])
        P.dma('sp', wrt_sb[:], wrt, w=['wrt'])
        mB1 = A.mark()
        c_sb = A.alloc("c", [128, 8, 2])
        s_sb = A.alloc("s", [128, 8, 2])
        bada_sb = A.alloc("bada", [128, 48])
        wgate_b = A.alloc("wgate_b", [128, 8, 3072], BF16)
        wbr_b = A.alloc("wbr_b", [128, 12, 1024], BF16)
        wout_b = A.alloc("wout_b", [128, 8, 1024], BF16)
        xt = [A.alloc("xt%d" % i, [128, 8, 512]) for i in range(2)]
        sq = A.alloc("sq", [128, 8, 512])
        rstd = A.alloc("rstd", [128, 512])
        xn1 = A.alloc("xn1", [128, 8, 512], BF16)
        brf = A.alloc("brf", [128, 4, 512])
        gTt = A.alloc("gTt", [32, 512])
        brb = A.alloc("brb", [128, 12, 512], BF16)
        sig = [A.alloc("sig%d" % i, [128, 512]) for i in range(2)]
        mrg = A.alloc("mrg", [128, 2, 512])
        mk_ = A.mark()
        mrgb = A.alloc("mrgb", [128, 8, 512], BF16)
        A.release(mk_)
        brsel = A.alloc("brsel", [128, 4, 512])
        x2 = A.alloc("x2", [128, 8, 512])
        rt = A.alloc("rt", [128, 80])
        g32 = A.alloc("g32", [128, 32])
        P.dma('sp', c_sb[:], cT, w=['c_sb'])
        P.dma('sp', bada_sb[:], bada, w=['bada'])
        ACT(s_sb[:], c_sb[:], AF.Silu, ['c_sb'], ['s_sb'])
        for piece in range(12):
            b = piece % 2
            P.dma('sp', xt[b][:], wada[:, :, piece * 512:(piece + 1) * 512], w=[('xt', b)])
            for occ in range(4):
                oc = piece * 4 + occ
                for k in range(8):
                    MM(bank[0][:, oc * 2:oc * 2 + 2], xt[b][:, k, occ * 128:(occ + 1) * 128], s_sb[:, k, :],
                       [('xt', b), 's_sb'], [('ps', 0)], start=(k == 0), stop=(k == 7))
        TT('dve', mod_sb[:], bank[0][:, 0:96].rearrange("p (a b) -> p a b", b=2),
           bada_sb[:].unsqueeze(2).broadcast_to([128, 48, 2]), ALU.add, [('ps', 0), 'bada'], ['mod'])
        for (Ax, m_scale, gi) in ((A1, 1, 0), (A2, 4, 1)):
            TS('dve', Ax[:], mod_sb[:, m_scale * 8:(m_scale + 1) * 8, :], 1.0, None, ALU.add, None, ['mod'], ['Ax%d' % gi])
            TT('dve', Ax[:], Ax[:], g12_sb[:, gi, :].unsqueeze(2).broadcast_to([128, 8, 2]), ALU.mult, ['Ax%d' % gi, 'g12'], ['Ax%d' % gi])
        wi = 0
        for (src, dstw, nk, ncol, key) in ((wgate, wgate_b, 8, 3072, 'wgate_b'), (wbr, wbr_b, 12, 1024, 'wbr_b'), (wout, wout_b, 8, 1024, 'wout_b')):
            for c0 in range(0, ncol, 512):
                for kb in range(0, nk, 8):
                    kn = min(8, nk - kb)
                    b = wi % 2
                    wi += 1
                    P.dma('sp', xt[b][:, :kn, :], src[:, kb:kb + kn, c0:c0 + 512], w=[('xt', b)])
                    P.op('pool' if wi % 2 else 'dve', lambda e, b=b, kn=kn, kb=kb, c0=c0, dstw=dstw: e.tensor_copy(
                        out=dstw[:, kb:kb + kn, c0:c0 + 512], in_=xt[b][:, :kn, :]), r=[('xt', b)], w=[key])
        tiles = [(i * 512, 512, 0) for i in range(NT_LAT // 512)]
        if NT_CTX:
            tiles.append((NT_LAT, NT_CTX, 1))

        def norm_mod(src, sz, j, Ax, m_shift, out_fn, okeys, srckeys):
            P.op('act', lambda e: e.activation(out=sq[:, :, :sz], in_=src[:, :, :sz], func=AF.Square), r=srckeys, w=['sq'])
            for k in range(8):
                MM(bank[0][:, :sz], ones[:], sq[:, k, :sz], ['ones', 'sq'], [('ps', 0)], start=(k == 0), stop=(k == 7))
            ACT(rstd[:, :sz], bank[0][:, :sz], AF.Sqrt, [('ps', 0), 'eps'], ['rstd'], scale=1.0 / 1024, bias=eps_sb[:, 0:1])
            P.op('dve', lambda e: e.reciprocal(out=rstd[:, :sz], in_=rstd[:, :sz]), r=['rstd'], w=['rstd'])
            TT('dve', sq[:, :, :sz], src[:, :, :sz], rstd[:, :sz].unsqueeze(1).broadcast_to([128, 8, sz]), ALU.mult,
               srckeys + ['rstd', 'sq'], ['sq'])
            for k in range(8):
                TS('dve' if k % 2 else 'pool', out_fn(k), sq[:, k, :sz], Ax[:, k, j:j + 1],
                   mod_sb[:, m_shift * 8 + k, j:j + 1], ALU.mult, ALU.add, ['sq', 'Ax0', 'Ax1', 'mod'], okeys)

        pi = 0
        for ti, (t0, sz, j) in enumerate(tiles):
            b = ti % 2
            x_load(xt[b], t0, sz, ('xt', b))
            for n3 in range(3):
                for q in range(4):
                    col0 = (off_lat + q * NT_LAT + t0) if j == 0 else (q * 64 + (t0 - NT_LAT))
                    P.dma('sp', brf[:, :, :sz], gath.gsrc(n3, col0, sz), r=['gath'], w=['brf'])
                    if q == 0:
                        TS('dve', brsel[:, :, :sz], brf[:, :, :sz], selq_sb[:, 0:1], None, ALU.mult, None, ['brf', 'selq'], ['mrgb'])
                    else:
                        STT(brsel[:, :, :sz], brf[:, :, :sz], selq_sb[:, q:q + 1], brsel[:, :, :sz], ALU.mult, ALU.add,
                            ['brf', 'selq', 'mrgb'], ['mrgb'])
                P.op('pool', lambda e, sz=sz, n3=n3: e.tensor_copy(out=brb[:, n3 * 4:(n3 + 1) * 4, :sz], in_=brsel[:, :, :sz]),
                     r=['mrgb'], w=['brb'])
            norm_mod(xt[b], sz, j, A1, 0, lambda k, sz=sz: xn1[:, k, :sz], ['xn1'], [('xt', b)])
            for dc in range(8):
                for n in range(3):
                    pg = bank[1 + pi % 2]
                    pgk = ('ps', 1 + pi % 2)
                    pbk = bank[3 + pi % 2]
                    pbkk = ('ps', 3 + pi % 2)
                    sg_ = sig[pi % 2]
                    sgk = ('sig', pi % 2)
                    pi += 1
                    for k in range(8):
                        MM(pg[:, :sz], wgate_b[:, k, n * 1024 + dc * 128:n * 1024 + (dc + 1) * 128], xn1[:, k, :sz],
                           ['wgate_b', 'xn1'], [pgk], start=(k == 0), stop=(k == 7))
                    for kc in range(4):
                        MM(pbk[:, :sz], wbr_b[:, n * 4 + kc, dc * 128:(dc + 1) * 128], brb[:, n * 4 + kc, :sz],
                           ['wbr_b', 'brb'], [pbkk], start=(kc == 0), stop=(kc == 3))
                    ACT(sg_[:, :sz], pg[:, :sz], AF.Sigmoid, [pgk], [sgk])
                    if n == 0:
                        TT('dve', mrg[:, dc % 2, :sz], sg_[:, :sz], pbk[:, :sz], ALU.mult, [sgk, pbkk], ['mrg'])
                    else:
                        TT('dve', sg_[:, :sz], sg_[:, :sz], pbk[:, :sz], ALU.mult, [sgk, pbkk], [sgk])
                        TT('pool', mrg[:, dc % 2, :sz], mrg[:, dc % 2, :sz], sg_[:, :sz], ALU.add, ['mrg', sgk], ['mrg'])
                P.op('act', lambda e, dc=dc, sz=sz: e.copy(out=mrgb[:, dc, :sz], in_=mrg[:, dc % 2, :sz]), r=['mrg'], w=['mrgb'])
            for dc in range(8):
                pb_ = bank[5 + dc % 2]
                pk_ = ('ps', 5 + dc % 2)
                for k in range(8):
                    MM(pb_[:, :sz], wout_b[:, k, dc * 128:(dc + 1) * 128], mrgb[:, k, :sz], ['wout_b', 'mrgb'], [pk_],
                       start=(k == 0), stop=(k == 7))
                STT(x2[:, dc, :sz], pb_[:, :sz], mod_sb[:, 2 * 8 + dc, j:j + 1], xt[b][:, dc, :sz], ALU.mult, ALU.add,
                    [pk_, 'mod', ('xt', b)], ['x2'])
            P.dma('pool', x2s[:, :, t0:t0 + sz], x2[:, :, :sz], r=['x2'], w=['x2s'])
            xn2f = xt[b]
            xfk = ('xt', b)
            norm_mod(x2, sz, j, A2, 3, lambda k, sz=sz, xn2f=xn2f: xn2f[:, k, :sz], [xfk], ['x2'])
            P.op('act', lambda e, sz=sz, xn2f=xn2f: e.copy(out=xn1[:, :, :sz], in_=xn2f[:, :, :sz]), r=[xfk], w=['xn1'])
            P.dma('pool', xn2s[:, :, t0:t0 + sz], xn1[:, :, :sz], r=['xn1'], w=['xn2s'])
            for s0 in range(0, sz, 128):
                ns = min(128, sz - s0)
                for k in range(8):
                    MM(bank[7][:ns, 0:36], xn2f[:, k, s0:s0 + ns], wrt_sb[:, k, :], [xfk, 'wrt'], [('ps', 7)],
                       start=(k == 0), stop=(k == 7))
                lg = rt[:ns, 0:36]
                P.op('dve', lambda e, ns=ns: e.tensor_copy(out=rt[:ns, 0:36], in_=bank[7][:ns, 0:36]), r=[('ps', 7)], w=['rt'])
                gmax = rt[:ns, 36:37]
                RED(gmax, rt[:ns, 0:4], ALU.max, ['rt'], ['rt'])
                ohg = rt[:ns, 37:41]
                TS('dve', ohg, rt[:ns, 0:4], gmax, None, ALU.is_equal, None, ['rt'], ['rt'])
                ngm = rt[:ns, 41:42]
                TS('dve', ngm, gmax, -1.0, None, ALU.mult, None, ['rt'], ['rt'])
                eg = rt[:ns, 42:46]
                ACT(eg, rt[:ns, 0:4], AF.Exp, ['rt'], ['rt'], bias=ngm)
                pgr = rt[:ns, 46:47]
                RED(pgr, eg, ALU.add, ['rt'], ['rt'])
                P.op('dve', lambda e, pgr=pgr: e.reciprocal(out=pgr, in_=pgr), r=['rt'], w=['rt'])
                TT('dve', g32[:ns, :].rearrange("p (g e) -> p g e", e=8), rt[:ns, 4:36].rearrange("p (g e) -> p g e", e=8),
                   ohg.unsqueeze(2).broadcast_to([ns, 4, 8]), ALU.mult, ['rt'], ['g32'])
                les = rt[:ns, 47:55]
                RED(les, g32[:ns, :].rearrange("p (g e) -> p e g", e=8), ALU.add, ['g32'], ['rt'])
                top1 = rt[:ns, 55:56]
                RED(top1, les, ALU.max, ['rt'], ['rt'])
                oh1 = rt[:ns, 56:64]
                TS('dve', oh1, les, top1, None, ALU.is_equal, None, ['rt'], ['rt'])
                le2 = rt[:ns, 64:72]
                STT(le2, oh1, -1e30, les, ALU.mult, ALU.add, ['rt'], ['rt'])
                top2 = rt[:ns, 72:73]
                RED(top2, le2, ALU.max, ['rt'], ['rt'])
                oh2 = rt[:ns, 73:81] if False else None
                d12 = rt[:ns, 41:42]
                TT('dve', d12, top1, top2, ALU.subtract, ['rt'], ['rt'])
                ga = rt[:ns, 42:43]
                gb = rt[:ns, 43:44]
                ACT(ga, d12, AF.Sigmoid, ['rt'], ['rt'])
                ACT(gb, d12, AF.Sigmoid, ['rt'], ['rt'], scale=-1.0)
                TT('dve', rt[:ns, 42:44], rt[:ns, 42:44], pgr.broadcast_to([ns, 2]), ALU.mult, ['rt'], ['rt'])
                TS('dve', le2, le2, top2, gb, ALU.is_equal, ALU.mult, ['rt'], ['rt'])
                STT(les, oh1, ga, le2, ALU.mult, ALU.add, ['rt'], ['rt'])
                TT('dve', g32[:ns, :].rearrange("p (g e) -> p g e", e=8), ohg.unsqueeze(2).broadcast_to([ns, 4, 8]),
                   les.unsqueeze(1).broadcast_to([ns, 4, 8]), ALU.mult, ['rt', 'g32'], ['g32'])
                MM(bank[7][0:32, 64:64 + ns], g32[:ns, :], ident_sb[:ns, :ns], ['g32', 'ident'], [('ps', 7)])
                P.op('act', lambda e, s0=s0, ns=ns: e.copy(out=gTt[:, s0:s0 + ns], in_=bank[7][0:32, 64:64 + ns]),
                     r=[('ps', 7)], w=['gTt'])
            P.dma('pool', gTs[:, t0:t0 + sz], gTt[:, :sz], r=['gTt'], w=['gTs'])
        P.fence()
        A.release(mB1)
        selE_sb = A.alloc("selE", [32, 32 * 128])
        P.dma('sp', selE_sb[:], selE, w=['selE'])
        lat_ = tiles[:NT_LAT // 512]
        groups = [lat_[i:i + 2] for i in range(0, len(lat_), 2)]
        if NT_CTX:
            groups[-1] = groups[-1] + [tiles[-1]]
        GMAX = max(sum(t[1] for t in g) for g in groups)
        yacc = A.alloc("yacc", [128, 8, GMAX])
        xn2 = A.alloc("xn2g", [128, 8, GMAX], BF16)
        gT = A.alloc("gTg", [32, GMAX])
        wst = [A.alloc("wst%d" % i, [128, 4, 512]) for i in range(4)]
        wgb = [A.alloc("wgb%d" % i, [128, 8, 512], BF16) for i in range(2)]
        wub = [A.alloc("wub%d" % i, [128, 8, 512], BF16) for i in range(2)]
        wdb = [A.alloc("wdb%d" % i, [128, 4, 1024], BF16) for i in range(2)]
        hs = [A.alloc("hs%d" % i, [128, 512]) for i in range(2)]
        actb = [A.alloc("actb%d" % i, [128, 4, 512], BF16) for i in range(2)]
        x2r = A.alloc("x2r", [128, 8, 512])
        si = 0
        ai = 0
        hi_ = 0
        for gi, grp in enumerate(groups):
            g0 = grp[0][0]
            gsz = sum(t[1] for t in grp)
            P.dma('sp', xn2[:, :, :gsz], xn2s[:, :, g0:g0 + gsz], r=['xn2s'], w=['xn2g'])
            P.dma('sp', gT[:, :gsz], gTs[:, g0:g0 + gsz], r=['gTs'], w=['gTg'])
            for e_ in range(NEXP):
                wb = e_ % 2
                pieces = [(wg[e_, :, 0:4, :], wgb[wb][:, 0:4, :], ('wgb', wb)), (wg[e_, :, 4:8, :], wgb[wb][:, 4:8, :], ('wgb', wb)),
                          (wu[e_, :, 0:4, :], wub[wb][:, 0:4, :], ('wub', wb)), (wu[e_, :, 4:8, :], wub[wb][:, 4:8, :], ('wub', wb)),
                          (wd[e_, :, :, 0:512], wdb[wb][:, :, 0:512], ('wdb', wb)), (wd[e_, :, :, 512:1024], wdb[wb][:, :, 512:1024], ('wdb', wb))]
                for (src, dst, key) in pieces:
                    sb_ = si % 4
                    si += 1
                    P.dma('sp', wst[sb_][:], src, w=[('wst', sb_)])
                    eng = ('pool', 'dve', 'act')[si % 3] if False else ('pool' if si % 2 else 'act')
                    if eng == 'act':
                        P.op('act', lambda e, dst=dst, sb_=sb_: e.copy(out=dst, in_=wst[sb_][:]), r=[('wst', sb_)], w=[key])
                    else:
                        P.op('pool', lambda e, dst=dst, sb_=sb_: e.tensor_copy(out=dst, in_=wst[sb_][:]), r=[('wst', sb_)], w=[key])
                for (t0, sz, j) in grp:
                    ti = tiles.index((t0, sz, j))
                    lo = t0 - g0
                    MM(bank[0][:, :sz], selE_sb[:, e_ * 128:(e_ + 1) * 128], gT[:, lo:lo + sz], ['selE', 'gTg'], [('ps', 0)])
                    ab = actb[ai % 2]
                    ak = ('actb', ai % 2)
                    ai += 1
                    for fc in range(4):
                        pgb = bank[1 + fc % 2]
                        pgk = ('ps', 1 + fc % 2)
                        pub = bank[3 + fc % 2]
                        puk = ('ps', 3 + fc % 2)
                        for k in range(8):
                            MM(pgb[:, :sz], wgb[wb][:, k, fc * 128:(fc + 1) * 128], xn2[:, k, lo:lo + sz],
                               [('wgb', wb), 'xn2g'], [pgk], start=(k == 0), stop=(k == 7))
                        for k in range(8):
                            MM(pub[:, :sz], wub[wb][:, k, fc * 128:(fc + 1) * 128], xn2[:, k, lo:lo + sz],
                               [('wub', wb), 'xn2g'], [puk], start=(k == 0), stop=(k == 7))
                        h_ = hs[hi_ % 2]
                        hk = ('hs', hi_ % 2)
                        hi_ += 1
                        ACT(h_[:, :sz], pgb[:, :sz], AF.Silu, [pgk], [hk])
                        TT('dve', h_[:, :sz], h_[:, :sz], pub[:, :sz], ALU.mult, [hk, puk], [hk])
                        TT('dve', ab[:, fc, :sz], h_[:, :sz], bank[0][:, :sz], ALU.mult, [hk, ('ps', 0)], [ak])
                    for dc in range(8):
                        pdb = bank[5 + dc % 3]
                        pdk = ('ps', 5 + dc % 3)
                        for fc in range(4):
                            MM(pdb[:, :sz], wdb[wb][:, fc, dc * 128:(dc + 1) * 128], ab[:, fc, :sz], [('wdb', wb), ak], [pdk],
                               start=(fc == 0), stop=(fc == 3))
                        if e_ == 0:
                            P.op('act', lambda e, dc=dc, lo=lo, sz=sz, pdb=pdb: e.copy(out=yacc[:, dc, lo:lo + sz], in_=pdb[:, :sz]),
                                 r=[pdk], w=['yacc'])
                        else:
                            TT('pool' if False else 'dve', yacc[:, dc, lo:lo + sz], yacc[:, dc, lo:lo + sz], pdb[:, :sz], ALU.add,
                               ['yacc', pdk], ['yacc'])
            for (t0, sz, j) in grp:
                lo = t0 - g0
                P.dma('sp', x2r[:, :, :sz], x2s[:, :, t0:t0 + sz], r=['x2s'], w=['x2r'])
                for dc in range(8):
                    STT(x2r[:, dc, :sz], yacc[:, dc, lo:lo + sz], mod_sb[:, 5 * 8 + dc, j:j + 1], x2r[:, dc, :sz], ALU.mult, ALU.add,
                        ['yacc', 'mod', 'x2r'], ['x2r'])
                x_store(x2r, t0, sz, ['x2r'])
        P.fence()
        A.release(base_mark)


POOL_WINDOWS = (2, 4, 8, 16)
def fm(a):
    return np.ascontiguousarray(a.reshape(8, 128, *a.shape[1:]).swapaxes(0, 1))
def colsel(hg):
    c = []
    c += list(range(hg * 128, hg * 128 + 128))
    c += list(range(512 + hg * 128, 512 + hg * 128 + 128))
    c += list(range(1024 + hg * 128, 1024 + hg * 128 + 128))
    c += list(range(1536 + hg * 128, 1536 + hg * 128 + 128))
    for j in range(3):
        c += list(range(2048 + j * 512 + hg * 128, 2048 + j * 512 + hg * 128 + 128))
    c += list(range(2048 + 1536, 2048 + 1920))
    return np.array(c)
def rope_tables(L):
    nrow = L // 64
    inv = (10000.0 ** (-np.arange(16, dtype=np.float32) / 16)).astype(np.float32)
    R = np.ones((128, 2, nrow), np.float32); C = np.ones((128, 2, 64), np.float32)
    perm = np.zeros((128, 128), np.float32)
    rows = np.arange(nrow, dtype=np.float32); cols = np.arange(64, dtype=np.float32)
    for p in range(128):
        d = p % 64
        blk = d // 16
        f = inv[d % 16]
        sign = -1.0 if blk % 2 == 0 else 1.0
        partner = p + 16 if blk % 2 == 0 else p - 16
        perm[partner, p] = 1.0
        if blk < 2:
            ang = (rows * f).astype(np.float32)
            R[p, 0] = np.cos(ang); R[p, 1] = sign * np.sin(ang)
        else:
            ang = (cols * f).astype(np.float32)
            C[p, 0] = np.cos(ang); C[p, 1] = sign * np.sin(ang)
    return R, C, perm
def edge_fix(w, L):
    t = np.arange(L)
    lo = np.clip(t - w // 2, 0, L - 1); hi = np.clip(t + w // 2 - 1, 0, L - 1)
    ratio = (w / (hi - lo + 1)).astype(np.float32)
    return np.concatenate([ratio[:8], ratio[-8:]])
def inputs_A(inp, l, b, hg, L, lam_init, x=None, ctx=None):
    if x is None:
        x = inp['x'][b, :L]; ctx = inp['ctx'][b]
    xa = np.concatenate([ctx, x], 0)
    R, C, perm = rope_tables(L)
    cs = colsel(hg)
    w = POOL_WINDOWS[hg]
    selw = np.zeros((128, 4), np.float32); selw[:, hg] = 1.0 / w
    d = dict(
        xT=fm(np.ascontiguousarray(xa.T)),
        cT=fm(np.stack([inp['c'][b], inp['c_ctx']], 1)),
        wada=fm(inp['w_ada'][l][:, :2048]),
        bada=np.ascontiguousarray(inp['b_ada'][l][:2048].reshape(16, 128).T),
        g1=np.ascontiguousarray(inp['norm1_g'][l].reshape(8, 128).T),
        win=fm(inp['w_in'][l][:, cs]),
        qkg=np.stack([np.tile(inp['q_norm_g'][l], 2), np.tile(inp['k_norm_g'][l], 2)], 1).astype(np.float32),
        ropeR=R, ropeC=C, perm=perm,
        lamqk=np.ascontiguousarray(np.broadcast_to(inp['lambda_qk'][l].reshape(1, 256), (128, 256))),
        subg=np.ascontiguousarray(np.broadcast_to((inp['subln_g'][l] * np.float32(1 - lam_init)).reshape(1, 128), (128, 128))).astype(np.float32),
        wpool=np.ascontiguousarray(inp['pool_w'][l][hg]),
        pscale=np.ascontiguousarray(inp['pool_scale'][l][hg * 128:(hg + 1) * 128].reshape(128, 1)),
        selw=selw,
        efix=np.ascontiguousarray(np.broadcast_to(edge_fix(w, L).reshape(1, 16), (128, 16))),
    )
    return d

def rw_consts():
    i = np.arange(64)
    incl = (i[:, None] <= i[None, :]).astype(np.float32)
    strict = (i[:, None] < i[None, :]).astype(np.float32)
    ones = np.ones((64, 64), np.float32)
    MkN = (i[None, :] < i[:, None]).astype(np.float32)
    return np.concatenate([incl, strict, ones, strict, incl, MkN, np.eye(64, dtype=np.float32)], 1)
def inputs_rw(inp, l, hg):
    mu = inp['shift_mu'][l]
    cmu = np.zeros((128, 6, 2), np.float32)
    p = np.arange(128)
    for g in range(3):
        cmu[:, g, :] = mu[:, g * 512 + hg * 128 + p].T
    for g, base in ((3, 1536), (4, 1664), (5, 1792)):
        cmu[:, g, :] = mu[:, base + p].T
    heads = [2 * hg, 2 * hg + 1]
    w2 = np.zeros((64, 2, 2, 64), np.float32); a2 = np.zeros((64, 2, 2, 64), np.float32)
    w0b = np.zeros((2, 2, 64), np.float32); a0f = np.zeros((64, 2, 2), np.float32)
    prm = np.zeros((64, 2, 5), np.float32)
    for h, hd in enumerate(heads):
        cs = slice(hd * 64, hd * 64 + 64)
        for d in range(2):
            w2[:, d, h, :] = inp['decay_w2'][l][d][:, cs]
            a2[:, d, h, :] = inp['aaa_a2'][l][d][:, cs]
            w0b[d, h, :] = inp['decay_w0'][l][d][cs]
            a0f[:, d, h] = inp['aaa_a0'][l][d][cs]
        prm[:, h, 0] = inp['k_k'][l][cs]; prm[:, h, 1] = inp['k_a'][l][cs]; prm[:, h, 2] = inp['r_k'][l][hd]
        prm[:, h, 3] = inp['gn_w'][l][cs]; prm[:, h, 4] = inp['gn_b'][l][cs]
    return dict(rw_cmu=cmu, rw_g2=np.ascontiguousarray(inp['gate_w2'][l][:, hg * 128:(hg + 1) * 128]),
                rw_w2=w2.reshape(64, 256), rw_a2=a2.reshape(64, 256),
                rw_w0b=np.ascontiguousarray(np.broadcast_to(w0b.reshape(1, 256), (64, 256))),
                rw_a0f=a0f.reshape(64, 4), rw_prm=prm, rw_cst=rw_consts())


def weights_B(inp, l):
    selE = np.zeros((32, 32, 128), np.float32)
    for e in range(32): selE[e, e, :] = 1.0
    return dict(
        wada=fm(inp['w_ada'][l]),
        bada=np.ascontiguousarray(inp['b_ada'][l].reshape(48, 128).T),
        g12=np.ascontiguousarray(np.stack([inp['norm1_g'][l].reshape(8, 128).T, inp['norm2_g'][l].reshape(8, 128).T], 1)),
        wgate=fm(inp['w_in'][l][:, 3968:]),
        wbr=np.ascontiguousarray(inp['w_br'][l].reshape(12, 128, 1024).transpose(1, 0, 2)),
        wout=fm(inp['w_out'][l]),
        wrt=fm(np.concatenate([inp['w_router_group'][l], inp['w_router_expert'][l]], 1)),
        wg=np.ascontiguousarray(inp['w_exp_gate'][l].reshape(32, 8, 128, 512).transpose(0, 2, 1, 3)),
        wu=np.ascontiguousarray(inp['w_exp_up'][l].reshape(32, 8, 128, 512).transpose(0, 2, 1, 3)),
        wd=np.ascontiguousarray(inp['w_exp_down'][l].reshape(32, 4, 128, 1024).transpose(0, 2, 1, 3)),
        selE=selE.reshape(32, 4096), ident=np.eye(128, dtype=np.float32))
def acts_B(xa, br, cvec, c_ctx):
    NT = xa.shape[0]
    return dict(xT=fm(np.ascontiguousarray(xa.T)),
                brT=np.ascontiguousarray(br.T.reshape(12, 128, NT).transpose(1, 0, 2)),
                cT=fm(np.stack([cvec, c_ctx], 1)))


GROUPS = [[0, 1, 2, 3], [4, 5, 6, 7]]


CC_COLS = 2048


class BrStore:
    def __init__(self, nc, name, NQ, ctx_out):
        self.splits = ([(0, 256)] if ctx_out else []) + [(c0, min(CC_COLS, NQ - c0)) for c0 in range(256 if ctx_out else 0, NQ, CC_COLS)]
        self.b = {}
        self.g = {}
        for n in range(3):
            for ci, (c0, cs) in enumerate(self.splits):
                self.b[(n, ci)] = nc.dram_tensor("%s_b%d_%d" % (name, n, ci), [128, cs], F32, kind="Internal").ap()
                self.g[(n, ci)] = nc.dram_tensor("%s_g%d_%d" % (name, n, ci), [4 * 128, cs], F32, kind="Internal").ap()

    def _find(self, col0, ncols):
        for ci, (c0, cs) in enumerate(self.splits):
            if c0 <= col0 and col0 + ncols <= c0 + cs:
                return ci, col0 - c0
        raise AssertionError(("chunk straddle", col0, ncols))

    def dst(self, n, col0, ncols):
        ci, o = self._find(col0, ncols)
        return self.b[(n, ci)][:, o:o + ncols]

    def gsrc(self, n, col0, ncols):
        ci, o = self._find(col0, ncols)
        return self.g[(n, ci)].rearrange("(g p) t -> p g t", g=4)[:, :, o:o + ncols]

    def exchange(self, P):
        for key in self.b:
            P.cc(self.b[key], self.g[key], GROUPS, r=['brb'], w=['gath'])


class XStore:
    def __init__(self, nc, name, NL):
        self.NL = NL
        NT0 = NL + 64
        self.splits = [(c0, min(CC_COLS, NL - c0)) for c0 in range(0, NL, CC_COLS)] + [(NL, 64)]
        self.b = {}
        self.g = {}
        for k in range(8):
            for ci, (c0, cs) in enumerate(self.splits):
                self.b[(k, ci)] = nc.dram_tensor("%s_b%d_%d" % (name, k, ci), [128, cs], F32, kind="Internal").ap()
                self.g[(k, ci)] = nc.dram_tensor("%s_g%d_%d" % (name, k, ci), [4 * 128, cs], F32, kind="Internal").ap()

    def _find(self, col0, ncols):
        for ci, (c0, cs) in enumerate(self.splits):
            if c0 <= col0 and col0 + ncols <= c0 + cs:
                return ci, col0 - c0
        raise AssertionError(("chunk straddle", col0, ncols))

    def store(self, P, src_tile, t0, sz, rkeys):
        ci, o = self._find(t0, sz)
        for k in range(8):
            P.dma('pool', self.b[(k, ci)][:, o:o + sz], src_tile[:, k, :sz], r=rkeys, w=['xnew'])

    def load_local(self, P, dst_tile, t0, sz, key):
        ci, o = self._find(t0, sz)
        for k in range(8):
            P.dma('sp', dst_tile[:, k, :sz], self.b[(k, ci)][:, o:o + sz], r=['xnew'], w=[key])

    def load_gathered(self, P, dst_tile, dcol, rank, t0, sz, key):
        ci, o = self._find(t0, sz)
        for k in range(8):
            P.dma('sp', dst_tile[:, k, dcol:dcol + sz], self.g[(k, ci)][rank * 128:(rank + 1) * 128, o:o + sz], r=['xg'], w=[key])

    def exchange(self, P):
        for key in self.b:
            P.cc(self.b[key], self.g[key], GROUPS, r=['xnew'], w=['xg'])


def build_fused(L=16384, nexp=32, stop_after=99):
    nc = bass.Bass("TRN2", target_bir_lowering=False)
    nc.allow_low_precision("bf16 matmul operands, fp32 accumulation")
    P = Prog(nc)
    A = Arena(nc)
    T = 256 + L
    NL = L // 4
    NT0 = NL + 64
    lam = [0.8 - 0.6 * math.exp(-0.3 * l) for l in range(2)]
    with contextlib.ExitStack() as st:
        bank = [st.enter_context(nc.psum_tensor("bank%d" % i, [128, 512], F32)) for i in range(8)]
        xout = nc.dram_tensor("xout", [128, 8, NL], F32, kind="ExternalOutput").ap()

        def finish():
            stats = P.emit()
            stats['sbuf_peak'] = A.peak
            return nc, stats
        br0 = BrStore(nc, "br0", T, True)
        phase_A(nc, P, A, bank, "A0_", L, True, lam[0], None, br0)
        P.fence()
        if stop_after == 1:
            return finish()
        br0.exchange(P)
        P.fence()
        if stop_after == 2:
            return finish()
        xsh = nc.dram_tensor("xsh", [128, 8, NT0], F32, kind="ExternalInput").ap()
        xs = XStore(nc, "xs", NL)

        def x_load0(dst, t0, sz, key):
            P.dma('sp', dst[:, :, :sz], xsh[:, :, t0:t0 + sz], w=[key])
        phase_B(nc, P, A, bank, "B0_", NL, 64, x_load0, br0, 256, lambda src, t0, sz, rk: xs.store(P, src, t0, sz, rk), NEXP=nexp)
        if stop_after == 3:
            return finish()
        xs.exchange(P)
        P.fence()
        if stop_after == 4:
            return finish()

        def x_loader(dst, t0, sz, key):
            if t0 < 256:
                for r in range(4):
                    xs.load_gathered(P, dst, r * 64, r, NL, 64, key)
            else:
                tt = t0 - 256
                xs.load_gathered(P, dst, 0, tt // NL, tt % NL, sz, key)
        br1 = BrStore(nc, "br1", L, False)
        phase_A(nc, P, A, bank, "A1_", L, False, lam[1], x_loader, br1)
        P.fence()
        if stop_after == 5:
            return finish()
        br1.exchange(P)
        P.fence()
        if stop_after == 6:
            return finish()

        def x_store1(src, t0, sz, rk):
            P.dma('pool', xout[:, :, t0:t0 + sz], src[:, :, :sz], r=rk, final=True)
        phase_B(nc, P, A, bank, "B1_", NL, 0, lambda dst, t0, sz, key: xs.load_local(P, dst, t0, sz, key), br1, 0, x_store1, NEXP=nexp)
        return finish()


def fused_inputs(inp, L, nexp=32, names=None):
    inp = {k: np.asarray(v) for k, v in inp.items()}
    NL = L // 4
    x = inp['x'][:, :L]
    ctx = inp['ctx']
    lam = [0.8 - 0.6 * math.exp(-0.3 * l) for l in range(2)]
    WB = [weights_B(inp, l) for l in range(2)]
    ident = np.eye(128, dtype=np.float32)
    maps = []
    for i in range(8):
        b, hg = i // 4, i % 4
        d = {}
        for l in range(2):
            a = inputs_A(inp, l, b, hg, L, lam[l], x=x[b], ctx=ctx[b])
            if l == 1:
                a.pop('xT')
            a.update(inputs_rw(inp, l, hg))
            a['identA'] = ident
            for k, v in a.items():
                d["A%d_" % l + k] = v
            for k, v in WB[l].items():
                d["B%d_" % l + k] = v[:nexp] if k in ('wg', 'wu', 'wd') else v
            d["B%d_cT" % l] = fm(np.stack([inp['c'][b], inp['c_ctx']], 1))
            selq = np.zeros((128, 4), np.float32)
            selq[:, hg] = 1.0
            d["B%d_selq" % l] = selq
        xa = np.concatenate([x[b, hg * NL:(hg + 1) * NL], ctx[b, hg * 64:(hg + 1) * 64]], 0)
        d['xsh'] = fm(np.ascontiguousarray(xa.T))
        if names is not None:
            d = {k: v for k, v in d.items() if k in names}
        maps.append(d)
    return maps


def fused_gather(results, L):
    NL = L // 4
    out = np.empty((2, L, 1024), np.float32)
    for i in range(8):
        b, q = i // 4, i % 4
        o = np.asarray(results[i]['xout']).transpose(1, 0, 2).reshape(1024, NL).T
        out[b, q * NL:(q + 1) * NL] = o
    return out


def kernel(**inp):
    inp = {k: np.asarray(v) for k, v in inp.items()}
    L = inp['x'].shape[1]
    nc, _ = build_fused(L)
    maps = fused_inputs(inp, L)
    res = run_bass_kernel_spmd(nc, maps, core_ids=list(range(8)))
    del maps
    return fused_gather(res.results, L)
```

```python
import math
import contextlib


import numpy as np
import concourse.bass as bass
import concourse.mybir as mybir
from concourse.bass_utils import run_bass_kernel_spmd

F32 = mybir.dt.float32
BF16 = mybir.dt.bfloat16
I32 = mybir.dt.int32
AF = mybir.ActivationFunctionType
ALU = mybir.AluOpType
AX = mybir.AxisListType

SEM_LIMIT = 8000
DMA_POOL = 40


class Prog:
    def __init__(self, nc):
        self.nc = nc
        self.ops = []

    def op(self, eng, fn, r=(), w=()):
        self.ops.append(dict(eng=eng, fn=fn, r=tuple(r), w=tuple(w), dma=False, final=False))

    def dma(self, eng, out, in_, r=(), w=(), final=False, **kw):
        def fn(e, out=out, in_=in_, kw=kw):
            return e.dma_start(out=out, in_=in_, **kw)
        self.ops.append(dict(eng=eng, fn=fn, r=tuple(r), w=tuple(w), dma=True, final=final))

    def cc(self, ins_ap, out_ap, groups, r=(), w=()):
        def fn(e, ins_ap=ins_ap, out_ap=out_ap, groups=groups):
            return e.collective_compute("AllGather", ALU.bypass, replica_groups=groups, ins=[ins_ap], outs=[out_ap])
        self.ops.append(dict(eng='pool', fn=fn, r=tuple(r), w=tuple(w), dma=True, final=False, cc=True))

    def fence(self):
        self.ops.append(dict(eng=None, fn=None, r=(), w=(), dma=False, final=False, fence=True))

    def emit(self):
        nc = self.nc
        raw_ops = self.ops
        ops = []
        fence_after = {}
        fence_pos = []
        for o in raw_ops:
            if o.get('fence'):
                fence_pos.append(len(ops))
            else:
                ops.append(o)
        self.ops = ops
        n = len(ops)
        fence_deps_at = {}
        prev = 0
        for fp in fence_pos:
            last = {}
            dm = set()
            for i in range(prev, fp):
                o = ops[i]
                if o['dma']:
                    dm.add(i)
                else:
                    last[o['eng']] = i
            fence_deps_at[fp] = set(last.values()) | dm
            prev = fp
        last_w = {}
        readers = {}
        deps = [None] * n
        cur_fence = set()
        first_after = {}
        for i, o in enumerate(ops):
            if i in fence_deps_at:
                cur_fence = fence_deps_at[i]
                first_after = {}
            d = {}

            def add(j, raw):
                d[j] = d.get(j, False) or raw
            for k in o['r']:
                if k in last_w:
                    add(last_w[k], True)
            for k in o['w']:
                if k in last_w:
                    add(last_w[k], False)
                for tok, j in readers.get(k, {}).items():
                    add(j, False)
            keep = set()
            for j, raw in d.items():
                if j == i:
                    continue
                oj = ops[j]
                if (not oj['dma']) and (not o['dma']) and oj['eng'] == o['eng']:
                    if o['eng'] == 'pe':
                        continue
                    if not raw:
                        continue
                keep.add(j)
            tok_e = o['eng']
            if cur_fence and tok_e not in first_after:
                first_after[tok_e] = i
                for j in cur_fence:
                    if ops[j]['dma'] or ops[j]['eng'] != tok_e:
                        keep.add(j)
            deps[i] = keep
            for k in o['r']:
                tok = ('d', i) if o['dma'] else o['eng']
                readers.setdefault(k, {})[tok] = i
            for k in o['w']:
                last_w[k] = i
                readers[k] = {}
        needed = [False] * n
        for i in range(n):
            for j in deps[i]:
                needed[j] = True
        engs = ['pe', 'act', 'dve', 'pool', 'sp']
        cnt = {e: 0 for e in engs}
        sig = [None] * n
        dma_uses = [0] * DMA_POOL
        dma_last = [None] * DMA_POOL
        ndma = 0
        semkeys = set()
        for i, o in enumerate(ops):
            if o.get('cc'):
                ncc_ = getattr(self, '_ncc', 0) + 1
                self._ncc = ncc_
                sig[i] = (('cc', 0), ncc_)
                semkeys.add(('cc', 0))
            elif o['dma']:
                j = ndma % DMA_POOL
                ndma += 1
                dma_uses[j] += 1
                if dma_last[j] is not None:
                    deps[i].add(dma_last[j])
                dma_last[j] = i
                sig[i] = (('dma', j), 16 * dma_uses[j])
                semkeys.add(('dma', j))
            elif needed[i]:
                e = o['eng']
                c = cnt[e]
                cnt[e] += 1
                sk = (e, c // SEM_LIMIT)
                sig[i] = (sk, c % SEM_LIMIT + 1)
                semkeys.add(sk)
        finals = [i for i, o in enumerate(ops) if o['final']]
        seen = {e: {} for e in engs}
        streams = {e: [] for e in engs}
        for i, o in enumerate(ops):
            e = o['eng']
            waits = {}
            for j in deps[i]:
                sk, v = sig[j]
                if seen[e].get(sk, 0) >= v:
                    continue
                waits[sk] = max(waits.get(sk, 0), v)
            for sk, v in waits.items():
                seen[e][sk] = v
            streams[e].append((list(waits.items()), o['fn'], sig[i]))
        fw = {}
        for i in finals:
            sk, v = sig[i]
            if seen['sp'].get(sk, 0) >= v:
                continue
            fw[sk] = max(fw.get(sk, 0), v)
        streams['sp'].append((list(fw.items()), None, None))
        self.stats = dict(n_ops=n, cnt=dict(cnt), ndma=ndma,
                          nwaits={e: sum(len(s[0]) for s in streams[e]) for e in engs})
        semkeys = sorted(semkeys, key=str)
        import contextlib
        with contextlib.ExitStack() as st:
            sems = {}
            for sk in semkeys:
                sems[sk] = st.enter_context(nc.semaphore("s_%s_%s" % (sk[0], sk[1])))
            block = st.enter_context(nc.Block())

            def run(engine, items):
                for waits, fn, sg in items:
                    for sk, v in waits:
                        engine.wait_ge(sems[sk], v)
                    if fn is None:
                        continue
                    ins = fn(engine)
                    if sg is not None:
                        inc = 16 if sg[0][0] == 'dma' else 1
                        ins.then_inc(sems[sg[0]], inc)

            @block.tensor
            def _(e):
                run(e, streams['pe'])

            @block.scalar
            def _(e):
                run(e, streams['act'])

            @block.vector
            def _(e):
                run(e, streams['dve'])

            @block.gpsimd
            def _(e):
                run(e, streams['pool'])

            @block.sync
            def _(e):
                run(e, streams['sp'])
        return self.stats


class Arena:
    LO = 16512
    HI = 229376

    def __init__(self, nc):
        self.nc = nc
        self.top = self.LO
        self.n = 0
        self.peak = self.LO

    def alloc(self, name, shape, dt=F32):
        esz = {F32: 4, BF16: 2, I32: 4}[dt]
        nb = esz
        for s_ in shape[1:]:
            nb *= s_
        off = (self.top + 63) // 64 * 64
        assert off + nb <= self.HI, ("SBUF overflow", name, off + nb)
        self.top = off + nb
        self.peak = max(self.peak, self.top)
        self.n += 1
        return self.nc.alloc_sbuf_tensor_at("%s_%d" % (name, self.n), list(shape), dt, offset=off)

    def mark(self):
        return self.top

    def release(self, m):
        self.top = m


GN_EPS = 64e-5
FINAL_OUT = False
KAPPA = 0.6065306597126334
CTXN = 256


def rwkv_phase(nc, P, A, bank, pT, L, ctx_out, out_ap, pfx=''):
    T = CTXN + L
    di = lambda name, shape: nc.dram_tensor(pfx + name, list(shape), F32, kind="ExternalInput").ap()
    cmu = di("rw_cmu", [128, 6, 2])
    g2s = di("rw_g2", [128, 128])
    w2s = di("rw_w2", [64, 256])
    a2s = di("rw_a2", [64, 256])
    w0b = di("rw_w0b", [64, 256])
    a0f = di("rw_a0f", [64, 4])
    prm = di("rw_prm", [64, 2, 5])
    cst = di("rw_cst", [64, 448])
    uT = nc.dram_tensor(pfx + "uT_scr", [6, 128, T], F32, kind="Internal").ap()
    Yd = nc.dram_tensor(pfx + "Yd_scr", [2, 128, T], F32, kind="Internal").ap()
    Bd = nc.dram_tensor(pfx + "Bd_scr", [2, 128, T], F32, kind="Internal").ap()

    def TT(eng, out, a, b, op, r, w):
        P.op(eng, lambda e: e.tensor_tensor(out=out, in0=a, in1=b, op=op), r=r, w=w)

    def TS(eng, out, a, s1, s2, op0, op1, r, w):
        if op1 is None:
            P.op(eng, lambda e: e.tensor_scalar(out=out, in0=a, scalar1=s1, scalar2=None, op0=op0), r=r, w=w)
        else:
            P.op(eng, lambda e: e.tensor_scalar(out=out, in0=a, scalar1=s1, scalar2=s2, op0=op0, op1=op1), r=r, w=w)

    def STT(out, a, s, b, op0, op1, r, w):
        P.op('dve', lambda e: e.scalar_tensor_tensor(out=out, in0=a, scalar=s, in1=b, op0=op0, op1=op1), r=r, w=w)

    def ACT(out, a, func, r, w, scale=1.0, bias=None):
        if bias is None:
            P.op('act', lambda e: e.activation(out=out, in_=a, func=func, scale=scale), r=r, w=w)
        else:
            P.op('act', lambda e: e.activation(out=out, in_=a, func=func, scale=scale, bias=bias), r=r, w=w)

    def MM(out, lhsT, rhs, r, w, start=True, stop=True):
        P.op('pe', lambda e: e.matmul(out, lhsT=lhsT, rhs=rhs, start=start, stop=stop), r=r, w=w)

    m0 = A.mark()
    cmu_sb = A.alloc("cmu", [128, 6, 2])
    c0_sb = A.alloc("c0", [128, 6])
    g2_sb = A.alloc("g2", [128, 128])
    raw = [A.alloc("raw%d" % i, [128, 6, 514]) for i in range(2)]
    ush = A.alloc("ush", [128, 6, 512])
    sg = A.alloc("sg", [128, 512])
    P.dma('sp', cmu_sb[:], cmu, w=['cmu'])
    P.dma('sp', g2_sb[:], g2s, w=['g2'])
    TT('dve', c0_sb[:], cmu_sb[:, :, 0], cmu_sb[:, :, 1], ALU.add, ['cmu'], ['c0'])
    TS('dve', c0_sb[:], c0_sb[:], -1.0, 1.0, ALU.mult, ALU.add, ['c0'], ['c0'])
    seqs = [(0, CTXN), (CTXN, L)]
    ti = 0
    for (s0, slen) in seqs:
        for b0 in range(0, slen, 512):
            n = min(512, slen - b0)
            rb = raw[ti % 2]
            rk = ('raw', ti % 2)
            ti += 1
            lo = max(0, b0 - 1)
            hi = min(slen, b0 + n + 1)
            if b0 == 0 or b0 + n == slen:
                P.op('pool', lambda e, rb=rb: e.memset(rb[:], 0.0), w=[rk])
            P.dma('sp', rb[:, :, 1 + lo - b0:1 + hi - b0], pT[1:7, :, s0 + lo:s0 + hi].rearrange("g p t -> p g t"),
                  r=['pTall'], w=[rk])
            for g in range(6):
                eng = 'dve'
                TS('pool', ush[:, g, :n], rb[:, g, 1:1 + n], c0_sb[:, g:g + 1], None, ALU.mult, None, [rk, 'c0'], ['ush'])
                STT(ush[:, g, :n], rb[:, g, 0:n], cmu_sb[:, g, 0:1], ush[:, g, :n], ALU.mult, ALU.add, [rk, 'cmu', 'ush'], ['ush'])
                STT(ush[:, g, :n], rb[:, g, 2:2 + n], cmu_sb[:, g, 1:2], ush[:, g, :n], ALU.mult, ALU.add, [rk, 'cmu', 'ush'], ['ush'])
            ACT(sg[:, :n], ush[:, 5, :n], AF.Sigmoid, ['ush'], ['sg'])
            MM(bank[0][:, :n], g2_sb[:], sg[:, :n], ['g2', 'sg'], [('ps', 0)])
            P.op('act', lambda e, n=n: e.copy(out=ush[:, 5, :n], in_=bank[0][:, :n]), r=[('ps', 0), 'ush'], w=['ush'])
            P.dma('pool', uT[:, :, s0 + b0:s0 + b0 + n].rearrange("g p t -> p g t"), ush[:, :, :n], r=['ush'], w=['uTall'])
    P.fence()
    A.release(m0)
    w2_sb = A.alloc("w2", [64, 256])
    a2_sb = A.alloc("a2", [64, 256])
    w0b_sb = A.alloc("w0b", [64, 256])
    a0f_sb = A.alloc("a0f", [64, 4])
    prm_sb = A.alloc("prm", [64, 2, 5])
    cst_sb = A.alloc("cst", [64, 448])
    ones64 = A.alloc("ones64", [64, 64])
    for nm, dst, src in [('w2', w2_sb, w2s), ('a2', a2_sb, a2s), ('w0b', w0b_sb, w0b), ('a0f', a0f_sb, a0f),
                         ('prm', prm_sb, prm), ('cst', cst_sb, cst)]:
        P.dma('sp', dst[:], src, w=[nm])
    P.op('pool', lambda e: e.memset(ones64[:], 1.0), w=['ones64'])
    Tri3 = cst_sb[:, 0:192]
    Mk = cst_sb[:, 192:320]
    MkN = cst_sb[:, 320:384]
    I64 = cst_sb[:, 384:448]
    ST = A.alloc("ST", [64, 4, 64])
    Stmp = A.alloc("Stmp", [64, 4, 64])
    P.op('pool', lambda e: e.memset(ST[:], 0.0), w=['ST'])
    KSEG = 4
    NBUF = KSEG + 1
    f4 = lambda name: A.alloc(name, [64, 4, 64])

    shr = dict(L_=dict(r=A.alloc("ld_r", [64, 2, 2, 64]), k=A.alloc("ld_k", [64, 2, 2, 64]), v=A.alloc("ld_v", [64, 2, 2, 64]),
                       wl=A.alloc("ld_wl", [64, 2, 64]), al=A.alloc("ld_al", [64, 2, 64])),
               uwl=A.alloc("uwl", [64, 2, 64]), ual=A.alloc("ual", [64, 2, 64]), tw=A.alloc("tw", [64, 2, 64]),
               swt=A.alloc("swt", [64, 256]), kk=f4("kk"), kk2=f4("kk2"), rn=f4("rn"), kkn=f4("kkn"), bb=f4("bb"), km=f4("km"),
               t1=f4("t1"), BhT=f4("BhT"), KhT=f4("KhT"), Xsb=f4("Xsb"), Usb=f4("Usb"), Ysb=f4("Ysb"))

    def alloc_set(i):
        n_ = lambda x: "%s_%d" % (x, i)
        return (shr['L_'], f4(n_("ur")), f4(n_("uk")), f4(n_("uv")), shr['uwl'], shr['ual'],
                shr['tw'], shr['swt'], f4(n_("alr")),
                f4(n_("eI")), f4(n_("eE")), f4(n_("eN")), f4(n_("eT")), A.alloc(n_("gC"), [64, 4]),
                shr['kk'], shr['kk2'], shr['rn'], shr['kkn'], shr['bb'], shr['km'], shr['t1'],
                A.alloc(n_("AR"), [64, 4, 128]), f4(n_("BT")), f4(n_("KTt")), shr['BhT'], shr['KhT'], f4(n_("bon")),
                A.alloc(n_("TM"), [64, 2, 4, 64]), f4(n_("Vt")), A.alloc(n_("Gb"), [64, 4, 128]), A.alloc(n_("Gk"), [64, 4, 128]),
                f4(n_("Nn")), [A.alloc(n_("Pk%d" % j), [64, 2, 4, 64]) for j in range(2)], [f4(n_("Q0")), f4(n_("Q1"))],
                shr['Xsb'], shr['Usb'], shr['Ysb'])
    sets = [alloc_set(i) for i in range(NBUF)]
    prmb = lambda j: prm_sb[:, :, j:j + 1].unsqueeze(1).broadcast_to([64, 2, 2, 64])
    v4 = lambda t: t[:].rearrange("p (d h) t -> p d h t", d=2)
    SHARED = set(['w2', 'a2', 'w0b', 'a0f', 'prm', 'cst', 'ones64', 'uTall', 'Bdall', 'Ydall', 'ST', 'Stmp',
                  'ld', 'wl', 'al', 'tw', 'swt', 'kk', 'kk2', 'rn', 'kkn', 'bb', 'km', 't1', 'BhT', 'KhT', 'Xsb', 'Usb', 'Ysb'])
    cur = {'b': None}

    def kmap(keys):
        if cur['b'] is None:
            return list(keys)
        return [k if (k in SHARED or (isinstance(k, tuple) and k[0] == 'ps')) else ('rw', k, cur['b']) for k in keys]
    _op, _dma = P.op, P.dma

    def Pop(eng, fn, r=(), w=()):
        _op(eng, fn, r=kmap(r), w=kmap(w))

    def Pdma(eng, out, in_, r=(), w=(), **kw):
        _dma(eng, out, in_, r=kmap(r), w=kmap(w), **kw)

    def TT(eng, out, a, b, op, r, w):
        Pop(eng, lambda e: e.tensor_tensor(out=out, in0=a, in1=b, op=op), r=r, w=w)

    def TS(eng, out, a, s1, s2, op0, op1, r, w):
        if op1 is None:
            Pop(eng, lambda e: e.tensor_scalar(out=out, in0=a, scalar1=s1, scalar2=None, op0=op0), r=r, w=w)
        else:
            Pop(eng, lambda e: e.tensor_scalar(out=out, in0=a, scalar1=s1, scalar2=s2, op0=op0, op1=op1), r=r, w=w)

    def STT(out, a, s, b, op0, op1, r, w):
        Pop('dve', lambda e: e.scalar_tensor_tensor(out=out, in0=a, scalar=s, in1=b, op0=op0, op1=op1), r=r, w=w)

    def ACT(out, a, func, r, w, scale=1.0, bias=None):
        if bias is None:
            Pop('act', lambda e: e.activation(out=out, in_=a, func=func, scale=scale), r=r, w=w)
        else:
            Pop('act', lambda e: e.activation(out=out, in_=a, func=func, scale=scale, bias=bias), r=r, w=w)

    def MM(out, lhsT, rhs, r, w, start=True, stop=True):
        Pop('pe', lambda e: e.matmul(out, lhsT=lhsT, rhs=rhs, start=start, stop=stop), r=r, w=w)

    nlat = L // 64
    steps = [(s, 3 - s) for s in range(4)] + [(4 + s, 4 + nlat - 1 - s) for s in range(nlat)]
    NS = len(steps)

    def gen_step(si):
        cf, cb = steps[si]
        cur['b'] = si % NBUF
        (L_, ur, uk, uv, uwl, ual, tw, swt, alr, eI, eE, eN, eT, gC, kk, kk2, rn, kkn, bb, km, t1, AR, BT, KTt, BhT, KhT, bon,
         TM, Vt, Gb, Gk, Nn, Pk, Qk, Xsb, Usb, Yb) = sets[si % NBUF]
        saved = P.ops
        P.ops = []
        marks = []
        lk = 'ld'
        for d, cidx in enumerate((cf, cb)):
            t0 = cidx * 64
            for nm, row in (('r', 0), ('k', 1), ('v', 2)):
                Pdma('sp', L_[nm][:, d, :, :], uT[row, :, t0:t0 + 64].rearrange("(h c) t -> c h t", h=2), r=['uTall'], w=[lk])
            Pdma('sp', L_['wl'][:, d, :], uT[3, d * 64:(d + 1) * 64, t0:t0 + 64], r=['uTall'], w=[lk])
            Pdma('sp', L_['al'][:, d, :], uT[4, d * 64:(d + 1) * 64, t0:t0 + 64], r=['uTall'], w=[lk])
        for nm, dst in (('r', ur), ('k', uk), ('v', uv)):
            d4 = v4(dst)
            Pop('pool', lambda e, d4=d4, src=L_[nm]: e.tensor_copy(out=d4[:, 0], in_=src[:, 0]), r=[lk], w=[nm])
            Pop('pool', lambda e, d4=d4, src=L_[nm]: e.tensor_copy(out=d4[:, 1], in_=src[:, 1, :, ::-1]), r=[lk], w=[nm])
        for nm, dst in (('wl', uwl), ('al', ual)):
            Pop('pool', lambda e, dst=dst, src=L_[nm]: e.tensor_copy(out=dst[:, 0, :], in_=src[:, 0, :]), r=[lk], w=[nm])
            Pop('pool', lambda e, dst=dst, src=L_[nm]: e.tensor_copy(out=dst[:, 1, :], in_=src[:, 1, ::-1]), r=[lk], w=[nm])
        ACT(tw[:], uwl[:], AF.Tanh, ['wl'], ['tw'])
        for d in range(2):
            for h in range(2):
                dh = d * 2 + h
                MM(bank[0][0:64, dh * 64:(dh + 1) * 64], tw[:, d, :], w2_sb[:, dh * 64:(dh + 1) * 64], ['tw', 'w2'], [('ps', 0)])
                MM(bank[0][0:64, 256 + dh * 64:256 + (dh + 1) * 64], a2_sb[:, dh * 64:(dh + 1) * 64], ual[:, d, :],
                   ['a2', 'al'], [('ps', 0)])
        TT('dve', swt[:], bank[0][0:64, 0:256], w0b_sb[:], ALU.add, [('ps', 0), 'w0b'], ['swt'])
        ACT(swt[:], swt[:], AF.Sigmoid, ['swt'], ['swt'])
        TT('dve', alr[:], bank[0][0:64, 256:512].rearrange("p (a t) -> p a t", t=64),
           a0f_sb[:].unsqueeze(2).broadcast_to([64, 4, 64]), ALU.add, [('ps', 0), 'a0f'], ['alr'])
        ACT(alr[:], alr[:], AF.Sigmoid, ['alr'], ['alr'])
        cbanks = (1, 0)
        for dh in range(4):
            bk = cbanks[dh // 2]
            MM(bank[bk][0:64, (dh % 2) * 192:(dh % 2) * 192 + 192], swt[:, dh * 64:(dh + 1) * 64], Tri3, ['swt', 'cst'], [('ps', bk)])
        for half in range(2):
            bk = cbanks[half]
            cv = bank[bk][0:64, 0:384].rearrange("p (a x) -> p a x", x=192)
            sl = slice(half * 2, half * 2 + 2)
            ACT(eI[:, sl, :], cv[:, :, 0:64], AF.Exp, [('ps', bk)], ['eI'], scale=-KAPPA)
            ACT(eE[:, sl, :], cv[:, :, 64:128], AF.Exp, [('ps', bk)], ['eE'], scale=-KAPPA)
            ACT(eN[:, sl, :], cv[:, :, 0:64], AF.Exp, [('ps', bk)], ['eN'], scale=KAPPA)
            ACT(gC[:, sl], cv[:, :, 128], AF.Exp, [('ps', bk)], ['gC'], scale=-KAPPA)
        TT('dve', eT[:], eN[:], gC[:].unsqueeze(2).broadcast_to([64, 4, 64]), ALU.mult, ['eN', 'gC'], ['eT'])
        marks.append(len(P.ops))
        TT('dve', v4(kk), v4(uk), prmb(0), ALU.mult, ['k', 'prm'], ['kk'])
        TT('pool', kk2[:], kk[:], kk[:], ALU.mult, ['kk'], ['kk2'])
        MM(bank[2][0:64, 0:256], ones64[:], kk2[:].rearrange("p a t -> p (a t)"), ['ones64', 'kk2'], [('ps', 2)])
        ACT(rn[:].rearrange("p a t -> p (a t)"), bank[2][0:64, 0:256], AF.Sqrt, [('ps', 2)], ['rn'])
        TS('dve', rn[:], rn[:], 1e-12, None, ALU.max, None, ['rn'], ['rn'])
        Pop('dve', lambda e: e.reciprocal(out=rn[:], in_=rn[:]), r=['rn'], w=['rn'])
        TT('dve', kkn[:], kk[:], rn[:], ALU.mult, ['kk', 'rn'], ['kkn'])
        TT('pool', bb[:], kkn[:], alr[:], ALU.mult, ['kkn', 'alr'], ['bb'])
        TS('pool', t1[:], alr[:], -1.0, None, ALU.add, None, ['alr'], ['t1'])
        TT('pool', v4(t1), v4(t1), prmb(1), ALU.mult, ['t1', 'prm'], ['t1'])
        STT(km[:], t1[:], 1.0, uk[:], ALU.add, ALU.mult, ['t1', 'k'], ['km'])
        STT(AR[:, :, 0:64], kkn[:], -1.0, eE[:], ALU.mult, ALU.mult, ['kkn', 'eE'], ['AR'])
        TT('pool', AR[:, :, 64:128], ur[:], eI[:], ALU.mult, ['r', 'eI'], ['AR'])
        TT('dve', BT[:], bb[:], eN[:], ALU.mult, ['bb', 'eN'], ['BT'])
        TT('pool', KTt[:], km[:], eN[:], ALU.mult, ['km', 'eN'], ['KTt'])
        TT('dve', BhT[:], bb[:], eT[:], ALU.mult, ['bb', 'eT'], ['BhT'])
        TT('pool', KhT[:], km[:], eT[:], ALU.mult, ['km', 'eT'], ['KhT'])
        TT('pool', t1[:], ur[:], km[:], ALU.mult, ['r', 'km', 't1'], ['t1'])
        TT('pool', v4(t1), v4(t1), prmb(2), ALU.mult, ['t1', 'prm'], ['t1'])
        MM(bank[2][0:64, 256:512], ones64[:], t1[:].rearrange("p a t -> p (a t)"), ['ones64', 't1'], [('ps', 2)])
        TT('dve', bon[:], bank[2][0:64, 256:512].rearrange("p (a t) -> p a t", t=64), uv[:], ALU.mult, [('ps', 2), 'v'], ['bon'])
        Pop('pool', lambda e: e.tensor_copy(out=kk2[:, 2:4, :], in_=bon[:, 2:4, ::-1]), r=['bon', 'kk2'], w=['kk2'])
        Pdma('pool', Bd[0, :, cf * 64:cf * 64 + 64].rearrange("(h c) t -> c h t", h=2), bon[:, 0:2, :], r=['bon'], w=['Bdall'])
        Pdma('pool', Bd[1, :, cb * 64:cb * 64 + 64].rearrange("(h c) t -> c h t", h=2), kk2[:, 2:4, :], r=['kk2'], w=['Bdall'])
        for dh in range(4):
            MM(bank[3][0:64, dh * 64:(dh + 1) * 64], BhT[:, dh, :], I64, ['BhT', 'cst'], [('ps', 3)])
            MM(bank[3][0:64, 256 + dh * 64:256 + (dh + 1) * 64], KhT[:, dh, :], I64, ['KhT', 'cst'], [('ps', 3)])
        Pop('act', lambda e: e.copy(out=TM[:].rearrange("p a b t -> p (a b t)"), in_=bank[3][0:64, :]), r=[('ps', 3)], w=['TM'])
        for dh in range(4):
            MM(bank[2][0:64, dh * 64:(dh + 1) * 64], uv[:, dh, :], I64, ['v', 'cst'], [('ps', 2)])
        Pop('dve', lambda e: e.tensor_copy(out=Vt[:].rearrange("p a t -> p (a t)"), in_=bank[2][0:64, 0:256]), r=[('ps', 2)], w=['Vt'])
        marks.append(len(P.ops))
        for dh in range(4):
            MM(bank[4][0:64, dh * 128:(dh + 1) * 128], BT[:, dh, :], AR[:, dh, :], ['BT', 'AR'], [('ps', 4)])
            MM(bank[5][0:64, dh * 128:(dh + 1) * 128], KTt[:, dh, :], AR[:, dh, :], ['KTt', 'AR'], [('ps', 5)])
        mk4 = Mk.unsqueeze(1).broadcast_to([64, 4, 128])
        TT('dve', Gb[:], bank[4][0:64, :].rearrange("p (a x) -> p a x", x=128), mk4, ALU.mult, [('ps', 4), 'cst'], ['Gb'])
        TT('dve', Gk[:], bank[5][0:64, :].rearrange("p (a x) -> p a x", x=128), mk4, ALU.mult, [('ps', 5), 'cst'], ['Gk'])
        for dh in range(4):
            MM(bank[4][0:64, 256 + dh * 64:256 + (dh + 1) * 64], AR[:, dh, 0:64], BT[:, dh, :], ['AR', 'BT'], [('ps', 4)])
        TT('dve', Nn[:], bank[4][0:64, 256:512].rearrange("p (a x) -> p a x", x=64),
           MkN.unsqueeze(1).broadcast_to([64, 4, 64]), ALU.mult, [('ps', 4), 'cst'], ['Nn'])
        TT('pool', Qk[0][:], Gb[:, :, 0:64], I64.unsqueeze(1).broadcast_to([64, 4, 64]), ALU.add, ['Gb', 'cst'], [('Q', 0)])
        pk_prev = (lambda dh: Gb[:, dh, 0:64], lambda dh: Nn[:, dh, :], ['Gb', 'Nn'])
        qi = 0
        for lv in range(1, 6):
            if lv == 3:
                marks.append(len(P.ops))
            pb = lv % 2
            Pn = Pk[pb]
            pkey = ('Pk', pb)
            bkp, bkq = (5, 4) if lv <= 2 else (6, 6)
            for dh in range(4):
                if lv < 5:
                    MM(bank[bkp][0:64, dh * 64:(dh + 1) * 64], pk_prev[1](dh), pk_prev[0](dh), pk_prev[2], [('ps', bkp)])
                MM(bank[bkp][0:64, 256 + dh * 64:256 + (dh + 1) * 64], pk_prev[0](dh), pk_prev[1](dh), pk_prev[2], [('ps', bkp)])
            if lv % 2:
                Pop('act', lambda e, Pn=Pn, bkp=bkp: e.copy(out=Pn[:].rearrange("p a b t -> p (a b t)"), in_=bank[bkp][0:64, :]),
                    r=[('ps', bkp)], w=[pkey])
            else:
                Pop('dve', lambda e, Pn=Pn, bkp=bkp: e.tensor_copy(out=Pn[:].rearrange("p a b t -> p (a b t)"), in_=bank[bkp][0:64, :]),
                    r=[('ps', bkp)], w=[pkey])
            pk_prev = (lambda dh, Pn=Pn: Pn[:, 0, dh, :], lambda dh, Pn=Pn: Pn[:, 1, dh, :], [pkey])
            for dh in range(4):
                MM(bank[bkq][0:64, dh * 64:(dh + 1) * 64], Pn[:, 1, dh, :], Qk[qi][:, dh, :], [pkey, ('Q', qi)], [('ps', bkq)])
            TT('dve', Qk[1 - qi][:], bank[bkq][0:64, 0:256].rearrange("p (a t) -> p a t", t=64), Qk[qi][:], ALU.add,
               [('ps', bkq), ('Q', qi)], [('Q', 1 - qi)])
            qi = 1 - qi
        TTm = Qk[qi]
        tkey = ('Q', qi)
        marks.append(len(P.ops))
        for dh in range(4):
            MM(bank[7][0:64, dh * 64:(dh + 1) * 64], AR[:, dh, 0:64], ST[:, dh, :], ['AR', 'ST'], [('ps', 7)], start=True, stop=False)
            MM(bank[7][0:64, dh * 64:(dh + 1) * 64], Gk[:, dh, 0:64], Vt[:, dh, :], ['Gk', 'Vt'], [('ps', 7)], start=False, stop=True)
        Pop('act', lambda e: e.copy(out=Xsb[:].rearrange("p a t -> p (a t)"), in_=bank[7][0:64, 0:256]), r=[('ps', 7)], w=['Xsb'])
        for dh in range(4):
            MM(bank[7][0:64, 256 + dh * 64:256 + (dh + 1) * 64], TTm[:, dh, :], Xsb[:, dh, :], [tkey, 'Xsb'], [('ps', 7)])
        Pop('act', lambda e: e.copy(out=Usb[:].rearrange("p a t -> p (a t)"), in_=bank[7][0:64, 256:512]), r=[('ps', 7)], w=['Usb'])
        for dh in range(4):
            o = bank[7][0:64, dh * 64:(dh + 1) * 64]
            MM(o, ST[:, dh, :], AR[:, dh, 64:128], ['ST', 'AR'], [('ps', 7)], start=True, stop=False)
            MM(o, Usb[:, dh, :], Gb[:, dh, 64:128], ['Usb', 'Gb'], [('ps', 7)], start=False, stop=False)
            MM(o, Vt[:, dh, :], Gk[:, dh, 64:128], ['Vt', 'Gk'], [('ps', 7)], start=False, stop=True)
        yv = bank[7][0:64, 0:256].rearrange("p (a t) -> p a t", t=64)
        Pop('act', lambda e, Yb=Yb, yv=yv: e.copy(out=Yb[:, 0:2, :], in_=yv[:, 0:2, :]), r=[('ps', 7)], w=['Ysb'])
        Pop('act', lambda e, Yb=Yb, yv=yv: e.copy(out=Yb[:, 2:4, ::-1], in_=yv[:, 2:4, :]), r=[('ps', 7)], w=['Ysb'])
        Pdma('pool', Yd[0, :, cf * 64:cf * 64 + 64].rearrange("(h c) t -> c h t", h=2), Yb[:, 0:2, :], r=['Ysb'], w=['Ydall'])
        Pdma('pool', Yd[1, :, cb * 64:cb * 64 + 64].rearrange("(h c) t -> c h t", h=2), Yb[:, 2:4, :], r=['Ysb'], w=['Ydall'])
        TT('pool', Stmp[:], ST[:], gC[:].unsqueeze(2).broadcast_to([64, 4, 64]), ALU.mult, ['ST', 'gC'], ['Stmp'])
        for dh in range(4):
            o = bank[7][0:64, 256 + dh * 64:256 + (dh + 1) * 64]
            MM(o, TM[:, 0, dh, :], Usb[:, dh, :], ['TM', 'Usb'], [('ps', 7)], start=True, stop=False)
            MM(o, TM[:, 1, dh, :], Vt[:, dh, :], ['TM', 'Vt'], [('ps', 7)], start=False, stop=True)
        TT('dve', ST[:], bank[7][0:64, 256:512].rearrange("p (a t) -> p a t", t=64), Stmp[:], ALU.add, [('ps', 7), 'Stmp'], ['ST'])
        ops = P.ops
        P.ops = saved
        cur['b'] = None
        bounds = [0] + marks + [len(ops)]
        return [ops[bounds[i]:bounds[i + 1]] for i in range(5)]

    def interleave(lists):
        items = []
        for li, lst in enumerate(lists):
            n_ = len(lst)
            for k_, o in enumerate(lst):
                items.append(((k_ + 0.5) / n_, li, k_, o))
        items.sort(key=lambda t: (t[0], t[1], t[2]))
        return [t[3] for t in items]
    gen = [gen_step(si) for si in range(NS)]
    for tau in range(-KSEG, NS):
        lists = []
        if 0 <= tau < NS:
            lists.append(gen[tau][KSEG])
        for j in range(1, KSEG + 1):
            s_ = tau + j
            if 0 <= s_ < NS:
                lists.append(gen[s_][KSEG - j])
        P.ops.extend(interleave(lists))
    P.fence()
    A.release(m0)
    prm2 = A.alloc("prm2", [64, 2, 5])
    on64 = A.alloc("on64", [64, 64])
    eps2 = A.alloc("eps2", [64, 1])
    P.dma('sp', prm2[:], prm, w=['prm2'])
    P.op('pool', lambda e: e.memset(on64[:], 1.0 / 64), w=['on64'])
    P.op('pool', lambda e: e.memset(eps2[:], GN_EPS), w=['eps2'])
    yb = [A.alloc("yb%d" % i, [64, 2, 2, 512]) for i in range(2)]
    bd = [A.alloc("bd%d" % i, [64, 2, 2, 512]) for i in range(2)]
    gg = [A.alloc("gg%d" % i, [64, 2, 512]) for i in range(2)]
    y = A.alloc("y", [64, 2, 512])
    yc = A.alloc("yc", [64, 2, 512])
    y2 = A.alloc("y2", [64, 2, 512])
    rs = A.alloc("rs", [64, 2, 512])
    jobs = []
    if ctx_out:
        jobs.append((0, CTXN, 0))
    o0 = CTXN if ctx_out else 0
    for b0 in range(0, L, 512):
        jobs.append((CTXN + b0, 512, o0 + b0))
    for ji, (t0, n, orow) in enumerate(jobs):
        b = ji % 2
        P.dma('sp', yb[b][:, :, :, :n], Yd[:, :, t0:t0 + n].rearrange("d (h c) t -> c d h t", h=2), r=['Ydall'], w=[('yb', b)])
        P.dma('sp', bd[b][:, :, :, :n], Bd[:, :, t0:t0 + n].rearrange("d (h c) t -> c d h t", h=2), r=['Bdall'], w=[('bd', b)])
        P.dma('sp', gg[b][:, :, :n], uT[5, :, t0:t0 + n].rearrange("(h c) t -> c h t", h=2), r=['uTall'], w=[('gg', b)])
        TT('dve', y[:, :, :n], yb[b][:, 0, :, :n], yb[b][:, 1, :, :n], ALU.add, [('yb', b)], ['y'])
        for h in range(2):
            MM(bank[h][0:64, :n], on64[:], y[:, h, :n], ['on64', 'y'], [('ps', h)])
            TT('dve', yc[:, h, :n], y[:, h, :n], bank[h][0:64, :n], ALU.subtract, ['y', ('ps', h)], ['yc'])
        TT('pool', y2[:, :, :n], yc[:, :, :n], yc[:, :, :n], ALU.mult, ['yc'], ['y2'])
        for h in range(2):
            MM(bank[2 + h][0:64, :n], on64[:], y2[:, h, :n], ['on64', 'y2'], [('ps', 2 + h)])
            ACT(rs[:, h, :n], bank[2 + h][0:64, :n], AF.Sqrt, [('ps', 2 + h), 'eps2'], ['rs'], bias=eps2[:, 0:1])
        P.op('dve', lambda e, n=n: e.reciprocal(out=rs[:, :, :n], in_=rs[:, :, :n]), r=['rs'], w=['rs'])
        TT('dve', yc[:, :, :n], yc[:, :, :n], rs[:, :, :n], ALU.mult, ['yc', 'rs'], ['yc'])
        for h in range(2):
            TS('pool', yc[:, h, :n], yc[:, h, :n], prm2[:, h, 3:4], prm2[:, h, 4:5], ALU.mult, ALU.add, ['yc', 'prm2'], ['yc'])
        TT('dve', yc[:, :, :n], yc[:, :, :n], bd[b][:, 0, :, :n], ALU.add, ['yc', ('bd', b)], ['yc'])
        TT('dve', yc[:, :, :n], yc[:, :, :n], bd[b][:, 1, :, :n], ALU.add, ['yc', ('bd', b)], ['yc'])
        TT('pool', y2[:, :, :n], yc[:, :, :n], gg[b][:, :, :n], ALU.mult, ['yc', ('gg', b), 'y2'], ['y2'])
        P.dma('pool', out_ap(orow, n).rearrange("(h c) t -> c h t", h=2), y2[:, :, :n], r=['y2'], w=['brb'], final=FINAL_OUT)
    P.fence()


NORM_EPS = 1e-6
CTXN = 256


def phase_A(nc, P, A, bank, pfx, L, ctx_out, lam_init, x_loader, brb, do_attn=True, do_pool=True, do_rwkv=True):
    T = CTXN + L
    NQ = T if ctx_out else L
    di = lambda name, shape: nc.dram_tensor(pfx + name, list(shape), F32, kind="ExternalInput").ap()
    if x_loader is None:
        xT = di("xT", [128, 8, T])

        def x_loader(dst, t0, sz, key):
            P.dma('sp', dst[:, :, :sz], xT[:, :, t0:t0 + sz], w=[key])
    identA = di("identA", [128, 128])
    cT = di("cT", [128, 8, 2])
    wada = di("wada", [128, 8, 2048])
    bada = di("bada", [128, 16])
    g1 = di("g1", [128, 8])
    win = di("win", [128, 8, 1280])
    qkg = di("qkg", [128, 2])
    ropeR = di("ropeR", [128, 2, L // 64])
    ropeC = di("ropeC", [128, 2, 64])
    perm = di("perm", [128, 128])
    lamqk = di("lamqk", [128, 256])
    subg = di("subg", [128, 128])
    wpool = di("wpool", [128, 128])
    pscale = di("pscale", [128, 1])
    selw = di("selw", [128, 4])
    efix = di("efix", [128, 16])
    pT = nc.dram_tensor(pfx + "pT_scr", [7, 128, T], F32, kind="Internal").ap()
    base_mark = A.mark()
    if True:
        ones = A.alloc("ones", [128, 128])
        blk = A.alloc("blk", [128, 128])
        eps_sb = A.alloc("eps", [128, 1])
        mod_sb = A.alloc("mod", [128, 16, 2])
        A_sb = A.alloc("A", [128, 8, 2])
        NKT = T // 128
        QT = A.alloc("QT", [128, T], BF16)
        KT = A.alloc("KT", [128, T], BF16)
        V = A.alloc("V", [128, NKT, 130], BF16)
        lam_sb = A.alloc("lam", [128, 1])
        subg_sb = A.alloc("subg", [128, 128])
        ident_sb = A.alloc("identA", [128, 128])
        P.dma('sp', ident_sb[:], identA, w=['identA'])
        P.op('pool', lambda e: e.memset(ones[:], 1.0), w=['ones'])
        P.op('pool', lambda e: e.memset(blk[:], 0.0), w=['blk'])
        P.op('pool', lambda e: e.memset(blk[0:64, 0:64], 1.0), w=['blk'])
        P.op('pool', lambda e: e.memset(blk[64:128, 64:128], 1.0), w=['blk'])
        P.op('pool', lambda e: e.memset(eps_sb[:], NORM_EPS), w=['eps'])
        P.op('pool', lambda e: e.memset(V[:, :, 128:130], 1.0), w=['Vones'])
        m_a1 = A.mark()
        c_sb = A.alloc("c", [128, 8, 2])
        s_sb = A.alloc("s", [128, 8, 2])
        bada_sb = A.alloc("bada", [128, 16])
        g1_sb = A.alloc("g1", [128, 8])
        qkg_sb = A.alloc("qkg", [128, 2])
        perm_sb = A.alloc("perm", [128, 128])
        ropeR_sb = A.alloc("ropeR", [128, 2, L // 64])
        ropeC_sb = A.alloc("ropeC", [128, 2, 64])
        lamqk_sb = A.alloc("lamqk", [128, 256])
        lamt = A.alloc("lamt", [128, 4])
        wbf = A.alloc("wbf", [128, 8, 1280], BF16)
        xt = [A.alloc("xt%d" % i, [128, 8, 512]) for i in range(2)]
        sq = A.alloc("sq", [128, 8, 512])
        rstd = A.alloc("rstd", [128, 512])
        xn = A.alloc("xn", [128, 8, 512], BF16)
        ob = [A.alloc("ob%d" % i, [128, 512]) for i in range(2)]
        qk32 = A.alloc("qk32", [128, 512])
        qksq = A.alloc("qksq", [128, 512])
        qkr = A.alloc("qkr", [128, 512])
        qkn = A.alloc("qkn", [128, 512])
        cs_t = A.alloc("cs_t", [128, 2, 512])
        rtmp = A.alloc("rtmp", [128, 2, 512])

        for nm, dst, src in [('c_sb', c_sb, cT), ('bada', bada_sb, bada), ('g1', g1_sb, g1), ('qkg', qkg_sb, qkg),
                             ('perm', perm_sb, perm), ('ropeR', ropeR_sb, ropeR), ('ropeC', ropeC_sb, ropeC),
                             ('lamqk', lamqk_sb, lamqk), ('subg', subg_sb, subg)]:
            P.dma('sp', dst[:], src, w=[nm])
        lq = lamqk_sb[:].rearrange("p (a b) -> p a b", b=64)
        P.op('dve', lambda e: e.tensor_tensor(out=sq[:, 0, 0:64], in0=lq[:, 0, :], in1=lq[:, 1, :], op=ALU.mult),
             r=['lamqk'], w=['sq'])
        P.op('dve', lambda e: e.tensor_tensor(out=sq[:, 0, 64:128], in0=lq[:, 2, :], in1=lq[:, 3, :], op=ALU.mult),
             r=['lamqk'], w=['sq'])
        P.op('dve', lambda e: e.tensor_reduce(out=lamt[:, 0:2], in_=sq[:, 0, 0:128].rearrange("p (a b) -> p a b", b=64),
                                              axis=AX.X, op=ALU.add), r=['sq'], w=['lamt'])
        P.op('act', lambda e: e.activation(out=lamt[:, 2:4], in_=lamt[:, 0:2], func=AF.Exp), r=['lamt'], w=['lamt2'])
        P.op('dve', lambda e: e.tensor_tensor(out=lam_sb[:], in0=lamt[:, 2:3], in1=lamt[:, 3:4], op=ALU.subtract),
             r=['lamt2'], w=['lam'])
        P.op('dve', lambda e: e.tensor_scalar(out=lam_sb[:], in0=lam_sb[:], scalar1=lam_init, scalar2=-1.0,
                                              op0=ALU.add, op1=ALU.mult), r=['lam'], w=['lam'])
        P.op('act', lambda e: e.activation(out=s_sb[:], in_=c_sb[:], func=AF.Silu), r=['c_sb'], w=['s_sb'])
        ps_mod = bank[0][:, 0:32]
        for piece in range(4):
            b = piece % 2
            P.dma('sp', xt[b][:], wada[:, :, piece * 512:(piece + 1) * 512], w=[('xt', b)])
            for occ in range(4):
                oc = piece * 4 + occ
                for k in range(8):
                    P.op('pe', lambda e, b=b, occ=occ, oc=oc, k=k: e.matmul(
                        ps_mod[:, oc * 2:oc * 2 + 2], lhsT=xt[b][:, k, occ * 128:(occ + 1) * 128],
                        rhs=s_sb[:, k, :], start=(k == 0), stop=(k == 7)),
                        r=[('xt', b), 's_sb'], w=[('ps', 0)])
        P.op('dve', lambda e: e.tensor_tensor(
            out=mod_sb[:], in0=ps_mod.rearrange("p (a b) -> p a b", b=2),
            in1=bada_sb[:].unsqueeze(2).broadcast_to([128, 16, 2]), op=ALU.add),
            r=[('ps', 0), 'bada'], w=['mod'])
        P.op('dve', lambda e: e.tensor_scalar(out=A_sb[:], in0=mod_sb[:, 8:16, :], scalar1=1.0, scalar2=None,
                                              op0=ALU.add), r=['mod'], w=['A'])
        P.op('dve', lambda e: e.tensor_tensor(out=A_sb[:], in0=A_sb[:],
                                              in1=g1_sb[:].unsqueeze(2).broadcast_to([128, 8, 2]), op=ALU.mult),
             r=['A', 'g1'], w=['A'])
        for piece, (c0, csz) in enumerate([(0, 512), (512, 512), (1024, 256)]):
            b = piece % 2
            P.dma('sp', xt[b][:, :, :csz], win[:, :, c0:c0 + csz], w=[('xt', b)])
            P.op('pool', lambda e, b=b, c0=c0, csz=csz: e.tensor_copy(out=wbf[:, :, c0:c0 + csz], in_=xt[b][:, :, :csz]),
                 r=[('xt', b)], w=['wbf'])
        tiles = [(0, CTXN, 1)] + [(CTXN + i * 512, 512, 0) for i in range(L // 512)]
        SCR = {0: 0, 4: 1, 5: 2, 6: 3, 7: 4, 8: 5, 9: 6}
        oi = 0
        for ti, (t0, sz, j) in enumerate(tiles):
            b = ti % 2
            x_loader(xt[b], t0, sz, ('xt', b))
            P.op('act', lambda e, b=b, sz=sz: e.activation(out=sq[:, :, :sz], in_=xt[b][:, :, :sz], func=AF.Square),
                 r=[('xt', b)], w=['sq'])
            for k in range(8):
                P.op('pe', lambda e, k=k, sz=sz: e.matmul(bank[0][:, :sz], lhsT=ones[:], rhs=sq[:, k, :sz],
                                                          start=(k == 0), stop=(k == 7)),
                     r=['ones', 'sq'], w=[('ps', 0)])
            P.op('act', lambda e, sz=sz: e.activation(out=rstd[:, :sz], in_=bank[0][:, :sz], func=AF.Sqrt,
                                                      scale=1.0 / 1024, bias=eps_sb[:, 0:1]),
                 r=[('ps', 0), 'eps'], w=['rstd'])
            P.op('dve', lambda e, sz=sz: e.reciprocal(out=rstd[:, :sz], in_=rstd[:, :sz]), r=['rstd'], w=['rstd'])
            P.op('dve', lambda e, b=b, sz=sz: e.tensor_tensor(
                out=sq[:, :, :sz], in0=xt[b][:, :, :sz],
                in1=rstd[:, :sz].unsqueeze(1).broadcast_to([128, 8, sz]), op=ALU.mult),
                r=[('xt', b), 'rstd', 'sq'], w=['sq'])
            for k in range(8):
                P.op('dve' if k % 2 else 'pool', lambda e, k=k, sz=sz, j=j: e.tensor_scalar(
                    out=xn[:, k, :sz], in0=sq[:, k, :sz], scalar1=A_sb[:, k, j:j + 1],
                    scalar2=mod_sb[:, k, j:j + 1], op0=ALU.mult, op1=ALU.add),
                    r=['sq', 'A', 'mod'], w=['xn'])
            if j == 0:
                r0 = (t0 - CTXN) // 64
                for cs in range(2):
                    P.op('pool', lambda e, cs=cs, r0=r0: e.tensor_tensor(
                        out=cs_t[:, cs, :].rearrange("p (r c) -> p r c", c=64),
                        in0=ropeR_sb[:, cs, r0:r0 + 8].unsqueeze(2).broadcast_to([128, 8, 64]),
                        in1=ropeC_sb[:, cs, :].unsqueeze(1).broadcast_to([128, 8, 64]), op=ALU.mult),
                        r=['ropeR', 'ropeC'], w=['cs_t'])
            for c in range(10):
                if c == 3:
                    for s in range(sz // 128):
                        kt = t0 // 128 + s
                        for k in range(8):
                            P.op('pe', lambda e, k=k, s=s: e.matmul(
                                bank[3][:, 0:128], lhsT=xn[:, k, s * 128:(s + 1) * 128], rhs=wbf[:, k, 384:512],
                                start=(k == 0), stop=(k == 7)), r=['xn', 'wbf'], w=[('ps', 3)])
                        P.op('act', lambda e, kt=kt: e.copy(out=V[:, kt, 0:128], in_=bank[3][:, 0:128]),
                             r=[('ps', 3)], w=['V'])
                    continue
                pb = 1 + (oi % 2)
                oi += 1
                for k in range(8):
                    P.op('pe', lambda e, c=c, k=k, sz=sz, pb=pb: e.matmul(
                        bank[pb][:, :sz], lhsT=wbf[:, k, c * 128:(c + 1) * 128], rhs=xn[:, k, :sz],
                        start=(k == 0), stop=(k == 7)), r=['wbf', 'xn'], w=[('ps', pb)])
                if c in SCR:
                    o = ob[oi % 2]
                    ok = ('ob', oi % 2)
                    if oi % 2:
                        P.op('act', lambda e, o=o, pb=pb, sz=sz: e.copy(out=o[:, :sz], in_=bank[pb][:, :sz]),
                             r=[('ps', pb)], w=[ok])
                    else:
                        P.op('dve', lambda e, o=o, pb=pb, sz=sz: e.tensor_copy(out=o[:, :sz], in_=bank[pb][:, :sz]),
                             r=[('ps', pb)], w=[ok])
                    P.dma('pool', pT[SCR[c], :, t0:t0 + sz], o[:, :sz], r=[ok], w=[('pT', SCR[c], ti)])
                else:
                    dst = QT if c == 1 else KT
                    gi = c - 1
                    P.op('act', lambda e, pb=pb, sz=sz: e.copy(out=qk32[:, :sz], in_=bank[pb][:, :sz]),
                         r=[('ps', pb)], w=['qk32'])
                    P.op('act', lambda e, sz=sz: e.activation(out=qksq[:, :sz], in_=qk32[:, :sz], func=AF.Square),
                         r=['qk32'], w=['qksq'])
                    P.op('pe', lambda e, sz=sz: e.matmul(bank[4][:, :sz], lhsT=blk[:], rhs=qksq[:, :sz],
                                                         start=True, stop=True), r=['blk', 'qksq'], w=[('ps', 4)])
                    P.op('act', lambda e, sz=sz: e.activation(out=qkr[:, :sz], in_=bank[4][:, :sz], func=AF.Sqrt,
                                                              scale=1.0 / 64, bias=eps_sb[:, 0:1]),
                         r=[('ps', 4), 'eps'], w=['qkr'])
                    P.op('dve', lambda e, sz=sz: e.reciprocal(out=qkr[:, :sz], in_=qkr[:, :sz]), r=['qkr'], w=['qkr'])
                    if j == 1:
                        P.op('dve', lambda e, sz=sz, gi=gi, dst=dst, t0=t0: e.scalar_tensor_tensor(
                            out=dst[:, t0:t0 + sz], in0=qk32[:, :sz], scalar=qkg_sb[:, gi:gi + 1], in1=qkr[:, :sz],
                            op0=ALU.mult, op1=ALU.mult), r=['qk32', 'qkg', 'qkr'], w=['QK'])
                    else:
                        P.op('dve', lambda e, sz=sz, gi=gi: e.scalar_tensor_tensor(
                            out=qkn[:, :sz], in0=qk32[:, :sz], scalar=qkg_sb[:, gi:gi + 1], in1=qkr[:, :sz],
                            op0=ALU.mult, op1=ALU.mult), r=['qk32', 'qkg', 'qkr'], w=['qkn'])
                        P.op('pe', lambda e, sz=sz: e.matmul(bank[5][:, :sz], lhsT=perm_sb[:], rhs=qkn[:, :sz],
                                                             start=True, stop=True), r=['perm', 'qkn'], w=[('ps', 5)])
                        P.op('pool', lambda e, sz=sz: e.tensor_tensor(out=rtmp[:, 0, :sz], in0=qkn[:, :sz],
                                                                      in1=cs_t[:, 0, :sz], op=ALU.mult),
                             r=['qkn', 'cs_t'], w=['rtmp0'])
                        P.op('dve', lambda e, sz=sz: e.tensor_tensor(out=rtmp[:, 1, :sz], in0=bank[5][:, :sz],
                                                                     in1=cs_t[:, 1, :sz], op=ALU.mult),
                             r=[('ps', 5), 'cs_t'], w=['rtmp1'])
                        P.op('dve', lambda e, sz=sz, dst=dst, t0=t0: e.tensor_tensor(
                            out=dst[:, t0:t0 + sz], in0=rtmp[:, 0, :sz], in1=rtmp[:, 1, :sz], op=ALU.add),
                            r=['rtmp0', 'rtmp1'], w=['QK'])
        P.fence()
        A.release(m_a1)
        m_ph = A.mark()
        if do_pool:
            wpool_f = A.alloc("wpool_f", [128, 128])
            wpool_b = A.alloc("wpool_b", [128, 128], BF16)
            pscale_sb = A.alloc("pscale", [128, 1])
            selw_sb = A.alloc("selw", [128, 4])
            efix_sb = A.alloc("efix", [128, 16])
            NB = 2048
            U = A.alloc("U", [128, NB + 32])
            W = [A.alloc("W%d" % i, [128, NB + 32]) for i in range(2)]
            comb = A.alloc("comb", [128, NB])
            diffb = A.alloc("diffb", [128, NB], BF16)
            pob = [A.alloc("pob%d" % i, [128, 512]) for i in range(2)]
            P.dma('sp', wpool_f[:], wpool, w=['wpool_f'])
            P.dma('sp', pscale_sb[:], pscale, w=['pscale'])
            P.dma('sp', selw_sb[:], selw, w=['selw'])
            P.dma('sp', efix_sb[:], efix, w=['efix'])
            P.op('dve', lambda e: e.tensor_copy(out=wpool_b[:], in_=wpool_f[:]), r=['wpool_f'], w=['wpool_b'])
            seqs = [(CTXN, L, (0 if not ctx_out else CTXN))]
            if ctx_out:
                seqs.append((0, CTXN, 0))
            pi = 0
            for (s0, slen, o0) in seqs:
                for b0 in range(0, slen, NB):
                    n = min(NB, slen - b0)
                    lo = max(0, b0 - 16)
                    hi = min(slen, b0 + n + 16)
                    P.op('pool', lambda e: e.memset(U[:], 0.0), w=['U'])
                    P.dma('sp', U[:, 16 + lo - b0:16 + hi - b0], pT[0, :, s0 + lo:s0 + hi], r=[('pT', 0, t) for t in range(len(tiles))], w=['U'])
                    NP = n + 32
                    src = U
                    for lv, sh in enumerate([1, 2, 4, 8]):
                        dstw = W[lv % 2]
                        P.op('dve', lambda e, src=src, dstw=dstw, sh=sh, NP=NP: e.tensor_tensor(
                            out=dstw[:, sh:NP], in0=src[:, sh:NP], in1=src[:, 0:NP - sh], op=ALU.add),
                            r=['U', ('W', 0), ('W', 1)], w=[('W', lv % 2)])
                        w_ = 2 * sh
                        off = 16 + w_ // 2 - 1
                        if lv == 0:
                            P.op('pool', lambda e, dstw=dstw, off=off, n=n, lv=lv: e.tensor_scalar(
                                out=comb[:, :n], in0=dstw[:, off:off + n], scalar1=selw_sb[:, lv:lv + 1], scalar2=None,
                                op0=ALU.mult), r=[('W', lv % 2), 'selw'], w=['comb'])
                        else:
                            P.op('dve', lambda e, dstw=dstw, off=off, n=n, lv=lv: e.scalar_tensor_tensor(
                                out=comb[:, :n], in0=dstw[:, off:off + n], scalar=selw_sb[:, lv:lv + 1], in1=comb[:, :n],
                                op0=ALU.mult, op1=ALU.add), r=[('W', lv % 2), 'selw', 'comb'], w=['comb'])
                        src = dstw
                    if b0 == 0:
                        P.op('pool', lambda e: e.tensor_tensor(out=comb[:, 0:8], in0=comb[:, 0:8], in1=efix_sb[:, 0:8],
                                                               op=ALU.mult), r=['comb', 'efix'], w=['comb'])
                    if b0 + n == slen:
                        P.op('pool', lambda e, n=n: e.tensor_tensor(out=comb[:, n - 8:n], in0=comb[:, n - 8:n],
                                                                    in1=efix_sb[:, 8:16], op=ALU.mult),
                             r=['comb', 'efix'], w=['comb'])
                    P.op('dve', lambda e, n=n: e.tensor_tensor(out=diffb[:, :n], in0=comb[:, :n], in1=U[:, 16:16 + n],
                                                               op=ALU.subtract), r=['comb', 'U'], w=['diffb'])
                    for c0 in range(0, n, 512):
                        cs = min(512, n - c0)
                        pb = 6 + pi % 2
                        o = pob[pi % 2]
                        ok = ('pob', pi % 2)
                        pi += 1
                        P.op('pe', lambda e, c0=c0, cs=cs, pb=pb: e.matmul(bank[pb][:, :cs], lhsT=wpool_b[:],
                                                                            rhs=diffb[:, c0:c0 + cs], start=True, stop=True),
                             r=['wpool_b', 'diffb'], w=[('ps', pb)])
                        P.op('act', lambda e, o=o, pb=pb, cs=cs: e.activation(out=o[:, :cs], in_=bank[pb][:, :cs],
                                                                               func=AF.Copy, scale=pscale_sb[:, 0:1]),
                             r=[('ps', pb), 'pscale'], w=[ok])
                        P.dma('pool', brb.dst(0, o0 + b0 + c0, cs), o[:, :cs], r=[ok], w=['brb'])
            P.fence()
            A.release(m_ph)
        if do_attn:
            PT = [[A.alloc("PT%d_%d" % (m, i), [128, 512], BF16) for i in range(2)] for m in range(2)]
            osb = A.alloc("osb", [128, 2, 4, 130])
            rec = A.alloc("rec", [128, 2, 4])
            o1 = A.alloc("o1", [128, 4, 128])
            o2 = A.alloc("o2", [128, 4, 128])
            ssq = A.alloc("ssq", [128, 4])
            oT = A.alloc("oT", [128, 512])
            def oacc(m, qs):
                i = m * 4 + qs
                return bank[4 + i // 3][:, (i % 3) * 130:(i % 3) * 130 + 130], ('ps', 4 + i // 3)
            qjobs = []
            if ctx_out:
                qjobs.append((0, CTXN, (0, 2), 0))
            for i in range(L // 512):
                qjobs.append((CTXN + i * 512, 512, (0, NKT), (CTXN if ctx_out else 0) + i * 512))
            si = 0
            for (q0, nq, (k0, k1), orow) in qjobs:
                nqs = nq // 128
                started = set()
                for kt in range(k0, k1):
                    for m in range(2):
                        sb_ = si % 2
                        pb = m * 2 + sb_
                        P.op('pe', lambda e, m=m, kt=kt, q0=q0, nq=nq, pb=pb: e.matmul(
                            bank[pb][:, :nq], lhsT=KT[m * 64:(m + 1) * 64, kt * 128:(kt + 1) * 128],
                            rhs=QT[m * 64:(m + 1) * 64, q0:q0 + nq], start=True, stop=True),
                            r=['QK'], w=[('ps', pb)])
                        P.op('act', lambda e, m=m, sb_=sb_, pb=pb, nq=nq: e.activation(
                            out=PT[m][sb_][:, :nq], in_=bank[pb][:, :nq], func=AF.Exp, scale=0.125),
                            r=[('ps', pb)], w=[('PT', m, sb_)])
                        for qs in range(nqs):
                            oap, okey = oacc(m, qs)
                            first_in_bank = (kt == k0) and (okey not in started)
                            started.add(okey)
                            P.op('pe', lambda e, m=m, sb_=sb_, qs=qs, kt=kt, oap=oap, fib=first_in_bank, k1=k1: e.matmul(
                                oap, lhsT=PT[m][sb_][:, qs * 128:(qs + 1) * 128], rhs=V[:, kt, :],
                                start=fib, stop=(kt == k1 - 1)),
                                r=[('PT', m, sb_), 'V', 'Vones'], w=[okey])
                    si += 1
                for m in range(2):
                    for qs in range(nqs):
                        oap, okey = oacc(m, qs)
                        P.op('dve' if (m + qs) % 2 else 'act',
                             (lambda e, m=m, qs=qs, oap=oap: e.tensor_copy(out=osb[:, m, qs, :], in_=oap)) if (m + qs) % 2
                             else (lambda e, m=m, qs=qs, oap=oap: e.copy(out=osb[:, m, qs, :], in_=oap)),
                             r=[okey], w=['osb'])
                P.op('dve', lambda e, nqs=nqs: e.reciprocal(out=rec[:, :, :nqs], in_=osb[:, :, :nqs, 128]),
                     r=['osb'], w=['rec'])
                P.op('dve', lambda e, nqs=nqs: e.tensor_scalar(out=rec[:, 1, :nqs], in0=rec[:, 1, :nqs],
                                                               scalar1=lam_sb[:, 0:1], scalar2=None, op0=ALU.mult),
                     r=['rec', 'lam'], w=['rec'])
                P.op('dve', lambda e, nqs=nqs: e.tensor_tensor(
                    out=o1[:, :nqs, :], in0=osb[:, 0, :nqs, 0:128],
                    in1=rec[:, 0, :nqs].unsqueeze(2).broadcast_to([128, nqs, 128]), op=ALU.mult),
                    r=['osb', 'rec'], w=['o1'])
                P.op('pool', lambda e, nqs=nqs: e.tensor_tensor(
                    out=o2[:, :nqs, :], in0=osb[:, 1, :nqs, 0:128],
                    in1=rec[:, 1, :nqs].unsqueeze(2).broadcast_to([128, nqs, 128]), op=ALU.mult),
                    r=['osb', 'rec'], w=['o2'])
                P.op('dve', lambda e, nqs=nqs: e.tensor_tensor(out=o1[:, :nqs, :], in0=o1[:, :nqs, :], in1=o2[:, :nqs, :],
                                                               op=ALU.add), r=['o1', 'o2'], w=['o1'])
                P.op('pool', lambda e, nqs=nqs: e.tensor_tensor(out=o2[:, :nqs, :], in0=o1[:, :nqs, :], in1=o1[:, :nqs, :],
                                                                op=ALU.mult), r=['o1', 'o2'], w=['o2'])
                P.op('dve', lambda e, nqs=nqs: e.tensor_reduce(out=ssq[:, :nqs], in_=o2[:, :nqs, :], axis=AX.X, op=ALU.add),
                     r=['o2'], w=['ssq'])
                P.op('act', lambda e, nqs=nqs: e.activation(out=ssq[:, :nqs], in_=ssq[:, :nqs], func=AF.Sqrt,
                                                            scale=1.0 / 128, bias=eps_sb[:, 0:1]),
                     r=['ssq', 'eps'], w=['ssq'])
                P.op('dve', lambda e, nqs=nqs: e.reciprocal(out=ssq[:, :nqs], in_=ssq[:, :nqs]), r=['ssq'], w=['ssq'])
                P.op('dve', lambda e, nqs=nqs: e.tensor_tensor(
                    out=o1[:, :nqs, :], in0=o1[:, :nqs, :],
                    in1=ssq[:, :nqs].unsqueeze(2).broadcast_to([128, nqs, 128]), op=ALU.mult),
                    r=['o1', 'ssq'], w=['o1'])
                P.op('pool', lambda e, nqs=nqs: e.tensor_tensor(
                    out=o2[:, :nqs, :], in0=o1[:, :nqs, :],
                    in1=subg_sb[:].unsqueeze(1).broadcast_to([128, nqs, 128]), op=ALU.mult),
                    r=['o1', 'subg', 'o2'], w=['o2'])
                for qs in range(nqs):
                    P.op('pe', lambda e, qs=qs: e.matmul(bank[7][:, qs * 128:(qs + 1) * 128], lhsT=o2[:, qs, :], rhs=ident_sb[:],
                                                         start=True, stop=True), r=['o2', 'identA'], w=[('ps', 7)])
                P.op('act', lambda e, nq=nq: e.copy(out=oT[:, :nq], in_=bank[7][:, :nq]), r=[('ps', 7)], w=['oT'])
                P.dma('sp', brb.dst(1, orow, nq), oT[:, :nq], r=['oT'], w=['brb'])
            P.fence()
        if do_rwkv:
            A.release(base_mark)
            rwkv_phase(nc, P, A, bank, pT, L, ctx_out, (lambda c0, n: brb.dst(2, c0, n)), pfx)
        A.release(base_mark)


NORM_EPS = 1e-6


def phase_B(nc, P, A, bank, pfx, NT_LAT, NT_CTX, x_load, gath, off_lat, x_store, NEXP=32):
    NT = NT_LAT + NT_CTX
    di = lambda name, shape: nc.dram_tensor(pfx + name, list(shape), F32, kind="ExternalInput").ap()
    selq = di("selq", [128, 4])
    cT = di("cT", [128, 8, 2])
    wada = di("wada", [128, 8, 6144])
    bada = di("bada", [128, 48])
    g12 = di("g12", [128, 2, 8])
    wgate = di("wgate", [128, 8, 3072])
    wbr = di("wbr", [128, 12, 1024])
    wout = di("wout", [128, 8, 1024])
    wrt = di("wrt", [128, 8, 36])
    wg = di("wg", [NEXP, 128, 8, 512])
    wu = di("wu", [NEXP, 128, 8, 512])
    wd = di("wd", [NEXP, 128, 4, 1024])
    selE = di("selE", [32, 32 * 128])
    ident = di("ident", [128, 128])
    x2s = nc.dram_tensor(pfx + "x2_scr", [128, 8, NT], F32, kind="Internal").ap()
    xn2s = nc.dram_tensor(pfx + "xn2_scr", [128, 8, NT], BF16, kind="Internal").ap()
    gTs = nc.dram_tensor(pfx + "gT_scr", [32, NT], F32, kind="Internal").ap()
    base_mark = A.mark()

    def TT(eng, out, a, b, op, r, w):
        P.op(eng, lambda e: e.tensor_tensor(out=out, in0=a, in1=b, op=op), r=r, w=w)

    def TS(eng, out, a, s1, s2, op0, op1, r, w):
        if op1 is None:
            P.op(eng, lambda e: e.tensor_scalar(out=out, in0=a, scalar1=s1, scalar2=None, op0=op0), r=r, w=w)
        else:
            P.op(eng, lambda e: e.tensor_scalar(out=out, in0=a, scalar1=s1, scalar2=s2, op0=op0, op1=op1), r=r, w=w)

    def STT(out, a, s, b, op0, op1, r, w):
        P.op('dve', lambda e: e.scalar_tensor_tensor(out=out, in0=a, scalar=s, in1=b, op0=op0, op1=op1), r=r, w=w)

    def ACT(out, a, func, r, w, scale=1.0, bias=None):
        if bias is None:
            P.op('act', lambda e: e.activation(out=out, in_=a, func=func, scale=scale), r=r, w=w)
        else:
            P.op('act', lambda e: e.activation(out=out, in_=a, func=func, scale=scale, bias=bias), r=r, w=w)

    def MM(out, lhsT, rhs, r, w, start=True, stop=True):
        P.op('pe', lambda e: e.matmul(out, lhsT=lhsT, rhs=rhs, start=start, stop=stop), r=r, w=w)

    def RED(out, a, op, r, w):
        P.op('dve', lambda e: e.tensor_reduce(out=out, in_=a, axis=AX.X, op=op), r=r, w=w)

    if True:
        ones = A.alloc("ones", [128, 128])
        selq_sb = A.alloc("selq", [128, 4])
        P.dma('sp', selq_sb[:], selq, w=['selq'])
        eps_sb = A.alloc("eps", [128, 1])
        mod_sb = A.alloc("mod", [128, 48, 2])
        A1 = A.alloc("A1", [128, 8, 2])
        A2 = A.alloc("A2", [128, 8, 2])
        g12_sb = A.alloc("g12", [128, 2, 8])
        ident_sb = A.alloc("ident", [128, 128])
        wrt_sb = A.alloc("wrt", [128, 8, 36])
        P.op('pool', lambda e: e.memset(ones[:], 1.0), w=['ones'])
        P.op('pool', lambda e: e.memset(eps_sb[:], NORM_EPS), w=['eps'])
        P.dma('sp', g12_sb[:], g12, w=['g12'])
        P.dma('sp', ident_sb[:], ident, w=['ident'])
        P.dma('sp', wrt_sb[:], wrt, w=['wrt'])
        mB1 = A.mark()
        c_sb = A.alloc("c", [128, 8, 2])
        s_sb = A.alloc("s", [128, 8, 2])
        bada_sb = A.alloc("bada", [128, 48])
        wgate_b = A.alloc("wgate_b", [128, 8, 3072], BF16)
        wbr_b = A.alloc("wbr_b", [128, 12, 1024], BF16)
        wout_b = A.alloc("wout_b", [128, 8, 1024], BF16)
        xt = [A.alloc("xt%d" % i, [128, 8, 512]) for i in range(2)]
        sq = A.alloc("sq", [128, 8, 512])
        rstd = A.alloc("rstd", [128, 512])
        xn1 = A.alloc("xn1", [128, 8, 512], BF16)
        brf = A.alloc("brf", [128, 4, 512])
        gTt = A.alloc("gTt", [32, 512])
        brb = A.alloc("brb", [128, 12, 512], BF16)
        sig = [A.alloc("sig%d" % i, [128, 512]) for i in range(2)]
        mrg = A.alloc("mrg", [128, 2, 512])
        mk_ = A.mark()
        mrgb = A.alloc("mrgb", [128, 8, 512], BF16)
        A.release(mk_)
        brsel = A.alloc("brsel", [128, 4, 512])
        x2 = A.alloc("x2", [128, 8, 512])
        rt = A.alloc("rt", [128, 80])
        g32 = A.alloc("g32", [128, 32])
        P.dma('sp', c_sb[:], cT, w=['c_sb'])
        P.dma('sp', bada_sb[:], bada, w=['bada'])
        ACT(s_sb[:], c_sb[:], AF.Silu, ['c_sb'], ['s_sb'])
        for piece in range(12):
            b = piece % 2
            P.dma('sp', xt[b][:], wada[:, :, piece * 512:(piece + 1) * 512], w=[('xt', b)])
            for occ in range(4):
                oc = piece * 4 + occ
                for k in range(8):
                    MM(bank[0][:, oc * 2:oc * 2 + 2], xt[b][:, k, occ * 128:(occ + 1) * 128], s_sb[:, k, :],
                       [('xt', b), 's_sb'], [('ps', 0)], start=(k == 0), stop=(k == 7))
        TT('dve', mod_sb[:], bank[0][:, 0:96].rearrange("p (a b) -> p a b", b=2),
           bada_sb[:].unsqueeze(2).broadcast_to([128, 48, 2]), ALU.add, [('ps', 0), 'bada'], ['mod'])
        for (Ax, m_scale, gi) in ((A1, 1, 0), (A2, 4, 1)):
            TS('dve', Ax[:], mod_sb[:, m_scale * 8:(m_scale + 1) * 8, :], 1.0, None, ALU.add, None, ['mod'], ['Ax%d' % gi])
            TT('dve', Ax[:], Ax[:], g12_sb[:, gi, :].unsqueeze(2).broadcast_to([128, 8, 2]), ALU.mult, ['Ax%d' % gi, 'g12'], ['Ax%d' % gi])
        wi = 0
        for (src, dstw, nk, ncol, key) in ((wgate, wgate_b, 8, 3072, 'wgate_b'), (wbr, wbr_b, 12, 1024, 'wbr_b'), (wout, wout_b, 8, 1024, 'wout_b')):
            for c0 in range(0, ncol, 512):
                for kb in range(0, nk, 8):
                    kn = min(8, nk - kb)
                    b = wi % 2
                    wi += 1
                    P.dma('sp', xt[b][:, :kn, :], src[:, kb:kb + kn, c0:c0 + 512], w=[('xt', b)])
                    P.op('pool' if wi % 2 else 'dve', lambda e, b=b, kn=kn, kb=kb, c0=c0, dstw=dstw: e.tensor_copy(
                        out=dstw[:, kb:kb + kn, c0:c0 + 512], in_=xt[b][:, :kn, :]), r=[('xt', b)], w=[key])
        tiles = [(i * 512, 512, 0) for i in range(NT_LAT // 512)]
        if NT_CTX:
            tiles.append((NT_LAT, NT_CTX, 1))

        def norm_mod(src, sz, j, Ax, m_shift, out_fn, okeys, srckeys):
            P.op('act', lambda e: e.activation(out=sq[:, :, :sz], in_=src[:, :, :sz], func=AF.Square), r=srckeys, w=['sq'])
            for k in range(8):
                MM(bank[0][:, :sz], ones[:], sq[:, k, :sz], ['ones', 'sq'], [('ps', 0)], start=(k == 0), stop=(k == 7))
            ACT(rstd[:, :sz], bank[0][:, :sz], AF.Sqrt, [('ps', 0), 'eps'], ['rstd'], scale=1.0 / 1024, bias=eps_sb[:, 0:1])
            P.op('dve', lambda e: e.reciprocal(out=rstd[:, :sz], in_=rstd[:, :sz]), r=['rstd'], w=['rstd'])
            TT('dve', sq[:, :, :sz], src[:, :, :sz], rstd[:, :sz].unsqueeze(1).broadcast_to([128, 8, sz]), ALU.mult,
               srckeys + ['rstd', 'sq'], ['sq'])
            for k in range(8):
                TS('dve' if k % 2 else 'pool', out_fn(k), sq[:, k, :sz], Ax[:, k, j:j + 1],
                   mod_sb[:, m_shift * 8 + k, j:j + 1], ALU.mult, ALU.add, ['sq', 'Ax0', 'Ax1', 'mod'], okeys)

        pi = 0
        for ti, (t0, sz, j) in enumerate(tiles):
            b = ti % 2
            x_load(xt[b], t0, sz, ('xt', b))
            for n3 in range(3):
                for q in range(4):
                    col0 = (off_lat + q * NT_LAT + t0) if j == 0 else (q * 64 + (t0 - NT_LAT))
                    P.dma('sp', brf[:, :, :sz], gath.gsrc(n3, col0, sz), r=['gath'], w=['brf'])
                    if q == 0:
                        TS('dve', brsel[:, :, :sz], brf[:, :, :sz], selq_sb[:, 0:1], None, ALU.mult, None, ['brf', 'selq'], ['mrgb'])
                    else:
                        STT(brsel[:, :, :sz], brf[:, :, :sz], selq_sb[:, q:q + 1], brsel[:, :, :sz], ALU.mult, ALU.add,
                            ['brf', 'selq', 'mrgb'], ['mrgb'])
                P.op('pool', lambda e, sz=sz, n3=n3: e.tensor_copy(out=brb[:, n3 * 4:(n3 + 1) * 4, :sz], in_=brsel[:, :, :sz]),
                     r=['mrgb'], w=['brb'])
            norm_mod(xt[b], sz, j, A1, 0, lambda k, sz=sz: xn1[:, k, :sz], ['xn1'], [('xt', b)])
            for dc in range(8):
                for n in range(3):
                    pg = bank[1 + pi % 2]
                    pgk = ('ps', 1 + pi % 2)
                    pbk = bank[3 + pi % 2]
                    pbkk = ('ps', 3 + pi % 2)
                    sg_ = sig[pi % 2]
                    sgk = ('sig', pi % 2)
                    pi += 1
                    for k in range(8):
                        MM(pg[:, :sz], wgate_b[:, k, n * 1024 + dc * 128:n * 1024 + (dc + 1) * 128], xn1[:, k, :sz],
                           ['wgate_b', 'xn1'], [pgk], start=(k == 0), stop=(k == 7))
                    for kc in range(4):
                        MM(pbk[:, :sz], wbr_b[:, n * 4 + kc, dc * 128:(dc + 1) * 128], brb[:, n * 4 + kc, :sz],
                           ['wbr_b', 'brb'], [pbkk], start=(kc == 0), stop=(kc == 3))
                    ACT(sg_[:, :sz], pg[:, :sz], AF.Sigmoid, [pgk], [sgk])
                    if n == 0:
                        TT('dve', mrg[:, dc % 2, :sz], sg_[:, :sz], pbk[:, :sz], ALU.mult, [sgk, pbkk], ['mrg'])
                    else:
                        TT('dve', sg_[:, :sz], sg_[:, :sz], pbk[:, :sz], ALU.mult, [sgk, pbkk], [sgk])
                        TT('pool', mrg[:, dc % 2, :sz], mrg[:, dc % 2, :sz], sg_[:, :sz], ALU.add, ['mrg', sgk], ['mrg'])
                P.op('act', lambda e, dc=dc, sz=sz: e.copy(out=mrgb[:, dc, :sz], in_=mrg[:, dc % 2, :sz]), r=['mrg'], w=['mrgb'])
            for dc in range(8):
                pb_ = bank[5 + dc % 2]
                pk_ = ('ps', 5 + dc % 2)
                for k in range(8):
                    MM(pb_[:, :sz], wout_b[:, k, dc * 128:(dc + 1) * 128], mrgb[:, k, :sz], ['wout_b', 'mrgb'], [pk_],
                       start=(k == 0), stop=(k == 7))
                STT(x2[:, dc, :sz], pb_[:, :sz], mod_sb[:, 2 * 8 + dc, j:j + 1], xt[b][:, dc, :sz], ALU.mult, ALU.add,
                    [pk_, 'mod', ('xt', b)], ['x2'])
            P.dma('pool', x2s[:, :, t0:t0 + sz], x2[:, :, :sz], r=['x2'], w=['x2s'])
            xn2f = xt[b]
            xfk = ('xt', b)
            norm_mod(x2, sz, j, A2, 3, lambda k, sz=sz, xn2f=xn2f: xn2f[:, k, :sz], [xfk], ['x2'])
            P.op('act', lambda e, sz=sz, xn2f=xn2f: e.copy(out=xn1[:, :, :sz], in_=xn2f[:, :, :sz]), r=[xfk], w=['xn1'])
            P.dma('pool', xn2s[:, :, t0:t0 + sz], xn1[:, :, :sz], r=['xn1'], w=['xn2s'])
            for s0 in range(0, sz, 128):
                ns = min(128, sz - s0)
                for k in range(8):
                    MM(bank[7][:ns, 0:36], xn2f[:, k, s0:s0 + ns], wrt_sb[:, k, :], [xfk, 'wrt'], [('ps', 7)],
                       start=(k == 0), stop=(k == 7))
                lg = rt[:ns, 0:36]
                P.op('dve', lambda e, ns=ns: e.tensor_copy(out=rt[:ns, 0:36], in_=bank[7][:ns, 0:36]), r=[('ps', 7)], w=['rt'])
                gmax = rt[:ns, 36:37]
                RED(gmax, rt[:ns, 0:4], ALU.max, ['rt'], ['rt'])
                ohg = rt[:ns, 37:41]
                TS('dve', ohg, rt[:ns, 0:4], gmax, None, ALU.is_equal, None, ['rt'], ['rt'])
                ngm = rt[:ns, 41:42]
                TS('dve', ngm, gmax, -1.0, None, ALU.mult, None, ['rt'], ['rt'])
                eg = rt[:ns, 42:46]
                ACT(eg, rt[:ns, 0:4], AF.Exp, ['rt'], ['rt'], bias=ngm)
                pgr = rt[:ns, 46:47]
                RED(pgr, eg, ALU.add, ['rt'], ['rt'])
                P.op('dve', lambda e, pgr=pgr: e.reciprocal(out=pgr, in_=pgr), r=['rt'], w=['rt'])
                TT('dve', g32[:ns, :].rearrange("p (g e) -> p g e", e=8), rt[:ns, 4:36].rearrange("p (g e) -> p g e", e=8),
                   ohg.unsqueeze(2).broadcast_to([ns, 4, 8]), ALU.mult, ['rt'], ['g32'])
                les = rt[:ns, 47:55]
                RED(les, g32[:ns, :].rearrange("p (g e) -> p e g", e=8), ALU.add, ['g32'], ['rt'])
                top1 = rt[:ns, 55:56]
                RED(top1, les, ALU.max, ['rt'], ['rt'])
                oh1 = rt[:ns, 56:64]
                TS('dve', oh1, les, top1, None, ALU.is_equal, None, ['rt'], ['rt'])
                le2 = rt[:ns, 64:72]
                STT(le2, oh1, -1e30, les, ALU.mult, ALU.add, ['rt'], ['rt'])
                top2 = rt[:ns, 72:73]
                RED(top2, le2, ALU.max, ['rt'], ['rt'])
                oh2 = rt[:ns, 73:81] if False else None
                d12 = rt[:ns, 41:42]
                TT('dve', d12, top1, top2, ALU.subtract, ['rt'], ['rt'])
                ga = rt[:ns, 42:43]
                gb = rt[:ns, 43:44]
                ACT(ga, d12, AF.Sigmoid, ['rt'], ['rt'])
                ACT(gb, d12, AF.Sigmoid, ['rt'], ['rt'], scale=-1.0)
                TT('dve', rt[:ns, 42:44], rt[:ns, 42:44], pgr.broadcast_to([ns, 2]), ALU.mult, ['rt'], ['rt'])
                TS('dve', le2, le2, top2, gb, ALU.is_equal, ALU.mult, ['rt'], ['rt'])
                STT(les, oh1, ga, le2, ALU.mult, ALU.add, ['rt'], ['rt'])
                TT('dve', g32[:ns, :].rearrange("p (g e) -> p g e", e=8), ohg.unsqueeze(2).broadcast_to([ns, 4, 8]),
                   les.unsqueeze(1).broadcast_to([ns, 4, 8]), ALU.mult, ['rt', 'g32'], ['g32'])
                MM(bank[7][0:32, 64:64 + ns], g32[:ns, :], ident_sb[:ns, :ns], ['g32', 'ident'], [('ps', 7)])
                P.op('act', lambda e, s0=s0, ns=ns: e.copy(out=gTt[:, s0:s0 + ns], in_=bank[7][0:32, 64:64 + ns]),
                     r=[('ps', 7)], w=['gTt'])
            P.dma('pool', gTs[:, t0:t0 + sz], gTt[:, :sz], r=['gTt'], w=['gTs'])
        P.fence()
        A.release(mB1)
        selE_sb = A.alloc("selE", [32, 32 * 128])
        P.dma('sp', selE_sb[:], selE, w=['selE'])
        lat_ = tiles[:NT_LAT // 512]
        groups = [lat_[i:i + 2] for i in range(0, len(lat_), 2)]
        if NT_CTX:
            groups[-1] = groups[-1] + [tiles[-1]]
        GMAX = max(sum(t[1] for t in g) for g in groups)
        yacc = A.alloc("yacc", [128, 8, GMAX])
        xn2 = A.alloc("xn2g", [128, 8, GMAX], BF16)
        gT = A.alloc("gTg", [32, GMAX])
        wst = [A.alloc("wst%d" % i, [128, 4, 512]) for i in range(4)]
        wgb = [A.alloc("wgb%d" % i, [128, 8, 512], BF16) for i in range(2)]
        wub = [A.alloc("wub%d" % i, [128, 8, 512], BF16) for i in range(2)]
        wdb = [A.alloc("wdb%d" % i, [128, 4, 1024], BF16) for i in range(2)]
        hs = [A.alloc("hs%d" % i, [128, 512]) for i in range(2)]
        actb = [A.alloc("actb%d" % i, [128, 4, 512], BF16) for i in range(2)]
        x2r = A.alloc("x2r", [128, 8, 512])
        si = 0
        ai = 0
        hi_ = 0
        for gi, grp in enumerate(groups):
            g0 = grp[0][0]
            gsz = sum(t[1] for t in grp)
            P.dma('sp', xn2[:, :, :gsz], xn2s[:, :, g0:g0 + gsz], r=['xn2s'], w=['xn2g'])
            P.dma('sp', gT[:, :gsz], gTs[:, g0:g0 + gsz], r=['gTs'], w=['gTg'])
            for e_ in range(NEXP):
                wb = e_ % 2
                pieces = [(wg[e_, :, 0:4, :], wgb[wb][:, 0:4, :], ('wgb', wb)), (wg[e_, :, 4:8, :], wgb[wb][:, 4:8, :], ('wgb', wb)),
                          (wu[e_, :, 0:4, :], wub[wb][:, 0:4, :], ('wub', wb)), (wu[e_, :, 4:8, :], wub[wb][:, 4:8, :], ('wub', wb)),
                          (wd[e_, :, :, 0:512], wdb[wb][:, :, 0:512], ('wdb', wb)), (wd[e_, :, :, 512:1024], wdb[wb][:, :, 512:1024], ('wdb', wb))]
                for (src, dst, key) in pieces:
                    sb_ = si % 4
                    si += 1
                    P.dma('sp', wst[sb_][:], src, w=[('wst', sb_)])
                    eng = ('pool', 'dve', 'act')[si % 3] if False else ('pool' if si % 2 else 'act')
                    if eng == 'act':
                        P.op('act', lambda e, dst=dst, sb_=sb_: e.copy(out=dst, in_=wst[sb_][:]), r=[('wst', sb_)], w=[key])
                    else:
                        P.op('pool', lambda e, dst=dst, sb_=sb_: e.tensor_copy(out=dst, in_=wst[sb_][:]), r=[('wst', sb_)], w=[key])
                for (t0, sz, j) in grp:
                    ti = tiles.index((t0, sz, j))
                    lo = t0 - g0
                    MM(bank[0][:, :sz], selE_sb[:, e_ * 128:(e_ + 1) * 128], gT[:, lo:lo + sz], ['selE', 'gTg'], [('ps', 0)])
                    ab = actb[ai % 2]
                    ak = ('actb', ai % 2)
                    ai += 1
                    for fc in range(4):
                        pgb = bank[1 + fc % 2]
                        pgk = ('ps', 1 + fc % 2)
                        pub = bank[3 + fc % 2]
                        puk = ('ps', 3 + fc % 2)
                        for k in range(8):
                            MM(pgb[:, :sz], wgb[wb][:, k, fc * 128:(fc + 1) * 128], xn2[:, k, lo:lo + sz],
                               [('wgb', wb), 'xn2g'], [pgk], start=(k == 0), stop=(k == 7))
                        for k in range(8):
                            MM(pub[:, :sz], wub[wb][:, k, fc * 128:(fc + 1) * 128], xn2[:, k, lo:lo + sz],
                               [('wub', wb), 'xn2g'], [puk], start=(k == 0), stop=(k == 7))
                        h_ = hs[hi_ % 2]
                        hk = ('hs', hi_ % 2)
                        hi_ += 1
                        ACT(h_[:, :sz], pgb[:, :sz], AF.Silu, [pgk], [hk])
                        TT('dve', h_[:, :sz], h_[:, :sz], pub[:, :sz], ALU.mult, [hk, puk], [hk])
                        TT('dve', ab[:, fc, :sz], h_[:, :sz], bank[0][:, :sz], ALU.mult, [hk, ('ps', 0)], [ak])
                    for dc in range(8):
                        pdb = bank[5 + dc % 3]
                        pdk = ('ps', 5 + dc % 3)
                        for fc in range(4):
                            MM(pdb[:, :sz], wdb[wb][:, fc, dc * 128:(dc + 1) * 128], ab[:, fc, :sz], [('wdb', wb), ak], [pdk],
                               start=(fc == 0), stop=(fc == 3))
                        if e_ == 0:
                            P.op('act', lambda e, dc=dc, lo=lo, sz=sz, pdb=pdb: e.copy(out=yacc[:, dc, lo:lo + sz], in_=pdb[:, :sz]),
                                 r=[pdk], w=['yacc'])
                        else:
                            TT('pool' if False else 'dve', yacc[:, dc, lo:lo + sz], yacc[:, dc, lo:lo + sz], pdb[:, :sz], ALU.add,
                               ['yacc', pdk], ['yacc'])
            for (t0, sz, j) in grp:
                lo = t0 - g0
                P.dma('sp', x2r[:, :, :sz], x2s[:, :, t0:t0 + sz], r=['x2s'], w=['x2r'])
                for dc in range(8):
                    STT(x2r[:, dc, :sz], yacc[:, dc, lo:lo + sz], mod_sb[:, 5 * 8 + dc, j:j + 1], x2r[:, dc, :sz], ALU.mult, ALU.add,
                        ['yacc', 'mod', 'x2r'], ['x2r'])
                x_store(x2r, t0, sz, ['x2r'])
        P.fence()
        A.release(base_mark)


POOL_WINDOWS = (2, 4, 8, 16)
def fm(a):
    return np.ascontiguousarray(a.reshape(8, 128, *a.shape[1:]).swapaxes(0, 1))
def colsel(hg):
    c = []
    c += list(range(hg * 128, hg * 128 + 128))
    c += list(range(512 + hg * 128, 512 + hg * 128 + 128))
    c += list(range(1024 + hg * 128, 1024 + hg * 128 + 128))
    c += list(range(1536 + hg * 128, 1536 + hg * 128 + 128))
    for j in range(3):
        c += list(range(2048 + j * 512 + hg * 128, 2048 + j * 512 + hg * 128 + 128))
    c += list(range(2048 + 1536, 2048 + 1920))
    return np.array(c)
def rope_tables(L):
    nrow = L // 64
    inv = (10000.0 ** (-np.arange(16, dtype=np.float32) / 16)).astype(np.float32)
    R = np.ones((128, 2, nrow), np.float32); C = np.ones((128, 2, 64), np.float32)
    perm = np.zeros((128, 128), np.float32)
    rows = np.arange(nrow, dtype=np.float32); cols = np.arange(64, dtype=np.float32)
    for p in range(128):
        d = p % 64
        blk = d // 16
        f = inv[d % 16]
        sign = -1.0 if blk % 2 == 0 else 1.0
        partner = p + 16 if blk % 2 == 0 else p - 16
        perm[partner, p] = 1.0
        if blk < 2:
            ang = (rows * f).astype(np.float32)
            R[p, 0] = np.cos(ang); R[p, 1] = sign * np.sin(ang)
        else:
            ang = (cols * f).astype(np.float32)
            C[p, 0] = np.cos(ang); C[p, 1] = sign * np.sin(ang)
    return R, C, perm
def edge_fix(w, L):
    t = np.arange(L)
    lo = np.clip(t - w // 2, 0, L - 1); hi = np.clip(t + w // 2 - 1, 0, L - 1)
    ratio = (w / (hi - lo + 1)).astype(np.float32)
    return np.concatenate([ratio[:8], ratio[-8:]])
def inputs_A(inp, l, b, hg, L, lam_init, x=None, ctx=None):
    if x is None:
        x = inp['x'][b, :L]; ctx = inp['ctx'][b]
    xa = np.concatenate([ctx, x], 0)
    R, C, perm = rope_tables(L)
    cs = colsel(hg)
    w = POOL_WINDOWS[hg]
    selw = np.zeros((128, 4), np.float32); selw[:, hg] = 1.0 / w
    d = dict(
        xT=fm(np.ascontiguousarray(xa.T)),
        cT=fm(np.stack([inp['c'][b], inp['c_ctx']], 1)),
        wada=fm(inp['w_ada'][l][:, :2048]),
        bada=np.ascontiguousarray(inp['b_ada'][l][:2048].reshape(16, 128).T),
        g1=np.ascontiguousarray(inp['norm1_g'][l].reshape(8, 128).T),
        win=fm(inp['w_in'][l][:, cs]),
        qkg=np.stack([np.tile(inp['q_norm_g'][l], 2), np.tile(inp['k_norm_g'][l], 2)], 1).astype(np.float32),
        ropeR=R, ropeC=C, perm=perm,
        lamqk=np.ascontiguousarray(np.broadcast_to(inp['lambda_qk'][l].reshape(1, 256), (128, 256))),
        subg=np.ascontiguousarray(np.broadcast_to((inp['subln_g'][l] * np.float32(1 - lam_init)).reshape(1, 128), (128, 128))).astype(np.float32),
        wpool=np.ascontiguousarray(inp['pool_w'][l][hg]),
        pscale=np.ascontiguousarray(inp['pool_scale'][l][hg * 128:(hg + 1) * 128].reshape(128, 1)),
        selw=selw,
        efix=np.ascontiguousarray(np.broadcast_to(edge_fix(w, L).reshape(1, 16), (128, 16))),
    )
    return d

def rw_consts():
    i = np.arange(64)
    incl = (i[:, None] <= i[None, :]).astype(np.float32)
    strict = (i[:, None] < i[None, :]).astype(np.float32)
    ones = np.ones((64, 64), np.float32)
    MkN = (i[None, :] < i[:, None]).astype(np.float32)
    return np.concatenate([incl, strict, ones, strict, incl, MkN, np.eye(64, dtype=np.float32)], 1)
def inputs_rw(inp, l, hg):
    mu = inp['shift_mu'][l]
    cmu = np.zeros((128, 6, 2), np.float32)
    p = np.arange(128)
    for g in range(3):
        cmu[:, g, :] = mu[:, g * 512 + hg * 128 + p].T
    for g, base in ((3, 1536), (4, 1664), (5, 1792)):
        cmu[:, g, :] = mu[:, base + p].T
    heads = [2 * hg, 2 * hg + 1]
    w2 = np.zeros((64, 2, 2, 64), np.float32); a2 = np.zeros((64, 2, 2, 64), np.float32)
    w0b = np.zeros((2, 2, 64), np.float32); a0f = np.zeros((64, 2, 2), np.float32)
    prm = np.zeros((64, 2, 5), np.float32)
    for h, hd in enumerate(heads):
        cs = slice(hd * 64, hd * 64 + 64)
        for d in range(2):
            w2[:, d, h, :] = inp['decay_w2'][l][d][:, cs]
            a2[:, d, h, :] = inp['aaa_a2'][l][d][:, cs]
            w0b[d, h, :] = inp['decay_w0'][l][d][cs]
            a0f[:, d, h] = inp['aaa_a0'][l][d][cs]
        prm[:, h, 0] = inp['k_k'][l][cs]; prm[:, h, 1] = inp['k_a'][l][cs]; prm[:, h, 2] = inp['r_k'][l][hd]
        prm[:, h, 3] = inp['gn_w'][l][cs]; prm[:, h, 4] = inp['gn_b'][l][cs]
    return dict(rw_cmu=cmu, rw_g2=np.ascontiguousarray(inp['gate_w2'][l][:, hg * 128:(hg + 1) * 128]),
                rw_w2=w2.reshape(64, 256), rw_a2=a2.reshape(64, 256),
                rw_w0b=np.ascontiguousarray(np.broadcast_to(w0b.reshape(1, 256), (64, 256))),
                rw_a0f=a0f.reshape(64, 4), rw_prm=prm, rw_cst=rw_consts())


def weights_B(inp, l):
    selE = np.zeros((32, 32, 128), np.float32)
    for e in range(32): selE[e, e, :] = 1.0
    return dict(
        wada=fm(inp['w_ada'][l]),
        bada=np.ascontiguousarray(inp['b_ada'][l].reshape(48, 128).T),
        g12=np.ascontiguousarray(np.stack([inp['norm1_g'][l].reshape(8, 128).T, inp['norm2_g'][l].reshape(8, 128).T], 1)),
        wgate=fm(inp['w_in'][l][:, 3968:]),
        wbr=np.ascontiguousarray(inp['w_br'][l].reshape(12, 128, 1024).transpose(1, 0, 2)),
        wout=fm(inp['w_out'][l]),
        wrt=fm(np.concatenate([inp['w_router_group'][l], inp['w_router_expert'][l]], 1)),
        wg=np.ascontiguousarray(inp['w_exp_gate'][l].reshape(32, 8, 128, 512).transpose(0, 2, 1, 3)),
        wu=np.ascontiguousarray(inp['w_exp_up'][l].reshape(32, 8, 128, 512).transpose(0, 2, 1, 3)),
        wd=np.ascontiguousarray(inp['w_exp_down'][l].reshape(32, 4, 128, 1024).transpose(0, 2, 1, 3)),
        selE=selE.reshape(32, 4096), ident=np.eye(128, dtype=np.float32))
def acts_B(xa, br, cvec, c_ctx):
    NT = xa.shape[0]
    return dict(xT=fm(np.ascontiguousarray(xa.T)),
                brT=np.ascontiguousarray(br.T.reshape(12, 128, NT).transpose(1, 0, 2)),
                cT=fm(np.stack([cvec, c_ctx], 1)))


GROUPS = [[0, 1, 2, 3], [4, 5, 6, 7]]


CC_COLS = 2048


class BrStore:
    def __init__(self, nc, name, NQ, ctx_out):
        self.splits = ([(0, 256)] if ctx_out else []) + [(c0, min(CC_COLS, NQ - c0)) for c0 in range(256 if ctx_out else 0, NQ, CC_COLS)]
        self.b = {}
        self.g = {}
        for n in range(3):
            for ci, (c0, cs) in enumerate(self.splits):
                self.b[(n, ci)] = nc.dram_tensor("%s_b%d_%d" % (name, n, ci), [128, cs], F32, kind="Internal").ap()
                self.g[(n, ci)] = nc.dram_tensor("%s_g%d_%d" % (name, n, ci), [4 * 128, cs], F32, kind="Internal").ap()

    def _find(self, col0, ncols):
        for ci, (c0, cs) in enumerate(self.splits):
            if c0 <= col0 and col0 + ncols <= c0 + cs:
                return ci, col0 - c0
        raise AssertionError(("chunk straddle", col0, ncols))

    def dst(self, n, col0, ncols):
        ci, o = self._find(col0, ncols)
        return self.b[(n, ci)][:, o:o + ncols]

    def gsrc(self, n, col0, ncols):
        ci, o = self._find(col0, ncols)
        return self.g[(n, ci)].rearrange("(g p) t -> p g t", g=4)[:, :, o:o + ncols]

    def exchange(self, P):
        for key in self.b:
            P.cc(self.b[key], self.g[key], GROUPS, r=['brb'], w=['gath'])


class XStore:
    def __init__(self, nc, name, NL):
        self.NL = NL
        NT0 = NL + 64
        self.splits = [(c0, min(CC_COLS, NL - c0)) for c0 in range(0, NL, CC_COLS)] + [(NL, 64)]
        self.b = {}
        self.g = {}
        for k in range(8):
            for ci, (c0, cs) in enumerate(self.splits):
                self.b[(k, ci)] = nc.dram_tensor("%s_b%d_%d" % (name, k, ci), [128, cs], F32, kind="Internal").ap()
                self.g[(k, ci)] = nc.dram_tensor("%s_g%d_%d" % (name, k, ci), [4 * 128, cs], F32, kind="Internal").ap()

    def _find(self, col0, ncols):
        for ci, (c0, cs) in enumerate(self.splits):
            if c0 <= col0 and col0 + ncols <= c0 + cs:
                return ci, col0 - c0
        raise AssertionError(("chunk straddle", col0, ncols))

    def store(self, P, src_tile, t0, sz, rkeys):
        ci, o = self._find(t0, sz)
        for k in range(8):
            P.dma('pool', self.b[(k, ci)][:, o:o + sz], src_tile[:, k, :sz], r=rkeys, w=['xnew'])

    def load_local(self, P, dst_tile, t0, sz, key):
        ci, o = self._find(t0, sz)
        for k in range(8):
            P.dma('sp', dst_tile[:, k, :sz], self.b[(k, ci)][:, o:o + sz], r=['xnew'], w=[key])

    def load_gathered(self, P, dst_tile, dcol, rank, t0, sz, key):
        ci, o = self._find(t0, sz)
        for k in range(8):
            P.dma('sp', dst_tile[:, k, dcol:dcol + sz], self.g[(k, ci)][rank * 128:(rank + 1) * 128, o:o + sz], r=['xg'], w=[key])

    def exchange(self, P):
        for key in self.b:
            P.cc(self.b[key], self.g[key], GROUPS, r=['xnew'], w=['xg'])


def build_fused(L=16384, nexp=32, stop_after=99):
    nc = bass.Bass("TRN2", target_bir_lowering=False)
    nc.allow_low_precision("bf16 matmul operands, fp32 accumulation")
    P = Prog(nc)
    A = Arena(nc)
    T = 256 + L
    NL = L // 4
    NT0 = NL + 64
    lam = [0.8 - 0.6 * math.exp(-0.3 * l) for l in range(2)]
    with contextlib.ExitStack() as st:
        bank = [st.enter_context(nc.psum_tensor("bank%d" % i, [128, 512], F32)) for i in range(8)]
        xout = nc.dram_tensor("xout", [128, 8, NL], F32, kind="ExternalOutput").ap()

        def finish():
            stats = P.emit()
            stats['sbuf_peak'] = A.peak
            return nc, stats
        br0 = BrStore(nc, "br0", T, True)
        phase_A(nc, P, A, bank, "A0_", L, True, lam[0], None, br0)
        P.fence()
        if stop_after == 1:
            return finish()
        br0.exchange(P)
        P.fence()
        if stop_after == 2:
            return finish()
        xsh = nc.dram_tensor("xsh", [128, 8, NT0], F32, kind="ExternalInput").ap()
        xs = XStore(nc, "xs", NL)

        def x_load0(dst, t0, sz, key):
            P.dma('sp', dst[:, :, :sz], xsh[:, :, t0:t0 + sz], w=[key])
        phase_B(nc, P, A, bank, "B0_", NL, 64, x_load0, br0, 256, lambda src, t0, sz, rk: xs.store(P, src, t0, sz, rk), NEXP=nexp)
        if stop_after == 3:
            return finish()
        xs.exchange(P)
        P.fence()
        if stop_after == 4:
            return finish()

        def x_loader(dst, t0, sz, key):
            if t0 < 256:
                for r in range(4):
                    xs.load_gathered(P, dst, r * 64, r, NL, 64, key)
            else:
                tt = t0 - 256
                xs.load_gathered(P, dst, 0, tt // NL, tt % NL, sz, key)
        br1 = BrStore(nc, "br1", L, False)
        phase_A(nc, P, A, bank, "A1_", L, False, lam[1], x_loader, br1)
        P.fence()
        if stop_after == 5:
            return finish()
        br1.exchange(P)
        P.fence()
        if stop_after == 6:
            return finish()

        def x_store1(src, t0, sz, rk):
            P.dma('pool', xout[:, :, t0:t0 + sz], src[:, :, :sz], r=rk, final=True)
        phase_B(nc, P, A, bank, "B1_", NL, 0, lambda dst, t0, sz, key: xs.load_local(P, dst, t0, sz, key), br1, 0, x_store1, NEXP=nexp)
        return finish()


def fused_inputs(inp, L, nexp=32, names=None):
    inp = {k: np.asarray(v) for k, v in inp.items()}
    NL = L // 4
    x = inp['x'][:, :L]
    ctx = inp['ctx']
    lam = [0.8 - 0.6 * math.exp(-0.3 * l) for l in range(2)]
    WB = [weights_B(inp, l) for l in range(2)]
    ident = np.eye(128, dtype=np.float32)
    maps = []
    for i in range(8):
        b, hg = i // 4, i % 4
        d = {}
        for l in range(2):
            a = inputs_A(inp, l, b, hg, L, lam[l], x=x[b], ctx=ctx[b])
            if l == 1:
                a.pop('xT')
            a.update(inputs_rw(inp, l, hg))
            a['identA'] = ident
            for k, v in a.items():
                d["A%d_" % l + k] = v
            for k, v in WB[l].items():
                d["B%d_" % l + k] = v[:nexp] if k in ('wg', 'wu', 'wd') else v
            d["B%d_cT" % l] = fm(np.stack([inp['c'][b], inp['c_ctx']], 1))
            selq = np.zeros((128, 4), np.float32)
            selq[:, hg] = 1.0
            d["B%d_selq" % l] = selq
        xa = np.concatenate([x[b, hg * NL:(hg + 1) * NL], ctx[b, hg * 64:(hg + 1) * 64]], 0)
        d['xsh'] = fm(np.ascontiguousarray(xa.T))
        if names is not None:
            d = {k: v for k, v in d.items() if k in names}
        maps.append(d)
    return maps


def fused_gather(results, L):
    NL = L // 4
    out = np.empty((2, L, 1024), np.float32)
    for i in range(8):
        b, q = i // 4, i % 4
        o = np.asarray(results[i]['xout']).transpose(1, 0, 2).reshape(1024, NL).T
        out[b, q * NL:(q + 1) * NL] = o
    return out


def kernel(**inp):
    inp = {k: np.asarray(v) for k, v in inp.items()}
    L = inp['x'].shape[1]
    nc, _ = build_fused(L)
    maps = fused_inputs(inp, L)
    res = run_bass_kernel_spmd(nc, maps, core_ids=list(range(8)))
    del maps
    return fused_gather(res.results, L)
```

```python
import math
import contextlib


import numpy as np
import concourse.bass as bass
import concourse.mybir as mybir
from concourse.bass_utils import run_bass_kernel_spmd

F32 = mybir.dt.float32
BF16 = mybir.dt.bfloat16
I32 = mybir.dt.int32
AF = mybir.ActivationFunctionType
ALU = mybir.AluOpType
AX = mybir.AxisListType

SEM_LIMIT = 8000
DMA_POOL = 40


class Prog:
    def __init__(self, nc):
        self.nc = nc
        self.ops = []

    def op(self, eng, fn, r=(), w=()):
        self.ops.append(dict(eng=eng, fn=fn, r=tuple(r), w=tuple(w), dma=False, final=False))

    def dma(self, eng, out, in_, r=(), w=(), final=False, **kw):
        def fn(e, out=out, in_=in_, kw=kw):
            return e.dma_start(out=out, in_=in_, **kw)
        self.ops.append(dict(eng=eng, fn=fn, r=tuple(r), w=tuple(w), dma=True, final=final))

    def cc(self, ins_ap, out_ap, groups, r=(), w=()):
        def fn(e, ins_ap=ins_ap, out_ap=out_ap, groups=groups):
            return e.collective_compute("AllGather", ALU.bypass, replica_groups=groups, ins=[ins_ap], outs=[out_ap])
        self.ops.append(dict(eng='pool', fn=fn, r=tuple(r), w=tuple(w), dma=True, final=False, cc=True))

    def fence(self):
        self.ops.append(dict(eng=None, fn=None, r=(), w=(), dma=False, final=False, fence=True))

    def emit(self):
        nc = self.nc
        raw_ops = self.ops
        ops = []
        fence_after = {}
        fence_pos = []
        for o in raw_ops:
            if o.get('fence'):
                fence_pos.append(len(ops))
            else:
                ops.append(o)
        self.ops = ops
        n = len(ops)
        fence_deps_at = {}
        prev = 0
        for fp in fence_pos:
            last = {}
            dm = set()
            for i in range(prev, fp):
                o = ops[i]
                if o['dma']:
                    dm.add(i)
                else:
                    last[o['eng']] = i
            fence_deps_at[fp] = set(last.values()) | dm
            prev = fp
        last_w = {}
        readers = {}
        deps = [None] * n
        cur_fence = set()
        first_after = {}
        for i, o in enumerate(ops):
            if i in fence_deps_at:
                cur_fence = fence_deps_at[i]
                first_after = {}
            d = {}

            def add(j, raw):
                d[j] = d.get(j, False) or raw
            for k in o['r']:
                if k in last_w:
                    add(last_w[k], True)
            for k in o['w']:
                if k in last_w:
                    add(last_w[k], False)
                for tok, j in readers.get(k, {}).items():
                    add(j, False)
            keep = set()
            for j, raw in d.items():
                if j == i:
                    continue
                oj = ops[j]
                if (not oj['dma']) and (not o['dma']) and oj['eng'] == o['eng']:
                    if o['eng'] == 'pe':
                        continue
                    if not raw:
                        continue
                keep.add(j)
            tok_e = o['eng']
            if cur_fence and tok_e not in first_after:
                first_after[tok_e] = i
                for j in cur_fence:
                    if ops[j]['dma'] or ops[j]['eng'] != tok_e:
                        keep.add(j)
            deps[i] = keep
            for k in o['r']:
                tok = ('d', i) if o['dma'] else o['eng']
                readers.setdefault(k, {})[tok] = i
            for k in o['w']:
                last_w[k] = i
                readers[k] = {}
        needed = [False] * n
        for i in range(n):
            for j in deps[i]:
                needed[j] = True
        engs = ['pe', 'act', 'dve', 'pool', 'sp']
        cnt = {e: 0 for e in engs}
        sig = [None] * n
        dma_uses = [0] * DMA_POOL
        dma_last = [None] * DMA_POOL
        ndma = 0
        semkeys = set()
        for i, o in enumerate(ops):
            if o.get('cc'):
                ncc_ = getattr(self, '_ncc', 0) + 1
                self._ncc = ncc_
                sig[i] = (('cc', 0), ncc_)
                semkeys.add(('cc', 0))
            elif o['dma']:
                j = ndma % DMA_POOL
                ndma += 1
                dma_uses[j] += 1
                if dma_last[j] is not None:
                    deps[i].add(dma_last[j])
                dma_last[j] = i
                sig[i] = (('dma', j), 16 * dma_uses[j])
                semkeys.add(('dma', j))
            elif needed[i]:
                e = o['eng']
                c = cnt[e]
                cnt[e] += 1
                sk = (e, c // SEM_LIMIT)
                sig[i] = (sk, c % SEM_LIMIT + 1)
                semkeys.add(sk)
        finals = [i for i, o in enumerate(ops) if o['final']]
        seen = {e: {} for e in engs}
        streams = {e: [] for e in engs}
        for i, o in enumerate(ops):
            e = o['eng']
            waits = {}
            for j in deps[i]:
                sk, v = sig[j]
                if seen[e].get(sk, 0) >= v:
                    continue
                waits[sk] = max(waits.get(sk, 0), v)
            for sk, v in waits.items():
                seen[e][sk] = v
            streams[e].append((list(waits.items()), o['fn'], sig[i]))
        fw = {}
        for i in finals:
            sk, v = sig[i]
            if seen['sp'].get(sk, 0) >= v:
                continue
            fw[sk] = max(fw.get(sk, 0), v)
        streams['sp'].append((list(fw.items()), None, None))
        self.stats = dict(n_ops=n, cnt=dict(cnt), ndma=ndma,
                          nwaits={e: sum(len(s[0]) for s in streams[e]) for e in engs})
        semkeys = sorted(semkeys, key=str)
        import contextlib
        with contextlib.ExitStack() as st:
            sems = {}
            for sk in semkeys:
                sems[sk] = st.enter_context(nc.semaphore("s_%s_%s" % (sk[0], sk[1])))
            block = st.enter_context(nc.Block())

            def run(engine, items):
                for waits, fn, sg in items:
                    for sk, v in waits:
                        engine.wait_ge(sems[sk], v)
                    if fn is None:
                        continue
                    ins = fn(engine)
                    if sg is not None:
                        inc = 16 if sg[0][0] == 'dma' else 1
                        ins.then_inc(sems[sg[0]], inc)

            @block.tensor
            def _(e):
                run(e, streams['pe'])

            @block.scalar
            def _(e):
                run(e, streams['act'])

            @block.vector
            def _(e):
                run(e, streams['dve'])

            @block.gpsimd
            def _(e):
                run(e, streams['pool'])

            @block.sync
            def _(e):
                run(e, streams['sp'])
        return self.stats


class Arena:
    LO = 16512
    HI = 229376

    def __init__(self, nc):
        self.nc = nc
        self.top = self.LO
        self.n = 0
        self.peak = self.LO

    def alloc(self, name, shape, dt=F32):
        esz = {F32: 4, BF16: 2, I32: 4}[dt]
        nb = esz
        for s_ in shape[1:]:
            nb *= s_
        off = (self.top + 63) // 64 * 64
        assert off + nb <= self.HI, ("SBUF overflow", name, off + nb)
        self.top = off + nb
        self.peak = max(self.peak, self.top)
        self.n += 1
        return self.nc.alloc_sbuf_tensor_at("%s_%d" % (name, self.n), list(shape), dt, offset=off)

    def mark(self):
        return self.top

    def release(self, m):
        self.top = m


GN_EPS = 64e-5
FINAL_OUT = False
KAPPA = 0.6065306597126334
CTXN = 256


def rwkv_phase(nc, P, A, bank, pT, L, ctx_out, out_ap, pfx=''):
    T = CTXN + L
    di = lambda name, shape: nc.dram_tensor(pfx + name, list(shape), F32, kind="ExternalInput").ap()
    cmu = di("rw_cmu", [128, 6, 2])
    g2s = di("rw_g2", [128, 128])
    w2s = di("rw_w2", [64, 256])
    a2s = di("rw_a2", [64, 256])
    w0b = di("rw_w0b", [64, 256])
    a0f = di("rw_a0f", [64, 4])
    prm = di("rw_prm", [64, 2, 5])
    cst = di("rw_cst", [64, 448])
    uT = nc.dram_tensor(pfx + "uT_scr", [6, 128, T], F32, kind="Internal").ap()
    Yd = nc.dram_tensor(pfx + "Yd_scr", [2, 128, T], F32, kind="Internal").ap()
    Bd = nc.dram_tensor(pfx + "Bd_scr", [2, 128, T], F32, kind="Internal").ap()

    def TT(eng, out, a, b, op, r, w):
        P.op(eng, lambda e: e.tensor_tensor(out=out, in0=a, in1=b, op=op), r=r, w=w)

    def TS(eng, out, a, s1, s2, op0, op1, r, w):
        if op1 is None:
            P.op(eng, lambda e: e.tensor_scalar(out=out, in0=a, scalar1=s1, scalar2=None, op0=op0), r=r, w=w)
        else:
            P.op(eng, lambda e: e.tensor_scalar(out=out, in0=a, scalar1=s1, scalar2=s2, op0=op0, op1=op1), r=r, w=w)

    def STT(out, a, s, b, op0, op1, r, w):
        P.op('dve', lambda e: e.scalar_tensor_tensor(out=out, in0=a, scalar=s, in1=b, op0=op0, op1=op1), r=r, w=w)

    def ACT(out, a, func, r, w, scale=1.0, bias=None):
        if bias is None:
            P.op('act', lambda e: e.activation(out=out, in_=a, func=func, scale=scale), r=r, w=w)
        else:
            P.op('act', lambda e: e.activation(out=out, in_=a, func=func, scale=scale, bias=bias), r=r, w=w)

    def MM(out, lhsT, rhs, r, w, start=True, stop=True):
        P.op('pe', lambda e: e.matmul(out, lhsT=lhsT, rhs=rhs, start=start, stop=stop), r=r, w=w)

    m0 = A.mark()
    cmu_sb = A.alloc("cmu", [128, 6, 2])
    c0_sb = A.alloc("c0", [128, 6])
    g2_sb = A.alloc("g2", [128, 128])
    raw = [A.alloc("raw%d" % i, [128, 6, 514]) for i in range(2)]
    ush = A.alloc("ush", [128, 6, 512])
    sg = A.alloc("sg", [128, 512])
    P.dma('sp', cmu_sb[:], cmu, w=['cmu'])
    P.dma('sp', g2_sb[:], g2s, w=['g2'])
    TT('dve', c0_sb[:], cmu_sb[:, :, 0], cmu_sb[:, :, 1], ALU.add, ['cmu'], ['c0'])
    TS('dve', c0_sb[:], c0_sb[:], -1.0, 1.0, ALU.mult, ALU.add, ['c0'], ['c0'])
    seqs = [(0, CTXN), (CTXN, L)]
    ti = 0
    for (s0, slen) in seqs:
        for b0 in range(0, slen, 512):
            n = min(512, slen - b0)
            rb = raw[ti % 2]
            rk = ('raw', ti % 2)
            ti += 1
            lo = max(0, b0 - 1)
            hi = min(slen, b0 + n + 1)
            if b0 == 0 or b0 + n == slen:
                P.op('pool', lambda e, rb=rb: e.memset(rb[:], 0.0), w=[rk])
            P.dma('sp', rb[:, :, 1 + lo - b0:1 + hi - b0], pT[1:7, :, s0 + lo:s0 + hi].rearrange("g p t -> p g t"),
                  r=['pTall'], w=[rk])
            for g in range(6):
                eng = 'dve'
                TS('pool', ush[:, g, :n], rb[:, g, 1:1 + n], c0_sb[:, g:g + 1], None, ALU.mult, None, [rk, 'c0'], ['ush'])
                STT(ush[:, g, :n], rb[:, g, 0:n], cmu_sb[:, g, 0:1], ush[:, g, :n], ALU.mult, ALU.add, [rk, 'cmu', 'ush'], ['ush'])
                STT(ush[:, g, :n], rb[:, g, 2:2 + n], cmu_sb[:, g, 1:2], ush[:, g, :n], ALU.mult, ALU.add, [rk, 'cmu', 'ush'], ['ush'])
            ACT(sg[:, :n], ush[:, 5, :n], AF.Sigmoid, ['ush'], ['sg'])
            MM(bank[0][:, :n], g2_sb[:], sg[:, :n], ['g2', 'sg'], [('ps', 0)])
            P.op('act', lambda e, n=n: e.copy(out=ush[:, 5, :n], in_=bank[0][:, :n]), r=[('ps', 0), 'ush'], w=['ush'])
            P.dma('pool', uT[:, :, s0 + b0:s0 + b0 + n].rearrange("g p t -> p g t"), ush[:, :, :n], r=['ush'], w=['uTall'])
    P.fence()
    A.release(m0)
    w2_sb = A.alloc("w2", [64, 256])
    a2_sb = A.alloc("a2", [64, 256])
    w0b_sb = A.alloc("w0b", [64, 256])
    a0f_sb = A.alloc("a0f", [64, 4])
    prm_sb = A.alloc("prm", [64, 2, 5])
    cst_sb = A.alloc("cst", [64, 448])
    ones64 = A.alloc("ones64", [64, 64])
    for nm, dst, src in [('w2', w2_sb, w2s), ('a2', a2_sb, a2s), ('w0b', w0b_sb, w0b), ('a0f', a0f_sb, a0f),
                         ('prm', prm_sb, prm), ('cst', cst_sb, cst)]:
        P.dma('sp', dst[:], src, w=[nm])
    P.op('pool', lambda e: e.memset(ones64[:], 1.0), w=['ones64'])
    Tri3 = cst_sb[:, 0:192]
    Mk = cst_sb[:, 192:320]
    MkN = cst_sb[:, 320:384]
    I64 = cst_sb[:, 384:448]
    ST = A.alloc("ST", [64, 4, 64])
    Stmp = A.alloc("Stmp", [64, 4, 64])
    P.op('pool', lambda e: e.memset(ST[:], 0.0), w=['ST'])
    KSEG = 4
    NBUF = KSEG + 1
    f4 = lambda name: A.alloc(name, [64, 4, 64])

    shr = dict(L_=dict(r=A.alloc("ld_r", [64, 2, 2, 64]), k=A.alloc("ld_k", [64, 2, 2, 64]), v=A.alloc("ld_v", [64, 2, 2, 64]),
                       wl=A.alloc("ld_wl", [64, 2, 64]), al=A.alloc("ld_al", [64, 2, 64])),
               uwl=A.alloc("uwl", [64, 2, 64]), ual=A.alloc("ual", [64, 2, 64]), tw=A.alloc("tw", [64, 2, 64]),
               swt=A.alloc("swt", [64, 256]), kk=f4("kk"), kk2=f4("kk2"), rn=f4("rn"), kkn=f4("kkn"), bb=f4("bb"), km=f4("km"),
               t1=f4("t1"), BhT=f4("BhT"), KhT=f4("KhT"), Xsb=f4("Xsb"), Usb=f4("Usb"), Ysb=f4("Ysb"))

    def alloc_set(i):
        n_ = lambda x: "%s_%d" % (x, i)
        return (shr['L_'], f4(n_("ur")), f4(n_("uk")), f4(n_("uv")), shr['uwl'], shr['ual'],
                shr['tw'], shr['swt'], f4(n_("alr")),
                f4(n_("eI")), f4(n_("eE")), f4(n_("eN")), f4(n_("eT")), A.alloc(n_("gC"), [64, 4]),
                shr['kk'], shr['kk2'], shr['rn'], shr['kkn'], shr['bb'], shr['km'], shr['t1'],
                A.alloc(n_("AR"), [64, 4, 128]), f4(n_("BT")), f4(n_("KTt")), shr['BhT'], shr['KhT'], f4(n_("bon")),
                A.alloc(n_("TM"), [64, 2, 4, 64]), f4(n_("Vt")), A.alloc(n_("Gb"), [64, 4, 128]), A.alloc(n_("Gk"), [64, 4, 128]),
                f4(n_("Nn")), [A.alloc(n_("Pk%d" % j), [64, 2, 4, 64]) for j in range(2)], [f4(n_("Q0")), f4(n_("Q1"))],
                shr['Xsb'], shr['Usb'], shr['Ysb'])
    sets = [alloc_set(i) for i in range(NBUF)]
    prmb = lambda j: prm_sb[:, :, j:j + 1].unsqueeze(1).broadcast_to([64, 2, 2, 64])
    v4 = lambda t: t[:].rearrange("p (d h) t -> p d h t", d=2)
    SHARED = set(['w2', 'a2', 'w0b', 'a0f', 'prm', 'cst', 'ones64', 'uTall', 'Bdall', 'Ydall', 'ST', 'Stmp',
                  'ld', 'wl', 'al', 'tw', 'swt', 'kk', 'kk2', 'rn', 'kkn', 'bb', 'km', 't1', 'BhT', 'KhT', 'Xsb', 'Usb', 'Ysb'])
    cur = {'b': None}

    def kmap(keys):
        if cur['b'] is None:
            return list(keys)
        return [k if (k in SHARED or (isinstance(k, tuple) and k[0] == 'ps')) else ('rw', k, cur['b']) for k in keys]
    _op, _dma = P.op, P.dma

    def Pop(eng, fn, r=(), w=()):
        _op(eng, fn, r=kmap(r), w=kmap(w))

    def Pdma(eng, out, in_, r=(), w=(), **kw):
        _dma(eng, out, in_, r=kmap(r), w=kmap(w), **kw)

    def TT(eng, out, a, b, op, r, w):
        Pop(eng, lambda e: e.tensor_tensor(out=out, in0=a, in1=b, op=op), r=r, w=w)

    def TS(eng, out, a, s1, s2, op0, op1, r, w):
        if op1 is None:
            Pop(eng, lambda e: e.tensor_scalar(out=out, in0=a, scalar1=s1, scalar2=None, op0=op0), r=r, w=w)
        else:
            Pop(eng, lambda e: e.tensor_scalar(out=out, in0=a, scalar1=s1, scalar2=s2, op0=op0, op1=op1), r=r, w=w)

    def STT(out, a, s, b, op0, op1, r, w):
        Pop('dve', lambda e: e.scalar_tensor_tensor(out=out, in0=a, scalar=s, in1=b, op0=op0, op1=op1), r=r, w=w)

    def ACT(out, a, func, r, w, scale=1.0, bias=None):
        if bias is None:
            Pop('act', lambda e: e.activation(out=out, in_=a, func=func, scale=scale), r=r, w=w)
        else:
            Pop('act', lambda e: e.activation(out=out, in_=a, func=func, scale=scale, bias=bias), r=r, w=w)

    def MM(out, lhsT, rhs, r, w, start=True, stop=True):
        Pop('pe', lambda e: e.matmul(out, lhsT=lhsT, rhs=rhs, start=start, stop=stop), r=r, w=w)

    nlat = L // 64
    steps = [(s, 3 - s) for s in range(4)] + [(4 + s, 4 + nlat - 1 - s) for s in range(nlat)]
    NS = len(steps)

    def gen_step(si):
        cf, cb = steps[si]
        cur['b'] = si % NBUF
        (L_, ur, uk, uv, uwl, ual, tw, swt, alr, eI, eE, eN, eT, gC, kk, kk2, rn, kkn, bb, km, t1, AR, BT, KTt, BhT, KhT, bon,
         TM, Vt, Gb, Gk, Nn, Pk, Qk, Xsb, Usb, Yb) = sets[si % NBUF]
        saved = P.ops
        P.ops = []
        marks = []
        lk = 'ld'
        for d, cidx in enumerate((cf, cb)):
            t0 = cidx * 64
            for nm, row in (('r', 0), ('k', 1), ('v', 2)):
                Pdma('sp', L_[nm][:, d, :, :], uT[row, :, t0:t0 + 64].rearrange("(h c) t -> c h t", h=2), r=['uTall'], w=[lk])
            Pdma('sp', L_['wl'][:, d, :], uT[3, d * 64:(d + 1) * 64, t0:t0 + 64], r=['uTall'], w=[lk])
            Pdma('sp', L_['al'][:, d, :], uT[4, d * 64:(d + 1) * 64, t0:t0 + 64], r=['uTall'], w=[lk])
        for nm, dst in (('r', ur), ('k', uk), ('v', uv)):
            d4 = v4(dst)
            Pop('pool', lambda e, d4=d4, src=L_[nm]: e.tensor_copy(out=d4[:, 0], in_=src[:, 0]), r=[lk], w=[nm])
            Pop('pool', lambda e, d4=d4, src=L_[nm]: e.tensor_copy(out=d4[:, 1], in_=src[:, 1, :, ::-1]), r=[lk], w=[nm])
        for nm, dst in (('wl', uwl), ('al', ual)):
            Pop('pool', lambda e, dst=dst, src=L_[nm]: e.tensor_copy(out=dst[:, 0, :], in_=src[:, 0, :]), r=[lk], w=[nm])
            Pop('pool', lambda e, dst=dst, src=L_[nm]: e.tensor_copy(out=dst[:, 1, :], in_=src[:, 1, ::-1]), r=[lk], w=[nm])
        ACT(tw[:], uwl[:], AF.Tanh, ['wl'], ['tw'])
        for d in range(2):
            for h in range(2):
                dh = d * 2 + h
                MM(bank[0][0:64, dh * 64:(dh + 1) * 64], tw[:, d, :], w2_sb[:, dh * 64:(dh + 1) * 64], ['tw', 'w2'], [('ps', 0)])
                MM(bank[0][0:64, 256 + dh * 64:256 + (dh + 1) * 64], a2_sb[:, dh * 64:(dh + 1) * 64], ual[:, d, :],
                   ['a2', 'al'], [('ps', 0)])
        TT('dve', swt[:], bank[0][0:64, 0:256], w0b_sb[:], ALU.add, [('ps', 0), 'w0b'], ['swt'])
        ACT(swt[:], swt[:], AF.Sigmoid, ['swt'], ['swt'])
        TT('dve', alr[:], bank[0][0:64, 256:512].rearrange("p (a t) -> p a t", t=64),
           a0f_sb[:].unsqueeze(2).broadcast_to([64, 4, 64]), ALU.add, [('ps', 0), 'a0f'], ['alr'])
        ACT(alr[:], alr[:], AF.Sigmoid, ['alr'], ['alr'])
        cbanks = (1, 0)
        for dh in range(4):
            bk = cbanks[dh // 2]
            MM(bank[bk][0:64, (dh % 2) * 192:(dh % 2) * 192 + 192], swt[:, dh * 64:(dh + 1) * 64], Tri3, ['swt', 'cst'], [('ps', bk)])
        for half in range(2):
            bk = cbanks[half]
            cv = bank[bk][0:64, 0:384].rearrange("p (a x) -> p a x", x=192)
            sl = slice(half * 2, half * 2 + 2)
            ACT(eI[:, sl, :], cv[:, :, 0:64], AF.Exp, [('ps', bk)], ['eI'], scale=-KAPPA)
            ACT(eE[:, sl, :], cv[:, :, 64:128], AF.Exp, [('ps', bk)], ['eE'], scale=-KAPPA)
            ACT(eN[:, sl, :], cv[:, :, 0:64], AF.Exp, [('ps', bk)], ['eN'], scale=KAPPA)
            ACT(gC[:, sl], cv[:, :, 128], AF.Exp, [('ps', bk)], ['gC'], scale=-KAPPA)
        TT('dve', eT[:], eN[:], gC[:].unsqueeze(2).broadcast_to([64, 4, 64]), ALU.mult, ['eN', 'gC'], ['eT'])
        marks.append(len(P.ops))
        TT('dve', v4(kk), v4(uk), prmb(0), ALU.mult, ['k', 'prm'], ['kk'])
        TT('pool', kk2[:], kk[:], kk[:], ALU.mult, ['kk'], ['kk2'])
        MM(bank[2][0:64, 0:256], ones64[:], kk2[:].rearrange("p a t -> p (a t)"), ['ones64', 'kk2'], [('ps', 2)])
        ACT(rn[:].rearrange("p a t -> p (a t)"), bank[2][0:64, 0:256], AF.Sqrt, [('ps', 2)], ['rn'])
        TS('dve', rn[:], rn[:], 1e-12, None, ALU.max, None, ['rn'], ['rn'])
        Pop('dve', lambda e: e.reciprocal(out=rn[:], in_=rn[:]), r=['rn'], w=['rn'])
        TT('dve', kkn[:], kk[:], rn[:], ALU.mult, ['kk', 'rn'], ['kkn'])
        TT('pool', bb[:], kkn[:], alr[:], ALU.mult, ['kkn', 'alr'], ['bb'])
        TS('pool', t1[:], alr[:], -1.0, None, ALU.add, None, ['alr'], ['t1'])
        TT('pool', v4(t1), v4(t1), prmb(1), ALU.mult, ['t1', 'prm'], ['t1'])
        STT(km[:], t1[:], 1.0, uk[:], ALU.add, ALU.mult, ['t1', 'k'], ['km'])
        STT(AR[:, :, 0:64], kkn[:], -1.0, eE[:], ALU.mult, ALU.mult, ['kkn', 'eE'], ['AR'])
        TT('pool', AR[:, :, 64:128], ur[:], eI[:], ALU.mult, ['r', 'eI'], ['AR'])
        TT('dve', BT[:], bb[:], eN[:], ALU.mult, ['bb', 'eN'], ['BT'])
        TT('pool', KTt[:], km[:], eN[:], ALU.mult, ['km', 'eN'], ['KTt'])
        TT('dve', BhT[:], bb[:], eT[:], ALU.mult, ['bb', 'eT'], ['BhT'])
        TT('pool', KhT[:], km[:], eT[:], ALU.mult, ['km', 'eT'], ['KhT'])
        TT('pool', t1[:], ur[:], km[:], ALU.mult, ['r', 'km', 't1'], ['t1'])
        TT('pool', v4(t1), v4(t1), prmb(2), ALU.mult, ['t1', 'prm'], ['t1'])
        MM(bank[2][0:64, 256:512], ones64[:], t1[:].rearrange("p a t -> p (a t)"), ['ones64', 't1'], [('ps', 2)])
        TT('dve', bon[:], bank[2][0:64, 256:512].rearrange("p (a t) -> p a t", t=64), uv[:], ALU.mult, [('ps', 2), 'v'], ['bon'])
        Pop('pool', lambda e: e.tensor_copy(out=kk2[:, 2:4, :], in_=bon[:, 2:4, ::-1]), r=['bon', 'kk2'], w=['kk2'])
        Pdma('pool', Bd[0, :, cf * 64:cf * 64 + 64].rearrange("(h c) t -> c h t", h=2), bon[:, 0:2, :], r=['bon'], w=['Bdall'])
        Pdma('pool', Bd[1, :, cb * 64:cb * 64 + 64].rearrange("(h c) t -> c h t", h=2), kk2[:, 2:4, :], r=['kk2'], w=['Bdall'])
        for dh in range(4):
            MM(bank[3][0:64, dh * 64:(dh + 1) * 64], BhT[:, dh, :], I64, ['BhT', 'cst'], [('ps', 3)])
            MM(bank[3][0:64, 256 + dh * 64:256 + (dh + 1) * 64], KhT[:, dh, :], I64, ['KhT', 'cst'], [('ps', 3)])
        Pop('act', lambda e: e.copy(out=TM[:].rearrange("p a b t -> p (a b t)"), in_=bank[3][0:64, :]), r=[('ps', 3)], w=['TM'])
        for dh in range(4):
            MM(bank[2][0:64, dh * 64:(dh + 1) * 64], uv[:, dh, :], I64, ['v', 'cst'], [('ps', 2)])
        Pop('dve', lambda e: e.tensor_copy(out=Vt[:].rearrange("p a t -> p (a t)"), in_=bank[2][0:64, 0:256]), r=[('ps', 2)], w=['Vt'])
        marks.append(len(P.ops))
        for dh in range(4):
            MM(bank[4][0:64, dh * 128:(dh + 1) * 128], BT[:, dh, :], AR[:, dh, :], ['BT', 'AR'], [('ps', 4)])
            MM(bank[5][0:64, dh * 128:(dh + 1) * 128], KTt[:, dh, :], AR[:, dh, :], ['KTt', 'AR'], [('ps', 5)])
        mk4 = Mk.unsqueeze(1).broadcast_to([64, 4, 128])
        TT('dve', Gb[:], bank[4][0:64, :].rearrange("p (a x) -> p a x", x=128), mk4, ALU.mult, [('ps', 4), 'cst'], ['Gb'])
        TT('dve', Gk[:], bank[5][0:64, :].rearrange("p (a x) -> p a x", x=128), mk4, ALU.mult, [('ps', 5), 'cst'], ['Gk'])
        for dh in range(4):
            MM(bank[4][0:64, 256 + dh * 64:256 + (dh + 1) * 64], AR[:, dh, 0:64], BT[:, dh, :], ['AR', 'BT'], [('ps', 4)])
        TT('dve', Nn[:], bank[4][0:64, 256:512].rearrange("p (a x) -> p a x", x=64),
           MkN.unsqueeze(1).broadcast_to([64, 4, 64]), ALU.mult, [('ps', 4), 'cst'], ['Nn'])
        TT('pool', Qk[0][:], Gb[:, :, 0:64], I64.unsqueeze(1).broadcast_to([64, 4, 64]), ALU.add, ['Gb', 'cst'], [('Q', 0)])
        pk_prev = (lambda dh: Gb[:, dh, 0:64], lambda dh: Nn[:, dh, :], ['Gb', 'Nn'])
        qi = 0
        for lv in range(1, 6):
            if lv == 3:
                marks.append(len(P.ops))
            pb = lv % 2
            Pn = Pk[pb]
            pkey = ('Pk', pb)
            bkp, bkq = (5, 4) if lv <= 2 else (6, 6)
            for dh in range(4):
                if lv < 5:
                    MM(bank[bkp][0:64, dh * 64:(dh + 1) * 64], pk_prev[1](dh), pk_prev[0](dh), pk_prev[2], [('ps', bkp)])
                MM(bank[bkp][0:64, 256 + dh * 64:256 + (dh + 1) * 64], pk_prev[0](dh), pk_prev[1](dh), pk_prev[2], [('ps', bkp)])
            if lv % 2:
                Pop('act', lambda e, Pn=Pn, bkp=bkp: e.copy(out=Pn[:].rearrange("p a b t -> p (a b t)"), in_=bank[bkp][0:64, :]),
                    r=[('ps', bkp)], w=[pkey])
            else:
                Pop('dve', lambda e, Pn=Pn, bkp=bkp: e.tensor_copy(out=Pn[:].rearrange("p a b t -> p (a b t)"), in_=bank[bkp][0:64, :]),
                    r=[('ps', bkp)], w=[pkey])
            pk_prev = (lambda dh, Pn=Pn: Pn[:, 0, dh, :], lambda dh, Pn=Pn: Pn[:, 1, dh, :], [pkey])
            for dh in range(4):
                MM(bank[bkq][0:64, dh * 64:(dh + 1) * 64], Pn[:, 1, dh, :], Qk[qi][:, dh, :], [pkey, ('Q', qi)], [('ps', bkq)])
            TT('dve', Qk[1 - qi][:], bank[bkq][0:64, 0:256].rearrange("p (a t) -> p a t", t=64), Qk[qi][:], ALU.add,
               [('ps', bkq), ('Q', qi)], [('Q', 1 - qi)])
            qi = 1 - qi
        TTm = Qk[qi]
        tkey = ('Q', qi)
        marks.append(len(P.ops))
        for dh in range(4):
            MM(bank[7][0:64, dh * 64:(dh + 1) * 64], AR[:, dh, 0:64], ST[:, dh, :], ['AR', 'ST'], [('ps', 7)], start=True, stop=False)
            MM(bank[7][0:64, dh * 64:(dh + 1) * 64], Gk[:, dh, 0:64], Vt[:, dh, :], ['Gk', 'Vt'], [('ps', 7)], start=False, stop=True)
        Pop('act', lambda e: e.copy(out=Xsb[:].rearrange("p a t -> p (a t)"), in_=bank[7][0:64, 0:256]), r=[('ps', 7)], w=['Xsb'])
        for dh in range(4):
            MM(bank[7][0:64, 256 + dh * 64:256 + (dh + 1) * 64], TTm[:, dh, :], Xsb[:, dh, :], [tkey, 'Xsb'], [('ps', 7)])
        Pop('act', lambda e: e.copy(out=Usb[:].rearrange("p a t -> p (a t)"), in_=bank[7][0:64, 256:512]), r=[('ps', 7)], w=['Usb'])
        for dh in range(4):
            o = bank[7][0:64, dh * 64:(dh + 1) * 64]
            MM(o, ST[:, dh, :], AR[:, dh, 64:128], ['ST', 'AR'], [('ps', 7)], start=True, stop=False)
            MM(o, Usb[:, dh, :], Gb[:, dh, 64:128], ['Usb', 'Gb'], [('ps', 7)], start=False, stop=False)
            MM(o, Vt[:, dh, :], Gk[:, dh, 64:128], ['Vt', 'Gk'], [('ps', 7)], start=False, stop=True)
        yv = bank[7][0:64, 0:256].rearrange("p (a t) -> p a t", t=64)
        Pop('act', lambda e, Yb=Yb, yv=yv: e.copy(out=Yb[:, 0:2, :], in_=yv[:, 0:2, :]), r=[('ps', 7)], w=['Ysb'])
        Pop('act', lambda e, Yb=Yb, yv=yv: e.copy(out=Yb[:, 2:4, ::-1], in_=yv[:, 2:4, :]), r=[('ps', 7)], w=['Ysb'])
        Pdma('pool', Yd[0, :, cf * 64:cf * 64 + 64].rearrange("(h c) t -> c h t", h=2), Yb[:, 0:2, :], r=['Ysb'], w=['Ydall'])
        Pdma('pool', Yd[1, :, cb * 64:cb * 64 + 64].rearrange("(h c) t -> c h t", h=2), Yb[:, 2:4, :], r=['Ysb'], w=['Ydall'])
        TT('pool', Stmp[:], ST[:], gC[:].unsqueeze(2).broadcast_to([64, 4, 64]), ALU.mult, ['ST', 'gC'], ['Stmp'])
        for dh in range(4):
            o = bank[7][0:64, 256 + dh * 64:256 + (dh + 1) * 64]
            MM(o, TM[:, 0, dh, :], Usb[:, dh, :], ['TM', 'Usb'], [('ps', 7)], start=True, stop=False)
            MM(o, TM[:, 1, dh, :], Vt[:, dh, :], ['TM', 'Vt'], [('ps', 7)], start=False, stop=True)
        TT('dve', ST[:], bank[7][0:64, 256:512].rearrange("p (a t) -> p a t", t=64), Stmp[:], ALU.add, [('ps', 7), 'Stmp'], ['ST'])
        ops = P.ops
        P.ops = saved
        cur['b'] = None
        bounds = [0] + marks + [len(ops)]
        return [ops[bounds[i]:bounds[i + 1]] for i in range(5)]

    def interleave(lists):
        items = []
        for li, lst in enumerate(lists):
            n_ = len(lst)
            for k_, o in enumerate(lst):
                items.append(((k_ + 0.5) / n_, li, k_, o))
        items.sort(key=lambda t: (t[0], t[1], t[2]))
        return [t[3] for t in items]
    gen = [gen_step(si) for si in range(NS)]
    for tau in range(-KSEG, NS):
        lists = []
        if 0 <= tau < NS:
            lists.append(gen[tau][KSEG])
        for j in range(1, KSEG + 1):
            s_ = tau + j
            if 0 <= s_ < NS:
                lists.append(gen[s_][KSEG - j])
        P.ops.extend(interleave(lists))
    P.fence()
    A.release(m0)
    prm2 = A.alloc("prm2", [64, 2, 5])
    on64 = A.alloc("on64", [64, 64])
    eps2 = A.alloc("eps2", [64, 1])
    P.dma('sp', prm2[:], prm, w=['prm2'])
    P.op('pool', lambda e: e.memset(on64[:], 1.0 / 64), w=['on64'])
    P.op('pool', lambda e: e.memset(eps2[:], GN_EPS), w=['eps2'])
    yb = [A.alloc("yb%d" % i, [64, 2, 2, 512]) for i in range(2)]
    bd = [A.alloc("bd%d" % i, [64, 2, 2, 512]) for i in range(2)]
    gg = [A.alloc("gg%d" % i, [64, 2, 512]) for i in range(2)]
    y = A.alloc("y", [64, 2, 512])
    yc = A.alloc("yc", [64, 2, 512])
    y2 = A.alloc("y2", [64, 2, 512])
    rs = A.alloc("rs", [64, 2, 512])
    jobs = []
    if ctx_out:
        jobs.append((0, CTXN, 0))
    o0 = CTXN if ctx_out else 0
    for b0 in range(0, L, 512):
        jobs.append((CTXN + b0, 512, o0 + b0))
    for ji, (t0, n, orow) in enumerate(jobs):
        b = ji % 2
        P.dma('sp', yb[b][:, :, :, :n], Yd[:, :, t0:t0 + n].rearrange("d (h c) t -> c d h t", h=2), r=['Ydall'], w=[('yb', b)])
        P.dma('sp', bd[b][:, :, :, :n], Bd[:, :, t0:t0 + n].rearrange("d (h c) t -> c d h t", h=2), r=['Bdall'], w=[('bd', b)])
        P.dma('sp', gg[b][:, :, :n], uT[5, :, t0:t0 + n].rearrange("(h c) t -> c h t", h=2), r=['uTall'], w=[('gg', b)])
        TT('dve', y[:, :, :n], yb[b][:, 0, :, :n], yb[b][:, 1, :, :n], ALU.add, [('yb', b)], ['y'])
        for h in range(2):
            MM(bank[h][0:64, :n], on64[:], y[:, h, :n], ['on64', 'y'], [('ps', h)])
            TT('dve', yc[:, h, :n], y[:, h, :n], bank[h][0:64, :n], ALU.subtract, ['y', ('ps', h)], ['yc'])
        TT('pool', y2[:, :, :n], yc[:, :, :n], yc[:, :, :n], ALU.mult, ['yc'], ['y2'])
        for h in range(2):
            MM(bank[2 + h][0:64, :n], on64[:], y2[:, h, :n], ['on64', 'y2'], [('ps', 2 + h)])
            ACT(rs[:, h, :n], bank[2 + h][0:64, :n], AF.Sqrt, [('ps', 2 + h), 'eps2'], ['rs'], bias=eps2[:, 0:1])
        P.op('dve', lambda e, n=n: e.reciprocal(out=rs[:, :, :n], in_=rs[:, :, :n]), r=['rs'], w=['rs'])
        TT('dve', yc[:, :, :n], yc[:, :, :n], rs[:, :, :n], ALU.mult, ['yc', 'rs'], ['yc'])
        for h in range(2):
            TS('pool', yc[:, h, :n], yc[:, h, :n], prm2[:, h, 3:4], prm2[:, h, 4:5], ALU.mult, ALU.add, ['yc', 'prm2'], ['yc'])
        TT('dve', yc[:, :, :n], yc[:, :, :n], bd[b][:, 0, :, :n], ALU.add, ['yc', ('bd', b)], ['yc'])
        TT('dve', yc[:, :, :n], yc[:, :, :n], bd[b][:, 1, :, :n], ALU.add, ['yc', ('bd', b)], ['yc'])
        TT('pool', y2[:, :, :n], yc[:, :, :n], gg[b][:, :, :n], ALU.mult, ['yc', ('gg', b), 'y2'], ['y2'])
        P.dma('pool', out_ap(orow, n).rearrange("(h c) t -> c h t", h=2), y2[:, :, :n], r=['y2'], w=['brb'], final=FINAL_OUT)
    P.fence()


NORM_EPS = 1e-6
CTXN = 256


def phase_A(nc, P, A, bank, pfx, L, ctx_out, lam_init, x_loader, brb, do_attn=True, do_pool=True, do_rwkv=True):
    T = CTXN + L
    NQ = T if ctx_out else L
    di = lambda name, shape: nc.dram_tensor(pfx + name, list(shape), F32, kind="ExternalInput").ap()
    if x_loader is None:
        xT = di("xT", [128, 8, T])

        def x_loader(dst, t0, sz, key):
            P.dma('sp', dst[:, :, :sz], xT[:, :, t0:t0 + sz], w=[key])
    identA = di("identA", [128, 128])
    cT = di("cT", [128, 8, 2])
    wada = di("wada", [128, 8, 2048])
    bada = di("bada", [128, 16])
    g1 = di("g1", [128, 8])
    win = di("win", [128, 8, 1280])
    qkg = di("qkg", [128, 2])
    ropeR = di("ropeR", [128, 2, L // 64])
    ropeC = di("ropeC", [128, 2, 64])
    perm = di("perm", [128, 128])
    lamqk = di("lamqk", [128, 256])
    subg = di("subg", [128, 128])
    wpool = di("wpool", [128, 128])
    pscale = di("pscale", [128, 1])
    selw = di("selw", [128, 4])
    efix = di("efix", [128, 16])
    pT = nc.dram_tensor(pfx + "pT_scr", [7, 128, T], F32, kind="Internal").ap()
    base_mark = A.mark()
    if True:
        ones = A.alloc("ones", [128, 128])
        blk = A.alloc("blk", [128, 128])
        eps_sb = A.alloc("eps", [128, 1])
        mod_sb = A.alloc("mod", [128, 16, 2])
        A_sb = A.alloc("A", [128, 8, 2])
        NKT = T // 128
        QT = A.alloc("QT", [128, T], BF16)
        KT = A.alloc("KT", [128, T], BF16)
        V = A.alloc("V", [128, NKT, 130], BF16)
        lam_sb = A.alloc("lam", [128, 1])
        subg_sb = A.alloc("subg", [128, 128])
        ident_sb = A.alloc("identA", [128, 128])
        P.dma('sp', ident_sb[:], identA, w=['identA'])
        P.op('pool', lambda e: e.memset(ones[:], 1.0), w=['ones'])
        P.op('pool', lambda e: e.memset(blk[:], 0.0), w=['blk'])
        P.op('pool', lambda e: e.memset(blk[0:64, 0:64], 1.0), w=['blk'])
        P.op('pool', lambda e: e.memset(blk[64:128, 64:128], 1.0), w=['blk'])
        P.op('pool', lambda e: e.memset(eps_sb[:], NORM_EPS), w=['eps'])
        P.op('pool', lambda e: e.memset(V[:, :, 128:130], 1.0), w=['Vones'])
        m_a1 = A.mark()
        c_sb = A.alloc("c", [128, 8, 2])
        s_sb = A.alloc("s", [128, 8, 2])
        bada_sb = A.alloc("bada", [128, 16])
        g1_sb = A.alloc("g1", [128, 8])
        qkg_sb = A.alloc("qkg", [128, 2])
        perm_sb = A.alloc("perm", [128, 128])
        ropeR_sb = A.alloc("ropeR", [128, 2, L // 64])
        ropeC_sb = A.alloc("ropeC", [128, 2, 64])
        lamqk_sb = A.alloc("lamqk", [128, 256])
        lamt = A.alloc("lamt", [128, 4])
        wbf = A.alloc("wbf", [128, 8, 1280], BF16)
        xt = [A.alloc("xt%d" % i, [128, 8, 512]) for i in range(2)]
        sq = A.alloc("sq", [128, 8, 512])
        rstd = A.alloc("rstd", [128, 512])
        xn = A.alloc("xn", [128, 8, 512], BF16)
        ob = [A.alloc("ob%d" % i, [128, 512]) for i in range(2)]
        qk32 = A.alloc("qk32", [128, 512])
        qksq = A.alloc("qksq", [128, 512])
        qkr = A.alloc("qkr", [128, 512])
        qkn = A.alloc("qkn", [128, 512])
        cs_t = A.alloc("cs_t", [128, 2, 512])
        rtmp = A.alloc("rtmp", [128, 2, 512])

        for nm, dst, src in [('c_sb', c_sb, cT), ('bada', bada_sb, bada), ('g1', g1_sb, g1), ('qkg', qkg_sb, qkg),
                             ('perm', perm_sb, perm), ('ropeR', ropeR_sb, ropeR), ('ropeC', ropeC_sb, ropeC),
                             ('lamqk', lamqk_sb, lamqk), ('subg', subg_sb, subg)]:
            P.dma('sp', dst[:], src, w=[nm])
        lq = lamqk_sb[:].rearrange("p (a b) -> p a b", b=64)
        P.op('dve', lambda e: e.tensor_tensor(out=sq[:, 0, 0:64], in0=lq[:, 0, :], in1=lq[:, 1, :], op=ALU.mult),
             r=['lamqk'], w=['sq'])
        P.op('dve', lambda e: e.tensor_tensor(out=sq[:, 0, 64:128], in0=lq[:, 2, :], in1=lq[:, 3, :], op=ALU.mult),
             r=['lamqk'], w=['sq'])
        P.op('dve', lambda e: e.tensor_reduce(out=lamt[:, 0:2], in_=sq[:, 0, 0:128].rearrange("p (a b) -> p a b", b=64),
                                              axis=AX.X, op=ALU.add), r=['sq'], w=['lamt'])
        P.op('act', lambda e: e.activation(out=lamt[:, 2:4], in_=lamt[:, 0:2], func=AF.Exp), r=['lamt'], w=['lamt2'])
        P.op('dve', lambda e: e.tensor_tensor(out=lam_sb[:], in0=lamt[:, 2:3], in1=lamt[:, 3:4], op=ALU.subtract),
             r=['lamt2'], w=['lam'])
        P.op('dve', lambda e: e.tensor_scalar(out=lam_sb[:], in0=lam_sb[:], scalar1=lam_init, scalar2=-1.0,
                                              op0=ALU.add, op1=ALU.mult), r=['lam'], w=['lam'])
        P.op('act', lambda e: e.activation(out=s_sb[:], in_=c_sb[:], func=AF.Silu), r=['c_sb'], w=['s_sb'])
        ps_mod = bank[0][:, 0:32]
        for piece in range(4):
            b = piece % 2
            P.dma('sp', xt[b][:], wada[:, :, piece * 512:(piece + 1) * 512], w=[('xt', b)])
            for occ in range(4):
                oc = piece * 4 + occ
                for k in range(8):
                    P.op('pe', lambda e, b=b, occ=occ, oc=oc, k=k: e.matmul(
                        ps_mod[:, oc * 2:oc * 2 + 2], lhsT=xt[b][:, k, occ * 128:(occ + 1) * 128],
                        rhs=s_sb[:, k, :], start=(k == 0), stop=(k == 7)),
                        r=[('xt', b), 's_sb'], w=[('ps', 0)])
        P.op('dve', lambda e: e.tensor_tensor(
            out=mod_sb[:], in0=ps_mod.rearrange("p (a b) -> p a b", b=2),
            in1=bada_sb[:].unsqueeze(2).broadcast_to([128, 16, 2]), op=ALU.add),
            r=[('ps', 0), 'bada'], w=['mod'])
        P.op('dve', lambda e: e.tensor_scalar(out=A_sb[:], in0=mod_sb[:, 8:16, :], scalar1=1.0, scalar2=None,
                                              op0=ALU.add), r=['mod'], w=['A'])
        P.op('dve', lambda e: e.tensor_tensor(out=A_sb[:], in0=A_sb[:],
                                              in1=g1_sb[:].unsqueeze(2).broadcast_to([128, 8, 2]), op=ALU.mult),
             r=['A', 'g1'], w=['A'])
        for piece, (c0, csz) in enumerate([(0, 512), (512, 512), (1024, 256)]):
            b = piece % 2
            P.dma('sp', xt[b][:, :, :csz], win[:, :, c0:c0 + csz], w=[('xt', b)])
            P.op('pool', lambda e, b=b, c0=c0, csz=csz: e.tensor_copy(out=wbf[:, :, c0:c0 + csz], in_=xt[b][:, :, :csz]),
                 r=[('xt', b)], w=['wbf'])
        tiles = [(0, CTXN, 1)] + [(CTXN + i * 512, 512, 0) for i in range(L // 512)]
        SCR = {0: 0, 4: 1, 5: 2, 6: 3, 7: 4, 8: 5, 9: 6}
        oi = 0
        for ti, (t0, sz, j) in enumerate(tiles):
            b = ti % 2
            x_loader(xt[b], t0, sz, ('xt', b))
            P.op('act', lambda e, b=b, sz=sz: e.activation(out=sq[:, :, :sz], in_=xt[b][:, :, :sz], func=AF.Square),
                 r=[('xt', b)], w=['sq'])
            for k in range(8):
                P.op('pe', lambda e, k=k, sz=sz: e.matmul(bank[0][:, :sz], lhsT=ones[:], rhs=sq[:, k, :sz],
                                                          start=(k == 0), stop=(k == 7)),
                     r=['ones', 'sq'], w=[('ps', 0)])
            P.op('act', lambda e, sz=sz: e.activation(out=rstd[:, :sz], in_=bank[0][:, :sz], func=AF.Sqrt,
                                                      scale=1.0 / 1024, bias=eps_sb[:, 0:1]),
                 r=[('ps', 0), 'eps'], w=['rstd'])
            P.op('dve', lambda e, sz=sz: e.reciprocal(out=rstd[:, :sz], in_=rstd[:, :sz]), r=['rstd'], w=['rstd'])
            P.op('dve', lambda e, b=b, sz=sz: e.tensor_tensor(
                out=sq[:, :, :sz], in0=xt[b][:, :, :sz],
                in1=rstd[:, :sz].unsqueeze(1).broadcast_to([128, 8, sz]), op=ALU.mult),
                r=[('xt', b), 'rstd', 'sq'], w=['sq'])
            for k in range(8):
                P.op('dve' if k % 2 else 'pool', lambda e, k=k, sz=sz, j=j: e.tensor_scalar(
                    out=xn[:, k, :sz], in0=sq[:, k, :sz], scalar1=A_sb[:, k, j:j + 1],
                    scalar2=mod_sb[:, k, j:j + 1], op0=ALU.mult, op1=ALU.add),
                    r=['sq', 'A', 'mod'], w=['xn'])
            if j == 0:
                r0 = (t0 - CTXN) // 64
                for cs in range(2):
                    P.op('pool', lambda e, cs=cs, r0=r0: e.tensor_tensor(
                        out=cs_t[:, cs, :].rearrange("p (r c) -> p r c", c=64),
                        in0=ropeR_sb[:, cs, r0:r0 + 8].unsqueeze(2).broadcast_to([128, 8, 64]),
                        in1=ropeC_sb[:, cs, :].unsqueeze(1).broadcast_to([128, 8, 64]), op=ALU.mult),
                        r=['ropeR', 'ropeC'], w=['cs_t'])
            for c in range(10):
                if c == 3:
                    for s in range(sz // 128):
                        kt = t0 // 128 + s
                        for k in range(8):
                            P.op('pe', lambda e, k=k, s=s: e.matmul(
                                bank[3][:, 0:128], lhsT=xn[:, k, s * 128:(s + 1) * 128], rhs=wbf[:, k, 384:512],
                                start=(k == 0), stop=(k == 7)), r=['xn', 'wbf'], w=[('ps', 3)])
                        P.op('act', lambda e, kt=kt: e.copy(out=V[:, kt, 0:128], in_=bank[3][:, 0:128]),
                             r=[('ps', 3)], w=['V'])
                    continue
                pb = 1 + (oi % 2)
                oi += 1
                for k in range(8):
                    P.op('pe', lambda e, c=c, k=k, sz=sz, pb=pb: e.matmul(
                        bank[pb][:, :sz], lhsT=wbf[:, k, c * 128:(c + 1) * 128], rhs=xn[:, k, :sz],
                        start=(k == 0), stop=(k == 7)), r=['wbf', 'xn'], w=[('ps', pb)])
                if c in SCR:
                    o = ob[oi % 2]
                    ok = ('ob', oi % 2)
                    if oi % 2:
                        P.op('act', lambda e, o=o, pb=pb, sz=sz: e.copy(out=o[:, :sz], in_=bank[pb][:, :sz]),
                             r=[('ps', pb)], w=[ok])
                    else:
                        P.op('dve', lambda e, o=o, pb=pb, sz=sz: e.tensor_copy(out=o[:, :sz], in_=bank[pb][:, :sz]),
                             r=[('ps', pb)], w=[ok])
                    P.dma('pool', pT[SCR[c], :, t0:t0 + sz], o[:, :sz], r=[ok], w=[('pT', SCR[c], ti)])
                else:
                    dst = QT if c == 1 else KT
                    gi = c - 1
                    P.op('act', lambda e, pb=pb, sz=sz: e.copy(out=qk32[:, :sz], in_=bank[pb][:, :sz]),
                         r=[('ps', pb)], w=['qk32'])
                    P.op('act', lambda e, sz=sz: e.activation(out=qksq[:, :sz], in_=qk32[:, :sz], func=AF.Square),
                         r=['qk32'], w=['qksq'])
                    P.op('pe', lambda e, sz=sz: e.matmul(bank[4][:, :sz], lhsT=blk[:], rhs=qksq[:, :sz],
                                                         start=True, stop=True), r=['blk', 'qksq'], w=[('ps', 4)])
                    P.op('act', lambda e, sz=sz: e.activation(out=qkr[:, :sz], in_=bank[4][:, :sz], func=AF.Sqrt,
                                                              scale=1.0 / 64, bias=eps_sb[:, 0:1]),
                         r=[('ps', 4), 'eps'], w=['qkr'])
                    P.op('dve', lambda e, sz=sz: e.reciprocal(out=qkr[:, :sz], in_=qkr[:, :sz]), r=['qkr'], w=['qkr'])
                    if j == 1:
                        P.op('dve', lambda e, sz=sz, gi=gi, dst=dst, t0=t0: e.scalar_tensor_tensor(
                            out=dst[:, t0:t0 + sz], in0=qk32[:, :sz], scalar=qkg_sb[:, gi:gi + 1], in1=qkr[:, :sz],
                            op0=ALU.mult, op1=ALU.mult), r=['qk32', 'qkg', 'qkr'], w=['QK'])
                    else:
                        P.op('dve', lambda e, sz=sz, gi=gi: e.scalar_tensor_tensor(
                            out=qkn[:, :sz], in0=qk32[:, :sz], scalar=qkg_sb[:, gi:gi + 1], in1=qkr[:, :sz],
                            op0=ALU.mult, op1=ALU.mult), r=['qk32', 'qkg', 'qkr'], w=['qkn'])
                        P.op('pe', lambda e, sz=sz: e.matmul(bank[5][:, :sz], lhsT=perm_sb[:], rhs=qkn[:, :sz],
                                                             start=True, stop=True), r=['perm', 'qkn'], w=[('ps', 5)])
                        P.op('pool', lambda e, sz=sz: e.tensor_tensor(out=rtmp[:, 0, :sz], in0=qkn[:, :sz],
                                                                      in1=cs_t[:, 0, :sz], op=ALU.mult),
                             r=['qkn', 'cs_t'], w=['rtmp0'])
                        P.op('dve', lambda e, sz=sz: e.tensor_tensor(out=rtmp[:, 1, :sz], in0=bank[5][:, :sz],
                                                                     in1=cs_t[:, 1, :sz], op=ALU.mult),
                             r=[('ps', 5), 'cs_t'], w=['rtmp1'])
                        P.op('dve', lambda e, sz=sz, dst=dst, t0=t0: e.tensor_tensor(
                            out=dst[:, t0:t0 + sz], in0=rtmp[:, 0, :sz], in1=rtmp[:, 1, :sz], op=ALU.add),
                            r=['rtmp0', 'rtmp1'], w=['QK'])
        P.fence()
        A.release(m_a1)
        m_ph = A.mark()
        if do_pool:
            wpool_f = A.alloc("wpool_f", [128, 128])
            wpool_b = A.alloc("wpool_b", [128, 128], BF16)
            pscale_sb = A.alloc("pscale", [128, 1])
            selw_sb = A.alloc("selw", [128, 4])
            efix_sb = A.alloc("efix", [128, 16])
            NB = 2048
            U = A.alloc("U", [128, NB + 32])
            W = [A.alloc("W%d" % i, [128, NB + 32]) for i in range(2)]
            comb = A.alloc("comb", [128, NB])
            diffb = A.alloc("diffb", [128, NB], BF16)
            pob = [A.alloc("pob%d" % i, [128, 512]) for i in range(2)]
            P.dma('sp', wpool_f[:], wpool, w=['wpool_f'])
            P.dma('sp', pscale_sb[:], pscale, w=['pscale'])
            P.dma('sp', selw_sb[:], selw, w=['selw'])
            P.dma('sp', efix_sb[:], efix, w=['efix'])
            P.op('dve', lambda e: e.tensor_copy(out=wpool_b[:], in_=wpool_f[:]), r=['wpool_f'], w=['wpool_b'])
            seqs = [(CTXN, L, (0 if not ctx_out else CTXN))]
            if ctx_out:
                seqs.append((0, CTXN, 0))
            pi = 0
            for (s0, slen, o0) in seqs:
                for b0 in range(0, slen, NB):
                    n = min(NB, slen - b0)
                    lo = max(0, b0 - 16)
                    hi = min(slen, b0 + n + 16)
                    P.op('pool', lambda e: e.memset(U[:], 0.0), w=['U'])
                    P.dma('sp', U[:, 16 + lo - b0:16 + hi - b0], pT[0, :, s0 + lo:s0 + hi], r=[('pT', 0, t) for t in range(len(tiles))], w=['U'])
                    NP = n + 32
                    src = U
                    for lv, sh in enumerate([1, 2, 4, 8]):
                        dstw = W[lv % 2]
                        P.op('dve', lambda e, src=src, dstw=dstw, sh=sh, NP=NP: e.tensor_tensor(
                            out=dstw[:, sh:NP], in0=src[:, sh:NP], in1=src[:, 0:NP - sh], op=ALU.add),
                            r=['U', ('W', 0), ('W', 1)], w=[('W', lv % 2)])
                        w_ = 2 * sh
                        off = 16 + w_ // 2 - 1
                        if lv == 0:
                            P.op('pool', lambda e, dstw=dstw, off=off, n=n, lv=lv: e.tensor_scalar(
                                out=comb[:, :n], in0=dstw[:, off:off + n], scalar1=selw_sb[:, lv:lv + 1], scalar2=None,
                                op0=ALU.mult), r=[('W', lv % 2), 'selw'], w=['comb'])
                        else:
                            P.op('dve', lambda e, dstw=dstw, off=off, n=n, lv=lv: e.scalar_tensor_tensor(
                                out=comb[:, :n], in0=dstw[:, off:off + n], scalar=selw_sb[:, lv:lv + 1], in1=comb[:, :n],
                                op0=ALU.mult, op1=ALU.add), r=[('W', lv % 2), 'selw', 'comb'], w=['comb'])
                        src = dstw
                    if b0 == 0:
                        P.op('pool', lambda e: e.tensor_tensor(out=comb[:, 0:8], in0=comb[:, 0:8], in1=efix_sb[:, 0:8],
                                                               op=ALU.mult), r=['comb', 'efix'], w=['comb'])
                    if b0 + n == slen:
                        P.op('pool', lambda e, n=n: e.tensor_tensor(out=comb[:, n - 8:n], in0=comb[:, n - 8:n],
                                                                    in1=efix_sb[:, 8:16], op=ALU.mult),
                             r=['comb', 'efix'], w=['comb'])
                    P.op('dve', lambda e, n=n: e.tensor_tensor(out=diffb[:, :n], in0=comb[:, :n], in1=U[:, 16:16 + n],
                                                               op=ALU.subtract), r=['comb', 'U'], w=['diffb'])
                    for c0 in range(0, n, 512):
                        cs = min(512, n - c0)
                        pb = 6 + pi % 2
                        o = pob[pi % 2]
                        ok = ('pob', pi % 2)
                        pi += 1
                        P.op('pe', lambda e, c0=c0, cs=cs, pb=pb: e.matmul(bank[pb][:, :cs], lhsT=wpool_b[:],
                                                                            rhs=diffb[:, c0:c0 + cs], start=True, stop=True),
                             r=['wpool_b', 'diffb'], w=[('ps', pb)])
                        P.op('act', lambda e, o=o, pb=pb, cs=cs: e.activation(out=o[:, :cs], in_=bank[pb][:, :cs],
                                                                               func=AF.Copy, scale=pscale_sb[:, 0:1]),
                             r=[('ps', pb), 'pscale'], w=[ok])
                        P.dma('pool', brb.dst(0, o0 + b0 + c0, cs), o[:, :cs], r=[ok], w=['brb'])
            P.fence()
            A.release(m_ph)
        if do_attn:
            PT = [[A.alloc("PT%d_%d" % (m, i), [128, 512], BF16) for i in range(2)] for m in range(2)]
            osb = A.alloc("osb", [128, 2, 4, 130])
            rec = A.alloc("rec", [128, 2, 4])
            o1 = A.alloc("o1", [128, 4, 128])
            o2 = A.alloc("o2", [128, 4, 128])
            ssq = A.alloc("ssq", [128, 4])
            oT = A.alloc("oT", [128, 512])
            def oacc(m, qs):
                i = m * 4 + qs
                return bank[4 + i // 3][:, (i % 3) * 130:(i % 3) * 130 + 130], ('ps', 4 + i // 3)
            qjobs = []
            if ctx_out:
                qjobs.append((0, CTXN, (0, 2), 0))
            for i in range(L // 512):
                qjobs.append((CTXN + i * 512, 512, (0, NKT), (CTXN if ctx_out else 0) + i * 512))
            si = 0
            for (q0, nq, (k0, k1), orow) in qjobs:
                nqs = nq // 128
                started = set()
                def emit_S(kt):
                    for m in range(2):
                        sb_ = kt % 2
                        pb = m * 2 + sb_
                        P.op('pe', lambda e, m=m, kt=kt, q0=q0, nq=nq, pb=pb: e.matmul(
                            bank[pb][:, :nq], lhsT=KT[m * 64:(m + 1) * 64, kt * 128:(kt + 1) * 128],
                            rhs=QT[m * 64:(m + 1) * 64, q0:q0 + nq], start=True, stop=True),
                            r=['QK'], w=[('ps', pb)])
                        P.op('act', lambda e, m=m, sb_=sb_, pb=pb, nq=nq: e.activation(
                            out=PT[m][sb_][:, :nq], in_=bank[pb][:, :nq], func=AF.Exp, scale=0.125),
                            r=[('ps', pb)], w=[('PT', m, sb_)])

                def emit_PV(kt):
                    for m in range(2):
                        sb_ = kt % 2
                        for qs in range(nqs):
                            oap, okey = oacc(m, qs)
                            first_in_bank = (kt == k0) and (okey not in started)
                            started.add(okey)
                            P.op('pe', lambda e, m=m, sb_=sb_, qs=qs, kt=kt, oap=oap, fib=first_in_bank, k1=k1: e.matmul(
                                oap, lhsT=PT[m][sb_][:, qs * 128:(qs + 1) * 128], rhs=V[:, kt, :],
                                start=fib, stop=(kt == k1 - 1)),
                                r=[('PT', m, sb_), 'V', 'Vones'], w=[okey])
                emit_S(k0)
                for kt in range(k0, k1):
                    if kt + 1 < k1:
                        emit_S(kt + 1)
                    emit_PV(kt)
                for m in range(2):
                    for qs in range(nqs):
                        oap, okey = oacc(m, qs)
                        P.op('dve' if (m + qs) % 2 else 'act',
                             (lambda e, m=m, qs=qs, oap=oap: e.tensor_copy(out=osb[:, m, qs, :], in_=oap)) if (m + qs) % 2
                             else (lambda e, m=m, qs=qs, oap=oap: e.copy(out=osb[:, m, qs, :], in_=oap)),
                             r=[okey], w=['osb'])
                P.op('dve', lambda e, nqs=nqs: e.reciprocal(out=rec[:, :, :nqs], in_=osb[:, :, :nqs, 128]),
                     r=['osb'], w=['rec'])
                P.op('dve', lambda e, nqs=nqs: e.tensor_scalar(out=rec[:, 1, :nqs], in0=rec[:, 1, :nqs],
                                                               scalar1=lam_sb[:, 0:1], scalar2=None, op0=ALU.mult),
                     r=['rec', 'lam'], w=['rec'])
                P.op('dve', lambda e, nqs=nqs: e.tensor_tensor(
                    out=o1[:, :nqs, :], in0=osb[:, 0, :nqs, 0:128],
                    in1=rec[:, 0, :nqs].unsqueeze(2).broadcast_to([128, nqs, 128]), op=ALU.mult),
                    r=['osb', 'rec'], w=['o1'])
                P.op('pool', lambda e, nqs=nqs: e.tensor_tensor(
                    out=o2[:, :nqs, :], in0=osb[:, 1, :nqs, 0:128],
                    in1=rec[:, 1, :nqs].unsqueeze(2).broadcast_to([128, nqs, 128]), op=ALU.mult),
                    r=['osb', 'rec'], w=['o2'])
                P.op('dve', lambda e, nqs=nqs: e.tensor_tensor(out=o1[:, :nqs, :], in0=o1[:, :nqs, :], in1=o2[:, :nqs, :],
                                                               op=ALU.add), r=['o1', 'o2'], w=['o1'])
                P.op('pool', lambda e, nqs=nqs: e.tensor_tensor(out=o2[:, :nqs, :], in0=o1[:, :nqs, :], in1=o1[:, :nqs, :],
                                                                op=ALU.mult), r=['o1', 'o2'], w=['o2'])
                P.op('dve', lambda e, nqs=nqs: e.tensor_reduce(out=ssq[:, :nqs], in_=o2[:, :nqs, :], axis=AX.X, op=ALU.add),
                     r=['o2'], w=['ssq'])
                P.op('act', lambda e, nqs=nqs: e.activation(out=ssq[:, :nqs], in_=ssq[:, :nqs], func=AF.Sqrt,
                                                            scale=1.0 / 128, bias=eps_sb[:, 0:1]),
                     r=['ssq', 'eps'], w=['ssq'])
                P.op('dve', lambda e, nqs=nqs: e.reciprocal(out=ssq[:, :nqs], in_=ssq[:, :nqs]), r=['ssq'], w=['ssq'])
                P.op('dve', lambda e, nqs=nqs: e.tensor_tensor(
                    out=o1[:, :nqs, :], in0=o1[:, :nqs, :],
                    in1=ssq[:, :nqs].unsqueeze(2).broadcast_to([128, nqs, 128]), op=ALU.mult),
                    r=['o1', 'ssq'], w=['o1'])
                P.op('pool', lambda e, nqs=nqs: e.tensor_tensor(
                    out=o2[:, :nqs, :], in0=o1[:, :nqs, :],
                    in1=subg_sb[:].unsqueeze(1).broadcast_to([128, nqs, 128]), op=ALU.mult),
                    r=['o1', 'subg', 'o2'], w=['o2'])
                for qs in range(nqs):
                    P.op('pe', lambda e, qs=qs: e.matmul(bank[7][:, qs * 128:(qs + 1) * 128], lhsT=o2[:, qs, :], rhs=ident_sb[:],
                                                         start=True, stop=True), r=['o2', 'identA'], w=[('ps', 7)])
                P.op('act', lambda e, nq=nq: e.copy(out=oT[:, :nq], in_=bank[7][:, :nq]), r=[('ps', 7)], w=['oT'])
                P.dma('sp', brb.dst(1, orow, nq), oT[:, :nq], r=['oT'], w=['brb'])
            P.fence()
        if do_rwkv:
            A.release(base_mark)
            rwkv_phase(nc, P, A, bank, pT, L, ctx_out, (lambda c0, n: brb.dst(2, c0, n)), pfx)
        A.release(base_mark)


NORM_EPS = 1e-6


def phase_B(nc, P, A, bank, pfx, NT_LAT, NT_CTX, x_load, gath, off_lat, x_store, NEXP=32):
    NT = NT_LAT + NT_CTX
    di = lambda name, shape: nc.dram_tensor(pfx + name, list(shape), F32, kind="ExternalInput").ap()
    selq = di("selq", [128, 4])
    cT = di("cT", [128, 8, 2])
    wada = di("wada", [128, 8, 6144])
    bada = di("bada", [128, 48])
    g12 = di("g12", [128, 2, 8])
    wgate = di("wgate", [128, 8, 3072])
    wbr = di("wbr", [128, 12, 1024])
    wout = di("wout", [128, 8, 1024])
    wrt = di("wrt", [128, 8, 36])
    wg = di("wg", [NEXP, 128, 8, 512])
    wu = di("wu", [NEXP, 128, 8, 512])
    wd = di("wd", [NEXP, 128, 4, 1024])
    selE = di("selE", [32, 32 * 128])
    ident = di("ident", [128, 128])
    x2s = nc.dram_tensor(pfx + "x2_scr", [128, 8, NT], F32, kind="Internal").ap()
    xn2s = nc.dram_tensor(pfx + "xn2_scr", [128, 8, NT], BF16, kind="Internal").ap()
    gTs = nc.dram_tensor(pfx + "gT_scr", [32, NT], F32, kind="Internal").ap()
    base_mark = A.mark()

    def TT(eng, out, a, b, op, r, w):
        P.op(eng, lambda e: e.tensor_tensor(out=out, in0=a, in1=b, op=op), r=r, w=w)

    def TS(eng, out, a, s1, s2, op0, op1, r, w):
        if op1 is None:
            P.op(eng, lambda e: e.tensor_scalar(out=out, in0=a, scalar1=s1, scalar2=None, op0=op0), r=r, w=w)
        else:
            P.op(eng, lambda e: e.tensor_scalar(out=out, in0=a, scalar1=s1, scalar2=s2, op0=op0, op1=op1), r=r, w=w)

    def STT(out, a, s, b, op0, op1, r, w):
        P.op('dve', lambda e: e.scalar_tensor_tensor(out=out, in0=a, scalar=s, in1=b, op0=op0, op1=op1), r=r, w=w)

    def ACT(out, a, func, r, w, scale=1.0, bias=None):
        if bias is None:
            P.op('act', lambda e: e.activation(out=out, in_=a, func=func, scale=scale), r=r, w=w)
        else:
            P.op('act', lambda e: e.activation(out=out, in_=a, func=func, scale=scale, bias=bias), r=r, w=w)

    def MM(out, lhsT, rhs, r, w, start=True, stop=True):
        P.op('pe', lambda e: e.matmul(out, lhsT=lhsT, rhs=rhs, start=start, stop=stop), r=r, w=w)

    def RED(out, a, op, r, w):
        P.op('dve', lambda e: e.tensor_reduce(out=out, in_=a, axis=AX.X, op=op), r=r, w=w)

    if True:
        ones = A.alloc("ones", [128, 128])
        selq_sb = A.alloc("selq", [128, 4])
        P.dma('sp', selq_sb[:], selq, w=['selq'])
        eps_sb = A.alloc("eps", [128, 1])
        mod_sb = A.alloc("mod", [128, 48, 2])
        A1 = A.alloc("A1", [128, 8, 2])
        A2 = A.alloc("A2", [128, 8, 2])
        g12_sb = A.alloc("g12", [128, 2, 8])
        ident_sb = A.alloc("ident", [128, 128])
        wrt_sb = A.alloc("wrt", [128, 8, 36])
        P.op('pool', lambda e: e.memset(ones[:], 1.0), w=['ones'])
        P.op('pool', lambda e: e.memset(eps_sb[:], NORM_EPS), w=['eps'])
        P.dma('sp', g12_sb[:], g12, w=['g12'])
        P.dma('sp', ident_sb[:], ident, w=['ident'])
        P.dma('sp', wrt_sb[:], wrt, w=['wrt'])
        mB1 = A.mark()
        c_sb = A.alloc("c", [128, 8, 2])
        s_sb = A.alloc("s", [128, 8, 2])
        bada_sb = A.alloc("bada", [128, 48])
        wgate_b = A.alloc("wgate_b", [128, 8, 3072], BF16)
        wbr_b = A.alloc("wbr_b", [128, 12, 1024], BF16)
        wout_b = A.alloc("wout_b", [128, 8, 1024], BF16)
        xt = [A.alloc("xt%d" % i, [128, 8, 512]) for i in range(2)]
        sq = A.alloc("sq", [128, 8, 512])
        rstd = A.alloc("rstd", [128, 512])
        xn1 = A.alloc("xn1", [128, 8, 512], BF16)
        brf = A.alloc("brf", [128, 4, 512])
        gTt = A.alloc("gTt", [32, 512])
        brb = A.alloc("brb", [128, 12, 512], BF16)
        sig = [A.alloc("sig%d" % i, [128, 512]) for i in range(2)]
        mrg = A.alloc("mrg", [128, 2, 512])
        mk_ = A.mark()
        mrgb = A.alloc("mrgb", [128, 8, 512], BF16)
        A.release(mk_)
        brsel = A.alloc("brsel", [128, 4, 512])
        x2 = A.alloc("x2", [128, 8, 512])
        rt = A.alloc("rt", [128, 80])
        g32 = A.alloc("g32", [128, 32])
        P.dma('sp', c_sb[:], cT, w=['c_sb'])
        P.dma('sp', bada_sb[:], bada, w=['bada'])
        ACT(s_sb[:], c_sb[:], AF.Silu, ['c_sb'], ['s_sb'])
        for piece in range(12):
            b = piece % 2
            P.dma('sp', xt[b][:], wada[:, :, piece * 512:(piece + 1) * 512], w=[('xt', b)])
            for occ in range(4):
                oc = piece * 4 + occ
                for k in range(8):
                    MM(bank[0][:, oc * 2:oc * 2 + 2], xt[b][:, k, occ * 128:(occ + 1) * 128], s_sb[:, k, :],
                       [('xt', b), 's_sb'], [('ps', 0)], start=(k == 0), stop=(k == 7))
        TT('dve', mod_sb[:], bank[0][:, 0:96].rearrange("p (a b) -> p a b", b=2),
           bada_sb[:].unsqueeze(2).broadcast_to([128, 48, 2]), ALU.add, [('ps', 0), 'bada'], ['mod'])
        for (Ax, m_scale, gi) in ((A1, 1, 0), (A2, 4, 1)):
            TS('dve', Ax[:], mod_sb[:, m_scale * 8:(m_scale + 1) * 8, :], 1.0, None, ALU.add, None, ['mod'], ['Ax%d' % gi])
            TT('dve', Ax[:], Ax[:], g12_sb[:, gi, :].unsqueeze(2).broadcast_to([128, 8, 2]), ALU.mult, ['Ax%d' % gi, 'g12'], ['Ax%d' % gi])
        wi = 0
        for (src, dstw, nk, ncol, key) in ((wgate, wgate_b, 8, 3072, 'wgate_b'), (wbr, wbr_b, 12, 1024, 'wbr_b'), (wout, wout_b, 8, 1024, 'wout_b')):
            for c0 in range(0, ncol, 512):
                for kb in range(0, nk, 8):
                    kn = min(8, nk - kb)
                    b = wi % 2
                    wi += 1
                    P.dma('sp', xt[b][:, :kn, :], src[:, kb:kb + kn, c0:c0 + 512], w=[('xt', b)])
                    P.op('pool' if wi % 2 else 'dve', lambda e, b=b, kn=kn, kb=kb, c0=c0, dstw=dstw: e.tensor_copy(
                        out=dstw[:, kb:kb + kn, c0:c0 + 512], in_=xt[b][:, :kn, :]), r=[('xt', b)], w=[key])
        tiles = [(i * 512, 512, 0) for i in range(NT_LAT // 512)]
        if NT_CTX:
            tiles.append((NT_LAT, NT_CTX, 1))

        def norm_mod(src, sz, j, Ax, m_shift, out_fn, okeys, srckeys):
            P.op('act', lambda e: e.activation(out=sq[:, :, :sz], in_=src[:, :, :sz], func=AF.Square), r=srckeys, w=['sq'])
            for k in range(8):
                MM(bank[0][:, :sz], ones[:], sq[:, k, :sz], ['ones', 'sq'], [('ps', 0)], start=(k == 0), stop=(k == 7))
            ACT(rstd[:, :sz], bank[0][:, :sz], AF.Sqrt, [('ps', 0), 'eps'], ['rstd'], scale=1.0 / 1024, bias=eps_sb[:, 0:1])
            P.op('dve', lambda e: e.reciprocal(out=rstd[:, :sz], in_=rstd[:, :sz]), r=['rstd'], w=['rstd'])
            TT('dve', sq[:, :, :sz], src[:, :, :sz], rstd[:, :sz].unsqueeze(1).broadcast_to([128, 8, sz]), ALU.mult,
               srckeys + ['rstd', 'sq'], ['sq'])
            for k in range(8):
                TS('dve' if k % 2 else 'pool', out_fn(k), sq[:, k, :sz], Ax[:, k, j:j + 1],
                   mod_sb[:, m_shift * 8 + k, j:j + 1], ALU.mult, ALU.add, ['sq', 'Ax0', 'Ax1', 'mod'], okeys)

        pi = 0
        for ti, (t0, sz, j) in enumerate(tiles):
            b = ti % 2
            x_load(xt[b], t0, sz, ('xt', b))
            for n3 in range(3):
                for q in range(4):
                    col0 = (off_lat + q * NT_LAT + t0) if j == 0 else (q * 64 + (t0 - NT_LAT))
                    P.dma('sp', brf[:, :, :sz], gath.gsrc(n3, col0, sz), r=['gath'], w=['brf'])
                    if q == 0:
                        TS('dve', brsel[:, :, :sz], brf[:, :, :sz], selq_sb[:, 0:1], None, ALU.mult, None, ['brf', 'selq'], ['mrgb'])
                    else:
                        STT(brsel[:, :, :sz], brf[:, :, :sz], selq_sb[:, q:q + 1], brsel[:, :, :sz], ALU.mult, ALU.add,
                            ['brf', 'selq', 'mrgb'], ['mrgb'])
                P.op('pool', lambda e, sz=sz, n3=n3: e.tensor_copy(out=brb[:, n3 * 4:(n3 + 1) * 4, :sz], in_=brsel[:, :, :sz]),
                     r=['mrgb'], w=['brb'])
            norm_mod(xt[b], sz, j, A1, 0, lambda k, sz=sz: xn1[:, k, :sz], ['xn1'], [('xt', b)])
            for dc in range(8):
                for n in range(3):
                    pg = bank[1 + pi % 2]
                    pgk = ('ps', 1 + pi % 2)
                    pbk = bank[3 + pi % 2]
                    pbkk = ('ps', 3 + pi % 2)
                    sg_ = sig[pi % 2]
                    sgk = ('sig', pi % 2)
                    pi += 1
                    for k in range(8):
                        MM(pg[:, :sz], wgate_b[:, k, n * 1024 + dc * 128:n * 1024 + (dc + 1) * 128], xn1[:, k, :sz],
                           ['wgate_b', 'xn1'], [pgk], start=(k == 0), stop=(k == 7))
                    for kc in range(4):
                        MM(pbk[:, :sz], wbr_b[:, n * 4 + kc, dc * 128:(dc + 1) * 128], brb[:, n * 4 + kc, :sz],
                           ['wbr_b', 'brb'], [pbkk], start=(kc == 0), stop=(kc == 3))
                    ACT(sg_[:, :sz], pg[:, :sz], AF.Sigmoid, [pgk], [sgk])
                    if n == 0:
                        TT('dve', mrg[:, dc % 2, :sz], sg_[:, :sz], pbk[:, :sz], ALU.mult, [sgk, pbkk], ['mrg'])
                    else:
                        TT('dve', sg_[:, :sz], sg_[:, :sz], pbk[:, :sz], ALU.mult, [sgk, pbkk], [sgk])
                        TT('pool', mrg[:, dc % 2, :sz], mrg[:, dc % 2, :sz], sg_[:, :sz], ALU.add, ['mrg', sgk], ['mrg'])
                P.op('act', lambda e, dc=dc, sz=sz: e.copy(out=mrgb[:, dc, :sz], in_=mrg[:, dc % 2, :sz]), r=['mrg'], w=['mrgb'])
            for dc in range(8):
                pb_ = bank[5 + dc % 2]
                pk_ = ('ps', 5 + dc % 2)
                for k in range(8):
                    MM(pb_[:, :sz], wout_b[:, k, dc * 128:(dc + 1) * 128], mrgb[:, k, :sz], ['wout_b', 'mrgb'], [pk_],
                       start=(k == 0), stop=(k == 7))
                STT(x2[:, dc, :sz], pb_[:, :sz], mod_sb[:, 2 * 8 + dc, j:j + 1], xt[b][:, dc, :sz], ALU.mult, ALU.add,
                    [pk_, 'mod', ('xt', b)], ['x2'])
            P.dma('pool', x2s[:, :, t0:t0 + sz], x2[:, :, :sz], r=['x2'], w=['x2s'])
            xn2f = xt[b]
            xfk = ('xt', b)
            norm_mod(x2, sz, j, A2, 3, lambda k, sz=sz, xn2f=xn2f: xn2f[:, k, :sz], [xfk], ['x2'])
            P.op('act', lambda e, sz=sz, xn2f=xn2f: e.copy(out=xn1[:, :, :sz], in_=xn2f[:, :, :sz]), r=[xfk], w=['xn1'])
            P.dma('pool', xn2s[:, :, t0:t0 + sz], xn1[:, :, :sz], r=['xn1'], w=['xn2s'])
            for s0 in range(0, sz, 128):
                ns = min(128, sz - s0)
                for k in range(8):
                    MM(bank[7][:ns, 0:36], xn2f[:, k, s0:s0 + ns], wrt_sb[:, k, :], [xfk, 'wrt'], [('ps', 7)],
                       start=(k == 0), stop=(k == 7))
                lg = rt[:ns, 0:36]
                P.op('dve', lambda e, ns=ns: e.tensor_copy(out=rt[:ns, 0:36], in_=bank[7][:ns, 0:36]), r=[('ps', 7)], w=['rt'])
                gmax = rt[:ns, 36:37]
                RED(gmax, rt[:ns, 0:4], ALU.max, ['rt'], ['rt'])
                ohg = rt[:ns, 37:41]
                TS('dve', ohg, rt[:ns, 0:4], gmax, None, ALU.is_equal, None, ['rt'], ['rt'])
                ngm = rt[:ns, 41:42]
                TS('dve', ngm, gmax, -1.0, None, ALU.mult, None, ['rt'], ['rt'])
                eg = rt[:ns, 42:46]
                ACT(eg, rt[:ns, 0:4], AF.Exp, ['rt'], ['rt'], bias=ngm)
                pgr = rt[:ns, 46:47]
                RED(pgr, eg, ALU.add, ['rt'], ['rt'])
                P.op('dve', lambda e, pgr=pgr: e.reciprocal(out=pgr, in_=pgr), r=['rt'], w=['rt'])
                TT('dve', g32[:ns, :].rearrange("p (g e) -> p g e", e=8), rt[:ns, 4:36].rearrange("p (g e) -> p g e", e=8),
                   ohg.unsqueeze(2).broadcast_to([ns, 4, 8]), ALU.mult, ['rt'], ['g32'])
                les = rt[:ns, 47:55]
                RED(les, g32[:ns, :].rearrange("p (g e) -> p e g", e=8), ALU.add, ['g32'], ['rt'])
                top1 = rt[:ns, 55:56]
                RED(top1, les, ALU.max, ['rt'], ['rt'])
                oh1 = rt[:ns, 56:64]
                TS('dve', oh1, les, top1, None, ALU.is_equal, None, ['rt'], ['rt'])
                le2 = rt[:ns, 64:72]
                STT(le2, oh1, -1e30, les, ALU.mult, ALU.add, ['rt'], ['rt'])
                top2 = rt[:ns, 72:73]
                RED(top2, le2, ALU.max, ['rt'], ['rt'])
                oh2 = rt[:ns, 73:81] if False else None
                d12 = rt[:ns, 41:42]
                TT('dve', d12, top1, top2, ALU.subtract, ['rt'], ['rt'])
                ga = rt[:ns, 42:43]
                gb = rt[:ns, 43:44]
                ACT(ga, d12, AF.Sigmoid, ['rt'], ['rt'])
                ACT(gb, d12, AF.Sigmoid, ['rt'], ['rt'], scale=-1.0)
                TT('dve', rt[:ns, 42:44], rt[:ns, 42:44], pgr.broadcast_to([ns, 2]), ALU.mult, ['rt'], ['rt'])
                TS('dve', le2, le2, top2, gb, ALU.is_equal, ALU.mult, ['rt'], ['rt'])
                STT(les, oh1, ga, le2, ALU.mult, ALU.add, ['rt'], ['rt'])
                TT('dve', g32[:ns, :].rearrange("p (g e) -> p g e", e=8), ohg.unsqueeze(2).broadcast_to([ns, 4, 8]),
                   les.unsqueeze(1).broadcast_to([ns, 4, 8]), ALU.mult, ['rt', 'g32'], ['g32'])
                MM(bank[7][0:32, 64:64 + ns], g32[:ns, :], ident_sb[:ns, :ns], ['g32', 'ident'], [('ps', 7)])
                P.op('act', lambda e, s0=s0, ns=ns: e.copy(out=gTt[:, s0:s0 + ns], in_=bank[7][0:32, 64:64 + ns]),
                     r=[('ps', 7)], w=['gTt'])
            P.dma('pool', gTs[:, t0:t0 + sz], gTt[:, :sz], r=['gTt'], w=['gTs'])
        P.fence()
        A.release(mB1)
        selE_sb = A.alloc("selE", [32, 32 * 128])
        P.dma('sp', selE_sb[:], selE, w=['selE'])
        lat_ = tiles[:NT_LAT // 512]
        groups = [lat_[i:i + 2] for i in range(0, len(lat_), 2)]
        if NT_CTX:
            groups[-1] = groups[-1] + [tiles[-1]]
        GMAX = max(sum(t[1] for t in g) for g in groups)
        yacc = A.alloc("yacc", [128, 8, GMAX])
        xn2 = A.alloc("xn2g", [128, 8, GMAX], BF16)
        gT = A.alloc("gTg", [32, GMAX])
        wst = [A.alloc("wst%d" % i, [128, 4, 512]) for i in range(4)]
        wgb = [A.alloc("wgb%d" % i, [128, 8, 512], BF16) for i in range(2)]
        wub = [A.alloc("wub%d" % i, [128, 8, 512], BF16) for i in range(2)]
        wdb = [A.alloc("wdb%d" % i, [128, 4, 1024], BF16) for i in range(2)]
        hs = [A.alloc("hs%d" % i, [128, 512]) for i in range(2)]
        actb = [A.alloc("actb%d" % i, [128, 4, 512], BF16) for i in range(2)]
        x2r = A.alloc("x2r", [128, 8, 512])
        si = 0
        ai = 0
        hi_ = 0
        for gi, grp in enumerate(groups):
            g0 = grp[0][0]
            gsz = sum(t[1] for t in grp)
            P.dma('sp', xn2[:, :, :gsz], xn2s[:, :, g0:g0 + gsz], r=['xn2s'], w=['xn2g'])
            P.dma('sp', gT[:, :gsz], gTs[:, g0:g0 + gsz], r=['gTs'], w=['gTg'])
            for e_ in range(NEXP):
                wb = e_ % 2
                pieces = [(wg[e_, :, 0:4, :], wgb[wb][:, 0:4, :], ('wgb', wb)), (wg[e_, :, 4:8, :], wgb[wb][:, 4:8, :], ('wgb', wb)),
                          (wu[e_, :, 0:4, :], wub[wb][:, 0:4, :], ('wub', wb)), (wu[e_, :, 4:8, :], wub[wb][:, 4:8, :], ('wub', wb)),
                          (wd[e_, :, :, 0:512], wdb[wb][:, :, 0:512], ('wdb', wb)), (wd[e_, :, :, 512:1024], wdb[wb][:, :, 512:1024], ('wdb', wb))]
                for (src, dst, key) in pieces:
                    sb_ = si % 4
                    si += 1
                    P.dma('sp', wst[sb_][:], src, w=[('wst', sb_)])
                    eng = ('pool', 'dve', 'act')[si % 3] if False else ('pool' if si % 2 else 'act')
                    if eng == 'act':
                        P.op('act', lambda e, dst=dst, sb_=sb_: e.copy(out=dst, in_=wst[sb_][:]), r=[('wst', sb_)], w=[key])
                    else:
                        P.op('pool', lambda e, dst=dst, sb_=sb_: e.tensor_copy(out=dst, in_=wst[sb_][:]), r=[('wst', sb_)], w=[key])
                for (t0, sz, j) in grp:
                    ti = tiles.index((t0, sz, j))
                    lo = t0 - g0
                    MM(bank[0][:, :sz], selE_sb[:, e_ * 128:(e_ + 1) * 128], gT[:, lo:lo + sz], ['selE', 'gTg'], [('ps', 0)])
                    ab = actb[ai % 2]
                    ak = ('actb', ai % 2)
                    ai += 1
                    for fc in range(4):
                        pgb = bank[1 + fc % 2]
                        pgk = ('ps', 1 + fc % 2)
                        pub = bank[3 + fc % 2]
                        puk = ('ps', 3 + fc % 2)
                        for k in range(8):
                            MM(pgb[:, :sz], wgb[wb][:, k, fc * 128:(fc + 1) * 128], xn2[:, k, lo:lo + sz],
                               [('wgb', wb), 'xn2g'], [pgk], start=(k == 0), stop=(k == 7))
                        for k in range(8):
                            MM(pub[:, :sz], wub[wb][:, k, fc * 128:(fc + 1) * 128], xn2[:, k, lo:lo + sz],
                               [('wub', wb), 'xn2g'], [puk], start=(k == 0), stop=(k == 7))
                        h_ = hs[hi_ % 2]
                        hk = ('hs', hi_ % 2)
                        hi_ += 1
                        ACT(h_[:, :sz], pgb[:, :sz], AF.Silu, [pgk], [hk])
                        TT('dve', h_[:, :sz], h_[:, :sz], pub[:, :sz], ALU.mult, [hk, puk], [hk])
                        TT('dve', ab[:, fc, :sz], h_[:, :sz], bank[0][:, :sz], ALU.mult, [hk, ('ps', 0)], [ak])
                    for dc in range(8):
                        pdb = bank[5 + dc % 3]
                        pdk = ('ps', 5 + dc % 3)
                        for fc in range(4):
                            MM(pdb[:, :sz], wdb[wb][:, fc, dc * 128:(dc + 1) * 128], ab[:, fc, :sz], [('wdb', wb), ak], [pdk],
                               start=(fc == 0), stop=(fc == 3))
                        if e_ == 0:
                            P.op('act', lambda e, dc=dc, lo=lo, sz=sz, pdb=pdb: e.copy(out=yacc[:, dc, lo:lo + sz], in_=pdb[:, :sz]),
                                 r=[pdk], w=['yacc'])
                        else:
                            TT('pool' if False else 'dve', yacc[:, dc, lo:lo + sz], yacc[:, dc, lo:lo + sz], pdb[:, :sz], ALU.add,
                               ['yacc', pdk], ['yacc'])
            for (t0, sz, j) in grp:
                lo = t0 - g0
                P.dma('sp', x2r[:, :, :sz], x2s[:, :, t0:t0 + sz], r=['x2s'], w=['x2r'])
                for dc in range(8):
                    STT(x2r[:, dc, :sz], yacc[:, dc, lo:lo + sz], mod_sb[:, 5 * 8 + dc, j:j + 1], x2r[:, dc, :sz], ALU.mult, ALU.add,
                        ['yacc', 'mod', 'x2r'], ['x2r'])
                x_store(x2r, t0, sz, ['x2r'])
        P.fence()
        A.release(base_mark)


POOL_WINDOWS = (2, 4, 8, 16)
def fm(a):
    return np.ascontiguousarray(a.reshape(8, 128, *a.shape[1:]).swapaxes(0, 1))
def colsel(hg):
    c = []
    c += list(range(hg * 128, hg * 128 + 128))
    c += list(range(512 + hg * 128, 512 + hg * 128 + 128))
    c += list(range(1024 + hg * 128, 1024 + hg * 128 + 128))
    c += list(range(1536 + hg * 128, 1536 + hg * 128 + 128))
    for j in range(3):
        c += list(range(2048 + j * 512 + hg * 128, 2048 + j * 512 + hg * 128 + 128))
    c += list(range(2048 + 1536, 2048 + 1920))
    return np.array(c)
def rope_tables(L):
    nrow = L // 64
    inv = (10000.0 ** (-np.arange(16, dtype=np.float32) / 16)).astype(np.float32)
    R = np.ones((128, 2, nrow), np.float32); C = np.ones((128, 2, 64), np.float32)
    perm = np.zeros((128, 128), np.float32)
    rows = np.arange(nrow, dtype=np.float32); cols = np.arange(64, dtype=np.float32)
    for p in range(128):
        d = p % 64
        blk = d // 16
        f = inv[d % 16]
        sign = -1.0 if blk % 2 == 0 else 1.0
        partner = p + 16 if blk % 2 == 0 else p - 16
        perm[partner, p] = 1.0
        if blk < 2:
            ang = (rows * f).astype(np.float32)
            R[p, 0] = np.cos(ang); R[p, 1] = sign * np.sin(ang)
        else:
            ang = (cols * f).astype(np.float32)
            C[p, 0] = np.cos(ang); C[p, 1] = sign * np.sin(ang)
    return R, C, perm
def edge_fix(w, L):
    t = np.arange(L)
    lo = np.clip(t - w // 2, 0, L - 1); hi = np.clip(t + w // 2 - 1, 0, L - 1)
    ratio = (w / (hi - lo + 1)).astype(np.float32)
    return np.concatenate([ratio[:8], ratio[-8:]])
def inputs_A(inp, l, b, hg, L, lam_init, x=None, ctx=None):
    if x is None:
        x = inp['x'][b, :L]; ctx = inp['ctx'][b]
    xa = np.concatenate([ctx, x], 0)
    R, C, perm = rope_tables(L)
    cs = colsel(hg)
    w = POOL_WINDOWS[hg]
    selw = np.zeros((128, 4), np.float32); selw[:, hg] = 1.0 / w
    d = dict(
        xT=fm(np.ascontiguousarray(xa.T)),
        cT=fm(np.stack([inp['c'][b], inp['c_ctx']], 1)),
        wada=fm(inp['w_ada'][l][:, :2048]),
        bada=np.ascontiguousarray(inp['b_ada'][l][:2048].reshape(16, 128).T),
        g1=np.ascontiguousarray(inp['norm1_g'][l].reshape(8, 128).T),
        win=fm(inp['w_in'][l][:, cs]),
        qkg=np.stack([np.tile(inp['q_norm_g'][l], 2), np.tile(inp['k_norm_g'][l], 2)], 1).astype(np.float32),
        ropeR=R, ropeC=C, perm=perm,
        lamqk=np.ascontiguousarray(np.broadcast_to(inp['lambda_qk'][l].reshape(1, 256), (128, 256))),
        subg=np.ascontiguousarray(np.broadcast_to((inp['subln_g'][l] * np.float32(1 - lam_init)).reshape(1, 128), (128, 128))).astype(np.float32),
        wpool=np.ascontiguousarray(inp['pool_w'][l][hg]),
        pscale=np.ascontiguousarray(inp['pool_scale'][l][hg * 128:(hg + 1) * 128].reshape(128, 1)),
        selw=selw,
        efix=np.ascontiguousarray(np.broadcast_to(edge_fix(w, L).reshape(1, 16), (128, 16))),
    )
    return d

def rw_consts():
    i = np.arange(64)
    incl = (i[:, None] <= i[None, :]).astype(np.float32)
    strict = (i[:, None] < i[None, :]).astype(np.float32)
    ones = np.ones((64, 64), np.float32)
    MkN = (i[None, :] < i[:, None]).astype(np.float32)
    return np.concatenate([incl, strict, ones, strict, incl, MkN, np.eye(64, dtype=np.float32)], 1)
def inputs_rw(inp, l, hg):
    mu = inp['shift_mu'][l]
    cmu = np.zeros((128, 6, 2), np.float32)
    p = np.arange(128)
    for g in range(3):
        cmu[:, g, :] = mu[:, g * 512 + hg * 128 + p].T
    for g, base in ((3, 1536), (4, 1664), (5, 1792)):
        cmu[:, g, :] = mu[:, base + p].T
    heads = [2 * hg, 2 * hg + 1]
    w2 = np.zeros((64, 2, 2, 64), np.float32); a2 = np.zeros((64, 2, 2, 64), np.float32)
    w0b = np.zeros((2, 2, 64), np.float32); a0f = np.zeros((64, 2, 2), np.float32)
    prm = np.zeros((64, 2, 5), np.float32)
    for h, hd in enumerate(heads):
        cs = slice(hd * 64, hd * 64 + 64)
        for d in range(2):
            w2[:, d, h, :] = inp['decay_w2'][l][d][:, cs]
            a2[:, d, h, :] = inp['aaa_a2'][l][d][:, cs]
            w0b[d, h, :] = inp['decay_w0'][l][d][cs]
            a0f[:, d, h] = inp['aaa_a0'][l][d][cs]
        prm[:, h, 0] = inp['k_k'][l][cs]; prm[:, h, 1] = inp['k_a'][l][cs]; prm[:, h, 2] = inp['r_k'][l][hd]
        prm[:, h, 3] = inp['gn_w'][l][cs]; prm[:, h, 4] = inp['gn_b'][l][cs]
    return dict(rw_cmu=cmu, rw_g2=np.ascontiguousarray(inp['gate_w2'][l][:, hg * 128:(hg + 1) * 128]),
                rw_w2=w2.reshape(64, 256), rw_a2=a2.reshape(64, 256),
                rw_w0b=np.ascontiguousarray(np.broadcast_to(w0b.reshape(1, 256), (64, 256))),
                rw_a0f=a0f.reshape(64, 4), rw_prm=prm, rw_cst=rw_consts())


def weights_B(inp, l):
    selE = np.zeros((32, 32, 128), np.float32)
    for e in range(32): selE[e, e, :] = 1.0
    return dict(
        wada=fm(inp['w_ada'][l]),
        bada=np.ascontiguousarray(inp['b_ada'][l].reshape(48, 128).T),
        g12=np.ascontiguousarray(np.stack([inp['norm1_g'][l].reshape(8, 128).T, inp['norm2_g'][l].reshape(8, 128).T], 1)),
        wgate=fm(inp['w_in'][l][:, 3968:]),
        wbr=np.ascontiguousarray(inp['w_br'][l].reshape(12, 128, 1024).transpose(1, 0, 2)),
        wout=fm(inp['w_out'][l]),
        wrt=fm(np.concatenate([inp['w_router_group'][l], inp['w_router_expert'][l]], 1)),
        wg=np.ascontiguousarray(inp['w_exp_gate'][l].reshape(32, 8, 128, 512).transpose(0, 2, 1, 3)),
        wu=np.ascontiguousarray(inp['w_exp_up'][l].reshape(32, 8, 128, 512).transpose(0, 2, 1, 3)),
        wd=np.ascontiguousarray(inp['w_exp_down'][l].reshape(32, 4, 128, 1024).transpose(0, 2, 1, 3)),
        selE=selE.reshape(32, 4096), ident=np.eye(128, dtype=np.float32))
def acts_B(xa, br, cvec, c_ctx):
    NT = xa.shape[0]
    return dict(xT=fm(np.ascontiguousarray(xa.T)),
                brT=np.ascontiguousarray(br.T.reshape(12, 128, NT).transpose(1, 0, 2)),
                cT=fm(np.stack([cvec, c_ctx], 1)))


GROUPS = [[0, 1, 2, 3], [4, 5, 6, 7]]


CC_COLS = 2048


class BrStore:
    def __init__(self, nc, name, NQ, ctx_out):
        self.splits = ([(0, 256)] if ctx_out else []) + [(c0, min(CC_COLS, NQ - c0)) for c0 in range(256 if ctx_out else 0, NQ, CC_COLS)]
        self.b = {}
        self.g = {}
        for n in range(3):
            for ci, (c0, cs) in enumerate(self.splits):
                self.b[(n, ci)] = nc.dram_tensor("%s_b%d_%d" % (name, n, ci), [128, cs], F32, kind="Internal").ap()
                self.g[(n, ci)] = nc.dram_tensor("%s_g%d_%d" % (name, n, ci), [4 * 128, cs], F32, kind="Internal").ap()

    def _find(self, col0, ncols):
        for ci, (c0, cs) in enumerate(self.splits):
            if c0 <= col0 and col0 + ncols <= c0 + cs:
                return ci, col0 - c0
        raise AssertionError(("chunk straddle", col0, ncols))

    def dst(self, n, col0, ncols):
        ci, o = self._find(col0, ncols)
        return self.b[(n, ci)][:, o:o + ncols]

    def gsrc(self, n, col0, ncols):
        ci, o = self._find(col0, ncols)
        return self.g[(n, ci)].rearrange("(g p) t -> p g t", g=4)[:, :, o:o + ncols]

    def exchange(self, P):
        for key in self.b:
            P.cc(self.b[key], self.g[key], GROUPS, r=['brb'], w=['gath'])


class XStore:
    def __init__(self, nc, name, NL):
        self.NL = NL
        NT0 = NL + 64
        self.splits = [(c0, min(CC_COLS, NL - c0)) for c0 in range(0, NL, CC_COLS)] + [(NL, 64)]
        self.b = {}
        self.g = {}
        for k in range(8):
            for ci, (c0, cs) in enumerate(self.splits):
                self.b[(k, ci)] = nc.dram_tensor("%s_b%d_%d" % (name, k, ci), [128, cs], F32, kind="Internal").ap()
                self.g[(k, ci)] = nc.dram_tensor("%s_g%d_%d" % (name, k, ci), [4 * 128, cs], F32, kind="Internal").ap()

    def _find(self, col0, ncols):
        for ci, (c0, cs) in enumerate(self.splits):
            if c0 <= col0 and col0 + ncols <= c0 + cs:
                return ci, col0 - c0
        raise AssertionError(("chunk straddle", col0, ncols))

    def store(self, P, src_tile, t0, sz, rkeys):
        ci, o = self._find(t0, sz)
        for k in range(8):
            P.dma('pool', self.b[(k, ci)][:, o:o + sz], src_tile[:, k, :sz], r=rkeys, w=['xnew'])

    def load_local(self, P, dst_tile, t0, sz, key):
        ci, o = self._find(t0, sz)
        for k in range(8):
            P.dma('sp', dst_tile[:, k, :sz], self.b[(k, ci)][:, o:o + sz], r=['xnew'], w=[key])

    def load_gathered(self, P, dst_tile, dcol, rank, t0, sz, key):
        ci, o = self._find(t0, sz)
        for k in range(8):
            P.dma('sp', dst_tile[:, k, dcol:dcol + sz], self.g[(k, ci)][rank * 128:(rank + 1) * 128, o:o + sz], r=['xg'], w=[key])

    def exchange(self, P):
        for key in self.b:
            P.cc(self.b[key], self.g[key], GROUPS, r=['xnew'], w=['xg'])


def build_fused(L=16384, nexp=32, stop_after=99):
    nc = bass.Bass("TRN2", target_bir_lowering=False)
    nc.allow_low_precision("bf16 matmul operands, fp32 accumulation")
    P = Prog(nc)
    A = Arena(nc)
    T = 256 + L
    NL = L // 4
    NT0 = NL + 64
    lam = [0.8 - 0.6 * math.exp(-0.3 * l) for l in range(2)]
    with contextlib.ExitStack() as st:
        bank = [st.enter_context(nc.psum_tensor("bank%d" % i, [128, 512], F32)) for i in range(8)]
        xout = nc.dram_tensor("xout", [128, 8, NL], F32, kind="ExternalOutput").ap()

        def finish():
            stats = P.emit()
            stats['sbuf_peak'] = A.peak
            return nc, stats
        br0 = BrStore(nc, "br0", T, True)
        phase_A(nc, P, A, bank, "A0_", L, True, lam[0], None, br0)
        P.fence()
        if stop_after == 1:
            return finish()
        br0.exchange(P)
        P.fence()
        if stop_after == 2:
            return finish()
        xsh = nc.dram_tensor("xsh", [128, 8, NT0], F32, kind="ExternalInput").ap()
        xs = XStore(nc, "xs", NL)

        def x_load0(dst, t0, sz, key):
            P.dma('sp', dst[:, :, :sz], xsh[:, :, t0:t0 + sz], w=[key])
        phase_B(nc, P, A, bank, "B0_", NL, 64, x_load0, br0, 256, lambda src, t0, sz, rk: xs.store(P, src, t0, sz, rk), NEXP=nexp)
        if stop_after == 3:
            return finish()
        xs.exchange(P)
        P.fence()
        if stop_after == 4:
            return finish()

        def x_loader(dst, t0, sz, key):
            if t0 < 256:
                for r in range(4):
                    xs.load_gathered(P, dst, r * 64, r, NL, 64, key)
            else:
                tt = t0 - 256
                xs.load_gathered(P, dst, 0, tt // NL, tt % NL, sz, key)
        br1 = BrStore(nc, "br1", L, False)
        phase_A(nc, P, A, bank, "A1_", L, False, lam[1], x_loader, br1)
        P.fence()
        if stop_after == 5:
            return finish()
        br1.exchange(P)
        P.fence()
        if stop_after == 6:
            return finish()

        def x_store1(src, t0, sz, rk):
            P.dma('pool', xout[:, :, t0:t0 + sz], src[:, :, :sz], r=rk, final=True)
        phase_B(nc, P, A, bank, "B1_", NL, 0, lambda dst, t0, sz, key: xs.load_local(P, dst, t0, sz, key), br1, 0, x_store1, NEXP=nexp)
        return finish()


def fused_inputs(inp, L, nexp=32, names=None):
    inp = {k: np.asarray(v) for k, v in inp.items()}
    NL = L // 4
    x = inp['x'][:, :L]
    ctx = inp['ctx']
    lam = [0.8 - 0.6 * math.exp(-0.3 * l) for l in range(2)]
    WB = [weights_B(inp, l) for l in range(2)]
    ident = np.eye(128, dtype=np.float32)
    maps = []
    for i in range(8):
        b, hg = i // 4, i % 4
        d = {}
        for l in range(2):
            a = inputs_A(inp, l, b, hg, L, lam[l], x=x[b], ctx=ctx[b])
            if l == 1:
                a.pop('xT')
            a.update(inputs_rw(inp, l, hg))
            a['identA'] = ident
            for k, v in a.items():
                d["A%d_" % l + k] = v
            for k, v in WB[l].items():
                d["B%d_" % l + k] = v[:nexp] if k in ('wg', 'wu', 'wd') else v
            d["B%d_cT" % l] = fm(np.stack([inp['c'][b], inp['c_ctx']], 1))
            selq = np.zeros((128, 4), np.float32)
            selq[:, hg] = 1.0
            d["B%d_selq" % l] = selq
        xa = np.concatenate([x[b, hg * NL:(hg + 1) * NL], ctx[b, hg * 64:(hg + 1) * 64]], 0)
        d['xsh'] = fm(np.ascontiguousarray(xa.T))
        if names is not None:
            d = {k: v for k, v in d.items() if k in names}
        maps.append(d)
    return maps


def fused_gather(results, L):
    NL = L // 4
    out = np.empty((2, L, 1024), np.float32)
    for i in range(8):
        b, q = i // 4, i % 4
        o = np.asarray(results[i]['xout']).transpose(1, 0, 2).reshape(1024, NL).T
        out[b, q * NL:(q + 1) * NL] = o
    return out


def kernel(**inp):
    inp = {k: np.asarray(v) for k, v in inp.items()}
    L = inp['x'].shape[1]
    nc, _ = build_fused(L)
    maps = fused_inputs(inp, L)
    res = run_bass_kernel_spmd(nc, maps, core_ids=list(range(8)))
    del maps
    return fused_gather(res.results, L)
```

```python
import math
import contextlib


import numpy as np
import concourse.bass as bass
import concourse.mybir as mybir
from concourse.bass_utils import run_bass_kernel_spmd

F32 = mybir.dt.float32
BF16 = mybir.dt.bfloat16
I32 = mybir.dt.int32
AF = mybir.ActivationFunctionType
ALU = mybir.AluOpType
AX = mybir.AxisListType

SEM_LIMIT = 8000
DMA_POOL = 40


class Prog:
    def __init__(self, nc):
        self.nc = nc
        self.ops = []

    def op(self, eng, fn, r=(), w=()):
        self.ops.append(dict(eng=eng, fn=fn, r=tuple(r), w=tuple(w), dma=False, final=False))

    def dma(self, eng, out, in_, r=(), w=(), final=False, **kw):
        def fn(e, out=out, in_=in_, kw=kw):
            return e.dma_start(out=out, in_=in_, **kw)
        self.ops.append(dict(eng=eng, fn=fn, r=tuple(r), w=tuple(w), dma=True, final=final))

    def cc(self, ins_ap, out_ap, groups, r=(), w=()):
        def fn(e, ins_ap=ins_ap, out_ap=out_ap, groups=groups):
            return e.collective_compute("AllGather", ALU.bypass, replica_groups=groups, ins=[ins_ap], outs=[out_ap])
        self.ops.append(dict(eng='pool', fn=fn, r=tuple(r), w=tuple(w), dma=True, final=False, cc=True))

    def fence(self):
        self.ops.append(dict(eng=None, fn=None, r=(), w=(), dma=False, final=False, fence=True))

    def emit(self):
        nc = self.nc
        raw_ops = self.ops
        ops = []
        fence_after = {}
        fence_pos = []
        for o in raw_ops:
            if o.get('fence'):
                fence_pos.append(len(ops))
            else:
                ops.append(o)
        self.ops = ops
        n = len(ops)
        fence_deps_at = {}
        prev = 0
        for fp in fence_pos:
            last = {}
            dm = set()
            for i in range(prev, fp):
                o = ops[i]
                if o['dma']:
                    dm.add(i)
                else:
                    last[o['eng']] = i
            fence_deps_at[fp] = set(last.values()) | dm
            prev = fp
        last_w = {}
        readers = {}
        deps = [None] * n
        cur_fence = set()
        first_after = {}
        for i, o in enumerate(ops):
            if i in fence_deps_at:
                cur_fence = fence_deps_at[i]
                first_after = {}
            d = {}

            def add(j, raw):
                d[j] = d.get(j, False) or raw
            for k in o['r']:
                if k in last_w:
                    add(last_w[k], True)
            for k in o['w']:
                if k in last_w:
                    add(last_w[k], False)
                for tok, j in readers.get(k, {}).items():
                    add(j, False)
            keep = set()
            for j, raw in d.items():
                if j == i:
                    continue
                oj = ops[j]
                if (not oj['dma']) and (not o['dma']) and oj['eng'] == o['eng']:
                    if o['eng'] == 'pe':
                        continue
                    if not raw:
                        continue
                keep.add(j)
            tok_e = o['eng']
            if cur_fence and tok_e not in first_after:
                first_after[tok_e] = i
                for j in cur_fence:
                    if ops[j]['dma'] or ops[j]['eng'] != tok_e:
                        keep.add(j)
            deps[i] = keep
            for k in o['r']:
                tok = ('d', i) if o['dma'] else o['eng']
                readers.setdefault(k, {})[tok] = i
            for k in o['w']:
                last_w[k] = i
                readers[k] = {}
        needed = [False] * n
        for i in range(n):
            for j in deps[i]:
                needed[j] = True
        engs = ['pe', 'act', 'dve', 'pool', 'sp']
        cnt = {e: 0 for e in engs}
        sig = [None] * n
        dma_uses = [0] * DMA_POOL
        dma_last = [None] * DMA_POOL
        ndma = 0
        semkeys = set()
        for i, o in enumerate(ops):
            if o.get('cc'):
                ncc_ = getattr(self, '_ncc', 0) + 1
                self._ncc = ncc_
                sig[i] = (('cc', 0), ncc_)
                semkeys.add(('cc', 0))
            elif o['dma']:
                j = ndma % DMA_POOL
                ndma += 1
                dma_uses[j] += 1
                if dma_last[j] is not None:
                    deps[i].add(dma_last[j])
                dma_last[j] = i
                sig[i] = (('dma', j), 16 * dma_uses[j])
                semkeys.add(('dma', j))
            elif needed[i]:
                e = o['eng']
                c = cnt[e]
                cnt[e] += 1
                sk = (e, c // SEM_LIMIT)
                sig[i] = (sk, c % SEM_LIMIT + 1)
                semkeys.add(sk)
        finals = [i for i, o in enumerate(ops) if o['final']]
        seen = {e: {} for e in engs}
        streams = {e: [] for e in engs}
        for i, o in enumerate(ops):
            e = o['eng']
            waits = {}
            for j in deps[i]:
                sk, v = sig[j]
                if seen[e].get(sk, 0) >= v:
                    continue
                waits[sk] = max(waits.get(sk, 0), v)
            for sk, v in waits.items():
                seen[e][sk] = v
            streams[e].append((list(waits.items()), o['fn'], sig[i]))
        fw = {}
        for i in finals:
            sk, v = sig[i]
            if seen['sp'].get(sk, 0) >= v:
                continue
            fw[sk] = max(fw.get(sk, 0), v)
        streams['sp'].append((list(fw.items()), None, None))
        self.stats = dict(n_ops=n, cnt=dict(cnt), ndma=ndma,
                          nwaits={e: sum(len(s[0]) for s in streams[e]) for e in engs})
        semkeys = sorted(semkeys, key=str)
        import contextlib
        with contextlib.ExitStack() as st:
            sems = {}
            for sk in semkeys:
                sems[sk] = st.enter_context(nc.semaphore("s_%s_%s" % (sk[0], sk[1])))
            block = st.enter_context(nc.Block())

            def run(engine, items):
                for waits, fn, sg in items:
                    for sk, v in waits:
                        engine.wait_ge(sems[sk], v)
                    if fn is None:
                        continue
                    ins = fn(engine)
                    if sg is not None:
                        inc = 16 if sg[0][0] == 'dma' else 1
                        ins.then_inc(sems[sg[0]], inc)

            @block.tensor
            def _(e):
                run(e, streams['pe'])

            @block.scalar
            def _(e):
                run(e, streams['act'])

            @block.vector
            def _(e):
                run(e, streams['dve'])

            @block.gpsimd
            def _(e):
                run(e, streams['pool'])

            @block.sync
            def _(e):
                run(e, streams['sp'])
        return self.stats


class Arena:
    LO = 16512
    HI = 229376

    def __init__(self, nc):
        self.nc = nc
        self.top = self.LO
        self.n = 0
        self.peak = self.LO

    def alloc(self, name, shape, dt=F32):
        esz = {F32: 4, BF16: 2, I32: 4}[dt]
        nb = esz
        for s_ in shape[1:]:
            nb *= s_
        off = (self.top + 63) // 64 * 64
        assert off + nb <= self.HI, ("SBUF overflow", name, off + nb)
        self.top = off + nb
        self.peak = max(self.peak, self.top)
        self.n += 1
        return self.nc.alloc_sbuf_tensor_at("%s_%d" % (name, self.n), list(shape), dt, offset=off)

    def mark(self):
        return self.top

    def release(self, m):
        self.top = m


GN_EPS = 64e-5
FINAL_OUT = False
KAPPA = 0.6065306597126334
CTXN = 256


def rwkv_phase(nc, P, A, bank, pT, L, ctx_out, out_ap, pfx=''):
    T = CTXN + L
    di = lambda name, shape: nc.dram_tensor(pfx + name, list(shape), F32, kind="ExternalInput").ap()
    cmu = di("rw_cmu", [128, 6, 2])
    g2s = di("rw_g2", [128, 128])
    w2s = di("rw_w2", [64, 256])
    a2s = di("rw_a2", [64, 256])
    w0b = di("rw_w0b", [64, 256])
    a0f = di("rw_a0f", [64, 4])
    prm = di("rw_prm", [64, 2, 5])
    cst = di("rw_cst", [64, 448])
    uT = nc.dram_tensor(pfx + "uT_scr", [6, 128, T], F32, kind="Internal").ap()
    Yd = nc.dram_tensor(pfx + "Yd_scr", [2, 128, T], F32, kind="Internal").ap()
    Bd = nc.dram_tensor(pfx + "Bd_scr", [2, 128, T], F32, kind="Internal").ap()

    def TT(eng, out, a, b, op, r, w):
        P.op(eng, lambda e: e.tensor_tensor(out=out, in0=a, in1=b, op=op), r=r, w=w)

    def TS(eng, out, a, s1, s2, op0, op1, r, w):
        if op1 is None:
            P.op(eng, lambda e: e.tensor_scalar(out=out, in0=a, scalar1=s1, scalar2=None, op0=op0), r=r, w=w)
        else:
            P.op(eng, lambda e: e.tensor_scalar(out=out, in0=a, scalar1=s1, scalar2=s2, op0=op0, op1=op1), r=r, w=w)

    def STT(out, a, s, b, op0, op1, r, w):
        P.op('dve', lambda e: e.scalar_tensor_tensor(out=out, in0=a, scalar=s, in1=b, op0=op0, op1=op1), r=r, w=w)

    def ACT(out, a, func, r, w, scale=1.0, bias=None):
        if bias is None:
            P.op('act', lambda e: e.activation(out=out, in_=a, func=func, scale=scale), r=r, w=w)
        else:
            P.op('act', lambda e: e.activation(out=out, in_=a, func=func, scale=scale, bias=bias), r=r, w=w)

    def MM(out, lhsT, rhs, r, w, start=True, stop=True):
        P.op('pe', lambda e: e.matmul(out, lhsT=lhsT, rhs=rhs, start=start, stop=stop), r=r, w=w)

    m0 = A.mark()
    cmu_sb = A.alloc("cmu", [128, 6, 2])
    c0_sb = A.alloc("c0", [128, 6])
    g2_sb = A.alloc("g2", [128, 128])
    raw = [A.alloc("raw%d" % i, [128, 6, 514]) for i in range(2)]
    ush = A.alloc("ush", [128, 6, 512])
    sg = A.alloc("sg", [128, 512])
    P.dma('sp', cmu_sb[:], cmu, w=['cmu'])
    P.dma('sp', g2_sb[:], g2s, w=['g2'])
    TT('dve', c0_sb[:], cmu_sb[:, :, 0], cmu_sb[:, :, 1], ALU.add, ['cmu'], ['c0'])
    TS('dve', c0_sb[:], c0_sb[:], -1.0, 1.0, ALU.mult, ALU.add, ['c0'], ['c0'])
    seqs = [(0, CTXN), (CTXN, L)]
    ti = 0
    for (s0, slen) in seqs:
        for b0 in range(0, slen, 512):
            n = min(512, slen - b0)
            rb = raw[ti % 2]
            rk = ('raw', ti % 2)
            ti += 1
            lo = max(0, b0 - 1)
            hi = min(slen, b0 + n + 1)
            if b0 == 0 or b0 + n == slen:
                P.op('pool', lambda e, rb=rb: e.memset(rb[:], 0.0), w=[rk])
            P.dma('sp', rb[:, :, 1 + lo - b0:1 + hi - b0], pT[1:7, :, s0 + lo:s0 + hi].rearrange("g p t -> p g t"),
                  r=['pTall'], w=[rk])
            for g in range(6):
                eng = 'dve'
                TS('pool', ush[:, g, :n], rb[:, g, 1:1 + n], c0_sb[:, g:g + 1], None, ALU.mult, None, [rk, 'c0'], ['ush'])
                STT(ush[:, g, :n], rb[:, g, 0:n], cmu_sb[:, g, 0:1], ush[:, g, :n], ALU.mult, ALU.add, [rk, 'cmu', 'ush'], ['ush'])
                STT(ush[:, g, :n], rb[:, g, 2:2 + n], cmu_sb[:, g, 1:2], ush[:, g, :n], ALU.mult, ALU.add, [rk, 'cmu', 'ush'], ['ush'])
            ACT(sg[:, :n], ush[:, 5, :n], AF.Sigmoid, ['ush'], ['sg'])
            MM(bank[0][:, :n], g2_sb[:], sg[:, :n], ['g2', 'sg'], [('ps', 0)])
            P.op('act', lambda e, n=n: e.copy(out=ush[:, 5, :n], in_=bank[0][:, :n]), r=[('ps', 0), 'ush'], w=['ush'])
            P.dma('pool', uT[:, :, s0 + b0:s0 + b0 + n].rearrange("g p t -> p g t"), ush[:, :, :n], r=['ush'], w=['uTall'])
    P.fence()
    A.release(m0)
    w2_sb = A.alloc("w2", [64, 256])
    a2_sb = A.alloc("a2", [64, 256])
    w0b_sb = A.alloc("w0b", [64, 256])
    a0f_sb = A.alloc("a0f", [64, 4])
    prm_sb = A.alloc("prm", [64, 2, 5])
    cst_sb = A.alloc("cst", [64, 448])
    ones64 = A.alloc("ones64", [64, 64])
    for nm, dst, src in [('w2', w2_sb, w2s), ('a2', a2_sb, a2s), ('w0b', w0b_sb, w0b), ('a0f', a0f_sb, a0f),
                         ('prm', prm_sb, prm), ('cst', cst_sb, cst)]:
        P.dma('sp', dst[:], src, w=[nm])
    P.op('pool', lambda e: e.memset(ones64[:], 1.0), w=['ones64'])
    Tri3 = cst_sb[:, 0:192]
    Mk = cst_sb[:, 192:320]
    MkN = cst_sb[:, 320:384]
    I64 = cst_sb[:, 384:448]
    ST = A.alloc("ST", [64, 4, 64])
    Stmp = A.alloc("Stmp", [64, 4, 64])
    P.op('pool', lambda e: e.memset(ST[:], 0.0), w=['ST'])
    KSEG = 4
    NBUF = KSEG + 1
    f4 = lambda name: A.alloc(name, [64, 4, 64])

    shr = dict(L_=dict(r=A.alloc("ld_r", [64, 2, 2, 64]), k=A.alloc("ld_k", [64, 2, 2, 64]), v=A.alloc("ld_v", [64, 2, 2, 64]),
                       wl=A.alloc("ld_wl", [64, 2, 64]), al=A.alloc("ld_al", [64, 2, 64])),
               uwl=A.alloc("uwl", [64, 2, 64]), ual=A.alloc("ual", [64, 2, 64]), tw=A.alloc("tw", [64, 2, 64]),
               swt=A.alloc("swt", [64, 256]), kk=f4("kk"), kk2=f4("kk2"), rn=f4("rn"), kkn=f4("kkn"), bb=f4("bb"), km=f4("km"),
               t1=f4("t1"), BhT=f4("BhT"), KhT=f4("KhT"), Xsb=f4("Xsb"), Usb=f4("Usb"), Ysb=f4("Ysb"))

    def alloc_set(i):
        n_ = lambda x: "%s_%d" % (x, i)
        return (shr['L_'], f4(n_("ur")), f4(n_("uk")), f4(n_("uv")), shr['uwl'], shr['ual'],
                shr['tw'], shr['swt'], f4(n_("alr")),
                f4(n_("eI")), f4(n_("eE")), f4(n_("eN")), f4(n_("eT")), A.alloc(n_("gC"), [64, 4]),
                shr['kk'], shr['kk2'], shr['rn'], shr['kkn'], shr['bb'], shr['km'], shr['t1'],
                A.alloc(n_("AR"), [64, 4, 128]), f4(n_("BT")), f4(n_("KTt")), shr['BhT'], shr['KhT'], f4(n_("bon")),
                A.alloc(n_("TM"), [64, 2, 4, 64]), f4(n_("Vt")), A.alloc(n_("Gb"), [64, 4, 128]), A.alloc(n_("Gk"), [64, 4, 128]),
                f4(n_("Nn")), [A.alloc(n_("Pk%d" % j), [64, 2, 4, 64]) for j in range(2)], [f4(n_("Q0")), f4(n_("Q1"))],
                shr['Xsb'], shr['Usb'], shr['Ysb'])
    sets = [alloc_set(i) for i in range(NBUF)]
    prmb = lambda j: prm_sb[:, :, j:j + 1].unsqueeze(1).broadcast_to([64, 2, 2, 64])
    v4 = lambda t: t[:].rearrange("p (d h) t -> p d h t", d=2)
    SHARED = set(['w2', 'a2', 'w0b', 'a0f', 'prm', 'cst', 'ones64', 'uTall', 'Bdall', 'Ydall', 'ST', 'Stmp',
                  'ld', 'wl', 'al', 'tw', 'swt', 'kk', 'kk2', 'rn', 'kkn', 'bb', 'km', 't1', 'BhT', 'KhT', 'Xsb', 'Usb', 'Ysb'])
    cur = {'b': None}

    def kmap(keys):
        if cur['b'] is None:
            return list(keys)
        return [k if (k in SHARED or (isinstance(k, tuple) and k[0] == 'ps')) else ('rw', k, cur['b']) for k in keys]
    _op, _dma = P.op, P.dma

    def Pop(eng, fn, r=(), w=()):
        _op(eng, fn, r=kmap(r), w=kmap(w))

    def Pdma(eng, out, in_, r=(), w=(), **kw):
        _dma(eng, out, in_, r=kmap(r), w=kmap(w), **kw)

    def TT(eng, out, a, b, op, r, w):
        Pop(eng, lambda e: e.tensor_tensor(out=out, in0=a, in1=b, op=op), r=r, w=w)

    def TS(eng, out, a, s1, s2, op0, op1, r, w):
        if op1 is None:
            Pop(eng, lambda e: e.tensor_scalar(out=out, in0=a, scalar1=s1, scalar2=None, op0=op0), r=r, w=w)
        else:
            Pop(eng, lambda e: e.tensor_scalar(out=out, in0=a, scalar1=s1, scalar2=s2, op0=op0, op1=op1), r=r, w=w)

    def STT(out, a, s, b, op0, op1, r, w):
        Pop('dve', lambda e: e.scalar_tensor_tensor(out=out, in0=a, scalar=s, in1=b, op0=op0, op1=op1), r=r, w=w)

    def ACT(out, a, func, r, w, scale=1.0, bias=None):
        if bias is None:
            Pop('act', lambda e: e.activation(out=out, in_=a, func=func, scale=scale), r=r, w=w)
        else:
            Pop('act', lambda e: e.activation(out=out, in_=a, func=func, scale=scale, bias=bias), r=r, w=w)

    def MM(out, lhsT, rhs, r, w, start=True, stop=True):
        Pop('pe', lambda e: e.matmul(out, lhsT=lhsT, rhs=rhs, start=start, stop=stop), r=r, w=w)

    nlat = L // 64
    steps = [(s, 3 - s) for s in range(4)] + [(4 + s, 4 + nlat - 1 - s) for s in range(nlat)]
    NS = len(steps)

    def gen_step(si):
        cf, cb = steps[si]
        cur['b'] = si % NBUF
        (L_, ur, uk, uv, uwl, ual, tw, swt, alr, eI, eE, eN, eT, gC, kk, kk2, rn, kkn, bb, km, t1, AR, BT, KTt, BhT, KhT, bon,
         TM, Vt, Gb, Gk, Nn, Pk, Qk, Xsb, Usb, Yb) = sets[si % NBUF]
        saved = P.ops
        P.ops = []
        marks = []
        lk = 'ld'
        for d, cidx in enumerate((cf, cb)):
            t0 = cidx * 64
            for nm, row in (('r', 0), ('k', 1), ('v', 2)):
                Pdma('sp', L_[nm][:, d, :, :], uT[row, :, t0:t0 + 64].rearrange("(h c) t -> c h t", h=2), r=['uTall'], w=[lk])
            Pdma('sp', L_['wl'][:, d, :], uT[3, d * 64:(d + 1) * 64, t0:t0 + 64], r=['uTall'], w=[lk])
            Pdma('sp', L_['al'][:, d, :], uT[4, d * 64:(d + 1) * 64, t0:t0 + 64], r=['uTall'], w=[lk])
        for nm, dst in (('r', ur), ('k', uk), ('v', uv)):
            d4 = v4(dst)
            Pop('pool', lambda e, d4=d4, src=L_[nm]: e.tensor_copy(out=d4[:, 0], in_=src[:, 0]), r=[lk], w=[nm])
            Pop('pool', lambda e, d4=d4, src=L_[nm]: e.tensor_copy(out=d4[:, 1], in_=src[:, 1, :, ::-1]), r=[lk], w=[nm])
        for nm, dst in (('wl', uwl), ('al', ual)):
            Pop('pool', lambda e, dst=dst, src=L_[nm]: e.tensor_copy(out=dst[:, 0, :], in_=src[:, 0, :]), r=[lk], w=[nm])
            Pop('pool', lambda e, dst=dst, src=L_[nm]: e.tensor_copy(out=dst[:, 1, :], in_=src[:, 1, ::-1]), r=[lk], w=[nm])
        ACT(tw[:], uwl[:], AF.Tanh, ['wl'], ['tw'])
        for d in range(2):
            for h in range(2):
                dh = d * 2 + h
                MM(bank[0][0:64, dh * 64:(dh + 1) * 64], tw[:, d, :], w2_sb[:, dh * 64:(dh + 1) * 64], ['tw', 'w2'], [('ps', 0)])
                MM(bank[0][0:64, 256 + dh * 64:256 + (dh + 1) * 64], a2_sb[:, dh * 64:(dh + 1) * 64], ual[:, d, :],
                   ['a2', 'al'], [('ps', 0)])
        TT('dve', swt[:], bank[0][0:64, 0:256], w0b_sb[:], ALU.add, [('ps', 0), 'w0b'], ['swt'])
        ACT(swt[:], swt[:], AF.Sigmoid, ['swt'], ['swt'])
        TT('dve', alr[:], bank[0][0:64, 256:512].rearrange("p (a t) -> p a t", t=64),
           a0f_sb[:].unsqueeze(2).broadcast_to([64, 4, 64]), ALU.add, [('ps', 0), 'a0f'], ['alr'])
        ACT(alr[:], alr[:], AF.Sigmoid, ['alr'], ['alr'])
        cbanks = (1, 0)
        for dh in range(4):
            bk = cbanks[dh // 2]
            MM(bank[bk][0:64, (dh % 2) * 192:(dh % 2) * 192 + 192], swt[:, dh * 64:(dh + 1) * 64], Tri3, ['swt', 'cst'], [('ps', bk)])
        for half in range(2):
            bk = cbanks[half]
            cv = bank[bk][0:64, 0:384].rearrange("p (a x) -> p a x", x=192)
            sl = slice(half * 2, half * 2 + 2)
            ACT(eI[:, sl, :], cv[:, :, 0:64], AF.Exp, [('ps', bk)], ['eI'], scale=-KAPPA)
            ACT(eE[:, sl, :], cv[:, :, 64:128], AF.Exp, [('ps', bk)], ['eE'], scale=-KAPPA)
            ACT(eN[:, sl, :], cv[:, :, 0:64], AF.Exp, [('ps', bk)], ['eN'], scale=KAPPA)
            ACT(gC[:, sl], cv[:, :, 128], AF.Exp, [('ps', bk)], ['gC'], scale=-KAPPA)
        TT('dve', eT[:], eN[:], gC[:].unsqueeze(2).broadcast_to([64, 4, 64]), ALU.mult, ['eN', 'gC'], ['eT'])
        marks.append(len(P.ops))
        TT('dve', v4(kk), v4(uk), prmb(0), ALU.mult, ['k', 'prm'], ['kk'])
        TT('pool', kk2[:], kk[:], kk[:], ALU.mult, ['kk'], ['kk2'])
        MM(bank[2][0:64, 0:256], ones64[:], kk2[:].rearrange("p a t -> p (a t)"), ['ones64', 'kk2'], [('ps', 2)])
        ACT(rn[:].rearrange("p a t -> p (a t)"), bank[2][0:64, 0:256], AF.Sqrt, [('ps', 2)], ['rn'])
        TS('dve', rn[:], rn[:], 1e-12, None, ALU.max, None, ['rn'], ['rn'])
        Pop('dve', lambda e: e.reciprocal(out=rn[:], in_=rn[:]), r=['rn'], w=['rn'])
        TT('dve', kkn[:], kk[:], rn[:], ALU.mult, ['kk', 'rn'], ['kkn'])
        TT('pool', bb[:], kkn[:], alr[:], ALU.mult, ['kkn', 'alr'], ['bb'])
        TS('pool', t1[:], alr[:], -1.0, None, ALU.add, None, ['alr'], ['t1'])
        TT('pool', v4(t1), v4(t1), prmb(1), ALU.mult, ['t1', 'prm'], ['t1'])
        STT(km[:], t1[:], 1.0, uk[:], ALU.add, ALU.mult, ['t1', 'k'], ['km'])
        STT(AR[:, :, 0:64], kkn[:], -1.0, eE[:], ALU.mult, ALU.mult, ['kkn', 'eE'], ['AR'])
        TT('pool', AR[:, :, 64:128], ur[:], eI[:], ALU.mult, ['r', 'eI'], ['AR'])
        TT('dve', BT[:], bb[:], eN[:], ALU.mult, ['bb', 'eN'], ['BT'])
        TT('pool', KTt[:], km[:], eN[:], ALU.mult, ['km', 'eN'], ['KTt'])
        TT('dve', BhT[:], bb[:], eT[:], ALU.mult, ['bb', 'eT'], ['BhT'])
        TT('pool', KhT[:], km[:], eT[:], ALU.mult, ['km', 'eT'], ['KhT'])
        TT('pool', t1[:], ur[:], km[:], ALU.mult, ['r', 'km', 't1'], ['t1'])
        TT('pool', v4(t1), v4(t1), prmb(2), ALU.mult, ['t1', 'prm'], ['t1'])
        MM(bank[2][0:64, 256:512], ones64[:], t1[:].rearrange("p a t -> p (a t)"), ['ones64', 't1'], [('ps', 2)])
        TT('dve', bon[:], bank[2][0:64, 256:512].rearrange("p (a t) -> p a t", t=64), uv[:], ALU.mult, [('ps', 2), 'v'], ['bon'])
        Pop('pool', lambda e: e.tensor_copy(out=kk2[:, 2:4, :], in_=bon[:, 2:4, ::-1]), r=['bon', 'kk2'], w=['kk2'])
        Pdma('pool', Bd[0, :, cf * 64:cf * 64 + 64].rearrange("(h c) t -> c h t", h=2), bon[:, 0:2, :], r=['bon'], w=['Bdall'])
        Pdma('pool', Bd[1, :, cb * 64:cb * 64 + 64].rearrange("(h c) t -> c h t", h=2), kk2[:, 2:4, :], r=['kk2'], w=['Bdall'])
        for dh in range(4):
            MM(bank[3][0:64, dh * 64:(dh + 1) * 64], BhT[:, dh, :], I64, ['BhT', 'cst'], [('ps', 3)])
            MM(bank[3][0:64, 256 + dh * 64:256 + (dh + 1) * 64], KhT[:, dh, :], I64, ['KhT', 'cst'], [('ps', 3)])
        Pop('act', lambda e: e.copy(out=TM[:].rearrange("p a b t -> p (a b t)"), in_=bank[3][0:64, :]), r=[('ps', 3)], w=['TM'])
        for dh in range(4):
            MM(bank[2][0:64, dh * 64:(dh + 1) * 64], uv[:, dh, :], I64, ['v', 'cst'], [('ps', 2)])
        Pop('dve', lambda e: e.tensor_copy(out=Vt[:].rearrange("p a t -> p (a t)"), in_=bank[2][0:64, 0:256]), r=[('ps', 2)], w=['Vt'])
        marks.append(len(P.ops))
        for dh in range(4):
            MM(bank[4][0:64, dh * 128:(dh + 1) * 128], BT[:, dh, :], AR[:, dh, :], ['BT', 'AR'], [('ps', 4)])
            MM(bank[5][0:64, dh * 128:(dh + 1) * 128], KTt[:, dh, :], AR[:, dh, :], ['KTt', 'AR'], [('ps', 5)])
        mk4 = Mk.unsqueeze(1).broadcast_to([64, 4, 128])
        TT('dve', Gb[:], bank[4][0:64, :].rearrange("p (a x) -> p a x", x=128), mk4, ALU.mult, [('ps', 4), 'cst'], ['Gb'])
        TT('dve', Gk[:], bank[5][0:64, :].rearrange("p (a x) -> p a x", x=128), mk4, ALU.mult, [('ps', 5), 'cst'], ['Gk'])
        for dh in range(4):
            MM(bank[4][0:64, 256 + dh * 64:256 + (dh + 1) * 64], AR[:, dh, 0:64], BT[:, dh, :], ['AR', 'BT'], [('ps', 4)])
        TT('dve', Nn[:], bank[4][0:64, 256:512].rearrange("p (a x) -> p a x", x=64),
           MkN.unsqueeze(1).broadcast_to([64, 4, 64]), ALU.mult, [('ps', 4), 'cst'], ['Nn'])
        TT('pool', Qk[0][:], Gb[:, :, 0:64], I64.unsqueeze(1).broadcast_to([64, 4, 64]), ALU.add, ['Gb', 'cst'], [('Q', 0)])
        pk_prev = (lambda dh: Gb[:, dh, 0:64], lambda dh: Nn[:, dh, :], ['Gb', 'Nn'])
        qi = 0
        for lv in range(1, 6):
            if lv == 3:
                marks.append(len(P.ops))
            pb = lv % 2
            Pn = Pk[pb]
            pkey = ('Pk', pb)
            bkp, bkq = (5, 4) if lv <= 2 else (6, 6)
            for dh in range(4):
                if lv < 5:
                    MM(bank[bkp][0:64, dh * 64:(dh + 1) * 64], pk_prev[1](dh), pk_prev[0](dh), pk_prev[2], [('ps', bkp)])
                MM(bank[bkp][0:64, 256 + dh * 64:256 + (dh + 1) * 64], pk_prev[0](dh), pk_prev[1](dh), pk_prev[2], [('ps', bkp)])
            if lv % 2:
                Pop('act', lambda e, Pn=Pn, bkp=bkp: e.copy(out=Pn[:].rearrange("p a b t -> p (a b t)"), in_=bank[bkp][0:64, :]),
                    r=[('ps', bkp)], w=[pkey])
            else:
                Pop('dve', lambda e, Pn=Pn, bkp=bkp: e.tensor_copy(out=Pn[:].rearrange("p a b t -> p (a b t)"), in_=bank[bkp][0:64, :]),
                    r=[('ps', bkp)], w=[pkey])
            pk_prev = (lambda dh, Pn=Pn: Pn[:, 0, dh, :], lambda dh, Pn=Pn: Pn[:, 1, dh, :], [pkey])
            for dh in range(4):
                MM(bank[bkq][0:64, dh * 64:(dh + 1) * 64], Pn[:, 1, dh, :], Qk[qi][:, dh, :], [pkey, ('Q', qi)], [('ps', bkq)])
            TT('dve', Qk[1 - qi][:], bank[bkq][0:64, 0:256].rearrange("p (a t) -> p a t", t=64), Qk[qi][:], ALU.add,
               [('ps', bkq), ('Q', qi)], [('Q', 1 - qi)])
            qi = 1 - qi
        TTm = Qk[qi]
        tkey = ('Q', qi)
        marks.append(len(P.ops))
        for dh in range(4):
            MM(bank[7][0:64, dh * 64:(dh + 1) * 64], AR[:, dh, 0:64], ST[:, dh, :], ['AR', 'ST'], [('ps', 7)], start=True, stop=False)
            MM(bank[7][0:64, dh * 64:(dh + 1) * 64], Gk[:, dh, 0:64], Vt[:, dh, :], ['Gk', 'Vt'], [('ps', 7)], start=False, stop=True)
        Pop('act', lambda e: e.copy(out=Xsb[:].rearrange("p a t -> p (a t)"), in_=bank[7][0:64, 0:256]), r=[('ps', 7)], w=['Xsb'])
        for dh in range(4):
            MM(bank[7][0:64, 256 + dh * 64:256 + (dh + 1) * 64], TTm[:, dh, :], Xsb[:, dh, :], [tkey, 'Xsb'], [('ps', 7)])
        Pop('act', lambda e: e.copy(out=Usb[:].rearrange("p a t -> p (a t)"), in_=bank[7][0:64, 256:512]), r=[('ps', 7)], w=['Usb'])
        for dh in range(4):
            o = bank[7][0:64, dh * 64:(dh + 1) * 64]
            MM(o, ST[:, dh, :], AR[:, dh, 64:128], ['ST', 'AR'], [('ps', 7)], start=True, stop=False)
            MM(o, Usb[:, dh, :], Gb[:, dh, 64:128], ['Usb', 'Gb'], [('ps', 7)], start=False, stop=False)
            MM(o, Vt[:, dh, :], Gk[:, dh, 64:128], ['Vt', 'Gk'], [('ps', 7)], start=False, stop=True)
        yv = bank[7][0:64, 0:256].rearrange("p (a t) -> p a t", t=64)
        Pop('act', lambda e, Yb=Yb, yv=yv: e.copy(out=Yb[:, 0:2, :], in_=yv[:, 0:2, :]), r=[('ps', 7)], w=['Ysb'])
        Pop('act', lambda e, Yb=Yb, yv=yv: e.copy(out=Yb[:, 2:4, ::-1], in_=yv[:, 2:4, :]), r=[('ps', 7)], w=['Ysb'])
        Pdma('pool', Yd[0, :, cf * 64:cf * 64 + 64].rearrange("(h c) t -> c h t", h=2), Yb[:, 0:2, :], r=['Ysb'], w=['Ydall'])
        Pdma('pool', Yd[1, :, cb * 64:cb * 64 + 64].rearrange("(h c) t -> c h t", h=2), Yb[:, 2:4, :], r=['Ysb'], w=['Ydall'])
        TT('pool', Stmp[:], ST[:], gC[:].unsqueeze(2).broadcast_to([64, 4, 64]), ALU.mult, ['ST', 'gC'], ['Stmp'])
        for dh in range(4):
            o = bank[7][0:64, 256 + dh * 64:256 + (dh + 1) * 64]
            MM(o, TM[:, 0, dh, :], Usb[:, dh, :], ['TM', 'Usb'], [('ps', 7)], start=True, stop=False)
            MM(o, TM[:, 1, dh, :], Vt[:, dh, :], ['TM', 'Vt'], [('ps', 7)], start=False, stop=True)
        TT('dve', ST[:], bank[7][0:64, 256:512].rearrange("p (a t) -> p a t", t=64), Stmp[:], ALU.add, [('ps', 7), 'Stmp'], ['ST'])
        ops = P.ops
        P.ops = saved
        cur['b'] = None
        bounds = [0] + marks + [len(ops)]
        return [ops[bounds[i]:bounds[i + 1]] for i in range(5)]

    def interleave(lists):
        items = []
        for li, lst in enumerate(lists):
            n_ = len(lst)
            for k_, o in enumerate(lst):
                items.append(((k_ + 0.5) / n_, li, k_, o))
        items.sort(key=lambda t: (t[0], t[1], t[2]))
        return [t[3] for t in items]
    gen = [gen_step(si) for si in range(NS)]
    for tau in range(-KSEG, NS):
        lists = []
        if 0 <= tau < NS:
            lists.append(gen[tau][KSEG])
        for j in range(1, KSEG + 1):
            s_ = tau + j
            if 0 <= s_ < NS:
                lists.append(gen[s_][KSEG - j])
        P.ops.extend(interleave(lists))
    P.fence()
    A.release(m0)
    prm2 = A.alloc("prm2", [64, 2, 5])
    on64 = A.alloc("on64", [64, 64])
    eps2 = A.alloc("eps2", [64, 1])
    P.dma('sp', prm2[:], prm, w=['prm2'])
    P.op('pool', lambda e: e.memset(on64[:], 1.0 / 64), w=['on64'])
    P.op('pool', lambda e: e.memset(eps2[:], GN_EPS), w=['eps2'])
    yb = [A.alloc("yb%d" % i, [64, 2, 2, 512]) for i in range(2)]
    bd = [A.alloc("bd%d" % i, [64, 2, 2, 512]) for i in range(2)]
    gg = [A.alloc("gg%d" % i, [64, 2, 512]) for i in range(2)]
    y = A.alloc("y", [64, 2, 512])
    yc = A.alloc("yc", [64, 2, 512])
    y2 = A.alloc("y2", [64, 2, 512])
    rs = A.alloc("rs", [64, 2, 512])
    jobs = []
    if ctx_out:
        jobs.append((0, CTXN, 0))
    o0 = CTXN if ctx_out else 0
    for b0 in range(0, L, 512):
        jobs.append((CTXN + b0, 512, o0 + b0))
    for ji, (t0, n, orow) in enumerate(jobs):
        b = ji % 2
        P.dma('sp', yb[b][:, :, :, :n], Yd[:, :, t0:t0 + n].rearrange("d (h c) t -> c d h t", h=2), r=['Ydall'], w=[('yb', b)])
        P.dma('sp', bd[b][:, :, :, :n], Bd[:, :, t0:t0 + n].rearrange("d (h c) t -> c d h t", h=2), r=['Bdall'], w=[('bd', b)])
        P.dma('sp', gg[b][:, :, :n], uT[5, :, t0:t0 + n].rearrange("(h c) t -> c h t", h=2), r=['uTall'], w=[('gg', b)])
        TT('dve', y[:, :, :n], yb[b][:, 0, :, :n], yb[b][:, 1, :, :n], ALU.add, [('yb', b)], ['y'])
        for h in range(2):
            MM(bank[h][0:64, :n], on64[:], y[:, h, :n], ['on64', 'y'], [('ps', h)])
            TT('dve', yc[:, h, :n], y[:, h, :n], bank[h][0:64, :n], ALU.subtract, ['y', ('ps', h)], ['yc'])
        TT('pool', y2[:, :, :n], yc[:, :, :n], yc[:, :, :n], ALU.mult, ['yc'], ['y2'])
        for h in range(2):
            MM(bank[2 + h][0:64, :n], on64[:], y2[:, h, :n], ['on64', 'y2'], [('ps', 2 + h)])
            ACT(rs[:, h, :n], bank[2 + h][0:64, :n], AF.Sqrt, [('ps', 2 + h), 'eps2'], ['rs'], bias=eps2[:, 0:1])
        P.op('dve', lambda e, n=n: e.reciprocal(out=rs[:, :, :n], in_=rs[:, :, :n]), r=['rs'], w=['rs'])
        TT('dve', yc[:, :, :n], yc[:, :, :n], rs[:, :, :n], ALU.mult, ['yc', 'rs'], ['yc'])
        for h in range(2):
            TS('pool', yc[:, h, :n], yc[:, h, :n], prm2[:, h, 3:4], prm2[:, h, 4:5], ALU.mult, ALU.add, ['yc', 'prm2'], ['yc'])
        TT('dve', yc[:, :, :n], yc[:, :, :n], bd[b][:, 0, :, :n], ALU.add, ['yc', ('bd', b)], ['yc'])
        TT('dve', yc[:, :, :n], yc[:, :, :n], bd[b][:, 1, :, :n], ALU.add, ['yc', ('bd', b)], ['yc'])
        TT('pool', y2[:, :, :n], yc[:, :, :n], gg[b][:, :, :n], ALU.mult, ['yc', ('gg', b), 'y2'], ['y2'])
        P.dma('pool', out_ap(orow, n).rearrange("(h c) t -> c h t", h=2), y2[:, :, :n], r=['y2'], w=['brb'], final=FINAL_OUT)
    P.fence()


NORM_EPS = 1e-6
CTXN = 256


def phase_A(nc, P, A, bank, pfx, L, ctx_out, lam_init, x_loader, brb, do_attn=True, do_pool=True, do_rwkv=True):
    T = CTXN + L
    NQ = T if ctx_out else L
    di = lambda name, shape: nc.dram_tensor(pfx + name, list(shape), F32, kind="ExternalInput").ap()
    if x_loader is None:
        xT = di("xT", [128, 8, T])

        def x_loader(dst, t0, sz, key):
            P.dma('sp', dst[:, :, :sz], xT[:, :, t0:t0 + sz], w=[key])
    identA = di("identA", [128, 128])
    cT = di("cT", [128, 8, 2])
    wada = di("wada", [128, 8, 2048])
    bada = di("bada", [128, 16])
    g1 = di("g1", [128, 8])
    win = di("win", [128, 8, 1280])
    qkg = di("qkg", [128, 2])
    ropeR = di("ropeR", [128, 2, L // 64])
    ropeC = di("ropeC", [128, 2, 64])
    perm = di("perm", [128, 128])
    lamqk = di("lamqk", [128, 256])
    subg = di("subg", [128, 128])
    wpool = di("wpool", [128, 128])
    pscale = di("pscale", [128, 1])
    selw = di("selw", [128, 4])
    efix = di("efix", [128, 16])
    pT = nc.dram_tensor(pfx + "pT_scr", [7, 128, T], F32, kind="Internal").ap()
    base_mark = A.mark()
    if True:
        ones = A.alloc("ones", [128, 128])
        blk = A.alloc("blk", [128, 128])
        eps_sb = A.alloc("eps", [128, 1])
        mod_sb = A.alloc("mod", [128, 16, 2])
        A_sb = A.alloc("A", [128, 8, 2])
        NKT = T // 128
        QT = A.alloc("QT", [128, T], BF16)
        KT = A.alloc("KT", [128, T], BF16)
        V = A.alloc("V", [128, NKT, 130], BF16)
        lam_sb = A.alloc("lam", [128, 1])
        subg_sb = A.alloc("subg", [128, 128])
        ident_sb = A.alloc("identA", [128, 128])
        P.dma('sp', ident_sb[:], identA, w=['identA'])
        P.op('pool', lambda e: e.memset(ones[:], 1.0), w=['ones'])
        P.op('pool', lambda e: e.memset(blk[:], 0.0), w=['blk'])
        P.op('pool', lambda e: e.memset(blk[0:64, 0:64], 1.0), w=['blk'])
        P.op('pool', lambda e: e.memset(blk[64:128, 64:128], 1.0), w=['blk'])
        P.op('pool', lambda e: e.memset(eps_sb[:], NORM_EPS), w=['eps'])
        P.op('pool', lambda e: e.memset(V[:, :, 128:130], 1.0), w=['Vones'])
        m_a1 = A.mark()
        c_sb = A.alloc("c", [128, 8, 2])
        s_sb = A.alloc("s", [128, 8, 2])
        bada_sb = A.alloc("bada", [128, 16])
        g1_sb = A.alloc("g1", [128, 8])
        qkg_sb = A.alloc("qkg", [128, 2])
        perm_sb = A.alloc("perm", [128, 128])
        ropeR_sb = A.alloc("ropeR", [128, 2, L // 64])
        ropeC_sb = A.alloc("ropeC", [128, 2, 64])
        lamqk_sb = A.alloc("lamqk", [128, 256])
        lamt = A.alloc("lamt", [128, 4])
        wbf = A.alloc("wbf", [128, 8, 1280], BF16)
        xt = [A.alloc("xt%d" % i, [128, 8, 512]) for i in range(2)]
        sq = A.alloc("sq", [128, 8, 512])
        rstd = A.alloc("rstd", [128, 512])
        xn = A.alloc("xn", [128, 8, 512], BF16)
        ob = [A.alloc("ob%d" % i, [128, 512]) for i in range(2)]
        qk32 = A.alloc("qk32", [128, 512])
        qksq = A.alloc("qksq", [128, 512])
        qkr = A.alloc("qkr", [128, 512])
        qkn = A.alloc("qkn", [128, 512])
        cs_t = A.alloc("cs_t", [128, 2, 512])
        rtmp = A.alloc("rtmp", [128, 2, 512])

        for nm, dst, src in [('c_sb', c_sb, cT), ('bada', bada_sb, bada), ('g1', g1_sb, g1), ('qkg', qkg_sb, qkg),
                             ('perm', perm_sb, perm), ('ropeR', ropeR_sb, ropeR), ('ropeC', ropeC_sb, ropeC),
                             ('lamqk', lamqk_sb, lamqk), ('subg', subg_sb, subg)]:
            P.dma('sp', dst[:], src, w=[nm])
        lq = lamqk_sb[:].rearrange("p (a b) -> p a b", b=64)
        P.op('dve', lambda e: e.tensor_tensor(out=sq[:, 0, 0:64], in0=lq[:, 0, :], in1=lq[:, 1, :], op=ALU.mult),
             r=['lamqk'], w=['sq'])
        P.op('dve', lambda e: e.tensor_tensor(out=sq[:, 0, 64:128], in0=lq[:, 2, :], in1=lq[:, 3, :], op=ALU.mult),
             r=['lamqk'], w=['sq'])
        P.op('dve', lambda e: e.tensor_reduce(out=lamt[:, 0:2], in_=sq[:, 0, 0:128].rearrange("p (a b) -> p a b", b=64),
                                              axis=AX.X, op=ALU.add), r=['sq'], w=['lamt'])
        P.op('act', lambda e: e.activation(out=lamt[:, 2:4], in_=lamt[:, 0:2], func=AF.Exp), r=['lamt'], w=['lamt2'])
        P.op('dve', lambda e: e.tensor_tensor(out=lam_sb[:], in0=lamt[:, 2:3], in1=lamt[:, 3:4], op=ALU.subtract),
             r=['lamt2'], w=['lam'])
        P.op('dve', lambda e: e.tensor_scalar(out=lam_sb[:], in0=lam_sb[:], scalar1=lam_init, scalar2=-1.0,
                                              op0=ALU.add, op1=ALU.mult), r=['lam'], w=['lam'])
        P.op('act', lambda e: e.activation(out=s_sb[:], in_=c_sb[:], func=AF.Silu), r=['c_sb'], w=['s_sb'])
        ps_mod = bank[0][:, 0:32]
        for piece in range(4):
            b = piece % 2
            P.dma('sp', xt[b][:], wada[:, :, piece * 512:(piece + 1) * 512], w=[('xt', b)])
            for occ in range(4):
                oc = piece * 4 + occ
                for k in range(8):
                    P.op('pe', lambda e, b=b, occ=occ, oc=oc, k=k: e.matmul(
                        ps_mod[:, oc * 2:oc * 2 + 2], lhsT=xt[b][:, k, occ * 128:(occ + 1) * 128],
                        rhs=s_sb[:, k, :], start=(k == 0), stop=(k == 7)),
                        r=[('xt', b), 's_sb'], w=[('ps', 0)])
        P.op('dve', lambda e: e.tensor_tensor(
            out=mod_sb[:], in0=ps_mod.rearrange("p (a b) -> p a b", b=2),
            in1=bada_sb[:].unsqueeze(2).broadcast_to([128, 16, 2]), op=ALU.add),
            r=[('ps', 0), 'bada'], w=['mod'])
        P.op('dve', lambda e: e.tensor_scalar(out=A_sb[:], in0=mod_sb[:, 8:16, :], scalar1=1.0, scalar2=None,
                                              op0=ALU.add), r=['mod'], w=['A'])
        P.op('dve', lambda e: e.tensor_tensor(out=A_sb[:], in0=A_sb[:],
                                              in1=g1_sb[:].unsqueeze(2).broadcast_to([128, 8, 2]), op=ALU.mult),
             r=['A', 'g1'], w=['A'])
        for piece, (c0, csz) in enumerate([(0, 512), (512, 512), (1024, 256)]):
            b = piece % 2
            P.dma('sp', xt[b][:, :, :csz], win[:, :, c0:c0 + csz], w=[('xt', b)])
            P.op('pool', lambda e, b=b, c0=c0, csz=csz: e.tensor_copy(out=wbf[:, :, c0:c0 + csz], in_=xt[b][:, :, :csz]),
                 r=[('xt', b)], w=['wbf'])
        tiles = [(0, CTXN, 1)] + [(CTXN + i * 512, 512, 0) for i in range(L // 512)]
        SCR = {0: 0, 4: 1, 5: 2, 6: 3, 7: 4, 8: 5, 9: 6}
        oi = 0
        for ti, (t0, sz, j) in enumerate(tiles):
            b = ti % 2
            x_loader(xt[b], t0, sz, ('xt', b))
            P.op('act', lambda e, b=b, sz=sz: e.activation(out=sq[:, :, :sz], in_=xt[b][:, :, :sz], func=AF.Square),
                 r=[('xt', b)], w=['sq'])
            for k in range(8):
                P.op('pe', lambda e, k=k, sz=sz: e.matmul(bank[0][:, :sz], lhsT=ones[:], rhs=sq[:, k, :sz],
                                                          start=(k == 0), stop=(k == 7)),
                     r=['ones', 'sq'], w=[('ps', 0)])
            P.op('act', lambda e, sz=sz: e.activation(out=rstd[:, :sz], in_=bank[0][:, :sz], func=AF.Sqrt,
                                                      scale=1.0 / 1024, bias=eps_sb[:, 0:1]),
                 r=[('ps', 0), 'eps'], w=['rstd'])
            P.op('dve', lambda e, sz=sz: e.reciprocal(out=rstd[:, :sz], in_=rstd[:, :sz]), r=['rstd'], w=['rstd'])
            P.op('dve', lambda e, b=b, sz=sz: e.tensor_tensor(
                out=sq[:, :, :sz], in0=xt[b][:, :, :sz],
                in1=rstd[:, :sz].unsqueeze(1).broadcast_to([128, 8, sz]), op=ALU.mult),
                r=[('xt', b), 'rstd', 'sq'], w=['sq'])
            for k in range(8):
                if k % 2:
                    P.op('dve', lambda e, k=k, sz=sz, j=j: e.tensor_scalar(
                        out=xn[:, k, :sz], in0=sq[:, k, :sz], scalar1=A_sb[:, k, j:j + 1],
                        scalar2=mod_sb[:, k, j:j + 1], op0=ALU.mult, op1=ALU.add),
                        r=['sq', 'A', 'mod'], w=['xn'])
                else:
                    P.op('act', lambda e, k=k, sz=sz, j=j: e.activation(
                        out=xn[:, k, :sz], in_=sq[:, k, :sz], func=AF.Identity, scale=A_sb[:, k, j:j + 1],
                        bias=mod_sb[:, k, j:j + 1]), r=['sq', 'A', 'mod'], w=['xn'])
            if j == 0:
                r0 = (t0 - CTXN) // 64
                for cs in range(2):
                    P.op('pool', lambda e, cs=cs, r0=r0: e.tensor_tensor(
                        out=cs_t[:, cs, :].rearrange("p (r c) -> p r c", c=64),
                        in0=ropeR_sb[:, cs, r0:r0 + 8].unsqueeze(2).broadcast_to([128, 8, 64]),
                        in1=ropeC_sb[:, cs, :].unsqueeze(1).broadcast_to([128, 8, 64]), op=ALU.mult),
                        r=['ropeR', 'ropeC'], w=['cs_t'])
            for c in range(10):
                if c == 3:
                    for s in range(sz // 128):
                        kt = t0 // 128 + s
                        for k in range(8):
                            P.op('pe', lambda e, k=k, s=s: e.matmul(
                                bank[3][:, 0:128], lhsT=xn[:, k, s * 128:(s + 1) * 128], rhs=wbf[:, k, 384:512],
                                start=(k == 0), stop=(k == 7)), r=['xn', 'wbf'], w=[('ps', 3)])
                        P.op('act', lambda e, kt=kt: e.copy(out=V[:, kt, 0:128], in_=bank[3][:, 0:128]),
                             r=[('ps', 3)], w=['V'])
                    continue
                pb = 1 + (oi % 2)
                oi += 1
                for k in range(8):
                    P.op('pe', lambda e, c=c, k=k, sz=sz, pb=pb: e.matmul(
                        bank[pb][:, :sz], lhsT=wbf[:, k, c * 128:(c + 1) * 128], rhs=xn[:, k, :sz],
                        start=(k == 0), stop=(k == 7)), r=['wbf', 'xn'], w=[('ps', pb)])
                if c in SCR:
                    o = ob[oi % 2]
                    ok = ('ob', oi % 2)
                    if oi % 2:
                        P.op('act', lambda e, o=o, pb=pb, sz=sz: e.copy(out=o[:, :sz], in_=bank[pb][:, :sz]),
                             r=[('ps', pb)], w=[ok])
                    else:
                        P.op('dve', lambda e, o=o, pb=pb, sz=sz: e.tensor_copy(out=o[:, :sz], in_=bank[pb][:, :sz]),
                             r=[('ps', pb)], w=[ok])
                    P.dma('pool', pT[SCR[c], :, t0:t0 + sz], o[:, :sz], r=[ok], w=[('pT', SCR[c], ti)])
                else:
                    dst = QT if c == 1 else KT
                    gi = c - 1
                    P.op('act', lambda e, pb=pb, sz=sz: e.copy(out=qk32[:, :sz], in_=bank[pb][:, :sz]),
                         r=[('ps', pb)], w=['qk32'])
                    P.op('act', lambda e, sz=sz: e.activation(out=qksq[:, :sz], in_=qk32[:, :sz], func=AF.Square),
                         r=['qk32'], w=['qksq'])
                    P.op('pe', lambda e, sz=sz: e.matmul(bank[4][:, :sz], lhsT=blk[:], rhs=qksq[:, :sz],
                                                         start=True, stop=True), r=['blk', 'qksq'], w=[('ps', 4)])
                    P.op('act', lambda e, sz=sz: e.activation(out=qkr[:, :sz], in_=bank[4][:, :sz], func=AF.Sqrt,
                                                              scale=1.0 / 64, bias=eps_sb[:, 0:1]),
                         r=[('ps', 4), 'eps'], w=['qkr'])
                    P.op('dve', lambda e, sz=sz: e.reciprocal(out=qkr[:, :sz], in_=qkr[:, :sz]), r=['qkr'], w=['qkr'])
                    if j == 1:
                        P.op('dve', lambda e, sz=sz, gi=gi, dst=dst, t0=t0: e.scalar_tensor_tensor(
                            out=dst[:, t0:t0 + sz], in0=qk32[:, :sz], scalar=qkg_sb[:, gi:gi + 1], in1=qkr[:, :sz],
                            op0=ALU.mult, op1=ALU.mult), r=['qk32', 'qkg', 'qkr'], w=['QK'])
                    else:
                        P.op('dve', lambda e, sz=sz, gi=gi: e.scalar_tensor_tensor(
                            out=qkn[:, :sz], in0=qk32[:, :sz], scalar=qkg_sb[:, gi:gi + 1], in1=qkr[:, :sz],
                            op0=ALU.mult, op1=ALU.mult), r=['qk32', 'qkg', 'qkr'], w=['qkn'])
                        P.op('pe', lambda e, sz=sz: e.matmul(bank[5][:, :sz], lhsT=perm_sb[:], rhs=qkn[:, :sz],
                                                             start=True, stop=True), r=['perm', 'qkn'], w=[('ps', 5)])
                        P.op('pool', lambda e, sz=sz: e.tensor_tensor(out=rtmp[:, 0, :sz], in0=qkn[:, :sz],
                                                                      in1=cs_t[:, 0, :sz], op=ALU.mult),
                             r=['qkn', 'cs_t'], w=['rtmp0'])
                        P.op('dve', lambda e, sz=sz: e.tensor_tensor(out=rtmp[:, 1, :sz], in0=bank[5][:, :sz],
                                                                     in1=cs_t[:, 1, :sz], op=ALU.mult),
                             r=[('ps', 5), 'cs_t'], w=['rtmp1'])
                        P.op('dve', lambda e, sz=sz, dst=dst, t0=t0: e.tensor_tensor(
                            out=dst[:, t0:t0 + sz], in0=rtmp[:, 0, :sz], in1=rtmp[:, 1, :sz], op=ALU.add),
                            r=['rtmp0', 'rtmp1'], w=['QK'])
        P.fence()
        A.release(m_a1)
        m_ph = A.mark()
        if do_pool:
            wpool_f = A.alloc("wpool_f", [128, 128])
            wpool_b = A.alloc("wpool_b", [128, 128], BF16)
            pscale_sb = A.alloc("pscale", [128, 1])
            selw_sb = A.alloc("selw", [128, 4])
            efix_sb = A.alloc("efix", [128, 16])
            NB = 2048
            U = A.alloc("U", [128, NB + 32])
            W = [A.alloc("W%d" % i, [128, NB + 32]) for i in range(2)]
            comb = A.alloc("comb", [128, NB])
            diffb = A.alloc("diffb", [128, NB], BF16)
            pob = [A.alloc("pob%d" % i, [128, 512]) for i in range(2)]
            P.dma('sp', wpool_f[:], wpool, w=['wpool_f'])
            P.dma('sp', pscale_sb[:], pscale, w=['pscale'])
            P.dma('sp', selw_sb[:], selw, w=['selw'])
            P.dma('sp', efix_sb[:], efix, w=['efix'])
            P.op('dve', lambda e: e.tensor_copy(out=wpool_b[:], in_=wpool_f[:]), r=['wpool_f'], w=['wpool_b'])
            seqs = [(CTXN, L, (0 if not ctx_out else CTXN))]
            if ctx_out:
                seqs.append((0, CTXN, 0))
            pi = 0
            for (s0, slen, o0) in seqs:
                for b0 in range(0, slen, NB):
                    n = min(NB, slen - b0)
                    lo = max(0, b0 - 16)
                    hi = min(slen, b0 + n + 16)
                    P.op('dve', lambda e: e.memset(U[:], 0.0), w=['U'])
                    P.dma('sp', U[:, 16 + lo - b0:16 + hi - b0], pT[0, :, s0 + lo:s0 + hi], r=[('pT', 0, t) for t in range(len(tiles))], w=['U'])
                    NP = n + 32
                    src = U
                    for lv, sh in enumerate([1, 2, 4, 8]):
                        dstw = W[lv % 2]
                        P.op('dve', lambda e, src=src, dstw=dstw, sh=sh, NP=NP: e.tensor_tensor(
                            out=dstw[:, sh:NP], in0=src[:, sh:NP], in1=src[:, 0:NP - sh], op=ALU.add),
                            r=['U', ('W', 0), ('W', 1)], w=[('W', lv % 2)])
                        w_ = 2 * sh
                        off = 16 + w_ // 2 - 1
                        if lv == 0:
                            P.op('dve', lambda e, dstw=dstw, off=off, n=n, lv=lv: e.tensor_scalar(
                                out=comb[:, :n], in0=dstw[:, off:off + n], scalar1=selw_sb[:, lv:lv + 1], scalar2=None,
                                op0=ALU.mult), r=[('W', lv % 2), 'selw'], w=['comb'])
                        else:
                            P.op('dve', lambda e, dstw=dstw, off=off, n=n, lv=lv: e.scalar_tensor_tensor(
                                out=comb[:, :n], in0=dstw[:, off:off + n], scalar=selw_sb[:, lv:lv + 1], in1=comb[:, :n],
                                op0=ALU.mult, op1=ALU.add), r=[('W', lv % 2), 'selw', 'comb'], w=['comb'])
                        src = dstw
                    if b0 == 0:
                        P.op('pool', lambda e: e.tensor_tensor(out=comb[:, 0:8], in0=comb[:, 0:8], in1=efix_sb[:, 0:8],
                                                               op=ALU.mult), r=['comb', 'efix'], w=['comb'])
                    if b0 + n == slen:
                        P.op('pool', lambda e, n=n: e.tensor_tensor(out=comb[:, n - 8:n], in0=comb[:, n - 8:n],
                                                                    in1=efix_sb[:, 8:16], op=ALU.mult),
                             r=['comb', 'efix'], w=['comb'])
                    P.op('dve', lambda e, n=n: e.tensor_tensor(out=diffb[:, :n], in0=comb[:, :n], in1=U[:, 16:16 + n],
                                                               op=ALU.subtract), r=['comb', 'U'], w=['diffb'])
                    for c0 in range(0, n, 512):
                        cs = min(512, n - c0)
                        pb = 6 + pi % 2
                        o = pob[pi % 2]
                        ok = ('pob', pi % 2)
                        pi += 1
                        P.op('pe', lambda e, c0=c0, cs=cs, pb=pb: e.matmul(bank[pb][:, :cs], lhsT=wpool_b[:],
                                                                            rhs=diffb[:, c0:c0 + cs], start=True, stop=True),
                             r=['wpool_b', 'diffb'], w=[('ps', pb)])
                        P.op('act', lambda e, o=o, pb=pb, cs=cs: e.activation(out=o[:, :cs], in_=bank[pb][:, :cs],
                                                                               func=AF.Copy, scale=pscale_sb[:, 0:1]),
                             r=[('ps', pb), 'pscale'], w=[ok])
                        P.dma('pool', brb.dst(0, o0 + b0 + c0, cs), o[:, :cs], r=[ok], w=['brb'])
            P.fence()
            A.release(m_ph)
        if do_attn:
            PT = [[A.alloc("PT%d_%d" % (m, i), [128, 512], BF16) for i in range(2)] for m in range(2)]
            osb = A.alloc("osb", [128, 2, 4, 130])
            rec = A.alloc("rec", [128, 2, 4])
            o1 = A.alloc("o1", [128, 4, 128])
            o2 = A.alloc("o2", [128, 4, 128])
            ssq = A.alloc("ssq", [128, 4])
            oT = A.alloc("oT", [128, 512])
            def oacc(m, qs):
                i = m * 4 + qs
                return bank[4 + i // 3][:, (i % 3) * 130:(i % 3) * 130 + 130], ('ps', 4 + i // 3)
            qjobs = []
            if ctx_out:
                qjobs.append((0, CTXN, (0, 2), 0))
            for i in range(L // 512):
                qjobs.append((CTXN + i * 512, 512, (0, NKT), (CTXN if ctx_out else 0) + i * 512))
            si = 0
            for (q0, nq, (k0, k1), orow) in qjobs:
                nqs = nq // 128
                started = set()
                def emit_S(kt):
                    for m in range(2):
                        sb_ = kt % 2
                        pb = m * 2 + sb_
                        P.op('pe', lambda e, m=m, kt=kt, q0=q0, nq=nq, pb=pb: e.matmul(
                            bank[pb][:, :nq], lhsT=KT[m * 64:(m + 1) * 64, kt * 128:(kt + 1) * 128],
                            rhs=QT[m * 64:(m + 1) * 64, q0:q0 + nq], start=True, stop=True),
                            r=['QK'], w=[('ps', pb)])
                        P.op('act', lambda e, m=m, sb_=sb_, pb=pb, nq=nq: e.activation(
                            out=PT[m][sb_][:, :nq], in_=bank[pb][:, :nq], func=AF.Exp, scale=0.125),
                            r=[('ps', pb)], w=[('PT', m, sb_)])

                def emit_PV(kt):
                    for m in range(2):
                        sb_ = kt % 2
                        for qs in range(nqs):
                            oap, okey = oacc(m, qs)
                            first_in_bank = (kt == k0) and (okey not in started)
                            started.add(okey)
                            P.op('pe', lambda e, m=m, sb_=sb_, qs=qs, kt=kt, oap=oap, fib=first_in_bank, k1=k1: e.matmul(
                                oap, lhsT=PT[m][sb_][:, qs * 128:(qs + 1) * 128], rhs=V[:, kt, :],
                                start=fib, stop=(kt == k1 - 1)),
                                r=[('PT', m, sb_), 'V', 'Vones'], w=[okey])
                emit_S(k0)
                for kt in range(k0, k1):
                    if kt + 1 < k1:
                        emit_S(kt + 1)
                    emit_PV(kt)
                for m in range(2):
                    for qs in range(nqs):
                        oap, okey = oacc(m, qs)
                        P.op('dve' if (m + qs) % 2 else 'act',
                             (lambda e, m=m, qs=qs, oap=oap: e.tensor_copy(out=osb[:, m, qs, :], in_=oap)) if (m + qs) % 2
                             else (lambda e, m=m, qs=qs, oap=oap: e.copy(out=osb[:, m, qs, :], in_=oap)),
                             r=[okey], w=['osb'])
                P.op('dve', lambda e, nqs=nqs: e.reciprocal(out=rec[:, :, :nqs], in_=osb[:, :, :nqs, 128]),
                     r=['osb'], w=['rec'])
                P.op('dve', lambda e, nqs=nqs: e.tensor_scalar(out=rec[:, 1, :nqs], in0=rec[:, 1, :nqs],
                                                               scalar1=lam_sb[:, 0:1], scalar2=None, op0=ALU.mult),
                     r=['rec', 'lam'], w=['rec'])
                P.op('dve', lambda e, nqs=nqs: e.tensor_tensor(
                    out=o1[:, :nqs, :], in0=osb[:, 0, :nqs, 0:128],
                    in1=rec[:, 0, :nqs].unsqueeze(2).broadcast_to([128, nqs, 128]), op=ALU.mult),
                    r=['osb', 'rec'], w=['o1'])
                P.op('pool', lambda e, nqs=nqs: e.tensor_tensor(
                    out=o2[:, :nqs, :], in0=osb[:, 1, :nqs, 0:128],
                    in1=rec[:, 1, :nqs].unsqueeze(2).broadcast_to([128, nqs, 128]), op=ALU.mult),
                    r=['osb', 'rec'], w=['o2'])
                P.op('dve', lambda e, nqs=nqs: e.tensor_tensor(out=o1[:, :nqs, :], in0=o1[:, :nqs, :], in1=o2[:, :nqs, :],
                                                               op=ALU.add), r=['o1', 'o2'], w=['o1'])
                P.op('pool', lambda e, nqs=nqs: e.tensor_tensor(out=o2[:, :nqs, :], in0=o1[:, :nqs, :], in1=o1[:, :nqs, :],
                                                                op=ALU.mult), r=['o1', 'o2'], w=['o2'])
                P.op('dve', lambda e, nqs=nqs: e.tensor_reduce(out=ssq[:, :nqs], in_=o2[:, :nqs, :], axis=AX.X, op=ALU.add),
                     r=['o2'], w=['ssq'])
                P.op('act', lambda e, nqs=nqs: e.activation(out=ssq[:, :nqs], in_=ssq[:, :nqs], func=AF.Sqrt,
                                                            scale=1.0 / 128, bias=eps_sb[:, 0:1]),
                     r=['ssq', 'eps'], w=['ssq'])
                P.op('dve', lambda e, nqs=nqs: e.reciprocal(out=ssq[:, :nqs], in_=ssq[:, :nqs]), r=['ssq'], w=['ssq'])
                P.op('dve', lambda e, nqs=nqs: e.tensor_tensor(
                    out=o1[:, :nqs, :], in0=o1[:, :nqs, :],
                    in1=ssq[:, :nqs].unsqueeze(2).broadcast_to([128, nqs, 128]), op=ALU.mult),
                    r=['o1', 'ssq'], w=['o1'])
                P.op('pool', lambda e, nqs=nqs: e.tensor_tensor(
                    out=o2[:, :nqs, :], in0=o1[:, :nqs, :],
                    in1=subg_sb[:].unsqueeze(1).broadcast_to([128, nqs, 128]), op=ALU.mult),
                    r=['o1', 'subg', 'o2'], w=['o2'])
                for qs in range(nqs):
                    P.op('pe', lambda e, qs=qs: e.matmul(bank[7][:, qs * 128:(qs + 1) * 128], lhsT=o2[:, qs, :], rhs=ident_sb[:],
                                                         start=True, stop=True), r=['o2', 'identA'], w=[('ps', 7)])
                P.op('act', lambda e, nq=nq: e.copy(out=oT[:, :nq], in_=bank[7][:, :nq]), r=[('ps', 7)], w=['oT'])
                P.dma('sp', brb.dst(1, orow, nq), oT[:, :nq], r=['oT'], w=['brb'])
            P.fence()
        if do_rwkv:
            A.release(base_mark)
            rwkv_phase(nc, P, A, bank, pT, L, ctx_out, (lambda c0, n: brb.dst(2, c0, n)), pfx)
        A.release(base_mark)


NORM_EPS = 1e-6


def phase_B(nc, P, A, bank, pfx, NT_LAT, NT_CTX, x_load, gath, off_lat, x_store, NEXP=32):
    NT = NT_LAT + NT_CTX
    di = lambda name, shape: nc.dram_tensor(pfx + name, list(shape), F32, kind="ExternalInput").ap()
    selq = di("selq", [128, 4])
    cT = di("cT", [128, 8, 2])
    wada = di("wada", [128, 8, 6144])
    bada = di("bada", [128, 48])
    g12 = di("g12", [128, 2, 8])
    wgate = di("wgate", [128, 8, 3072])
    wbr = di("wbr", [128, 12, 1024])
    wout = di("wout", [128, 8, 1024])
    wrt = di("wrt", [128, 8, 36])
    wg = di("wg", [NEXP, 128, 8, 512])
    wu = di("wu", [NEXP, 128, 8, 512])
    wd = di("wd", [NEXP, 128, 4, 1024])
    selE = di("selE", [32, 32 * 128])
    ident = di("ident", [128, 128])
    x2s = nc.dram_tensor(pfx + "x2_scr", [128, 8, NT], F32, kind="Internal").ap()
    xn2s = nc.dram_tensor(pfx + "xn2_scr", [128, 8, NT], BF16, kind="Internal").ap()
    gTs = nc.dram_tensor(pfx + "gT_scr", [32, NT], F32, kind="Internal").ap()
    base_mark = A.mark()

    def TT(eng, out, a, b, op, r, w):
        P.op(eng, lambda e: e.tensor_tensor(out=out, in0=a, in1=b, op=op), r=r, w=w)

    def TS(eng, out, a, s1, s2, op0, op1, r, w):
        if op1 is None:
            P.op(eng, lambda e: e.tensor_scalar(out=out, in0=a, scalar1=s1, scalar2=None, op0=op0), r=r, w=w)
        else:
            P.op(eng, lambda e: e.tensor_scalar(out=out, in0=a, scalar1=s1, scalar2=s2, op0=op0, op1=op1), r=r, w=w)

    def STT(out, a, s, b, op0, op1, r, w):
        P.op('dve', lambda e: e.scalar_tensor_tensor(out=out, in0=a, scalar=s, in1=b, op0=op0, op1=op1), r=r, w=w)

    def ACT(out, a, func, r, w, scale=1.0, bias=None):
        if bias is None:
            P.op('act', lambda e: e.activation(out=out, in_=a, func=func, scale=scale), r=r, w=w)
        else:
            P.op('act', lambda e: e.activation(out=out, in_=a, func=func, scale=scale, bias=bias), r=r, w=w)

    def MM(out, lhsT, rhs, r, w, start=True, stop=True):
        P.op('pe', lambda e: e.matmul(out, lhsT=lhsT, rhs=rhs, start=start, stop=stop), r=r, w=w)

    def RED(out, a, op, r, w):
        P.op('dve', lambda e: e.tensor_reduce(out=out, in_=a, axis=AX.X, op=op), r=r, w=w)

    if True:
        ones = A.alloc("ones", [128, 128])
        selq_sb = A.alloc("selq", [128, 4])
        P.dma('sp', selq_sb[:], selq, w=['selq'])
        eps_sb = A.alloc("eps", [128, 1])
        mod_sb = A.alloc("mod", [128, 48, 2])
        A1 = A.alloc("A1", [128, 8, 2])
        A2 = A.alloc("A2", [128, 8, 2])
        g12_sb = A.alloc("g12", [128, 2, 8])
        ident_sb = A.alloc("ident", [128, 128])
        wrt_sb = A.alloc("wrt", [128, 8, 36])
        P.op('pool', lambda e: e.memset(ones[:], 1.0), w=['ones'])
        P.op('pool', lambda e: e.memset(eps_sb[:], NORM_EPS), w=['eps'])
        P.dma('sp', g12_sb[:], g12, w=['g12'])
        P.dma('sp', ident_sb[:], ident, w=['ident'])
        P.dma('sp', wrt_sb[:], wrt, w=['wrt'])
        mB1 = A.mark()
        c_sb = A.alloc("c", [128, 8, 2])
        s_sb = A.alloc("s", [128, 8, 2])
        bada_sb = A.alloc("bada", [128, 48])
        wgate_b = A.alloc("wgate_b", [128, 8, 3072], BF16)
        wbr_b = A.alloc("wbr_b", [128, 12, 1024], BF16)
        wout_b = A.alloc("wout_b", [128, 8, 1024], BF16)
        xt = [A.alloc("xt%d" % i, [128, 8, 512]) for i in range(2)]
        sq = A.alloc("sq", [128, 8, 512])
        rstd = A.alloc("rstd", [128, 512])
        xn1 = A.alloc("xn1", [128, 8, 512], BF16)
        brf = A.alloc("brf", [128, 4, 512])
        gTt = A.alloc("gTt", [32, 512])
        brb = A.alloc("brb", [128, 12, 512], BF16)
        sig = [A.alloc("sig%d" % i, [128, 512]) for i in range(2)]
        mrg = A.alloc("mrg", [128, 2, 512])
        mk_ = A.mark()
        mrgb = A.alloc("mrgb", [128, 8, 512], BF16)
        A.release(mk_)
        brsel = A.alloc("brsel", [128, 4, 512])
        x2 = A.alloc("x2", [128, 8, 512])
        rt = A.alloc("rt", [128, 80])
        g32 = A.alloc("g32", [128, 32])
        P.dma('sp', c_sb[:], cT, w=['c_sb'])
        P.dma('sp', bada_sb[:], bada, w=['bada'])
        ACT(s_sb[:], c_sb[:], AF.Silu, ['c_sb'], ['s_sb'])
        for piece in range(12):
            b = piece % 2
            P.dma('sp', xt[b][:], wada[:, :, piece * 512:(piece + 1) * 512], w=[('xt', b)])
            for occ in range(4):
                oc = piece * 4 + occ
                for k in range(8):
                    MM(bank[0][:, oc * 2:oc * 2 + 2], xt[b][:, k, occ * 128:(occ + 1) * 128], s_sb[:, k, :],
                       [('xt', b), 's_sb'], [('ps', 0)], start=(k == 0), stop=(k == 7))
        TT('dve', mod_sb[:], bank[0][:, 0:96].rearrange("p (a b) -> p a b", b=2),
           bada_sb[:].unsqueeze(2).broadcast_to([128, 48, 2]), ALU.add, [('ps', 0), 'bada'], ['mod'])
        for (Ax, m_scale, gi) in ((A1, 1, 0), (A2, 4, 1)):
            TS('dve', Ax[:], mod_sb[:, m_scale * 8:(m_scale + 1) * 8, :], 1.0, None, ALU.add, None, ['mod'], ['Ax%d' % gi])
            TT('dve', Ax[:], Ax[:], g12_sb[:, gi, :].unsqueeze(2).broadcast_to([128, 8, 2]), ALU.mult, ['Ax%d' % gi, 'g12'], ['Ax%d' % gi])
        wi = 0
        for (src, dstw, nk, ncol, key) in ((wgate, wgate_b, 8, 3072, 'wgate_b'), (wbr, wbr_b, 12, 1024, 'wbr_b'), (wout, wout_b, 8, 1024, 'wout_b')):
            for c0 in range(0, ncol, 512):
                for kb in range(0, nk, 8):
                    kn = min(8, nk - kb)
                    b = wi % 2
                    wi += 1
                    P.dma('sp', xt[b][:, :kn, :], src[:, kb:kb + kn, c0:c0 + 512], w=[('xt', b)])
                    P.op('pool' if wi % 2 else 'dve', lambda e, b=b, kn=kn, kb=kb, c0=c0, dstw=dstw: e.tensor_copy(
                        out=dstw[:, kb:kb + kn, c0:c0 + 512], in_=xt[b][:, :kn, :]), r=[('xt', b)], w=[key])
        tiles = [(i * 512, 512, 0) for i in range(NT_LAT // 512)]
        if NT_CTX:
            tiles.append((NT_LAT, NT_CTX, 1))

        def norm_mod(src, sz, j, Ax, m_shift, out_fn, okeys, srckeys):
            P.op('act', lambda e: e.activation(out=sq[:, :, :sz], in_=src[:, :, :sz], func=AF.Square), r=srckeys, w=['sq'])
            for k in range(8):
                MM(bank[0][:, :sz], ones[:], sq[:, k, :sz], ['ones', 'sq'], [('ps', 0)], start=(k == 0), stop=(k == 7))
            ACT(rstd[:, :sz], bank[0][:, :sz], AF.Sqrt, [('ps', 0), 'eps'], ['rstd'], scale=1.0 / 1024, bias=eps_sb[:, 0:1])
            P.op('dve', lambda e: e.reciprocal(out=rstd[:, :sz], in_=rstd[:, :sz]), r=['rstd'], w=['rstd'])
            TT('dve', sq[:, :, :sz], src[:, :, :sz], rstd[:, :sz].unsqueeze(1).broadcast_to([128, 8, sz]), ALU.mult,
               srckeys + ['rstd', 'sq'], ['sq'])
            for k in range(8):
                if k % 2:
                    TS('dve', out_fn(k), sq[:, k, :sz], Ax[:, k, j:j + 1],
                       mod_sb[:, m_shift * 8 + k, j:j + 1], ALU.mult, ALU.add, ['sq', 'Ax0', 'Ax1', 'mod'], okeys)
                else:
                    ACT(out_fn(k), sq[:, k, :sz], AF.Identity, ['sq', 'Ax0', 'Ax1', 'mod'], okeys, scale=Ax[:, k, j:j + 1],
                        bias=mod_sb[:, m_shift * 8 + k, j:j + 1])

        pi = 0
        for ti, (t0, sz, j) in enumerate(tiles):
            b = ti % 2
            x_load(xt[b], t0, sz, ('xt', b))
            for n3 in range(3):
                for q in range(4):
                    col0 = (off_lat + q * NT_LAT + t0) if j == 0 else (q * 64 + (t0 - NT_LAT))
                    P.dma('sp', brf[:, :, :sz], gath.gsrc(n3, col0, sz), r=['gath'], w=['brf'])
                    if q == 0:
                        TS('dve', brsel[:, :, :sz], brf[:, :, :sz], selq_sb[:, 0:1], None, ALU.mult, None, ['brf', 'selq'], ['mrgb'])
                    else:
                        STT(brsel[:, :, :sz], brf[:, :, :sz], selq_sb[:, q:q + 1], brsel[:, :, :sz], ALU.mult, ALU.add,
                            ['brf', 'selq', 'mrgb'], ['mrgb'])
                P.op('pool', lambda e, sz=sz, n3=n3: e.tensor_copy(out=brb[:, n3 * 4:(n3 + 1) * 4, :sz], in_=brsel[:, :, :sz]),
                     r=['mrgb'], w=['brb'])
            norm_mod(xt[b], sz, j, A1, 0, lambda k, sz=sz: xn1[:, k, :sz], ['xn1'], [('xt', b)])
            for dc in range(8):
                for n in range(3):
                    pg = bank[1 + pi % 2]
                    pgk = ('ps', 1 + pi % 2)
                    pbk = bank[3 + pi % 2]
                    pbkk = ('ps', 3 + pi % 2)
                    sg_ = sig[pi % 2]
                    sgk = ('sig', pi % 2)
                    pi += 1
                    for k in range(8):
                        MM(pg[:, :sz], wgate_b[:, k, n * 1024 + dc * 128:n * 1024 + (dc + 1) * 128], xn1[:, k, :sz],
                           ['wgate_b', 'xn1'], [pgk], start=(k == 0), stop=(k == 7))
                    for kc in range(4):
                        MM(pbk[:, :sz], wbr_b[:, n * 4 + kc, dc * 128:(dc + 1) * 128], brb[:, n * 4 + kc, :sz],
                           ['wbr_b', 'brb'], [pbkk], start=(kc == 0), stop=(kc == 3))
                    ACT(sg_[:, :sz], pg[:, :sz], AF.Sigmoid, [pgk], [sgk])
                    if n == 0:
                        TT('dve', mrg[:, dc % 2, :sz], sg_[:, :sz], pbk[:, :sz], ALU.mult, [sgk, pbkk], ['mrg'])
                    else:
                        TT('dve', sg_[:, :sz], sg_[:, :sz], pbk[:, :sz], ALU.mult, [sgk, pbkk], [sgk])
                        TT('pool', mrg[:, dc % 2, :sz], mrg[:, dc % 2, :sz], sg_[:, :sz], ALU.add, ['mrg', sgk], ['mrg'])
                P.op('act', lambda e, dc=dc, sz=sz: e.copy(out=mrgb[:, dc, :sz], in_=mrg[:, dc % 2, :sz]), r=['mrg'], w=['mrgb'])
            for dc in range(8):
                pb_ = bank[5 + dc % 2]
                pk_ = ('ps', 5 + dc % 2)
                for k in range(8):
                    MM(pb_[:, :sz], wout_b[:, k, dc * 128:(dc + 1) * 128], mrgb[:, k, :sz], ['wout_b', 'mrgb'], [pk_],
                       start=(k == 0), stop=(k == 7))
                STT(x2[:, dc, :sz], pb_[:, :sz], mod_sb[:, 2 * 8 + dc, j:j + 1], xt[b][:, dc, :sz], ALU.mult, ALU.add,
                    [pk_, 'mod', ('xt', b)], ['x2'])
            P.dma('pool', x2s[:, :, t0:t0 + sz], x2[:, :, :sz], r=['x2'], w=['x2s'])
            xn2f = xt[b]
            xfk = ('xt', b)
            norm_mod(x2, sz, j, A2, 3, lambda k, sz=sz, xn2f=xn2f: xn2f[:, k, :sz], [xfk], ['x2'])
            P.op('act', lambda e, sz=sz, xn2f=xn2f: e.copy(out=xn1[:, :, :sz], in_=xn2f[:, :, :sz]), r=[xfk], w=['xn1'])
            P.dma('pool', xn2s[:, :, t0:t0 + sz], xn1[:, :, :sz], r=['xn1'], w=['xn2s'])
            for s0 in range(0, sz, 128):
                ns = min(128, sz - s0)
                for k in range(8):
                    MM(bank[7][:ns, 0:36], xn2f[:, k, s0:s0 + ns], wrt_sb[:, k, :], [xfk, 'wrt'], [('ps', 7)],
                       start=(k == 0), stop=(k == 7))
                lg = rt[:ns, 0:36]
                P.op('dve', lambda e, ns=ns: e.tensor_copy(out=rt[:ns, 0:36], in_=bank[7][:ns, 0:36]), r=[('ps', 7)], w=['rt'])
                gmax = rt[:ns, 36:37]
                RED(gmax, rt[:ns, 0:4], ALU.max, ['rt'], ['rt'])
                ohg = rt[:ns, 37:41]
                TS('dve', ohg, rt[:ns, 0:4], gmax, None, ALU.is_equal, None, ['rt'], ['rt'])
                ngm = rt[:ns, 41:42]
                TS('dve', ngm, gmax, -1.0, None, ALU.mult, None, ['rt'], ['rt'])
                eg = rt[:ns, 42:46]
                ACT(eg, rt[:ns, 0:4], AF.Exp, ['rt'], ['rt'], bias=ngm)
                pgr = rt[:ns, 46:47]
                RED(pgr, eg, ALU.add, ['rt'], ['rt'])
                P.op('dve', lambda e, pgr=pgr: e.reciprocal(out=pgr, in_=pgr), r=['rt'], w=['rt'])
                TT('dve', g32[:ns, :].rearrange("p (g e) -> p g e", e=8), rt[:ns, 4:36].rearrange("p (g e) -> p g e", e=8),
                   ohg.unsqueeze(2).broadcast_to([ns, 4, 8]), ALU.mult, ['rt'], ['g32'])
                les = rt[:ns, 47:55]
                RED(les, g32[:ns, :].rearrange("p (g e) -> p e g", e=8), ALU.add, ['g32'], ['rt'])
                top1 = rt[:ns, 55:56]
                RED(top1, les, ALU.max, ['rt'], ['rt'])
                oh1 = rt[:ns, 56:64]
                TS('dve', oh1, les, top1, None, ALU.is_equal, None, ['rt'], ['rt'])
                le2 = rt[:ns, 64:72]
                STT(le2, oh1, -1e30, les, ALU.mult, ALU.add, ['rt'], ['rt'])
                top2 = rt[:ns, 72:73]
                RED(top2, le2, ALU.max, ['rt'], ['rt'])
                oh2 = rt[:ns, 73:81] if False else None
                d12 = rt[:ns, 41:42]
                TT('dve', d12, top1, top2, ALU.subtract, ['rt'], ['rt'])
                ga = rt[:ns, 42:43]
                gb = rt[:ns, 43:44]
                ACT(ga, d12, AF.Sigmoid, ['rt'], ['rt'])
                ACT(gb, d12, AF.Sigmoid, ['rt'], ['rt'], scale=-1.0)
                TT('dve', rt[:ns, 42:44], rt[:ns, 42:44], pgr.broadcast_to([ns, 2]), ALU.mult, ['rt'], ['rt'])
                TS('dve', le2, le2, top2, gb, ALU.is_equal, ALU.mult, ['rt'], ['rt'])
                STT(les, oh1, ga, le2, ALU.mult, ALU.add, ['rt'], ['rt'])
                TT('dve', g32[:ns, :].rearrange("p (g e) -> p g e", e=8), ohg.unsqueeze(2).broadcast_to([ns, 4, 8]),
                   les.unsqueeze(1).broadcast_to([ns, 4, 8]), ALU.mult, ['rt', 'g32'], ['g32'])
                MM(bank[7][0:32, 64:64 + ns], g32[:ns, :], ident_sb[:ns, :ns], ['g32', 'ident'], [('ps', 7)])
                P.op('act', lambda e, s0=s0, ns=ns: e.copy(out=gTt[:, s0:s0 + ns], in_=bank[7][0:32, 64:64 + ns]),
                     r=[('ps', 7)], w=['gTt'])
            P.dma('pool', gTs[:, t0:t0 + sz], gTt[:, :sz], r=['gTt'], w=['gTs'])
        P.fence()
        A.release(mB1)
        selE_sb = A.alloc("selE", [32, 32 * 128])
        P.dma('sp', selE_sb[:], selE, w=['selE'])
        lat_ = tiles[:NT_LAT // 512]
        groups = [lat_[i:i + 2] for i in range(0, len(lat_), 2)]
        if NT_CTX:
            groups[-1] = groups[-1] + [tiles[-1]]
        GMAX = max(sum(t[1] for t in g) for g in groups)
        yacc = A.alloc("yacc", [128, 8, GMAX])
        xn2 = A.alloc("xn2g", [128, 8, GMAX], BF16)
        gT = A.alloc("gTg", [32, GMAX])
        wst = [A.alloc("wst%d" % i, [128, 4, 512]) for i in range(4)]
        wgb = [A.alloc("wgb%d" % i, [128, 8, 512], BF16) for i in range(2)]
        wub = [A.alloc("wub%d" % i, [128, 8, 512], BF16) for i in range(2)]
        wdb = [A.alloc("wdb%d" % i, [128, 4, 1024], BF16) for i in range(2)]
        hs = [A.alloc("hs%d" % i, [128, 512]) for i in range(2)]
        actb = [A.alloc("actb%d" % i, [128, 4, 512], BF16) for i in range(2)]
        x2r = A.alloc("x2r", [128, 8, 512])
        si = 0
        ai = 0
        hi_ = 0
        for gi, grp in enumerate(groups):
            g0 = grp[0][0]
            gsz = sum(t[1] for t in grp)
            P.dma('sp', xn2[:, :, :gsz], xn2s[:, :, g0:g0 + gsz], r=['xn2s'], w=['xn2g'])
            P.dma('sp', gT[:, :gsz], gTs[:, g0:g0 + gsz], r=['gTs'], w=['gTg'])
            for e_ in range(NEXP):
                wb = e_ % 2
                pieces = [(wg[e_, :, 0:4, :], wgb[wb][:, 0:4, :], ('wgb', wb)), (wg[e_, :, 4:8, :], wgb[wb][:, 4:8, :], ('wgb', wb)),
                          (wu[e_, :, 0:4, :], wub[wb][:, 0:4, :], ('wub', wb)), (wu[e_, :, 4:8, :], wub[wb][:, 4:8, :], ('wub', wb)),
                          (wd[e_, :, :, 0:512], wdb[wb][:, :, 0:512], ('wdb', wb)), (wd[e_, :, :, 512:1024], wdb[wb][:, :, 512:1024], ('wdb', wb))]
                for (src, dst, key) in pieces:
                    sb_ = si % 4
                    si += 1
                    P.dma('sp', wst[sb_][:], src, w=[('wst', sb_)])
                    eng = ('pool', 'dve', 'act')[si % 3] if False else ('pool' if si % 2 else 'act')
                    if eng == 'act':
                        P.op('act', lambda e, dst=dst, sb_=sb_: e.copy(out=dst, in_=wst[sb_][:]), r=[('wst', sb_)], w=[key])
                    else:
                        P.op('pool', lambda e, dst=dst, sb_=sb_: e.tensor_copy(out=dst, in_=wst[sb_][:]), r=[('wst', sb_)], w=[key])
                for (t0, sz, j) in grp:
                    ti = tiles.index((t0, sz, j))
                    lo = t0 - g0
                    MM(bank[0][:, :sz], selE_sb[:, e_ * 128:(e_ + 1) * 128], gT[:, lo:lo + sz], ['selE', 'gTg'], [('ps', 0)])
                    ab = actb[ai % 2]
                    ak = ('actb', ai % 2)
                    ai += 1
                    for fc in range(4):
                        pgb = bank[1 + fc % 2]
                        pgk = ('ps', 1 + fc % 2)
                        pub = bank[3 + fc % 2]
                        puk = ('ps', 3 + fc % 2)
                        for k in range(8):
                            MM(pgb[:, :sz], wgb[wb][:, k, fc * 128:(fc + 1) * 128], xn2[:, k, lo:lo + sz],
                               [('wgb', wb), 'xn2g'], [pgk], start=(k == 0), stop=(k == 7))
                        for k in range(8):
                            MM(pub[:, :sz], wub[wb][:, k, fc * 128:(fc + 1) * 128], xn2[:, k, lo:lo + sz],
                               [('wub', wb), 'xn2g'], [puk], start=(k == 0), stop=(k == 7))
                        h_ = hs[hi_ % 2]
                        hk = ('hs', hi_ % 2)
                        hi_ += 1
                        ACT(h_[:, :sz], pgb[:, :sz], AF.Silu, [pgk], [hk])
                        TT('dve', h_[:, :sz], h_[:, :sz], pub[:, :sz], ALU.mult, [hk, puk], [hk])
                        TT('dve', ab[:, fc, :sz], h_[:, :sz], bank[0][:, :sz], ALU.mult, [hk, ('ps', 0)], [ak])
                    for dc in range(8):
                        pdb = bank[5 + dc % 3]
                        pdk = ('ps', 5 + dc % 3)
                        for fc in range(4):
                            MM(pdb[:, :sz], wdb[wb][:, fc, dc * 128:(dc + 1) * 128], ab[:, fc, :sz], [('wdb', wb), ak], [pdk],
                               start=(fc == 0), stop=(fc == 3))
                        if e_ == 0:
                            P.op('act', lambda e, dc=dc, lo=lo, sz=sz, pdb=pdb: e.copy(out=yacc[:, dc, lo:lo + sz], in_=pdb[:, :sz]),
                                 r=[pdk], w=['yacc'])
                        else:
                            TT('pool' if False else 'dve', yacc[:, dc, lo:lo + sz], yacc[:, dc, lo:lo + sz], pdb[:, :sz], ALU.add,
                               ['yacc', pdk], ['yacc'])
            for (t0, sz, j) in grp:
                lo = t0 - g0
                P.dma('sp', x2r[:, :, :sz], x2s[:, :, t0:t0 + sz], r=['x2s'], w=['x2r'])
                for dc in range(8):
                    STT(x2r[:, dc, :sz], yacc[:, dc, lo:lo + sz], mod_sb[:, 5 * 8 + dc, j:j + 1], x2r[:, dc, :sz], ALU.mult, ALU.add,
                        ['yacc', 'mod', 'x2r'], ['x2r'])
                x_store(x2r, t0, sz, ['x2r'])
        P.fence()
        A.release(base_mark)


POOL_WINDOWS = (2, 4, 8, 16)
def fm(a):
    return np.ascontiguousarray(a.reshape(8, 128, *a.shape[1:]).swapaxes(0, 1))
def colsel(hg):
    c = []
    c += list(range(hg * 128, hg * 128 + 128))
    c += list(range(512 + hg * 128, 512 + hg * 128 + 128))
    c += list(range(1024 + hg * 128, 1024 + hg * 128 + 128))
    c += list(range(1536 + hg * 128, 1536 + hg * 128 + 128))
    for j in range(3):
        c += list(range(2048 + j * 512 + hg * 128, 2048 + j * 512 + hg * 128 + 128))
    c += list(range(2048 + 1536, 2048 + 1920))
    return np.array(c)
def rope_tables(L):
    nrow = L // 64
    inv = (10000.0 ** (-np.arange(16, dtype=np.float32) / 16)).astype(np.float32)
    R = np.ones((128, 2, nrow), np.float32); C = np.ones((128, 2, 64), np.float32)
    perm = np.zeros((128, 128), np.float32)
    rows = np.arange(nrow, dtype=np.float32); cols = np.arange(64, dtype=np.float32)
    for p in range(128):
        d = p % 64
        blk = d // 16
        f = inv[d % 16]
        sign = -1.0 if blk % 2 == 0 else 1.0
        partner = p + 16 if blk % 2 == 0 else p - 16
        perm[partner, p] = 1.0
        if blk < 2:
            ang = (rows * f).astype(np.float32)
            R[p, 0] = np.cos(ang); R[p, 1] = sign * np.sin(ang)
        else:
            ang = (cols * f).astype(np.float32)
            C[p, 0] = np.cos(ang); C[p, 1] = sign * np.sin(ang)
    return R, C, perm
def edge_fix(w, L):
    t = np.arange(L)
    lo = np.clip(t - w // 2, 0, L - 1); hi = np.clip(t + w // 2 - 1, 0, L - 1)
    ratio = (w / (hi - lo + 1)).astype(np.float32)
    return np.concatenate([ratio[:8], ratio[-8:]])
def inputs_A(inp, l, b, hg, L, lam_init, x=None, ctx=None):
    if x is None:
        x = inp['x'][b, :L]; ctx = inp['ctx'][b]
    xa = np.concatenate([ctx, x], 0)
    R, C, perm = rope_tables(L)
    cs = colsel(hg)
    w = POOL_WINDOWS[hg]
    selw = np.zeros((128, 4), np.float32); selw[:, hg] = 1.0 / w
    d = dict(
        xT=fm(np.ascontiguousarray(xa.T)),
        cT=fm(np.stack([inp['c'][b], inp['c_ctx']], 1)),
        wada=fm(inp['w_ada'][l][:, :2048]),
        bada=np.ascontiguousarray(inp['b_ada'][l][:2048].reshape(16, 128).T),
        g1=np.ascontiguousarray(inp['norm1_g'][l].reshape(8, 128).T),
        win=fm(inp['w_in'][l][:, cs]),
        qkg=np.stack([np.tile(inp['q_norm_g'][l], 2), np.tile(inp['k_norm_g'][l], 2)], 1).astype(np.float32),
        ropeR=R, ropeC=C, perm=perm,
        lamqk=np.ascontiguousarray(np.broadcast_to(inp['lambda_qk'][l].reshape(1, 256), (128, 256))),
        subg=np.ascontiguousarray(np.broadcast_to((inp['subln_g'][l] * np.float32(1 - lam_init)).reshape(1, 128), (128, 128))).astype(np.float32),
        wpool=np.ascontiguousarray(inp['pool_w'][l][hg]),
        pscale=np.ascontiguousarray(inp['pool_scale'][l][hg * 128:(hg + 1) * 128].reshape(128, 1)),
        selw=selw,
        efix=np.ascontiguousarray(np.broadcast_to(edge_fix(w, L).reshape(1, 16), (128, 16))),
    )
    return d

def rw_consts():
    i = np.arange(64)
    incl = (i[:, None] <= i[None, :]).astype(np.float32)
    strict = (i[:, None] < i[None, :]).astype(np.float32)
    ones = np.ones((64, 64), np.float32)
    MkN = (i[None, :] < i[:, None]).astype(np.float32)
    return np.concatenate([incl, strict, ones, strict, incl, MkN, np.eye(64, dtype=np.float32)], 1)
def inputs_rw(inp, l, hg):
    mu = inp['shift_mu'][l]
    cmu = np.zeros((128, 6, 2), np.float32)
    p = np.arange(128)
    for g in range(3):
        cmu[:, g, :] = mu[:, g * 512 + hg * 128 + p].T
    for g, base in ((3, 1536), (4, 1664), (5, 1792)):
        cmu[:, g, :] = mu[:, base + p].T
    heads = [2 * hg, 2 * hg + 1]
    w2 = np.zeros((64, 2, 2, 64), np.float32); a2 = np.zeros((64, 2, 2, 64), np.float32)
    w0b = np.zeros((2, 2, 64), np.float32); a0f = np.zeros((64, 2, 2), np.float32)
    prm = np.zeros((64, 2, 5), np.float32)
    for h, hd in enumerate(heads):
        cs = slice(hd * 64, hd * 64 + 64)
        for d in range(2):
            w2[:, d, h, :] = inp['decay_w2'][l][d][:, cs]
            a2[:, d, h, :] = inp['aaa_a2'][l][d][:, cs]
            w0b[d, h, :] = inp['decay_w0'][l][d][cs]
            a0f[:, d, h] = inp['aaa_a0'][l][d][cs]
        prm[:, h, 0] = inp['k_k'][l][cs]; prm[:, h, 1] = inp['k_a'][l][cs]; prm[:, h, 2] = inp['r_k'][l][hd]
        prm[:, h, 3] = inp['gn_w'][l][cs]; prm[:, h, 4] = inp['gn_b'][l][cs]
    return dict(rw_cmu=cmu, rw_g2=np.ascontiguousarray(inp['gate_w2'][l][:, hg * 128:(hg + 1) * 128]),
                rw_w2=w2.reshape(64, 256), rw_a2=a2.reshape(64, 256),
                rw_w0b=np.ascontiguousarray(np.broadcast_to(w0b.reshape(1, 256), (64, 256))),
                rw_a0f=a0f.reshape(64, 4), rw_prm=prm, rw_cst=rw_consts())


def weights_B(inp, l):
    selE = np.zeros((32, 32, 128), np.float32)
    for e in range(32): selE[e, e, :] = 1.0
    return dict(
        wada=fm(inp['w_ada'][l]),
        bada=np.ascontiguousarray(inp['b_ada'][l].reshape(48, 128).T),
        g12=np.ascontiguousarray(np.stack([inp['norm1_g'][l].reshape(8, 128).T, inp['norm2_g'][l].reshape(8, 128).T], 1)),
        wgate=fm(inp['w_in'][l][:, 3968:]),
        wbr=np.ascontiguousarray(inp['w_br'][l].reshape(12, 128, 1024).transpose(1, 0, 2)),
        wout=fm(inp['w_out'][l]),
        wrt=fm(np.concatenate([inp['w_router_group'][l], inp['w_router_expert'][l]], 1)),
        wg=np.ascontiguousarray(inp['w_exp_gate'][l].reshape(32, 8, 128, 512).transpose(0, 2, 1, 3)),
        wu=np.ascontiguousarray(inp['w_exp_up'][l].reshape(32, 8, 128, 512).transpose(0, 2, 1, 3)),
        wd=np.ascontiguousarray(inp['w_exp_down'][l].reshape(32, 4, 128, 1024).transpose(0, 2, 1, 3)),
        selE=selE.reshape(32, 4096), ident=np.eye(128, dtype=np.float32))
def acts_B(xa, br, cvec, c_ctx):
    NT = xa.shape[0]
    return dict(xT=fm(np.ascontiguousarray(xa.T)),
                brT=np.ascontiguousarray(br.T.reshape(12, 128, NT).transpose(1, 0, 2)),
                cT=fm(np.stack([cvec, c_ctx], 1)))


GROUPS = [[0, 1, 2, 3], [4, 5, 6, 7]]


CC_COLS = 2048


class BrStore:
    def __init__(self, nc, name, NQ, ctx_out):
        self.splits = ([(0, 256)] if ctx_out else []) + [(c0, min(CC_COLS, NQ - c0)) for c0 in range(256 if ctx_out else 0, NQ, CC_COLS)]
        self.b = {}
        self.g = {}
        for n in range(3):
            for ci, (c0, cs) in enumerate(self.splits):
                self.b[(n, ci)] = nc.dram_tensor("%s_b%d_%d" % (name, n, ci), [128, cs], F32, kind="Internal").ap()
                self.g[(n, ci)] = nc.dram_tensor("%s_g%d_%d" % (name, n, ci), [4 * 128, cs], F32, kind="Internal").ap()

    def _find(self, col0, ncols):
        for ci, (c0, cs) in enumerate(self.splits):
            if c0 <= col0 and col0 + ncols <= c0 + cs:
                return ci, col0 - c0
        raise AssertionError(("chunk straddle", col0, ncols))

    def dst(self, n, col0, ncols):
        ci, o = self._find(col0, ncols)
        return self.b[(n, ci)][:, o:o + ncols]

    def gsrc(self, n, col0, ncols):
        ci, o = self._find(col0, ncols)
        return self.g[(n, ci)].rearrange("(g p) t -> p g t", g=4)[:, :, o:o + ncols]

    def exchange(self, P):
        for key in self.b:
            P.cc(self.b[key], self.g[key], GROUPS, r=['brb'], w=['gath'])


class XStore:
    def __init__(self, nc, name, NL):
        self.NL = NL
        NT0 = NL + 64
        self.splits = [(c0, min(CC_COLS, NL - c0)) for c0 in range(0, NL, CC_COLS)] + [(NL, 64)]
        self.b = {}
        self.g = {}
        for k in range(8):
            for ci, (c0, cs) in enumerate(self.splits):
                self.b[(k, ci)] = nc.dram_tensor("%s_b%d_%d" % (name, k, ci), [128, cs], F32, kind="Internal").ap()
                self.g[(k, ci)] = nc.dram_tensor("%s_g%d_%d" % (name, k, ci), [4 * 128, cs], F32, kind="Internal").ap()

    def _find(self, col0, ncols):
        for ci, (c0, cs) in enumerate(self.splits):
            if c0 <= col0 and col0 + ncols <= c0 + cs:
                return ci, col0 - c0
        raise AssertionError(("chunk straddle", col0, ncols))

    def store(self, P, src_tile, t0, sz, rkeys):
        ci, o = self._find(t0, sz)
        for k in range(8):
            P.dma('pool', self.b[(k, ci)][:, o:o + sz], src_tile[:, k, :sz], r=rkeys, w=['xnew'])

    def load_local(self, P, dst_tile, t0, sz, key):
        ci, o = self._find(t0, sz)
        for k in range(8):
            P.dma('sp', dst_tile[:, k, :sz], self.b[(k, ci)][:, o:o + sz], r=['xnew'], w=[key])

    def load_gathered(self, P, dst_tile, dcol, rank, t0, sz, key):
        ci, o = self._find(t0, sz)
        for k in range(8):
            P.dma('sp', dst_tile[:, k, dcol:dcol + sz], self.g[(k, ci)][rank * 128:(rank + 1) * 128, o:o + sz], r=['xg'], w=[key])

    def exchange(self, P):
        for key in self.b:
            P.cc(self.b[key], self.g[key], GROUPS, r=['xnew'], w=['xg'])


def build_fused(L=16384, nexp=32, stop_after=99):
    nc = bass.Bass("TRN2", target_bir_lowering=False)
    nc.allow_low_precision("bf16 matmul operands, fp32 accumulation")
    P = Prog(nc)
    A = Arena(nc)
    T = 256 + L
    NL = L // 4
    NT0 = NL + 64
    lam = [0.8 - 0.6 * math.exp(-0.3 * l) for l in range(2)]
    with contextlib.ExitStack() as st:
        bank = [st.enter_context(nc.psum_tensor("bank%d" % i, [128, 512], F32)) for i in range(8)]
        xout = nc.dram_tensor("xout", [128, 8, NL], F32, kind="ExternalOutput").ap()

        def finish():
            stats = P.emit()
            stats['sbuf_peak'] = A.peak
            return nc, stats
        br0 = BrStore(nc, "br0", T, True)
        phase_A(nc, P, A, bank, "A0_", L, True, lam[0], None, br0)
        P.fence()
        if stop_after == 1:
            return finish()
        br0.exchange(P)
        P.fence()
        if stop_after == 2:
            return finish()
        xsh = nc.dram_tensor("xsh", [128, 8, NT0], F32, kind="ExternalInput").ap()
        xs = XStore(nc, "xs", NL)

        def x_load0(dst, t0, sz, key):
            P.dma('sp', dst[:, :, :sz], xsh[:, :, t0:t0 + sz], w=[key])
        phase_B(nc, P, A, bank, "B0_", NL, 64, x_load0, br0, 256, lambda src, t0, sz, rk: xs.store(P, src, t0, sz, rk), NEXP=nexp)
        if stop_after == 3:
            return finish()
        xs.exchange(P)
        P.fence()
        if stop_after == 4:
            return finish()

        def x_loader(dst, t0, sz, key):
            if t0 < 256:
                for r in range(4):
                    xs.load_gathered(P, dst, r * 64, r, NL, 64, key)
            else:
                tt = t0 - 256
                xs.load_gathered(P, dst, 0, tt // NL, tt % NL, sz, key)
        br1 = BrStore(nc, "br1", L, False)
        phase_A(nc, P, A, bank, "A1_", L, False, lam[1], x_loader, br1)
        P.fence()
        if stop_after == 5:
            return finish()
        br1.exchange(P)
        P.fence()
        if stop_after == 6:
            return finish()

        def x_store1(src, t0, sz, rk):
            P.dma('pool', xout[:, :, t0:t0 + sz], src[:, :, :sz], r=rk, final=True)
        phase_B(nc, P, A, bank, "B1_", NL, 0, lambda dst, t0, sz, key: xs.load_local(P, dst, t0, sz, key), br1, 0, x_store1, NEXP=nexp)
        return finish()


def fused_inputs(inp, L, nexp=32, names=None):
    inp = {k: np.asarray(v) for k, v in inp.items()}
    NL = L // 4
    x = inp['x'][:, :L]
    ctx = inp['ctx']
    lam = [0.8 - 0.6 * math.exp(-0.3 * l) for l in range(2)]
    WB = [weights_B(inp, l) for l in range(2)]
    ident = np.eye(128, dtype=np.float32)
    maps = []
    for i in range(8):
        b, hg = i // 4, i % 4
        d = {}
        for l in range(2):
            a = inputs_A(inp, l, b, hg, L, lam[l], x=x[b], ctx=ctx[b])
            if l == 1:
                a.pop('xT')
            a.update(inputs_rw(inp, l, hg))
            a['identA'] = ident
            for k, v in a.items():
                d["A%d_" % l + k] = v
            for k, v in WB[l].items():
                d["B%d_" % l + k] = v[:nexp] if k in ('wg', 'wu', 'wd') else v
            d["B%d_cT" % l] = fm(np.stack([inp['c'][b], inp['c_ctx']], 1))
            selq = np.zeros((128, 4), np.float32)
            selq[:, hg] = 1.0
            d["B%d_selq" % l] = selq
        xa = np.concatenate([x[b, hg * NL:(hg + 1) * NL], ctx[b, hg * 64:(hg + 1) * 64]], 0)
        d['xsh'] = fm(np.ascontiguousarray(xa.T))
        if names is not None:
            d = {k: v for k, v in d.items() if k in names}
        maps.append(d)
    return maps


def fused_gather(results, L):
    NL = L // 4
    out = np.empty((2, L, 1024), np.float32)
    for i in range(8):
        b, q = i // 4, i % 4
        o = np.asarray(results[i]['xout']).transpose(1, 0, 2).reshape(1024, NL).T
        out[b, q * NL:(q + 1) * NL] = o
    return out


def kernel(**inp):
    inp = {k: np.asarray(v) for k, v in inp.items()}
    L = inp['x'].shape[1]
    nc, _ = build_fused(L)
    maps = fused_inputs(inp, L)
    res = run_bass_kernel_spmd(nc, maps, core_ids=list(range(8)))
    del maps
    return fused_gather(res.results, L)
```

```python
import math
import contextlib


import numpy as np
import concourse.bass as bass
import concourse.mybir as mybir
from concourse.bass_utils import run_bass_kernel_spmd

F32 = mybir.dt.float32
BF16 = mybir.dt.bfloat16
I32 = mybir.dt.int32
AF = mybir.ActivationFunctionType
ALU = mybir.AluOpType
AX = mybir.AxisListType

SEM_LIMIT = 8000
DMA_POOL = 40


class Prog:
    def __init__(self, nc):
        self.nc = nc
        self.ops = []

    def op(self, eng, fn, r=(), w=()):
        self.ops.append(dict(eng=eng, fn=fn, r=tuple(r), w=tuple(w), dma=False, final=False))

    def dma(self, eng, out, in_, r=(), w=(), final=False, **kw):
        def fn(e, out=out, in_=in_, kw=kw):
            return e.dma_start(out=out, in_=in_, **kw)
        self.ops.append(dict(eng=eng, fn=fn, r=tuple(r), w=tuple(w), dma=True, final=final))

    def cc(self, ins_ap, out_ap, groups, r=(), w=()):
        def fn(e, ins_ap=ins_ap, out_ap=out_ap, groups=groups):
            return e.collective_compute("AllGather", ALU.bypass, replica_groups=groups, ins=[ins_ap], outs=[out_ap])
        self.ops.append(dict(eng='pool', fn=fn, r=tuple(r), w=tuple(w), dma=True, final=False, cc=True))

    def fence(self):
        self.ops.append(dict(eng=None, fn=None, r=(), w=(), dma=False, final=False, fence=True))

    def emit(self):
        nc = self.nc
        raw_ops = self.ops
        ops = []
        fence_after = {}
        fence_pos = []
        for o in raw_ops:
            if o.get('fence'):
                fence_pos.append(len(ops))
            else:
                ops.append(o)
        self.ops = ops
        n = len(ops)
        fence_deps_at = {}
        prev = 0
        for fp in fence_pos:
            last = {}
            dm = set()
            for i in range(prev, fp):
                o = ops[i]
                if o['dma']:
                    dm.add(i)
                else:
                    last[o['eng']] = i
            fence_deps_at[fp] = set(last.values()) | dm
            prev = fp
        last_w = {}
        readers = {}
        deps = [None] * n
        cur_fence = set()
        first_after = {}
        for i, o in enumerate(ops):
            if i in fence_deps_at:
                cur_fence = fence_deps_at[i]
                first_after = {}
            d = {}

            def add(j, raw):
                d[j] = d.get(j, False) or raw
            for k in o['r']:
                if k in last_w:
                    add(last_w[k], True)
            for k in o['w']:
                if k in last_w:
                    add(last_w[k], False)
                for tok, j in readers.get(k, {}).items():
                    add(j, False)
            keep = set()
            for j, raw in d.items():
                if j == i:
                    continue
                oj = ops[j]
                if (not oj['dma']) and (not o['dma']) and oj['eng'] == o['eng']:
                    if o['eng'] == 'pe':
                        continue
                    if not raw:
                        continue
                keep.add(j)
            tok_e = o['eng']
            if cur_fence and tok_e not in first_after:
                first_after[tok_e] = i
                for j in cur_fence:
                    if ops[j]['dma'] or ops[j]['eng'] != tok_e:
                        keep.add(j)
            deps[i] = keep
            for k in o['r']:
                tok = ('d', i) if o['dma'] else o['eng']
                readers.setdefault(k, {})[tok] = i
            for k in o['w']:
                last_w[k] = i
                readers[k] = {}
        needed = [False] * n
        for i in range(n):
            for j in deps[i]:
                needed[j] = True
        engs = ['pe', 'act', 'dve', 'pool', 'sp']
        cnt = {e: 0 for e in engs}
        sig = [None] * n
        dma_uses = [0] * DMA_POOL
        dma_last = [None] * DMA_POOL
        ndma = 0
        semkeys = set()
        for i, o in enumerate(ops):
            if o.get('cc'):
                ncc_ = getattr(self, '_ncc', 0) + 1
                self._ncc = ncc_
                sig[i] = (('cc', 0), ncc_)
                semkeys.add(('cc', 0))
            elif o['dma']:
                j = ndma % DMA_POOL
                ndma += 1
                dma_uses[j] += 1
                if dma_last[j] is not None:
                    deps[i].add(dma_last[j])
                dma_last[j] = i
                sig[i] = (('dma', j), 16 * dma_uses[j])
                semkeys.add(('dma', j))
            elif needed[i]:
                e = o['eng']
                c = cnt[e]
                cnt[e] += 1
                sk = (e, c // SEM_LIMIT)
                sig[i] = (sk, c % SEM_LIMIT + 1)
                semkeys.add(sk)
        finals = [i for i, o in enumerate(ops) if o['final']]
        seen = {e: {} for e in engs}
        streams = {e: [] for e in engs}
        for i, o in enumerate(ops):
            e = o['eng']
            waits = {}
            for j in deps[i]:
                sk, v = sig[j]
                if seen[e].get(sk, 0) >= v:
                    continue
                waits[sk] = max(waits.get(sk, 0), v)
            for sk, v in waits.items():
                seen[e][sk] = v
            streams[e].append((list(waits.items()), o['fn'], sig[i]))
        fw = {}
        for i in finals:
            sk, v = sig[i]
            if seen['sp'].get(sk, 0) >= v:
                continue
            fw[sk] = max(fw.get(sk, 0), v)
        streams['sp'].append((list(fw.items()), None, None))
        self.stats = dict(n_ops=n, cnt=dict(cnt), ndma=ndma,
                          nwaits={e: sum(len(s[0]) for s in streams[e]) for e in engs})
        semkeys = sorted(semkeys, key=str)
        import contextlib
        with contextlib.ExitStack() as st:
            sems = {}
            for sk in semkeys:
                sems[sk] = st.enter_context(nc.semaphore("s_%s_%s" % (sk[0], sk[1])))
            block = st.enter_context(nc.Block())

            def run(engine, items):
                for waits, fn, sg in items:
                    for sk, v in waits:
                        engine.wait_ge(sems[sk], v)
                    if fn is None:
                        continue
                    ins = fn(engine)
                    if sg is not None:
                        inc = 16 if sg[0][0] == 'dma' else 1
                        ins.then_inc(sems[sg[0]], inc)

            @block.tensor
            def _(e):
                run(e, streams['pe'])

            @block.scalar
            def _(e):
                run(e, streams['act'])

            @block.vector
            def _(e):
                run(e, streams['dve'])

            @block.gpsimd
            def _(e):
                run(e, streams['pool'])

            @block.sync
            def _(e):
                run(e, streams['sp'])
        return self.stats


class Arena:
    LO = 16512
    HI = 229376

    def __init__(self, nc):
        self.nc = nc
        self.top = self.LO
        self.n = 0
        self.peak = self.LO

    def alloc(self, name, shape, dt=F32):
        esz = {F32: 4, BF16: 2, I32: 4}[dt]
        nb = esz
        for s_ in shape[1:]:
            nb *= s_
        off = (self.top + 63) // 64 * 64
        assert off + nb <= self.HI, ("SBUF overflow", name, off + nb)
        self.top = off + nb
        self.peak = max(self.peak, self.top)
        self.n += 1
        return self.nc.alloc_sbuf_tensor_at("%s_%d" % (name, self.n), list(shape), dt, offset=off)

    def mark(self):
        return self.top

    def release(self, m):
        self.top = m


GN_EPS = 64e-5
FINAL_OUT = False
KAPPA = 0.6065306597126334
CTXN = 256


def rwkv_phase(nc, P, A, bank, pT, L, ctx_out, out_ap, pfx=''):
    T = CTXN + L
    di = lambda name, shape: nc.dram_tensor(pfx + name, list(shape), F32, kind="ExternalInput").ap()
    cmu = di("rw_cmu", [128, 6, 2])
    g2s = di("rw_g2", [128, 128])
    w2s = di("rw_w2", [64, 256])
    a2s = di("rw_a2", [64, 256])
    w0b = di("rw_w0b", [64, 256])
    a0f = di("rw_a0f", [64, 4])
    prm = di("rw_prm", [64, 2, 5])
    cst = di("rw_cst", [64, 448])
    uT = nc.dram_tensor(pfx + "uT_scr", [6, 128, T], F32, kind="Internal").ap()
    Yd = nc.dram_tensor(pfx + "Yd_scr", [2, 128, T], F32, kind="Internal").ap()
    Bd = nc.dram_tensor(pfx + "Bd_scr", [2, 128, T], F32, kind="Internal").ap()

    def TT(eng, out, a, b, op, r, w):
        P.op(eng, lambda e: e.tensor_tensor(out=out, in0=a, in1=b, op=op), r=r, w=w)

    def TS(eng, out, a, s1, s2, op0, op1, r, w):
        if op1 is None:
            P.op(eng, lambda e: e.tensor_scalar(out=out, in0=a, scalar1=s1, scalar2=None, op0=op0), r=r, w=w)
        else:
            P.op(eng, lambda e: e.tensor_scalar(out=out, in0=a, scalar1=s1, scalar2=s2, op0=op0, op1=op1), r=r, w=w)

    def STT(out, a, s, b, op0, op1, r, w):
        P.op('dve', lambda e: e.scalar_tensor_tensor(out=out, in0=a, scalar=s, in1=b, op0=op0, op1=op1), r=r, w=w)

    def ACT(out, a, func, r, w, scale=1.0, bias=None):
        if bias is None:
            P.op('act', lambda e: e.activation(out=out, in_=a, func=func, scale=scale), r=r, w=w)
        else:
            P.op('act', lambda e: e.activation(out=out, in_=a, func=func, scale=scale, bias=bias), r=r, w=w)

    def MM(out, lhsT, rhs, r, w, start=True, stop=True):
        P.op('pe', lambda e: e.matmul(out, lhsT=lhsT, rhs=rhs, start=start, stop=stop), r=r, w=w)

    m0 = A.mark()
    cmu_sb = A.alloc("cmu", [128, 6, 2])
    c0_sb = A.alloc("c0", [128, 6])
    g2_sb = A.alloc("g2", [128, 128])
    raw = [A.alloc("raw%d" % i, [128, 6, 514]) for i in range(2)]
    ush = A.alloc("ush", [128, 6, 512])
    sg = A.alloc("sg", [128, 512])
    P.dma('sp', cmu_sb[:], cmu, w=['cmu'])
    P.dma('sp', g2_sb[:], g2s, w=['g2'])
    TT('dve', c0_sb[:], cmu_sb[:, :, 0], cmu_sb[:, :, 1], ALU.add, ['cmu'], ['c0'])
    TS('dve', c0_sb[:], c0_sb[:], -1.0, 1.0, ALU.mult, ALU.add, ['c0'], ['c0'])
    seqs = [(0, CTXN), (CTXN, L)]
    ti = 0
    for (s0, slen) in seqs:
        for b0 in range(0, slen, 512):
            n = min(512, slen - b0)
            rb = raw[ti % 2]
            rk = ('raw', ti % 2)
            ti += 1
            lo = max(0, b0 - 1)
            hi = min(slen, b0 + n + 1)
            if b0 == 0 or b0 + n == slen:
                P.op('pool', lambda e, rb=rb: e.memset(rb[:], 0.0), w=[rk])
            P.dma('sp', rb[:, :, 1 + lo - b0:1 + hi - b0], pT[1:7, :, s0 + lo:s0 + hi].rearrange("g p t -> p g t"),
                  r=['pTall'], w=[rk])
            for g in range(6):
                eng = 'dve'
                TS('pool', ush[:, g, :n], rb[:, g, 1:1 + n], c0_sb[:, g:g + 1], None, ALU.mult, None, [rk, 'c0'], ['ush'])
                STT(ush[:, g, :n], rb[:, g, 0:n], cmu_sb[:, g, 0:1], ush[:, g, :n], ALU.mult, ALU.add, [rk, 'cmu', 'ush'], ['ush'])
                STT(ush[:, g, :n], rb[:, g, 2:2 + n], cmu_sb[:, g, 1:2], ush[:, g, :n], ALU.mult, ALU.add, [rk, 'cmu', 'ush'], ['ush'])
            ACT(sg[:, :n], ush[:, 5, :n], AF.Sigmoid, ['ush'], ['sg'])
            MM(bank[0][:, :n], g2_sb[:], sg[:, :n], ['g2', 'sg'], [('ps', 0)])
            P.op('act', lambda e, n=n: e.copy(out=ush[:, 5, :n], in_=bank[0][:, :n]), r=[('ps', 0), 'ush'], w=['ush'])
            P.dma('pool', uT[:, :, s0 + b0:s0 + b0 + n].rearrange("g p t -> p g t"), ush[:, :, :n], r=['ush'], w=['uTall'])
    P.fence()
    A.release(m0)
    w2_sb = A.alloc("w2", [64, 256])
    a2_sb = A.alloc("a2", [64, 256])
    w0b_sb = A.alloc("w0b", [64, 256])
    a0f_sb = A.alloc("a0f", [64, 4])
    prm_sb = A.alloc("prm", [64, 2, 5])
    cst_sb = A.alloc("cst", [64, 448])
    ones64 = A.alloc("ones64", [64, 64])
    for nm, dst, src in [('w2', w2_sb, w2s), ('a2', a2_sb, a2s), ('w0b', w0b_sb, w0b), ('a0f', a0f_sb, a0f),
                         ('prm', prm_sb, prm), ('cst', cst_sb, cst)]:
        P.dma('sp', dst[:], src, w=[nm])
    P.op('pool', lambda e: e.memset(ones64[:], 1.0), w=['ones64'])
    Tri3 = cst_sb[:, 0:192]
    Mk = cst_sb[:, 192:320]
    MkN = cst_sb[:, 320:384]
    I64 = cst_sb[:, 384:448]
    ST = A.alloc("ST", [64, 4, 64])
    Stmp = A.alloc("Stmp", [64, 4, 64])
    P.op('pool', lambda e: e.memset(ST[:], 0.0), w=['ST'])
    KSEG = 4
    NBUF = KSEG + 1
    f4 = lambda name: A.alloc(name, [64, 4, 64])

    shr = dict(L_=dict(r=A.alloc("ld_r", [64, 2, 2, 64]), k=A.alloc("ld_k", [64, 2, 2, 64]), v=A.alloc("ld_v", [64, 2, 2, 64]),
                       wl=A.alloc("ld_wl", [64, 2, 64]), al=A.alloc("ld_al", [64, 2, 64])),
               uwl=A.alloc("uwl", [64, 2, 64]), ual=A.alloc("ual", [64, 2, 64]), tw=A.alloc("tw", [64, 2, 64]),
               swt=A.alloc("swt", [64, 256]), kk=f4("kk"), kk2=f4("kk2"), rn=f4("rn"), kkn=f4("kkn"), bb=f4("bb"), km=f4("km"),
               t1=f4("t1"), BhT=f4("BhT"), KhT=f4("KhT"), Xsb=f4("Xsb"), Usb=f4("Usb"), Ysb=f4("Ysb"))

    def alloc_set(i):
        n_ = lambda x: "%s_%d" % (x, i)
        return (shr['L_'], f4(n_("ur")), f4(n_("uk")), f4(n_("uv")), shr['uwl'], shr['ual'],
                shr['tw'], shr['swt'], f4(n_("alr")),
                f4(n_("eI")), f4(n_("eE")), f4(n_("eN")), f4(n_("eT")), A.alloc(n_("gC"), [64, 4]),
                shr['kk'], shr['kk2'], shr['rn'], shr['kkn'], shr['bb'], shr['km'], shr['t1'],
                A.alloc(n_("AR"), [64, 4, 128]), f4(n_("BT")), f4(n_("KTt")), shr['BhT'], shr['KhT'], f4(n_("bon")),
                A.alloc(n_("TM"), [64, 2, 4, 64]), f4(n_("Vt")), A.alloc(n_("Gb"), [64, 4, 128]), A.alloc(n_("Gk"), [64, 4, 128]),
                f4(n_("Nn")), [A.alloc(n_("Pk%d" % j), [64, 2, 4, 64]) for j in range(2)], [f4(n_("Q0")), f4(n_("Q1"))],
                shr['Xsb'], shr['Usb'], shr['Ysb'])
    sets = [alloc_set(i) for i in range(NBUF)]
    prmb = lambda j: prm_sb[:, :, j:j + 1].unsqueeze(1).broadcast_to([64, 2, 2, 64])
    v4 = lambda t: t[:].rearrange("p (d h) t -> p d h t", d=2)
    SHARED = set(['w2', 'a2', 'w0b', 'a0f', 'prm', 'cst', 'ones64', 'uTall', 'Bdall', 'Ydall', 'ST', 'Stmp',
                  'ld', 'wl', 'al', 'tw', 'swt', 'kk', 'kk2', 'rn', 'kkn', 'bb', 'km', 't1', 'BhT', 'KhT', 'Xsb', 'Usb', 'Ysb'])
    cur = {'b': None}

    def kmap(keys):
        if cur['b'] is None:
            return list(keys)
        return [k if (k in SHARED or (isinstance(k, tuple) and k[0] == 'ps')) else ('rw', k, cur['b']) for k in keys]
    _op, _dma = P.op, P.dma

    def Pop(eng, fn, r=(), w=()):
        _op(eng, fn, r=kmap(r), w=kmap(w))

    def Pdma(eng, out, in_, r=(), w=(), **kw):
        _dma(eng, out, in_, r=kmap(r), w=kmap(w), **kw)

    def TT(eng, out, a, b, op, r, w):
        Pop(eng, lambda e: e.tensor_tensor(out=out, in0=a, in1=b, op=op), r=r, w=w)

    def TS(eng, out, a, s1, s2, op0, op1, r, w):
        if op1 is None:
            Pop(eng, lambda e: e.tensor_scalar(out=out, in0=a, scalar1=s1, scalar2=None, op0=op0), r=r, w=w)
        else:
            Pop(eng, lambda e: e.tensor_scalar(out=out, in0=a, scalar1=s1, scalar2=s2, op0=op0, op1=op1), r=r, w=w)

    def STT(out, a, s, b, op0, op1, r, w):
        Pop('dve', lambda e: e.scalar_tensor_tensor(out=out, in0=a, scalar=s, in1=b, op0=op0, op1=op1), r=r, w=w)

    def ACT(out, a, func, r, w, scale=1.0, bias=None):
        if bias is None:
            Pop('act', lambda e: e.activation(out=out, in_=a, func=func, scale=scale), r=r, w=w)
        else:
            Pop('act', lambda e: e.activation(out=out, in_=a, func=func, scale=scale, bias=bias), r=r, w=w)

    def MM(out, lhsT, rhs, r, w, start=True, stop=True):
        Pop('pe', lambda e: e.matmul(out, lhsT=lhsT, rhs=rhs, start=start, stop=stop), r=r, w=w)

    nlat = L // 64
    steps = [(s, 3 - s) for s in range(4)] + [(4 + s, 4 + nlat - 1 - s) for s in range(nlat)]
    NS = len(steps)

    def gen_step(si):
        cf, cb = steps[si]
        cur['b'] = si % NBUF
        (L_, ur, uk, uv, uwl, ual, tw, swt, alr, eI, eE, eN, eT, gC, kk, kk2, rn, kkn, bb, km, t1, AR, BT, KTt, BhT, KhT, bon,
         TM, Vt, Gb, Gk, Nn, Pk, Qk, Xsb, Usb, Yb) = sets[si % NBUF]
        saved = P.ops
        P.ops = []
        marks = []
        lk = 'ld'
        for d, cidx in enumerate((cf, cb)):
            t0 = cidx * 64
            for nm, row in (('r', 0), ('k', 1), ('v', 2)):
                Pdma('sp', L_[nm][:, d, :, :], uT[row, :, t0:t0 + 64].rearrange("(h c) t -> c h t", h=2), r=['uTall'], w=[lk])
            Pdma('sp', L_['wl'][:, d, :], uT[3, d * 64:(d + 1) * 64, t0:t0 + 64], r=['uTall'], w=[lk])
            Pdma('sp', L_['al'][:, d, :], uT[4, d * 64:(d + 1) * 64, t0:t0 + 64], r=['uTall'], w=[lk])
        for nm, dst in (('r', ur), ('k', uk), ('v', uv)):
            d4 = v4(dst)
            Pop('pool', lambda e, d4=d4, src=L_[nm]: e.tensor_copy(out=d4[:, 0], in_=src[:, 0]), r=[lk], w=[nm])
            Pop('pool', lambda e, d4=d4, src=L_[nm]: e.tensor_copy(out=d4[:, 1], in_=src[:, 1, :, ::-1]), r=[lk], w=[nm])
        for nm, dst in (('wl', uwl), ('al', ual)):
            Pop('pool', lambda e, dst=dst, src=L_[nm]: e.tensor_copy(out=dst[:, 0, :], in_=src[:, 0, :]), r=[lk], w=[nm])
            Pop('pool', lambda e, dst=dst, src=L_[nm]: e.tensor_copy(out=dst[:, 1, :], in_=src[:, 1, ::-1]), r=[lk], w=[nm])
        ACT(tw[:], uwl[:], AF.Tanh, ['wl'], ['tw'])
        for d in range(2):
            for h in range(2):
                dh = d * 2 + h
                MM(bank[0][0:64, dh * 64:(dh + 1) * 64], tw[:, d, :], w2_sb[:, dh * 64:(dh + 1) * 64], ['tw', 'w2'], [('ps', 0)])
                MM(bank[0][0:64, 256 + dh * 64:256 + (dh + 1) * 64], a2_sb[:, dh * 64:(dh + 1) * 64], ual[:, d, :],
                   ['a2', 'al'], [('ps', 0)])
        TT('dve', swt[:], bank[0][0:64, 0:256], w0b_sb[:], ALU.add, [('ps', 0), 'w0b'], ['swt'])
        ACT(swt[:], swt[:], AF.Sigmoid, ['swt'], ['swt'])
        TT('dve', alr[:], bank[0][0:64, 256:512].rearrange("p (a t) -> p a t", t=64),
           a0f_sb[:].unsqueeze(2).broadcast_to([64, 4, 64]), ALU.add, [('ps', 0), 'a0f'], ['alr'])
        ACT(alr[:], alr[:], AF.Sigmoid, ['alr'], ['alr'])
        cbanks = (1, 0)
        for dh in range(4):
            bk = cbanks[dh // 2]
            MM(bank[bk][0:64, (dh % 2) * 192:(dh % 2) * 192 + 192], swt[:, dh * 64:(dh + 1) * 64], Tri3, ['swt', 'cst'], [('ps', bk)])
        for half in range(2):
            bk = cbanks[half]
            cv = bank[bk][0:64, 0:384].rearrange("p (a x) -> p a x", x=192)
            sl = slice(half * 2, half * 2 + 2)
            ACT(eI[:, sl, :], cv[:, :, 0:64], AF.Exp, [('ps', bk)], ['eI'], scale=-KAPPA)
            ACT(eE[:, sl, :], cv[:, :, 64:128], AF.Exp, [('ps', bk)], ['eE'], scale=-KAPPA)
            ACT(eN[:, sl, :], cv[:, :, 0:64], AF.Exp, [('ps', bk)], ['eN'], scale=KAPPA)
            ACT(gC[:, sl], cv[:, :, 128], AF.Exp, [('ps', bk)], ['gC'], scale=-KAPPA)
        TT('dve', eT[:], eN[:], gC[:].unsqueeze(2).broadcast_to([64, 4, 64]), ALU.mult, ['eN', 'gC'], ['eT'])
        marks.append(len(P.ops))
        TT('dve', v4(kk), v4(uk), prmb(0), ALU.mult, ['k', 'prm'], ['kk'])
        TT('pool', kk2[:], kk[:], kk[:], ALU.mult, ['kk'], ['kk2'])
        MM(bank[2][0:64, 0:256], ones64[:], kk2[:].rearrange("p a t -> p (a t)"), ['ones64', 'kk2'], [('ps', 2)])
        ACT(rn[:].rearrange("p a t -> p (a t)"), bank[2][0:64, 0:256], AF.Sqrt, [('ps', 2)], ['rn'])
        TS('dve', rn[:], rn[:], 1e-12, None, ALU.max, None, ['rn'], ['rn'])
        Pop('dve', lambda e: e.reciprocal(out=rn[:], in_=rn[:]), r=['rn'], w=['rn'])
        TT('dve', kkn[:], kk[:], rn[:], ALU.mult, ['kk', 'rn'], ['kkn'])
        TT('pool', bb[:], kkn[:], alr[:], ALU.mult, ['kkn', 'alr'], ['bb'])
        TS('pool', t1[:], alr[:], -1.0, None, ALU.add, None, ['alr'], ['t1'])
        TT('pool', v4(t1), v4(t1), prmb(1), ALU.mult, ['t1', 'prm'], ['t1'])
        STT(km[:], t1[:], 1.0, uk[:], ALU.add, ALU.mult, ['t1', 'k'], ['km'])
        STT(AR[:, :, 0:64], kkn[:], -1.0, eE[:], ALU.mult, ALU.mult, ['kkn', 'eE'], ['AR'])
        TT('pool', AR[:, :, 64:128], ur[:], eI[:], ALU.mult, ['r', 'eI'], ['AR'])
        TT('dve', BT[:], bb[:], eN[:], ALU.mult, ['bb', 'eN'], ['BT'])
        TT('pool', KTt[:], km[:], eN[:], ALU.mult, ['km', 'eN'], ['KTt'])
        TT('dve', BhT[:], bb[:], eT[:], ALU.mult, ['bb', 'eT'], ['BhT'])
        TT('pool', KhT[:], km[:], eT[:], ALU.mult, ['km', 'eT'], ['KhT'])
        TT('pool', t1[:], ur[:], km[:], ALU.mult, ['r', 'km', 't1'], ['t1'])
        TT('pool', v4(t1), v4(t1), prmb(2), ALU.mult, ['t1', 'prm'], ['t1'])
        MM(bank[2][0:64, 256:512], ones64[:], t1[:].rearrange("p a t -> p (a t)"), ['ones64', 't1'], [('ps', 2)])
        TT('dve', bon[:], bank[2][0:64, 256:512].rearrange("p (a t) -> p a t", t=64), uv[:], ALU.mult, [('ps', 2), 'v'], ['bon'])
        Pop('pool', lambda e: e.tensor_copy(out=kk2[:, 2:4, :], in_=bon[:, 2:4, ::-1]), r=['bon', 'kk2'], w=['kk2'])
        Pdma('pool', Bd[0, :, cf * 64:cf * 64 + 64].rearrange("(h c) t -> c h t", h=2), bon[:, 0:2, :], r=['bon'], w=['Bdall'])
        Pdma('pool', Bd[1, :, cb * 64:cb * 64 + 64].rearrange("(h c) t -> c h t", h=2), kk2[:, 2:4, :], r=['kk2'], w=['Bdall'])
        for dh in range(4):
            MM(bank[3][0:64, dh * 64:(dh + 1) * 64], BhT[:, dh, :], I64, ['BhT', 'cst'], [('ps', 3)])
            MM(bank[3][0:64, 256 + dh * 64:256 + (dh + 1) * 64], KhT[:, dh, :], I64, ['KhT', 'cst'], [('ps', 3)])
        Pop('act', lambda e: e.copy(out=TM[:].rearrange("p a b t -> p (a b t)"), in_=bank[3][0:64, :]), r=[('ps', 3)], w=['TM'])
        for dh in range(4):
            MM(bank[2][0:64, dh * 64:(dh + 1) * 64], uv[:, dh, :], I64, ['v', 'cst'], [('ps', 2)])
        Pop('dve', lambda e: e.tensor_copy(out=Vt[:].rearrange("p a t -> p (a t)"), in_=bank[2][0:64, 0:256]), r=[('ps', 2)], w=['Vt'])
        marks.append(len(P.ops))
        for dh in range(4):
            MM(bank[4][0:64, dh * 128:(dh + 1) * 128], BT[:, dh, :], AR[:, dh, :], ['BT', 'AR'], [('ps', 4)])
            MM(bank[5][0:64, dh * 128:(dh + 1) * 128], KTt[:, dh, :], AR[:, dh, :], ['KTt', 'AR'], [('ps', 5)])
        mk4 = Mk.unsqueeze(1).broadcast_to([64, 4, 128])
        TT('dve', Gb[:], bank[4][0:64, :].rearrange("p (a x) -> p a x", x=128), mk4, ALU.mult, [('ps', 4), 'cst'], ['Gb'])
        TT('dve', Gk[:], bank[5][0:64, :].rearrange("p (a x) -> p a x", x=128), mk4, ALU.mult, [('ps', 5), 'cst'], ['Gk'])
        for dh in range(4):
            MM(bank[4][0:64, 256 + dh * 64:256 + (dh + 1) * 64], AR[:, dh, 0:64], BT[:, dh, :], ['AR', 'BT'], [('ps', 4)])
        TT('dve', Nn[:], bank[4][0:64, 256:512].rearrange("p (a x) -> p a x", x=64),
           MkN.unsqueeze(1).broadcast_to([64, 4, 64]), ALU.mult, [('ps', 4), 'cst'], ['Nn'])
        TT('pool', Qk[0][:], Gb[:, :, 0:64], I64.unsqueeze(1).broadcast_to([64, 4, 64]), ALU.add, ['Gb', 'cst'], [('Q', 0)])
        pk_prev = (lambda dh: Gb[:, dh, 0:64], lambda dh: Nn[:, dh, :], ['Gb', 'Nn'])
        qi = 0
        for lv in range(1, 6):
            if lv == 3:
                marks.append(len(P.ops))
            pb = lv % 2
            Pn = Pk[pb]
            pkey = ('Pk', pb)
            bkp, bkq = (5, 4) if lv <= 2 else (6, 6)
            for dh in range(4):
                if lv < 5:
                    MM(bank[bkp][0:64, dh * 64:(dh + 1) * 64], pk_prev[1](dh), pk_prev[0](dh), pk_prev[2], [('ps', bkp)])
                MM(bank[bkp][0:64, 256 + dh * 64:256 + (dh + 1) * 64], pk_prev[0](dh), pk_prev[1](dh), pk_prev[2], [('ps', bkp)])
            if lv % 2:
                Pop('act', lambda e, Pn=Pn, bkp=bkp: e.copy(out=Pn[:].rearrange("p a b t -> p (a b t)"), in_=bank[bkp][0:64, :]),
                    r=[('ps', bkp)], w=[pkey])
            else:
                Pop('dve', lambda e, Pn=Pn, bkp=bkp: e.tensor_copy(out=Pn[:].rearrange("p a b t -> p (a b t)"), in_=bank[bkp][0:64, :]),
                    r=[('ps', bkp)], w=[pkey])
            pk_prev = (lambda dh, Pn=Pn: Pn[:, 0, dh, :], lambda dh, Pn=Pn: Pn[:, 1, dh, :], [pkey])
            for dh in range(4):
                MM(bank[bkq][0:64, dh * 64:(dh + 1) * 64], Pn[:, 1, dh, :], Qk[qi][:, dh, :], [pkey, ('Q', qi)], [('ps', bkq)])
            TT('dve', Qk[1 - qi][:], bank[bkq][0:64, 0:256].rearrange("p (a t) -> p a t", t=64), Qk[qi][:], ALU.add,
               [('ps', bkq), ('Q', qi)], [('Q', 1 - qi)])
            qi = 1 - qi
        TTm = Qk[qi]
        tkey = ('Q', qi)
        marks.append(len(P.ops))
        for dh in range(4):
            MM(bank[7][0:64, dh * 64:(dh + 1) * 64], AR[:, dh, 0:64], ST[:, dh, :], ['AR', 'ST'], [('ps', 7)], start=True, stop=False)
            MM(bank[7][0:64, dh * 64:(dh + 1) * 64], Gk[:, dh, 0:64], Vt[:, dh, :], ['Gk', 'Vt'], [('ps', 7)], start=False, stop=True)
        Pop('act', lambda e: e.copy(out=Xsb[:].rearrange("p a t -> p (a t)"), in_=bank[7][0:64, 0:256]), r=[('ps', 7)], w=['Xsb'])
        for dh in range(4):
            MM(bank[7][0:64, 256 + dh * 64:256 + (dh + 1) * 64], TTm[:, dh, :], Xsb[:, dh, :], [tkey, 'Xsb'], [('ps', 7)])
        Pop('act', lambda e: e.copy(out=Usb[:].rearrange("p a t -> p (a t)"), in_=bank[7][0:64, 256:512]), r=[('ps', 7)], w=['Usb'])
        for dh in range(4):
            o = bank[7][0:64, dh * 64:(dh + 1) * 64]
            MM(o, ST[:, dh, :], AR[:, dh, 64:128], ['ST', 'AR'], [('ps', 7)], start=True, stop=False)
            MM(o, Usb[:, dh, :], Gb[:, dh, 64:128], ['Usb', 'Gb'], [('ps', 7)], start=False, stop=False)
            MM(o, Vt[:, dh, :], Gk[:, dh, 64:128], ['Vt', 'Gk'], [('ps', 7)], start=False, stop=True)
        yv = bank[7][0:64, 0:256].rearrange("p (a t) -> p a t", t=64)
        Pop('act', lambda e, Yb=Yb, yv=yv: e.copy(out=Yb[:, 0:2, :], in_=yv[:, 0:2, :]), r=[('ps', 7)], w=['Ysb'])
        Pop('act', lambda e, Yb=Yb, yv=yv: e.copy(out=Yb[:, 2:4, ::-1], in_=yv[:, 2:4, :]), r=[('ps', 7)], w=['Ysb'])
        Pdma('pool', Yd[0, :, cf * 64:cf * 64 + 64].rearrange("(h c) t -> c h t", h=2), Yb[:, 0:2, :], r=['Ysb'], w=['Ydall'])
        Pdma('pool', Yd[1, :, cb * 64:cb * 64 + 64].rearrange("(h c) t -> c h t", h=2), Yb[:, 2:4, :], r=['Ysb'], w=['Ydall'])
        TT('pool', Stmp[:], ST[:], gC[:].unsqueeze(2).broadcast_to([64, 4, 64]), ALU.mult, ['ST', 'gC'], ['Stmp'])
        for dh in range(4):
            o = bank[7][0:64, 256 + dh * 64:256 + (dh + 1) * 64]
            MM(o, TM[:, 0, dh, :], Usb[:, dh, :], ['TM', 'Usb'], [('ps', 7)], start=True, stop=False)
            MM(o, TM[:, 1, dh, :], Vt[:, dh, :], ['TM', 'Vt'], [('ps', 7)], start=False, stop=True)
        TT('dve', ST[:], bank[7][0:64, 256:512].rearrange("p (a t) -> p a t", t=64), Stmp[:], ALU.add, [('ps', 7), 'Stmp'], ['ST'])
        ops = P.ops
        P.ops = saved
        cur['b'] = None
        bounds = [0] + marks + [len(ops)]
        return [ops[bounds[i]:bounds[i + 1]] for i in range(5)]

    def interleave(lists):
        items = []
        for li, lst in enumerate(lists):
            n_ = len(lst)
            for k_, o in enumerate(lst):
                items.append(((k_ + 0.5) / n_, li, k_, o))
        items.sort(key=lambda t: (t[0], t[1], t[2]))
        return [t[3] for t in items]
    gen = [gen_step(si) for si in range(NS)]
    for tau in range(-KSEG, NS):
        lists = []
        if 0 <= tau < NS:
            lists.append(gen[tau][KSEG])
        for j in range(1, KSEG + 1):
            s_ = tau + j
            if 0 <= s_ < NS:
                lists.append(gen[s_][KSEG - j])
        P.ops.extend(interleave(lists))
    P.fence()
    A.release(m0)
    prm2 = A.alloc("prm2", [64, 2, 5])
    on64 = A.alloc("on64", [64, 64])
    eps2 = A.alloc("eps2", [64, 1])
    P.dma('sp', prm2[:], prm, w=['prm2'])
    P.op('pool', lambda e: e.memset(on64[:], 1.0 / 64), w=['on64'])
    P.op('pool', lambda e: e.memset(eps2[:], GN_EPS), w=['eps2'])
    yb = [A.alloc("yb%d" % i, [64, 2, 2, 512]) for i in range(2)]
    bd = [A.alloc("bd%d" % i, [64, 2, 2, 512]) for i in range(2)]
    gg = [A.alloc("gg%d" % i, [64, 2, 512]) for i in range(2)]
    y = A.alloc("y", [64, 2, 512])
    yc = A.alloc("yc", [64, 2, 512])
    y2 = A.alloc("y2", [64, 2, 512])
    rs = A.alloc("rs", [64, 2, 512])
    jobs = []
    if ctx_out:
        jobs.append((0, CTXN, 0))
    o0 = CTXN if ctx_out else 0
    for b0 in range(0, L, 512):
        jobs.append((CTXN + b0, 512, o0 + b0))
    for ji, (t0, n, orow) in enumerate(jobs):
        b = ji % 2
        P.dma('sp', yb[b][:, :, :, :n], Yd[:, :, t0:t0 + n].rearrange("d (h c) t -> c d h t", h=2), r=['Ydall'], w=[('yb', b)])
        P.dma('sp', bd[b][:, :, :, :n], Bd[:, :, t0:t0 + n].rearrange("d (h c) t -> c d h t", h=2), r=['Bdall'], w=[('bd', b)])
        P.dma('sp', gg[b][:, :, :n], uT[5, :, t0:t0 + n].rearrange("(h c) t -> c h t", h=2), r=['uTall'], w=[('gg', b)])
        TT('dve', y[:, :, :n], yb[b][:, 0, :, :n], yb[b][:, 1, :, :n], ALU.add, [('yb', b)], ['y'])
        for h in range(2):
            MM(bank[h][0:64, :n], on64[:], y[:, h, :n], ['on64', 'y'], [('ps', h)])
            TT('dve', yc[:, h, :n], y[:, h, :n], bank[h][0:64, :n], ALU.subtract, ['y', ('ps', h)], ['yc'])
        TT('pool', y2[:, :, :n], yc[:, :, :n], yc[:, :, :n], ALU.mult, ['yc'], ['y2'])
        for h in range(2):
            MM(bank[2 + h][0:64, :n], on64[:], y2[:, h, :n], ['on64', 'y2'], [('ps', 2 + h)])
            ACT(rs[:, h, :n], bank[2 + h][0:64, :n], AF.Sqrt, [('ps', 2 + h), 'eps2'], ['rs'], bias=eps2[:, 0:1])
        P.op('dve', lambda e, n=n: e.reciprocal(out=rs[:, :, :n], in_=rs[:, :, :n]), r=['rs'], w=['rs'])
        TT('dve', yc[:, :, :n], yc[:, :, :n], rs[:, :, :n], ALU.mult, ['yc', 'rs'], ['yc'])
        for h in range(2):
            TS('pool', yc[:, h, :n], yc[:, h, :n], prm2[:, h, 3:4], prm2[:, h, 4:5], ALU.mult, ALU.add, ['yc', 'prm2'], ['yc'])
        TT('dve', yc[:, :, :n], yc[:, :, :n], bd[b][:, 0, :, :n], ALU.add, ['yc', ('bd', b)], ['yc'])
        TT('dve', yc[:, :, :n], yc[:, :, :n], bd[b][:, 1, :, :n], ALU.add, ['yc', ('bd', b)], ['yc'])
        TT('pool', y2[:, :, :n], yc[:, :, :n], gg[b][:, :, :n], ALU.mult, ['yc', ('gg', b), 'y2'], ['y2'])
        P.dma('pool', out_ap(orow, n).rearrange("(h c) t -> c h t", h=2), y2[:, :, :n], r=['y2'], w=['brb'], final=FINAL_OUT)
    P.fence()


NORM_EPS = 1e-6
CTXN = 256


def phase_A(nc, P, A, bank, pfx, L, ctx_out, lam_init, x_loader, brb, do_attn=True, do_pool=True, do_rwkv=True):
    T = CTXN + L
    NQ = T if ctx_out else L
    di = lambda name, shape: nc.dram_tensor(pfx + name, list(shape), F32, kind="ExternalInput").ap()
    if x_loader is None:
        xT = di("xT", [128, 8, T])

        def x_loader(dst, t0, sz, key):
            P.dma('sp', dst[:, :, :sz], xT[:, :, t0:t0 + sz], w=[key])
    identA = di("identA", [128, 128])
    cT = di("cT", [128, 8, 2])
    wada = di("wada", [128, 8, 2048])
    bada = di("bada", [128, 16])
    g1 = di("g1", [128, 8])
    win = di("win", [128, 8, 1280])
    qkg = di("qkg", [128, 2])
    ropeR = di("ropeR", [128, 2, L // 64])
    ropeC = di("ropeC", [128, 2, 64])
    perm = di("perm", [128, 128])
    lamqk = di("lamqk", [128, 256])
    subg = di("subg", [128, 128])
    wpool = di("wpool", [128, 128])
    pscale = di("pscale", [128, 1])
    selw = di("selw", [128, 4])
    efix = di("efix", [128, 16])
    pT = nc.dram_tensor(pfx + "pT_scr", [7, 128, T], F32, kind="Internal").ap()
    base_mark = A.mark()
    if True:
        ones = A.alloc("ones", [128, 128])
        blk = A.alloc("blk", [128, 128])
        eps_sb = A.alloc("eps", [128, 1])
        mod_sb = A.alloc("mod", [128, 16, 2])
        A_sb = A.alloc("A", [128, 8, 2])
        NKT = T // 128
        QT = A.alloc("QT", [128, T], BF16)
        KT = A.alloc("KT", [128, T], BF16)
        V = A.alloc("V", [128, NKT, 130], BF16)
        lam_sb = A.alloc("lam", [128, 1])
        subg_sb = A.alloc("subg", [128, 128])
        ident_sb = A.alloc("identA", [128, 128])
        P.dma('sp', ident_sb[:], identA, w=['identA'])
        P.op('pool', lambda e: e.memset(ones[:], 1.0), w=['ones'])
        P.op('pool', lambda e: e.memset(blk[:], 0.0), w=['blk'])
        P.op('pool', lambda e: e.memset(blk[0:64, 0:64], 1.0), w=['blk'])
        P.op('pool', lambda e: e.memset(blk[64:128, 64:128], 1.0), w=['blk'])
        P.op('pool', lambda e: e.memset(eps_sb[:], NORM_EPS), w=['eps'])
        P.op('pool', lambda e: e.memset(V[:, :, 128:130], 1.0), w=['Vones'])
        m_a1 = A.mark()
        c_sb = A.alloc("c", [128, 8, 2])
        s_sb = A.alloc("s", [128, 8, 2])
        bada_sb = A.alloc("bada", [128, 16])
        g1_sb = A.alloc("g1", [128, 8])
        qkg_sb = A.alloc("qkg", [128, 2])
        perm_sb = A.alloc("perm", [128, 128])
        ropeR_sb = A.alloc("ropeR", [128, 2, L // 64])
        ropeC_sb = A.alloc("ropeC", [128, 2, 64])
        lamqk_sb = A.alloc("lamqk", [128, 256])
        lamt = A.alloc("lamt", [128, 4])
        wbf = A.alloc("wbf", [128, 8, 1280], BF16)
        xt = [A.alloc("xt%d" % i, [128, 8, 512]) for i in range(2)]
        sq = A.alloc("sq", [128, 8, 512])
        rstd = A.alloc("rstd", [128, 512])
        xn = A.alloc("xn", [128, 8, 512], BF16)
        ob = [A.alloc("ob%d" % i, [128, 512]) for i in range(2)]
        qk32 = A.alloc("qk32", [128, 512])
        qksq = A.alloc("qksq", [128, 512])
        qkr = A.alloc("qkr", [128, 512])
        qkn = A.alloc("qkn", [128, 512])
        cs_t = A.alloc("cs_t", [128, 2, 512])
        rtmp = A.alloc("rtmp", [128, 2, 512])

        for nm, dst, src in [('c_sb', c_sb, cT), ('bada', bada_sb, bada), ('g1', g1_sb, g1), ('qkg', qkg_sb, qkg),
                             ('perm', perm_sb, perm), ('ropeR', ropeR_sb, ropeR), ('ropeC', ropeC_sb, ropeC),
                             ('lamqk', lamqk_sb, lamqk), ('subg', subg_sb, subg)]:
            P.dma('sp', dst[:], src, w=[nm])
        lq = lamqk_sb[:].rearrange("p (a b) -> p a b", b=64)
        P.op('dve', lambda e: e.tensor_tensor(out=sq[:, 0, 0:64], in0=lq[:, 0, :], in1=lq[:, 1, :], op=ALU.mult),
             r=['lamqk'], w=['sq'])
        P.op('dve', lambda e: e.tensor_tensor(out=sq[:, 0, 64:128], in0=lq[:, 2, :], in1=lq[:, 3, :], op=ALU.mult),
             r=['lamqk'], w=['sq'])
        P.op('dve', lambda e: e.tensor_reduce(out=lamt[:, 0:2], in_=sq[:, 0, 0:128].rearrange("p (a b) -> p a b", b=64),
                                              axis=AX.X, op=ALU.add), r=['sq'], w=['lamt'])
        P.op('act', lambda e: e.activation(out=lamt[:, 2:4], in_=lamt[:, 0:2], func=AF.Exp), r=['lamt'], w=['lamt2'])
        P.op('dve', lambda e: e.tensor_tensor(out=lam_sb[:], in0=lamt[:, 2:3], in1=lamt[:, 3:4], op=ALU.subtract),
             r=['lamt2'], w=['lam'])
        P.op('dve', lambda e: e.tensor_scalar(out=lam_sb[:], in0=lam_sb[:], scalar1=lam_init, scalar2=-1.0,
                                              op0=ALU.add, op1=ALU.mult), r=['lam'], w=['lam'])
        P.op('act', lambda e: e.activation(out=s_sb[:], in_=c_sb[:], func=AF.Silu), r=['c_sb'], w=['s_sb'])
        ps_mod = bank[0][:, 0:32]
        for piece in range(4):
            b = piece % 2
            P.dma('sp', xt[b][:], wada[:, :, piece * 512:(piece + 1) * 512], w=[('xt', b)])
            for occ in range(4):
                oc = piece * 4 + occ
                for k in range(8):
                    P.op('pe', lambda e, b=b, occ=occ, oc=oc, k=k: e.matmul(
                        ps_mod[:, oc * 2:oc * 2 + 2], lhsT=xt[b][:, k, occ * 128:(occ + 1) * 128],
                        rhs=s_sb[:, k, :], start=(k == 0), stop=(k == 7)),
                        r=[('xt', b), 's_sb'], w=[('ps', 0)])
        P.op('dve', lambda e: e.tensor_tensor(
            out=mod_sb[:], in0=ps_mod.rearrange("p (a b) -> p a b", b=2),
            in1=bada_sb[:].unsqueeze(2).broadcast_to([128, 16, 2]), op=ALU.add),
            r=[('ps', 0), 'bada'], w=['mod'])
        P.op('dve', lambda e: e.tensor_scalar(out=A_sb[:], in0=mod_sb[:, 8:16, :], scalar1=1.0, scalar2=None,
                                              op0=ALU.add), r=['mod'], w=['A'])
        P.op('dve', lambda e: e.tensor_tensor(out=A_sb[:], in0=A_sb[:],
                                              in1=g1_sb[:].unsqueeze(2).broadcast_to([128, 8, 2]), op=ALU.mult),
             r=['A', 'g1'], w=['A'])
        for piece, (c0, csz) in enumerate([(0, 512), (512, 512), (1024, 256)]):
            b = piece % 2
            P.dma('sp', xt[b][:, :, :csz], win[:, :, c0:c0 + csz], w=[('xt', b)])
            P.op('pool', lambda e, b=b, c0=c0, csz=csz: e.tensor_copy(out=wbf[:, :, c0:c0 + csz], in_=xt[b][:, :, :csz]),
                 r=[('xt', b)], w=['wbf'])
        tiles = [(0, CTXN, 1)] + [(CTXN + i * 512, 512, 0) for i in range(L // 512)]
        SCR = {0: 0, 4: 1, 5: 2, 6: 3, 7: 4, 8: 5, 9: 6}
        oi = 0
        for ti, (t0, sz, j) in enumerate(tiles):
            b = ti % 2
            x_loader(xt[b], t0, sz, ('xt', b))
            P.op('act', lambda e, b=b, sz=sz: e.activation(out=sq[:, :, :sz], in_=xt[b][:, :, :sz], func=AF.Square),
                 r=[('xt', b)], w=['sq'])
            for k in range(8):
                P.op('pe', lambda e, k=k, sz=sz: e.matmul(bank[0][:, :sz], lhsT=ones[:], rhs=sq[:, k, :sz],
                                                          start=(k == 0), stop=(k == 7)),
                     r=['ones', 'sq'], w=[('ps', 0)])
            P.op('act', lambda e, sz=sz: e.activation(out=rstd[:, :sz], in_=bank[0][:, :sz], func=AF.Sqrt,
                                                      scale=1.0 / 1024, bias=eps_sb[:, 0:1]),
                 r=[('ps', 0), 'eps'], w=['rstd'])
            P.op('dve', lambda e, sz=sz: e.reciprocal(out=rstd[:, :sz], in_=rstd[:, :sz]), r=['rstd'], w=['rstd'])
            P.op('dve', lambda e, b=b, sz=sz: e.tensor_tensor(
                out=sq[:, :, :sz], in0=xt[b][:, :, :sz],
                in1=rstd[:, :sz].unsqueeze(1).broadcast_to([128, 8, sz]), op=ALU.mult),
                r=[('xt', b), 'rstd', 'sq'], w=['sq'])
            for k in range(8):
                if k % 2:
                    P.op('dve', lambda e, k=k, sz=sz, j=j: e.tensor_scalar(
                        out=xn[:, k, :sz], in0=sq[:, k, :sz], scalar1=A_sb[:, k, j:j + 1],
                        scalar2=mod_sb[:, k, j:j + 1], op0=ALU.mult, op1=ALU.add),
                        r=['sq', 'A', 'mod'], w=['xn'])
                else:
                    P.op('act', lambda e, k=k, sz=sz, j=j: e.activation(
                        out=xn[:, k, :sz], in_=sq[:, k, :sz], func=AF.Identity, scale=A_sb[:, k, j:j + 1],
                        bias=mod_sb[:, k, j:j + 1]), r=['sq', 'A', 'mod'], w=['xn'])
            if j == 0:
                r0 = (t0 - CTXN) // 64
                for cs in range(2):
                    P.op('pool', lambda e, cs=cs, r0=r0: e.tensor_tensor(
                        out=cs_t[:, cs, :].rearrange("p (r c) -> p r c", c=64),
                        in0=ropeR_sb[:, cs, r0:r0 + 8].unsqueeze(2).broadcast_to([128, 8, 64]),
                        in1=ropeC_sb[:, cs, :].unsqueeze(1).broadcast_to([128, 8, 64]), op=ALU.mult),
                        r=['ropeR', 'ropeC'], w=['cs_t'])
            for c in range(10):
                if c == 3:
                    for s in range(sz // 128):
                        kt = t0 // 128 + s
                        for k in range(8):
                            P.op('pe', lambda e, k=k, s=s: e.matmul(
                                bank[3][:, 0:128], lhsT=xn[:, k, s * 128:(s + 1) * 128], rhs=wbf[:, k, 384:512],
                                start=(k == 0), stop=(k == 7)), r=['xn', 'wbf'], w=[('ps', 3)])
                        P.op('act', lambda e, kt=kt: e.copy(out=V[:, kt, 0:128], in_=bank[3][:, 0:128]),
                             r=[('ps', 3)], w=['V'])
                    continue
                pb = 1 + (oi % 2)
                oi += 1
                for k in range(8):
                    P.op('pe', lambda e, c=c, k=k, sz=sz, pb=pb: e.matmul(
                        bank[pb][:, :sz], lhsT=wbf[:, k, c * 128:(c + 1) * 128], rhs=xn[:, k, :sz],
                        start=(k == 0), stop=(k == 7)), r=['wbf', 'xn'], w=[('ps', pb)])
                if c in SCR:
                    o = ob[oi % 2]
                    ok = ('ob', oi % 2)
                    if oi % 2:
                        P.op('act', lambda e, o=o, pb=pb, sz=sz: e.copy(out=o[:, :sz], in_=bank[pb][:, :sz]),
                             r=[('ps', pb)], w=[ok])
                    else:
                        P.op('dve', lambda e, o=o, pb=pb, sz=sz: e.tensor_copy(out=o[:, :sz], in_=bank[pb][:, :sz]),
                             r=[('ps', pb)], w=[ok])
                    P.dma('pool', pT[SCR[c], :, t0:t0 + sz], o[:, :sz], r=[ok], w=[('pT', SCR[c], ti)])
                else:
                    dst = QT if c == 1 else KT
                    gi = c - 1
                    P.op('act', lambda e, pb=pb, sz=sz: e.copy(out=qk32[:, :sz], in_=bank[pb][:, :sz]),
                         r=[('ps', pb)], w=['qk32'])
                    P.op('act', lambda e, sz=sz: e.activation(out=qksq[:, :sz], in_=qk32[:, :sz], func=AF.Square),
                         r=['qk32'], w=['qksq'])
                    P.op('pe', lambda e, sz=sz: e.matmul(bank[4][:, :sz], lhsT=blk[:], rhs=qksq[:, :sz],
                                                         start=True, stop=True), r=['blk', 'qksq'], w=[('ps', 4)])
                    P.op('act', lambda e, sz=sz: e.activation(out=qkr[:, :sz], in_=bank[4][:, :sz], func=AF.Sqrt,
                                                              scale=1.0 / 64, bias=eps_sb[:, 0:1]),
                         r=[('ps', 4), 'eps'], w=['qkr'])
                    P.op('dve', lambda e, sz=sz: e.reciprocal(out=qkr[:, :sz], in_=qkr[:, :sz]), r=['qkr'], w=['qkr'])
                    if j == 1:
                        P.op('dve', lambda e, sz=sz, gi=gi, dst=dst, t0=t0: e.scalar_tensor_tensor(
                            out=dst[:, t0:t0 + sz], in0=qk32[:, :sz], scalar=qkg_sb[:, gi:gi + 1], in1=qkr[:, :sz],
                            op0=ALU.mult, op1=ALU.mult), r=['qk32', 'qkg', 'qkr'], w=['QK'])
                    else:
                        P.op('dve', lambda e, sz=sz, gi=gi: e.scalar_tensor_tensor(
                            out=qkn[:, :sz], in0=qk32[:, :sz], scalar=qkg_sb[:, gi:gi + 1], in1=qkr[:, :sz],
                            op0=ALU.mult, op1=ALU.mult), r=['qk32', 'qkg', 'qkr'], w=['qkn'])
                        P.op('pe', lambda e, sz=sz: e.matmul(bank[5][:, :sz], lhsT=perm_sb[:], rhs=qkn[:, :sz],
                                                             start=True, stop=True), r=['perm', 'qkn'], w=[('ps', 5)])
                        P.op('pool', lambda e, sz=sz: e.tensor_tensor(out=rtmp[:, 0, :sz], in0=qkn[:, :sz],
                                                                      in1=cs_t[:, 0, :sz], op=ALU.mult),
                             r=['qkn', 'cs_t'], w=['rtmp0'])
                        P.op('dve', lambda e, sz=sz: e.tensor_tensor(out=rtmp[:, 1, :sz], in0=bank[5][:, :sz],
                                                                     in1=cs_t[:, 1, :sz], op=ALU.mult),
                             r=[('ps', 5), 'cs_t'], w=['rtmp1'])
                        P.op('dve', lambda e, sz=sz, dst=dst, t0=t0: e.tensor_tensor(
                            out=dst[:, t0:t0 + sz], in0=rtmp[:, 0, :sz], in1=rtmp[:, 1, :sz], op=ALU.add),
                            r=['rtmp0', 'rtmp1'], w=['QK'])
        P.fence()
        A.release(m_a1)
        m_ph = A.mark()
        if do_pool:
            wpool_f = A.alloc("wpool_f", [128, 128])
            wpool_b = A.alloc("wpool_b", [128, 128], BF16)
            pscale_sb = A.alloc("pscale", [128, 1])
            selw_sb = A.alloc("selw", [128, 4])
            efix_sb = A.alloc("efix", [128, 16])
            NB = 2048
            U = A.alloc("U", [128, NB + 32])
            W = [A.alloc("W%d" % i, [128, NB + 32]) for i in range(2)]
            comb = A.alloc("comb", [128, NB])
            diffb = A.alloc("diffb", [128, NB], BF16)
            pob = [A.alloc("pob%d" % i, [128, 512]) for i in range(2)]
            P.dma('sp', wpool_f[:], wpool, w=['wpool_f'])
            P.dma('sp', pscale_sb[:], pscale, w=['pscale'])
            P.dma('sp', selw_sb[:], selw, w=['selw'])
            P.dma('sp', efix_sb[:], efix, w=['efix'])
            P.op('dve', lambda e: e.tensor_copy(out=wpool_b[:], in_=wpool_f[:]), r=['wpool_f'], w=['wpool_b'])
            seqs = [(CTXN, L, (0 if not ctx_out else CTXN))]
            if ctx_out:
                seqs.append((0, CTXN, 0))
            pi = 0
            for (s0, slen, o0) in seqs:
                for b0 in range(0, slen, NB):
                    n = min(NB, slen - b0)
                    lo = max(0, b0 - 16)
                    hi = min(slen, b0 + n + 16)
                    P.op('dve', lambda e: e.memset(U[:], 0.0), w=['U'])
                    P.dma('sp', U[:, 16 + lo - b0:16 + hi - b0], pT[0, :, s0 + lo:s0 + hi], r=[('pT', 0, t) for t in range(len(tiles))], w=['U'])
                    NP = n + 32
                    src = U
                    for lv, sh in enumerate([1, 2, 4, 8]):
                        dstw = W[lv % 2]
                        P.op('dve', lambda e, src=src, dstw=dstw, sh=sh, NP=NP: e.tensor_tensor(
                            out=dstw[:, sh:NP], in0=src[:, sh:NP], in1=src[:, 0:NP - sh], op=ALU.add),
                            r=['U', ('W', 0), ('W', 1)], w=[('W', lv % 2)])
                        w_ = 2 * sh
                        off = 16 + w_ // 2 - 1
                        if lv == 0:
                            P.op('dve', lambda e, dstw=dstw, off=off, n=n, lv=lv: e.tensor_scalar(
                                out=comb[:, :n], in0=dstw[:, off:off + n], scalar1=selw_sb[:, lv:lv + 1], scalar2=None,
                                op0=ALU.mult), r=[('W', lv % 2), 'selw'], w=['comb'])
                        else:
                            P.op('dve', lambda e, dstw=dstw, off=off, n=n, lv=lv: e.scalar_tensor_tensor(
                                out=comb[:, :n], in0=dstw[:, off:off + n], scalar=selw_sb[:, lv:lv + 1], in1=comb[:, :n],
                                op0=ALU.mult, op1=ALU.add), r=[('W', lv % 2), 'selw', 'comb'], w=['comb'])
                        src = dstw
                    if b0 == 0:
                        P.op('pool', lambda e: e.tensor_tensor(out=comb[:, 0:8], in0=comb[:, 0:8], in1=efix_sb[:, 0:8],
                                                               op=ALU.mult), r=['comb', 'efix'], w=['comb'])
                    if b0 + n == slen:
                        P.op('pool', lambda e, n=n: e.tensor_tensor(out=comb[:, n - 8:n], in0=comb[:, n - 8:n],
                                                                    in1=efix_sb[:, 8:16], op=ALU.mult),
                             r=['comb', 'efix'], w=['comb'])
                    P.op('dve', lambda e, n=n: e.tensor_tensor(out=diffb[:, :n], in0=comb[:, :n], in1=U[:, 16:16 + n],
                                                               op=ALU.subtract), r=['comb', 'U'], w=['diffb'])
                    for c0 in range(0, n, 512):
                        cs = min(512, n - c0)
                        pb = 6 + pi % 2
                        o = pob[pi % 2]
                        ok = ('pob', pi % 2)
                        pi += 1
                        P.op('pe', lambda e, c0=c0, cs=cs, pb=pb: e.matmul(bank[pb][:, :cs], lhsT=wpool_b[:],
                                                                            rhs=diffb[:, c0:c0 + cs], start=True, stop=True),
                             r=['wpool_b', 'diffb'], w=[('ps', pb)])
                        P.op('act', lambda e, o=o, pb=pb, cs=cs: e.activation(out=o[:, :cs], in_=bank[pb][:, :cs],
                                                                               func=AF.Copy, scale=pscale_sb[:, 0:1]),
                             r=[('ps', pb), 'pscale'], w=[ok])
                        P.dma('pool', brb.dst(0, o0 + b0 + c0, cs), o[:, :cs], r=[ok], w=['brb'])
            P.fence()
            A.release(m_ph)
        if do_attn:
            PT = [[A.alloc("PT%d_%d" % (m, i), [128, 512], BF16) for i in range(2)] for m in range(2)]
            osb = A.alloc("osb", [128, 2, 4, 130])
            Qz = [A.alloc("Qz%d" % i, [128, 2, 512], BF16) for i in range(2)]
            for i in range(2):
                P.op('dve', lambda e, i=i: e.memset(Qz[i][:], 0.0), w=[('Qz', i)])
            rec = A.alloc("rec", [128, 2, 4])
            o1 = A.alloc("o1", [128, 4, 128])
            o2 = A.alloc("o2", [128, 4, 128])
            ssq = A.alloc("ssq", [128, 4])
            oT = A.alloc("oT", [128, 512])
            def oacc(m, qs):
                i = m * 4 + qs
                return bank[4 + i // 3][:, (i % 3) * 130:(i % 3) * 130 + 130], ('ps', 4 + i // 3)
            qjobs = []
            if ctx_out:
                qjobs.append((0, CTXN, (0, 2), 0))
            for i in range(L // 512):
                qjobs.append((CTXN + i * 512, 512, (0, NKT), (CTXN if ctx_out else 0) + i * 512))
            si = 0
            for qji, (q0, nq, (k0, k1), orow) in enumerate(qjobs):
                nqs = nq // 128
                started = set()
                qz = Qz[qji % 2]
                qzk = ('Qz', qji % 2)
                P.op('dve', lambda e, qz=qz, q0=q0, nq=nq: e.tensor_copy(out=qz[0:64, 0, :nq], in_=QT[0:64, q0:q0 + nq]),
                     r=['QK', qzk], w=[qzk])
                P.op('pool', lambda e, qz=qz, q0=q0, nq=nq: e.tensor_copy(out=qz[64:128, 1, :nq], in_=QT[64:128, q0:q0 + nq]),
                     r=['QK', qzk], w=[qzk])
                def emit_S(kt):
                    for m in range(2):
                        sb_ = kt % 2
                        pb = m * 2 + sb_
                        P.op('pe', lambda e, m=m, kt=kt, nq=nq, pb=pb, qz=qz: e.matmul(
                            bank[pb][:, :nq], lhsT=KT[:, kt * 128:(kt + 1) * 128],
                            rhs=qz[:, m, :nq], start=True, stop=True),
                            r=['QK', qzk], w=[('ps', pb)])
                        P.op('act', lambda e, m=m, sb_=sb_, pb=pb, nq=nq: e.activation(
                            out=PT[m][sb_][:, :nq], in_=bank[pb][:, :nq], func=AF.Exp, scale=0.125),
                            r=[('ps', pb)], w=[('PT', m, sb_)])

                def emit_PV(kt):
                    for m in range(2):
                        sb_ = kt % 2
                        for qs in range(nqs):
                            oap, okey = oacc(m, qs)
                            first_in_bank = (kt == k0) and (okey not in started)
                            started.add(okey)
                            P.op('pe', lambda e, m=m, sb_=sb_, qs=qs, kt=kt, oap=oap, fib=first_in_bank, k1=k1: e.matmul(
                                oap, lhsT=PT[m][sb_][:, qs * 128:(qs + 1) * 128], rhs=V[:, kt, :],
                                start=fib, stop=(kt == k1 - 1)),
                                r=[('PT', m, sb_), 'V', 'Vones'], w=[okey])
                emit_S(k0)
                for kt in range(k0, k1):
                    if kt + 1 < k1:
                        emit_S(kt + 1)
                    emit_PV(kt)
                for m in range(2):
                    for qs in range(nqs):
                        oap, okey = oacc(m, qs)
                        P.op('dve' if (m + qs) % 2 else 'act',
                             (lambda e, m=m, qs=qs, oap=oap: e.tensor_copy(out=osb[:, m, qs, :], in_=oap)) if (m + qs) % 2
                             else (lambda e, m=m, qs=qs, oap=oap: e.copy(out=osb[:, m, qs, :], in_=oap)),
                             r=[okey], w=['osb'])
                P.op('dve', lambda e, nqs=nqs: e.reciprocal(out=rec[:, :, :nqs], in_=osb[:, :, :nqs, 128]),
                     r=['osb'], w=['rec'])
                P.op('dve', lambda e, nqs=nqs: e.tensor_scalar(out=rec[:, 1, :nqs], in0=rec[:, 1, :nqs],
                                                               scalar1=lam_sb[:, 0:1], scalar2=None, op0=ALU.mult),
                     r=['rec', 'lam'], w=['rec'])
                P.op('dve', lambda e, nqs=nqs: e.tensor_tensor(
                    out=o1[:, :nqs, :], in0=osb[:, 0, :nqs, 0:128],
                    in1=rec[:, 0, :nqs].unsqueeze(2).broadcast_to([128, nqs, 128]), op=ALU.mult),
                    r=['osb', 'rec'], w=['o1'])
                P.op('pool', lambda e, nqs=nqs: e.tensor_tensor(
                    out=o2[:, :nqs, :], in0=osb[:, 1, :nqs, 0:128],
                    in1=rec[:, 1, :nqs].unsqueeze(2).broadcast_to([128, nqs, 128]), op=ALU.mult),
                    r=['osb', 'rec'], w=['o2'])
                P.op('dve', lambda e, nqs=nqs: e.tensor_tensor(out=o1[:, :nqs, :], in0=o1[:, :nqs, :], in1=o2[:, :nqs, :],
                                                               op=ALU.add), r=['o1', 'o2'], w=['o1'])
                P.op('pool', lambda e, nqs=nqs: e.tensor_tensor(out=o2[:, :nqs, :], in0=o1[:, :nqs, :], in1=o1[:, :nqs, :],
                                                                op=ALU.mult), r=['o1', 'o2'], w=['o2'])
                P.op('dve', lambda e, nqs=nqs: e.tensor_reduce(out=ssq[:, :nqs], in_=o2[:, :nqs, :], axis=AX.X, op=ALU.add),
                     r=['o2'], w=['ssq'])
                P.op('act', lambda e, nqs=nqs: e.activation(out=ssq[:, :nqs], in_=ssq[:, :nqs], func=AF.Sqrt,
                                                            scale=1.0 / 128, bias=eps_sb[:, 0:1]),
                     r=['ssq', 'eps'], w=['ssq'])
                P.op('dve', lambda e, nqs=nqs: e.reciprocal(out=ssq[:, :nqs], in_=ssq[:, :nqs]), r=['ssq'], w=['ssq'])
                P.op('dve', lambda e, nqs=nqs: e.tensor_tensor(
                    out=o1[:, :nqs, :], in0=o1[:, :nqs, :],
                    in1=ssq[:, :nqs].unsqueeze(2).broadcast_to([128, nqs, 128]), op=ALU.mult),
                    r=['o1', 'ssq'], w=['o1'])
                P.op('pool', lambda e, nqs=nqs: e.tensor_tensor(
                    out=o2[:, :nqs, :], in0=o1[:, :nqs, :],
                    in1=subg_sb[:].unsqueeze(1).broadcast_to([128, nqs, 128]), op=ALU.mult),
                    r=['o1', 'subg', 'o2'], w=['o2'])
                for qs in range(nqs):
                    P.op('pe', lambda e, qs=qs: e.matmul(bank[7][:, qs * 128:(qs + 1) * 128], lhsT=o2[:, qs, :], rhs=ident_sb[:],
                                                         start=True, stop=True), r=['o2', 'identA'], w=[('ps', 7)])
                P.op('act', lambda e, nq=nq: e.copy(out=oT[:, :nq], in_=bank[7][:, :nq]), r=[('ps', 7)], w=['oT'])
                P.dma('sp', brb.dst(1, orow, nq), oT[:, :nq], r=['oT'], w=['brb'])
            P.fence()
        if do_rwkv:
            A.release(base_mark)
            rwkv_phase(nc, P, A, bank, pT, L, ctx_out, (lambda c0, n: brb.dst(2, c0, n)), pfx)
        A.release(base_mark)


NORM_EPS = 1e-6


def phase_B(nc, P, A, bank, pfx, NT_LAT, NT_CTX, x_load, gath, off_lat, x_store, NEXP=32):
    NT = NT_LAT + NT_CTX
    di = lambda name, shape: nc.dram_tensor(pfx + name, list(shape), F32, kind="ExternalInput").ap()
    selq = di("selq", [128, 4])
    cT = di("cT", [128, 8, 2])
    wada = di("wada", [128, 8, 6144])
    bada = di("bada", [128, 48])
    g12 = di("g12", [128, 2, 8])
    wgate = di("wgate", [128, 8, 3072])
    wbr = di("wbr", [128, 12, 1024])
    wout = di("wout", [128, 8, 1024])
    wrt = di("wrt", [128, 8, 36])
    wg = di("wg", [NEXP, 128, 8, 512])
    wu = di("wu", [NEXP, 128, 8, 512])
    wd = di("wd", [NEXP, 128, 4, 1024])
    selE = di("selE", [32, 32 * 128])
    ident = di("ident", [128, 128])
    x2s = nc.dram_tensor(pfx + "x2_scr", [128, 8, NT], F32, kind="Internal").ap()
    xn2s = nc.dram_tensor(pfx + "xn2_scr", [128, 8, NT], BF16, kind="Internal").ap()
    gTs = nc.dram_tensor(pfx + "gT_scr", [32, NT], F32, kind="Internal").ap()
    base_mark = A.mark()

    def TT(eng, out, a, b, op, r, w):
        P.op(eng, lambda e: e.tensor_tensor(out=out, in0=a, in1=b, op=op), r=r, w=w)

    def TS(eng, out, a, s1, s2, op0, op1, r, w):
        if op1 is None:
            P.op(eng, lambda e: e.tensor_scalar(out=out, in0=a, scalar1=s1, scalar2=None, op0=op0), r=r, w=w)
        else:
            P.op(eng, lambda e: e.tensor_scalar(out=out, in0=a, scalar1=s1, scalar2=s2, op0=op0, op1=op1), r=r, w=w)

    def STT(out, a, s, b, op0, op1, r, w):
        P.op('dve', lambda e: e.scalar_tensor_tensor(out=out, in0=a, scalar=s, in1=b, op0=op0, op1=op1), r=r, w=w)

    def ACT(out, a, func, r, w, scale=1.0, bias=None):
        if bias is None:
            P.op('act', lambda e: e.activation(out=out, in_=a, func=func, scale=scale), r=r, w=w)
        else:
            P.op('act', lambda e: e.activation(out=out, in_=a, func=func, scale=scale, bias=bias), r=r, w=w)

    def MM(out, lhsT, rhs, r, w, start=True, stop=True):
        P.op('pe', lambda e: e.matmul(out, lhsT=lhsT, rhs=rhs, start=start, stop=stop), r=r, w=w)

    def RED(out, a, op, r, w):
        P.op('dve', lambda e: e.tensor_reduce(out=out, in_=a, axis=AX.X, op=op), r=r, w=w)

    if True:
        ones = A.alloc("ones", [128, 128])
        selq_sb = A.alloc("selq", [128, 4])
        P.dma('sp', selq_sb[:], selq, w=['selq'])
        eps_sb = A.alloc("eps", [128, 1])
        mod_sb = A.alloc("mod", [128, 48, 2])
        A1 = A.alloc("A1", [128, 8, 2])
        A2 = A.alloc("A2", [128, 8, 2])
        g12_sb = A.alloc("g12", [128, 2, 8])
        ident_sb = A.alloc("ident", [128, 128])
        wrt_sb = A.alloc("wrt", [128, 8, 36])
        P.op('pool', lambda e: e.memset(ones[:], 1.0), w=['ones'])
        P.op('pool', lambda e: e.memset(eps_sb[:], NORM_EPS), w=['eps'])
        P.dma('sp', g12_sb[:], g12, w=['g12'])
        P.dma('sp', ident_sb[:], ident, w=['ident'])
        P.dma('sp', wrt_sb[:], wrt, w=['wrt'])
        mB1 = A.mark()
        c_sb = A.alloc("c", [128, 8, 2])
        s_sb = A.alloc("s", [128, 8, 2])
        bada_sb = A.alloc("bada", [128, 48])
        wgate_b = A.alloc("wgate_b", [128, 8, 3072], BF16)
        wbr_b = A.alloc("wbr_b", [128, 12, 1024], BF16)
        wout_b = A.alloc("wout_b", [128, 8, 1024], BF16)
        xt = [A.alloc("xt%d" % i, [128, 8, 512]) for i in range(2)]
        sq = A.alloc("sq", [128, 8, 512])
        rstd = A.alloc("rstd", [128, 512])
        xn1 = A.alloc("xn1", [128, 8, 512], BF16)
        brf = A.alloc("brf", [128, 4, 512])
        gTt = A.alloc("gTt", [32, 512])
        brb = A.alloc("brb", [128, 12, 512], BF16)
        sig = [A.alloc("sig%d" % i, [128, 512]) for i in range(2)]
        mrg = A.alloc("mrg", [128, 2, 512])
        mk_ = A.mark()
        mrgb = A.alloc("mrgb", [128, 8, 512], BF16)
        A.release(mk_)
        brsel = A.alloc("brsel", [128, 4, 512])
        x2 = A.alloc("x2", [128, 8, 512])
        rt = A.alloc("rt", [128, 80])
        g32 = A.alloc("g32", [128, 32])
        P.dma('sp', c_sb[:], cT, w=['c_sb'])
        P.dma('sp', bada_sb[:], bada, w=['bada'])
        ACT(s_sb[:], c_sb[:], AF.Silu, ['c_sb'], ['s_sb'])
        for piece in range(12):
            b = piece % 2
            P.dma('sp', xt[b][:], wada[:, :, piece * 512:(piece + 1) * 512], w=[('xt', b)])
            for occ in range(4):
                oc = piece * 4 + occ
                for k in range(8):
                    MM(bank[0][:, oc * 2:oc * 2 + 2], xt[b][:, k, occ * 128:(occ + 1) * 128], s_sb[:, k, :],
                       [('xt', b), 's_sb'], [('ps', 0)], start=(k == 0), stop=(k == 7))
        TT('dve', mod_sb[:], bank[0][:, 0:96].rearrange("p (a b) -> p a b", b=2),
           bada_sb[:].unsqueeze(2).broadcast_to([128, 48, 2]), ALU.add, [('ps', 0), 'bada'], ['mod'])
        for (Ax, m_scale, gi) in ((A1, 1, 0), (A2, 4, 1)):
            TS('dve', Ax[:], mod_sb[:, m_scale * 8:(m_scale + 1) * 8, :], 1.0, None, ALU.add, None, ['mod'], ['Ax%d' % gi])
            TT('dve', Ax[:], Ax[:], g12_sb[:, gi, :].unsqueeze(2).broadcast_to([128, 8, 2]), ALU.mult, ['Ax%d' % gi, 'g12'], ['Ax%d' % gi])
        wi = 0
        for (src, dstw, nk, ncol, key) in ((wgate, wgate_b, 8, 3072, 'wgate_b'), (wbr, wbr_b, 12, 1024, 'wbr_b'), (wout, wout_b, 8, 1024, 'wout_b')):
            for c0 in range(0, ncol, 512):
                for kb in range(0, nk, 8):
                    kn = min(8, nk - kb)
                    b = wi % 2
                    wi += 1
                    P.dma('sp', xt[b][:, :kn, :], src[:, kb:kb + kn, c0:c0 + 512], w=[('xt', b)])
                    P.op('pool' if wi % 2 else 'dve', lambda e, b=b, kn=kn, kb=kb, c0=c0, dstw=dstw: e.tensor_copy(
                        out=dstw[:, kb:kb + kn, c0:c0 + 512], in_=xt[b][:, :kn, :]), r=[('xt', b)], w=[key])
        tiles = [(i * 512, 512, 0) for i in range(NT_LAT // 512)]
        if NT_CTX:
            tiles.append((NT_LAT, NT_CTX, 1))

        def norm_mod(src, sz, j, Ax, m_shift, out_fn, okeys, srckeys):
            P.op('act', lambda e: e.activation(out=sq[:, :, :sz], in_=src[:, :, :sz], func=AF.Square), r=srckeys, w=['sq'])
            for k in range(8):
                MM(bank[0][:, :sz], ones[:], sq[:, k, :sz], ['ones', 'sq'], [('ps', 0)], start=(k == 0), stop=(k == 7))
            ACT(rstd[:, :sz], bank[0][:, :sz], AF.Sqrt, [('ps', 0), 'eps'], ['rstd'], scale=1.0 / 1024, bias=eps_sb[:, 0:1])
            P.op('dve', lambda e: e.reciprocal(out=rstd[:, :sz], in_=rstd[:, :sz]), r=['rstd'], w=['rstd'])
            TT('dve', sq[:, :, :sz], src[:, :, :sz], rstd[:, :sz].unsqueeze(1).broadcast_to([128, 8, sz]), ALU.mult,
               srckeys + ['rstd', 'sq'], ['sq'])
            for k in range(8):
                if k % 2:
                    TS('dve', out_fn(k), sq[:, k, :sz], Ax[:, k, j:j + 1],
                       mod_sb[:, m_shift * 8 + k, j:j + 1], ALU.mult, ALU.add, ['sq', 'Ax0', 'Ax1', 'mod'], okeys)
                else:
                    ACT(out_fn(k), sq[:, k, :sz], AF.Identity, ['sq', 'Ax0', 'Ax1', 'mod'], okeys, scale=Ax[:, k, j:j + 1],
                        bias=mod_sb[:, m_shift * 8 + k, j:j + 1])

        pi = 0
        for ti, (t0, sz, j) in enumerate(tiles):
            b = ti % 2
            x_load(xt[b], t0, sz, ('xt', b))
            for n3 in range(3):
                for q in range(4):
                    col0 = (off_lat + q * NT_LAT + t0) if j == 0 else (q * 64 + (t0 - NT_LAT))
                    P.dma('sp', brf[:, :, :sz], gath.gsrc(n3, col0, sz), r=['gath'], w=['brf'])
                    if q == 0:
                        TS('dve', brsel[:, :, :sz], brf[:, :, :sz], selq_sb[:, 0:1], None, ALU.mult, None, ['brf', 'selq'], ['mrgb'])
                    else:
                        STT(brsel[:, :, :sz], brf[:, :, :sz], selq_sb[:, q:q + 1], brsel[:, :, :sz], ALU.mult, ALU.add,
                            ['brf', 'selq', 'mrgb'], ['mrgb'])
                P.op('pool', lambda e, sz=sz, n3=n3: e.tensor_copy(out=brb[:, n3 * 4:(n3 + 1) * 4, :sz], in_=brsel[:, :, :sz]),
                     r=['mrgb'], w=['brb'])
            norm_mod(xt[b], sz, j, A1, 0, lambda k, sz=sz: xn1[:, k, :sz], ['xn1'], [('xt', b)])
            for dc in range(8):
                for n in range(3):
                    pg = bank[1 + pi % 2]
                    pgk = ('ps', 1 + pi % 2)
                    pbk = bank[3 + pi % 2]
                    pbkk = ('ps', 3 + pi % 2)
                    sg_ = sig[pi % 2]
                    sgk = ('sig', pi % 2)
                    pi += 1
                    for k in range(8):
                        MM(pg[:, :sz], wgate_b[:, k, n * 1024 + dc * 128:n * 1024 + (dc + 1) * 128], xn1[:, k, :sz],
                           ['wgate_b', 'xn1'], [pgk], start=(k == 0), stop=(k == 7))
                    for kc in range(4):
                        MM(pbk[:, :sz], wbr_b[:, n * 4 + kc, dc * 128:(dc + 1) * 128], brb[:, n * 4 + kc, :sz],
                           ['wbr_b', 'brb'], [pbkk], start=(kc == 0), stop=(kc == 3))
                    ACT(sg_[:, :sz], pg[:, :sz], AF.Sigmoid, [pgk], [sgk])
                    if n == 0:
                        TT('dve', mrg[:, dc % 2, :sz], sg_[:, :sz], pbk[:, :sz], ALU.mult, [sgk, pbkk], ['mrg'])
                    else:
                        TT('dve', sg_[:, :sz], sg_[:, :sz], pbk[:, :sz], ALU.mult, [sgk, pbkk], [sgk])
                        TT('pool', mrg[:, dc % 2, :sz], mrg[:, dc % 2, :sz], sg_[:, :sz], ALU.add, ['mrg', sgk], ['mrg'])
                P.op('act', lambda e, dc=dc, sz=sz: e.copy(out=mrgb[:, dc, :sz], in_=mrg[:, dc % 2, :sz]), r=['mrg'], w=['mrgb'])
            for dc in range(8):
                pb_ = bank[5 + dc % 2]
                pk_ = ('ps', 5 + dc % 2)
                for k in range(8):
                    MM(pb_[:, :sz], wout_b[:, k, dc * 128:(dc + 1) * 128], mrgb[:, k, :sz], ['wout_b', 'mrgb'], [pk_],
                       start=(k == 0), stop=(k == 7))
                STT(x2[:, dc, :sz], pb_[:, :sz], mod_sb[:, 2 * 8 + dc, j:j + 1], xt[b][:, dc, :sz], ALU.mult, ALU.add,
                    [pk_, 'mod', ('xt', b)], ['x2'])
            P.dma('pool', x2s[:, :, t0:t0 + sz], x2[:, :, :sz], r=['x2'], w=['x2s'])
            xn2f = xt[b]
            xfk = ('xt', b)
            norm_mod(x2, sz, j, A2, 3, lambda k, sz=sz, xn2f=xn2f: xn2f[:, k, :sz], [xfk], ['x2'])
            P.op('act', lambda e, sz=sz, xn2f=xn2f: e.copy(out=xn1[:, :, :sz], in_=xn2f[:, :, :sz]), r=[xfk], w=['xn1'])
            P.dma('pool', xn2s[:, :, t0:t0 + sz], xn1[:, :, :sz], r=['xn1'], w=['xn2s'])
            for s0 in range(0, sz, 128):
                ns = min(128, sz - s0)
                for k in range(8):
                    MM(bank[7][:ns, 0:36], xn2f[:, k, s0:s0 + ns], wrt_sb[:, k, :], [xfk, 'wrt'], [('ps', 7)],
                       start=(k == 0), stop=(k == 7))
                lg = rt[:ns, 0:36]
                P.op('dve', lambda e, ns=ns: e.tensor_copy(out=rt[:ns, 0:36], in_=bank[7][:ns, 0:36]), r=[('ps', 7)], w=['rt'])
                gmax = rt[:ns, 36:37]
                RED(gmax, rt[:ns, 0:4], ALU.max, ['rt'], ['rt'])
                ohg = rt[:ns, 37:41]
                TS('dve', ohg, rt[:ns, 0:4], gmax, None, ALU.is_equal, None, ['rt'], ['rt'])
                ngm = rt[:ns, 41:42]
                TS('dve', ngm, gmax, -1.0, None, ALU.mult, None, ['rt'], ['rt'])
                eg = rt[:ns, 42:46]
                ACT(eg, rt[:ns, 0:4], AF.Exp, ['rt'], ['rt'], bias=ngm)
                pgr = rt[:ns, 46:47]
                RED(pgr, eg, ALU.add, ['rt'], ['rt'])
                P.op('dve', lambda e, pgr=pgr: e.reciprocal(out=pgr, in_=pgr), r=['rt'], w=['rt'])
                TT('dve', g32[:ns, :].rearrange("p (g e) -> p g e", e=8), rt[:ns, 4:36].rearrange("p (g e) -> p g e", e=8),
                   ohg.unsqueeze(2).broadcast_to([ns, 4, 8]), ALU.mult, ['rt'], ['g32'])
                les = rt[:ns, 47:55]
                RED(les, g32[:ns, :].rearrange("p (g e) -> p e g", e=8), ALU.add, ['g32'], ['rt'])
                top1 = rt[:ns, 55:56]
                RED(top1, les, ALU.max, ['rt'], ['rt'])
                oh1 = rt[:ns, 56:64]
                TS('dve', oh1, les, top1, None, ALU.is_equal, None, ['rt'], ['rt'])
                le2 = rt[:ns, 64:72]
                STT(le2, oh1, -1e30, les, ALU.mult, ALU.add, ['rt'], ['rt'])
                top2 = rt[:ns, 72:73]
                RED(top2, le2, ALU.max, ['rt'], ['rt'])
                oh2 = rt[:ns, 73:81] if False else None
                d12 = rt[:ns, 41:42]
                TT('dve', d12, top1, top2, ALU.subtract, ['rt'], ['rt'])
                ga = rt[:ns, 42:43]
                gb = rt[:ns, 43:44]
                ACT(ga, d12, AF.Sigmoid, ['rt'], ['rt'])
                ACT(gb, d12, AF.Sigmoid, ['rt'], ['rt'], scale=-1.0)
                TT('dve', rt[:ns, 42:44], rt[:ns, 42:44], pgr.broadcast_to([ns, 2]), ALU.mult, ['rt'], ['rt'])
                TS('dve', le2, le2, top2, gb, ALU.is_equal, ALU.mult, ['rt'], ['rt'])
                STT(les, oh1, ga, le2, ALU.mult, ALU.add, ['rt'], ['rt'])
                TT('dve', g32[:ns, :].rearrange("p (g e) -> p g e", e=8), ohg.unsqueeze(2).broadcast_to([ns, 4, 8]),
                   les.unsqueeze(1).broadcast_to([ns, 4, 8]), ALU.mult, ['rt', 'g32'], ['g32'])
                MM(bank[7][0:32, 64:64 + ns], g32[:ns, :], ident_sb[:ns, :ns], ['g32', 'ident'], [('ps', 7)])
                P.op('act', lambda e, s0=s0, ns=ns: e.copy(out=gTt[:, s0:s0 + ns], in_=bank[7][0:32, 64:64 + ns]),
                     r=[('ps', 7)], w=['gTt'])
            P.dma('pool', gTs[:, t0:t0 + sz], gTt[:, :sz], r=['gTt'], w=['gTs'])
        P.fence()
        A.release(mB1)
        selE_sb = A.alloc("selE", [32, 32 * 128])
        P.dma('sp', selE_sb[:], selE, w=['selE'])
        lat_ = tiles[:NT_LAT // 512]
        groups = [lat_[i:i + 2] for i in range(0, len(lat_), 2)]
        if NT_CTX:
            groups[-1] = groups[-1] + [tiles[-1]]
        GMAX = max(sum(t[1] for t in g) for g in groups)
        yacc = A.alloc("yacc", [128, 8, GMAX])
        xn2 = A.alloc("xn2g", [128, 8, GMAX], BF16)
        gT = A.alloc("gTg", [32, GMAX])
        wst = [A.alloc("wst%d" % i, [128, 4, 512]) for i in range(4)]
        wgb = [A.alloc("wgb%d" % i, [128, 8, 512], BF16) for i in range(2)]
        wub = [A.alloc("wub%d" % i, [128, 8, 512], BF16) for i in range(2)]
        wdb = [A.alloc("wdb%d" % i, [128, 4, 1024], BF16) for i in range(2)]
        hs = [A.alloc("hs%d" % i, [128, 512]) for i in range(2)]
        actb = [A.alloc("actb%d" % i, [128, 4, 512], BF16) for i in range(2)]
        x2r = A.alloc("x2r", [128, 8, 512])
        si = 0
        ai = 0
        hi_ = 0
        for gi, grp in enumerate(groups):
            g0 = grp[0][0]
            gsz = sum(t[1] for t in grp)
            P.dma('sp', xn2[:, :, :gsz], xn2s[:, :, g0:g0 + gsz], r=['xn2s'], w=['xn2g'])
            P.dma('sp', gT[:, :gsz], gTs[:, g0:g0 + gsz], r=['gTs'], w=['gTg'])
            for e_ in range(NEXP):
                wb = e_ % 2
                pieces = [(wg[e_, :, 0:4, :], wgb[wb][:, 0:4, :], ('wgb', wb)), (wg[e_, :, 4:8, :], wgb[wb][:, 4:8, :], ('wgb', wb)),
                          (wu[e_, :, 0:4, :], wub[wb][:, 0:4, :], ('wub', wb)), (wu[e_, :, 4:8, :], wub[wb][:, 4:8, :], ('wub', wb)),
                          (wd[e_, :, :, 0:512], wdb[wb][:, :, 0:512], ('wdb', wb)), (wd[e_, :, :, 512:1024], wdb[wb][:, :, 512:1024], ('wdb', wb))]
                for (src, dst, key) in pieces:
                    sb_ = si % 4
                    si += 1
                    P.dma('sp', wst[sb_][:], src, w=[('wst', sb_)])
                    eng = ('pool', 'dve', 'act')[si % 3] if False else ('pool' if si % 2 else 'act')
                    if eng == 'act':
                        P.op('act', lambda e, dst=dst, sb_=sb_: e.copy(out=dst, in_=wst[sb_][:]), r=[('wst', sb_)], w=[key])
                    else:
                        P.op('pool', lambda e, dst=dst, sb_=sb_: e.tensor_copy(out=dst, in_=wst[sb_][:]), r=[('wst', sb_)], w=[key])
                for (t0, sz, j) in grp:
                    ti = tiles.index((t0, sz, j))
                    lo = t0 - g0
                    MM(bank[0][:, :sz], selE_sb[:, e_ * 128:(e_ + 1) * 128], gT[:, lo:lo + sz], ['selE', 'gTg'], [('ps', 0)])
                    ab = actb[ai % 2]
                    ak = ('actb', ai % 2)
                    ai += 1
                    for fc in range(4):
                        pgb = bank[1 + fc % 2]
                        pgk = ('ps', 1 + fc % 2)
                        pub = bank[3 + fc % 2]
                        puk = ('ps', 3 + fc % 2)
                        for k in range(8):
                            MM(pgb[:, :sz], wgb[wb][:, k, fc * 128:(fc + 1) * 128], xn2[:, k, lo:lo + sz],
                               [('wgb', wb), 'xn2g'], [pgk], start=(k == 0), stop=(k == 7))
                        for k in range(8):
                            MM(pub[:, :sz], wub[wb][:, k, fc * 128:(fc + 1) * 128], xn2[:, k, lo:lo + sz],
                               [('wub', wb), 'xn2g'], [puk], start=(k == 0), stop=(k == 7))
                        h_ = hs[hi_ % 2]
                        hk = ('hs', hi_ % 2)
                        hi_ += 1
                        ACT(h_[:, :sz], pgb[:, :sz], AF.Silu, [pgk], [hk])
                        TT('dve', h_[:, :sz], h_[:, :sz], pub[:, :sz], ALU.mult, [hk, puk], [hk])
                        TT('dve', ab[:, fc, :sz], h_[:, :sz], bank[0][:, :sz], ALU.mult, [hk, ('ps', 0)], [ak])
                    for dc in range(8):
                        pdb = bank[5 + dc % 3]
                        pdk = ('ps', 5 + dc % 3)
                        for fc in range(4):
                            MM(pdb[:, :sz], wdb[wb][:, fc, dc * 128:(dc + 1) * 128], ab[:, fc, :sz], [('wdb', wb), ak], [pdk],
                               start=(fc == 0), stop=(fc == 3))
                        if e_ == 0:
                            P.op('act', lambda e, dc=dc, lo=lo, sz=sz, pdb=pdb: e.copy(out=yacc[:, dc, lo:lo + sz], in_=pdb[:, :sz]),
                                 r=[pdk], w=['yacc'])
                        else:
                            TT('pool' if False else 'dve', yacc[:, dc, lo:lo + sz], yacc[:, dc, lo:lo + sz], pdb[:, :sz], ALU.add,
                               ['yacc', pdk], ['yacc'])
            for (t0, sz, j) in grp:
                lo = t0 - g0
                P.dma('sp', x2r[:, :, :sz], x2s[:, :, t0:t0 + sz], r=['x2s'], w=['x2r'])
                for dc in range(8):
                    STT(x2r[:, dc, :sz], yacc[:, dc, lo:lo + sz], mod_sb[:, 5 * 8 + dc, j:j + 1], x2r[:, dc, :sz], ALU.mult, ALU.add,
                        ['yacc', 'mod', 'x2r'], ['x2r'])
                x_store(x2r, t0, sz, ['x2r'])
        P.fence()
        A.release(base_mark)


POOL_WINDOWS = (2, 4, 8, 16)
def fm(a):
    return np.ascontiguousarray(a.reshape(8, 128, *a.shape[1:]).swapaxes(0, 1))
def colsel(hg):
    c = []
    c += list(range(hg * 128, hg * 128 + 128))
    c += list(range(512 + hg * 128, 512 + hg * 128 + 128))
    c += list(range(1024 + hg * 128, 1024 + hg * 128 + 128))
    c += list(range(1536 + hg * 128, 1536 + hg * 128 + 128))
    for j in range(3):
        c += list(range(2048 + j * 512 + hg * 128, 2048 + j * 512 + hg * 128 + 128))
    c += list(range(2048 + 1536, 2048 + 1920))
    return np.array(c)
def rope_tables(L):
    nrow = L // 64
    inv = (10000.0 ** (-np.arange(16, dtype=np.float32) / 16)).astype(np.float32)
    R = np.ones((128, 2, nrow), np.float32); C = np.ones((128, 2, 64), np.float32)
    perm = np.zeros((128, 128), np.float32)
    rows = np.arange(nrow, dtype=np.float32); cols = np.arange(64, dtype=np.float32)
    for p in range(128):
        d = p % 64
        blk = d // 16
        f = inv[d % 16]
        sign = -1.0 if blk % 2 == 0 else 1.0
        partner = p + 16 if blk % 2 == 0 else p - 16
        perm[partner, p] = 1.0
        if blk < 2:
            ang = (rows * f).astype(np.float32)
            R[p, 0] = np.cos(ang); R[p, 1] = sign * np.sin(ang)
        else:
            ang = (cols * f).astype(np.float32)
            C[p, 0] = np.cos(ang); C[p, 1] = sign * np.sin(ang)
    return R, C, perm
def edge_fix(w, L):
    t = np.arange(L)
    lo = np.clip(t - w // 2, 0, L - 1); hi = np.clip(t + w // 2 - 1, 0, L - 1)
    ratio = (w / (hi - lo + 1)).astype(np.float32)
    return np.concatenate([ratio[:8], ratio[-8:]])
def inputs_A(inp, l, b, hg, L, lam_init, x=None, ctx=None):
    if x is None:
        x = inp['x'][b, :L]; ctx = inp['ctx'][b]
    xa = np.concatenate([ctx, x], 0)
    R, C, perm = rope_tables(L)
    cs = colsel(hg)
    w = POOL_WINDOWS[hg]
    selw = np.zeros((128, 4), np.float32); selw[:, hg] = 1.0 / w
    d = dict(
        xT=fm(np.ascontiguousarray(xa.T)),
        cT=fm(np.stack([inp['c'][b], inp['c_ctx']], 1)),
        wada=fm(inp['w_ada'][l][:, :2048]),
        bada=np.ascontiguousarray(inp['b_ada'][l][:2048].reshape(16, 128).T),
        g1=np.ascontiguousarray(inp['norm1_g'][l].reshape(8, 128).T),
        win=fm(inp['w_in'][l][:, cs]),
        qkg=np.stack([np.tile(inp['q_norm_g'][l], 2), np.tile(inp['k_norm_g'][l], 2)], 1).astype(np.float32),
        ropeR=R, ropeC=C, perm=perm,
        lamqk=np.ascontiguousarray(np.broadcast_to(inp['lambda_qk'][l].reshape(1, 256), (128, 256))),
        subg=np.ascontiguousarray(np.broadcast_to((inp['subln_g'][l] * np.float32(1 - lam_init)).reshape(1, 128), (128, 128))).astype(np.float32),
        wpool=np.ascontiguousarray(inp['pool_w'][l][hg]),
        pscale=np.ascontiguousarray(inp['pool_scale'][l][hg * 128:(hg + 1) * 128].reshape(128, 1)),
        selw=selw,
        efix=np.ascontiguousarray(np.broadcast_to(edge_fix(w, L).reshape(1, 16), (128, 16))),
    )
    return d

def rw_consts():
    i = np.arange(64)
    incl = (i[:, None] <= i[None, :]).astype(np.float32)
    strict = (i[:, None] < i[None, :]).astype(np.float32)
    ones = np.ones((64, 64), np.float32)
    MkN = (i[None, :] < i[:, None]).astype(np.float32)
    return np.concatenate([incl, strict, ones, strict, incl, MkN, np.eye(64, dtype=np.float32)], 1)
def inputs_rw(inp, l, hg):
    mu = inp['shift_mu'][l]
    cmu = np.zeros((128, 6, 2), np.float32)
    p = np.arange(128)
    for g in range(3):
        cmu[:, g, :] = mu[:, g * 512 + hg * 128 + p].T
    for g, base in ((3, 1536), (4, 1664), (5, 1792)):
        cmu[:, g, :] = mu[:, base + p].T
    heads = [2 * hg, 2 * hg + 1]
    w2 = np.zeros((64, 2, 2, 64), np.float32); a2 = np.zeros((64, 2, 2, 64), np.float32)
    w0b = np.zeros((2, 2, 64), np.float32); a0f = np.zeros((64, 2, 2), np.float32)
    prm = np.zeros((64, 2, 5), np.float32)
    for h, hd in enumerate(heads):
        cs = slice(hd * 64, hd * 64 + 64)
        for d in range(2):
            w2[:, d, h, :] = inp['decay_w2'][l][d][:, cs]
            a2[:, d, h, :] = inp['aaa_a2'][l][d][:, cs]
            w0b[d, h, :] = inp['decay_w0'][l][d][cs]
            a0f[:, d, h] = inp['aaa_a0'][l][d][cs]
        prm[:, h, 0] = inp['k_k'][l][cs]; prm[:, h, 1] = inp['k_a'][l][cs]; prm[:, h, 2] = inp['r_k'][l][hd]
        prm[:, h, 3] = inp['gn_w'][l][cs]; prm[:, h, 4] = inp['gn_b'][l][cs]
    return dict(rw_cmu=cmu, rw_g2=np.ascontiguousarray(inp['gate_w2'][l][:, hg * 128:(hg + 1) * 128]),
                rw_w2=w2.reshape(64, 256), rw_a2=a2.reshape(64, 256),
                rw_w0b=np.ascontiguousarray(np.broadcast_to(w0b.reshape(1, 256), (64, 256))),
                rw_a0f=a0f.reshape(64, 4), rw_prm=prm, rw_cst=rw_consts())


def weights_B(inp, l):
    selE = np.zeros((32, 32, 128), np.float32)
    for e in range(32): selE[e, e, :] = 1.0
    return dict(
        wada=fm(inp['w_ada'][l]),
        bada=np.ascontiguousarray(inp['b_ada'][l].reshape(48, 128).T),
        g12=np.ascontiguousarray(np.stack([inp['norm1_g'][l].reshape(8, 128).T, inp['norm2_g'][l].reshape(8, 128).T], 1)),
        wgate=fm(inp['w_in'][l][:, 3968:]),
        wbr=np.ascontiguousarray(inp['w_br'][l].reshape(12, 128, 1024).transpose(1, 0, 2)),
        wout=fm(inp['w_out'][l]),
        wrt=fm(np.concatenate([inp['w_router_group'][l], inp['w_router_expert'][l]], 1)),
        wg=np.ascontiguousarray(inp['w_exp_gate'][l].reshape(32, 8, 128, 512).transpose(0, 2, 1, 3)),
        wu=np.ascontiguousarray(inp['w_exp_up'][l].reshape(32, 8, 128, 512).transpose(0, 2, 1, 3)),
        wd=np.ascontiguousarray(inp['w_exp_down'][l].reshape(32, 4, 128, 1024).transpose(0, 2, 1, 3)),
        selE=selE.reshape(32, 4096), ident=np.eye(128, dtype=np.float32))
def acts_B(xa, br, cvec, c_ctx):
    NT = xa.shape[0]
    return dict(xT=fm(np.ascontiguousarray(xa.T)),
                brT=np.ascontiguousarray(br.T.reshape(12, 128, NT).transpose(1, 0, 2)),
                cT=fm(np.stack([cvec, c_ctx], 1)))


GROUPS = [[0, 1, 2, 3], [4, 5, 6, 7]]


CC_COLS = 2048


class BrStore:
    def __init__(self, nc, name, NQ, ctx_out):
        self.splits = ([(0, 256)] if ctx_out else []) + [(c0, min(CC_COLS, NQ - c0)) for c0 in range(256 if ctx_out else 0, NQ, CC_COLS)]
        self.b = {}
        self.g = {}
        for n in range(3):
            for ci, (c0, cs) in enumerate(self.splits):
                self.b[(n, ci)] = nc.dram_tensor("%s_b%d_%d" % (name, n, ci), [128, cs], F32, kind="Internal").ap()
                self.g[(n, ci)] = nc.dram_tensor("%s_g%d_%d" % (name, n, ci), [4 * 128, cs], F32, kind="Internal").ap()

    def _find(self, col0, ncols):
        for ci, (c0, cs) in enumerate(self.splits):
            if c0 <= col0 and col0 + ncols <= c0 + cs:
                return ci, col0 - c0
        raise AssertionError(("chunk straddle", col0, ncols))

    def dst(self, n, col0, ncols):
        ci, o = self._find(col0, ncols)
        return self.b[(n, ci)][:, o:o + ncols]

    def gsrc(self, n, col0, ncols):
        ci, o = self._find(col0, ncols)
        return self.g[(n, ci)].rearrange("(g p) t -> p g t", g=4)[:, :, o:o + ncols]

    def exchange(self, P):
        for key in self.b:
            P.cc(self.b[key], self.g[key], GROUPS, r=['brb'], w=['gath'])


class XStore:
    def __init__(self, nc, name, NL):
        self.NL = NL
        NT0 = NL + 64
        self.splits = [(c0, min(CC_COLS, NL - c0)) for c0 in range(0, NL, CC_COLS)] + [(NL, 64)]
        self.b = {}
        self.g = {}
        for k in range(8):
            for ci, (c0, cs) in enumerate(self.splits):
                self.b[(k, ci)] = nc.dram_tensor("%s_b%d_%d" % (name, k, ci), [128, cs], F32, kind="Internal").ap()
                self.g[(k, ci)] = nc.dram_tensor("%s_g%d_%d" % (name, k, ci), [4 * 128, cs], F32, kind="Internal").ap()

    def _find(self, col0, ncols):
        for ci, (c0, cs) in enumerate(self.splits):
            if c0 <= col0 and col0 + ncols <= c0 + cs:
                return ci, col0 - c0
        raise AssertionError(("chunk straddle", col0, ncols))

    def store(self, P, src_tile, t0, sz, rkeys):
        ci, o = self._find(t0, sz)
        for k in range(8):
            P.dma('pool', self.b[(k, ci)][:, o:o + sz], src_tile[:, k, :sz], r=rkeys, w=['xnew'])

    def load_local(self, P, dst_tile, t0, sz, key):
        ci, o = self._find(t0, sz)
        for k in range(8):
            P.dma('sp', dst_tile[:, k, :sz], self.b[(k, ci)][:, o:o + sz], r=['xnew'], w=[key])

    def load_gathered(self, P, dst_tile, dcol, rank, t0, sz, key):
        ci, o = self._find(t0, sz)
        for k in range(8):
            P.dma('sp', dst_tile[:, k, dcol:dcol + sz], self.g[(k, ci)][rank * 128:(rank + 1) * 128, o:o + sz], r=['xg'], w=[key])

    def exchange(self, P):
        for key in self.b:
            P.cc(self.b[key], self.g[key], GROUPS, r=['xnew'], w=['xg'])


def build_fused(L=16384, nexp=32, stop_after=99):
    nc = bass.Bass("TRN2", target_bir_lowering=False)
    nc.allow_low_precision("bf16 matmul operands, fp32 accumulation")
    P = Prog(nc)
    A = Arena(nc)
    T = 256 + L
    NL = L // 4
    NT0 = NL + 64
    lam = [0.8 - 0.6 * math.exp(-0.3 * l) for l in range(2)]
    with contextlib.ExitStack() as st:
        bank = [st.enter_context(nc.psum_tensor("bank%d" % i, [128, 512], F32)) for i in range(8)]
        xout = nc.dram_tensor("xout", [128, 8, NL], F32, kind="ExternalOutput").ap()

        def finish():
            stats = P.emit()
            stats['sbuf_peak'] = A.peak
            return nc, stats
        br0 = BrStore(nc, "br0", T, True)
        phase_A(nc, P, A, bank, "A0_", L, True, lam[0], None, br0)
        P.fence()
        if stop_after == 1:
            return finish()
        br0.exchange(P)
        P.fence()
        if stop_after == 2:
            return finish()
        xsh = nc.dram_tensor("xsh", [128, 8, NT0], F32, kind="ExternalInput").ap()
        xs = XStore(nc, "xs", NL)

        def x_load0(dst, t0, sz, key):
            P.dma('sp', dst[:, :, :sz], xsh[:, :, t0:t0 + sz], w=[key])
        phase_B(nc, P, A, bank, "B0_", NL, 64, x_load0, br0, 256, lambda src, t0, sz, rk: xs.store(P, src, t0, sz, rk), NEXP=nexp)
        if stop_after == 3:
            return finish()
        xs.exchange(P)
        P.fence()
        if stop_after == 4:
            return finish()

        def x_loader(dst, t0, sz, key):
            if t0 < 256:
                for r in range(4):
                    xs.load_gathered(P, dst, r * 64, r, NL, 64, key)
            else:
                tt = t0 - 256
                xs.load_gathered(P, dst, 0, tt // NL, tt % NL, sz, key)
        br1 = BrStore(nc, "br1", L, False)
        phase_A(nc, P, A, bank, "A1_", L, False, lam[1], x_loader, br1)
        P.fence()
        if stop_after == 5:
            return finish()
        br1.exchange(P)
        P.fence()
        if stop_after == 6:
            return finish()

        def x_store1(src, t0, sz, rk):
            P.dma('pool', xout[:, :, t0:t0 + sz], src[:, :, :sz], r=rk, final=True)
        phase_B(nc, P, A, bank, "B1_", NL, 0, lambda dst, t0, sz, key: xs.load_local(P, dst, t0, sz, key), br1, 0, x_store1, NEXP=nexp)
        return finish()


def fused_inputs(inp, L, nexp=32, names=None):
    inp = {k: np.asarray(v) for k, v in inp.items()}
    NL = L // 4
    x = inp['x'][:, :L]
    ctx = inp['ctx']
    lam = [0.8 - 0.6 * math.exp(-0.3 * l) for l in range(2)]
    WB = [weights_B(inp, l) for l in range(2)]
    ident = np.eye(128, dtype=np.float32)
    maps = []
    for i in range(8):
        b, hg = i // 4, i % 4
        d = {}
        for l in range(2):
            a = inputs_A(inp, l, b, hg, L, lam[l], x=x[b], ctx=ctx[b])
            if l == 1:
                a.pop('xT')
            a.update(inputs_rw(inp, l, hg))
            a['identA'] = ident
            for k, v in a.items():
                d["A%d_" % l + k] = v
            for k, v in WB[l].items():
                d["B%d_" % l + k] = v[:nexp] if k in ('wg', 'wu', 'wd') else v
            d["B%d_cT" % l] = fm(np.stack([inp['c'][b], inp['c_ctx']], 1))
            selq = np.zeros((128, 4), np.float32)
            selq[:, hg] = 1.0
            d["B%d_selq" % l] = selq
        xa = np.concatenate([x[b, hg * NL:(hg + 1) * NL], ctx[b, hg * 64:(hg + 1) * 64]], 0)
        d['xsh'] = fm(np.ascontiguousarray(xa.T))
        if names is not None:
            d = {k: v for k, v in d.items() if k in names}
        maps.append(d)
    return maps


def fused_gather(results, L):
    NL = L // 4
    out = np.empty((2, L, 1024), np.float32)
    for i in range(8):
        b, q = i // 4, i % 4
        o = np.asarray(results[i]['xout']).transpose(1, 0, 2).reshape(1024, NL).T
        out[b, q * NL:(q + 1) * NL] = o
    return out


def kernel(**inp):
    inp = {k: np.asarray(v) for k, v in inp.items()}
    L = inp['x'].shape[1]
    nc, _ = build_fused(L)
    maps = fused_inputs(inp, L)
    res = run_bass_kernel_spmd(nc, maps, core_ids=list(range(8)))
    del maps
    return fused_gather(res.results, L)
```

```python
import math
import contextlib


import numpy as np
import concourse.bass as bass
import concourse.mybir as mybir
from concourse.bass_utils import run_bass_kernel_spmd

F32 = mybir.dt.float32
BF16 = mybir.dt.bfloat16
I32 = mybir.dt.int32
AF = mybir.ActivationFunctionType
ALU = mybir.AluOpType
AX = mybir.AxisListType

SEM_LIMIT = 8000
DMA_POOL = 40


class Prog:
    def __init__(self, nc):
        self.nc = nc
        self.ops = []

    def op(self, eng, fn, r=(), w=()):
        self.ops.append(dict(eng=eng, fn=fn, r=tuple(r), w=tuple(w), dma=False, final=False))

    def dma(self, eng, out, in_, r=(), w=(), final=False, **kw):
        def fn(e, out=out, in_=in_, kw=kw):
            return e.dma_start(out=out, in_=in_, **kw)
        self.ops.append(dict(eng=eng, fn=fn, r=tuple(r), w=tuple(w), dma=True, final=final))

    def cc(self, ins_ap, out_ap, groups, r=(), w=()):
        def fn(e, ins_ap=ins_ap, out_ap=out_ap, groups=groups):
            return e.collective_compute("AllGather", ALU.bypass, replica_groups=groups, ins=[ins_ap], outs=[out_ap])
        self.ops.append(dict(eng='pool', fn=fn, r=tuple(r), w=tuple(w), dma=True, final=False, cc=True))

    def fence(self):
        self.ops.append(dict(eng=None, fn=None, r=(), w=(), dma=False, final=False, fence=True))

    def emit(self):
        nc = self.nc
        raw_ops = self.ops
        ops = []
        fence_after = {}
        fence_pos = []
        for o in raw_ops:
            if o.get('fence'):
                fence_pos.append(len(ops))
            else:
                ops.append(o)
        self.ops = ops
        n = len(ops)
        fence_deps_at = {}
        prev = 0
        for fp in fence_pos:
            last = {}
            dm = set()
            for i in range(prev, fp):
                o = ops[i]
                if o['dma']:
                    dm.add(i)
                else:
                    last[o['eng']] = i
            fence_deps_at[fp] = set(last.values()) | dm
            prev = fp
        last_w = {}
        readers = {}
        deps = [None] * n
        cur_fence = set()
        first_after = {}
        for i, o in enumerate(ops):
            if i in fence_deps_at:
                cur_fence = fence_deps_at[i]
                first_after = {}
            d = {}

            def add(j, raw):
                d[j] = d.get(j, False) or raw
            for k in o['r']:
                if k in last_w:
                    add(last_w[k], True)
            for k in o['w']:
                if k in last_w:
                    add(last_w[k], False)
                for tok, j in readers.get(k, {}).items():
                    add(j, False)
            keep = set()
            for j, raw in d.items():
                if j == i:
                    continue
                oj = ops[j]
                if (not oj['dma']) and (not o['dma']) and oj['eng'] == o['eng']:
                    if o['eng'] == 'pe':
                        continue
                    if not raw:
                        continue
                keep.add(j)
            tok_e = o['eng']
            if cur_fence and tok_e not in first_after:
                first_after[tok_e] = i
                for j in cur_fence:
                    if ops[j]['dma'] or ops[j]['eng'] != tok_e:
                        keep.add(j)
            deps[i] = keep
            for k in o['r']:
                tok = ('d', i) if o['dma'] else o['eng']
                readers.setdefault(k, {})[tok] = i
            for k in o['w']:
                last_w[k] = i
                readers[k] = {}
        needed = [False] * n
        for i in range(n):
            for j in deps[i]:
                needed[j] = True
        engs = ['pe', 'act', 'dve', 'pool', 'sp']
        cnt = {e: 0 for e in engs}
        sig = [None] * n
        dma_uses = [0] * DMA_POOL
        dma_last = [None] * DMA_POOL
        ndma = 0
        semkeys = set()
        for i, o in enumerate(ops):
            if o.get('cc'):
                ncc_ = getattr(self, '_ncc', 0) + 1
                self._ncc = ncc_
                sig[i] = (('cc', 0), ncc_)
                semkeys.add(('cc', 0))
            elif o['dma']:
                j = ndma % DMA_POOL
                ndma += 1
                dma_uses[j] += 1
                if dma_last[j] is not None:
                    deps[i].add(dma_last[j])
                dma_last[j] = i
                sig[i] = (('dma', j), 16 * dma_uses[j])
                semkeys.add(('dma', j))
            elif needed[i]:
                e = o['eng']
                c = cnt[e]
                cnt[e] += 1
                sk = (e, c // SEM_LIMIT)
                sig[i] = (sk, c % SEM_LIMIT + 1)
                semkeys.add(sk)
        finals = [i for i, o in enumerate(ops) if o['final']]
        seen = {e: {} for e in engs}
        streams = {e: [] for e in engs}
        for i, o in enumerate(ops):
            e = o['eng']
            waits = {}
            for j in deps[i]:
                sk, v = sig[j]
                if seen[e].get(sk, 0) >= v:
                    continue
                waits[sk] = max(waits.get(sk, 0), v)
            for sk, v in waits.items():
                seen[e][sk] = v
            streams[e].append((list(waits.items()), o['fn'], sig[i]))
        fw = {}
        for i in finals:
            sk, v = sig[i]
            if seen['sp'].get(sk, 0) >= v:
                continue
            fw[sk] = max(fw.get(sk, 0), v)
        streams['sp'].append((list(fw.items()), None, None))
        self.stats = dict(n_ops=n, cnt=dict(cnt), ndma=ndma,
                          nwaits={e: sum(len(s[0]) for s in streams[e]) for e in engs})
        semkeys = sorted(semkeys, key=str)
        import contextlib
        with contextlib.ExitStack() as st:
            sems = {}
            for sk in semkeys:
                sems[sk] = st.enter_context(nc.semaphore("s_%s_%s" % (sk[0], sk[1])))
            block = st.enter_context(nc.Block())

            def run(engine, items):
                for waits, fn, sg in items:
                    for sk, v in waits:
                        engine.wait_ge(sems[sk], v)
                    if fn is None:
                        continue
                    ins = fn(engine)
                    if sg is not None:
                        inc = 16 if sg[0][0] == 'dma' else 1
                        ins.then_inc(sems[sg[0]], inc)

            @block.tensor
            def _(e):
                run(e, streams['pe'])

            @block.scalar
            def _(e):
                run(e, streams['act'])

            @block.vector
            def _(e):
                run(e, streams['dve'])

            @block.gpsimd
            def _(e):
                run(e, streams['pool'])

            @block.sync
            def _(e):
                run(e, streams['sp'])
        return self.stats


class Arena:
    LO = 16512
    HI = 229376

    def __init__(self, nc):
        self.nc = nc
        self.top = self.LO
        self.n = 0
        self.peak = self.LO

    def alloc(self, name, shape, dt=F32):
        esz = {F32: 4, BF16: 2, I32: 4}[dt]
        nb = esz
        for s_ in shape[1:]:
            nb *= s_
        off = (self.top + 63) // 64 * 64
        assert off + nb <= self.HI, ("SBUF overflow", name, off + nb)
        self.top = off + nb
        self.peak = max(self.peak, self.top)
        self.n += 1
        return self.nc.alloc_sbuf_tensor_at("%s_%d" % (name, self.n), list(shape), dt, offset=off)

    def mark(self):
        return self.top

    def release(self, m):
        self.top = m


GN_EPS = 64e-5
FINAL_OUT = False
KAPPA = 0.6065306597126334
CTXN = 256


def rwkv_phase(nc, P, A, bank, pT, L, ctx_out, out_ap, pfx=''):
    T = CTXN + L
    di = lambda name, shape: nc.dram_tensor(pfx + name, list(shape), F32, kind="ExternalInput").ap()
    cmu = di("rw_cmu", [128, 6, 2])
    g2s = di("rw_g2", [128, 128])
    w2s = di("rw_w2", [64, 256])
    a2s = di("rw_a2", [64, 256])
    w0b = di("rw_w0b", [64, 256])
    a0f = di("rw_a0f", [64, 4])
    prm = di("rw_prm", [64, 2, 5])
    cst = di("rw_cst", [64, 448])
    uT = nc.dram_tensor(pfx + "uT_scr", [6, 128, T], F32, kind="Internal").ap()
    Yd = nc.dram_tensor(pfx + "Yd_scr", [2, 128, T], F32, kind="Internal").ap()
    Bd = nc.dram_tensor(pfx + "Bd_scr", [2, 128, T], F32, kind="Internal").ap()

    def TT(eng, out, a, b, op, r, w):
        P.op(eng, lambda e: e.tensor_tensor(out=out, in0=a, in1=b, op=op), r=r, w=w)

    def TS(eng, out, a, s1, s2, op0, op1, r, w):
        if op1 is None:
            P.op(eng, lambda e: e.tensor_scalar(out=out, in0=a, scalar1=s1, scalar2=None, op0=op0), r=r, w=w)
        else:
            P.op(eng, lambda e: e.tensor_scalar(out=out, in0=a, scalar1=s1, scalar2=s2, op0=op0, op1=op1), r=r, w=w)

    def STT(out, a, s, b, op0, op1, r, w):
        P.op('dve', lambda e: e.scalar_tensor_tensor(out=out, in0=a, scalar=s, in1=b, op0=op0, op1=op1), r=r, w=w)

    def ACT(out, a, func, r, w, scale=1.0, bias=None):
        if bias is None:
            P.op('act', lambda e: e.activation(out=out, in_=a, func=func, scale=scale), r=r, w=w)
        else:
            P.op('act', lambda e: e.activation(out=out, in_=a, func=func, scale=scale, bias=bias), r=r, w=w)

    def MM(out, lhsT, rhs, r, w, start=True, stop=True):
        P.op('pe', lambda e: e.matmul(out, lhsT=lhsT, rhs=rhs, start=start, stop=stop), r=r, w=w)

    m0 = A.mark()
    cmu_sb = A.alloc("cmu", [128, 6, 2])
    c0_sb = A.alloc("c0", [128, 6])
    g2_sb = A.alloc("g2", [128, 128])
    raw = [A.alloc("raw%d" % i, [128, 6, 514]) for i in range(2)]
    ush = A.alloc("ush", [128, 6, 512])
    sg = A.alloc("sg", [128, 512])
    P.dma('sp', cmu_sb[:], cmu, w=['cmu'])
    P.dma('sp', g2_sb[:], g2s, w=['g2'])
    TT('dve', c0_sb[:], cmu_sb[:, :, 0], cmu_sb[:, :, 1], ALU.add, ['cmu'], ['c0'])
    TS('dve', c0_sb[:], c0_sb[:], -1.0, 1.0, ALU.mult, ALU.add, ['c0'], ['c0'])
    seqs = [(0, CTXN), (CTXN, L)]
    ti = 0
    for (s0, slen) in seqs:
        for b0 in range(0, slen, 512):
            n = min(512, slen - b0)
            rb = raw[ti % 2]
            rk = ('raw', ti % 2)
            ti += 1
            lo = max(0, b0 - 1)
            hi = min(slen, b0 + n + 1)
            if b0 == 0 or b0 + n == slen:
                P.op('pool', lambda e, rb=rb: e.memset(rb[:], 0.0), w=[rk])
            P.dma('sp', rb[:, :, 1 + lo - b0:1 + hi - b0], pT[1:7, :, s0 + lo:s0 + hi].rearrange("g p t -> p g t"),
                  r=['pTall'], w=[rk])
            for g in range(6):
                eng = 'dve'
                TS('pool', ush[:, g, :n], rb[:, g, 1:1 + n], c0_sb[:, g:g + 1], None, ALU.mult, None, [rk, 'c0'], ['ush'])
                STT(ush[:, g, :n], rb[:, g, 0:n], cmu_sb[:, g, 0:1], ush[:, g, :n], ALU.mult, ALU.add, [rk, 'cmu', 'ush'], ['ush'])
                STT(ush[:, g, :n], rb[:, g, 2:2 + n], cmu_sb[:, g, 1:2], ush[:, g, :n], ALU.mult, ALU.add, [rk, 'cmu', 'ush'], ['ush'])
            ACT(sg[:, :n], ush[:, 5, :n], AF.Sigmoid, ['ush'], ['sg'])
            MM(bank[0][:, :n], g2_sb[:], sg[:, :n], ['g2', 'sg'], [('ps', 0)])
            P.op('act', lambda e, n=n: e.copy(out=ush[:, 5, :n], in_=bank[0][:, :n]), r=[('ps', 0), 'ush'], w=['ush'])
            P.dma('pool', uT[:, :, s0 + b0:s0 + b0 + n].rearrange("g p t -> p g t"), ush[:, :, :n], r=['ush'], w=['uTall'])
    P.fence()
    A.release(m0)
    w2_sb = A.alloc("w2", [64, 256])
    a2_sb = A.alloc("a2", [64, 256])
    w0b_sb = A.alloc("w0b", [64, 256])
    a0f_sb = A.alloc("a0f", [64, 4])
    prm_sb = A.alloc("prm", [64, 2, 5])
    cst_sb = A.alloc("cst", [64, 448])
    ones64 = A.alloc("ones64", [64, 64])
    for nm, dst, src in [('w2', w2_sb, w2s), ('a2', a2_sb, a2s), ('w0b', w0b_sb, w0b), ('a0f', a0f_sb, a0f),
                         ('prm', prm_sb, prm), ('cst', cst_sb, cst)]:
        P.dma('sp', dst[:], src, w=[nm])
    P.op('pool', lambda e: e.memset(ones64[:], 1.0), w=['ones64'])
    Tri3 = cst_sb[:, 0:192]
    Mk = cst_sb[:, 192:320]
    MkN = cst_sb[:, 320:384]
    I64 = cst_sb[:, 384:448]
    I64b = A.alloc("I64b", [64, 64], BF16)
    P.op('dve', lambda e: e.tensor_copy(out=I64b[:], in_=cst_sb[:, 384:448]), r=['cst'], w=['I64b'])
    ST = A.alloc("ST", [64, 4, 64])
    Stmp = A.alloc("Stmp", [64, 4, 64])
    P.op('pool', lambda e: e.memset(ST[:], 0.0), w=['ST'])
    KSEG = 4
    NBUF = KSEG + 1
    f4 = lambda name: A.alloc(name, [64, 4, 64])

    shr = dict(L_=dict(r=A.alloc("ld_r", [64, 2, 2, 64]), k=A.alloc("ld_k", [64, 2, 2, 64]), v=A.alloc("ld_v", [64, 2, 2, 64]),
                       wl=A.alloc("ld_wl", [64, 2, 64]), al=A.alloc("ld_al", [64, 2, 64])),
               uwl=A.alloc("uwl", [64, 2, 64]), ual=A.alloc("ual", [64, 2, 64]), tw=A.alloc("tw", [64, 2, 64]),
               swt=A.alloc("swt", [64, 256]), kk=f4("kk"), kk2=f4("kk2"), rn=f4("rn"), kkn=f4("kkn"), bb=f4("bb"), km=f4("km"),
               t1=f4("t1"), BhT=A.alloc("BhT", [64, 4, 64], BF16), KhT=A.alloc("KhT", [64, 4, 64], BF16), Xsb=f4("Xsb"), Usb=f4("Usb"), Ysb=f4("Ysb"))

    def alloc_set(i):
        n_ = lambda x: "%s_%d" % (x, i)
        return (shr['L_'], f4(n_("ur")), f4(n_("uk")), f4(n_("uv")), shr['uwl'], shr['ual'],
                shr['tw'], shr['swt'], f4(n_("alr")),
                f4(n_("eI")), f4(n_("eE")), f4(n_("eN")), f4(n_("eT")), A.alloc(n_("gC"), [64, 4]),
                shr['kk'], shr['kk2'], shr['rn'], shr['kkn'], shr['bb'], shr['km'], shr['t1'],
                A.alloc(n_("AR"), [64, 4, 128]), f4(n_("BT")), f4(n_("KTt")), shr['BhT'], shr['KhT'], f4(n_("bon")),
                A.alloc(n_("TM"), [64, 2, 4, 64]), f4(n_("Vt")), A.alloc(n_("Gb"), [64, 4, 128]), A.alloc(n_("Gk"), [64, 4, 128]),
                A.alloc(n_("Nn"), [64, 4, 64], BF16), [A.alloc(n_("Pk%d" % j), [64, 2, 4, 64], BF16) for j in range(2)],
                [A.alloc(n_("Q0"), [64, 4, 64], BF16), A.alloc(n_("Q1"), [64, 4, 64], BF16), f4(n_("Qf")), A.alloc(n_("P0b"), [64, 4, 64], BF16)],
                shr['Xsb'], shr['Usb'], shr['Ysb'])
    sets = [alloc_set(i) for i in range(NBUF)]
    prmb = lambda j: prm_sb[:, :, j:j + 1].unsqueeze(1).broadcast_to([64, 2, 2, 64])
    v4 = lambda t: t[:].rearrange("p (d h) t -> p d h t", d=2)
    SHARED = set(['w2', 'a2', 'w0b', 'a0f', 'prm', 'cst', 'ones64', 'uTall', 'Bdall', 'Ydall', 'ST', 'Stmp', 'I64b',
                  'ld', 'wl', 'al', 'tw', 'swt', 'kk', 'kk2', 'rn', 'kkn', 'bb', 'km', 't1', 'BhT', 'KhT', 'Xsb', 'Usb', 'Ysb'])
    cur = {'b': None}

    def kmap(keys):
        if cur['b'] is None:
            return list(keys)
        return [k if (k in SHARED or (isinstance(k, tuple) and k[0] == 'ps')) else ('rw', k, cur['b']) for k in keys]
    _op, _dma = P.op, P.dma

    def Pop(eng, fn, r=(), w=()):
        _op(eng, fn, r=kmap(r), w=kmap(w))

    def Pdma(eng, out, in_, r=(), w=(), **kw):
        _dma(eng, out, in_, r=kmap(r), w=kmap(w), **kw)

    def TT(eng, out, a, b, op, r, w):
        Pop(eng, lambda e: e.tensor_tensor(out=out, in0=a, in1=b, op=op), r=r, w=w)

    def TS(eng, out, a, s1, s2, op0, op1, r, w):
        if op1 is None:
            Pop(eng, lambda e: e.tensor_scalar(out=out, in0=a, scalar1=s1, scalar2=None, op0=op0), r=r, w=w)
        else:
            Pop(eng, lambda e: e.tensor_scalar(out=out, in0=a, scalar1=s1, scalar2=s2, op0=op0, op1=op1), r=r, w=w)

    def STT(out, a, s, b, op0, op1, r, w):
        Pop('dve', lambda e: e.scalar_tensor_tensor(out=out, in0=a, scalar=s, in1=b, op0=op0, op1=op1), r=r, w=w)

    def ACT(out, a, func, r, w, scale=1.0, bias=None):
        if bias is None:
            Pop('act', lambda e: e.activation(out=out, in_=a, func=func, scale=scale), r=r, w=w)
        else:
            Pop('act', lambda e: e.activation(out=out, in_=a, func=func, scale=scale, bias=bias), r=r, w=w)

    def MM(out, lhsT, rhs, r, w, start=True, stop=True):
        Pop('pe', lambda e: e.matmul(out, lhsT=lhsT, rhs=rhs, start=start, stop=stop), r=r, w=w)

    nlat = L // 64
    steps = [(s, 3 - s) for s in range(4)] + [(4 + s, 4 + nlat - 1 - s) for s in range(nlat)]
    NS = len(steps)

    def gen_step(si):
        cf, cb = steps[si]
        cur['b'] = si % NBUF
        (L_, ur, uk, uv, uwl, ual, tw, swt, alr, eI, eE, eN, eT, gC, kk, kk2, rn, kkn, bb, km, t1, AR, BT, KTt, BhT, KhT, bon,
         TM, Vt, Gb, Gk, Nn, Pk, Qk, Xsb, Usb, Yb) = sets[si % NBUF]
        saved = P.ops
        P.ops = []
        marks = []
        lk = 'ld'
        for d, cidx in enumerate((cf, cb)):
            t0 = cidx * 64
            for nm, row in (('r', 0), ('k', 1), ('v', 2)):
                Pdma('sp', L_[nm][:, d, :, :], uT[row, :, t0:t0 + 64].rearrange("(h c) t -> c h t", h=2), r=['uTall'], w=[lk])
            Pdma('sp', L_['wl'][:, d, :], uT[3, d * 64:(d + 1) * 64, t0:t0 + 64], r=['uTall'], w=[lk])
            Pdma('sp', L_['al'][:, d, :], uT[4, d * 64:(d + 1) * 64, t0:t0 + 64], r=['uTall'], w=[lk])
        for nm, dst in (('r', ur), ('k', uk), ('v', uv)):
            d4 = v4(dst)
            Pop('pool', lambda e, d4=d4, src=L_[nm]: e.tensor_copy(out=d4[:, 0], in_=src[:, 0]), r=[lk], w=[nm])
            Pop('pool', lambda e, d4=d4, src=L_[nm]: e.tensor_copy(out=d4[:, 1], in_=src[:, 1, :, ::-1]), r=[lk], w=[nm])
        for nm, dst in (('wl', uwl), ('al', ual)):
            Pop('pool', lambda e, dst=dst, src=L_[nm]: e.tensor_copy(out=dst[:, 0, :], in_=src[:, 0, :]), r=[lk], w=[nm])
            Pop('pool', lambda e, dst=dst, src=L_[nm]: e.tensor_copy(out=dst[:, 1, :], in_=src[:, 1, ::-1]), r=[lk], w=[nm])
        ACT(tw[:], uwl[:], AF.Tanh, ['wl'], ['tw'])
        for d in range(2):
            for h in range(2):
                dh = d * 2 + h
                MM(bank[0][0:64, dh * 64:(dh + 1) * 64], tw[:, d, :], w2_sb[:, dh * 64:(dh + 1) * 64], ['tw', 'w2'], [('ps', 0)])
                MM(bank[0][0:64, 256 + dh * 64:256 + (dh + 1) * 64], a2_sb[:, dh * 64:(dh + 1) * 64], ual[:, d, :],
                   ['a2', 'al'], [('ps', 0)])
        TT('dve', swt[:], bank[0][0:64, 0:256], w0b_sb[:], ALU.add, [('ps', 0), 'w0b'], ['swt'])
        ACT(swt[:], swt[:], AF.Sigmoid, ['swt'], ['swt'])
        TT('dve', alr[:], bank[0][0:64, 256:512].rearrange("p (a t) -> p a t", t=64),
           a0f_sb[:].unsqueeze(2).broadcast_to([64, 4, 64]), ALU.add, [('ps', 0), 'a0f'], ['alr'])
        ACT(alr[:], alr[:], AF.Sigmoid, ['alr'], ['alr'])
        cbanks = (1, 0)
        for dh in range(4):
            bk = cbanks[dh // 2]
            MM(bank[bk][0:64, (dh % 2) * 192:(dh % 2) * 192 + 192], swt[:, dh * 64:(dh + 1) * 64], Tri3, ['swt', 'cst'], [('ps', bk)])
        for half in range(2):
            bk = cbanks[half]
            cv = bank[bk][0:64, 0:384].rearrange("p (a x) -> p a x", x=192)
            sl = slice(half * 2, half * 2 + 2)
            ACT(eI[:, sl, :], cv[:, :, 0:64], AF.Exp, [('ps', bk)], ['eI'], scale=-KAPPA)
            ACT(eE[:, sl, :], cv[:, :, 64:128], AF.Exp, [('ps', bk)], ['eE'], scale=-KAPPA)
            ACT(eN[:, sl, :], cv[:, :, 0:64], AF.Exp, [('ps', bk)], ['eN'], scale=KAPPA)
            ACT(gC[:, sl], cv[:, :, 128], AF.Exp, [('ps', bk)], ['gC'], scale=-KAPPA)
        TT('dve', eT[:], eN[:], gC[:].unsqueeze(2).broadcast_to([64, 4, 64]), ALU.mult, ['eN', 'gC'], ['eT'])
        marks.append(len(P.ops))
        TT('dve', v4(kk), v4(uk), prmb(0), ALU.mult, ['k', 'prm'], ['kk'])
        TT('pool', kk2[:], kk[:], kk[:], ALU.mult, ['kk'], ['kk2'])
        MM(bank[2][0:64, 0:256], ones64[:], kk2[:].rearrange("p a t -> p (a t)"), ['ones64', 'kk2'], [('ps', 2)])
        ACT(rn[:].rearrange("p a t -> p (a t)"), bank[2][0:64, 0:256], AF.Sqrt, [('ps', 2)], ['rn'])
        TS('dve', rn[:], rn[:], 1e-12, None, ALU.max, None, ['rn'], ['rn'])
        Pop('dve', lambda e: e.reciprocal(out=rn[:], in_=rn[:]), r=['rn'], w=['rn'])
        TT('dve', kkn[:], kk[:], rn[:], ALU.mult, ['kk', 'rn'], ['kkn'])
        TT('pool', bb[:], kkn[:], alr[:], ALU.mult, ['kkn', 'alr'], ['bb'])
        TS('pool', t1[:], alr[:], -1.0, None, ALU.add, None, ['alr'], ['t1'])
        TT('pool', v4(t1), v4(t1), prmb(1), ALU.mult, ['t1', 'prm'], ['t1'])
        STT(km[:], t1[:], 1.0, uk[:], ALU.add, ALU.mult, ['t1', 'k'], ['km'])
        STT(AR[:, :, 0:64], kkn[:], -1.0, eE[:], ALU.mult, ALU.mult, ['kkn', 'eE'], ['AR'])
        TT('pool', AR[:, :, 64:128], ur[:], eI[:], ALU.mult, ['r', 'eI'], ['AR'])
        TT('dve', BT[:], bb[:], eN[:], ALU.mult, ['bb', 'eN'], ['BT'])
        TT('pool', KTt[:], km[:], eN[:], ALU.mult, ['km', 'eN'], ['KTt'])
        TT('dve', BhT[:], bb[:], eT[:], ALU.mult, ['bb', 'eT'], ['BhT'])
        TT('pool', KhT[:], km[:], eT[:], ALU.mult, ['km', 'eT'], ['KhT'])
        TT('pool', t1[:], ur[:], km[:], ALU.mult, ['r', 'km', 't1'], ['t1'])
        TT('pool', v4(t1), v4(t1), prmb(2), ALU.mult, ['t1', 'prm'], ['t1'])
        MM(bank[2][0:64, 256:512], ones64[:], t1[:].rearrange("p a t -> p (a t)"), ['ones64', 't1'], [('ps', 2)])
        TT('dve', bon[:], bank[2][0:64, 256:512].rearrange("p (a t) -> p a t", t=64), uv[:], ALU.mult, [('ps', 2), 'v'], ['bon'])
        Pop('pool', lambda e: e.tensor_copy(out=kk2[:, 2:4, :], in_=bon[:, 2:4, ::-1]), r=['bon', 'kk2'], w=['kk2'])
        Pdma('pool', Bd[0, :, cf * 64:cf * 64 + 64].rearrange("(h c) t -> c h t", h=2), bon[:, 0:2, :], r=['bon'], w=['Bdall'])
        Pdma('pool', Bd[1, :, cb * 64:cb * 64 + 64].rearrange("(h c) t -> c h t", h=2), kk2[:, 2:4, :], r=['kk2'], w=['Bdall'])
        for dh in range(4):
            MM(bank[3][0:64, dh * 64:(dh + 1) * 64], BhT[:, dh, :], I64b[:], ['BhT', 'I64b'], [('ps', 3)])
            MM(bank[3][0:64, 256 + dh * 64:256 + (dh + 1) * 64], KhT[:, dh, :], I64b[:], ['KhT', 'I64b'], [('ps', 3)])
        Pop('act', lambda e: e.copy(out=TM[:].rearrange("p a b t -> p (a b t)"), in_=bank[3][0:64, :]), r=[('ps', 3)], w=['TM'])
        for dh in range(4):
            MM(bank[2][0:64, dh * 64:(dh + 1) * 64], uv[:, dh, :], I64, ['v', 'cst'], [('ps', 2)])
        Pop('dve', lambda e: e.tensor_copy(out=Vt[:].rearrange("p a t -> p (a t)"), in_=bank[2][0:64, 0:256]), r=[('ps', 2)], w=['Vt'])
        marks.append(len(P.ops))
        for dh in range(4):
            MM(bank[4][0:64, dh * 128:(dh + 1) * 128], BT[:, dh, :], AR[:, dh, :], ['BT', 'AR'], [('ps', 4)])
            MM(bank[5][0:64, dh * 128:(dh + 1) * 128], KTt[:, dh, :], AR[:, dh, :], ['KTt', 'AR'], [('ps', 5)])
        mk4 = Mk.unsqueeze(1).broadcast_to([64, 4, 128])
        TT('dve', Gb[:], bank[4][0:64, :].rearrange("p (a x) -> p a x", x=128), mk4, ALU.mult, [('ps', 4), 'cst'], ['Gb'])
        TT('dve', Gk[:], bank[5][0:64, :].rearrange("p (a x) -> p a x", x=128), mk4, ALU.mult, [('ps', 5), 'cst'], ['Gk'])
        for dh in range(4):
            MM(bank[4][0:64, 256 + dh * 64:256 + (dh + 1) * 64], AR[:, dh, 0:64], BT[:, dh, :], ['AR', 'BT'], [('ps', 4)])
        TT('dve', Nn[:], bank[4][0:64, 256:512].rearrange("p (a x) -> p a x", x=64),
           MkN.unsqueeze(1).broadcast_to([64, 4, 64]), ALU.mult, [('ps', 4), 'cst'], ['Nn'])
        P0b = Qk[3]
        Qf = Qk[2]
        Pop('act', lambda e: e.copy(out=P0b[:], in_=Gb[:, :, 0:64]), r=['Gb'], w=['P0b'])
        TT('pool', Qk[0][:], Gb[:, :, 0:64], I64.unsqueeze(1).broadcast_to([64, 4, 64]), ALU.add, ['Gb', 'cst'], [('Q', 0)])
        pk_prev = (lambda dh: P0b[:, dh, :], lambda dh: Nn[:, dh, :], ['P0b', 'Nn'])
        qi = 0
        for lv in range(1, 6):
            if lv == 3:
                marks.append(len(P.ops))
            pb = lv % 2
            Pn = Pk[pb]
            pkey = ('Pk', pb)
            bkp, bkq = (5, 4) if lv <= 2 else (6, 6)
            for dh in range(4):
                if lv < 5:
                    MM(bank[bkp][0:64, dh * 64:(dh + 1) * 64], pk_prev[1](dh), pk_prev[0](dh), pk_prev[2], [('ps', bkp)])
                MM(bank[bkp][0:64, 256 + dh * 64:256 + (dh + 1) * 64], pk_prev[0](dh), pk_prev[1](dh), pk_prev[2], [('ps', bkp)])
            if lv % 2:
                Pop('act', lambda e, Pn=Pn, bkp=bkp: e.copy(out=Pn[:].rearrange("p a b t -> p (a b t)"), in_=bank[bkp][0:64, :]),
                    r=[('ps', bkp)], w=[pkey])
            else:
                Pop('dve', lambda e, Pn=Pn, bkp=bkp: e.tensor_copy(out=Pn[:].rearrange("p a b t -> p (a b t)"), in_=bank[bkp][0:64, :]),
                    r=[('ps', bkp)], w=[pkey])
            pk_prev = (lambda dh, Pn=Pn: Pn[:, 0, dh, :], lambda dh, Pn=Pn: Pn[:, 1, dh, :], [pkey])
            for dh in range(4):
                MM(bank[bkq][0:64, dh * 64:(dh + 1) * 64], Pn[:, 1, dh, :], Qk[qi][:, dh, :], [pkey, ('Q', qi)], [('ps', bkq)])
            if lv < 5:
                TT('dve', Qk[1 - qi][:], bank[bkq][0:64, 0:256].rearrange("p (a t) -> p a t", t=64), Qk[qi][:], ALU.add,
                   [('ps', bkq), ('Q', qi)], [('Q', 1 - qi)])
                qi = 1 - qi
            else:
                TT('dve', Qf[:], bank[bkq][0:64, 0:256].rearrange("p (a t) -> p a t", t=64), Qk[qi][:], ALU.add,
                   [('ps', bkq), ('Q', qi)], ['Qf'])
        TTm = Qf
        tkey = 'Qf'
        marks.append(len(P.ops))
        for dh in range(4):
            MM(bank[7][0:64, dh * 64:(dh + 1) * 64], AR[:, dh, 0:64], ST[:, dh, :], ['AR', 'ST'], [('ps', 7)], start=True, stop=False)
            MM(bank[7][0:64, dh * 64:(dh + 1) * 64], Gk[:, dh, 0:64], Vt[:, dh, :], ['Gk', 'Vt'], [('ps', 7)], start=False, stop=True)
        Pop('act', lambda e: e.copy(out=Xsb[:].rearrange("p a t -> p (a t)"), in_=bank[7][0:64, 0:256]), r=[('ps', 7)], w=['Xsb'])
        for dh in range(4):
            MM(bank[7][0:64, 256 + dh * 64:256 + (dh + 1) * 64], TTm[:, dh, :], Xsb[:, dh, :], [tkey, 'Xsb'], [('ps', 7)])
        Pop('act', lambda e: e.copy(out=Usb[:].rearrange("p a t -> p (a t)"), in_=bank[7][0:64, 256:512]), r=[('ps', 7)], w=['Usb'])
        for dh in range(4):
            o = bank[7][0:64, dh * 64:(dh + 1) * 64]
            MM(o, ST[:, dh, :], AR[:, dh, 64:128], ['ST', 'AR'], [('ps', 7)], start=True, stop=False)
            MM(o, Usb[:, dh, :], Gb[:, dh, 64:128], ['Usb', 'Gb'], [('ps', 7)], start=False, stop=False)
            MM(o, Vt[:, dh, :], Gk[:, dh, 64:128], ['Vt', 'Gk'], [('ps', 7)], start=False, stop=True)
        yv = bank[7][0:64, 0:256].rearrange("p (a t) -> p a t", t=64)
        Pop('act', lambda e, Yb=Yb, yv=yv: e.copy(out=Yb[:, 0:2, :], in_=yv[:, 0:2, :]), r=[('ps', 7)], w=['Ysb'])
        Pop('act', lambda e, Yb=Yb, yv=yv: e.copy(out=Yb[:, 2:4, ::-1], in_=yv[:, 2:4, :]), r=[('ps', 7)], w=['Ysb'])
        Pdma('pool', Yd[0, :, cf * 64:cf * 64 + 64].rearrange("(h c) t -> c h t", h=2), Yb[:, 0:2, :], r=['Ysb'], w=['Ydall'])
        Pdma('pool', Yd[1, :, cb * 64:cb * 64 + 64].rearrange("(h c) t -> c h t", h=2), Yb[:, 2:4, :], r=['Ysb'], w=['Ydall'])
        TT('pool', Stmp[:], ST[:], gC[:].unsqueeze(2).broadcast_to([64, 4, 64]), ALU.mult, ['ST', 'gC'], ['Stmp'])
        for dh in range(4):
            o = bank[7][0:64, 256 + dh * 64:256 + (dh + 1) * 64]
            MM(o, TM[:, 0, dh, :], Usb[:, dh, :], ['TM', 'Usb'], [('ps', 7)], start=True, stop=False)
            MM(o, TM[:, 1, dh, :], Vt[:, dh, :], ['TM', 'Vt'], [('ps', 7)], start=False, stop=True)
        TT('dve', ST[:], bank[7][0:64, 256:512].rearrange("p (a t) -> p a t", t=64), Stmp[:], ALU.add, [('ps', 7), 'Stmp'], ['ST'])
        ops = P.ops
        P.ops = saved
        cur['b'] = None
        bounds = [0] + marks + [len(ops)]
        return [ops[bounds[i]:bounds[i + 1]] for i in range(5)]

    def interleave(lists):
        items = []
        for li, lst in enumerate(lists):
            n_ = len(lst)
            for k_, o in enumerate(lst):
                items.append(((k_ + 0.5) / n_, li, k_, o))
        items.sort(key=lambda t: (t[0], t[1], t[2]))
        return [t[3] for t in items]
    gen = [gen_step(si) for si in range(NS)]
    for tau in range(-KSEG, NS):
        lists = []
        if 0 <= tau < NS:
            lists.append(gen[tau][KSEG])
        for j in range(1, KSEG + 1):
            s_ = tau + j
            if 0 <= s_ < NS:
                lists.append(gen[s_][KSEG - j])
        P.ops.extend(interleave(lists))
    P.fence()
    A.release(m0)
    prm2 = A.alloc("prm2", [64, 2, 5])
    on64 = A.alloc("on64", [64, 64])
    eps2 = A.alloc("eps2", [64, 1])
    P.dma('sp', prm2[:], prm, w=['prm2'])
    P.op('pool', lambda e: e.memset(on64[:], 1.0 / 64), w=['on64'])
    P.op('pool', lambda e: e.memset(eps2[:], GN_EPS), w=['eps2'])
    yb = [A.alloc("yb%d" % i, [64, 2, 2, 512]) for i in range(2)]
    bd = [A.alloc("bd%d" % i, [64, 2, 2, 512]) for i in range(2)]
    gg = [A.alloc("gg%d" % i, [64, 2, 512]) for i in range(2)]
    y = A.alloc("y", [64, 2, 512])
    yc = A.alloc("yc", [64, 2, 512])
    y2 = A.alloc("y2", [64, 2, 512])
    rs = A.alloc("rs", [64, 2, 512])
    jobs = []
    if ctx_out:
        jobs.append((0, CTXN, 0))
    o0 = CTXN if ctx_out else 0
    for b0 in range(0, L, 512):
        jobs.append((CTXN + b0, 512, o0 + b0))
    for ji, (t0, n, orow) in enumerate(jobs):
        b = ji % 2
        P.dma('sp', yb[b][:, :, :, :n], Yd[:, :, t0:t0 + n].rearrange("d (h c) t -> c d h t", h=2), r=['Ydall'], w=[('yb', b)])
        P.dma('sp', bd[b][:, :, :, :n], Bd[:, :, t0:t0 + n].rearrange("d (h c) t -> c d h t", h=2), r=['Bdall'], w=[('bd', b)])
        P.dma('sp', gg[b][:, :, :n], uT[5, :, t0:t0 + n].rearrange("(h c) t -> c h t", h=2), r=['uTall'], w=[('gg', b)])
        TT('dve', y[:, :, :n], yb[b][:, 0, :, :n], yb[b][:, 1, :, :n], ALU.add, [('yb', b)], ['y'])
        for h in range(2):
            MM(bank[h][0:64, :n], on64[:], y[:, h, :n], ['on64', 'y'], [('ps', h)])
            TT('dve', yc[:, h, :n], y[:, h, :n], bank[h][0:64, :n], ALU.subtract, ['y', ('ps', h)], ['yc'])
        TT('pool', y2[:, :, :n], yc[:, :, :n], yc[:, :, :n], ALU.mult, ['yc'], ['y2'])
        for h in range(2):
            MM(bank[2 + h][0:64, :n], on64[:], y2[:, h, :n], ['on64', 'y2'], [('ps', 2 + h)])
            ACT(rs[:, h, :n], bank[2 + h][0:64, :n], AF.Sqrt, [('ps', 2 + h), 'eps2'], ['rs'], bias=eps2[:, 0:1])
        P.op('dve', lambda e, n=n: e.reciprocal(out=rs[:, :, :n], in_=rs[:, :, :n]), r=['rs'], w=['rs'])
        TT('dve', yc[:, :, :n], yc[:, :, :n], rs[:, :, :n], ALU.mult, ['yc', 'rs'], ['yc'])
        for h in range(2):
            TS('pool', yc[:, h, :n], yc[:, h, :n], prm2[:, h, 3:4], prm2[:, h, 4:5], ALU.mult, ALU.add, ['yc', 'prm2'], ['yc'])
        TT('dve', yc[:, :, :n], yc[:, :, :n], bd[b][:, 0, :, :n], ALU.add, ['yc', ('bd', b)], ['yc'])
        TT('dve', yc[:, :, :n], yc[:, :, :n], bd[b][:, 1, :, :n], ALU.add, ['yc', ('bd', b)], ['yc'])
        TT('pool', y2[:, :, :n], yc[:, :, :n], gg[b][:, :, :n], ALU.mult, ['yc', ('gg', b), 'y2'], ['y2'])
        P.dma('pool', out_ap(orow, n).rearrange("(h c) t -> c h t", h=2), y2[:, :, :n], r=['y2'], w=['brb'], final=FINAL_OUT)
    P.fence()


NORM_EPS = 1e-6
CTXN = 256


def phase_A(nc, P, A, bank, pfx, L, ctx_out, lam_init, x_loader, brb, do_attn=True, do_pool=True, do_rwkv=True):
    T = CTXN + L
    NQ = T if ctx_out else L
    di = lambda name, shape: nc.dram_tensor(pfx + name, list(shape), F32, kind="ExternalInput").ap()
    if x_loader is None:
        xT = di("xT", [128, 8, T])

        def x_loader(dst, t0, sz, key):
            P.dma('sp', dst[:, :, :sz], xT[:, :, t0:t0 + sz], w=[key])
    identA = di("identA", [128, 128])
    cT = di("cT", [128, 8, 2])
    wada = di("wada", [128, 8, 2048])
    bada = di("bada", [128, 16])
    g1 = di("g1", [128, 8])
    win = di("win", [128, 8, 1280])
    qkg = di("qkg", [128, 2])
    ropeR = di("ropeR", [128, 2, L // 64])
    ropeC = di("ropeC", [128, 2, 64])
    perm = di("perm", [128, 128])
    lamqk = di("lamqk", [128, 256])
    subg = di("subg", [128, 128])
    wpool = di("wpool", [128, 128])
    pscale = di("pscale", [128, 1])
    selw = di("selw", [128, 4])
    efix = di("efix", [128, 16])
    pT = nc.dram_tensor(pfx + "pT_scr", [7, 128, T], F32, kind="Internal").ap()
    base_mark = A.mark()
    if True:
        ones = A.alloc("ones", [128, 128])
        blk = A.alloc("blk", [128, 128])
        eps_sb = A.alloc("eps", [128, 1])
        mod_sb = A.alloc("mod", [128, 16, 2])
        A_sb = A.alloc("A", [128, 8, 2])
        NKT = T // 128
        QT = A.alloc("QT", [128, T], BF16)
        KT = A.alloc("KT", [128, T], BF16)
        V = A.alloc("V", [128, NKT, 130], BF16)
        lam_sb = A.alloc("lam", [128, 1])
        subg_sb = A.alloc("subg", [128, 128])
        ident_sb = A.alloc("identA", [128, 128])
        P.dma('sp', ident_sb[:], identA, w=['identA'])
        P.op('pool', lambda e: e.memset(ones[:], 1.0), w=['ones'])
        P.op('pool', lambda e: e.memset(blk[:], 0.0), w=['blk'])
        P.op('pool', lambda e: e.memset(blk[0:64, 0:64], 1.0), w=['blk'])
        P.op('pool', lambda e: e.memset(blk[64:128, 64:128], 1.0), w=['blk'])
        P.op('pool', lambda e: e.memset(eps_sb[:], NORM_EPS), w=['eps'])
        P.op('pool', lambda e: e.memset(V[:, :, 128:130], 1.0), w=['Vones'])
        m_a1 = A.mark()
        c_sb = A.alloc("c", [128, 8, 2])
        s_sb = A.alloc("s", [128, 8, 2])
        bada_sb = A.alloc("bada", [128, 16])
        g1_sb = A.alloc("g1", [128, 8])
        qkg_sb = A.alloc("qkg", [128, 2])
        perm_sb = A.alloc("perm", [128, 128])
        ropeR_sb = A.alloc("ropeR", [128, 2, L // 64])
        ropeC_sb = A.alloc("ropeC", [128, 2, 64])
        lamqk_sb = A.alloc("lamqk", [128, 256])
        lamt = A.alloc("lamt", [128, 4])
        wbf = A.alloc("wbf", [128, 8, 1280], BF16)
        xt = [A.alloc("xt%d" % i, [128, 8, 512]) for i in range(2)]
        sq = A.alloc("sq", [128, 8, 512])
        rstd = A.alloc("rstd", [128, 512])
        xn = A.alloc("xn", [128, 8, 512], BF16)
        ob = [A.alloc("ob%d" % i, [128, 512]) for i in range(2)]
        qk32 = A.alloc("qk32", [128, 512])
        qksq = A.alloc("qksq", [128, 512])
        qkr = A.alloc("qkr", [128, 512])
        qkn = A.alloc("qkn", [128, 512])
        cs_t = A.alloc("cs_t", [128, 2, 512])
        rtmp = A.alloc("rtmp", [128, 2, 512])

        for nm, dst, src in [('c_sb', c_sb, cT), ('bada', bada_sb, bada), ('g1', g1_sb, g1), ('qkg', qkg_sb, qkg),
                             ('perm', perm_sb, perm), ('ropeR', ropeR_sb, ropeR), ('ropeC', ropeC_sb, ropeC),
                             ('lamqk', lamqk_sb, lamqk), ('subg', subg_sb, subg)]:
            P.dma('sp', dst[:], src, w=[nm])
        lq = lamqk_sb[:].rearrange("p (a b) -> p a b", b=64)
        P.op('dve', lambda e: e.tensor_tensor(out=sq[:, 0, 0:64], in0=lq[:, 0, :], in1=lq[:, 1, :], op=ALU.mult),
             r=['lamqk'], w=['sq'])
        P.op('dve', lambda e: e.tensor_tensor(out=sq[:, 0, 64:128], in0=lq[:, 2, :], in1=lq[:, 3, :], op=ALU.mult),
             r=['lamqk'], w=['sq'])
        P.op('dve', lambda e: e.tensor_reduce(out=lamt[:, 0:2], in_=sq[:, 0, 0:128].rearrange("p (a b) -> p a b", b=64),
                                              axis=AX.X, op=ALU.add), r=['sq'], w=['lamt'])
        P.op('act', lambda e: e.activation(out=lamt[:, 2:4], in_=lamt[:, 0:2], func=AF.Exp), r=['lamt'], w=['lamt2'])
        P.op('dve', lambda e: e.tensor_tensor(out=lam_sb[:], in0=lamt[:, 2:3], in1=lamt[:, 3:4], op=ALU.subtract),
             r=['lamt2'], w=['lam'])
        P.op('dve', lambda e: e.tensor_scalar(out=lam_sb[:], in0=lam_sb[:], scalar1=lam_init, scalar2=-1.0,
                                              op0=ALU.add, op1=ALU.mult), r=['lam'], w=['lam'])
        P.op('act', lambda e: e.activation(out=s_sb[:], in_=c_sb[:], func=AF.Silu), r=['c_sb'], w=['s_sb'])
        ps_mod = bank[0][:, 0:32]
        for piece in range(4):
            b = piece % 2
            P.dma('sp', xt[b][:], wada[:, :, piece * 512:(piece + 1) * 512], w=[('xt', b)])
            for occ in range(4):
                oc = piece * 4 + occ
                for k in range(8):
                    P.op('pe', lambda e, b=b, occ=occ, oc=oc, k=k: e.matmul(
                        ps_mod[:, oc * 2:oc * 2 + 2], lhsT=xt[b][:, k, occ * 128:(occ + 1) * 128],
                        rhs=s_sb[:, k, :], start=(k == 0), stop=(k == 7)),
                        r=[('xt', b), 's_sb'], w=[('ps', 0)])
        P.op('dve', lambda e: e.tensor_tensor(
            out=mod_sb[:], in0=ps_mod.rearrange("p (a b) -> p a b", b=2),
            in1=bada_sb[:].unsqueeze(2).broadcast_to([128, 16, 2]), op=ALU.add),
            r=[('ps', 0), 'bada'], w=['mod'])
        P.op('dve', lambda e: e.tensor_scalar(out=A_sb[:], in0=mod_sb[:, 8:16, :], scalar1=1.0, scalar2=None,
                                              op0=ALU.add), r=['mod'], w=['A'])
        P.op('dve', lambda e: e.tensor_tensor(out=A_sb[:], in0=A_sb[:],
                                              in1=g1_sb[:].unsqueeze(2).broadcast_to([128, 8, 2]), op=ALU.mult),
             r=['A', 'g1'], w=['A'])
        for piece, (c0, csz) in enumerate([(0, 512), (512, 512), (1024, 256)]):
            b = piece % 2
            P.dma('sp', xt[b][:, :, :csz], win[:, :, c0:c0 + csz], w=[('xt', b)])
            P.op('pool', lambda e, b=b, c0=c0, csz=csz: e.tensor_copy(out=wbf[:, :, c0:c0 + csz], in_=xt[b][:, :, :csz]),
                 r=[('xt', b)], w=['wbf'])
        tiles = [(0, CTXN, 1)] + [(CTXN + i * 512, 512, 0) for i in range(L // 512)]
        SCR = {0: 0, 4: 1, 5: 2, 6: 3, 7: 4, 8: 5, 9: 6}
        oi = 0
        for ti, (t0, sz, j) in enumerate(tiles):
            b = ti % 2
            x_loader(xt[b], t0, sz, ('xt', b))
            P.op('act', lambda e, b=b, sz=sz: e.activation(out=sq[:, :, :sz], in_=xt[b][:, :, :sz], func=AF.Square),
                 r=[('xt', b)], w=['sq'])
            for k in range(8):
                P.op('pe', lambda e, k=k, sz=sz: e.matmul(bank[0][:, :sz], lhsT=ones[:], rhs=sq[:, k, :sz],
                                                          start=(k == 0), stop=(k == 7)),
                     r=['ones', 'sq'], w=[('ps', 0)])
            P.op('act', lambda e, sz=sz: e.activation(out=rstd[:, :sz], in_=bank[0][:, :sz], func=AF.Sqrt,
                                                      scale=1.0 / 1024, bias=eps_sb[:, 0:1]),
                 r=[('ps', 0), 'eps'], w=['rstd'])
            P.op('dve', lambda e, sz=sz: e.reciprocal(out=rstd[:, :sz], in_=rstd[:, :sz]), r=['rstd'], w=['rstd'])
            P.op('dve', lambda e, b=b, sz=sz: e.tensor_tensor(
                out=sq[:, :, :sz], in0=xt[b][:, :, :sz],
                in1=rstd[:, :sz].unsqueeze(1).broadcast_to([128, 8, sz]), op=ALU.mult),
                r=[('xt', b), 'rstd', 'sq'], w=['sq'])
            for k in range(8):
                if k % 2:
                    P.op('dve', lambda e, k=k, sz=sz, j=j: e.tensor_scalar(
                        out=xn[:, k, :sz], in0=sq[:, k, :sz], scalar1=A_sb[:, k, j:j + 1],
                        scalar2=mod_sb[:, k, j:j + 1], op0=ALU.mult, op1=ALU.add),
                        r=['sq', 'A', 'mod'], w=['xn'])
                else:
                    P.op('act', lambda e, k=k, sz=sz, j=j: e.activation(
                        out=xn[:, k, :sz], in_=sq[:, k, :sz], func=AF.Identity, scale=A_sb[:, k, j:j + 1],
                        bias=mod_sb[:, k, j:j + 1]), r=['sq', 'A', 'mod'], w=['xn'])
            if j == 0:
                r0 = (t0 - CTXN) // 64
                for cs in range(2):
                    P.op('pool', lambda e, cs=cs, r0=r0: e.tensor_tensor(
                        out=cs_t[:, cs, :].rearrange("p (r c) -> p r c", c=64),
                        in0=ropeR_sb[:, cs, r0:r0 + 8].unsqueeze(2).broadcast_to([128, 8, 64]),
                        in1=ropeC_sb[:, cs, :].unsqueeze(1).broadcast_to([128, 8, 64]), op=ALU.mult),
                        r=['ropeR', 'ropeC'], w=['cs_t'])
            for c in range(10):
                if c == 3:
                    for s in range(sz // 128):
                        kt = t0 // 128 + s
                        for k in range(8):
                            P.op('pe', lambda e, k=k, s=s: e.matmul(
                                bank[3][:, 0:128], lhsT=xn[:, k, s * 128:(s + 1) * 128], rhs=wbf[:, k, 384:512],
                                start=(k == 0), stop=(k == 7)), r=['xn', 'wbf'], w=[('ps', 3)])
                        P.op('act', lambda e, kt=kt: e.copy(out=V[:, kt, 0:128], in_=bank[3][:, 0:128]),
                             r=[('ps', 3)], w=['V'])
                    continue
                pb = 1 + (oi % 2)
                oi += 1
                for k in range(8):
                    P.op('pe', lambda e, c=c, k=k, sz=sz, pb=pb: e.matmul(
                        bank[pb][:, :sz], lhsT=wbf[:, k, c * 128:(c + 1) * 128], rhs=xn[:, k, :sz],
                        start=(k == 0), stop=(k == 7)), r=['wbf', 'xn'], w=[('ps', pb)])
                if c in SCR:
                    o = ob[oi % 2]
                    ok = ('ob', oi % 2)
                    if oi % 2:
                        P.op('act', lambda e, o=o, pb=pb, sz=sz: e.copy(out=o[:, :sz], in_=bank[pb][:, :sz]),
                             r=[('ps', pb)], w=[ok])
                    else:
                        P.op('dve', lambda e, o=o, pb=pb, sz=sz: e.tensor_copy(out=o[:, :sz], in_=bank[pb][:, :sz]),
                             r=[('ps', pb)], w=[ok])
                    P.dma('pool', pT[SCR[c], :, t0:t0 + sz], o[:, :sz], r=[ok], w=[('pT', SCR[c], ti)])
                else:
                    dst = QT if c == 1 else KT
                    gi = c - 1
                    P.op('act', lambda e, pb=pb, sz=sz: e.copy(out=qk32[:, :sz], in_=bank[pb][:, :sz]),
                         r=[('ps', pb)], w=['qk32'])
                    P.op('act', lambda e, sz=sz: e.activation(out=qksq[:, :sz], in_=qk32[:, :sz], func=AF.Square),
                         r=['qk32'], w=['qksq'])
                    P.op('pe', lambda e, sz=sz: e.matmul(bank[4][:, :sz], lhsT=blk[:], rhs=qksq[:, :sz],
                                                         start=True, stop=True), r=['blk', 'qksq'], w=[('ps', 4)])
                    P.op('act', lambda e, sz=sz: e.activation(out=qkr[:, :sz], in_=bank[4][:, :sz], func=AF.Sqrt,
                                                              scale=1.0 / 64, bias=eps_sb[:, 0:1]),
                         r=[('ps', 4), 'eps'], w=['qkr'])
                    P.op('dve', lambda e, sz=sz: e.reciprocal(out=qkr[:, :sz], in_=qkr[:, :sz]), r=['qkr'], w=['qkr'])
                    if j == 1:
                        P.op('dve', lambda e, sz=sz, gi=gi, dst=dst, t0=t0: e.scalar_tensor_tensor(
                            out=dst[:, t0:t0 + sz], in0=qk32[:, :sz], scalar=qkg_sb[:, gi:gi + 1], in1=qkr[:, :sz],
                            op0=ALU.mult, op1=ALU.mult), r=['qk32', 'qkg', 'qkr'], w=['QK'])
                    else:
                        P.op('dve', lambda e, sz=sz, gi=gi: e.scalar_tensor_tensor(
                            out=qkn[:, :sz], in0=qk32[:, :sz], scalar=qkg_sb[:, gi:gi + 1], in1=qkr[:, :sz],
                            op0=ALU.mult, op1=ALU.mult), r=['qk32', 'qkg', 'qkr'], w=['qkn'])
                        P.op('pe', lambda e, sz=sz: e.matmul(bank[5][:, :sz], lhsT=perm_sb[:], rhs=qkn[:, :sz],
                                                             start=True, stop=True), r=['perm', 'qkn'], w=[('ps', 5)])
                        P.op('pool', lambda e, sz=sz: e.tensor_tensor(out=rtmp[:, 0, :sz], in0=qkn[:, :sz],
                                                                      in1=cs_t[:, 0, :sz], op=ALU.mult),
                             r=['qkn', 'cs_t'], w=['rtmp0'])
                        P.op('dve', lambda e, sz=sz: e.tensor_tensor(out=rtmp[:, 1, :sz], in0=bank[5][:, :sz],
                                                                     in1=cs_t[:, 1, :sz], op=ALU.mult),
                             r=[('ps', 5), 'cs_t'], w=['rtmp1'])
                        P.op('dve', lambda e, sz=sz, dst=dst, t0=t0: e.tensor_tensor(
                            out=dst[:, t0:t0 + sz], in0=rtmp[:, 0, :sz], in1=rtmp[:, 1, :sz], op=ALU.add),
                            r=['rtmp0', 'rtmp1'], w=['QK'])
        P.fence()
        A.release(m_a1)
        m_ph = A.mark()
        if do_pool:
            wpool_f = A.alloc("wpool_f", [128, 128])
            wpool_b = A.alloc("wpool_b", [128, 128], BF16)
            pscale_sb = A.alloc("pscale", [128, 1])
            selw_sb = A.alloc("selw", [128, 4])
            efix_sb = A.alloc("efix", [128, 16])
            NB = 2048
            U = A.alloc("U", [128, NB + 32])
            W = [A.alloc("W%d" % i, [128, NB + 32]) for i in range(2)]
            comb = A.alloc("comb", [128, NB])
            diffb = A.alloc("diffb", [128, NB], BF16)
            pob = [A.alloc("pob%d" % i, [128, 512]) for i in range(2)]
            P.dma('sp', wpool_f[:], wpool, w=['wpool_f'])
            P.dma('sp', pscale_sb[:], pscale, w=['pscale'])
            P.dma('sp', selw_sb[:], selw, w=['selw'])
            P.dma('sp', efix_sb[:], efix, w=['efix'])
            P.op('dve', lambda e: e.tensor_copy(out=wpool_b[:], in_=wpool_f[:]), r=['wpool_f'], w=['wpool_b'])
            seqs = [(CTXN, L, (0 if not ctx_out else CTXN))]
            if ctx_out:
                seqs.append((0, CTXN, 0))
            pi = 0
            for (s0, slen, o0) in seqs:
                for b0 in range(0, slen, NB):
                    n = min(NB, slen - b0)
                    lo = max(0, b0 - 16)
                    hi = min(slen, b0 + n + 16)
                    P.op('dve', lambda e: e.memset(U[:], 0.0), w=['U'])
                    P.dma('sp', U[:, 16 + lo - b0:16 + hi - b0], pT[0, :, s0 + lo:s0 + hi], r=[('pT', 0, t) for t in range(len(tiles))], w=['U'])
                    NP = n + 32
                    src = U
                    for lv, sh in enumerate([1, 2, 4, 8]):
                        dstw = W[lv % 2]
                        P.op('dve', lambda e, src=src, dstw=dstw, sh=sh, NP=NP: e.tensor_tensor(
                            out=dstw[:, sh:NP], in0=src[:, sh:NP], in1=src[:, 0:NP - sh], op=ALU.add),
                            r=['U', ('W', 0), ('W', 1)], w=[('W', lv % 2)])
                        w_ = 2 * sh
                        off = 16 + w_ // 2 - 1
                        if lv == 0:
                            P.op('dve', lambda e, dstw=dstw, off=off, n=n, lv=lv: e.tensor_scalar(
                                out=comb[:, :n], in0=dstw[:, off:off + n], scalar1=selw_sb[:, lv:lv + 1], scalar2=None,
                                op0=ALU.mult), r=[('W', lv % 2), 'selw'], w=['comb'])
                        else:
                            P.op('dve', lambda e, dstw=dstw, off=off, n=n, lv=lv: e.scalar_tensor_tensor(
                                out=comb[:, :n], in0=dstw[:, off:off + n], scalar=selw_sb[:, lv:lv + 1], in1=comb[:, :n],
                                op0=ALU.mult, op1=ALU.add), r=[('W', lv % 2), 'selw', 'comb'], w=['comb'])
                        src = dstw
                    if b0 == 0:
                        P.op('pool', lambda e: e.tensor_tensor(out=comb[:, 0:8], in0=comb[:, 0:8], in1=efix_sb[:, 0:8],
                                                               op=ALU.mult), r=['comb', 'efix'], w=['comb'])
                    if b0 + n == slen:
                        P.op('pool', lambda e, n=n: e.tensor_tensor(out=comb[:, n - 8:n], in0=comb[:, n - 8:n],
                                                                    in1=efix_sb[:, 8:16], op=ALU.mult),
                             r=['comb', 'efix'], w=['comb'])
                    P.op('dve', lambda e, n=n: e.tensor_tensor(out=diffb[:, :n], in0=comb[:, :n], in1=U[:, 16:16 + n],
                                                               op=ALU.subtract), r=['comb', 'U'], w=['diffb'])
                    for c0 in range(0, n, 512):
                        cs = min(512, n - c0)
                        pb = 6 + pi % 2
                        o = pob[pi % 2]
                        ok = ('pob', pi % 2)
                        pi += 1
                        P.op('pe', lambda e, c0=c0, cs=cs, pb=pb: e.matmul(bank[pb][:, :cs], lhsT=wpool_b[:],
                                                                            rhs=diffb[:, c0:c0 + cs], start=True, stop=True),
                             r=['wpool_b', 'diffb'], w=[('ps', pb)])
                        P.op('act', lambda e, o=o, pb=pb, cs=cs: e.activation(out=o[:, :cs], in_=bank[pb][:, :cs],
                                                                               func=AF.Copy, scale=pscale_sb[:, 0:1]),
                             r=[('ps', pb), 'pscale'], w=[ok])
                        P.dma('pool', brb.dst(0, o0 + b0 + c0, cs), o[:, :cs], r=[ok], w=['brb'])
            P.fence()
            A.release(m_ph)
        if do_attn:
            PT = [[A.alloc("PT%d_%d" % (m, i), [128, 512], BF16) for i in range(2)] for m in range(2)]
            osb = A.alloc("osb", [128, 2, 4, 130])
            Qz = [A.alloc("Qz%d" % i, [128, 2, 512], BF16) for i in range(2)]
            for i in range(2):
                P.op('dve', lambda e, i=i: e.memset(Qz[i][:], 0.0), w=[('Qz', i)])
            rec = A.alloc("rec", [128, 2, 4])
            o1 = A.alloc("o1", [128, 4, 128])
            o2 = A.alloc("o2", [128, 4, 128])
            ssq = A.alloc("ssq", [128, 4])
            oT = A.alloc("oT", [128, 512])
            def oacc(m, qs):
                i = m * 4 + qs
                return bank[4 + i // 3][:, (i % 3) * 130:(i % 3) * 130 + 130], ('ps', 4 + i // 3)
            qjobs = []
            if ctx_out:
                qjobs.append((0, CTXN, (0, 2), 0))
            for i in range(L // 512):
                qjobs.append((CTXN + i * 512, 512, (0, NKT), (CTXN if ctx_out else 0) + i * 512))
            si = 0
            for qji, (q0, nq, (k0, k1), orow) in enumerate(qjobs):
                nqs = nq // 128
                started = set()
                qz = Qz[qji % 2]
                qzk = ('Qz', qji % 2)
                P.op('dve', lambda e, qz=qz, q0=q0, nq=nq: e.tensor_copy(out=qz[0:64, 0, :nq], in_=QT[0:64, q0:q0 + nq]),
                     r=['QK', qzk], w=[qzk])
                P.op('pool', lambda e, qz=qz, q0=q0, nq=nq: e.tensor_copy(out=qz[64:128, 1, :nq], in_=QT[64:128, q0:q0 + nq]),
                     r=['QK', qzk], w=[qzk])
                def emit_S(kt):
                    for m in range(2):
                        sb_ = kt % 2
                        pb = m * 2 + sb_
                        P.op('pe', lambda e, m=m, kt=kt, nq=nq, pb=pb, qz=qz: e.matmul(
                            bank[pb][:, :nq], lhsT=KT[:, kt * 128:(kt + 1) * 128],
                            rhs=qz[:, m, :nq], start=True, stop=True),
                            r=['QK', qzk], w=[('ps', pb)])
                        P.op('act', lambda e, m=m, sb_=sb_, pb=pb, nq=nq: e.activation(
                            out=PT[m][sb_][:, :nq], in_=bank[pb][:, :nq], func=AF.Exp, scale=0.125),
                            r=[('ps', pb)], w=[('PT', m, sb_)])

                def emit_PV(kt):
                    for m in range(2):
                        sb_ = kt % 2
                        for qs in range(nqs):
                            oap, okey = oacc(m, qs)
                            first_in_bank = (kt == k0) and (okey not in started)
                            started.add(okey)
                            P.op('pe', lambda e, m=m, sb_=sb_, qs=qs, kt=kt, oap=oap, fib=first_in_bank, k1=k1: e.matmul(
                                oap, lhsT=PT[m][sb_][:, qs * 128:(qs + 1) * 128], rhs=V[:, kt, :],
                                start=fib, stop=(kt == k1 - 1)),
                                r=[('PT', m, sb_), 'V', 'Vones'], w=[okey])
                emit_S(k0)
                for kt in range(k0, k1):
                    if kt + 1 < k1:
                        emit_S(kt + 1)
                    emit_PV(kt)
                for m in range(2):
                    for qs in range(nqs):
                        oap, okey = oacc(m, qs)
                        P.op('dve' if (m + qs) % 2 else 'act',
                             (lambda e, m=m, qs=qs, oap=oap: e.tensor_copy(out=osb[:, m, qs, :], in_=oap)) if (m + qs) % 2
                             else (lambda e, m=m, qs=qs, oap=oap: e.copy(out=osb[:, m, qs, :], in_=oap)),
                             r=[okey], w=['osb'])
                P.op('dve', lambda e, nqs=nqs: e.reciprocal(out=rec[:, :, :nqs], in_=osb[:, :, :nqs, 128]),
                     r=['osb'], w=['rec'])
                P.op('dve', lambda e, nqs=nqs: e.tensor_scalar(out=rec[:, 1, :nqs], in0=rec[:, 1, :nqs],
                                                               scalar1=lam_sb[:, 0:1], scalar2=None, op0=ALU.mult),
                     r=['rec', 'lam'], w=['rec'])
                P.op('dve', lambda e, nqs=nqs: e.tensor_tensor(
                    out=o1[:, :nqs, :], in0=osb[:, 0, :nqs, 0:128],
                    in1=rec[:, 0, :nqs].unsqueeze(2).broadcast_to([128, nqs, 128]), op=ALU.mult),
                    r=['osb', 'rec'], w=['o1'])
                P.op('pool', lambda e, nqs=nqs: e.tensor_tensor(
                    out=o2[:, :nqs, :], in0=osb[:, 1, :nqs, 0:128],
                    in1=rec[:, 1, :nqs].unsqueeze(2).broadcast_to([128, nqs, 128]), op=ALU.mult),
                    r=['osb', 'rec'], w=['o2'])
                P.op('dve', lambda e, nqs=nqs: e.tensor_tensor(out=o1[:, :nqs, :], in0=o1[:, :nqs, :], in1=o2[:, :nqs, :],
                                                               op=ALU.add), r=['o1', 'o2'], w=['o1'])
                P.op('pool', lambda e, nqs=nqs: e.tensor_tensor(out=o2[:, :nqs, :], in0=o1[:, :nqs, :], in1=o1[:, :nqs, :],
                                                                op=ALU.mult), r=['o1', 'o2'], w=['o2'])
                P.op('dve', lambda e, nqs=nqs: e.tensor_reduce(out=ssq[:, :nqs], in_=o2[:, :nqs, :], axis=AX.X, op=ALU.add),
                     r=['o2'], w=['ssq'])
                P.op('act', lambda e, nqs=nqs: e.activation(out=ssq[:, :nqs], in_=ssq[:, :nqs], func=AF.Sqrt,
                                                            scale=1.0 / 128, bias=eps_sb[:, 0:1]),
                     r=['ssq', 'eps'], w=['ssq'])
                P.op('dve', lambda e, nqs=nqs: e.reciprocal(out=ssq[:, :nqs], in_=ssq[:, :nqs]), r=['ssq'], w=['ssq'])
                P.op('dve', lambda e, nqs=nqs: e.tensor_tensor(
                    out=o1[:, :nqs, :], in0=o1[:, :nqs, :],
                    in1=ssq[:, :nqs].unsqueeze(2).broadcast_to([128, nqs, 128]), op=ALU.mult),
                    r=['o1', 'ssq'], w=['o1'])
                P.op('pool', lambda e, nqs=nqs: e.tensor_tensor(
                    out=o2[:, :nqs, :], in0=o1[:, :nqs, :],
                    in1=subg_sb[:].unsqueeze(1).broadcast_to([128, nqs, 128]), op=ALU.mult),
                    r=['o1', 'subg', 'o2'], w=['o2'])
                for qs in range(nqs):
                    P.op('pe', lambda e, qs=qs: e.matmul(bank[7][:, qs * 128:(qs + 1) * 128], lhsT=o2[:, qs, :], rhs=ident_sb[:],
                                                         start=True, stop=True), r=['o2', 'identA'], w=[('ps', 7)])
                P.op('act', lambda e, nq=nq: e.copy(out=oT[:, :nq], in_=bank[7][:, :nq]), r=[('ps', 7)], w=['oT'])
                P.dma('sp', brb.dst(1, orow, nq), oT[:, :nq], r=['oT'], w=['brb'])
            P.fence()
        if do_rwkv:
            A.release(base_mark)
            rwkv_phase(nc, P, A, bank, pT, L, ctx_out, (lambda c0, n: brb.dst(2, c0, n)), pfx)
        A.release(base_mark)


NORM_EPS = 1e-6


def phase_B(nc, P, A, bank, pfx, NT_LAT, NT_CTX, x_load, gath, off_lat, x_store, NEXP=32):
    NT = NT_LAT + NT_CTX
    di = lambda name, shape: nc.dram_tensor(pfx + name, list(shape), F32, kind="ExternalInput").ap()
    selq = di("selq", [128, 4])
    cT = di("cT", [128, 8, 2])
    wada = di("wada", [128, 8, 6144])
    bada = di("bada", [128, 48])
    g12 = di("g12", [128, 2, 8])
    wgate = di("wgate", [128, 8, 3072])
    wbr = di("wbr", [128, 12, 1024])
    wout = di("wout", [128, 8, 1024])
    wrt = di("wrt", [128, 8, 36])
    wg = di("wg", [NEXP, 128, 8, 512])
    wu = di("wu", [NEXP, 128, 8, 512])
    wd = di("wd", [NEXP, 128, 4, 1024])
    selE = di("selE", [32, 32 * 128])
    ident = di("ident", [128, 128])
    x2s = nc.dram_tensor(pfx + "x2_scr", [128, 8, NT], F32, kind="Internal").ap()
    xn2s = nc.dram_tensor(pfx + "xn2_scr", [128, 8, NT], BF16, kind="Internal").ap()
    gTs = nc.dram_tensor(pfx + "gT_scr", [32, NT], F32, kind="Internal").ap()
    base_mark = A.mark()

    def TT(eng, out, a, b, op, r, w):
        P.op(eng, lambda e: e.tensor_tensor(out=out, in0=a, in1=b, op=op), r=r, w=w)

    def TS(eng, out, a, s1, s2, op0, op1, r, w):
        if op1 is None:
            P.op(eng, lambda e: e.tensor_scalar(out=out, in0=a, scalar1=s1, scalar2=None, op0=op0), r=r, w=w)
        else:
            P.op(eng, lambda e: e.tensor_scalar(out=out, in0=a, scalar1=s1, scalar2=s2, op0=op0, op1=op1), r=r, w=w)

    def STT(out, a, s, b, op0, op1, r, w):
        P.op('dve', lambda e: e.scalar_tensor_tensor(out=out, in0=a, scalar=s, in1=b, op0=op0, op1=op1), r=r, w=w)

    def ACT(out, a, func, r, w, scale=1.0, bias=None):
        if bias is None:
            P.op('act', lambda e: e.activation(out=out, in_=a, func=func, scale=scale), r=r, w=w)
        else:
            P.op('act', lambda e: e.activation(out=out, in_=a, func=func, scale=scale, bias=bias), r=r, w=w)

    def MM(out, lhsT, rhs, r, w, start=True, stop=True):
        P.op('pe', lambda e: e.matmul(out, lhsT=lhsT, rhs=rhs, start=start, stop=stop), r=r, w=w)

    def RED(out, a, op, r, w):
        P.op('dve', lambda e: e.tensor_reduce(out=out, in_=a, axis=AX.X, op=op), r=r, w=w)

    if True:
        ones = A.alloc("ones", [128, 128])
        selq_sb = A.alloc("selq", [128, 4])
        P.dma('sp', selq_sb[:], selq, w=['selq'])
        eps_sb = A.alloc("eps", [128, 1])
        mod_sb = A.alloc("mod", [128, 48, 2])
        A1 = A.alloc("A1", [128, 8, 2])
        A2 = A.alloc("A2", [128, 8, 2])
        g12_sb = A.alloc("g12", [128, 2, 8])
        ident_sb = A.alloc("ident", [128, 128])
        wrt_sb = A.alloc("wrt", [128, 8, 36])
        P.op('pool', lambda e: e.memset(ones[:], 1.0), w=['ones'])
        P.op('pool', lambda e: e.memset(eps_sb[:], NORM_EPS), w=['eps'])
        P.dma('sp', g12_sb[:], g12, w=['g12'])
        P.dma('sp', ident_sb[:], ident, w=['ident'])
        P.dma('sp', wrt_sb[:], wrt, w=['wrt'])
        mB1 = A.mark()
        c_sb = A.alloc("c", [128, 8, 2])
        s_sb = A.alloc("s", [128, 8, 2])
        bada_sb = A.alloc("bada", [128, 48])
        wgate_b = A.alloc("wgate_b", [128, 8, 3072], BF16)
        wbr_b = A.alloc("wbr_b", [128, 12, 1024], BF16)
        wout_b = A.alloc("wout_b", [128, 8, 1024], BF16)
        xt = [A.alloc("xt%d" % i, [128, 8, 512]) for i in range(2)]
        sq = A.alloc("sq", [128, 8, 512])
        rstd = A.alloc("rstd", [128, 512])
        xn1 = A.alloc("xn1", [128, 8, 512], BF16)
        brf = A.alloc("brf", [128, 4, 512])
        gTt = A.alloc("gTt", [32, 512])
        brb = A.alloc("brb", [128, 12, 512], BF16)
        sig = [A.alloc("sig%d" % i, [128, 512]) for i in range(2)]
        mrg = A.alloc("mrg", [128, 2, 512])
        mk_ = A.mark()
        mrgb = A.alloc("mrgb", [128, 8, 512], BF16)
        A.release(mk_)
        brsel = A.alloc("brsel", [128, 4, 512])
        x2 = A.alloc("x2", [128, 8, 512])
        rt = A.alloc("rt", [128, 80])
        g32 = A.alloc("g32", [128, 32])
        P.dma('sp', c_sb[:], cT, w=['c_sb'])
        P.dma('sp', bada_sb[:], bada, w=['bada'])
        ACT(s_sb[:], c_sb[:], AF.Silu, ['c_sb'], ['s_sb'])
        for piece in range(12):
            b = piece % 2
            P.dma('sp', xt[b][:], wada[:, :, piece * 512:(piece + 1) * 512], w=[('xt', b)])
            for occ in range(4):
                oc = piece * 4 + occ
                for k in range(8):
                    MM(bank[0][:, oc * 2:oc * 2 + 2], xt[b][:, k, occ * 128:(occ + 1) * 128], s_sb[:, k, :],
                       [('xt', b), 's_sb'], [('ps', 0)], start=(k == 0), stop=(k == 7))
        TT('dve', mod_sb[:], bank[0][:, 0:96].rearrange("p (a b) -> p a b", b=2),
           bada_sb[:].unsqueeze(2).broadcast_to([128, 48, 2]), ALU.add, [('ps', 0), 'bada'], ['mod'])
        for (Ax, m_scale, gi) in ((A1, 1, 0), (A2, 4, 1)):
            TS('dve', Ax[:], mod_sb[:, m_scale * 8:(m_scale + 1) * 8, :], 1.0, None, ALU.add, None, ['mod'], ['Ax%d' % gi])
            TT('dve', Ax[:], Ax[:], g12_sb[:, gi, :].unsqueeze(2).broadcast_to([128, 8, 2]), ALU.mult, ['Ax%d' % gi, 'g12'], ['Ax%d' % gi])
        wi = 0
        for (src, dstw, nk, ncol, key) in ((wgate, wgate_b, 8, 3072, 'wgate_b'), (wbr, wbr_b, 12, 1024, 'wbr_b'), (wout, wout_b, 8, 1024, 'wout_b')):
            for c0 in range(0, ncol, 512):
                for kb in range(0, nk, 8):
                    kn = min(8, nk - kb)
                    b = wi % 2
                    wi += 1
                    P.dma('sp', xt[b][:, :kn, :], src[:, kb:kb + kn, c0:c0 + 512], w=[('xt', b)])
                    P.op('pool' if wi % 2 else 'dve', lambda e, b=b, kn=kn, kb=kb, c0=c0, dstw=dstw: e.tensor_copy(
                        out=dstw[:, kb:kb + kn, c0:c0 + 512], in_=xt[b][:, :kn, :]), r=[('xt', b)], w=[key])
        tiles = [(i * 512, 512, 0) for i in range(NT_LAT // 512)]
        if NT_CTX:
            tiles.append((NT_LAT, NT_CTX, 1))

        def norm_mod(src, sz, j, Ax, m_shift, out_fn, okeys, srckeys):
            P.op('act', lambda e: e.activation(out=sq[:, :, :sz], in_=src[:, :, :sz], func=AF.Square), r=srckeys, w=['sq'])
            for k in range(8):
                MM(bank[0][:, :sz], ones[:], sq[:, k, :sz], ['ones', 'sq'], [('ps', 0)], start=(k == 0), stop=(k == 7))
            ACT(rstd[:, :sz], bank[0][:, :sz], AF.Sqrt, [('ps', 0), 'eps'], ['rstd'], scale=1.0 / 1024, bias=eps_sb[:, 0:1])
            P.op('dve', lambda e: e.reciprocal(out=rstd[:, :sz], in_=rstd[:, :sz]), r=['rstd'], w=['rstd'])
            TT('dve', sq[:, :, :sz], src[:, :, :sz], rstd[:, :sz].unsqueeze(1).broadcast_to([128, 8, sz]), ALU.mult,
               srckeys + ['rstd', 'sq'], ['sq'])
            for k in range(8):
                if k % 2:
                    TS('dve', out_fn(k), sq[:, k, :sz], Ax[:, k, j:j + 1],
                       mod_sb[:, m_shift * 8 + k, j:j + 1], ALU.mult, ALU.add, ['sq', 'Ax0', 'Ax1', 'mod'], okeys)
                else:
                    ACT(out_fn(k), sq[:, k, :sz], AF.Identity, ['sq', 'Ax0', 'Ax1', 'mod'], okeys, scale=Ax[:, k, j:j + 1],
                        bias=mod_sb[:, m_shift * 8 + k, j:j + 1])

        pi = 0
        for ti, (t0, sz, j) in enumerate(tiles):
            b = ti % 2
            x_load(xt[b], t0, sz, ('xt', b))
            for n3 in range(3):
                for q in range(4):
                    col0 = (off_lat + q * NT_LAT + t0) if j == 0 else (q * 64 + (t0 - NT_LAT))
                    P.dma('sp', brf[:, :, :sz], gath.gsrc(n3, col0, sz), r=['gath'], w=['brf'])
                    if q == 0:
                        TS('dve', brsel[:, :, :sz], brf[:, :, :sz], selq_sb[:, 0:1], None, ALU.mult, None, ['brf', 'selq'], ['mrgb'])
                    else:
                        STT(brsel[:, :, :sz], brf[:, :, :sz], selq_sb[:, q:q + 1], brsel[:, :, :sz], ALU.mult, ALU.add,
                            ['brf', 'selq', 'mrgb'], ['mrgb'])
                P.op('pool', lambda e, sz=sz, n3=n3: e.tensor_copy(out=brb[:, n3 * 4:(n3 + 1) * 4, :sz], in_=brsel[:, :, :sz]),
                     r=['mrgb'], w=['brb'])
            norm_mod(xt[b], sz, j, A1, 0, lambda k, sz=sz: xn1[:, k, :sz], ['xn1'], [('xt', b)])
            for dc in range(8):
                for n in range(3):
                    pg = bank[1 + pi % 2]
                    pgk = ('ps', 1 + pi % 2)
                    pbk = bank[3 + pi % 2]
                    pbkk = ('ps', 3 + pi % 2)
                    sg_ = sig[pi % 2]
                    sgk = ('sig', pi % 2)
                    pi += 1
                    for k in range(8):
                        MM(pg[:, :sz], wgate_b[:, k, n * 1024 + dc * 128:n * 1024 + (dc + 1) * 128], xn1[:, k, :sz],
                           ['wgate_b', 'xn1'], [pgk], start=(k == 0), stop=(k == 7))
                    for kc in range(4):
                        MM(pbk[:, :sz], wbr_b[:, n * 4 + kc, dc * 128:(dc + 1) * 128], brb[:, n * 4 + kc, :sz],
                           ['wbr_b', 'brb'], [pbkk], start=(kc == 0), stop=(kc == 3))
                    ACT(sg_[:, :sz], pg[:, :sz], AF.Sigmoid, [pgk], [sgk])
                    if n == 0:
                        TT('dve', mrg[:, dc % 2, :sz], sg_[:, :sz], pbk[:, :sz], ALU.mult, [sgk, pbkk], ['mrg'])
                    else:
                        TT('dve', sg_[:, :sz], sg_[:, :sz], pbk[:, :sz], ALU.mult, [sgk, pbkk], [sgk])
                        TT('pool', mrg[:, dc % 2, :sz], mrg[:, dc % 2, :sz], sg_[:, :sz], ALU.add, ['mrg', sgk], ['mrg'])
                P.op('act', lambda e, dc=dc, sz=sz: e.copy(out=mrgb[:, dc, :sz], in_=mrg[:, dc % 2, :sz]), r=['mrg'], w=['mrgb'])
            for dc in range(8):
                pb_ = bank[5 + dc % 2]
                pk_ = ('ps', 5 + dc % 2)
                for k in range(8):
                    MM(pb_[:, :sz], wout_b[:, k, dc * 128:(dc + 1) * 128], mrgb[:, k, :sz], ['wout_b', 'mrgb'], [pk_],
                       start=(k == 0), stop=(k == 7))
                STT(x2[:, dc, :sz], pb_[:, :sz], mod_sb[:, 2 * 8 + dc, j:j + 1], xt[b][:, dc, :sz], ALU.mult, ALU.add,
                    [pk_, 'mod', ('xt', b)], ['x2'])
            P.dma('pool', x2s[:, :, t0:t0 + sz], x2[:, :, :sz], r=['x2'], w=['x2s'])
            xn2f = xt[b]
            xfk = ('xt', b)
            norm_mod(x2, sz, j, A2, 3, lambda k, sz=sz, xn2f=xn2f: xn2f[:, k, :sz], [xfk], ['x2'])
            P.op('act', lambda e, sz=sz, xn2f=xn2f: e.copy(out=xn1[:, :, :sz], in_=xn2f[:, :, :sz]), r=[xfk], w=['xn1'])
            P.dma('pool', xn2s[:, :, t0:t0 + sz], xn1[:, :, :sz], r=['xn1'], w=['xn2s'])
            for s0 in range(0, sz, 128):
                ns = min(128, sz - s0)
                for k in range(8):
                    MM(bank[7][:ns, 0:36], xn2f[:, k, s0:s0 + ns], wrt_sb[:, k, :], [xfk, 'wrt'], [('ps', 7)],
                       start=(k == 0), stop=(k == 7))
                lg = rt[:ns, 0:36]
                P.op('dve', lambda e, ns=ns: e.tensor_copy(out=rt[:ns, 0:36], in_=bank[7][:ns, 0:36]), r=[('ps', 7)], w=['rt'])
                gmax = rt[:ns, 36:37]
                RED(gmax, rt[:ns, 0:4], ALU.max, ['rt'], ['rt'])
                ohg = rt[:ns, 37:41]
                TS('dve', ohg, rt[:ns, 0:4], gmax, None, ALU.is_equal, None, ['rt'], ['rt'])
                ngm = rt[:ns, 41:42]
                TS('dve', ngm, gmax, -1.0, None, ALU.mult, None, ['rt'], ['rt'])
                eg = rt[:ns, 42:46]
                ACT(eg, rt[:ns, 0:4], AF.Exp, ['rt'], ['rt'], bias=ngm)
                pgr = rt[:ns, 46:47]
                RED(pgr, eg, ALU.add, ['rt'], ['rt'])
                P.op('dve', lambda e, pgr=pgr: e.reciprocal(out=pgr, in_=pgr), r=['rt'], w=['rt'])
                TT('dve', g32[:ns, :].rearrange("p (g e) -> p g e", e=8), rt[:ns, 4:36].rearrange("p (g e) -> p g e", e=8),
                   ohg.unsqueeze(2).broadcast_to([ns, 4, 8]), ALU.mult, ['rt'], ['g32'])
                les = rt[:ns, 47:55]
                RED(les, g32[:ns, :].rearrange("p (g e) -> p e g", e=8), ALU.add, ['g32'], ['rt'])
                top1 = rt[:ns, 55:56]
                RED(top1, les, ALU.max, ['rt'], ['rt'])
                oh1 = rt[:ns, 56:64]
                TS('dve', oh1, les, top1, None, ALU.is_equal, None, ['rt'], ['rt'])
                le2 = rt[:ns, 64:72]
                STT(le2, oh1, -1e30, les, ALU.mult, ALU.add, ['rt'], ['rt'])
                top2 = rt[:ns, 72:73]
                RED(top2, le2, ALU.max, ['rt'], ['rt'])
                oh2 = rt[:ns, 73:81] if False else None
                d12 = rt[:ns, 41:42]
                TT('dve', d12, top1, top2, ALU.subtract, ['rt'], ['rt'])
                ga = rt[:ns, 42:43]
                gb = rt[:ns, 43:44]
                ACT(ga, d12, AF.Sigmoid, ['rt'], ['rt'])
                ACT(gb, d12, AF.Sigmoid, ['rt'], ['rt'], scale=-1.0)
                TT('dve', rt[:ns, 42:44], rt[:ns, 42:44], pgr.broadcast_to([ns, 2]), ALU.mult, ['rt'], ['rt'])
                TS('dve', le2, le2, top2, gb, ALU.is_equal, ALU.mult, ['rt'], ['rt'])
                STT(les, oh1, ga, le2, ALU.mult, ALU.add, ['rt'], ['rt'])
                TT('dve', g32[:ns, :].rearrange("p (g e) -> p g e", e=8), ohg.unsqueeze(2).broadcast_to([ns, 4, 8]),
                   les.unsqueeze(1).broadcast_to([ns, 4, 8]), ALU.mult, ['rt', 'g32'], ['g32'])
                MM(bank[7][0:32, 64:64 + ns], g32[:ns, :], ident_sb[:ns, :ns], ['g32', 'ident'], [('ps', 7)])
                P.op('act', lambda e, s0=s0, ns=ns: e.copy(out=gTt[:, s0:s0 + ns], in_=bank[7][0:32, 64:64 + ns]),
                     r=[('ps', 7)], w=['gTt'])
            P.dma('pool', gTs[:, t0:t0 + sz], gTt[:, :sz], r=['gTt'], w=['gTs'])
        P.fence()
        A.release(mB1)
        selE_sb = A.alloc("selE", [32, 32 * 128])
        P.dma('sp', selE_sb[:], selE, w=['selE'])
        lat_ = tiles[:NT_LAT // 512]
        groups = [lat_[i:i + 2] for i in range(0, len(lat_), 2)]
        if NT_CTX:
            groups[-1] = groups[-1] + [tiles[-1]]
        GMAX = max(sum(t[1] for t in g) for g in groups)
        yacc = A.alloc("yacc", [128, 8, GMAX])
        xn2 = A.alloc("xn2g", [128, 8, GMAX], BF16)
        gT = A.alloc("gTg", [32, GMAX])
        wst = [A.alloc("wst%d" % i, [128, 4, 512]) for i in range(4)]
        wgb = [A.alloc("wgb%d" % i, [128, 8, 512], BF16) for i in range(2)]
        wub = [A.alloc("wub%d" % i, [128, 8, 512], BF16) for i in range(2)]
        wdb = [A.alloc("wdb%d" % i, [128, 4, 1024], BF16) for i in range(2)]
        hs = [A.alloc("hs%d" % i, [128, 512]) for i in range(2)]
        actb = [A.alloc("actb%d" % i, [128, 4, 512], BF16) for i in range(2)]
        x2r = A.alloc("x2r", [128, 8, 512])
        si = 0
        ai = 0
        hi_ = 0
        for gi, grp in enumerate(groups):
            g0 = grp[0][0]
            gsz = sum(t[1] for t in grp)
            P.dma('sp', xn2[:, :, :gsz], xn2s[:, :, g0:g0 + gsz], r=['xn2s'], w=['xn2g'])
            P.dma('sp', gT[:, :gsz], gTs[:, g0:g0 + gsz], r=['gTs'], w=['gTg'])
            for e_ in range(NEXP):
                wb = e_ % 2
                pieces = [(wg[e_, :, 0:4, :], wgb[wb][:, 0:4, :], ('wgb', wb)), (wg[e_, :, 4:8, :], wgb[wb][:, 4:8, :], ('wgb', wb)),
                          (wu[e_, :, 0:4, :], wub[wb][:, 0:4, :], ('wub', wb)), (wu[e_, :, 4:8, :], wub[wb][:, 4:8, :], ('wub', wb)),
                          (wd[e_, :, :, 0:512], wdb[wb][:, :, 0:512], ('wdb', wb)), (wd[e_, :, :, 512:1024], wdb[wb][:, :, 512:1024], ('wdb', wb))]
                for (src, dst, key) in pieces:
                    sb_ = si % 4
                    si += 1
                    P.dma('sp', wst[sb_][:], src, w=[('wst', sb_)])
                    eng = ('pool', 'dve', 'act')[si % 3] if False else ('pool' if si % 2 else 'act')
                    if eng == 'act':
                        P.op('act', lambda e, dst=dst, sb_=sb_: e.copy(out=dst, in_=wst[sb_][:]), r=[('wst', sb_)], w=[key])
                    else:
                        P.op('pool', lambda e, dst=dst, sb_=sb_: e.tensor_copy(out=dst, in_=wst[sb_][:]), r=[('wst', sb_)], w=[key])
                for (t0, sz, j) in grp:
                    ti = tiles.index((t0, sz, j))
                    lo = t0 - g0
                    MM(bank[0][:, :sz], selE_sb[:, e_ * 128:(e_ + 1) * 128], gT[:, lo:lo + sz], ['selE', 'gTg'], [('ps', 0)])
                    ab = actb[ai % 2]
                    ak = ('actb', ai % 2)
                    ai += 1
                    for fc in range(4):
                        pgb = bank[1 + fc % 2]
                        pgk = ('ps', 1 + fc % 2)
                        pub = bank[3 + fc % 2]
                        puk = ('ps', 3 + fc % 2)
                        for k in range(8):
                            MM(pgb[:, :sz], wgb[wb][:, k, fc * 128:(fc + 1) * 128], xn2[:, k, lo:lo + sz],
                               [('wgb', wb), 'xn2g'], [pgk], start=(k == 0), stop=(k == 7))
                        for k in range(8):
                            MM(pub[:, :sz], wub[wb][:, k, fc * 128:(fc + 1) * 128], xn2[:, k, lo:lo + sz],
                               [('wub', wb), 'xn2g'], [puk], start=(k == 0), stop=(k == 7))
                        h_ = hs[hi_ % 2]
                        hk = ('hs', hi_ % 2)
                        hi_ += 1
                        ACT(h_[:, :sz], pgb[:, :sz], AF.Silu, [pgk], [hk])
                        TT('dve', h_[:, :sz], h_[:, :sz], pub[:, :sz], ALU.mult, [hk, puk], [hk])
                        TT('dve', ab[:, fc, :sz], h_[:, :sz], bank[0][:, :sz], ALU.mult, [hk, ('ps', 0)], [ak])
                    for dc in range(8):
                        pdb = bank[5 + dc % 3]
                        pdk = ('ps', 5 + dc % 3)
                        for fc in range(4):
                            MM(pdb[:, :sz], wdb[wb][:, fc, dc * 128:(dc + 1) * 128], ab[:, fc, :sz], [('wdb', wb), ak], [pdk],
                               start=(fc == 0), stop=(fc == 3))
                        if e_ == 0:
                            P.op('act', lambda e, dc=dc, lo=lo, sz=sz, pdb=pdb: e.copy(out=yacc[:, dc, lo:lo + sz], in_=pdb[:, :sz]),
                                 r=[pdk], w=['yacc'])
                        else:
                            TT('pool' if False else 'dve', yacc[:, dc, lo:lo + sz], yacc[:, dc, lo:lo + sz], pdb[:, :sz], ALU.add,
                               ['yacc', pdk], ['yacc'])
            for (t0, sz, j) in grp:
                lo = t0 - g0
                P.dma('sp', x2r[:, :, :sz], x2s[:, :, t0:t0 + sz], r=['x2s'], w=['x2r'])
                for dc in range(8):
                    STT(x2r[:, dc, :sz], yacc[:, dc, lo:lo + sz], mod_sb[:, 5 * 8 + dc, j:j + 1], x2r[:, dc, :sz], ALU.mult, ALU.add,
                        ['yacc', 'mod', 'x2r'], ['x2r'])
                x_store(x2r, t0, sz, ['x2r'])
        P.fence()
        A.release(base_mark)


POOL_WINDOWS = (2, 4, 8, 16)
def fm(a):
    return np.ascontiguousarray(a.reshape(8, 128, *a.shape[1:]).swapaxes(0, 1))
def colsel(hg):
    c = []
    c += list(range(hg * 128, hg * 128 + 128))
    c += list(range(512 + hg * 128, 512 + hg * 128 + 128))
    c += list(range(1024 + hg * 128, 1024 + hg * 128 + 128))
    c += list(range(1536 + hg * 128, 1536 + hg * 128 + 128))
    for j in range(3):
        c += list(range(2048 + j * 512 + hg * 128, 2048 + j * 512 + hg * 128 + 128))
    c += list(range(2048 + 1536, 2048 + 1920))
    return np.array(c)
def rope_tables(L):
    nrow = L // 64
    inv = (10000.0 ** (-np.arange(16, dtype=np.float32) / 16)).astype(np.float32)
    R = np.ones((128, 2, nrow), np.float32); C = np.ones((128, 2, 64), np.float32)
    perm = np.zeros((128, 128), np.float32)
    rows = np.arange(nrow, dtype=np.float32); cols = np.arange(64, dtype=np.float32)
    for p in range(128):
        d = p % 64
        blk = d // 16
        f = inv[d % 16]
        sign = -1.0 if blk % 2 == 0 else 1.0
        partner = p + 16 if blk % 2 == 0 else p - 16
        perm[partner, p] = 1.0
        if blk < 2:
            ang = (rows * f).astype(np.float32)
            R[p, 0] = np.cos(ang); R[p, 1] = sign * np.sin(ang)
        else:
            ang = (cols * f).astype(np.float32)
            C[p, 0] = np.cos(ang); C[p, 1] = sign * np.sin(ang)
    return R, C, perm
def edge_fix(w, L):
    t = np.arange(L)
    lo = np.clip(t - w // 2, 0, L - 1); hi = np.clip(t + w // 2 - 1, 0, L - 1)
    ratio = (w / (hi - lo + 1)).astype(np.float32)
    return np.concatenate([ratio[:8], ratio[-8:]])
def inputs_A(inp, l, b, hg, L, lam_init, x=None, ctx=None):
    if x is None:
        x = inp['x'][b, :L]; ctx = inp['ctx'][b]
    xa = np.concatenate([ctx, x], 0)
    R, C, perm = rope_tables(L)
    cs = colsel(hg)
    w = POOL_WINDOWS[hg]
    selw = np.zeros((128, 4), np.float32); selw[:, hg] = 1.0 / w
    d = dict(
        xT=fm(np.ascontiguousarray(xa.T)),
        cT=fm(np.stack([inp['c'][b], inp['c_ctx']], 1)),
        wada=fm(inp['w_ada'][l][:, :2048]),
        bada=np.ascontiguousarray(inp['b_ada'][l][:2048].reshape(16, 128).T),
        g1=np.ascontiguousarray(inp['norm1_g'][l].reshape(8, 128).T),
        win=fm(inp['w_in'][l][:, cs]),
        qkg=np.stack([np.tile(inp['q_norm_g'][l], 2), np.tile(inp['k_norm_g'][l], 2)], 1).astype(np.float32),
        ropeR=R, ropeC=C, perm=perm,
        lamqk=np.ascontiguousarray(np.broadcast_to(inp['lambda_qk'][l].reshape(1, 256), (128, 256))),
        subg=np.ascontiguousarray(np.broadcast_to((inp['subln_g'][l] * np.float32(1 - lam_init)).reshape(1, 128), (128, 128))).astype(np.float32),
        wpool=np.ascontiguousarray(inp['pool_w'][l][hg]),
        pscale=np.ascontiguousarray(inp['pool_scale'][l][hg * 128:(hg + 1) * 128].reshape(128, 1)),
        selw=selw,
        efix=np.ascontiguousarray(np.broadcast_to(edge_fix(w, L).reshape(1, 16), (128, 16))),
    )
    return d

def rw_consts():
    i = np.arange(64)
    incl = (i[:, None] <= i[None, :]).astype(np.float32)
    strict = (i[:, None] < i[None, :]).astype(np.float32)
    ones = np.ones((64, 64), np.float32)
    MkN = (i[None, :] < i[:, None]).astype(np.float32)
    return np.concatenate([incl, strict, ones, strict, incl, MkN, np.eye(64, dtype=np.float32)], 1)
def inputs_rw(inp, l, hg):
    mu = inp['shift_mu'][l]
    cmu = np.zeros((128, 6, 2), np.float32)
    p = np.arange(128)
    for g in range(3):
        cmu[:, g, :] = mu[:, g * 512 + hg * 128 + p].T
    for g, base in ((3, 1536), (4, 1664), (5, 1792)):
        cmu[:, g, :] = mu[:, base + p].T
    heads = [2 * hg, 2 * hg + 1]
    w2 = np.zeros((64, 2, 2, 64), np.float32); a2 = np.zeros((64, 2, 2, 64), np.float32)
    w0b = np.zeros((2, 2, 64), np.float32); a0f = np.zeros((64, 2, 2), np.float32)
    prm = np.zeros((64, 2, 5), np.float32)
    for h, hd in enumerate(heads):
        cs = slice(hd * 64, hd * 64 + 64)
        for d in range(2):
            w2[:, d, h, :] = inp['decay_w2'][l][d][:, cs]
            a2[:, d, h, :] = inp['aaa_a2'][l][d][:, cs]
            w0b[d, h, :] = inp['decay_w0'][l][d][cs]
            a0f[:, d, h] = inp['aaa_a0'][l][d][cs]
        prm[:, h, 0] = inp['k_k'][l][cs]; prm[:, h, 1] = inp['k_a'][l][cs]; prm[:, h, 2] = inp['r_k'][l][hd]
        prm[:, h, 3] = inp['gn_w'][l][cs]; prm[:, h, 4] = inp['gn_b'][l][cs]
    return dict(rw_cmu=cmu, rw_g2=np.ascontiguousarray(inp['gate_w2'][l][:, hg * 128:(hg + 1) * 128]),
                rw_w2=w2.reshape(64, 256), rw_a2=a2.reshape(64, 256),
                rw_w0b=np.ascontiguousarray(np.broadcast_to(w0b.reshape(1, 256), (64, 256))),
                rw_a0f=a0f.reshape(64, 4), rw_prm=prm, rw_cst=rw_consts())


def weights_B(inp, l):
    selE = np.zeros((32, 32, 128), np.float32)
    for e in range(32): selE[e, e, :] = 1.0
    return dict(
        wada=fm(inp['w_ada'][l]),
        bada=np.ascontiguousarray(inp['b_ada'][l].reshape(48, 128).T),
        g12=np.ascontiguousarray(np.stack([inp['norm1_g'][l].reshape(8, 128).T, inp['norm2_g'][l].reshape(8, 128).T], 1)),
        wgate=fm(inp['w_in'][l][:, 3968:]),
        wbr=np.ascontiguousarray(inp['w_br'][l].reshape(12, 128, 1024).transpose(1, 0, 2)),
        wout=fm(inp['w_out'][l]),
        wrt=fm(np.concatenate([inp['w_router_group'][l], inp['w_router_expert'][l]], 1)),
        wg=np.ascontiguousarray(inp['w_exp_gate'][l].reshape(32, 8, 128, 512).transpose(0, 2, 1, 3)),
        wu=np.ascontiguousarray(inp['w_exp_up'][l].reshape(32, 8, 128, 512).transpose(0, 2, 1, 3)),
        wd=np.ascontiguousarray(inp['w_exp_down'][l].reshape(32, 4, 128, 1024).transpose(0, 2, 1, 3)),
        selE=selE.reshape(32, 4096), ident=np.eye(128, dtype=np.float32))
def acts_B(xa, br, cvec, c_ctx):
    NT = xa.shape[0]
    return dict(xT=fm(np.ascontiguousarray(xa.T)),
                brT=np.ascontiguousarray(br.T.reshape(12, 128, NT).transpose(1, 0, 2)),
                cT=fm(np.stack([cvec, c_ctx], 1)))


GROUPS = [[0, 1, 2, 3], [4, 5, 6, 7]]


CC_COLS = 2048


class BrStore:
    def __init__(self, nc, name, NQ, ctx_out):
        self.splits = ([(0, 256)] if ctx_out else []) + [(c0, min(CC_COLS, NQ - c0)) for c0 in range(256 if ctx_out else 0, NQ, CC_COLS)]
        self.b = {}
        self.g = {}
        for n in range(3):
            for ci, (c0, cs) in enumerate(self.splits):
                self.b[(n, ci)] = nc.dram_tensor("%s_b%d_%d" % (name, n, ci), [128, cs], F32, kind="Internal").ap()
                self.g[(n, ci)] = nc.dram_tensor("%s_g%d_%d" % (name, n, ci), [4 * 128, cs], F32, kind="Internal").ap()

    def _find(self, col0, ncols):
        for ci, (c0, cs) in enumerate(self.splits):
            if c0 <= col0 and col0 + ncols <= c0 + cs:
                return ci, col0 - c0
        raise AssertionError(("chunk straddle", col0, ncols))

    def dst(self, n, col0, ncols):
        ci, o = self._find(col0, ncols)
        return self.b[(n, ci)][:, o:o + ncols]

    def gsrc(self, n, col0, ncols):
        ci, o = self._find(col0, ncols)
        return self.g[(n, ci)].rearrange("(g p) t -> p g t", g=4)[:, :, o:o + ncols]

    def exchange(self, P):
        for key in self.b:
            P.cc(self.b[key], self.g[key], GROUPS, r=['brb'], w=['gath'])


class XStore:
    def __init__(self, nc, name, NL):
        self.NL = NL
        NT0 = NL + 64
        self.splits = [(c0, min(CC_COLS, NL - c0)) for c0 in range(0, NL, CC_COLS)] + [(NL, 64)]
        self.b = {}
        self.g = {}
        for k in range(8):
            for ci, (c0, cs) in enumerate(self.splits):
                self.b[(k, ci)] = nc.dram_tensor("%s_b%d_%d" % (name, k, ci), [128, cs], F32, kind="Internal").ap()
                self.g[(k, ci)] = nc.dram_tensor("%s_g%d_%d" % (name, k, ci), [4 * 128, cs], F32, kind="Internal").ap()

    def _find(self, col0, ncols):
        for ci, (c0, cs) in enumerate(self.splits):
            if c0 <= col0 and col0 + ncols <= c0 + cs:
                return ci, col0 - c0
        raise AssertionError(("chunk straddle", col0, ncols))

    def store(self, P, src_tile, t0, sz, rkeys):
        ci, o = self._find(t0, sz)
        for k in range(8):
            P.dma('pool', self.b[(k, ci)][:, o:o + sz], src_tile[:, k, :sz], r=rkeys, w=['xnew'])

    def load_local(self, P, dst_tile, t0, sz, key):
        ci, o = self._find(t0, sz)
        for k in range(8):
            P.dma('sp', dst_tile[:, k, :sz], self.b[(k, ci)][:, o:o + sz], r=['xnew'], w=[key])

    def load_gathered(self, P, dst_tile, dcol, rank, t0, sz, key):
        ci, o = self._find(t0, sz)
        for k in range(8):
            P.dma('sp', dst_tile[:, k, dcol:dcol + sz], self.g[(k, ci)][rank * 128:(rank + 1) * 128, o:o + sz], r=['xg'], w=[key])

    def exchange(self, P):
        for key in self.b:
            P.cc(self.b[key], self.g[key], GROUPS, r=['xnew'], w=['xg'])


def build_fused(L=16384, nexp=32, stop_after=99):
    nc = bass.Bass("TRN2", target_bir_lowering=False)
    nc.allow_low_precision("bf16 matmul operands, fp32 accumulation")
    P = Prog(nc)
    A = Arena(nc)
    T = 256 + L
    NL = L // 4
    NT0 = NL + 64
    lam = [0.8 - 0.6 * math.exp(-0.3 * l) for l in range(2)]
    with contextlib.ExitStack() as st:
        bank = [st.enter_context(nc.psum_tensor("bank%d" % i, [128, 512], F32)) for i in range(8)]
        xout = nc.dram_tensor("xout", [128, 8, NL], F32, kind="ExternalOutput").ap()

        def finish():
            stats = P.emit()
            stats['sbuf_peak'] = A.peak
            return nc, stats
        br0 = BrStore(nc, "br0", T, True)
        phase_A(nc, P, A, bank, "A0_", L, True, lam[0], None, br0)
        P.fence()
        if stop_after == 1:
            return finish()
        br0.exchange(P)
        P.fence()
        if stop_after == 2:
            return finish()
        xsh = nc.dram_tensor("xsh", [128, 8, NT0], F32, kind="ExternalInput").ap()
        xs = XStore(nc, "xs", NL)

        def x_load0(dst, t0, sz, key):
            P.dma('sp', dst[:, :, :sz], xsh[:, :, t0:t0 + sz], w=[key])
        phase_B(nc, P, A, bank, "B0_", NL, 64, x_load0, br0, 256, lambda src, t0, sz, rk: xs.store(P, src, t0, sz, rk), NEXP=nexp)
        if stop_after == 3:
            return finish()
        xs.exchange(P)
        P.fence()
        if stop_after == 4:
            return finish()

        def x_loader(dst, t0, sz, key):
            if t0 < 256:
                for r in range(4):
                    xs.load_gathered(P, dst, r * 64, r, NL, 64, key)
            else:
                tt = t0 - 256
                xs.load_gathered(P, dst, 0, tt // NL, tt % NL, sz, key)
        br1 = BrStore(nc, "br1", L, False)
        phase_A(nc, P, A, bank, "A1_", L, False, lam[1], x_loader, br1)
        P.fence()
        if stop_after == 5:
            return finish()
        br1.exchange(P)
        P.fence()
        if stop_after == 6:
            return finish()

        def x_store1(src, t0, sz, rk):
            P.dma('pool', xout[:, :, t0:t0 + sz], src[:, :, :sz], r=rk, final=True)
        phase_B(nc, P, A, bank, "B1_", NL, 0, lambda dst, t0, sz, key: xs.load_local(P, dst, t0, sz, key), br1, 0, x_store1, NEXP=nexp)
        return finish()


def fused_inputs(inp, L, nexp=32, names=None):
    inp = {k: np.asarray(v) for k, v in inp.items()}
    NL = L // 4
    x = inp['x'][:, :L]
    ctx = inp['ctx']
    lam = [0.8 - 0.6 * math.exp(-0.3 * l) for l in range(2)]
    WB = [weights_B(inp, l) for l in range(2)]
    ident = np.eye(128, dtype=np.float32)
    maps = []
    for i in range(8):
        b, hg = i // 4, i % 4
        d = {}
        for l in range(2):
            a = inputs_A(inp, l, b, hg, L, lam[l], x=x[b], ctx=ctx[b])
            if l == 1:
                a.pop('xT')
            a.update(inputs_rw(inp, l, hg))
            a['identA'] = ident
            for k, v in a.items():
                d["A%d_" % l + k] = v
            for k, v in WB[l].items():
                d["B%d_" % l + k] = v[:nexp] if k in ('wg', 'wu', 'wd') else v
            d["B%d_cT" % l] = fm(np.stack([inp['c'][b], inp['c_ctx']], 1))
            selq = np.zeros((128, 4), np.float32)
            selq[:, hg] = 1.0
            d["B%d_selq" % l] = selq
        xa = np.concatenate([x[b, hg * NL:(hg + 1) * NL], ctx[b, hg * 64:(hg + 1) * 64]], 0)
        d['xsh'] = fm(np.ascontiguousarray(xa.T))
        if names is not None:
            d = {k: v for k, v in d.items() if k in names}
        maps.append(d)
    return maps


def fused_gather(results, L):
    NL = L // 4
    out = np.empty((2, L, 1024), np.float32)
    for i in range(8):
        b, q = i // 4, i % 4
        o = np.asarray(results[i]['xout']).transpose(1, 0, 2).reshape(1024, NL).T
        out[b, q * NL:(q + 1) * NL] = o
    return out


def kernel(**inp):
    inp = {k: np.asarray(v) for k, v in inp.items()}
    L = inp['x'].shape[1]
    nc, _ = build_fused(L)
    maps = fused_inputs(inp, L)
    res = run_bass_kernel_spmd(nc, maps, core_ids=list(range(8)))
    del maps
    return fused_gather(res.results, L)
```

```python
import math
import contextlib


import numpy as np
import concourse.bass as bass
import concourse.mybir as mybir
from concourse.bass_utils import run_bass_kernel_spmd

F32 = mybir.dt.float32
BF16 = mybir.dt.bfloat16
I32 = mybir.dt.int32
AF = mybir.ActivationFunctionType
ALU = mybir.AluOpType
AX = mybir.AxisListType

SEM_LIMIT = 8000
DMA_POOL = 40


class Prog:
    def __init__(self, nc):
        self.nc = nc
        self.ops = []

    def op(self, eng, fn, r=(), w=()):
        self.ops.append(dict(eng=eng, fn=fn, r=tuple(r), w=tuple(w), dma=False, final=False))

    def dma(self, eng, out, in_, r=(), w=(), final=False, **kw):
        def fn(e, out=out, in_=in_, kw=kw):
            return e.dma_start(out=out, in_=in_, **kw)
        self.ops.append(dict(eng=eng, fn=fn, r=tuple(r), w=tuple(w), dma=True, final=final))

    def cc(self, ins_ap, out_ap, groups, r=(), w=()):
        def fn(e, ins_ap=ins_ap, out_ap=out_ap, groups=groups):
            return e.collective_compute("AllGather", ALU.bypass, replica_groups=groups, ins=[ins_ap], outs=[out_ap])
        self.ops.append(dict(eng='pool', fn=fn, r=tuple(r), w=tuple(w), dma=True, final=False, cc=True))

    def fence(self):
        self.ops.append(dict(eng=None, fn=None, r=(), w=(), dma=False, final=False, fence=True))

    def emit(self):
        nc = self.nc
        raw_ops = self.ops
        ops = []
        fence_after = {}
        fence_pos = []
        for o in raw_ops:
            if o.get('fence'):
                fence_pos.append(len(ops))
            else:
                ops.append(o)
        self.ops = ops
        n = len(ops)
        fence_deps_at = {}
        prev = 0
        for fp in fence_pos:
            last = {}
            dm = set()
            for i in range(prev, fp):
                o = ops[i]
                if o['dma']:
                    dm.add(i)
                else:
                    last[o['eng']] = i
            fence_deps_at[fp] = set(last.values()) | dm
            prev = fp
        last_w = {}
        readers = {}
        deps = [None] * n
        cur_fence = set()
        first_after = {}
        for i, o in enumerate(ops):
            if i in fence_deps_at:
                cur_fence = fence_deps_at[i]
                first_after = {}
            d = {}

            def add(j, raw):
                d[j] = d.get(j, False) or raw
            for k in o['r']:
                if k in last_w:
                    add(last_w[k], True)
            for k in o['w']:
                if k in last_w:
                    add(last_w[k], False)
                for tok, j in readers.get(k, {}).items():
                    add(j, False)
            keep = set()
            for j, raw in d.items():
                if j == i:
                    continue
                oj = ops[j]
                if (not oj['dma']) and (not o['dma']) and oj['eng'] == o['eng']:
                    if o['eng'] == 'pe':
                        continue
                    if not raw:
                        continue
                keep.add(j)
            tok_e = o['eng']
            if cur_fence and tok_e not in first_after:
                first_after[tok_e] = i
                for j in cur_fence:
                    if ops[j]['dma'] or ops[j]['eng'] != tok_e:
                        keep.add(j)
            deps[i] = keep
            for k in o['r']:
                tok = ('d', i) if o['dma'] else o['eng']
                readers.setdefault(k, {})[tok] = i
            for k in o['w']:
                last_w[k] = i
                readers[k] = {}
        needed = [False] * n
        for i in range(n):
            for j in deps[i]:
                needed[j] = True
        engs = ['pe', 'act', 'dve', 'pool', 'sp']
        cnt = {e: 0 for e in engs}
        sig = [None] * n
        dma_uses = [0] * DMA_POOL
        dma_last = [None] * DMA_POOL
        ndma = 0
        semkeys = set()
        for i, o in enumerate(ops):
            if o.get('cc'):
                ncc_ = getattr(self, '_ncc', 0) + 1
                self._ncc = ncc_
                sig[i] = (('cc', 0), ncc_)
                semkeys.add(('cc', 0))
            elif o['dma']:
                j = ndma % DMA_POOL
                ndma += 1
                dma_uses[j] += 1
                if dma_last[j] is not None:
                    deps[i].add(dma_last[j])
                dma_last[j] = i
                sig[i] = (('dma', j), 16 * dma_uses[j])
                semkeys.add(('dma', j))
            elif needed[i]:
                e = o['eng']
                c = cnt[e]
                cnt[e] += 1
                sk = (e, c // SEM_LIMIT)
                sig[i] = (sk, c % SEM_LIMIT + 1)
                semkeys.add(sk)
        finals = [i for i, o in enumerate(ops) if o['final']]
        seen = {e: {} for e in engs}
        streams = {e: [] for e in engs}
        for i, o in enumerate(ops):
            e = o['eng']
            waits = {}
            for j in deps[i]:
                sk, v = sig[j]
                if seen[e].get(sk, 0) >= v:
                    continue
                waits[sk] = max(waits.get(sk, 0), v)
            for sk, v in waits.items():
                seen[e][sk] = v
            streams[e].append((list(waits.items()), o['fn'], sig[i]))
        fw = {}
        for i in finals:
            sk, v = sig[i]
            if seen['sp'].get(sk, 0) >= v:
                continue
            fw[sk] = max(fw.get(sk, 0), v)
        streams['sp'].append((list(fw.items()), None, None))
        self.stats = dict(n_ops=n, cnt=dict(cnt), ndma=ndma,
                          nwaits={e: sum(len(s[0]) for s in streams[e]) for e in engs})
        semkeys = sorted(semkeys, key=str)
        import contextlib
        with contextlib.ExitStack() as st:
            sems = {}
            for sk in semkeys:
                sems[sk] = st.enter_context(nc.semaphore("s_%s_%s" % (sk[0], sk[1])))
            block = st.enter_context(nc.Block())

            def run(engine, items):
                for waits, fn, sg in items:
                    for sk, v in waits:
                        engine.wait_ge(sems[sk], v)
                    if fn is None:
                        continue
                    ins = fn(engine)
                    if sg is not None:
                        inc = 16 if sg[0][0] == 'dma' else 1
                        ins.then_inc(sems[sg[0]], inc)

            @block.tensor
            def _(e):
                run(e, streams['pe'])

            @block.scalar
            def _(e):
                run(e, streams['act'])

            @block.vector
            def _(e):
                run(e, streams['dve'])

            @block.gpsimd
            def _(e):
                run(e, streams['pool'])

            @block.sync
            def _(e):
                run(e, streams['sp'])
        return self.stats


class Arena:
    LO = 16512
    HI = 229376

    def __init__(self, nc):
        self.nc = nc
        self.top = self.LO
        self.n = 0
        self.peak = self.LO

    def alloc(self, name, shape, dt=F32):
        esz = {F32: 4, BF16: 2, I32: 4}[dt]
        nb = esz
        for s_ in shape[1:]:
            nb *= s_
        off = (self.top + 63) // 64 * 64
        assert off + nb <= self.HI, ("SBUF overflow", name, off + nb)
        self.top = off + nb
        self.peak = max(self.peak, self.top)
        self.n += 1
        return self.nc.alloc_sbuf_tensor_at("%s_%d" % (name, self.n), list(shape), dt, offset=off)

    def mark(self):
        return self.top

    def release(self, m):
        self.top = m


GN_EPS = 64e-5
FINAL_OUT = False
KAPPA = 0.6065306597126334
CTXN = 256


def rwkv_phase(nc, P, A, bank, pT, L, ctx_out, out_ap, pfx=''):
    T = CTXN + L
    di = lambda name, shape: nc.dram_tensor(pfx + name, list(shape), F32, kind="ExternalInput").ap()
    cmu = di("rw_cmu", [128, 6, 2])
    g2s = di("rw_g2", [128, 128])
    w2s = di("rw_w2", [64, 256])
    a2s = di("rw_a2", [64, 256])
    w0b = di("rw_w0b", [64, 256])
    a0f = di("rw_a0f", [64, 4])
    prm = di("rw_prm", [64, 2, 5])
    cst = di("rw_cst", [64, 448])
    uT = nc.dram_tensor(pfx + "uT_scr", [6, 128, T], F32, kind="Internal").ap()
    Yd = nc.dram_tensor(pfx + "Yd_scr", [2, 128, T], F32, kind="Internal").ap()
    Bd = nc.dram_tensor(pfx + "Bd_scr", [2, 128, T], F32, kind="Internal").ap()

    def TT(eng, out, a, b, op, r, w):
        P.op(eng, lambda e: e.tensor_tensor(out=out, in0=a, in1=b, op=op), r=r, w=w)

    def TS(eng, out, a, s1, s2, op0, op1, r, w):
        if op1 is None:
            P.op(eng, lambda e: e.tensor_scalar(out=out, in0=a, scalar1=s1, scalar2=None, op0=op0), r=r, w=w)
        else:
            P.op(eng, lambda e: e.tensor_scalar(out=out, in0=a, scalar1=s1, scalar2=s2, op0=op0, op1=op1), r=r, w=w)

    def STT(out, a, s, b, op0, op1, r, w):
        P.op('dve', lambda e: e.scalar_tensor_tensor(out=out, in0=a, scalar=s, in1=b, op0=op0, op1=op1), r=r, w=w)

    def ACT(out, a, func, r, w, scale=1.0, bias=None):
        if bias is None:
            P.op('act', lambda e: e.activation(out=out, in_=a, func=func, scale=scale), r=r, w=w)
        else:
            P.op('act', lambda e: e.activation(out=out, in_=a, func=func, scale=scale, bias=bias), r=r, w=w)

    def MM(out, lhsT, rhs, r, w, start=True, stop=True):
        P.op('pe', lambda e: e.matmul(out, lhsT=lhsT, rhs=rhs, start=start, stop=stop), r=r, w=w)

    m0 = A.mark()
    cmu_sb = A.alloc("cmu", [128, 6, 2])
    c0_sb = A.alloc("c0", [128, 6])
    g2_sb = A.alloc("g2", [128, 128])
    raw = [A.alloc("raw%d" % i, [128, 6, 514]) for i in range(2)]
    ush = A.alloc("ush", [128, 6, 512])
    sg = A.alloc("sg", [128, 512])
    P.dma('sp', cmu_sb[:], cmu, w=['cmu'])
    P.dma('sp', g2_sb[:], g2s, w=['g2'])
    TT('dve', c0_sb[:], cmu_sb[:, :, 0], cmu_sb[:, :, 1], ALU.add, ['cmu'], ['c0'])
    TS('dve', c0_sb[:], c0_sb[:], -1.0, 1.0, ALU.mult, ALU.add, ['c0'], ['c0'])
    seqs = [(0, CTXN), (CTXN, L)]
    ti = 0
    for (s0, slen) in seqs:
        for b0 in range(0, slen, 512):
            n = min(512, slen - b0)
            rb = raw[ti % 2]
            rk = ('raw', ti % 2)
            ti += 1
            lo = max(0, b0 - 1)
            hi = min(slen, b0 + n + 1)
            if b0 == 0 or b0 + n == slen:
                P.op('pool', lambda e, rb=rb: e.memset(rb[:], 0.0), w=[rk])
            P.dma('sp', rb[:, :, 1 + lo - b0:1 + hi - b0], pT[1:7, :, s0 + lo:s0 + hi].rearrange("g p t -> p g t"),
                  r=['pTall'], w=[rk])
            for g in range(6):
                eng = 'dve'
                TS('pool', ush[:, g, :n], rb[:, g, 1:1 + n], c0_sb[:, g:g + 1], None, ALU.mult, None, [rk, 'c0'], ['ush'])
                STT(ush[:, g, :n], rb[:, g, 0:n], cmu_sb[:, g, 0:1], ush[:, g, :n], ALU.mult, ALU.add, [rk, 'cmu', 'ush'], ['ush'])
                STT(ush[:, g, :n], rb[:, g, 2:2 + n], cmu_sb[:, g, 1:2], ush[:, g, :n], ALU.mult, ALU.add, [rk, 'cmu', 'ush'], ['ush'])
            ACT(sg[:, :n], ush[:, 5, :n], AF.Sigmoid, ['ush'], ['sg'])
            MM(bank[0][:, :n], g2_sb[:], sg[:, :n], ['g2', 'sg'], [('ps', 0)])
            P.op('act', lambda e, n=n: e.copy(out=ush[:, 5, :n], in_=bank[0][:, :n]), r=[('ps', 0), 'ush'], w=['ush'])
            P.dma('pool', uT[:, :, s0 + b0:s0 + b0 + n].rearrange("g p t -> p g t"), ush[:, :, :n], r=['ush'], w=['uTall'])
    P.fence()
    A.release(m0)
    w2_sb = A.alloc("w2", [64, 256])
    a2_sb = A.alloc("a2", [64, 256])
    w0b_sb = A.alloc("w0b", [64, 256])
    a0f_sb = A.alloc("a0f", [64, 4])
    prm_sb = A.alloc("prm", [64, 2, 5])
    cst_sb = A.alloc("cst", [64, 448])
    ones64 = A.alloc("ones64", [64, 64])
    for nm, dst, src in [('w2', w2_sb, w2s), ('a2', a2_sb, a2s), ('w0b', w0b_sb, w0b), ('a0f', a0f_sb, a0f),
                         ('prm', prm_sb, prm), ('cst', cst_sb, cst)]:
        P.dma('sp', dst[:], src, w=[nm])
    P.op('pool', lambda e: e.memset(ones64[:], 1.0), w=['ones64'])
    Tri3 = cst_sb[:, 0:192]
    Mk = cst_sb[:, 192:320]
    MkN = cst_sb[:, 320:384]
    I64 = cst_sb[:, 384:448]
    I64b = A.alloc("I64b", [64, 64], BF16)
    P.op('dve', lambda e: e.tensor_copy(out=I64b[:], in_=cst_sb[:, 384:448]), r=['cst'], w=['I64b'])
    ST = A.alloc("ST", [64, 4, 64])
    Stmp = A.alloc("Stmp", [64, 4, 64])
    P.op('pool', lambda e: e.memset(ST[:], 0.0), w=['ST'])
    KSEG = 4
    NBUF = KSEG + 1
    f4 = lambda name: A.alloc(name, [64, 4, 64])

    shr = dict(L_=dict(r=A.alloc("ld_r", [64, 2, 2, 64]), k=A.alloc("ld_k", [64, 2, 2, 64]), v=A.alloc("ld_v", [64, 2, 2, 64]),
                       wl=A.alloc("ld_wl", [64, 2, 64]), al=A.alloc("ld_al", [64, 2, 64])),
               uwl=A.alloc("uwl", [64, 2, 64]), ual=A.alloc("ual", [64, 2, 64]), tw=A.alloc("tw", [64, 2, 64]),
               swt=A.alloc("swt", [64, 256]), kk=f4("kk"), kk2=f4("kk2"), rn=f4("rn"), kkn=f4("kkn"), bb=f4("bb"), km=f4("km"),
               t1=f4("t1"), BhT=A.alloc("BhT", [64, 4, 64], BF16), KhT=A.alloc("KhT", [64, 4, 64], BF16), Xsb=f4("Xsb"), Usb=f4("Usb"), Ysb=f4("Ysb"))

    def alloc_set(i):
        n_ = lambda x: "%s_%d" % (x, i)
        return (shr['L_'], f4(n_("ur")), f4(n_("uk")), f4(n_("uv")), shr['uwl'], shr['ual'],
                shr['tw'], shr['swt'], f4(n_("alr")),
                f4(n_("eI")), f4(n_("eE")), f4(n_("eN")), f4(n_("eT")), A.alloc(n_("gC"), [64, 4]),
                shr['kk'], shr['kk2'], shr['rn'], shr['kkn'], shr['bb'], shr['km'], shr['t1'],
                A.alloc(n_("AR"), [64, 4, 128]), f4(n_("BT")), f4(n_("KTt")), shr['BhT'], shr['KhT'], f4(n_("bon")),
                A.alloc(n_("TM"), [64, 2, 4, 64]), f4(n_("Vt")), A.alloc(n_("Gb"), [64, 4, 128]), A.alloc(n_("Gk"), [64, 4, 128]),
                A.alloc(n_("Nn"), [64, 4, 64], BF16), [A.alloc(n_("Pk%d" % j), [64, 2, 4, 64], BF16) for j in range(2)],
                [A.alloc(n_("Q0"), [64, 4, 64], BF16), A.alloc(n_("Q1"), [64, 4, 64], BF16), f4(n_("Qf")), A.alloc(n_("P0b"), [64, 4, 64], BF16)],
                shr['Xsb'], shr['Usb'], shr['Ysb'])
    sets = [alloc_set(i) for i in range(NBUF)]
    prmb = lambda j: prm_sb[:, :, j:j + 1].unsqueeze(1).broadcast_to([64, 2, 2, 64])
    v4 = lambda t: t[:].rearrange("p (d h) t -> p d h t", d=2)
    SHARED = set(['w2', 'a2', 'w0b', 'a0f', 'prm', 'cst', 'ones64', 'uTall', 'Bdall', 'Ydall', 'ST', 'Stmp', 'I64b',
                  'ld', 'wl', 'al', 'tw', 'swt', 'kk', 'kk2', 'rn', 'kkn', 'bb', 'km', 't1', 'BhT', 'KhT', 'Xsb', 'Usb', 'Ysb'])
    cur = {'b': None}

    def kmap(keys):
        if cur['b'] is None:
            return list(keys)
        return [k if (k in SHARED or (isinstance(k, tuple) and k[0] == 'ps')) else ('rw', k, cur['b']) for k in keys]
    _op, _dma = P.op, P.dma

    def Pop(eng, fn, r=(), w=()):
        _op(eng, fn, r=kmap(r), w=kmap(w))

    def Pdma(eng, out, in_, r=(), w=(), **kw):
        _dma(eng, out, in_, r=kmap(r), w=kmap(w), **kw)

    def TT(eng, out, a, b, op, r, w):
        Pop(eng, lambda e: e.tensor_tensor(out=out, in0=a, in1=b, op=op), r=r, w=w)

    def TS(eng, out, a, s1, s2, op0, op1, r, w):
        if op1 is None:
            Pop(eng, lambda e: e.tensor_scalar(out=out, in0=a, scalar1=s1, scalar2=None, op0=op0), r=r, w=w)
        else:
            Pop(eng, lambda e: e.tensor_scalar(out=out, in0=a, scalar1=s1, scalar2=s2, op0=op0, op1=op1), r=r, w=w)

    def STT(out, a, s, b, op0, op1, r, w):
        Pop('dve', lambda e: e.scalar_tensor_tensor(out=out, in0=a, scalar=s, in1=b, op0=op0, op1=op1), r=r, w=w)

    def ACT(out, a, func, r, w, scale=1.0, bias=None):
        if bias is None:
            Pop('act', lambda e: e.activation(out=out, in_=a, func=func, scale=scale), r=r, w=w)
        else:
            Pop('act', lambda e: e.activation(out=out, in_=a, func=func, scale=scale, bias=bias), r=r, w=w)

    def MM(out, lhsT, rhs, r, w, start=True, stop=True):
        Pop('pe', lambda e: e.matmul(out, lhsT=lhsT, rhs=rhs, start=start, stop=stop), r=r, w=w)

    nlat = L // 64
    steps = [(s, 3 - s) for s in range(4)] + [(4 + s, 4 + nlat - 1 - s) for s in range(nlat)]
    NS = len(steps)

    def gen_step(si):
        cf, cb = steps[si]
        cur['b'] = si % NBUF
        (L_, ur, uk, uv, uwl, ual, tw, swt, alr, eI, eE, eN, eT, gC, kk, kk2, rn, kkn, bb, km, t1, AR, BT, KTt, BhT, KhT, bon,
         TM, Vt, Gb, Gk, Nn, Pk, Qk, Xsb, Usb, Yb) = sets[si % NBUF]
        saved = P.ops
        P.ops = []
        marks = []
        lk = 'ld'
        for d, cidx in enumerate((cf, cb)):
            t0 = cidx * 64
            for nm, row in (('r', 0), ('k', 1), ('v', 2)):
                Pdma('sp', L_[nm][:, d, :, :], uT[row, :, t0:t0 + 64].rearrange("(h c) t -> c h t", h=2), r=['uTall'], w=[lk])
            Pdma('sp', L_['wl'][:, d, :], uT[3, d * 64:(d + 1) * 64, t0:t0 + 64], r=['uTall'], w=[lk])
            Pdma('sp', L_['al'][:, d, :], uT[4, d * 64:(d + 1) * 64, t0:t0 + 64], r=['uTall'], w=[lk])
        for nm, dst in (('r', ur), ('k', uk), ('v', uv)):
            d4 = v4(dst)
            Pop('pool', lambda e, d4=d4, src=L_[nm]: e.tensor_copy(out=d4[:, 0], in_=src[:, 0]), r=[lk], w=[nm])
            Pop('pool', lambda e, d4=d4, src=L_[nm]: e.tensor_copy(out=d4[:, 1], in_=src[:, 1, :, ::-1]), r=[lk], w=[nm])
        for nm, dst in (('wl', uwl), ('al', ual)):
            Pop('pool', lambda e, dst=dst, src=L_[nm]: e.tensor_copy(out=dst[:, 0, :], in_=src[:, 0, :]), r=[lk], w=[nm])
            Pop('pool', lambda e, dst=dst, src=L_[nm]: e.tensor_copy(out=dst[:, 1, :], in_=src[:, 1, ::-1]), r=[lk], w=[nm])
        ACT(tw[:], uwl[:], AF.Tanh, ['wl'], ['tw'])
        for d in range(2):
            for h in range(2):
                dh = d * 2 + h
                MM(bank[0][0:64, dh * 64:(dh + 1) * 64], tw[:, d, :], w2_sb[:, dh * 64:(dh + 1) * 64], ['tw', 'w2'], [('ps', 0)])
                MM(bank[0][0:64, 256 + dh * 64:256 + (dh + 1) * 64], a2_sb[:, dh * 64:(dh + 1) * 64], ual[:, d, :],
                   ['a2', 'al'], [('ps', 0)])
        TT('dve', swt[:], bank[0][0:64, 0:256], w0b_sb[:], ALU.add, [('ps', 0), 'w0b'], ['swt'])
        ACT(swt[:], swt[:], AF.Sigmoid, ['swt'], ['swt'])
        TT('dve', alr[:], bank[0][0:64, 256:512].rearrange("p (a t) -> p a t", t=64),
           a0f_sb[:].unsqueeze(2).broadcast_to([64, 4, 64]), ALU.add, [('ps', 0), 'a0f'], ['alr'])
        ACT(alr[:], alr[:], AF.Sigmoid, ['alr'], ['alr'])
        cbanks = (1, 0)
        for dh in range(4):
            bk = cbanks[dh // 2]
            MM(bank[bk][0:64, (dh % 2) * 192:(dh % 2) * 192 + 192], swt[:, dh * 64:(dh + 1) * 64], Tri3, ['swt', 'cst'], [('ps', bk)])
        for half in range(2):
            bk = cbanks[half]
            cv = bank[bk][0:64, 0:384].rearrange("p (a x) -> p a x", x=192)
            sl = slice(half * 2, half * 2 + 2)
            ACT(eI[:, sl, :], cv[:, :, 0:64], AF.Exp, [('ps', bk)], ['eI'], scale=-KAPPA)
            ACT(eE[:, sl, :], cv[:, :, 64:128], AF.Exp, [('ps', bk)], ['eE'], scale=-KAPPA)
            ACT(eN[:, sl, :], cv[:, :, 0:64], AF.Exp, [('ps', bk)], ['eN'], scale=KAPPA)
            ACT(gC[:, sl], cv[:, :, 128], AF.Exp, [('ps', bk)], ['gC'], scale=-KAPPA)
        TT('dve', eT[:], eN[:], gC[:].unsqueeze(2).broadcast_to([64, 4, 64]), ALU.mult, ['eN', 'gC'], ['eT'])
        marks.append(len(P.ops))
        TT('dve', v4(kk), v4(uk), prmb(0), ALU.mult, ['k', 'prm'], ['kk'])
        TT('pool', kk2[:], kk[:], kk[:], ALU.mult, ['kk'], ['kk2'])
        MM(bank[2][0:64, 0:256], ones64[:], kk2[:].rearrange("p a t -> p (a t)"), ['ones64', 'kk2'], [('ps', 2)])
        ACT(rn[:].rearrange("p a t -> p (a t)"), bank[2][0:64, 0:256], AF.Sqrt, [('ps', 2)], ['rn'])
        TS('dve', rn[:], rn[:], 1e-12, None, ALU.max, None, ['rn'], ['rn'])
        Pop('dve', lambda e: e.reciprocal(out=rn[:], in_=rn[:]), r=['rn'], w=['rn'])
        TT('dve', kkn[:], kk[:], rn[:], ALU.mult, ['kk', 'rn'], ['kkn'])
        TT('pool', bb[:], kkn[:], alr[:], ALU.mult, ['kkn', 'alr'], ['bb'])
        TS('pool', t1[:], alr[:], -1.0, None, ALU.add, None, ['alr'], ['t1'])
        TT('pool', v4(t1), v4(t1), prmb(1), ALU.mult, ['t1', 'prm'], ['t1'])
        STT(km[:], t1[:], 1.0, uk[:], ALU.add, ALU.mult, ['t1', 'k'], ['km'])
        STT(AR[:, :, 0:64], kkn[:], -1.0, eE[:], ALU.mult, ALU.mult, ['kkn', 'eE'], ['AR'])
        TT('pool', AR[:, :, 64:128], ur[:], eI[:], ALU.mult, ['r', 'eI'], ['AR'])
        TT('dve', BT[:], bb[:], eN[:], ALU.mult, ['bb', 'eN'], ['BT'])
        TT('pool', KTt[:], km[:], eN[:], ALU.mult, ['km', 'eN'], ['KTt'])
        TT('dve', BhT[:], bb[:], eT[:], ALU.mult, ['bb', 'eT'], ['BhT'])
        TT('pool', KhT[:], km[:], eT[:], ALU.mult, ['km', 'eT'], ['KhT'])
        TT('pool', t1[:], ur[:], km[:], ALU.mult, ['r', 'km', 't1'], ['t1'])
        TT('pool', v4(t1), v4(t1), prmb(2), ALU.mult, ['t1', 'prm'], ['t1'])
        MM(bank[2][0:64, 256:512], ones64[:], t1[:].rearrange("p a t -> p (a t)"), ['ones64', 't1'], [('ps', 2)])
        TT('dve', bon[:], bank[2][0:64, 256:512].rearrange("p (a t) -> p a t", t=64), uv[:], ALU.mult, [('ps', 2), 'v'], ['bon'])
        Pop('pool', lambda e: e.tensor_copy(out=kk2[:, 2:4, :], in_=bon[:, 2:4, ::-1]), r=['bon', 'kk2'], w=['kk2'])
        Pdma('pool', Bd[0, :, cf * 64:cf * 64 + 64].rearrange("(h c) t -> c h t", h=2), bon[:, 0:2, :], r=['bon'], w=['Bdall'])
        Pdma('pool', Bd[1, :, cb * 64:cb * 64 + 64].rearrange("(h c) t -> c h t", h=2), kk2[:, 2:4, :], r=['kk2'], w=['Bdall'])
        for dh in range(4):
            MM(bank[3][0:64, dh * 64:(dh + 1) * 64], BhT[:, dh, :], I64b[:], ['BhT', 'I64b'], [('ps', 3)])
            MM(bank[3][0:64, 256 + dh * 64:256 + (dh + 1) * 64], KhT[:, dh, :], I64b[:], ['KhT', 'I64b'], [('ps', 3)])
        Pop('act', lambda e: e.copy(out=TM[:].rearrange("p a b t -> p (a b t)"), in_=bank[3][0:64, :]), r=[('ps', 3)], w=['TM'])
        for dh in range(4):
            MM(bank[2][0:64, dh * 64:(dh + 1) * 64], uv[:, dh, :], I64, ['v', 'cst'], [('ps', 2)])
        Pop('dve', lambda e: e.tensor_copy(out=Vt[:].rearrange("p a t -> p (a t)"), in_=bank[2][0:64, 0:256]), r=[('ps', 2)], w=['Vt'])
        marks.append(len(P.ops))
        for dh in range(4):
            MM(bank[4][0:64, dh * 128:(dh + 1) * 128], BT[:, dh, :], AR[:, dh, :], ['BT', 'AR'], [('ps', 4)])
            MM(bank[5][0:64, dh * 128:(dh + 1) * 128], KTt[:, dh, :], AR[:, dh, :], ['KTt', 'AR'], [('ps', 5)])
        mk4 = Mk.unsqueeze(1).broadcast_to([64, 4, 128])
        TT('dve', Gb[:], bank[4][0:64, :].rearrange("p (a x) -> p a x", x=128), mk4, ALU.mult, [('ps', 4), 'cst'], ['Gb'])
        TT('dve', Gk[:], bank[5][0:64, :].rearrange("p (a x) -> p a x", x=128), mk4, ALU.mult, [('ps', 5), 'cst'], ['Gk'])
        for dh in range(4):
            MM(bank[4][0:64, 256 + dh * 64:256 + (dh + 1) * 64], AR[:, dh, 0:64], BT[:, dh, :], ['AR', 'BT'], [('ps', 4)])
        TT('dve', Nn[:], bank[4][0:64, 256:512].rearrange("p (a x) -> p a x", x=64),
           MkN.unsqueeze(1).broadcast_to([64, 4, 64]), ALU.mult, [('ps', 4), 'cst'], ['Nn'])
        P0b = Qk[3]
        Qf = Qk[2]
        Pop('act', lambda e: e.copy(out=P0b[:], in_=Gb[:, :, 0:64]), r=['Gb'], w=['P0b'])
        TT('pool', Qk[0][:], Gb[:, :, 0:64], I64.unsqueeze(1).broadcast_to([64, 4, 64]), ALU.add, ['Gb', 'cst'], [('Q', 0)])
        pk_prev = (lambda dh: P0b[:, dh, :], lambda dh: Nn[:, dh, :], ['P0b', 'Nn'])
        qi = 0
        for lv in range(1, 6):
            if lv == 3:
                marks.append(len(P.ops))
            pb = lv % 2
            Pn = Pk[pb]
            pkey = ('Pk', pb)
            bkp, bkq = (5, 4) if lv <= 2 else (6, 6)
            for dh in range(4):
                if lv < 5:
                    MM(bank[bkp][0:64, dh * 64:(dh + 1) * 64], pk_prev[1](dh), pk_prev[0](dh), pk_prev[2], [('ps', bkp)])
                MM(bank[bkp][0:64, 256 + dh * 64:256 + (dh + 1) * 64], pk_prev[0](dh), pk_prev[1](dh), pk_prev[2], [('ps', bkp)])
            if lv % 2:
                Pop('act', lambda e, Pn=Pn, bkp=bkp: e.copy(out=Pn[:].rearrange("p a b t -> p (a b t)"), in_=bank[bkp][0:64, :]),
                    r=[('ps', bkp)], w=[pkey])
            else:
                Pop('dve', lambda e, Pn=Pn, bkp=bkp: e.tensor_copy(out=Pn[:].rearrange("p a b t -> p (a b t)"), in_=bank[bkp][0:64, :]),
                    r=[('ps', bkp)], w=[pkey])
            pk_prev = (lambda dh, Pn=Pn: Pn[:, 0, dh, :], lambda dh, Pn=Pn: Pn[:, 1, dh, :], [pkey])
            for dh in range(4):
                MM(bank[bkq][0:64, dh * 64:(dh + 1) * 64], Pn[:, 1, dh, :], Qk[qi][:, dh, :], [pkey, ('Q', qi)], [('ps', bkq)])
            if lv < 5:
                TT('dve', Qk[1 - qi][:], bank[bkq][0:64, 0:256].rearrange("p (a t) -> p a t", t=64), Qk[qi][:], ALU.add,
                   [('ps', bkq), ('Q', qi)], [('Q', 1 - qi)])
                qi = 1 - qi
            else:
                TT('dve', Qf[:], bank[bkq][0:64, 0:256].rearrange("p (a t) -> p a t", t=64), Qk[qi][:], ALU.add,
                   [('ps', bkq), ('Q', qi)], ['Qf'])
        TTm = Qf
        tkey = 'Qf'
        marks.append(len(P.ops))
        for dh in range(4):
            MM(bank[7][0:64, dh * 64:(dh + 1) * 64], AR[:, dh, 0:64], ST[:, dh, :], ['AR', 'ST'], [('ps', 7)], start=True, stop=False)
            MM(bank[7][0:64, dh * 64:(dh + 1) * 64], Gk[:, dh, 0:64], Vt[:, dh, :], ['Gk', 'Vt'], [('ps', 7)], start=False, stop=True)
        Pop('act', lambda e: e.copy(out=Xsb[:].rearrange("p a t -> p (a t)"), in_=bank[7][0:64, 0:256]), r=[('ps', 7)], w=['Xsb'])
        for dh in range(4):
            MM(bank[7][0:64, 256 + dh * 64:256 + (dh + 1) * 64], TTm[:, dh, :], Xsb[:, dh, :], [tkey, 'Xsb'], [('ps', 7)])
        Pop('act', lambda e: e.copy(out=Usb[:].rearrange("p a t -> p (a t)"), in_=bank[7][0:64, 256:512]), r=[('ps', 7)], w=['Usb'])
        for dh in range(4):
            o = bank[7][0:64, dh * 64:(dh + 1) * 64]
            MM(o, ST[:, dh, :], AR[:, dh, 64:128], ['ST', 'AR'], [('ps', 7)], start=True, stop=False)
            MM(o, Usb[:, dh, :], Gb[:, dh, 64:128], ['Usb', 'Gb'], [('ps', 7)], start=False, stop=False)
            MM(o, Vt[:, dh, :], Gk[:, dh, 64:128], ['Vt', 'Gk'], [('ps', 7)], start=False, stop=True)
        yv = bank[7][0:64, 0:256].rearrange("p (a t) -> p a t", t=64)
        Pop('act', lambda e, Yb=Yb, yv=yv: e.copy(out=Yb[:, 0:2, :], in_=yv[:, 0:2, :]), r=[('ps', 7)], w=['Ysb'])
        Pop('act', lambda e, Yb=Yb, yv=yv: e.copy(out=Yb[:, 2:4, ::-1], in_=yv[:, 2:4, :]), r=[('ps', 7)], w=['Ysb'])
        Pdma('pool', Yd[0, :, cf * 64:cf * 64 + 64].rearrange("(h c) t -> c h t", h=2), Yb[:, 0:2, :], r=['Ysb'], w=['Ydall'])
        Pdma('pool', Yd[1, :, cb * 64:cb * 64 + 64].rearrange("(h c) t -> c h t", h=2), Yb[:, 2:4, :], r=['Ysb'], w=['Ydall'])
        TT('pool', Stmp[:], ST[:], gC[:].unsqueeze(2).broadcast_to([64, 4, 64]), ALU.mult, ['ST', 'gC'], ['Stmp'])
        for dh in range(4):
            o = bank[7][0:64, 256 + dh * 64:256 + (dh + 1) * 64]
            MM(o, TM[:, 0, dh, :], Usb[:, dh, :], ['TM', 'Usb'], [('ps', 7)], start=True, stop=False)
            MM(o, TM[:, 1, dh, :], Vt[:, dh, :], ['TM', 'Vt'], [('ps', 7)], start=False, stop=True)
        TT('dve', ST[:], bank[7][0:64, 256:512].rearrange("p (a t) -> p a t", t=64), Stmp[:], ALU.add, [('ps', 7), 'Stmp'], ['ST'])
        ops = P.ops
        P.ops = saved
        cur['b'] = None
        bounds = [0] + marks + [len(ops)]
        return [ops[bounds[i]:bounds[i + 1]] for i in range(5)]

    def interleave(lists):
        items = []
        for li, lst in enumerate(lists):
            n_ = len(lst)
            for k_, o in enumerate(lst):
                items.append(((k_ + 0.5) / n_, li, k_, o))
        items.sort(key=lambda t: (t[0], t[1], t[2]))
        return [t[3] for t in items]
    gen = [gen_step(si) for si in range(NS)]
    for tau in range(-KSEG, NS):
        lists = []
        if 0 <= tau < NS:
            lists.append(gen[tau][KSEG])
        for j in range(1, KSEG + 1):
            s_ = tau + j
            if 0 <= s_ < NS:
                lists.append(gen[s_][KSEG - j])
        P.ops.extend(interleave(lists))
    P.fence()
    A.release(m0)
    prm2 = A.alloc("prm2", [64, 2, 5])
    on64 = A.alloc("on64", [64, 64])
    eps2 = A.alloc("eps2", [64, 1])
    P.dma('sp', prm2[:], prm, w=['prm2'])
    P.op('pool', lambda e: e.memset(on64[:], 1.0 / 64), w=['on64'])
    P.op('pool', lambda e: e.memset(eps2[:], GN_EPS), w=['eps2'])
    yb = [A.alloc("yb%d" % i, [64, 2, 2, 512]) for i in range(2)]
    bd = [A.alloc("bd%d" % i, [64, 2, 2, 512]) for i in range(2)]
    gg = [A.alloc("gg%d" % i, [64, 2, 512]) for i in range(2)]
    y = A.alloc("y", [64, 2, 512])
    yc = A.alloc("yc", [64, 2, 512])
    y2 = A.alloc("y2", [64, 2, 512])
    rs = A.alloc("rs", [64, 2, 512])
    jobs = []
    if ctx_out:
        jobs.append((0, CTXN, 0))
    o0 = CTXN if ctx_out else 0
    for b0 in range(0, L, 512):
        jobs.append((CTXN + b0, 512, o0 + b0))
    for ji, (t0, n, orow) in enumerate(jobs):
        b = ji % 2
        P.dma('sp', yb[b][:, :, :, :n], Yd[:, :, t0:t0 + n].rearrange("d (h c) t -> c d h t", h=2), r=['Ydall'], w=[('yb', b)])
        P.dma('sp', bd[b][:, :, :, :n], Bd[:, :, t0:t0 + n].rearrange("d (h c) t -> c d h t", h=2), r=['Bdall'], w=[('bd', b)])
        P.dma('sp', gg[b][:, :, :n], uT[5, :, t0:t0 + n].rearrange("(h c) t -> c h t", h=2), r=['uTall'], w=[('gg', b)])
        TT('dve', y[:, :, :n], yb[b][:, 0, :, :n], yb[b][:, 1, :, :n], ALU.add, [('yb', b)], ['y'])
        for h in range(2):
            MM(bank[h][0:64, :n], on64[:], y[:, h, :n], ['on64', 'y'], [('ps', h)])
            TT('dve', yc[:, h, :n], y[:, h, :n], bank[h][0:64, :n], ALU.subtract, ['y', ('ps', h)], ['yc'])
        TT('pool', y2[:, :, :n], yc[:, :, :n], yc[:, :, :n], ALU.mult, ['yc'], ['y2'])
        for h in range(2):
            MM(bank[2 + h][0:64, :n], on64[:], y2[:, h, :n], ['on64', 'y2'], [('ps', 2 + h)])
            ACT(rs[:, h, :n], bank[2 + h][0:64, :n], AF.Sqrt, [('ps', 2 + h), 'eps2'], ['rs'], bias=eps2[:, 0:1])
        P.op('dve', lambda e, n=n: e.reciprocal(out=rs[:, :, :n], in_=rs[:, :, :n]), r=['rs'], w=['rs'])
        TT('dve', yc[:, :, :n], yc[:, :, :n], rs[:, :, :n], ALU.mult, ['yc', 'rs'], ['yc'])
        for h in range(2):
            TS('pool', yc[:, h, :n], yc[:, h, :n], prm2[:, h, 3:4], prm2[:, h, 4:5], ALU.mult, ALU.add, ['yc', 'prm2'], ['yc'])
        TT('dve', yc[:, :, :n], yc[:, :, :n], bd[b][:, 0, :, :n], ALU.add, ['yc', ('bd', b)], ['yc'])
        TT('dve', yc[:, :, :n], yc[:, :, :n], bd[b][:, 1, :, :n], ALU.add, ['yc', ('bd', b)], ['yc'])
        TT('pool', y2[:, :, :n], yc[:, :, :n], gg[b][:, :, :n], ALU.mult, ['yc', ('gg', b), 'y2'], ['y2'])
        P.dma('pool', out_ap(orow, n).rearrange("(h c) t -> c h t", h=2), y2[:, :, :n], r=['y2'], w=[('brb', 2)], final=FINAL_OUT)
    P.fence()


NORM_EPS = 1e-6
CTXN = 256


def phase_A(nc, P, A, bank, pfx, L, ctx_out, lam_init, x_loader, brb, do_attn=True, do_pool=True, do_rwkv=True, pre_rwkv=None):
    T = CTXN + L
    NQ = T if ctx_out else L
    di = lambda name, shape: nc.dram_tensor(pfx + name, list(shape), F32, kind="ExternalInput").ap()
    if x_loader is None:
        xT = di("xT", [128, 8, T])

        def x_loader(dst, t0, sz, key):
            P.dma('sp', dst[:, :, :sz], xT[:, :, t0:t0 + sz], w=[key])
    identA = di("identA", [128, 128])
    cT = di("cT", [128, 8, 2])
    wada = di("wada", [128, 8, 2048])
    bada = di("bada", [128, 16])
    g1 = di("g1", [128, 8])
    win = di("win", [128, 8, 1280])
    qkg = di("qkg", [128, 2])
    ropeR = di("ropeR", [128, 2, L // 64])
    ropeC = di("ropeC", [128, 2, 64])
    perm = di("perm", [128, 128])
    lamqk = di("lamqk", [128, 256])
    subg = di("subg", [128, 128])
    wpool = di("wpool", [128, 128])
    pscale = di("pscale", [128, 1])
    selw = di("selw", [128, 4])
    efix = di("efix", [128, 16])
    pT = nc.dram_tensor(pfx + "pT_scr", [7, 128, T], F32, kind="Internal").ap()
    base_mark = A.mark()
    if True:
        ones = A.alloc("ones", [128, 128])
        blk = A.alloc("blk", [128, 128])
        eps_sb = A.alloc("eps", [128, 1])
        mod_sb = A.alloc("mod", [128, 16, 2])
        A_sb = A.alloc("A", [128, 8, 2])
        NKT = T // 128
        QT = A.alloc("QT", [128, T], BF16)
        KT = A.alloc("KT", [128, T], BF16)
        V = A.alloc("V", [128, NKT, 130], BF16)
        lam_sb = A.alloc("lam", [128, 1])
        subg_sb = A.alloc("subg", [128, 128])
        ident_sb = A.alloc("identA", [128, 128])
        P.dma('sp', ident_sb[:], identA, w=['identA'])
        P.op('pool', lambda e: e.memset(ones[:], 1.0), w=['ones'])
        P.op('pool', lambda e: e.memset(blk[:], 0.0), w=['blk'])
        P.op('pool', lambda e: e.memset(blk[0:64, 0:64], 1.0), w=['blk'])
        P.op('pool', lambda e: e.memset(blk[64:128, 64:128], 1.0), w=['blk'])
        P.op('pool', lambda e: e.memset(eps_sb[:], NORM_EPS), w=['eps'])
        P.op('pool', lambda e: e.memset(V[:, :, 128:130], 1.0), w=['Vones'])
        m_a1 = A.mark()
        c_sb = A.alloc("c", [128, 8, 2])
        s_sb = A.alloc("s", [128, 8, 2])
        bada_sb = A.alloc("bada", [128, 16])
        g1_sb = A.alloc("g1", [128, 8])
        qkg_sb = A.alloc("qkg", [128, 2])
        perm_sb = A.alloc("perm", [128, 128])
        ropeR_sb = A.alloc("ropeR", [128, 2, L // 64])
        ropeC_sb = A.alloc("ropeC", [128, 2, 64])
        lamqk_sb = A.alloc("lamqk", [128, 256])
        lamt = A.alloc("lamt", [128, 4])
        wbf = A.alloc("wbf", [128, 8, 1280], BF16)
        xt = [A.alloc("xt%d" % i, [128, 8, 512]) for i in range(2)]
        sq = A.alloc("sq", [128, 8, 512])
        rstd = A.alloc("rstd", [128, 512])
        xn = A.alloc("xn", [128, 8, 512], BF16)
        ob = [A.alloc("ob%d" % i, [128, 512]) for i in range(2)]
        qk32 = A.alloc("qk32", [128, 512])
        qksq = A.alloc("qksq", [128, 512])
        qkr = A.alloc("qkr", [128, 512])
        qkn = A.alloc("qkn", [128, 512])
        cs_t = A.alloc("cs_t", [128, 2, 512])
        rtmp = A.alloc("rtmp", [128, 2, 512])

        for nm, dst, src in [('c_sb', c_sb, cT), ('bada', bada_sb, bada), ('g1', g1_sb, g1), ('qkg', qkg_sb, qkg),
                             ('perm', perm_sb, perm), ('ropeR', ropeR_sb, ropeR), ('ropeC', ropeC_sb, ropeC),
                             ('lamqk', lamqk_sb, lamqk), ('subg', subg_sb, subg)]:
            P.dma('sp', dst[:], src, w=[nm])
        lq = lamqk_sb[:].rearrange("p (a b) -> p a b", b=64)
        P.op('dve', lambda e: e.tensor_tensor(out=sq[:, 0, 0:64], in0=lq[:, 0, :], in1=lq[:, 1, :], op=ALU.mult),
             r=['lamqk'], w=['sq'])
        P.op('dve', lambda e: e.tensor_tensor(out=sq[:, 0, 64:128], in0=lq[:, 2, :], in1=lq[:, 3, :], op=ALU.mult),
             r=['lamqk'], w=['sq'])
        P.op('dve', lambda e: e.tensor_reduce(out=lamt[:, 0:2], in_=sq[:, 0, 0:128].rearrange("p (a b) -> p a b", b=64),
                                              axis=AX.X, op=ALU.add), r=['sq'], w=['lamt'])
        P.op('act', lambda e: e.activation(out=lamt[:, 2:4], in_=lamt[:, 0:2], func=AF.Exp), r=['lamt'], w=['lamt2'])
        P.op('dve', lambda e: e.tensor_tensor(out=lam_sb[:], in0=lamt[:, 2:3], in1=lamt[:, 3:4], op=ALU.subtract),
             r=['lamt2'], w=['lam'])
        P.op('dve', lambda e: e.tensor_scalar(out=lam_sb[:], in0=lam_sb[:], scalar1=lam_init, scalar2=-1.0,
                                              op0=ALU.add, op1=ALU.mult), r=['lam'], w=['lam'])
        P.op('act', lambda e: e.activation(out=s_sb[:], in_=c_sb[:], func=AF.Silu), r=['c_sb'], w=['s_sb'])
        ps_mod = bank[0][:, 0:32]
        for piece in range(4):
            b = piece % 2
            P.dma('sp', xt[b][:], wada[:, :, piece * 512:(piece + 1) * 512], w=[('xt', b)])
            for occ in range(4):
                oc = piece * 4 + occ
                for k in range(8):
                    P.op('pe', lambda e, b=b, occ=occ, oc=oc, k=k: e.matmul(
                        ps_mod[:, oc * 2:oc * 2 + 2], lhsT=xt[b][:, k, occ * 128:(occ + 1) * 128],
                        rhs=s_sb[:, k, :], start=(k == 0), stop=(k == 7)),
                        r=[('xt', b), 's_sb'], w=[('ps', 0)])
        P.op('dve', lambda e: e.tensor_tensor(
            out=mod_sb[:], in0=ps_mod.rearrange("p (a b) -> p a b", b=2),
            in1=bada_sb[:].unsqueeze(2).broadcast_to([128, 16, 2]), op=ALU.add),
            r=[('ps', 0), 'bada'], w=['mod'])
        P.op('dve', lambda e: e.tensor_scalar(out=A_sb[:], in0=mod_sb[:, 8:16, :], scalar1=1.0, scalar2=None,
                                              op0=ALU.add), r=['mod'], w=['A'])
        P.op('dve', lambda e: e.tensor_tensor(out=A_sb[:], in0=A_sb[:],
                                              in1=g1_sb[:].unsqueeze(2).broadcast_to([128, 8, 2]), op=ALU.mult),
             r=['A', 'g1'], w=['A'])
        for piece, (c0, csz) in enumerate([(0, 512), (512, 512), (1024, 256)]):
            b = piece % 2
            P.dma('sp', xt[b][:, :, :csz], win[:, :, c0:c0 + csz], w=[('xt', b)])
            P.op('pool', lambda e, b=b, c0=c0, csz=csz: e.tensor_copy(out=wbf[:, :, c0:c0 + csz], in_=xt[b][:, :, :csz]),
                 r=[('xt', b)], w=['wbf'])
        tiles = [(0, CTXN, 1)] + [(CTXN + i * 512, 512, 0) for i in range(L // 512)]
        SCR = {0: 0, 4: 1, 5: 2, 6: 3, 7: 4, 8: 5, 9: 6}
        oi = 0
        for ti, (t0, sz, j) in enumerate(tiles):
            b = ti % 2
            x_loader(xt[b], t0, sz, ('xt', b))
            P.op('act', lambda e, b=b, sz=sz: e.activation(out=sq[:, :, :sz], in_=xt[b][:, :, :sz], func=AF.Square),
                 r=[('xt', b)], w=['sq'])
            for k in range(8):
                P.op('pe', lambda e, k=k, sz=sz: e.matmul(bank[0][:, :sz], lhsT=ones[:], rhs=sq[:, k, :sz],
                                                          start=(k == 0), stop=(k == 7)),
                     r=['ones', 'sq'], w=[('ps', 0)])
            P.op('act', lambda e, sz=sz: e.activation(out=rstd[:, :sz], in_=bank[0][:, :sz], func=AF.Sqrt,
                                                      scale=1.0 / 1024, bias=eps_sb[:, 0:1]),
                 r=[('ps', 0), 'eps'], w=['rstd'])
            P.op('dve', lambda e, sz=sz: e.reciprocal(out=rstd[:, :sz], in_=rstd[:, :sz]), r=['rstd'], w=['rstd'])
            P.op('dve', lambda e, b=b, sz=sz: e.tensor_tensor(
                out=sq[:, :, :sz], in0=xt[b][:, :, :sz],
                in1=rstd[:, :sz].unsqueeze(1).broadcast_to([128, 8, sz]), op=ALU.mult),
                r=[('xt', b), 'rstd', 'sq'], w=['sq'])
            for k in range(8):
                if k % 2:
                    P.op('dve', lambda e, k=k, sz=sz, j=j: e.tensor_scalar(
                        out=xn[:, k, :sz], in0=sq[:, k, :sz], scalar1=A_sb[:, k, j:j + 1],
                        scalar2=mod_sb[:, k, j:j + 1], op0=ALU.mult, op1=ALU.add),
                        r=['sq', 'A', 'mod'], w=['xn'])
                else:
                    P.op('act', lambda e, k=k, sz=sz, j=j: e.activation(
                        out=xn[:, k, :sz], in_=sq[:, k, :sz], func=AF.Identity, scale=A_sb[:, k, j:j + 1],
                        bias=mod_sb[:, k, j:j + 1]), r=['sq', 'A', 'mod'], w=['xn'])
            if j == 0:
                r0 = (t0 - CTXN) // 64
                for cs in range(2):
                    P.op('pool', lambda e, cs=cs, r0=r0: e.tensor_tensor(
                        out=cs_t[:, cs, :].rearrange("p (r c) -> p r c", c=64),
                        in0=ropeR_sb[:, cs, r0:r0 + 8].unsqueeze(2).broadcast_to([128, 8, 64]),
                        in1=ropeC_sb[:, cs, :].unsqueeze(1).broadcast_to([128, 8, 64]), op=ALU.mult),
                        r=['ropeR', 'ropeC'], w=['cs_t'])
            for c in range(10):
                if c == 3:
                    for s in range(sz // 128):
                        kt = t0 // 128 + s
                        for k in range(8):
                            P.op('pe', lambda e, k=k, s=s: e.matmul(
                                bank[3][:, 0:128], lhsT=xn[:, k, s * 128:(s + 1) * 128], rhs=wbf[:, k, 384:512],
                                start=(k == 0), stop=(k == 7)), r=['xn', 'wbf'], w=[('ps', 3)])
                        P.op('act', lambda e, kt=kt: e.copy(out=V[:, kt, 0:128], in_=bank[3][:, 0:128]),
                             r=[('ps', 3)], w=['V'])
                    continue
                pb = 1 + (oi % 2)
                oi += 1
                for k in range(8):
                    P.op('pe', lambda e, c=c, k=k, sz=sz, pb=pb: e.matmul(
                        bank[pb][:, :sz], lhsT=wbf[:, k, c * 128:(c + 1) * 128], rhs=xn[:, k, :sz],
                        start=(k == 0), stop=(k == 7)), r=['wbf', 'xn'], w=[('ps', pb)])
                if c in SCR:
                    o = ob[oi % 2]
                    ok = ('ob', oi % 2)
                    if oi % 2:
                        P.op('act', lambda e, o=o, pb=pb, sz=sz: e.copy(out=o[:, :sz], in_=bank[pb][:, :sz]),
                             r=[('ps', pb)], w=[ok])
                    else:
                        P.op('dve', lambda e, o=o, pb=pb, sz=sz: e.tensor_copy(out=o[:, :sz], in_=bank[pb][:, :sz]),
                             r=[('ps', pb)], w=[ok])
                    P.dma('pool', pT[SCR[c], :, t0:t0 + sz], o[:, :sz], r=[ok], w=[('pT', SCR[c], ti)])
                else:
                    dst = QT if c == 1 else KT
                    gi = c - 1
                    P.op('act', lambda e, pb=pb, sz=sz: e.copy(out=qk32[:, :sz], in_=bank[pb][:, :sz]),
                         r=[('ps', pb)], w=['qk32'])
                    P.op('act', lambda e, sz=sz: e.activation(out=qksq[:, :sz], in_=qk32[:, :sz], func=AF.Square),
                         r=['qk32'], w=['qksq'])
                    P.op('pe', lambda e, sz=sz: e.matmul(bank[4][:, :sz], lhsT=blk[:], rhs=qksq[:, :sz],
                                                         start=True, stop=True), r=['blk', 'qksq'], w=[('ps', 4)])
                    P.op('act', lambda e, sz=sz: e.activation(out=qkr[:, :sz], in_=bank[4][:, :sz], func=AF.Sqrt,
                                                              scale=1.0 / 64, bias=eps_sb[:, 0:1]),
                         r=[('ps', 4), 'eps'], w=['qkr'])
                    P.op('dve', lambda e, sz=sz: e.reciprocal(out=qkr[:, :sz], in_=qkr[:, :sz]), r=['qkr'], w=['qkr'])
                    if j == 1:
                        P.op('dve', lambda e, sz=sz, gi=gi, dst=dst, t0=t0: e.scalar_tensor_tensor(
                            out=dst[:, t0:t0 + sz], in0=qk32[:, :sz], scalar=qkg_sb[:, gi:gi + 1], in1=qkr[:, :sz],
                            op0=ALU.mult, op1=ALU.mult), r=['qk32', 'qkg', 'qkr'], w=['QK'])
                    else:
                        P.op('dve', lambda e, sz=sz, gi=gi: e.scalar_tensor_tensor(
                            out=qkn[:, :sz], in0=qk32[:, :sz], scalar=qkg_sb[:, gi:gi + 1], in1=qkr[:, :sz],
                            op0=ALU.mult, op1=ALU.mult), r=['qk32', 'qkg', 'qkr'], w=['qkn'])
                        P.op('pe', lambda e, sz=sz: e.matmul(bank[5][:, :sz], lhsT=perm_sb[:], rhs=qkn[:, :sz],
                                                             start=True, stop=True), r=['perm', 'qkn'], w=[('ps', 5)])
                        P.op('pool', lambda e, sz=sz: e.tensor_tensor(out=rtmp[:, 0, :sz], in0=qkn[:, :sz],
                                                                      in1=cs_t[:, 0, :sz], op=ALU.mult),
                             r=['qkn', 'cs_t'], w=['rtmp0'])
                        P.op('dve', lambda e, sz=sz: e.tensor_tensor(out=rtmp[:, 1, :sz], in0=bank[5][:, :sz],
                                                                     in1=cs_t[:, 1, :sz], op=ALU.mult),
                             r=[('ps', 5), 'cs_t'], w=['rtmp1'])
                        P.op('dve', lambda e, sz=sz, dst=dst, t0=t0: e.tensor_tensor(
                            out=dst[:, t0:t0 + sz], in0=rtmp[:, 0, :sz], in1=rtmp[:, 1, :sz], op=ALU.add),
                            r=['rtmp0', 'rtmp1'], w=['QK'])
        P.fence()
        A.release(m_a1)
        m_ph = A.mark()
        if do_pool:
            wpool_f = A.alloc("wpool_f", [128, 128])
            wpool_b = A.alloc("wpool_b", [128, 128], BF16)
            pscale_sb = A.alloc("pscale", [128, 1])
            selw_sb = A.alloc("selw", [128, 4])
            efix_sb = A.alloc("efix", [128, 16])
            NB = 2048
            U = A.alloc("U", [128, NB + 32])
            W = [A.alloc("W%d" % i, [128, NB + 32]) for i in range(2)]
            comb = A.alloc("comb", [128, NB])
            diffb = A.alloc("diffb", [128, NB], BF16)
            pob = [A.alloc("pob%d" % i, [128, 512]) for i in range(2)]
            P.dma('sp', wpool_f[:], wpool, w=['wpool_f'])
            P.dma('sp', pscale_sb[:], pscale, w=['pscale'])
            P.dma('sp', selw_sb[:], selw, w=['selw'])
            P.dma('sp', efix_sb[:], efix, w=['efix'])
            P.op('dve', lambda e: e.tensor_copy(out=wpool_b[:], in_=wpool_f[:]), r=['wpool_f'], w=['wpool_b'])
            seqs = [(CTXN, L, (0 if not ctx_out else CTXN))]
            if ctx_out:
                seqs.append((0, CTXN, 0))
            pi = 0
            for (s0, slen, o0) in seqs:
                for b0 in range(0, slen, NB):
                    n = min(NB, slen - b0)
                    lo = max(0, b0 - 16)
                    hi = min(slen, b0 + n + 16)
                    P.op('dve', lambda e: e.memset(U[:], 0.0), w=['U'])
                    P.dma('sp', U[:, 16 + lo - b0:16 + hi - b0], pT[0, :, s0 + lo:s0 + hi], r=[('pT', 0, t) for t in range(len(tiles))], w=['U'])
                    NP = n + 32
                    src = U
                    for lv, sh in enumerate([1, 2, 4, 8]):
                        dstw = W[lv % 2]
                        P.op('dve', lambda e, src=src, dstw=dstw, sh=sh, NP=NP: e.tensor_tensor(
                            out=dstw[:, sh:NP], in0=src[:, sh:NP], in1=src[:, 0:NP - sh], op=ALU.add),
                            r=['U', ('W', 0), ('W', 1)], w=[('W', lv % 2)])
                        w_ = 2 * sh
                        off = 16 + w_ // 2 - 1
                        if lv == 0:
                            P.op('dve', lambda e, dstw=dstw, off=off, n=n, lv=lv: e.tensor_scalar(
                                out=comb[:, :n], in0=dstw[:, off:off + n], scalar1=selw_sb[:, lv:lv + 1], scalar2=None,
                                op0=ALU.mult), r=[('W', lv % 2), 'selw'], w=['comb'])
                        else:
                            P.op('dve', lambda e, dstw=dstw, off=off, n=n, lv=lv: e.scalar_tensor_tensor(
                                out=comb[:, :n], in0=dstw[:, off:off + n], scalar=selw_sb[:, lv:lv + 1], in1=comb[:, :n],
                                op0=ALU.mult, op1=ALU.add), r=[('W', lv % 2), 'selw', 'comb'], w=['comb'])
                        src = dstw
                    if b0 == 0:
                        P.op('pool', lambda e: e.tensor_tensor(out=comb[:, 0:8], in0=comb[:, 0:8], in1=efix_sb[:, 0:8],
                                                               op=ALU.mult), r=['comb', 'efix'], w=['comb'])
                    if b0 + n == slen:
                        P.op('pool', lambda e, n=n: e.tensor_tensor(out=comb[:, n - 8:n], in0=comb[:, n - 8:n],
                                                                    in1=efix_sb[:, 8:16], op=ALU.mult),
                             r=['comb', 'efix'], w=['comb'])
                    P.op('dve', lambda e, n=n: e.tensor_tensor(out=diffb[:, :n], in0=comb[:, :n], in1=U[:, 16:16 + n],
                                                               op=ALU.subtract), r=['comb', 'U'], w=['diffb'])
                    for c0 in range(0, n, 512):
                        cs = min(512, n - c0)
                        pb = 6 + pi % 2
                        o = pob[pi % 2]
                        ok = ('pob', pi % 2)
                        pi += 1
                        P.op('pe', lambda e, c0=c0, cs=cs, pb=pb: e.matmul(bank[pb][:, :cs], lhsT=wpool_b[:],
                                                                            rhs=diffb[:, c0:c0 + cs], start=True, stop=True),
                             r=['wpool_b', 'diffb'], w=[('ps', pb)])
                        P.op('act', lambda e, o=o, pb=pb, cs=cs: e.activation(out=o[:, :cs], in_=bank[pb][:, :cs],
                                                                               func=AF.Copy, scale=pscale_sb[:, 0:1]),
                             r=[('ps', pb), 'pscale'], w=[ok])
                        P.dma('pool', brb.dst(0, o0 + b0 + c0, cs), o[:, :cs], r=[ok], w=[('brb', 0)])
            P.fence()
            A.release(m_ph)
        if do_attn:
            PT = [[A.alloc("PT%d_%d" % (m, i), [128, 512], BF16) for i in range(2)] for m in range(2)]
            osb = A.alloc("osb", [128, 2, 4, 130])
            Qz = [A.alloc("Qz%d" % i, [128, 2, 512], BF16) for i in range(2)]
            for i in range(2):
                P.op('dve', lambda e, i=i: e.memset(Qz[i][:], 0.0), w=[('Qz', i)])
            rec = A.alloc("rec", [128, 2, 4])
            o1 = A.alloc("o1", [128, 4, 128])
            o2 = A.alloc("o2", [128, 4, 128])
            ssq = A.alloc("ssq", [128, 4])
            oT = A.alloc("oT", [128, 512])
            def oacc(m, qs):
                i = m * 4 + qs
                return bank[4 + i // 3][:, (i % 3) * 130:(i % 3) * 130 + 130], ('ps', 4 + i // 3)
            qjobs = []
            if ctx_out:
                qjobs.append((0, CTXN, (0, 2), 0))
            for i in range(L // 512):
                qjobs.append((CTXN + i * 512, 512, (0, NKT), (CTXN if ctx_out else 0) + i * 512))
            si = 0
            for qji, (q0, nq, (k0, k1), orow) in enumerate(qjobs):
                nqs = nq // 128
                started = set()
                qz = Qz[qji % 2]
                qzk = ('Qz', qji % 2)
                P.op('dve', lambda e, qz=qz, q0=q0, nq=nq: e.tensor_copy(out=qz[0:64, 0, :nq], in_=QT[0:64, q0:q0 + nq]),
                     r=['QK', qzk], w=[qzk])
                P.op('pool', lambda e, qz=qz, q0=q0, nq=nq: e.tensor_copy(out=qz[64:128, 1, :nq], in_=QT[64:128, q0:q0 + nq]),
                     r=['QK', qzk], w=[qzk])
                def emit_S(kt):
                    for m in range(2):
                        sb_ = kt % 2
                        pb = m * 2 + sb_
                        P.op('pe', lambda e, m=m, kt=kt, nq=nq, pb=pb, qz=qz: e.matmul(
                            bank[pb][:, :nq], lhsT=KT[:, kt * 128:(kt + 1) * 128],
                            rhs=qz[:, m, :nq], start=True, stop=True),
                            r=['QK', qzk], w=[('ps', pb)])
                        P.op('act', lambda e, m=m, sb_=sb_, pb=pb, nq=nq: e.activation(
                            out=PT[m][sb_][:, :nq], in_=bank[pb][:, :nq], func=AF.Exp, scale=0.125),
                            r=[('ps', pb)], w=[('PT', m, sb_)])

                def emit_PV(kt):
                    for m in range(2):
                        sb_ = kt % 2
                        for qs in range(nqs):
                            oap, okey = oacc(m, qs)
                            first_in_bank = (kt == k0) and (okey not in started)
                            started.add(okey)
                            P.op('pe', lambda e, m=m, sb_=sb_, qs=qs, kt=kt, oap=oap, fib=first_in_bank, k1=k1: e.matmul(
                                oap, lhsT=PT[m][sb_][:, qs * 128:(qs + 1) * 128], rhs=V[:, kt, :],
                                start=fib, stop=(kt == k1 - 1)),
                                r=[('PT', m, sb_), 'V', 'Vones'], w=[okey])
                emit_S(k0)
                for kt in range(k0, k1):
                    if kt + 1 < k1:
                        emit_S(kt + 1)
                    emit_PV(kt)
                for m in range(2):
                    for qs in range(nqs):
                        oap, okey = oacc(m, qs)
                        P.op('dve' if (m + qs) % 2 else 'act',
                             (lambda e, m=m, qs=qs, oap=oap: e.tensor_copy(out=osb[:, m, qs, :], in_=oap)) if (m + qs) % 2
                             else (lambda e, m=m, qs=qs, oap=oap: e.copy(out=osb[:, m, qs, :], in_=oap)),
                             r=[okey], w=['osb'])
                P.op('dve', lambda e, nqs=nqs: e.reciprocal(out=rec[:, :, :nqs], in_=osb[:, :, :nqs, 128]),
                     r=['osb'], w=['rec'])
                P.op('dve', lambda e, nqs=nqs: e.tensor_scalar(out=rec[:, 1, :nqs], in0=rec[:, 1, :nqs],
                                                               scalar1=lam_sb[:, 0:1], scalar2=None, op0=ALU.mult),
                     r=['rec', 'lam'], w=['rec'])
                P.op('dve', lambda e, nqs=nqs: e.tensor_tensor(
                    out=o1[:, :nqs, :], in0=osb[:, 0, :nqs, 0:128],
                    in1=rec[:, 0, :nqs].unsqueeze(2).broadcast_to([128, nqs, 128]), op=ALU.mult),
                    r=['osb', 'rec'], w=['o1'])
                P.op('pool', lambda e, nqs=nqs: e.tensor_tensor(
                    out=o2[:, :nqs, :], in0=osb[:, 1, :nqs, 0:128],
                    in1=rec[:, 1, :nqs].unsqueeze(2).broadcast_to([128, nqs, 128]), op=ALU.mult),
                    r=['osb', 'rec'], w=['o2'])
                P.op('dve', lambda e, nqs=nqs: e.tensor_tensor(out=o1[:, :nqs, :], in0=o1[:, :nqs, :], in1=o2[:, :nqs, :],
                                                               op=ALU.add), r=['o1', 'o2'], w=['o1'])
                P.op('pool', lambda e, nqs=nqs: e.tensor_tensor(out=o2[:, :nqs, :], in0=o1[:, :nqs, :], in1=o1[:, :nqs, :],
                                                                op=ALU.mult), r=['o1', 'o2'], w=['o2'])
                P.op('dve', lambda e, nqs=nqs: e.tensor_reduce(out=ssq[:, :nqs], in_=o2[:, :nqs, :], axis=AX.X, op=ALU.add),
                     r=['o2'], w=['ssq'])
                P.op('act', lambda e, nqs=nqs: e.activation(out=ssq[:, :nqs], in_=ssq[:, :nqs], func=AF.Sqrt,
                                                            scale=1.0 / 128, bias=eps_sb[:, 0:1]),
                     r=['ssq', 'eps'], w=['ssq'])
                P.op('dve', lambda e, nqs=nqs: e.reciprocal(out=ssq[:, :nqs], in_=ssq[:, :nqs]), r=['ssq'], w=['ssq'])
                P.op('dve', lambda e, nqs=nqs: e.tensor_tensor(
                    out=o1[:, :nqs, :], in0=o1[:, :nqs, :],
                    in1=ssq[:, :nqs].unsqueeze(2).broadcast_to([128, nqs, 128]), op=ALU.mult),
                    r=['o1', 'ssq'], w=['o1'])
                P.op('pool', lambda e, nqs=nqs: e.tensor_tensor(
                    out=o2[:, :nqs, :], in0=o1[:, :nqs, :],
                    in1=subg_sb[:].unsqueeze(1).broadcast_to([128, nqs, 128]), op=ALU.mult),
                    r=['o1', 'subg', 'o2'], w=['o2'])
                for qs in range(nqs):
                    P.op('pe', lambda e, qs=qs: e.matmul(bank[7][:, qs * 128:(qs + 1) * 128], lhsT=o2[:, qs, :], rhs=ident_sb[:],
                                                         start=True, stop=True), r=['o2', 'identA'], w=[('ps', 7)])
                P.op('act', lambda e, nq=nq: e.copy(out=oT[:, :nq], in_=bank[7][:, :nq]), r=[('ps', 7)], w=['oT'])
                P.dma('sp', brb.dst(1, orow, nq), oT[:, :nq], r=['oT'], w=[('brb', 1)])
            P.fence()
        if pre_rwkv is not None:
            pre_rwkv()
        if do_rwkv:
            A.release(base_mark)
            rwkv_phase(nc, P, A, bank, pT, L, ctx_out, (lambda c0, n: brb.dst(2, c0, n)), pfx)
        A.release(base_mark)


NORM_EPS = 1e-6


def phase_B(nc, P, A, bank, pfx, NT_LAT, NT_CTX, x_load, gath, off_lat, x_store, NEXP=32):
    NT = NT_LAT + NT_CTX
    di = lambda name, shape: nc.dram_tensor(pfx + name, list(shape), F32, kind="ExternalInput").ap()
    selq = di("selq", [128, 4])
    cT = di("cT", [128, 8, 2])
    wada = di("wada", [128, 8, 6144])
    bada = di("bada", [128, 48])
    g12 = di("g12", [128, 2, 8])
    wgate = di("wgate", [128, 8, 3072])
    wbr = di("wbr", [128, 12, 1024])
    wout = di("wout", [128, 8, 1024])
    wrt = di("wrt", [128, 8, 36])
    wg = di("wg", [NEXP, 128, 8, 512])
    wu = di("wu", [NEXP, 128, 8, 512])
    wd = di("wd", [NEXP, 128, 4, 1024])
    selE = di("selE", [32, 32 * 128])
    ident = di("ident", [128, 128])
    x2s = nc.dram_tensor(pfx + "x2_scr", [128, 8, NT], F32, kind="Internal").ap()
    xn2s = nc.dram_tensor(pfx + "xn2_scr", [128, 8, NT], BF16, kind="Internal").ap()
    gTs = nc.dram_tensor(pfx + "gT_scr", [32, NT], F32, kind="Internal").ap()
    base_mark = A.mark()

    def TT(eng, out, a, b, op, r, w):
        P.op(eng, lambda e: e.tensor_tensor(out=out, in0=a, in1=b, op=op), r=r, w=w)

    def TS(eng, out, a, s1, s2, op0, op1, r, w):
        if op1 is None:
            P.op(eng, lambda e: e.tensor_scalar(out=out, in0=a, scalar1=s1, scalar2=None, op0=op0), r=r, w=w)
        else:
            P.op(eng, lambda e: e.tensor_scalar(out=out, in0=a, scalar1=s1, scalar2=s2, op0=op0, op1=op1), r=r, w=w)

    def STT(out, a, s, b, op0, op1, r, w):
        P.op('dve', lambda e: e.scalar_tensor_tensor(out=out, in0=a, scalar=s, in1=b, op0=op0, op1=op1), r=r, w=w)

    def ACT(out, a, func, r, w, scale=1.0, bias=None):
        if bias is None:
            P.op('act', lambda e: e.activation(out=out, in_=a, func=func, scale=scale), r=r, w=w)
        else:
            P.op('act', lambda e: e.activation(out=out, in_=a, func=func, scale=scale, bias=bias), r=r, w=w)

    def MM(out, lhsT, rhs, r, w, start=True, stop=True):
        P.op('pe', lambda e: e.matmul(out, lhsT=lhsT, rhs=rhs, start=start, stop=stop), r=r, w=w)

    def RED(out, a, op, r, w):
        P.op('dve', lambda e: e.tensor_reduce(out=out, in_=a, axis=AX.X, op=op), r=r, w=w)

    if True:
        ones = A.alloc("ones", [128, 128])
        selq_sb = A.alloc("selq", [128, 4])
        P.dma('sp', selq_sb[:], selq, w=['selq'])
        eps_sb = A.alloc("eps", [128, 1])
        mod_sb = A.alloc("mod", [128, 48, 2])
        A1 = A.alloc("A1", [128, 8, 2])
        A2 = A.alloc("A2", [128, 8, 2])
        g12_sb = A.alloc("g12", [128, 2, 8])
        ident_sb = A.alloc("ident", [128, 128])
        wrt_sb = A.alloc("wrt", [128, 8, 36])
        P.op('pool', lambda e: e.memset(ones[:], 1.0), w=['ones'])
        P.op('pool', lambda e: e.memset(eps_sb[:], NORM_EPS), w=['eps'])
        P.dma('sp', g12_sb[:], g12, w=['g12'])
        P.dma('sp', ident_sb[:], ident, w=['ident'])
        P.dma('sp', wrt_sb[:], wrt, w=['wrt'])
        mB1 = A.mark()
        c_sb = A.alloc("c", [128, 8, 2])
        s_sb = A.alloc("s", [128, 8, 2])
        bada_sb = A.alloc("bada", [128, 48])
        wgate_b = A.alloc("wgate_b", [128, 8, 3072], BF16)
        wbr_b = A.alloc("wbr_b", [128, 12, 1024], BF16)
        wout_b = A.alloc("wout_b", [128, 8, 1024], BF16)
        xt = [A.alloc("xt%d" % i, [128, 8, 512]) for i in range(2)]
        sq = A.alloc("sq", [128, 8, 512])
        rstd = A.alloc("rstd", [128, 512])
        xn1 = A.alloc("xn1", [128, 8, 512], BF16)
        brf = A.alloc("brf", [128, 4, 512])
        gTt = A.alloc("gTt", [32, 512])
        brb = A.alloc("brb", [128, 12, 512], BF16)
        sig = [A.alloc("sig%d" % i, [128, 512]) for i in range(2)]
        mrg = A.alloc("mrg", [128, 2, 512])
        mk_ = A.mark()
        mrgb = A.alloc("mrgb", [128, 8, 512], BF16)
        A.release(mk_)
        brsel = A.alloc("brsel", [128, 4, 512])
        x2 = A.alloc("x2", [128, 8, 512])
        rt = A.alloc("rt", [128, 80])
        g32 = A.alloc("g32", [128, 32])
        P.dma('sp', c_sb[:], cT, w=['c_sb'])
        P.dma('sp', bada_sb[:], bada, w=['bada'])
        ACT(s_sb[:], c_sb[:], AF.Silu, ['c_sb'], ['s_sb'])
        for piece in range(12):
            b = piece % 2
            P.dma('sp', xt[b][:], wada[:, :, piece * 512:(piece + 1) * 512], w=[('xt', b)])
            for occ in range(4):
                oc = piece * 4 + occ
                for k in range(8):
                    MM(bank[0][:, oc * 2:oc * 2 + 2], xt[b][:, k, occ * 128:(occ + 1) * 128], s_sb[:, k, :],
                       [('xt', b), 's_sb'], [('ps', 0)], start=(k == 0), stop=(k == 7))
        TT('dve', mod_sb[:], bank[0][:, 0:96].rearrange("p (a b) -> p a b", b=2),
           bada_sb[:].unsqueeze(2).broadcast_to([128, 48, 2]), ALU.add, [('ps', 0), 'bada'], ['mod'])
        for (Ax, m_scale, gi) in ((A1, 1, 0), (A2, 4, 1)):
            TS('dve', Ax[:], mod_sb[:, m_scale * 8:(m_scale + 1) * 8, :], 1.0, None, ALU.add, None, ['mod'], ['Ax%d' % gi])
            TT('dve', Ax[:], Ax[:], g12_sb[:, gi, :].unsqueeze(2).broadcast_to([128, 8, 2]), ALU.mult, ['Ax%d' % gi, 'g12'], ['Ax%d' % gi])
        wi = 0
        for (src, dstw, nk, ncol, key) in ((wgate, wgate_b, 8, 3072, 'wgate_b'), (wbr, wbr_b, 12, 1024, 'wbr_b'), (wout, wout_b, 8, 1024, 'wout_b')):
            for c0 in range(0, ncol, 512):
                for kb in range(0, nk, 8):
                    kn = min(8, nk - kb)
                    b = wi % 2
                    wi += 1
                    P.dma('sp', xt[b][:, :kn, :], src[:, kb:kb + kn, c0:c0 + 512], w=[('xt', b)])
                    P.op('pool' if wi % 2 else 'dve', lambda e, b=b, kn=kn, kb=kb, c0=c0, dstw=dstw: e.tensor_copy(
                        out=dstw[:, kb:kb + kn, c0:c0 + 512], in_=xt[b][:, :kn, :]), r=[('xt', b)], w=[key])
        tiles = [(i * 512, 512, 0) for i in range(NT_LAT // 512)]
        if NT_CTX:
            tiles.append((NT_LAT, NT_CTX, 1))

        def norm_mod(src, sz, j, Ax, m_shift, out_fn, okeys, srckeys):
            P.op('act', lambda e: e.activation(out=sq[:, :, :sz], in_=src[:, :, :sz], func=AF.Square), r=srckeys, w=['sq'])
            for k in range(8):
                MM(bank[0][:, :sz], ones[:], sq[:, k, :sz], ['ones', 'sq'], [('ps', 0)], start=(k == 0), stop=(k == 7))
            ACT(rstd[:, :sz], bank[0][:, :sz], AF.Sqrt, [('ps', 0), 'eps'], ['rstd'], scale=1.0 / 1024, bias=eps_sb[:, 0:1])
            P.op('dve', lambda e: e.reciprocal(out=rstd[:, :sz], in_=rstd[:, :sz]), r=['rstd'], w=['rstd'])
            TT('dve', sq[:, :, :sz], src[:, :, :sz], rstd[:, :sz].unsqueeze(1).broadcast_to([128, 8, sz]), ALU.mult,
               srckeys + ['rstd', 'sq'], ['sq'])
            for k in range(8):
                if k % 2:
                    TS('dve', out_fn(k), sq[:, k, :sz], Ax[:, k, j:j + 1],
                       mod_sb[:, m_shift * 8 + k, j:j + 1], ALU.mult, ALU.add, ['sq', 'Ax0', 'Ax1', 'mod'], okeys)
                else:
                    ACT(out_fn(k), sq[:, k, :sz], AF.Identity, ['sq', 'Ax0', 'Ax1', 'mod'], okeys, scale=Ax[:, k, j:j + 1],
                        bias=mod_sb[:, m_shift * 8 + k, j:j + 1])

        pi = 0
        for ti, (t0, sz, j) in enumerate(tiles):
            b = ti % 2
            x_load(xt[b], t0, sz, ('xt', b))
            for n3 in range(3):
                for q in range(4):
                    col0 = (off_lat + q * NT_LAT + t0) if j == 0 else (q * 64 + (t0 - NT_LAT))
                    P.dma('sp', brf[:, :, :sz], gath.gsrc(n3, col0, sz), r=[('gath', n3)], w=['brf'])
                    if q == 0:
                        TS('dve', brsel[:, :, :sz], brf[:, :, :sz], selq_sb[:, 0:1], None, ALU.mult, None, ['brf', 'selq'], ['mrgb'])
                    else:
                        STT(brsel[:, :, :sz], brf[:, :, :sz], selq_sb[:, q:q + 1], brsel[:, :, :sz], ALU.mult, ALU.add,
                            ['brf', 'selq', 'mrgb'], ['mrgb'])
                P.op('pool', lambda e, sz=sz, n3=n3: e.tensor_copy(out=brb[:, n3 * 4:(n3 + 1) * 4, :sz], in_=brsel[:, :, :sz]),
                     r=['mrgb'], w=['brb'])
            norm_mod(xt[b], sz, j, A1, 0, lambda k, sz=sz: xn1[:, k, :sz], ['xn1'], [('xt', b)])
            for dc in range(8):
                for n in range(3):
                    pg = bank[1 + pi % 2]
                    pgk = ('ps', 1 + pi % 2)
                    pbk = bank[3 + pi % 2]
                    pbkk = ('ps', 3 + pi % 2)
                    sg_ = sig[pi % 2]
                    sgk = ('sig', pi % 2)
                    pi += 1
                    for k in range(8):
                        MM(pg[:, :sz], wgate_b[:, k, n * 1024 + dc * 128:n * 1024 + (dc + 1) * 128], xn1[:, k, :sz],
                           ['wgate_b', 'xn1'], [pgk], start=(k == 0), stop=(k == 7))
                    for kc in range(4):
                        MM(pbk[:, :sz], wbr_b[:, n * 4 + kc, dc * 128:(dc + 1) * 128], brb[:, n * 4 + kc, :sz],
                           ['wbr_b', 'brb'], [pbkk], start=(kc == 0), stop=(kc == 3))
                    ACT(sg_[:, :sz], pg[:, :sz], AF.Sigmoid, [pgk], [sgk])
                    if n == 0:
                        TT('dve', mrg[:, dc % 2, :sz], sg_[:, :sz], pbk[:, :sz], ALU.mult, [sgk, pbkk], ['mrg'])
                    else:
                        TT('dve', sg_[:, :sz], sg_[:, :sz], pbk[:, :sz], ALU.mult, [sgk, pbkk], [sgk])
                        TT('pool', mrg[:, dc % 2, :sz], mrg[:, dc % 2, :sz], sg_[:, :sz], ALU.add, ['mrg', sgk], ['mrg'])
                P.op('act', lambda e, dc=dc, sz=sz: e.copy(out=mrgb[:, dc, :sz], in_=mrg[:, dc % 2, :sz]), r=['mrg'], w=['mrgb'])
            for dc in range(8):
                pb_ = bank[5 + dc % 2]
                pk_ = ('ps', 5 + dc % 2)
                for k in range(8):
                    MM(pb_[:, :sz], wout_b[:, k, dc * 128:(dc + 1) * 128], mrgb[:, k, :sz], ['wout_b', 'mrgb'], [pk_],
                       start=(k == 0), stop=(k == 7))
                STT(x2[:, dc, :sz], pb_[:, :sz], mod_sb[:, 2 * 8 + dc, j:j + 1], xt[b][:, dc, :sz], ALU.mult, ALU.add,
                    [pk_, 'mod', ('xt', b)], ['x2'])
            P.dma('pool', x2s[:, :, t0:t0 + sz], x2[:, :, :sz], r=['x2'], w=['x2s'])
            xn2f = xt[b]
            xfk = ('xt', b)
            norm_mod(x2, sz, j, A2, 3, lambda k, sz=sz, xn2f=xn2f: xn2f[:, k, :sz], [xfk], ['x2'])
            P.op('act', lambda e, sz=sz, xn2f=xn2f: e.copy(out=xn1[:, :, :sz], in_=xn2f[:, :, :sz]), r=[xfk], w=['xn1'])
            P.dma('pool', xn2s[:, :, t0:t0 + sz], xn1[:, :, :sz], r=['xn1'], w=['xn2s'])
            for s0 in range(0, sz, 128):
                ns = min(128, sz - s0)
                for k in range(8):
                    MM(bank[7][:ns, 0:36], xn2f[:, k, s0:s0 + ns], wrt_sb[:, k, :], [xfk, 'wrt'], [('ps', 7)],
                       start=(k == 0), stop=(k == 7))
                lg = rt[:ns, 0:36]
                P.op('dve', lambda e, ns=ns: e.tensor_copy(out=rt[:ns, 0:36], in_=bank[7][:ns, 0:36]), r=[('ps', 7)], w=['rt'])
                gmax = rt[:ns, 36:37]
                RED(gmax, rt[:ns, 0:4], ALU.max, ['rt'], ['rt'])
                ohg = rt[:ns, 37:41]
                TS('dve', ohg, rt[:ns, 0:4], gmax, None, ALU.is_equal, None, ['rt'], ['rt'])
                ngm = rt[:ns, 41:42]
                TS('dve', ngm, gmax, -1.0, None, ALU.mult, None, ['rt'], ['rt'])
                eg = rt[:ns, 42:46]
                ACT(eg, rt[:ns, 0:4], AF.Exp, ['rt'], ['rt'], bias=ngm)
                pgr = rt[:ns, 46:47]
                RED(pgr, eg, ALU.add, ['rt'], ['rt'])
                P.op('dve', lambda e, pgr=pgr: e.reciprocal(out=pgr, in_=pgr), r=['rt'], w=['rt'])
                TT('dve', g32[:ns, :].rearrange("p (g e) -> p g e", e=8), rt[:ns, 4:36].rearrange("p (g e) -> p g e", e=8),
                   ohg.unsqueeze(2).broadcast_to([ns, 4, 8]), ALU.mult, ['rt'], ['g32'])
                les = rt[:ns, 47:55]
                RED(les, g32[:ns, :].rearrange("p (g e) -> p e g", e=8), ALU.add, ['g32'], ['rt'])
                top1 = rt[:ns, 55:56]
                RED(top1, les, ALU.max, ['rt'], ['rt'])
                oh1 = rt[:ns, 56:64]
                TS('dve', oh1, les, top1, None, ALU.is_equal, None, ['rt'], ['rt'])
                le2 = rt[:ns, 64:72]
                STT(le2, oh1, -1e30, les, ALU.mult, ALU.add, ['rt'], ['rt'])
                top2 = rt[:ns, 72:73]
                RED(top2, le2, ALU.max, ['rt'], ['rt'])
                oh2 = rt[:ns, 73:81] if False else None
                d12 = rt[:ns, 41:42]
                TT('dve', d12, top1, top2, ALU.subtract, ['rt'], ['rt'])
                ga = rt[:ns, 42:43]
                gb = rt[:ns, 43:44]
                ACT(ga, d12, AF.Sigmoid, ['rt'], ['rt'])
                ACT(gb, d12, AF.Sigmoid, ['rt'], ['rt'], scale=-1.0)
                TT('dve', rt[:ns, 42:44], rt[:ns, 42:44], pgr.broadcast_to([ns, 2]), ALU.mult, ['rt'], ['rt'])
                TS('dve', le2, le2, top2, gb, ALU.is_equal, ALU.mult, ['rt'], ['rt'])
                STT(les, oh1, ga, le2, ALU.mult, ALU.add, ['rt'], ['rt'])
                TT('dve', g32[:ns, :].rearrange("p (g e) -> p g e", e=8), ohg.unsqueeze(2).broadcast_to([ns, 4, 8]),
                   les.unsqueeze(1).broadcast_to([ns, 4, 8]), ALU.mult, ['rt', 'g32'], ['g32'])
                MM(bank[7][0:32, 64:64 + ns], g32[:ns, :], ident_sb[:ns, :ns], ['g32', 'ident'], [('ps', 7)])
                P.op('act', lambda e, s0=s0, ns=ns: e.copy(out=gTt[:, s0:s0 + ns], in_=bank[7][0:32, 64:64 + ns]),
                     r=[('ps', 7)], w=['gTt'])
            P.dma('pool', gTs[:, t0:t0 + sz], gTt[:, :sz], r=['gTt'], w=['gTs'])
        P.fence()
        A.release(mB1)
        selE_sb = A.alloc("selE", [32, 32 * 128])
        P.dma('sp', selE_sb[:], selE, w=['selE'])
        lat_ = tiles[:NT_LAT // 512]
        groups = [lat_[i:i + 2] for i in range(0, len(lat_), 2)]
        if NT_CTX:
            groups[-1] = groups[-1] + [tiles[-1]]
        GMAX = max(sum(t[1] for t in g) for g in groups)
        yacc = A.alloc("yacc", [128, 8, GMAX])
        xn2 = A.alloc("xn2g", [128, 8, GMAX], BF16)
        gT = A.alloc("gTg", [32, GMAX])
        wst = [A.alloc("wst%d" % i, [128, 4, 512]) for i in range(4)]
        wgb = [A.alloc("wgb%d" % i, [128, 8, 512], BF16) for i in range(2)]
        wub = [A.alloc("wub%d" % i, [128, 8, 512], BF16) for i in range(2)]
        wdb = [A.alloc("wdb%d" % i, [128, 4, 1024], BF16) for i in range(2)]
        hs = [A.alloc("hs%d" % i, [128, 512]) for i in range(2)]
        actb = [A.alloc("actb%d" % i, [128, 4, 512], BF16) for i in range(2)]
        x2r = A.alloc("x2r", [128, 8, 512])
        si = 0
        ai = 0
        hi_ = 0
        for gi, grp in enumerate(groups):
            g0 = grp[0][0]
            gsz = sum(t[1] for t in grp)
            P.dma('sp', xn2[:, :, :gsz], xn2s[:, :, g0:g0 + gsz], r=['xn2s'], w=['xn2g'])
            P.dma('sp', gT[:, :gsz], gTs[:, g0:g0 + gsz], r=['gTs'], w=['gTg'])
            for e_ in range(NEXP):
                wb = e_ % 2
                pieces = [(wg[e_, :, 0:4, :], wgb[wb][:, 0:4, :], ('wgb', wb)), (wg[e_, :, 4:8, :], wgb[wb][:, 4:8, :], ('wgb', wb)),
                          (wu[e_, :, 0:4, :], wub[wb][:, 0:4, :], ('wub', wb)), (wu[e_, :, 4:8, :], wub[wb][:, 4:8, :], ('wub', wb)),
                          (wd[e_, :, :, 0:512], wdb[wb][:, :, 0:512], ('wdb', wb)), (wd[e_, :, :, 512:1024], wdb[wb][:, :, 512:1024], ('wdb', wb))]
                for (src, dst, key) in pieces:
                    sb_ = si % 4
                    si += 1
                    P.dma('sp', wst[sb_][:], src, w=[('wst', sb_)])
                    eng = ('pool', 'dve', 'act')[si % 3] if False else ('pool' if si % 2 else 'act')
                    if eng == 'act':
                        P.op('act', lambda e, dst=dst, sb_=sb_: e.copy(out=dst, in_=wst[sb_][:]), r=[('wst', sb_)], w=[key])
                    else:
                        P.op('pool', lambda e, dst=dst, sb_=sb_: e.tensor_copy(out=dst, in_=wst[sb_][:]), r=[('wst', sb_)], w=[key])
                for (t0, sz, j) in grp:
                    ti = tiles.index((t0, sz, j))
                    lo = t0 - g0
                    MM(bank[0][:, :sz], selE_sb[:, e_ * 128:(e_ + 1) * 128], gT[:, lo:lo + sz], ['selE', 'gTg'], [('ps', 0)])
                    ab = actb[ai % 2]
                    ak = ('actb', ai % 2)
                    ai += 1
                    for fc in range(4):
                        pgb = bank[1 + fc % 2]
                        pgk = ('ps', 1 + fc % 2)
                        pub = bank[3 + fc % 2]
                        puk = ('ps', 3 + fc % 2)
                        for k in range(8):
                            MM(pgb[:, :sz], wgb[wb][:, k, fc * 128:(fc + 1) * 128], xn2[:, k, lo:lo + sz],
                               [('wgb', wb), 'xn2g'], [pgk], start=(k == 0), stop=(k == 7))
                        for k in range(8):
                            MM(pub[:, :sz], wub[wb][:, k, fc * 128:(fc + 1) * 128], xn2[:, k, lo:lo + sz],
                               [('wub', wb), 'xn2g'], [puk], start=(k == 0), stop=(k == 7))
                        h_ = hs[hi_ % 2]
                        hk = ('hs', hi_ % 2)
                        hi_ += 1
                        ACT(h_[:, :sz], pgb[:, :sz], AF.Silu, [pgk], [hk])
                        TT('dve', h_[:, :sz], h_[:, :sz], pub[:, :sz], ALU.mult, [hk, puk], [hk])
                        TT('dve', ab[:, fc, :sz], h_[:, :sz], bank[0][:, :sz], ALU.mult, [hk, ('ps', 0)], [ak])
                    for dc in range(8):
                        pdb = bank[5 + dc % 3]
                        pdk = ('ps', 5 + dc % 3)
                        for fc in range(4):
                            MM(pdb[:, :sz], wdb[wb][:, fc, dc * 128:(dc + 1) * 128], ab[:, fc, :sz], [('wdb', wb), ak], [pdk],
                               start=(fc == 0), stop=(fc == 3))
                        if e_ == 0:
                            P.op('act', lambda e, dc=dc, lo=lo, sz=sz, pdb=pdb: e.copy(out=yacc[:, dc, lo:lo + sz], in_=pdb[:, :sz]),
                                 r=[pdk], w=['yacc'])
                        else:
                            TT('pool' if False else 'dve', yacc[:, dc, lo:lo + sz], yacc[:, dc, lo:lo + sz], pdb[:, :sz], ALU.add,
                               ['yacc', pdk], ['yacc'])
            for (t0, sz, j) in grp:
                lo = t0 - g0
                P.dma('sp', x2r[:, :, :sz], x2s[:, :, t0:t0 + sz], r=['x2s'], w=['x2r'])
                for dc in range(8):
                    STT(x2r[:, dc, :sz], yacc[:, dc, lo:lo + sz], mod_sb[:, 5 * 8 + dc, j:j + 1], x2r[:, dc, :sz], ALU.mult, ALU.add,
                        ['yacc', 'mod', 'x2r'], ['x2r'])
                x_store(x2r, t0, sz, ['x2r'])
        P.fence()
        A.release(base_mark)


POOL_WINDOWS = (2, 4, 8, 16)
def fm(a):
    return np.ascontiguousarray(a.reshape(8, 128, *a.shape[1:]).swapaxes(0, 1))
def colsel(hg):
    c = []
    c += list(range(hg * 128, hg * 128 + 128))
    c += list(range(512 + hg * 128, 512 + hg * 128 + 128))
    c += list(range(1024 + hg * 128, 1024 + hg * 128 + 128))
    c += list(range(1536 + hg * 128, 1536 + hg * 128 + 128))
    for j in range(3):
        c += list(range(2048 + j * 512 + hg * 128, 2048 + j * 512 + hg * 128 + 128))
    c += list(range(2048 + 1536, 2048 + 1920))
    return np.array(c)
def rope_tables(L):
    nrow = L // 64
    inv = (10000.0 ** (-np.arange(16, dtype=np.float32) / 16)).astype(np.float32)
    R = np.ones((128, 2, nrow), np.float32); C = np.ones((128, 2, 64), np.float32)
    perm = np.zeros((128, 128), np.float32)
    rows = np.arange(nrow, dtype=np.float32); cols = np.arange(64, dtype=np.float32)
    for p in range(128):
        d = p % 64
        blk = d // 16
        f = inv[d % 16]
        sign = -1.0 if blk % 2 == 0 else 1.0
        partner = p + 16 if blk % 2 == 0 else p - 16
        perm[partner, p] = 1.0
        if blk < 2:
            ang = (rows * f).astype(np.float32)
            R[p, 0] = np.cos(ang); R[p, 1] = sign * np.sin(ang)
        else:
            ang = (cols * f).astype(np.float32)
            C[p, 0] = np.cos(ang); C[p, 1] = sign * np.sin(ang)
    return R, C, perm
def edge_fix(w, L):
    t = np.arange(L)
    lo = np.clip(t - w // 2, 0, L - 1); hi = np.clip(t + w // 2 - 1, 0, L - 1)
    ratio = (w / (hi - lo + 1)).astype(np.float32)
    return np.concatenate([ratio[:8], ratio[-8:]])
def inputs_A(inp, l, b, hg, L, lam_init, x=None, ctx=None):
    if x is None:
        x = inp['x'][b, :L]; ctx = inp['ctx'][b]
    xa = np.concatenate([ctx, x], 0)
    R, C, perm = rope_tables(L)
    cs = colsel(hg)
    w = POOL_WINDOWS[hg]
    selw = np.zeros((128, 4), np.float32); selw[:, hg] = 1.0 / w
    d = dict(
        xT=fm(np.ascontiguousarray(xa.T)),
        cT=fm(np.stack([inp['c'][b], inp['c_ctx']], 1)),
        wada=fm(inp['w_ada'][l][:, :2048]),
        bada=np.ascontiguousarray(inp['b_ada'][l][:2048].reshape(16, 128).T),
        g1=np.ascontiguousarray(inp['norm1_g'][l].reshape(8, 128).T),
        win=fm(inp['w_in'][l][:, cs]),
        qkg=np.stack([np.tile(inp['q_norm_g'][l], 2), np.tile(inp['k_norm_g'][l], 2)], 1).astype(np.float32),
        ropeR=R, ropeC=C, perm=perm,
        lamqk=np.ascontiguousarray(np.broadcast_to(inp['lambda_qk'][l].reshape(1, 256), (128, 256))),
        subg=np.ascontiguousarray(np.broadcast_to((inp['subln_g'][l] * np.float32(1 - lam_init)).reshape(1, 128), (128, 128))).astype(np.float32),
        wpool=np.ascontiguousarray(inp['pool_w'][l][hg]),
        pscale=np.ascontiguousarray(inp['pool_scale'][l][hg * 128:(hg + 1) * 128].reshape(128, 1)),
        selw=selw,
        efix=np.ascontiguousarray(np.broadcast_to(edge_fix(w, L).reshape(1, 16), (128, 16))),
    )
    return d

def rw_consts():
    i = np.arange(64)
    incl = (i[:, None] <= i[None, :]).astype(np.float32)
    strict = (i[:, None] < i[None, :]).astype(np.float32)
    ones = np.ones((64, 64), np.float32)
    MkN = (i[None, :] < i[:, None]).astype(np.float32)
    return np.concatenate([incl, strict, ones, strict, incl, MkN, np.eye(64, dtype=np.float32)], 1)
def inputs_rw(inp, l, hg):
    mu = inp['shift_mu'][l]
    cmu = np.zeros((128, 6, 2), np.float32)
    p = np.arange(128)
    for g in range(3):
        cmu[:, g, :] = mu[:, g * 512 + hg * 128 + p].T
    for g, base in ((3, 1536), (4, 1664), (5, 1792)):
        cmu[:, g, :] = mu[:, base + p].T
    heads = [2 * hg, 2 * hg + 1]
    w2 = np.zeros((64, 2, 2, 64), np.float32); a2 = np.zeros((64, 2, 2, 64), np.float32)
    w0b = np.zeros((2, 2, 64), np.float32); a0f = np.zeros((64, 2, 2), np.float32)
    prm = np.zeros((64, 2, 5), np.float32)
    for h, hd in enumerate(heads):
        cs = slice(hd * 64, hd * 64 + 64)
        for d in range(2):
            w2[:, d, h, :] = inp['decay_w2'][l][d][:, cs]
            a2[:, d, h, :] = inp['aaa_a2'][l][d][:, cs]
            w0b[d, h, :] = inp['decay_w0'][l][d][cs]
            a0f[:, d, h] = inp['aaa_a0'][l][d][cs]
        prm[:, h, 0] = inp['k_k'][l][cs]; prm[:, h, 1] = inp['k_a'][l][cs]; prm[:, h, 2] = inp['r_k'][l][hd]
        prm[:, h, 3] = inp['gn_w'][l][cs]; prm[:, h, 4] = inp['gn_b'][l][cs]
    return dict(rw_cmu=cmu, rw_g2=np.ascontiguousarray(inp['gate_w2'][l][:, hg * 128:(hg + 1) * 128]),
                rw_w2=w2.reshape(64, 256), rw_a2=a2.reshape(64, 256),
                rw_w0b=np.ascontiguousarray(np.broadcast_to(w0b.reshape(1, 256), (64, 256))),
                rw_a0f=a0f.reshape(64, 4), rw_prm=prm, rw_cst=rw_consts())


def weights_B(inp, l):
    selE = np.zeros((32, 32, 128), np.float32)
    for e in range(32): selE[e, e, :] = 1.0
    return dict(
        wada=fm(inp['w_ada'][l]),
        bada=np.ascontiguousarray(inp['b_ada'][l].reshape(48, 128).T),
        g12=np.ascontiguousarray(np.stack([inp['norm1_g'][l].reshape(8, 128).T, inp['norm2_g'][l].reshape(8, 128).T], 1)),
        wgate=fm(inp['w_in'][l][:, 3968:]),
        wbr=np.ascontiguousarray(inp['w_br'][l].reshape(12, 128, 1024).transpose(1, 0, 2)),
        wout=fm(inp['w_out'][l]),
        wrt=fm(np.concatenate([inp['w_router_group'][l], inp['w_router_expert'][l]], 1)),
        wg=np.ascontiguousarray(inp['w_exp_gate'][l].reshape(32, 8, 128, 512).transpose(0, 2, 1, 3)),
        wu=np.ascontiguousarray(inp['w_exp_up'][l].reshape(32, 8, 128, 512).transpose(0, 2, 1, 3)),
        wd=np.ascontiguousarray(inp['w_exp_down'][l].reshape(32, 4, 128, 1024).transpose(0, 2, 1, 3)),
        selE=selE.reshape(32, 4096), ident=np.eye(128, dtype=np.float32))
def acts_B(xa, br, cvec, c_ctx):
    NT = xa.shape[0]
    return dict(xT=fm(np.ascontiguousarray(xa.T)),
                brT=np.ascontiguousarray(br.T.reshape(12, 128, NT).transpose(1, 0, 2)),
                cT=fm(np.stack([cvec, c_ctx], 1)))


GROUPS = [[0, 1, 2, 3], [4, 5, 6, 7]]


CC_COLS = 2048


class BrStore:
    def __init__(self, nc, name, NQ, ctx_out):
        self.splits = ([(0, 256)] if ctx_out else []) + [(c0, min(CC_COLS, NQ - c0)) for c0 in range(256 if ctx_out else 0, NQ, CC_COLS)]
        self.b = {}
        self.g = {}
        for n in range(3):
            for ci, (c0, cs) in enumerate(self.splits):
                self.b[(n, ci)] = nc.dram_tensor("%s_b%d_%d" % (name, n, ci), [128, cs], F32, kind="Internal").ap()
                self.g[(n, ci)] = nc.dram_tensor("%s_g%d_%d" % (name, n, ci), [4 * 128, cs], F32, kind="Internal").ap()

    def _find(self, col0, ncols):
        for ci, (c0, cs) in enumerate(self.splits):
            if c0 <= col0 and col0 + ncols <= c0 + cs:
                return ci, col0 - c0
        raise AssertionError(("chunk straddle", col0, ncols))

    def dst(self, n, col0, ncols):
        ci, o = self._find(col0, ncols)
        return self.b[(n, ci)][:, o:o + ncols]

    def gsrc(self, n, col0, ncols):
        ci, o = self._find(col0, ncols)
        return self.g[(n, ci)].rearrange("(g p) t -> p g t", g=4)[:, :, o:o + ncols]

    def exchange(self, P, rows=(0, 1, 2)):
        for key in self.b:
            if key[0] in rows:
                P.cc(self.b[key], self.g[key], GROUPS, r=[('brb', key[0])], w=[('gath', key[0])])


class XStore:
    def __init__(self, nc, name, NL):
        self.NL = NL
        NT0 = NL + 64
        self.splits = [(c0, min(CC_COLS, NL - c0)) for c0 in range(0, NL, CC_COLS)] + [(NL, 64)]
        self.b = {}
        self.g = {}
        for k in range(8):
            for ci, (c0, cs) in enumerate(self.splits):
                self.b[(k, ci)] = nc.dram_tensor("%s_b%d_%d" % (name, k, ci), [128, cs], F32, kind="Internal").ap()
                self.g[(k, ci)] = nc.dram_tensor("%s_g%d_%d" % (name, k, ci), [4 * 128, cs], F32, kind="Internal").ap()

    def _find(self, col0, ncols):
        for ci, (c0, cs) in enumerate(self.splits):
            if c0 <= col0 and col0 + ncols <= c0 + cs:
                return ci, col0 - c0
        raise AssertionError(("chunk straddle", col0, ncols))

    def store(self, P, src_tile, t0, sz, rkeys):
        ci, o = self._find(t0, sz)
        for k in range(8):
            P.dma('pool', self.b[(k, ci)][:, o:o + sz], src_tile[:, k, :sz], r=rkeys, w=['xnew'])

    def load_local(self, P, dst_tile, t0, sz, key):
        ci, o = self._find(t0, sz)
        for k in range(8):
            P.dma('sp', dst_tile[:, k, :sz], self.b[(k, ci)][:, o:o + sz], r=['xnew'], w=[key])

    def load_gathered(self, P, dst_tile, dcol, rank, t0, sz, key):
        ci, o = self._find(t0, sz)
        for k in range(8):
            P.dma('sp', dst_tile[:, k, dcol:dcol + sz], self.g[(k, ci)][rank * 128:(rank + 1) * 128, o:o + sz], r=['xg'], w=[key])

    def exchange(self, P):
        for key in self.b:
            P.cc(self.b[key], self.g[key], GROUPS, r=['xnew'], w=['xg'])


def build_fused(L=16384, nexp=32, stop_after=99):
    nc = bass.Bass("TRN2", target_bir_lowering=False)
    nc.allow_low_precision("bf16 matmul operands, fp32 accumulation")
    P = Prog(nc)
    A = Arena(nc)
    T = 256 + L
    NL = L // 4
    NT0 = NL + 64
    lam = [0.8 - 0.6 * math.exp(-0.3 * l) for l in range(2)]
    with contextlib.ExitStack() as st:
        bank = [st.enter_context(nc.psum_tensor("bank%d" % i, [128, 512], F32)) for i in range(8)]
        xout = nc.dram_tensor("xout", [128, 8, NL], F32, kind="ExternalOutput").ap()

        def finish():
            stats = P.emit()
            stats['sbuf_peak'] = A.peak
            return nc, stats
        br0 = BrStore(nc, "br0", T, True)
        phase_A(nc, P, A, bank, "A0_", L, True, lam[0], None, br0, pre_rwkv=lambda: br0.exchange(P, (0, 1)))
        P.fence()
        if stop_after == 1:
            return finish()
        br0.exchange(P, (2,))
        P.fence()
        if stop_after == 2:
            return finish()
        xsh = nc.dram_tensor("xsh", [128, 8, NT0], F32, kind="ExternalInput").ap()
        xs = XStore(nc, "xs", NL)

        def x_load0(dst, t0, sz, key):
            P.dma('sp', dst[:, :, :sz], xsh[:, :, t0:t0 + sz], w=[key])
        phase_B(nc, P, A, bank, "B0_", NL, 64, x_load0, br0, 256, lambda src, t0, sz, rk: xs.store(P, src, t0, sz, rk), NEXP=nexp)
        if stop_after == 3:
            return finish()
        xs.exchange(P)
        P.fence()
        if stop_after == 4:
            return finish()

        def x_loader(dst, t0, sz, key):
            if t0 < 256:
                for r in range(4):
                    xs.load_gathered(P, dst, r * 64, r, NL, 64, key)
            else:
                tt = t0 - 256
                xs.load_gathered(P, dst, 0, tt // NL, tt % NL, sz, key)
        br1 = BrStore(nc, "br1", L, False)
        phase_A(nc, P, A, bank, "A1_", L, False, lam[1], x_loader, br1, pre_rwkv=lambda: br1.exchange(P, (0, 1)))
        P.fence()
        if stop_after == 5:
            return finish()
        br1.exchange(P, (2,))
        P.fence()
        if stop_after == 6:
            return finish()

        def x_store1(src, t0, sz, rk):
            P.dma('pool', xout[:, :, t0:t0 + sz], src[:, :, :sz], r=rk, final=True)
        phase_B(nc, P, A, bank, "B1_", NL, 0, lambda dst, t0, sz, key: xs.load_local(P, dst, t0, sz, key), br1, 0, x_store1, NEXP=nexp)
        return finish()


def fused_inputs(inp, L, nexp=32, names=None):
    inp = {k: np.asarray(v) for k, v in inp.items()}
    NL = L // 4
    x = inp['x'][:, :L]
    ctx = inp['ctx']
    lam = [0.8 - 0.6 * math.exp(-0.3 * l) for l in range(2)]
    WB = [weights_B(inp, l) for l in range(2)]
    ident = np.eye(128, dtype=np.float32)
    maps = []
    for i in range(8):
        b, hg = i // 4, i % 4
        d = {}
        for l in range(2):
            a = inputs_A(inp, l, b, hg, L, lam[l], x=x[b], ctx=ctx[b])
            if l == 1:
                a.pop('xT')
            a.update(inputs_rw(inp, l, hg))
            a['identA'] = ident
            for k, v in a.items():
                d["A%d_" % l + k] = v
            for k, v in WB[l].items():
                d["B%d_" % l + k] = v[:nexp] if k in ('wg', 'wu', 'wd') else v
            d["B%d_cT" % l] = fm(np.stack([inp['c'][b], inp['c_ctx']], 1))
            selq = np.zeros((128, 4), np.float32)
            selq[:, hg] = 1.0
            d["B%d_selq" % l] = selq
        xa = np.concatenate([x[b, hg * NL:(hg + 1) * NL], ctx[b, hg * 64:(hg + 1) * 64]], 0)
        d['xsh'] = fm(np.ascontiguousarray(xa.T))
        if names is not None:
            d = {k: v for k, v in d.items() if k in names}
        maps.append(d)
    return maps


def fused_gather(results, L):
    NL = L // 4
    out = np.empty((2, L, 1024), np.float32)
    for i in range(8):
        b, q = i // 4, i % 4
        o = np.asarray(results[i]['xout']).transpose(1, 0, 2).reshape(1024, NL).T
        out[b, q * NL:(q + 1) * NL] = o
    return out


def kernel(**inp):
    inp = {k: np.asarray(v) for k, v in inp.items()}
    L = inp['x'].shape[1]
    nc, _ = build_fused(L)
    maps = fused_inputs(inp, L)
    res = run_bass_kernel_spmd(nc, maps, core_ids=list(range(8)))
    del maps
    return fused_gather(res.results, L)
```

```python
import math
import contextlib


import numpy as np
import concourse.bass as bass
import concourse.mybir as mybir
from concourse.bass_utils import run_bass_kernel_spmd

F32 = mybir.dt.float32
BF16 = mybir.dt.bfloat16
I32 = mybir.dt.int32
AF = mybir.ActivationFunctionType
ALU = mybir.AluOpType
AX = mybir.AxisListType

SEM_LIMIT = 8000
DMA_POOL = 40


class Prog:
    def __init__(self, nc):
        self.nc = nc
        self.ops = []

    def op(self, eng, fn, r=(), w=()):
        self.ops.append(dict(eng=eng, fn=fn, r=tuple(r), w=tuple(w), dma=False, final=False))

    def dma(self, eng, out, in_, r=(), w=(), final=False, **kw):
        def fn(e, out=out, in_=in_, kw=kw):
            return e.dma_start(out=out, in_=in_, **kw)
        self.ops.append(dict(eng=eng, fn=fn, r=tuple(r), w=tuple(w), dma=True, final=final))

    def cc(self, ins_ap, out_ap, groups, r=(), w=()):
        def fn(e, ins_ap=ins_ap, out_ap=out_ap, groups=groups):
            return e.collective_compute("AllGather", ALU.bypass, replica_groups=groups, ins=[ins_ap], outs=[out_ap])
        self.ops.append(dict(eng='pool', fn=fn, r=tuple(r), w=tuple(w), dma=True, final=False, cc=True))

    def fence(self):
        self.ops.append(dict(eng=None, fn=None, r=(), w=(), dma=False, final=False, fence=True))

    def emit(self):
        nc = self.nc
        raw_ops = self.ops
        ops = []
        fence_after = {}
        fence_pos = []
        for o in raw_ops:
            if o.get('fence'):
                fence_pos.append(len(ops))
            else:
                ops.append(o)
        self.ops = ops
        n = len(ops)
        fence_deps_at = {}
        prev = 0
        for fp in fence_pos:
            last = {}
            dm = set()
            for i in range(prev, fp):
                o = ops[i]
                if o['dma']:
                    dm.add(i)
                else:
                    last[o['eng']] = i
            fence_deps_at[fp] = set(last.values()) | dm
            prev = fp
        last_w = {}
        readers = {}
        deps = [None] * n
        cur_fence = set()
        first_after = {}
        for i, o in enumerate(ops):
            if i in fence_deps_at:
                cur_fence = fence_deps_at[i]
                first_after = {}
            d = {}

            def add(j, raw):
                d[j] = d.get(j, False) or raw
            for k in o['r']:
                if k in last_w:
                    add(last_w[k], True)
            for k in o['w']:
                if k in last_w:
                    add(last_w[k], False)
                for tok, j in readers.get(k, {}).items():
                    add(j, False)
            keep = set()
            for j, raw in d.items():
                if j == i:
                    continue
                oj = ops[j]
                if (not oj['dma']) and (not o['dma']) and oj['eng'] == o['eng']:
                    if o['eng'] == 'pe':
                        continue
                    if not raw:
                        continue
                keep.add(j)
            tok_e = o['eng']
            if cur_fence and tok_e not in first_after:
                first_after[tok_e] = i
                for j in cur_fence:
                    if ops[j]['dma'] or ops[j]['eng'] != tok_e:
                        keep.add(j)
            deps[i] = keep
            for k in o['r']:
                tok = ('d', i) if o['dma'] else o['eng']
                readers.setdefault(k, {})[tok] = i
            for k in o['w']:
                last_w[k] = i
                readers[k] = {}
        needed = [False] * n
        for i in range(n):
            for j in deps[i]:
                needed[j] = True
        engs = ['pe', 'act', 'dve', 'pool', 'sp']
        cnt = {e: 0 for e in engs}
        sig = [None] * n
        dma_uses = [0] * DMA_POOL
        dma_last = [None] * DMA_POOL
        ndma = 0
        semkeys = set()
        for i, o in enumerate(ops):
            if o.get('cc'):
                ncc_ = getattr(self, '_ncc', 0) + 1
                self._ncc = ncc_
                sig[i] = (('cc', 0), ncc_)
                semkeys.add(('cc', 0))
            elif o['dma']:
                j = ndma % DMA_POOL
                ndma += 1
                dma_uses[j] += 1
                if dma_last[j] is not None:
                    deps[i].add(dma_last[j])
                dma_last[j] = i
                sig[i] = (('dma', j), 16 * dma_uses[j])
                semkeys.add(('dma', j))
            elif needed[i]:
                e = o['eng']
                c = cnt[e]
                cnt[e] += 1
                sk = (e, c // SEM_LIMIT)
                sig[i] = (sk, c % SEM_LIMIT + 1)
                semkeys.add(sk)
        finals = [i for i, o in enumerate(ops) if o['final']]
        seen = {e: {} for e in engs}
        streams = {e: [] for e in engs}
        for i, o in enumerate(ops):
            e = o['eng']
            waits = {}
            for j in deps[i]:
                sk, v = sig[j]
                if seen[e].get(sk, 0) >= v:
                    continue
                waits[sk] = max(waits.get(sk, 0), v)
            for sk, v in waits.items():
                seen[e][sk] = v
            streams[e].append((list(waits.items()), o['fn'], sig[i]))
        fw = {}
        for i in finals:
            sk, v = sig[i]
            if seen['sp'].get(sk, 0) >= v:
                continue
            fw[sk] = max(fw.get(sk, 0), v)
        streams['sp'].append((list(fw.items()), None, None))
        self.stats = dict(n_ops=n, cnt=dict(cnt), ndma=ndma,
                          nwaits={e: sum(len(s[0]) for s in streams[e]) for e in engs})
        semkeys = sorted(semkeys, key=str)
        import contextlib
        with contextlib.ExitStack() as st:
            sems = {}
            for sk in semkeys:
                sems[sk] = st.enter_context(nc.semaphore("s_%s_%s" % (sk[0], sk[1])))
            block = st.enter_context(nc.Block())

            def run(engine, items):
                for waits, fn, sg in items:
                    for sk, v in waits:
                        engine.wait_ge(sems[sk], v)
                    if fn is None:
                        continue
                    ins = fn(engine)
                    if sg is not None:
                        inc = 16 if sg[0][0] == 'dma' else 1
                        ins.then_inc(sems[sg[0]], inc)

            @block.tensor
            def _(e):
                run(e, streams['pe'])

            @block.scalar
            def _(e):
                run(e, streams['act'])

            @block.vector
            def _(e):
                run(e, streams['dve'])

            @block.gpsimd
            def _(e):
                run(e, streams['pool'])

            @block.sync
            def _(e):
                run(e, streams['sp'])
        return self.stats


class Arena:
    LO = 16512
    HI = 229376

    def __init__(self, nc):
        self.nc = nc
        self.top = self.LO
        self.n = 0
        self.peak = self.LO

    def alloc(self, name, shape, dt=F32):
        esz = {F32: 4, BF16: 2, I32: 4}[dt]
        nb = esz
        for s_ in shape[1:]:
            nb *= s_
        off = (self.top + 63) // 64 * 64
        assert off + nb <= self.HI, ("SBUF overflow", name, off + nb)
        self.top = off + nb
        self.peak = max(self.peak, self.top)
        self.n += 1
        return self.nc.alloc_sbuf_tensor_at("%s_%d" % (name, self.n), list(shape), dt, offset=off)

    def mark(self):
        return self.top

    def release(self, m):
        self.top = m


GN_EPS = 64e-5
FINAL_OUT = False
KAPPA = 0.6065306597126334
CTXN = 256


def rwkv_phase(nc, P, A, bank, pT, L, ctx_out, out_ap, pfx=''):
    T = CTXN + L
    di = lambda name, shape: nc.dram_tensor(pfx + name, list(shape), F32, kind="ExternalInput").ap()
    cmu = di("rw_cmu", [128, 6, 2])
    g2s = di("rw_g2", [128, 128])
    w2s = di("rw_w2", [64, 256])
    a2s = di("rw_a2", [64, 256])
    w0b = di("rw_w0b", [64, 256])
    a0f = di("rw_a0f", [64, 4])
    prm = di("rw_prm", [64, 2, 5])
    cst = di("rw_cst", [64, 448])
    uT = nc.dram_tensor(pfx + "uT_scr", [6, 128, T], F32, kind="Internal").ap()
    Yd = nc.dram_tensor(pfx + "Yd_scr", [2, 128, T], F32, kind="Internal").ap()
    Bd = nc.dram_tensor(pfx + "Bd_scr", [2, 128, T], F32, kind="Internal").ap()

    def TT(eng, out, a, b, op, r, w):
        P.op(eng, lambda e: e.tensor_tensor(out=out, in0=a, in1=b, op=op), r=r, w=w)

    def TS(eng, out, a, s1, s2, op0, op1, r, w):
        if op1 is None:
            P.op(eng, lambda e: e.tensor_scalar(out=out, in0=a, scalar1=s1, scalar2=None, op0=op0), r=r, w=w)
        else:
            P.op(eng, lambda e: e.tensor_scalar(out=out, in0=a, scalar1=s1, scalar2=s2, op0=op0, op1=op1), r=r, w=w)

    def STT(out, a, s, b, op0, op1, r, w):
        P.op('dve', lambda e: e.scalar_tensor_tensor(out=out, in0=a, scalar=s, in1=b, op0=op0, op1=op1), r=r, w=w)

    def ACT(out, a, func, r, w, scale=1.0, bias=None):
        if bias is None:
            P.op('act', lambda e: e.activation(out=out, in_=a, func=func, scale=scale), r=r, w=w)
        else:
            P.op('act', lambda e: e.activation(out=out, in_=a, func=func, scale=scale, bias=bias), r=r, w=w)

    def MM(out, lhsT, rhs, r, w, start=True, stop=True):
        P.op('pe', lambda e: e.matmul(out, lhsT=lhsT, rhs=rhs, start=start, stop=stop), r=r, w=w)

    m0 = A.mark()
    cmu_sb = A.alloc("cmu", [128, 6, 2])
    c0_sb = A.alloc("c0", [128, 6])
    g2_sb = A.alloc("g2", [128, 128])
    raw = [A.alloc("raw%d" % i, [128, 6, 514]) for i in range(2)]
    ush = A.alloc("ush", [128, 6, 512])
    sg = A.alloc("sg", [128, 512])
    P.dma('sp', cmu_sb[:], cmu, w=['cmu'])
    P.dma('sp', g2_sb[:], g2s, w=['g2'])
    TT('dve', c0_sb[:], cmu_sb[:, :, 0], cmu_sb[:, :, 1], ALU.add, ['cmu'], ['c0'])
    TS('dve', c0_sb[:], c0_sb[:], -1.0, 1.0, ALU.mult, ALU.add, ['c0'], ['c0'])
    seqs = [(0, CTXN), (CTXN, L)]
    ti = 0
    for (s0, slen) in seqs:
        for b0 in range(0, slen, 512):
            n = min(512, slen - b0)
            rb = raw[ti % 2]
            rk = ('raw', ti % 2)
            ti += 1
            lo = max(0, b0 - 1)
            hi = min(slen, b0 + n + 1)
            if b0 == 0 or b0 + n == slen:
                P.op('pool', lambda e, rb=rb: e.memset(rb[:], 0.0), w=[rk])
            P.dma('sp', rb[:, :, 1 + lo - b0:1 + hi - b0], pT[1:7, :, s0 + lo:s0 + hi].rearrange("g p t -> p g t"),
                  r=['pTall'], w=[rk])
            for g in range(6):
                eng = 'dve'
                TS('pool', ush[:, g, :n], rb[:, g, 1:1 + n], c0_sb[:, g:g + 1], None, ALU.mult, None, [rk, 'c0'], ['ush'])
                STT(ush[:, g, :n], rb[:, g, 0:n], cmu_sb[:, g, 0:1], ush[:, g, :n], ALU.mult, ALU.add, [rk, 'cmu', 'ush'], ['ush'])
                STT(ush[:, g, :n], rb[:, g, 2:2 + n], cmu_sb[:, g, 1:2], ush[:, g, :n], ALU.mult, ALU.add, [rk, 'cmu', 'ush'], ['ush'])
            ACT(sg[:, :n], ush[:, 5, :n], AF.Sigmoid, ['ush'], ['sg'])
            MM(bank[0][:, :n], g2_sb[:], sg[:, :n], ['g2', 'sg'], [('ps', 0)])
            P.op('act', lambda e, n=n: e.copy(out=ush[:, 5, :n], in_=bank[0][:, :n]), r=[('ps', 0), 'ush'], w=['ush'])
            P.dma('pool', uT[:, :, s0 + b0:s0 + b0 + n].rearrange("g p t -> p g t"), ush[:, :, :n], r=['ush'], w=['uTall'])
    P.fence()
    A.release(m0)
    w2_sb = A.alloc("w2", [64, 256])
    a2_sb = A.alloc("a2", [64, 256])
    w0b_sb = A.alloc("w0b", [64, 256])
    a0f_sb = A.alloc("a0f", [64, 4])
    prm_sb = A.alloc("prm", [64, 2, 5])
    cst_sb = A.alloc("cst", [64, 448])
    ones64 = A.alloc("ones64", [64, 64])
    for nm, dst, src in [('w2', w2_sb, w2s), ('a2', a2_sb, a2s), ('w0b', w0b_sb, w0b), ('a0f', a0f_sb, a0f),
                         ('prm', prm_sb, prm), ('cst', cst_sb, cst)]:
        P.dma('sp', dst[:], src, w=[nm])
    P.op('pool', lambda e: e.memset(ones64[:], 1.0), w=['ones64'])
    Tri3 = cst_sb[:, 0:192]
    Mk = cst_sb[:, 192:320]
    MkN = cst_sb[:, 320:384]
    I64 = cst_sb[:, 384:448]
    I64b = A.alloc("I64b", [64, 64], BF16)
    P.op('dve', lambda e: e.tensor_copy(out=I64b[:], in_=cst_sb[:, 384:448]), r=['cst'], w=['I64b'])
    ST = A.alloc("ST", [64, 4, 64])
    Stmp = A.alloc("Stmp", [64, 4, 64])
    P.op('pool', lambda e: e.memset(ST[:], 0.0), w=['ST'])
    KSEG = 4
    NBUF = KSEG + 1
    f4 = lambda name: A.alloc(name, [64, 4, 64])

    shr = dict(L_=dict(r=A.alloc("ld_r", [64, 2, 2, 64]), k=A.alloc("ld_k", [64, 2, 2, 64]), v=A.alloc("ld_v", [64, 2, 2, 64]),
                       wl=A.alloc("ld_wl", [64, 2, 64]), al=A.alloc("ld_al", [64, 2, 64])),
               uwl=A.alloc("uwl", [64, 2, 64]), ual=A.alloc("ual", [64, 2, 64]), tw=A.alloc("tw", [64, 2, 64]),
               swt=A.alloc("swt", [64, 256]), kk=f4("kk"), kk2=f4("kk2"), rn=f4("rn"), kkn=f4("kkn"), bb=f4("bb"), km=f4("km"),
               t1=f4("t1"), BhT=A.alloc("BhT", [64, 4, 64], BF16), KhT=A.alloc("KhT", [64, 4, 64], BF16), Xsb=f4("Xsb"), Usb=f4("Usb"), Ysb=f4("Ysb"))

    def alloc_set(i):
        n_ = lambda x: "%s_%d" % (x, i)
        return (shr['L_'], f4(n_("ur")), f4(n_("uk")), f4(n_("uv")), shr['uwl'], shr['ual'],
                shr['tw'], shr['swt'], f4(n_("alr")),
                f4(n_("eI")), f4(n_("eE")), f4(n_("eN")), f4(n_("eT")), A.alloc(n_("gC"), [64, 4]),
                shr['kk'], shr['kk2'], shr['rn'], shr['kkn'], shr['bb'], shr['km'], shr['t1'],
                A.alloc(n_("AR"), [64, 4, 128]), f4(n_("BT")), f4(n_("KTt")), shr['BhT'], shr['KhT'], f4(n_("bon")),
                A.alloc(n_("TM"), [64, 2, 4, 64]), f4(n_("Vt")), A.alloc(n_("Gb"), [64, 4, 128]), A.alloc(n_("Gk"), [64, 4, 128]),
                A.alloc(n_("Nn"), [64, 4, 64], BF16), [A.alloc(n_("Pk%d" % j), [64, 2, 4, 64], BF16) for j in range(2)],
                [A.alloc(n_("Q0"), [64, 4, 64], BF16), A.alloc(n_("Q1"), [64, 4, 64], BF16), f4(n_("Qf")), A.alloc(n_("P0b"), [64, 4, 64], BF16)],
                shr['Xsb'], shr['Usb'], shr['Ysb'])
    sets = [alloc_set(i) for i in range(NBUF)]
    prmb = lambda j: prm_sb[:, :, j:j + 1].unsqueeze(1).broadcast_to([64, 2, 2, 64])
    v4 = lambda t: t[:].rearrange("p (d h) t -> p d h t", d=2)
    SHARED = set(['w2', 'a2', 'w0b', 'a0f', 'prm', 'cst', 'ones64', 'uTall', 'Bdall', 'Ydall', 'ST', 'Stmp', 'I64b',
                  'ld', 'wl', 'al', 'tw', 'swt', 'kk', 'kk2', 'rn', 'kkn', 'bb', 'km', 't1', 'BhT', 'KhT', 'Xsb', 'Usb', 'Ysb'])
    cur = {'b': None}

    def kmap(keys):
        if cur['b'] is None:
            return list(keys)
        return [k if (k in SHARED or (isinstance(k, tuple) and k[0] == 'ps')) else ('rw', k, cur['b']) for k in keys]
    _op, _dma = P.op, P.dma

    def Pop(eng, fn, r=(), w=()):
        _op(eng, fn, r=kmap(r), w=kmap(w))

    def Pdma(eng, out, in_, r=(), w=(), **kw):
        _dma(eng, out, in_, r=kmap(r), w=kmap(w), **kw)

    def TT(eng, out, a, b, op, r, w):
        Pop(eng, lambda e: e.tensor_tensor(out=out, in0=a, in1=b, op=op), r=r, w=w)

    def TS(eng, out, a, s1, s2, op0, op1, r, w):
        if op1 is None:
            Pop(eng, lambda e: e.tensor_scalar(out=out, in0=a, scalar1=s1, scalar2=None, op0=op0), r=r, w=w)
        else:
            Pop(eng, lambda e: e.tensor_scalar(out=out, in0=a, scalar1=s1, scalar2=s2, op0=op0, op1=op1), r=r, w=w)

    def STT(out, a, s, b, op0, op1, r, w):
        Pop('dve', lambda e: e.scalar_tensor_tensor(out=out, in0=a, scalar=s, in1=b, op0=op0, op1=op1), r=r, w=w)

    def ACT(out, a, func, r, w, scale=1.0, bias=None):
        if bias is None:
            Pop('act', lambda e: e.activation(out=out, in_=a, func=func, scale=scale), r=r, w=w)
        else:
            Pop('act', lambda e: e.activation(out=out, in_=a, func=func, scale=scale, bias=bias), r=r, w=w)

    def MM(out, lhsT, rhs, r, w, start=True, stop=True):
        Pop('pe', lambda e: e.matmul(out, lhsT=lhsT, rhs=rhs, start=start, stop=stop), r=r, w=w)

    nlat = L // 64
    steps = [(s, 3 - s) for s in range(4)] + [(4 + s, 4 + nlat - 1 - s) for s in range(nlat)]
    NS = len(steps)

    def gen_step(si):
        cf, cb = steps[si]
        cur['b'] = si % NBUF
        (L_, ur, uk, uv, uwl, ual, tw, swt, alr, eI, eE, eN, eT, gC, kk, kk2, rn, kkn, bb, km, t1, AR, BT, KTt, BhT, KhT, bon,
         TM, Vt, Gb, Gk, Nn, Pk, Qk, Xsb, Usb, Yb) = sets[si % NBUF]
        saved = P.ops
        P.ops = []
        marks = []
        lk = 'ld'
        for d, cidx in enumerate((cf, cb)):
            t0 = cidx * 64
            for nm, row in (('r', 0), ('k', 1), ('v', 2)):
                Pdma('sp', L_[nm][:, d, :, :], uT[row, :, t0:t0 + 64].rearrange("(h c) t -> c h t", h=2), r=['uTall'], w=[lk])
            Pdma('sp', L_['wl'][:, d, :], uT[3, d * 64:(d + 1) * 64, t0:t0 + 64], r=['uTall'], w=[lk])
            Pdma('sp', L_['al'][:, d, :], uT[4, d * 64:(d + 1) * 64, t0:t0 + 64], r=['uTall'], w=[lk])
        for nm, dst in (('r', ur), ('k', uk), ('v', uv)):
            d4 = v4(dst)
            Pop('pool', lambda e, d4=d4, src=L_[nm]: e.tensor_copy(out=d4[:, 0], in_=src[:, 0]), r=[lk], w=[nm])
            Pop('pool', lambda e, d4=d4, src=L_[nm]: e.tensor_copy(out=d4[:, 1], in_=src[:, 1, :, ::-1]), r=[lk], w=[nm])
        for nm, dst in (('wl', uwl), ('al', ual)):
            Pop('pool', lambda e, dst=dst, src=L_[nm]: e.tensor_copy(out=dst[:, 0, :], in_=src[:, 0, :]), r=[lk], w=[nm])
            Pop('pool', lambda e, dst=dst, src=L_[nm]: e.tensor_copy(out=dst[:, 1, :], in_=src[:, 1, ::-1]), r=[lk], w=[nm])
        ACT(tw[:], uwl[:], AF.Tanh, ['wl'], ['tw'])
        for d in range(2):
            for h in range(2):
                dh = d * 2 + h
                MM(bank[0][0:64, dh * 64:(dh + 1) * 64], tw[:, d, :], w2_sb[:, dh * 64:(dh + 1) * 64], ['tw', 'w2'], [('ps', 0)])
                MM(bank[0][0:64, 256 + dh * 64:256 + (dh + 1) * 64], a2_sb[:, dh * 64:(dh + 1) * 64], ual[:, d, :],
                   ['a2', 'al'], [('ps', 0)])
        TT('dve', swt[:], bank[0][0:64, 0:256], w0b_sb[:], ALU.add, [('ps', 0), 'w0b'], ['swt'])
        ACT(swt[:], swt[:], AF.Sigmoid, ['swt'], ['swt'])
        TT('dve', alr[:], bank[0][0:64, 256:512].rearrange("p (a t) -> p a t", t=64),
           a0f_sb[:].unsqueeze(2).broadcast_to([64, 4, 64]), ALU.add, [('ps', 0), 'a0f'], ['alr'])
        ACT(alr[:], alr[:], AF.Sigmoid, ['alr'], ['alr'])
        cbanks = (1, 0)
        for dh in range(4):
            bk = cbanks[dh // 2]
            MM(bank[bk][0:64, (dh % 2) * 192:(dh % 2) * 192 + 192], swt[:, dh * 64:(dh + 1) * 64], Tri3, ['swt', 'cst'], [('ps', bk)])
        for half in range(2):
            bk = cbanks[half]
            cv = bank[bk][0:64, 0:384].rearrange("p (a x) -> p a x", x=192)
            sl = slice(half * 2, half * 2 + 2)
            ACT(eI[:, sl, :], cv[:, :, 0:64], AF.Exp, [('ps', bk)], ['eI'], scale=-KAPPA)
            ACT(eE[:, sl, :], cv[:, :, 64:128], AF.Exp, [('ps', bk)], ['eE'], scale=-KAPPA)
            ACT(eN[:, sl, :], cv[:, :, 0:64], AF.Exp, [('ps', bk)], ['eN'], scale=KAPPA)
            ACT(gC[:, sl], cv[:, :, 128], AF.Exp, [('ps', bk)], ['gC'], scale=-KAPPA)
        TT('dve', eT[:], eN[:], gC[:].unsqueeze(2).broadcast_to([64, 4, 64]), ALU.mult, ['eN', 'gC'], ['eT'])
        marks.append(len(P.ops))
        TT('dve', v4(kk), v4(uk), prmb(0), ALU.mult, ['k', 'prm'], ['kk'])
        TT('pool', kk2[:], kk[:], kk[:], ALU.mult, ['kk'], ['kk2'])
        MM(bank[2][0:64, 0:256], ones64[:], kk2[:].rearrange("p a t -> p (a t)"), ['ones64', 'kk2'], [('ps', 2)])
        ACT(rn[:].rearrange("p a t -> p (a t)"), bank[2][0:64, 0:256], AF.Sqrt, [('ps', 2)], ['rn'])
        TS('dve', rn[:], rn[:], 1e-12, None, ALU.max, None, ['rn'], ['rn'])
        Pop('dve', lambda e: e.reciprocal(out=rn[:], in_=rn[:]), r=['rn'], w=['rn'])
        TT('dve', kkn[:], kk[:], rn[:], ALU.mult, ['kk', 'rn'], ['kkn'])
        TT('pool', bb[:], kkn[:], alr[:], ALU.mult, ['kkn', 'alr'], ['bb'])
        TS('pool', t1[:], alr[:], -1.0, None, ALU.add, None, ['alr'], ['t1'])
        TT('pool', v4(t1), v4(t1), prmb(1), ALU.mult, ['t1', 'prm'], ['t1'])
        STT(km[:], t1[:], 1.0, uk[:], ALU.add, ALU.mult, ['t1', 'k'], ['km'])
        STT(AR[:, :, 0:64], kkn[:], -1.0, eE[:], ALU.mult, ALU.mult, ['kkn', 'eE'], ['AR'])
        TT('pool', AR[:, :, 64:128], ur[:], eI[:], ALU.mult, ['r', 'eI'], ['AR'])
        TT('dve', BT[:], bb[:], eN[:], ALU.mult, ['bb', 'eN'], ['BT'])
        TT('pool', KTt[:], km[:], eN[:], ALU.mult, ['km', 'eN'], ['KTt'])
        TT('dve', BhT[:], bb[:], eT[:], ALU.mult, ['bb', 'eT'], ['BhT'])
        TT('pool', KhT[:], km[:], eT[:], ALU.mult, ['km', 'eT'], ['KhT'])
        TT('pool', t1[:], ur[:], km[:], ALU.mult, ['r', 'km', 't1'], ['t1'])
        TT('pool', v4(t1), v4(t1), prmb(2), ALU.mult, ['t1', 'prm'], ['t1'])
        MM(bank[2][0:64, 256:512], ones64[:], t1[:].rearrange("p a t -> p (a t)"), ['ones64', 't1'], [('ps', 2)])
        TT('dve', bon[:], bank[2][0:64, 256:512].rearrange("p (a t) -> p a t", t=64), uv[:], ALU.mult, [('ps', 2), 'v'], ['bon'])
        Pop('pool', lambda e: e.tensor_copy(out=kk2[:, 2:4, :], in_=bon[:, 2:4, ::-1]), r=['bon', 'kk2'], w=['kk2'])
        Pdma('pool', Bd[0, :, cf * 64:cf * 64 + 64].rearrange("(h c) t -> c h t", h=2), bon[:, 0:2, :], r=['bon'], w=['Bdall'])
        Pdma('pool', Bd[1, :, cb * 64:cb * 64 + 64].rearrange("(h c) t -> c h t", h=2), kk2[:, 2:4, :], r=['kk2'], w=['Bdall'])
        for dh in range(4):
            MM(bank[3][0:64, dh * 64:(dh + 1) * 64], BhT[:, dh, :], I64b[:], ['BhT', 'I64b'], [('ps', 3)])
            MM(bank[3][0:64, 256 + dh * 64:256 + (dh + 1) * 64], KhT[:, dh, :], I64b[:], ['KhT', 'I64b'], [('ps', 3)])
        Pop('act', lambda e: e.copy(out=TM[:].rearrange("p a b t -> p (a b t)"), in_=bank[3][0:64, :]), r=[('ps', 3)], w=['TM'])
        for dh in range(4):
            MM(bank[2][0:64, dh * 64:(dh + 1) * 64], uv[:, dh, :], I64, ['v', 'cst'], [('ps', 2)])
        Pop('dve', lambda e: e.tensor_copy(out=Vt[:].rearrange("p a t -> p (a t)"), in_=bank[2][0:64, 0:256]), r=[('ps', 2)], w=['Vt'])
        marks.append(len(P.ops))
        for dh in range(4):
            MM(bank[4][0:64, dh * 128:(dh + 1) * 128], BT[:, dh, :], AR[:, dh, :], ['BT', 'AR'], [('ps', 4)])
            MM(bank[5][0:64, dh * 128:(dh + 1) * 128], KTt[:, dh, :], AR[:, dh, :], ['KTt', 'AR'], [('ps', 5)])
        mk4 = Mk.unsqueeze(1).broadcast_to([64, 4, 128])
        TT('dve', Gb[:], bank[4][0:64, :].rearrange("p (a x) -> p a x", x=128), mk4, ALU.mult, [('ps', 4), 'cst'], ['Gb'])
        TT('dve', Gk[:], bank[5][0:64, :].rearrange("p (a x) -> p a x", x=128), mk4, ALU.mult, [('ps', 5), 'cst'], ['Gk'])
        for dh in range(4):
            MM(bank[4][0:64, 256 + dh * 64:256 + (dh + 1) * 64], AR[:, dh, 0:64], BT[:, dh, :], ['AR', 'BT'], [('ps', 4)])
        TT('dve', Nn[:], bank[4][0:64, 256:512].rearrange("p (a x) -> p a x", x=64),
           MkN.unsqueeze(1).broadcast_to([64, 4, 64]), ALU.mult, [('ps', 4), 'cst'], ['Nn'])
        P0b = Qk[3]
        Qf = Qk[2]
        Pop('act', lambda e: e.copy(out=P0b[:], in_=Gb[:, :, 0:64]), r=['Gb'], w=['P0b'])
        TT('pool', Qk[0][:], Gb[:, :, 0:64], I64.unsqueeze(1).broadcast_to([64, 4, 64]), ALU.add, ['Gb', 'cst'], [('Q', 0)])
        pk_prev = (lambda dh: P0b[:, dh, :], lambda dh: Nn[:, dh, :], ['P0b', 'Nn'])
        qi = 0
        for lv in range(1, 6):
            if lv == 3:
                marks.append(len(P.ops))
            pb = lv % 2
            Pn = Pk[pb]
            pkey = ('Pk', pb)
            bkp, bkq = (5, 4) if lv <= 2 else (6, 6)
            for dh in range(4):
                if lv < 5:
                    MM(bank[bkp][0:64, dh * 64:(dh + 1) * 64], pk_prev[1](dh), pk_prev[0](dh), pk_prev[2], [('ps', bkp)])
                MM(bank[bkp][0:64, 256 + dh * 64:256 + (dh + 1) * 64], pk_prev[0](dh), pk_prev[1](dh), pk_prev[2], [('ps', bkp)])
            if lv % 2:
                Pop('act', lambda e, Pn=Pn, bkp=bkp: e.copy(out=Pn[:].rearrange("p a b t -> p (a b t)"), in_=bank[bkp][0:64, :]),
                    r=[('ps', bkp)], w=[pkey])
            else:
                Pop('dve', lambda e, Pn=Pn, bkp=bkp: e.tensor_copy(out=Pn[:].rearrange("p a b t -> p (a b t)"), in_=bank[bkp][0:64, :]),
                    r=[('ps', bkp)], w=[pkey])
            pk_prev = (lambda dh, Pn=Pn: Pn[:, 0, dh, :], lambda dh, Pn=Pn: Pn[:, 1, dh, :], [pkey])
            for dh in range(4):
                MM(bank[bkq][0:64, dh * 64:(dh + 1) * 64], Pn[:, 1, dh, :], Qk[qi][:, dh, :], [pkey, ('Q', qi)], [('ps', bkq)])
            if lv < 5:
                TT('dve', Qk[1 - qi][:], bank[bkq][0:64, 0:256].rearrange("p (a t) -> p a t", t=64), Qk[qi][:], ALU.add,
                   [('ps', bkq), ('Q', qi)], [('Q', 1 - qi)])
                qi = 1 - qi
            else:
                TT('dve', Qf[:], bank[bkq][0:64, 0:256].rearrange("p (a t) -> p a t", t=64), Qk[qi][:], ALU.add,
                   [('ps', bkq), ('Q', qi)], ['Qf'])
        TTm = Qf
        tkey = 'Qf'
        marks.append(len(P.ops))
        for dh in range(4):
            MM(bank[7][0:64, dh * 64:(dh + 1) * 64], AR[:, dh, 0:64], ST[:, dh, :], ['AR', 'ST'], [('ps', 7)], start=True, stop=False)
            MM(bank[7][0:64, dh * 64:(dh + 1) * 64], Gk[:, dh, 0:64], Vt[:, dh, :], ['Gk', 'Vt'], [('ps', 7)], start=False, stop=True)
        Pop('act', lambda e: e.copy(out=Xsb[:].rearrange("p a t -> p (a t)"), in_=bank[7][0:64, 0:256]), r=[('ps', 7)], w=['Xsb'])
        for dh in range(4):
            MM(bank[7][0:64, 256 + dh * 64:256 + (dh + 1) * 64], TTm[:, dh, :], Xsb[:, dh, :], [tkey, 'Xsb'], [('ps', 7)])
        Pop('act', lambda e: e.copy(out=Usb[:].rearrange("p a t -> p (a t)"), in_=bank[7][0:64, 256:512]), r=[('ps', 7)], w=['Usb'])
        for dh in range(4):
            o = bank[7][0:64, dh * 64:(dh + 1) * 64]
            MM(o, ST[:, dh, :], AR[:, dh, 64:128], ['ST', 'AR'], [('ps', 7)], start=True, stop=False)
            MM(o, Usb[:, dh, :], Gb[:, dh, 64:128], ['Usb', 'Gb'], [('ps', 7)], start=False, stop=False)
            MM(o, Vt[:, dh, :], Gk[:, dh, 64:128], ['Vt', 'Gk'], [('ps', 7)], start=False, stop=True)
        yv = bank[7][0:64, 0:256].rearrange("p (a t) -> p a t", t=64)
        Pop('act', lambda e, Yb=Yb, yv=yv: e.copy(out=Yb[:, 0:2, :], in_=yv[:, 0:2, :]), r=[('ps', 7)], w=['Ysb'])
        Pop('act', lambda e, Yb=Yb, yv=yv: e.copy(out=Yb[:, 2:4, ::-1], in_=yv[:, 2:4, :]), r=[('ps', 7)], w=['Ysb'])
        Pdma('pool', Yd[0, :, cf * 64:cf * 64 + 64].rearrange("(h c) t -> c h t", h=2), Yb[:, 0:2, :], r=['Ysb'], w=['Ydall'])
        Pdma('pool', Yd[1, :, cb * 64:cb * 64 + 64].rearrange("(h c) t -> c h t", h=2), Yb[:, 2:4, :], r=['Ysb'], w=['Ydall'])
        TT('pool', Stmp[:], ST[:], gC[:].unsqueeze(2).broadcast_to([64, 4, 64]), ALU.mult, ['ST', 'gC'], ['Stmp'])
        for dh in range(4):
            o = bank[7][0:64, 256 + dh * 64:256 + (dh + 1) * 64]
            MM(o, TM[:, 0, dh, :], Usb[:, dh, :], ['TM', 'Usb'], [('ps', 7)], start=True, stop=False)
            MM(o, TM[:, 1, dh, :], Vt[:, dh, :], ['TM', 'Vt'], [('ps', 7)], start=False, stop=True)
        TT('dve', ST[:], bank[7][0:64, 256:512].rearrange("p (a t) -> p a t", t=64), Stmp[:], ALU.add, [('ps', 7), 'Stmp'], ['ST'])
        ops = P.ops
        P.ops = saved
        cur['b'] = None
        bounds = [0] + marks + [len(ops)]
        return [ops[bounds[i]:bounds[i + 1]] for i in range(5)]

    def interleave(lists):
        items = []
        for li, lst in enumerate(lists):
            n_ = len(lst)
            for k_, o in enumerate(lst):
                items.append(((k_ + 0.5) / n_, li, k_, o))
        items.sort(key=lambda t: (t[0], t[1], t[2]))
        return [t[3] for t in items]
    gen = [gen_step(si) for si in range(NS)]
    for tau in range(-KSEG, NS):
        lists = []
        if 0 <= tau < NS:
            lists.append(gen[tau][KSEG])
        for j in range(1, KSEG + 1):
            s_ = tau + j
            if 0 <= s_ < NS:
                lists.append(gen[s_][KSEG - j])
        P.ops.extend(interleave(lists))
    P.fence()
    A.release(m0)
    prm2 = A.alloc("prm2", [64, 2, 5])
    on64 = A.alloc("on64", [64, 64])
    eps2 = A.alloc("eps2", [64, 1])
    P.dma('sp', prm2[:], prm, w=['prm2'])
    P.op('pool', lambda e: e.memset(on64[:], 1.0 / 64), w=['on64'])
    P.op('pool', lambda e: e.memset(eps2[:], GN_EPS), w=['eps2'])
    yb = [A.alloc("yb%d" % i, [64, 2, 2, 512]) for i in range(2)]
    bd = [A.alloc("bd%d" % i, [64, 2, 2, 512]) for i in range(2)]
    gg = [A.alloc("gg%d" % i, [64, 2, 512]) for i in range(2)]
    y = A.alloc("y", [64, 2, 512])
    yc = A.alloc("yc", [64, 2, 512])
    y2 = A.alloc("y2", [64, 2, 512])
    rs = A.alloc("rs", [64, 2, 512])
    jobs = []
    if ctx_out:
        jobs.append((0, CTXN, 0))
    o0 = CTXN if ctx_out else 0
    for b0 in range(0, L, 512):
        jobs.append((CTXN + b0, 512, o0 + b0))
    for ji, (t0, n, orow) in enumerate(jobs):
        b = ji % 2
        P.dma('sp', yb[b][:, :, :, :n], Yd[:, :, t0:t0 + n].rearrange("d (h c) t -> c d h t", h=2), r=['Ydall'], w=[('yb', b)])
        P.dma('sp', bd[b][:, :, :, :n], Bd[:, :, t0:t0 + n].rearrange("d (h c) t -> c d h t", h=2), r=['Bdall'], w=[('bd', b)])
        P.dma('sp', gg[b][:, :, :n], uT[5, :, t0:t0 + n].rearrange("(h c) t -> c h t", h=2), r=['uTall'], w=[('gg', b)])
        TT('dve', y[:, :, :n], yb[b][:, 0, :, :n], yb[b][:, 1, :, :n], ALU.add, [('yb', b)], ['y'])
        for h in range(2):
            MM(bank[h][0:64, :n], on64[:], y[:, h, :n], ['on64', 'y'], [('ps', h)])
            TT('dve', yc[:, h, :n], y[:, h, :n], bank[h][0:64, :n], ALU.subtract, ['y', ('ps', h)], ['yc'])
        TT('pool', y2[:, :, :n], yc[:, :, :n], yc[:, :, :n], ALU.mult, ['yc'], ['y2'])
        for h in range(2):
            MM(bank[2 + h][0:64, :n], on64[:], y2[:, h, :n], ['on64', 'y2'], [('ps', 2 + h)])
            ACT(rs[:, h, :n], bank[2 + h][0:64, :n], AF.Sqrt, [('ps', 2 + h), 'eps2'], ['rs'], bias=eps2[:, 0:1])
        P.op('dve', lambda e, n=n: e.reciprocal(out=rs[:, :, :n], in_=rs[:, :, :n]), r=['rs'], w=['rs'])
        TT('dve', yc[:, :, :n], yc[:, :, :n], rs[:, :, :n], ALU.mult, ['yc', 'rs'], ['yc'])
        for h in range(2):
            TS('pool', yc[:, h, :n], yc[:, h, :n], prm2[:, h, 3:4], prm2[:, h, 4:5], ALU.mult, ALU.add, ['yc', 'prm2'], ['yc'])
        TT('dve', yc[:, :, :n], yc[:, :, :n], bd[b][:, 0, :, :n], ALU.add, ['yc', ('bd', b)], ['yc'])
        TT('dve', yc[:, :, :n], yc[:, :, :n], bd[b][:, 1, :, :n], ALU.add, ['yc', ('bd', b)], ['yc'])
        TT('pool', y2[:, :, :n], yc[:, :, :n], gg[b][:, :, :n], ALU.mult, ['yc', ('gg', b), 'y2'], ['y2'])
        P.dma('pool', out_ap(orow, n).rearrange("(h c) t -> c h t", h=2), y2[:, :, :n], r=['y2'], w=[('brb', 2)], final=FINAL_OUT)
    P.fence()


NORM_EPS = 1e-6
CTXN = 256


def phase_A(nc, P, A, bank, pfx, L, ctx_out, lam_init, x_loader, brb, do_attn=True, do_pool=True, do_rwkv=True, pre_rwkv=None):
    T = CTXN + L
    NQ = T if ctx_out else L
    di = lambda name, shape: nc.dram_tensor(pfx + name, list(shape), F32, kind="ExternalInput").ap()
    if x_loader is None:
        xT = di("xT", [128, 8, T])

        def x_loader(dst, t0, sz, key):
            P.dma('sp', dst[:, :, :sz], xT[:, :, t0:t0 + sz], w=[key])
    identA = di("identA", [128, 128])
    cT = di("cT", [128, 8, 2])
    wada = di("wada", [128, 8, 2048])
    bada = di("bada", [128, 16])
    g1 = di("g1", [128, 8])
    win = di("win", [128, 8, 1280])
    qkg = di("qkg", [128, 2])
    ropeR = di("ropeR", [128, 2, L // 64])
    ropeC = di("ropeC", [128, 2, 64])
    perm = di("perm", [128, 128])
    lamqk = di("lamqk", [128, 256])
    subg = di("subg", [128, 128])
    wpool = di("wpool", [128, 128])
    pscale = di("pscale", [128, 1])
    selw = di("selw", [128, 4])
    efix = di("efix", [128, 16])
    pT = nc.dram_tensor(pfx + "pT_scr", [7, 128, T], F32, kind="Internal").ap()
    base_mark = A.mark()
    if True:
        ones = A.alloc("ones", [128, 128])
        blk = A.alloc("blk", [128, 128])
        eps_sb = A.alloc("eps", [128, 1])
        mod_sb = A.alloc("mod", [128, 16, 2])
        A_sb = A.alloc("A", [128, 8, 2])
        NKT = T // 128
        QT = A.alloc("QT", [128, T], BF16)
        KT = A.alloc("KT", [128, T], BF16)
        V = A.alloc("V", [128, NKT, 130], BF16)
        lam_sb = A.alloc("lam", [128, 1])
        subg_sb = A.alloc("subg", [128, 128])
        ident_sb = A.alloc("identA", [128, 128])
        P.dma('sp', ident_sb[:], identA, w=['identA'])
        P.op('pool', lambda e: e.memset(ones[:], 1.0), w=['ones'])
        P.op('pool', lambda e: e.memset(blk[:], 0.0), w=['blk'])
        P.op('pool', lambda e: e.memset(blk[0:64, 0:64], 1.0), w=['blk'])
        P.op('pool', lambda e: e.memset(blk[64:128, 64:128], 1.0), w=['blk'])
        P.op('pool', lambda e: e.memset(eps_sb[:], NORM_EPS), w=['eps'])
        P.op('pool', lambda e: e.memset(V[:, :, 128:130], 1.0), w=['Vones'])
        m_a1 = A.mark()
        c_sb = A.alloc("c", [128, 8, 2])
        s_sb = A.alloc("s", [128, 8, 2])
        bada_sb = A.alloc("bada", [128, 16])
        g1_sb = A.alloc("g1", [128, 8])
        qkg_sb = A.alloc("qkg", [128, 2])
        perm_sb = A.alloc("perm", [128, 128])
        ropeR_sb = A.alloc("ropeR", [128, 2, L // 64])
        ropeC_sb = A.alloc("ropeC", [128, 2, 64])
        lamqk_sb = A.alloc("lamqk", [128, 256])
        lamt = A.alloc("lamt", [128, 4])
        wbf = A.alloc("wbf", [128, 8, 1280], BF16)
        xt = [A.alloc("xt%d" % i, [128, 8, 512]) for i in range(2)]
        sq = A.alloc("sq", [128, 8, 512])
        rstd = A.alloc("rstd", [128, 512])
        xn = A.alloc("xn", [128, 8, 512], BF16)
        ob = [A.alloc("ob%d" % i, [128, 512]) for i in range(2)]
        qk32 = A.alloc("qk32", [128, 512])
        qksq = A.alloc("qksq", [128, 512])
        qkr = A.alloc("qkr", [128, 512])
        qkn = A.alloc("qkn", [128, 512])
        cs_t = A.alloc("cs_t", [128, 2, 512])
        rtmp = A.alloc("rtmp", [128, 2, 512])

        for nm, dst, src in [('c_sb', c_sb, cT), ('bada', bada_sb, bada), ('g1', g1_sb, g1), ('qkg', qkg_sb, qkg),
                             ('perm', perm_sb, perm), ('ropeR', ropeR_sb, ropeR), ('ropeC', ropeC_sb, ropeC),
                             ('lamqk', lamqk_sb, lamqk), ('subg', subg_sb, subg)]:
            P.dma('sp', dst[:], src, w=[nm])
        lq = lamqk_sb[:].rearrange("p (a b) -> p a b", b=64)
        P.op('dve', lambda e: e.tensor_tensor(out=sq[:, 0, 0:64], in0=lq[:, 0, :], in1=lq[:, 1, :], op=ALU.mult),
             r=['lamqk'], w=['sq'])
        P.op('dve', lambda e: e.tensor_tensor(out=sq[:, 0, 64:128], in0=lq[:, 2, :], in1=lq[:, 3, :], op=ALU.mult),
             r=['lamqk'], w=['sq'])
        P.op('dve', lambda e: e.tensor_reduce(out=lamt[:, 0:2], in_=sq[:, 0, 0:128].rearrange("p (a b) -> p a b", b=64),
                                              axis=AX.X, op=ALU.add), r=['sq'], w=['lamt'])
        P.op('act', lambda e: e.activation(out=lamt[:, 2:4], in_=lamt[:, 0:2], func=AF.Exp), r=['lamt'], w=['lamt2'])
        P.op('dve', lambda e: e.tensor_tensor(out=lam_sb[:], in0=lamt[:, 2:3], in1=lamt[:, 3:4], op=ALU.subtract),
             r=['lamt2'], w=['lam'])
        P.op('dve', lambda e: e.tensor_scalar(out=lam_sb[:], in0=lam_sb[:], scalar1=lam_init, scalar2=-1.0,
                                              op0=ALU.add, op1=ALU.mult), r=['lam'], w=['lam'])
        P.op('act', lambda e: e.activation(out=s_sb[:], in_=c_sb[:], func=AF.Silu), r=['c_sb'], w=['s_sb'])
        ps_mod = bank[0][:, 0:32]
        for piece in range(4):
            b = piece % 2
            P.dma('sp', xt[b][:], wada[:, :, piece * 512:(piece + 1) * 512], w=[('xt', b)])
            for occ in range(4):
                oc = piece * 4 + occ
                for k in range(8):
                    P.op('pe', lambda e, b=b, occ=occ, oc=oc, k=k: e.matmul(
                        ps_mod[:, oc * 2:oc * 2 + 2], lhsT=xt[b][:, k, occ * 128:(occ + 1) * 128],
                        rhs=s_sb[:, k, :], start=(k == 0), stop=(k == 7)),
                        r=[('xt', b), 's_sb'], w=[('ps', 0)])
        P.op('dve', lambda e: e.tensor_tensor(
            out=mod_sb[:], in0=ps_mod.rearrange("p (a b) -> p a b", b=2),
            in1=bada_sb[:].unsqueeze(2).broadcast_to([128, 16, 2]), op=ALU.add),
            r=[('ps', 0), 'bada'], w=['mod'])
        P.op('dve', lambda e: e.tensor_scalar(out=A_sb[:], in0=mod_sb[:, 8:16, :], scalar1=1.0, scalar2=None,
                                              op0=ALU.add), r=['mod'], w=['A'])
        P.op('dve', lambda e: e.tensor_tensor(out=A_sb[:], in0=A_sb[:],
                                              in1=g1_sb[:].unsqueeze(2).broadcast_to([128, 8, 2]), op=ALU.mult),
             r=['A', 'g1'], w=['A'])
        for piece, (c0, csz) in enumerate([(0, 512), (512, 512), (1024, 256)]):
            b = piece % 2
            P.dma('sp', xt[b][:, :, :csz], win[:, :, c0:c0 + csz], w=[('xt', b)])
            P.op('pool', lambda e, b=b, c0=c0, csz=csz: e.tensor_copy(out=wbf[:, :, c0:c0 + csz], in_=xt[b][:, :, :csz]),
                 r=[('xt', b)], w=['wbf'])
        tiles = [(0, CTXN, 1)] + [(CTXN + i * 512, 512, 0) for i in range(L // 512)]
        SCR = {0: 0, 4: 1, 5: 2, 6: 3, 7: 4, 8: 5, 9: 6}
        oi = 0
        for ti, (t0, sz, j) in enumerate(tiles):
            b = ti % 2
            x_loader(xt[b], t0, sz, ('xt', b))
            P.op('act', lambda e, b=b, sz=sz: e.activation(out=sq[:, :, :sz], in_=xt[b][:, :, :sz], func=AF.Square),
                 r=[('xt', b)], w=['sq'])
            for k in range(8):
                P.op('pe', lambda e, k=k, sz=sz: e.matmul(bank[0][:, :sz], lhsT=ones[:], rhs=sq[:, k, :sz],
                                                          start=(k == 0), stop=(k == 7)),
                     r=['ones', 'sq'], w=[('ps', 0)])
            P.op('act', lambda e, sz=sz: e.activation(out=rstd[:, :sz], in_=bank[0][:, :sz], func=AF.Sqrt,
                                                      scale=1.0 / 1024, bias=eps_sb[:, 0:1]),
                 r=[('ps', 0), 'eps'], w=['rstd'])
            P.op('dve', lambda e, sz=sz: e.reciprocal(out=rstd[:, :sz], in_=rstd[:, :sz]), r=['rstd'], w=['rstd'])
            P.op('dve', lambda e, b=b, sz=sz: e.tensor_tensor(
                out=sq[:, :, :sz], in0=xt[b][:, :, :sz],
                in1=rstd[:, :sz].unsqueeze(1).broadcast_to([128, 8, sz]), op=ALU.mult),
                r=[('xt', b), 'rstd', 'sq'], w=['sq'])
            for k in range(8):
                if k % 2:
                    P.op('dve', lambda e, k=k, sz=sz, j=j: e.tensor_scalar(
                        out=xn[:, k, :sz], in0=sq[:, k, :sz], scalar1=A_sb[:, k, j:j + 1],
                        scalar2=mod_sb[:, k, j:j + 1], op0=ALU.mult, op1=ALU.add),
                        r=['sq', 'A', 'mod'], w=['xn'])
                else:
                    P.op('act', lambda e, k=k, sz=sz, j=j: e.activation(
                        out=xn[:, k, :sz], in_=sq[:, k, :sz], func=AF.Identity, scale=A_sb[:, k, j:j + 1],
                        bias=mod_sb[:, k, j:j + 1]), r=['sq', 'A', 'mod'], w=['xn'])
            if j == 0:
                r0 = (t0 - CTXN) // 64
                for cs in range(2):
                    P.op('pool', lambda e, cs=cs, r0=r0: e.tensor_tensor(
                        out=cs_t[:, cs, :].rearrange("p (r c) -> p r c", c=64),
                        in0=ropeR_sb[:, cs, r0:r0 + 8].unsqueeze(2).broadcast_to([128, 8, 64]),
                        in1=ropeC_sb[:, cs, :].unsqueeze(1).broadcast_to([128, 8, 64]), op=ALU.mult),
                        r=['ropeR', 'ropeC'], w=['cs_t'])
            for c in range(10):
                if c == 3:
                    for s in range(sz // 128):
                        kt = t0 // 128 + s
                        for k in range(8):
                            P.op('pe', lambda e, k=k, s=s: e.matmul(
                                bank[3][:, 0:128], lhsT=xn[:, k, s * 128:(s + 1) * 128], rhs=wbf[:, k, 384:512],
                                start=(k == 0), stop=(k == 7)), r=['xn', 'wbf'], w=[('ps', 3)])
                        P.op('act', lambda e, kt=kt: e.copy(out=V[:, kt, 0:128], in_=bank[3][:, 0:128]),
                             r=[('ps', 3)], w=['V'])
                    continue
                pb = 1 + (oi % 2)
                oi += 1
                for k in range(8):
                    P.op('pe', lambda e, c=c, k=k, sz=sz, pb=pb: e.matmul(
                        bank[pb][:, :sz], lhsT=wbf[:, k, c * 128:(c + 1) * 128], rhs=xn[:, k, :sz],
                        start=(k == 0), stop=(k == 7)), r=['wbf', 'xn'], w=[('ps', pb)])
                if c in SCR:
                    o = ob[oi % 2]
                    ok = ('ob', oi % 2)
                    if oi % 2:
                        P.op('act', lambda e, o=o, pb=pb, sz=sz: e.copy(out=o[:, :sz], in_=bank[pb][:, :sz]),
                             r=[('ps', pb)], w=[ok])
                    else:
                        P.op('dve', lambda e, o=o, pb=pb, sz=sz: e.tensor_copy(out=o[:, :sz], in_=bank[pb][:, :sz]),
                             r=[('ps', pb)], w=[ok])
                    P.dma('pool', pT[SCR[c], :, t0:t0 + sz], o[:, :sz], r=[ok], w=[('pT', SCR[c], ti)])
                else:
                    dst = QT if c == 1 else KT
                    gi = c - 1
                    P.op('act', lambda e, pb=pb, sz=sz: e.copy(out=qk32[:, :sz], in_=bank[pb][:, :sz]),
                         r=[('ps', pb)], w=['qk32'])
                    P.op('act', lambda e, sz=sz: e.activation(out=qksq[:, :sz], in_=qk32[:, :sz], func=AF.Square),
                         r=['qk32'], w=['qksq'])
                    P.op('pe', lambda e, sz=sz: e.matmul(bank[4][:, :sz], lhsT=blk[:], rhs=qksq[:, :sz],
                                                         start=True, stop=True), r=['blk', 'qksq'], w=[('ps', 4)])
                    P.op('act', lambda e, sz=sz: e.activation(out=qkr[:, :sz], in_=bank[4][:, :sz], func=AF.Sqrt,
                                                              scale=1.0 / 64, bias=eps_sb[:, 0:1]),
                         r=[('ps', 4), 'eps'], w=['qkr'])
                    P.op('dve', lambda e, sz=sz: e.reciprocal(out=qkr[:, :sz], in_=qkr[:, :sz]), r=['qkr'], w=['qkr'])
                    if j == 1:
                        P.op('dve', lambda e, sz=sz, gi=gi, dst=dst, t0=t0: e.scalar_tensor_tensor(
                            out=dst[:, t0:t0 + sz], in0=qk32[:, :sz], scalar=qkg_sb[:, gi:gi + 1], in1=qkr[:, :sz],
                            op0=ALU.mult, op1=ALU.mult), r=['qk32', 'qkg', 'qkr'], w=['QK'])
                    else:
                        P.op('dve', lambda e, sz=sz, gi=gi: e.scalar_tensor_tensor(
                            out=qkn[:, :sz], in0=qk32[:, :sz], scalar=qkg_sb[:, gi:gi + 1], in1=qkr[:, :sz],
                            op0=ALU.mult, op1=ALU.mult), r=['qk32', 'qkg', 'qkr'], w=['qkn'])
                        P.op('pe', lambda e, sz=sz: e.matmul(bank[5][:, :sz], lhsT=perm_sb[:], rhs=qkn[:, :sz],
                                                             start=True, stop=True), r=['perm', 'qkn'], w=[('ps', 5)])
                        P.op('pool', lambda e, sz=sz: e.tensor_tensor(out=rtmp[:, 0, :sz], in0=qkn[:, :sz],
                                                                      in1=cs_t[:, 0, :sz], op=ALU.mult),
                             r=['qkn', 'cs_t'], w=['rtmp0'])
                        P.op('dve', lambda e, sz=sz: e.tensor_tensor(out=rtmp[:, 1, :sz], in0=bank[5][:, :sz],
                                                                     in1=cs_t[:, 1, :sz], op=ALU.mult),
                             r=[('ps', 5), 'cs_t'], w=['rtmp1'])
                        P.op('dve', lambda e, sz=sz, dst=dst, t0=t0: e.tensor_tensor(
                            out=dst[:, t0:t0 + sz], in0=rtmp[:, 0, :sz], in1=rtmp[:, 1, :sz], op=ALU.add),
                            r=['rtmp0', 'rtmp1'], w=['QK'])
        P.fence()
        A.release(m_a1)
        m_ph = A.mark()
        if do_pool:
            wpool_f = A.alloc("wpool_f", [128, 128])
            wpool_b = A.alloc("wpool_b", [128, 128], BF16)
            pscale_sb = A.alloc("pscale", [128, 1])
            selw_sb = A.alloc("selw", [128, 4])
            efix_sb = A.alloc("efix", [128, 16])
            NB = 2048
            U = A.alloc("U", [128, NB + 32])
            W = [A.alloc("W%d" % i, [128, NB + 32]) for i in range(2)]
            comb = A.alloc("comb", [128, NB])
            diffb = A.alloc("diffb", [128, NB], BF16)
            pob = [A.alloc("pob%d" % i, [128, 512]) for i in range(2)]
            P.dma('sp', wpool_f[:], wpool, w=['wpool_f'])
            P.dma('sp', pscale_sb[:], pscale, w=['pscale'])
            P.dma('sp', selw_sb[:], selw, w=['selw'])
            P.dma('sp', efix_sb[:], efix, w=['efix'])
            P.op('dve', lambda e: e.tensor_copy(out=wpool_b[:], in_=wpool_f[:]), r=['wpool_f'], w=['wpool_b'])
            seqs = [(CTXN, L, (0 if not ctx_out else CTXN))]
            if ctx_out:
                seqs.append((0, CTXN, 0))
            pi = 0
            for (s0, slen, o0) in seqs:
                for b0 in range(0, slen, NB):
                    n = min(NB, slen - b0)
                    lo = max(0, b0 - 16)
                    hi = min(slen, b0 + n + 16)
                    P.op('dve', lambda e: e.memset(U[:], 0.0), w=['U'])
                    P.dma('sp', U[:, 16 + lo - b0:16 + hi - b0], pT[0, :, s0 + lo:s0 + hi], r=[('pT', 0, t) for t in range(len(tiles))], w=['U'])
                    NP = n + 32
                    src = U
                    for lv, sh in enumerate([1, 2, 4, 8]):
                        dstw = W[lv % 2]
                        P.op('dve', lambda e, src=src, dstw=dstw, sh=sh, NP=NP: e.tensor_tensor(
                            out=dstw[:, sh:NP], in0=src[:, sh:NP], in1=src[:, 0:NP - sh], op=ALU.add),
                            r=['U', ('W', 0), ('W', 1)], w=[('W', lv % 2)])
                        w_ = 2 * sh
                        off = 16 + w_ // 2 - 1
                        if lv == 0:
                            P.op('dve', lambda e, dstw=dstw, off=off, n=n, lv=lv: e.tensor_scalar(
                                out=comb[:, :n], in0=dstw[:, off:off + n], scalar1=selw_sb[:, lv:lv + 1], scalar2=None,
                                op0=ALU.mult), r=[('W', lv % 2), 'selw'], w=['comb'])
                        else:
                            P.op('dve', lambda e, dstw=dstw, off=off, n=n, lv=lv: e.scalar_tensor_tensor(
                                out=comb[:, :n], in0=dstw[:, off:off + n], scalar=selw_sb[:, lv:lv + 1], in1=comb[:, :n],
                                op0=ALU.mult, op1=ALU.add), r=[('W', lv % 2), 'selw', 'comb'], w=['comb'])
                        src = dstw
                    if b0 == 0:
                        P.op('pool', lambda e: e.tensor_tensor(out=comb[:, 0:8], in0=comb[:, 0:8], in1=efix_sb[:, 0:8],
                                                               op=ALU.mult), r=['comb', 'efix'], w=['comb'])
                    if b0 + n == slen:
                        P.op('pool', lambda e, n=n: e.tensor_tensor(out=comb[:, n - 8:n], in0=comb[:, n - 8:n],
                                                                    in1=efix_sb[:, 8:16], op=ALU.mult),
                             r=['comb', 'efix'], w=['comb'])
                    P.op('dve', lambda e, n=n: e.tensor_tensor(out=diffb[:, :n], in0=comb[:, :n], in1=U[:, 16:16 + n],
                                                               op=ALU.subtract), r=['comb', 'U'], w=['diffb'])
                    for c0 in range(0, n, 512):
                        cs = min(512, n - c0)
                        pb = 6 + pi % 2
                        o = pob[pi % 2]
                        ok = ('pob', pi % 2)
                        pi += 1
                        P.op('pe', lambda e, c0=c0, cs=cs, pb=pb: e.matmul(bank[pb][:, :cs], lhsT=wpool_b[:],
                                                                            rhs=diffb[:, c0:c0 + cs], start=True, stop=True),
                             r=['wpool_b', 'diffb'], w=[('ps', pb)])
                        P.op('act', lambda e, o=o, pb=pb, cs=cs: e.activation(out=o[:, :cs], in_=bank[pb][:, :cs],
                                                                               func=AF.Copy, scale=pscale_sb[:, 0:1]),
                             r=[('ps', pb), 'pscale'], w=[ok])
                        P.dma('pool', brb.dst(0, o0 + b0 + c0, cs), o[:, :cs], r=[ok], w=[('brb', 0)])
            P.fence()
            A.release(m_ph)
        if do_attn:
            PT = [[A.alloc("PT%d_%d" % (m, i), [128, 512], BF16) for i in range(2)] for m in range(2)]
            osb = A.alloc("osb", [128, 2, 4, 130])
            Qz = [A.alloc("Qz%d" % i, [128, 2, 512], BF16) for i in range(2)]
            for i in range(2):
                P.op('dve', lambda e, i=i: e.memset(Qz[i][:], 0.0), w=[('Qz', i)])
            rec = A.alloc("rec", [128, 2, 4])
            o1 = A.alloc("o1", [128, 4, 128])
            o2 = A.alloc("o2", [128, 4, 128])
            ssq = A.alloc("ssq", [128, 4])
            oT = A.alloc("oT", [128, 512])
            def oacc(m, qs):
                i = m * 4 + qs
                return bank[4 + i // 3][:, (i % 3) * 130:(i % 3) * 130 + 130], ('ps', 4 + i // 3)
            qjobs = []
            if ctx_out:
                qjobs.append((0, CTXN, (0, 2), 0))
            for i in range(L // 512):
                qjobs.append((CTXN + i * 512, 512, (0, NKT), (CTXN if ctx_out else 0) + i * 512))
            si = 0
            for qji, (q0, nq, (k0, k1), orow) in enumerate(qjobs):
                nqs = nq // 128
                started = set()
                qz = Qz[qji % 2]
                qzk = ('Qz', qji % 2)
                P.op('dve', lambda e, qz=qz, q0=q0, nq=nq: e.tensor_copy(out=qz[0:64, 0, :nq], in_=QT[0:64, q0:q0 + nq]),
                     r=['QK', qzk], w=[qzk])
                P.op('pool', lambda e, qz=qz, q0=q0, nq=nq: e.tensor_copy(out=qz[64:128, 1, :nq], in_=QT[64:128, q0:q0 + nq]),
                     r=['QK', qzk], w=[qzk])
                def emit_S(kt):
                    for m in range(2):
                        sb_ = kt % 2
                        pb = m * 2 + sb_
                        P.op('pe', lambda e, m=m, kt=kt, nq=nq, pb=pb, qz=qz: e.matmul(
                            bank[pb][:, :nq], lhsT=KT[:, kt * 128:(kt + 1) * 128],
                            rhs=qz[:, m, :nq], start=True, stop=True),
                            r=['QK', qzk], w=[('ps', pb)])
                        P.op('act', lambda e, m=m, sb_=sb_, pb=pb, nq=nq: e.activation(
                            out=PT[m][sb_][:, :nq], in_=bank[pb][:, :nq], func=AF.Exp, scale=0.125),
                            r=[('ps', pb)], w=[('PT', m, sb_)])

                def emit_PV(kt):
                    for m in range(2):
                        sb_ = kt % 2
                        for qs in range(nqs):
                            oap, okey = oacc(m, qs)
                            first_in_bank = (kt == k0) and (okey not in started)
                            started.add(okey)
                            P.op('pe', lambda e, m=m, sb_=sb_, qs=qs, kt=kt, oap=oap, fib=first_in_bank, k1=k1: e.matmul(
                                oap, lhsT=PT[m][sb_][:, qs * 128:(qs + 1) * 128], rhs=V[:, kt, :],
                                start=fib, stop=(kt == k1 - 1)),
                                r=[('PT', m, sb_), 'V', 'Vones'], w=[okey])
                emit_S(k0)
                for kt in range(k0, k1):
                    if kt + 1 < k1:
                        emit_S(kt + 1)
                    emit_PV(kt)
                for m in range(2):
                    for qs in range(nqs):
                        oap, okey = oacc(m, qs)
                        P.op('dve' if (m + qs) % 2 else 'act',
                             (lambda e, m=m, qs=qs, oap=oap: e.tensor_copy(out=osb[:, m, qs, :], in_=oap)) if (m + qs) % 2
                             else (lambda e, m=m, qs=qs, oap=oap: e.copy(out=osb[:, m, qs, :], in_=oap)),
                             r=[okey], w=['osb'])
                P.op('dve', lambda e, nqs=nqs: e.reciprocal(out=rec[:, :, :nqs], in_=osb[:, :, :nqs, 128]),
                     r=['osb'], w=['rec'])
                P.op('dve', lambda e, nqs=nqs: e.tensor_scalar(out=rec[:, 1, :nqs], in0=rec[:, 1, :nqs],
                                                               scalar1=lam_sb[:, 0:1], scalar2=None, op0=ALU.mult),
                     r=['rec', 'lam'], w=['rec'])
                P.op('dve', lambda e, nqs=nqs: e.tensor_tensor(
                    out=o1[:, :nqs, :], in0=osb[:, 0, :nqs, 0:128],
                    in1=rec[:, 0, :nqs].unsqueeze(2).broadcast_to([128, nqs, 128]), op=ALU.mult),
                    r=['osb', 'rec'], w=['o1'])
                P.op('pool', lambda e, nqs=nqs: e.tensor_tensor(
                    out=o2[:, :nqs, :], in0=osb[:, 1, :nqs, 0:128],
                    in1=rec[:, 1, :nqs].unsqueeze(2).broadcast_to([128, nqs, 128]), op=ALU.mult),
                    r=['osb', 'rec'], w=['o2'])
                P.op('dve', lambda e, nqs=nqs: e.tensor_tensor(out=o1[:, :nqs, :], in0=o1[:, :nqs, :], in1=o2[:, :nqs, :],
                                                               op=ALU.add), r=['o1', 'o2'], w=['o1'])
                P.op('pool', lambda e, nqs=nqs: e.tensor_tensor(out=o2[:, :nqs, :], in0=o1[:, :nqs, :], in1=o1[:, :nqs, :],
                                                                op=ALU.mult), r=['o1', 'o2'], w=['o2'])
                P.op('dve', lambda e, nqs=nqs: e.tensor_reduce(out=ssq[:, :nqs], in_=o2[:, :nqs, :], axis=AX.X, op=ALU.add),
                     r=['o2'], w=['ssq'])
                P.op('act', lambda e, nqs=nqs: e.activation(out=ssq[:, :nqs], in_=ssq[:, :nqs], func=AF.Sqrt,
                                                            scale=1.0 / 128, bias=eps_sb[:, 0:1]),
                     r=['ssq', 'eps'], w=['ssq'])
                P.op('dve', lambda e, nqs=nqs: e.reciprocal(out=ssq[:, :nqs], in_=ssq[:, :nqs]), r=['ssq'], w=['ssq'])
                P.op('dve', lambda e, nqs=nqs: e.tensor_tensor(
                    out=o1[:, :nqs, :], in0=o1[:, :nqs, :],
                    in1=ssq[:, :nqs].unsqueeze(2).broadcast_to([128, nqs, 128]), op=ALU.mult),
                    r=['o1', 'ssq'], w=['o1'])
                P.op('pool', lambda e, nqs=nqs: e.tensor_tensor(
                    out=o2[:, :nqs, :], in0=o1[:, :nqs, :],
                    in1=subg_sb[:].unsqueeze(1).broadcast_to([128, nqs, 128]), op=ALU.mult),
                    r=['o1', 'subg', 'o2'], w=['o2'])
                for qs in range(nqs):
                    P.op('pe', lambda e, qs=qs: e.matmul(bank[7][:, qs * 128:(qs + 1) * 128], lhsT=o2[:, qs, :], rhs=ident_sb[:],
                                                         start=True, stop=True), r=['o2', 'identA'], w=[('ps', 7)])
                P.op('act', lambda e, nq=nq: e.copy(out=oT[:, :nq], in_=bank[7][:, :nq]), r=[('ps', 7)], w=['oT'])
                P.dma('sp', brb.dst(1, orow, nq), oT[:, :nq], r=['oT'], w=[('brb', 1)])
            P.fence()
        if pre_rwkv is not None:
            pre_rwkv()
        if do_rwkv:
            A.release(base_mark)
            rwkv_phase(nc, P, A, bank, pT, L, ctx_out, (lambda c0, n: brb.dst(2, c0, n)), pfx)
        A.release(base_mark)


NORM_EPS = 1e-6


def phase_B(nc, P, A, bank, pfx, NT_LAT, NT_CTX, x_load, gath, off_lat, x_store, NEXP=32):
    NT = NT_LAT + NT_CTX
    di = lambda name, shape: nc.dram_tensor(pfx + name, list(shape), F32, kind="ExternalInput").ap()
    selq = di("selq", [128, 4])
    cT = di("cT", [128, 8, 2])
    wada = di("wada", [128, 8, 6144])
    bada = di("bada", [128, 48])
    g12 = di("g12", [128, 2, 8])
    wgate = di("wgate", [128, 8, 3072])
    wbr = di("wbr", [128, 12, 1024])
    wout = di("wout", [128, 8, 1024])
    wrt = di("wrt", [128, 8, 36])
    wg = di("wg", [NEXP, 128, 8, 512])
    wu = di("wu", [NEXP, 128, 8, 512])
    wd = di("wd", [NEXP, 128, 4, 1024])
    selE = di("selE", [32, 32 * 128])
    ident = di("ident", [128, 128])
    x2s = nc.dram_tensor(pfx + "x2_scr", [128, 8, NT], F32, kind="Internal").ap()
    xn2s = nc.dram_tensor(pfx + "xn2_scr", [128, 8, NT], BF16, kind="Internal").ap()
    gTs = nc.dram_tensor(pfx + "gT_scr", [32, NT], F32, kind="Internal").ap()
    base_mark = A.mark()

    def TT(eng, out, a, b, op, r, w):
        P.op(eng, lambda e: e.tensor_tensor(out=out, in0=a, in1=b, op=op), r=r, w=w)

    def TS(eng, out, a, s1, s2, op0, op1, r, w):
        if op1 is None:
            P.op(eng, lambda e: e.tensor_scalar(out=out, in0=a, scalar1=s1, scalar2=None, op0=op0), r=r, w=w)
        else:
            P.op(eng, lambda e: e.tensor_scalar(out=out, in0=a, scalar1=s1, scalar2=s2, op0=op0, op1=op1), r=r, w=w)

    def STT(out, a, s, b, op0, op1, r, w):
        P.op('dve', lambda e: e.scalar_tensor_tensor(out=out, in0=a, scalar=s, in1=b, op0=op0, op1=op1), r=r, w=w)

    def ACT(out, a, func, r, w, scale=1.0, bias=None):
        if bias is None:
            P.op('act', lambda e: e.activation(out=out, in_=a, func=func, scale=scale), r=r, w=w)
        else:
            P.op('act', lambda e: e.activation(out=out, in_=a, func=func, scale=scale, bias=bias), r=r, w=w)

    def MM(out, lhsT, rhs, r, w, start=True, stop=True):
        P.op('pe', lambda e: e.matmul(out, lhsT=lhsT, rhs=rhs, start=start, stop=stop), r=r, w=w)

    def RED(out, a, op, r, w):
        P.op('dve', lambda e: e.tensor_reduce(out=out, in_=a, axis=AX.X, op=op), r=r, w=w)

    if True:
        ones = A.alloc("ones", [128, 128])
        selq_sb = A.alloc("selq", [128, 4])
        P.dma('sp', selq_sb[:], selq, w=['selq'])
        eps_sb = A.alloc("eps", [128, 1])
        mod_sb = A.alloc("mod", [128, 48, 2])
        A1 = A.alloc("A1", [128, 8, 2])
        A2 = A.alloc("A2", [128, 8, 2])
        g12_sb = A.alloc("g12", [128, 2, 8])
        ident_sb = A.alloc("ident", [128, 128])
        wrt_sb = A.alloc("wrt", [128, 8, 36])
        P.op('pool', lambda e: e.memset(ones[:], 1.0), w=['ones'])
        P.op('pool', lambda e: e.memset(eps_sb[:], NORM_EPS), w=['eps'])
        P.dma('sp', g12_sb[:], g12, w=['g12'])
        P.dma('sp', ident_sb[:], ident, w=['ident'])
        P.dma('sp', wrt_sb[:], wrt, w=['wrt'])
        mB1 = A.mark()
        c_sb = A.alloc("c", [128, 8, 2])
        s_sb = A.alloc("s", [128, 8, 2])
        bada_sb = A.alloc("bada", [128, 48])
        wgate_b = A.alloc("wgate_b", [128, 8, 3072], BF16)
        wbr_b = A.alloc("wbr_b", [128, 12, 1024], BF16)
        wout_b = A.alloc("wout_b", [128, 8, 1024], BF16)
        xt = [A.alloc("xt%d" % i, [128, 8, 512]) for i in range(2)]
        sq = A.alloc("sq", [128, 8, 512])
        rstd = A.alloc("rstd", [128, 512])
        xn1 = A.alloc("xn1", [128, 8, 512], BF16)
        brf = A.alloc("brf", [128, 4, 512])
        gTt = A.alloc("gTt", [32, 512])
        brb = A.alloc("brb", [128, 12, 512], BF16)
        sig = [A.alloc("sig%d" % i, [128, 512]) for i in range(2)]
        mrg = A.alloc("mrg", [128, 2, 512])
        mk_ = A.mark()
        mrgb = A.alloc("mrgb", [128, 8, 512], BF16)
        A.release(mk_)
        brsel = A.alloc("brsel", [128, 4, 512])
        x2 = A.alloc("x2", [128, 8, 512])
        rt = A.alloc("rt", [128, 80])
        g32 = A.alloc("g32", [128, 32])
        P.dma('sp', c_sb[:], cT, w=['c_sb'])
        P.dma('sp', bada_sb[:], bada, w=['bada'])
        ACT(s_sb[:], c_sb[:], AF.Silu, ['c_sb'], ['s_sb'])
        for piece in range(12):
            b = piece % 2
            P.dma('sp', xt[b][:], wada[:, :, piece * 512:(piece + 1) * 512], w=[('xt', b)])
            for occ in range(4):
                oc = piece * 4 + occ
                for k in range(8):
                    MM(bank[0][:, oc * 2:oc * 2 + 2], xt[b][:, k, occ * 128:(occ + 1) * 128], s_sb[:, k, :],
                       [('xt', b), 's_sb'], [('ps', 0)], start=(k == 0), stop=(k == 7))
        TT('dve', mod_sb[:], bank[0][:, 0:96].rearrange("p (a b) -> p a b", b=2),
           bada_sb[:].unsqueeze(2).broadcast_to([128, 48, 2]), ALU.add, [('ps', 0), 'bada'], ['mod'])
        for (Ax, m_scale, gi) in ((A1, 1, 0), (A2, 4, 1)):
            TS('dve', Ax[:], mod_sb[:, m_scale * 8:(m_scale + 1) * 8, :], 1.0, None, ALU.add, None, ['mod'], ['Ax%d' % gi])
            TT('dve', Ax[:], Ax[:], g12_sb[:, gi, :].unsqueeze(2).broadcast_to([128, 8, 2]), ALU.mult, ['Ax%d' % gi, 'g12'], ['Ax%d' % gi])
        wi = 0
        for (src, dstw, nk, ncol, key) in ((wgate, wgate_b, 8, 3072, 'wgate_b'), (wbr, wbr_b, 12, 1024, 'wbr_b'), (wout, wout_b, 8, 1024, 'wout_b')):
            for c0 in range(0, ncol, 512):
                for kb in range(0, nk, 8):
                    kn = min(8, nk - kb)
                    b = wi % 2
                    wi += 1
                    P.dma('sp', xt[b][:, :kn, :], src[:, kb:kb + kn, c0:c0 + 512], w=[('xt', b)])
                    P.op('pool' if wi % 2 else 'dve', lambda e, b=b, kn=kn, kb=kb, c0=c0, dstw=dstw: e.tensor_copy(
                        out=dstw[:, kb:kb + kn, c0:c0 + 512], in_=xt[b][:, :kn, :]), r=[('xt', b)], w=[key])
        tiles = [(i * 512, 512, 0) for i in range(NT_LAT // 512)]
        if NT_CTX:
            tiles.append((NT_LAT, NT_CTX, 1))

        def norm_mod(src, sz, j, Ax, m_shift, out_fn, okeys, srckeys):
            P.op('act', lambda e: e.activation(out=sq[:, :, :sz], in_=src[:, :, :sz], func=AF.Square), r=srckeys, w=['sq'])
            for k in range(8):
                MM(bank[0][:, :sz], ones[:], sq[:, k, :sz], ['ones', 'sq'], [('ps', 0)], start=(k == 0), stop=(k == 7))
            ACT(rstd[:, :sz], bank[0][:, :sz], AF.Sqrt, [('ps', 0), 'eps'], ['rstd'], scale=1.0 / 1024, bias=eps_sb[:, 0:1])
            P.op('dve', lambda e: e.reciprocal(out=rstd[:, :sz], in_=rstd[:, :sz]), r=['rstd'], w=['rstd'])
            TT('dve', sq[:, :, :sz], src[:, :, :sz], rstd[:, :sz].unsqueeze(1).broadcast_to([128, 8, sz]), ALU.mult,
               srckeys + ['rstd', 'sq'], ['sq'])
            for k in range(8):
                if k % 2:
                    TS('dve', out_fn(k), sq[:, k, :sz], Ax[:, k, j:j + 1],
                       mod_sb[:, m_shift * 8 + k, j:j + 1], ALU.mult, ALU.add, ['sq', 'Ax0', 'Ax1', 'mod'], okeys)
                else:
                    ACT(out_fn(k), sq[:, k, :sz], AF.Identity, ['sq', 'Ax0', 'Ax1', 'mod'], okeys, scale=Ax[:, k, j:j + 1],
                        bias=mod_sb[:, m_shift * 8 + k, j:j + 1])

        pi = 0
        for ti, (t0, sz, j) in enumerate(tiles):
            b = ti % 2
            x_load(xt[b], t0, sz, ('xt', b))
            for n3 in range(3):
                for q in range(4):
                    col0 = (off_lat + q * NT_LAT + t0) if j == 0 else (q * 64 + (t0 - NT_LAT))
                    P.dma('sp', brf[:, :, :sz], gath.gsrc(n3, col0, sz), r=[('gath', n3)], w=['brf'])
                    if q == 0:
                        TS('dve', brsel[:, :, :sz], brf[:, :, :sz], selq_sb[:, 0:1], None, ALU.mult, None, ['brf', 'selq'], ['mrgb'])
                    else:
                        STT(brsel[:, :, :sz], brf[:, :, :sz], selq_sb[:, q:q + 1], brsel[:, :, :sz], ALU.mult, ALU.add,
                            ['brf', 'selq', 'mrgb'], ['mrgb'])
                P.op('pool', lambda e, sz=sz, n3=n3: e.tensor_copy(out=brb[:, n3 * 4:(n3 + 1) * 4, :sz], in_=brsel[:, :, :sz]),
                     r=['mrgb'], w=['brb'])
            norm_mod(xt[b], sz, j, A1, 0, lambda k, sz=sz: xn1[:, k, :sz], ['xn1'], [('xt', b)])
            for dc in range(8):
                for n in range(3):
                    pg = bank[1 + pi % 2]
                    pgk = ('ps', 1 + pi % 2)
                    pbk = bank[3 + pi % 2]
                    pbkk = ('ps', 3 + pi % 2)
                    sg_ = sig[pi % 2]
                    sgk = ('sig', pi % 2)
                    pi += 1
                    for k in range(8):
                        MM(pg[:, :sz], wgate_b[:, k, n * 1024 + dc * 128:n * 1024 + (dc + 1) * 128], xn1[:, k, :sz],
                           ['wgate_b', 'xn1'], [pgk], start=(k == 0), stop=(k == 7))
                    for kc in range(4):
                        MM(pbk[:, :sz], wbr_b[:, n * 4 + kc, dc * 128:(dc + 1) * 128], brb[:, n * 4 + kc, :sz],
                           ['wbr_b', 'brb'], [pbkk], start=(kc == 0), stop=(kc == 3))
                    ACT(sg_[:, :sz], pg[:, :sz], AF.Sigmoid, [pgk], [sgk])
                    if n == 0:
                        TT('dve', mrg[:, dc % 2, :sz], sg_[:, :sz], pbk[:, :sz], ALU.mult, [sgk, pbkk], ['mrg'])
                    else:
                        TT('dve', sg_[:, :sz], sg_[:, :sz], pbk[:, :sz], ALU.mult, [sgk, pbkk], [sgk])
                        TT('pool', mrg[:, dc % 2, :sz], mrg[:, dc % 2, :sz], sg_[:, :sz], ALU.add, ['mrg', sgk], ['mrg'])
                P.op('act', lambda e, dc=dc, sz=sz: e.copy(out=mrgb[:, dc, :sz], in_=mrg[:, dc % 2, :sz]), r=['mrg'], w=['mrgb'])
            for dc in range(8):
                pb_ = bank[5 + dc % 2]
                pk_ = ('ps', 5 + dc % 2)
                for k in range(8):
                    MM(pb_[:, :sz], wout_b[:, k, dc * 128:(dc + 1) * 128], mrgb[:, k, :sz], ['wout_b', 'mrgb'], [pk_],
                       start=(k == 0), stop=(k == 7))
                STT(x2[:, dc, :sz], pb_[:, :sz], mod_sb[:, 2 * 8 + dc, j:j + 1], xt[b][:, dc, :sz], ALU.mult, ALU.add,
                    [pk_, 'mod', ('xt', b)], ['x2'])
            P.dma('pool', x2s[:, :, t0:t0 + sz], x2[:, :, :sz], r=['x2'], w=['x2s'])
            xn2f = xt[b]
            xfk = ('xt', b)
            norm_mod(x2, sz, j, A2, 3, lambda k, sz=sz, xn2f=xn2f: xn2f[:, k, :sz], [xfk], ['x2'])
            P.op('act', lambda e, sz=sz, xn2f=xn2f: e.copy(out=xn1[:, :, :sz], in_=xn2f[:, :, :sz]), r=[xfk], w=['xn1'])
            P.dma('pool', xn2s[:, :, t0:t0 + sz], xn1[:, :, :sz], r=['xn1'], w=['xn2s'])
            for s0 in range(0, sz, 128):
                ns = min(128, sz - s0)
                for k in range(8):
                    MM(bank[7][:ns, 0:36], xn2f[:, k, s0:s0 + ns], wrt_sb[:, k, :], [xfk, 'wrt'], [('ps', 7)],
                       start=(k == 0), stop=(k == 7))
                lg = rt[:ns, 0:36]
                P.op('dve', lambda e, ns=ns: e.tensor_copy(out=rt[:ns, 0:36], in_=bank[7][:ns, 0:36]), r=[('ps', 7)], w=['rt'])
                gmax = rt[:ns, 36:37]
                RED(gmax, rt[:ns, 0:4], ALU.max, ['rt'], ['rt'])
                ohg = rt[:ns, 37:41]
                TS('dve', ohg, rt[:ns, 0:4], gmax, None, ALU.is_equal, None, ['rt'], ['rt'])
                ngm = rt[:ns, 41:42]
                TS('dve', ngm, gmax, -1.0, None, ALU.mult, None, ['rt'], ['rt'])
                eg = rt[:ns, 42:46]
                ACT(eg, rt[:ns, 0:4], AF.Exp, ['rt'], ['rt'], bias=ngm)
                pgr = rt[:ns, 46:47]
                RED(pgr, eg, ALU.add, ['rt'], ['rt'])
                P.op('dve', lambda e, pgr=pgr: e.reciprocal(out=pgr, in_=pgr), r=['rt'], w=['rt'])
                TT('dve', g32[:ns, :].rearrange("p (g e) -> p g e", e=8), rt[:ns, 4:36].rearrange("p (g e) -> p g e", e=8),
                   ohg.unsqueeze(2).broadcast_to([ns, 4, 8]), ALU.mult, ['rt'], ['g32'])
                les = rt[:ns, 47:55]
                RED(les, g32[:ns, :].rearrange("p (g e) -> p e g", e=8), ALU.add, ['g32'], ['rt'])
                top1 = rt[:ns, 55:56]
                RED(top1, les, ALU.max, ['rt'], ['rt'])
                oh1 = rt[:ns, 56:64]
                TS('dve', oh1, les, top1, None, ALU.is_equal, None, ['rt'], ['rt'])
                le2 = rt[:ns, 64:72]
                STT(le2, oh1, -1e30, les, ALU.mult, ALU.add, ['rt'], ['rt'])
                top2 = rt[:ns, 72:73]
                RED(top2, le2, ALU.max, ['rt'], ['rt'])
                oh2 = rt[:ns, 73:81] if False else None
                d12 = rt[:ns, 41:42]
                TT('dve', d12, top1, top2, ALU.subtract, ['rt'], ['rt'])
                ga = rt[:ns, 42:43]
                gb = rt[:ns, 43:44]
                ACT(ga, d12, AF.Sigmoid, ['rt'], ['rt'])
                ACT(gb, d12, AF.Sigmoid, ['rt'], ['rt'], scale=-1.0)
                TT('dve', rt[:ns, 42:44], rt[:ns, 42:44], pgr.broadcast_to([ns, 2]), ALU.mult, ['rt'], ['rt'])
                TS('dve', le2, le2, top2, gb, ALU.is_equal, ALU.mult, ['rt'], ['rt'])
                STT(les, oh1, ga, le2, ALU.mult, ALU.add, ['rt'], ['rt'])
                TT('dve', g32[:ns, :].rearrange("p (g e) -> p g e", e=8), ohg.unsqueeze(2).broadcast_to([ns, 4, 8]),
                   les.unsqueeze(1).broadcast_to([ns, 4, 8]), ALU.mult, ['rt', 'g32'], ['g32'])
                MM(bank[7][0:32, 64:64 + ns], g32[:ns, :], ident_sb[:ns, :ns], ['g32', 'ident'], [('ps', 7)])
                P.op('act', lambda e, s0=s0, ns=ns: e.copy(out=gTt[:, s0:s0 + ns], in_=bank[7][0:32, 64:64 + ns]),
                     r=[('ps', 7)], w=['gTt'])
            P.dma('pool', gTs[:, t0:t0 + sz], gTt[:, :sz], r=['gTt'], w=['gTs'])
        P.fence()
        A.release(mB1)
        selE_sb = A.alloc("selE", [32, 32 * 128])
        P.dma('sp', selE_sb[:], selE, w=['selE'])
        lat_ = tiles[:NT_LAT // 512]
        groups = [lat_[i:i + 2] for i in range(0, len(lat_), 2)]
        if NT_CTX:
            groups[-1] = groups[-1] + [tiles[-1]]
        GMAX = max(sum(t[1] for t in g) for g in groups)
        yacc = A.alloc("yacc", [128, 8, GMAX])
        xn2 = A.alloc("xn2g", [128, 8, GMAX], BF16)
        gT = A.alloc("gTg", [32, GMAX])
        gTb = A.alloc("gTgb", [32, GMAX], BF16)
        selEb = A.alloc("selEb", [32, 32 * 128], BF16)
        P.op('dve', lambda e: e.tensor_copy(out=selEb[:], in_=selE_sb[:]), r=['selE'], w=['selEb'])
        wst = [A.alloc("wst%d" % i, [128, 4, 512]) for i in range(4)]
        wgb = [A.alloc("wgb%d" % i, [128, 8, 512], BF16) for i in range(2)]
        wub = [A.alloc("wub%d" % i, [128, 8, 512], BF16) for i in range(2)]
        wdb = [A.alloc("wdb%d" % i, [128, 4, 1024], BF16) for i in range(2)]
        hs = [A.alloc("hs%d" % i, [128, 512]) for i in range(2)]
        actb = [A.alloc("actb%d" % i, [128, 4, 512], BF16) for i in range(2)]
        x2r = A.alloc("x2r", [128, 8, 512])
        si = 0
        ai = 0
        hi_ = 0
        for gi, grp in enumerate(groups):
            g0 = grp[0][0]
            gsz = sum(t[1] for t in grp)
            P.dma('sp', xn2[:, :, :gsz], xn2s[:, :, g0:g0 + gsz], r=['xn2s'], w=['xn2g'])
            P.dma('sp', gT[:, :gsz], gTs[:, g0:g0 + gsz], r=['gTs'], w=['gTg'])
            P.op('dve', lambda e, gsz=gsz: e.tensor_copy(out=gTb[:, :gsz], in_=gT[:, :gsz]), r=['gTg'], w=['gTgb'])
            for e_ in range(NEXP):
                wb = e_ % 2
                pieces = [(wg[e_, :, 0:4, :], wgb[wb][:, 0:4, :], ('wgb', wb)), (wg[e_, :, 4:8, :], wgb[wb][:, 4:8, :], ('wgb', wb)),
                          (wu[e_, :, 0:4, :], wub[wb][:, 0:4, :], ('wub', wb)), (wu[e_, :, 4:8, :], wub[wb][:, 4:8, :], ('wub', wb)),
                          (wd[e_, :, :, 0:512], wdb[wb][:, :, 0:512], ('wdb', wb)), (wd[e_, :, :, 512:1024], wdb[wb][:, :, 512:1024], ('wdb', wb))]
                for (src, dst, key) in pieces:
                    sb_ = si % 4
                    si += 1
                    P.dma('sp', wst[sb_][:], src, w=[('wst', sb_)])
                    eng = ('pool', 'dve', 'act')[si % 3] if False else ('pool' if si % 2 else 'act')
                    if eng == 'act':
                        P.op('act', lambda e, dst=dst, sb_=sb_: e.copy(out=dst, in_=wst[sb_][:]), r=[('wst', sb_)], w=[key])
                    else:
                        P.op('pool', lambda e, dst=dst, sb_=sb_: e.tensor_copy(out=dst, in_=wst[sb_][:]), r=[('wst', sb_)], w=[key])
                for (t0, sz, j) in grp:
                    ti = tiles.index((t0, sz, j))
                    lo = t0 - g0
                    MM(bank[0][:, :sz], selEb[:, e_ * 128:(e_ + 1) * 128], gTb[:, lo:lo + sz], ['selEb', 'gTgb'], [('ps', 0)])
                    ab = actb[ai % 2]
                    ak = ('actb', ai % 2)
                    ai += 1
                    for fc in range(4):
                        pgb = bank[1 + fc % 2]
                        pgk = ('ps', 1 + fc % 2)
                        pub = bank[3 + fc % 2]
                        puk = ('ps', 3 + fc % 2)
                        for k in range(8):
                            MM(pgb[:, :sz], wgb[wb][:, k, fc * 128:(fc + 1) * 128], xn2[:, k, lo:lo + sz],
                               [('wgb', wb), 'xn2g'], [pgk], start=(k == 0), stop=(k == 7))
                        for k in range(8):
                            MM(pub[:, :sz], wub[wb][:, k, fc * 128:(fc + 1) * 128], xn2[:, k, lo:lo + sz],
                               [('wub', wb), 'xn2g'], [puk], start=(k == 0), stop=(k == 7))
                        h_ = hs[hi_ % 2]
                        hk = ('hs', hi_ % 2)
                        hi_ += 1
                        ACT(h_[:, :sz], pgb[:, :sz], AF.Silu, [pgk], [hk])
                        TT('dve', h_[:, :sz], h_[:, :sz], pub[:, :sz], ALU.mult, [hk, puk], [hk])
                        TT('dve', ab[:, fc, :sz], h_[:, :sz], bank[0][:, :sz], ALU.mult, [hk, ('ps', 0)], [ak])
                    for dc in range(8):
                        pdb = bank[5 + dc % 3]
                        pdk = ('ps', 5 + dc % 3)
                        for fc in range(4):
                            MM(pdb[:, :sz], wdb[wb][:, fc, dc * 128:(dc + 1) * 128], ab[:, fc, :sz], [('wdb', wb), ak], [pdk],
                               start=(fc == 0), stop=(fc == 3))
                        if e_ == 0:
                            P.op('act', lambda e, dc=dc, lo=lo, sz=sz, pdb=pdb: e.copy(out=yacc[:, dc, lo:lo + sz], in_=pdb[:, :sz]),
                                 r=[pdk], w=['yacc'])
                        else:
                            TT('pool' if False else 'dve', yacc[:, dc, lo:lo + sz], yacc[:, dc, lo:lo + sz], pdb[:, :sz], ALU.add,
                               ['yacc', pdk], ['yacc'])
            for (t0, sz, j) in grp:
                lo = t0 - g0
                P.dma('sp', x2r[:, :, :sz], x2s[:, :, t0:t0 + sz], r=['x2s'], w=['x2r'])
                for dc in range(8):
                    STT(x2r[:, dc, :sz], yacc[:, dc, lo:lo + sz], mod_sb[:, 5 * 8 + dc, j:j + 1], x2r[:, dc, :sz], ALU.mult, ALU.add,
                        ['yacc', 'mod', 'x2r'], ['x2r'])
                x_store(x2r, t0, sz, ['x2r'])
        P.fence()
        A.release(base_mark)


POOL_WINDOWS = (2, 4, 8, 16)
def fm(a):
    return np.ascontiguousarray(a.reshape(8, 128, *a.shape[1:]).swapaxes(0, 1))
def colsel(hg):
    c = []
    c += list(range(hg * 128, hg * 128 + 128))
    c += list(range(512 + hg * 128, 512 + hg * 128 + 128))
    c += list(range(1024 + hg * 128, 1024 + hg * 128 + 128))
    c += list(range(1536 + hg * 128, 1536 + hg * 128 + 128))
    for j in range(3):
        c += list(range(2048 + j * 512 + hg * 128, 2048 + j * 512 + hg * 128 + 128))
    c += list(range(2048 + 1536, 2048 + 1920))
    return np.array(c)
def rope_tables(L):
    nrow = L // 64
    inv = (10000.0 ** (-np.arange(16, dtype=np.float32) / 16)).astype(np.float32)
    R = np.ones((128, 2, nrow), np.float32); C = np.ones((128, 2, 64), np.float32)
    perm = np.zeros((128, 128), np.float32)
    rows = np.arange(nrow, dtype=np.float32); cols = np.arange(64, dtype=np.float32)
    for p in range(128):
        d = p % 64
        blk = d // 16
        f = inv[d % 16]
        sign = -1.0 if blk % 2 == 0 else 1.0
        partner = p + 16 if blk % 2 == 0 else p - 16
        perm[partner, p] = 1.0
        if blk < 2:
            ang = (rows * f).astype(np.float32)
            R[p, 0] = np.cos(ang); R[p, 1] = sign * np.sin(ang)
        else:
            ang = (cols * f).astype(np.float32)
            C[p, 0] = np.cos(ang); C[p, 1] = sign * np.sin(ang)
    return R, C, perm
def edge_fix(w, L):
    t = np.arange(L)
    lo = np.clip(t - w // 2, 0, L - 1); hi = np.clip(t + w // 2 - 1, 0, L - 1)
    ratio = (w / (hi - lo + 1)).astype(np.float32)
    return np.concatenate([ratio[:8], ratio[-8:]])
def inputs_A(inp, l, b, hg, L, lam_init, x=None, ctx=None):
    if x is None:
        x = inp['x'][b, :L]; ctx = inp['ctx'][b]
    xa = np.concatenate([ctx, x], 0)
    R, C, perm = rope_tables(L)
    cs = colsel(hg)
    w = POOL_WINDOWS[hg]
    selw = np.zeros((128, 4), np.float32); selw[:, hg] = 1.0 / w
    d = dict(
        xT=fm(np.ascontiguousarray(xa.T)),
        cT=fm(np.stack([inp['c'][b], inp['c_ctx']], 1)),
        wada=fm(inp['w_ada'][l][:, :2048]),
        bada=np.ascontiguousarray(inp['b_ada'][l][:2048].reshape(16, 128).T),
        g1=np.ascontiguousarray(inp['norm1_g'][l].reshape(8, 128).T),
        win=fm(inp['w_in'][l][:, cs]),
        qkg=np.stack([np.tile(inp['q_norm_g'][l], 2), np.tile(inp['k_norm_g'][l], 2)], 1).astype(np.float32),
        ropeR=R, ropeC=C, perm=perm,
        lamqk=np.ascontiguousarray(np.broadcast_to(inp['lambda_qk'][l].reshape(1, 256), (128, 256))),
        subg=np.ascontiguousarray(np.broadcast_to((inp['subln_g'][l] * np.float32(1 - lam_init)).reshape(1, 128), (128, 128))).astype(np.float32),
        wpool=np.ascontiguousarray(inp['pool_w'][l][hg]),
        pscale=np.ascontiguousarray(inp['pool_scale'][l][hg * 128:(hg + 1) * 128].reshape(128, 1)),
        selw=selw,
        efix=np.ascontiguousarray(np.broadcast_to(edge_fix(w, L).reshape(1, 16), (128, 16))),
    )
    return d

def rw_consts():
    i = np.arange(64)
    incl = (i[:, None] <= i[None, :]).astype(np.float32)
    strict = (i[:, None] < i[None, :]).astype(np.float32)
    ones = np.ones((64, 64), np.float32)
    MkN = (i[None, :] < i[:, None]).astype(np.float32)
    return np.concatenate([incl, strict, ones, strict, incl, MkN, np.eye(64, dtype=np.float32)], 1)
def inputs_rw(inp, l, hg):
    mu = inp['shift_mu'][l]
    cmu = np.zeros((128, 6, 2), np.float32)
    p = np.arange(128)
    for g in range(3):
        cmu[:, g, :] = mu[:, g * 512 + hg * 128 + p].T
    for g, base in ((3, 1536), (4, 1664), (5, 1792)):
        cmu[:, g, :] = mu[:, base + p].T
    heads = [2 * hg, 2 * hg + 1]
    w2 = np.zeros((64, 2, 2, 64), np.float32); a2 = np.zeros((64, 2, 2, 64), np.float32)
    w0b = np.zeros((2, 2, 64), np.float32); a0f = np.zeros((64, 2, 2), np.float32)
    prm = np.zeros((64, 2, 5), np.float32)
    for h, hd in enumerate(heads):
        cs = slice(hd * 64, hd * 64 + 64)
        for d in range(2):
            w2[:, d, h, :] = inp['decay_w2'][l][d][:, cs]
            a2[:, d, h, :] = inp['aaa_a2'][l][d][:, cs]
            w0b[d, h, :] = inp['decay_w0'][l][d][cs]
            a0f[:, d, h] = inp['aaa_a0'][l][d][cs]
        prm[:, h, 0] = inp['k_k'][l][cs]; prm[:, h, 1] = inp['k_a'][l][cs]; prm[:, h, 2] = inp['r_k'][l][hd]
        prm[:, h, 3] = inp['gn_w'][l][cs]; prm[:, h, 4] = inp['gn_b'][l][cs]
    return dict(rw_cmu=cmu, rw_g2=np.ascontiguousarray(inp['gate_w2'][l][:, hg * 128:(hg + 1) * 128]),
                rw_w2=w2.reshape(64, 256), rw_a2=a2.reshape(64, 256),
                rw_w0b=np.ascontiguousarray(np.broadcast_to(w0b.reshape(1, 256), (64, 256))),
                rw_a0f=a0f.reshape(64, 4), rw_prm=prm, rw_cst=rw_consts())


def weights_B(inp, l):
    selE = np.zeros((32, 32, 128), np.float32)
    for e in range(32): selE[e, e, :] = 1.0
    return dict(
        wada=fm(inp['w_ada'][l]),
        bada=np.ascontiguousarray(inp['b_ada'][l].reshape(48, 128).T),
        g12=np.ascontiguousarray(np.stack([inp['norm1_g'][l].reshape(8, 128).T, inp['norm2_g'][l].reshape(8, 128).T], 1)),
        wgate=fm(inp['w_in'][l][:, 3968:]),
        wbr=np.ascontiguousarray(inp['w_br'][l].reshape(12, 128, 1024).transpose(1, 0, 2)),
        wout=fm(inp['w_out'][l]),
        wrt=fm(np.concatenate([inp['w_router_group'][l], inp['w_router_expert'][l]], 1)),
        wg=np.ascontiguousarray(inp['w_exp_gate'][l].reshape(32, 8, 128, 512).transpose(0, 2, 1, 3)),
        wu=np.ascontiguousarray(inp['w_exp_up'][l].reshape(32, 8, 128, 512).transpose(0, 2, 1, 3)),
        wd=np.ascontiguousarray(inp['w_exp_down'][l].reshape(32, 4, 128, 1024).transpose(0, 2, 1, 3)),
        selE=selE.reshape(32, 4096), ident=np.eye(128, dtype=np.float32))
def acts_B(xa, br, cvec, c_ctx):
    NT = xa.shape[0]
    return dict(xT=fm(np.ascontiguousarray(xa.T)),
                brT=np.ascontiguousarray(br.T.reshape(12, 128, NT).transpose(1, 0, 2)),
                cT=fm(np.stack([cvec, c_ctx], 1)))


GROUPS = [[0, 1, 2, 3], [4, 5, 6, 7]]


CC_COLS = 2048


class BrStore:
    def __init__(self, nc, name, NQ, ctx_out):
        self.splits = ([(0, 256)] if ctx_out else []) + [(c0, min(CC_COLS, NQ - c0)) for c0 in range(256 if ctx_out else 0, NQ, CC_COLS)]
        self.b = {}
        self.g = {}
        for n in range(3):
            for ci, (c0, cs) in enumerate(self.splits):
                self.b[(n, ci)] = nc.dram_tensor("%s_b%d_%d" % (name, n, ci), [128, cs], F32, kind="Internal").ap()
                self.g[(n, ci)] = nc.dram_tensor("%s_g%d_%d" % (name, n, ci), [4 * 128, cs], F32, kind="Internal").ap()

    def _find(self, col0, ncols):
        for ci, (c0, cs) in enumerate(self.splits):
            if c0 <= col0 and col0 + ncols <= c0 + cs:
                return ci, col0 - c0
        raise AssertionError(("chunk straddle", col0, ncols))

    def dst(self, n, col0, ncols):
        ci, o = self._find(col0, ncols)
        return self.b[(n, ci)][:, o:o + ncols]

    def gsrc(self, n, col0, ncols):
        ci, o = self._find(col0, ncols)
        return self.g[(n, ci)].rearrange("(g p) t -> p g t", g=4)[:, :, o:o + ncols]

    def exchange(self, P, rows=(0, 1, 2)):
        for key in self.b:
            if key[0] in rows:
                P.cc(self.b[key], self.g[key], GROUPS, r=[('brb', key[0])], w=[('gath', key[0])])


class XStore:
    def __init__(self, nc, name, NL):
        self.NL = NL
        NT0 = NL + 64
        self.splits = [(c0, min(CC_COLS, NL - c0)) for c0 in range(0, NL, CC_COLS)] + [(NL, 64)]
        self.b = {}
        self.g = {}
        for k in range(8):
            for ci, (c0, cs) in enumerate(self.splits):
                self.b[(k, ci)] = nc.dram_tensor("%s_b%d_%d" % (name, k, ci), [128, cs], F32, kind="Internal").ap()
                self.g[(k, ci)] = nc.dram_tensor("%s_g%d_%d" % (name, k, ci), [4 * 128, cs], F32, kind="Internal").ap()

    def _find(self, col0, ncols):
        for ci, (c0, cs) in enumerate(self.splits):
            if c0 <= col0 and col0 + ncols <= c0 + cs:
                return ci, col0 - c0
        raise AssertionError(("chunk straddle", col0, ncols))

    def store(self, P, src_tile, t0, sz, rkeys):
        ci, o = self._find(t0, sz)
        for k in range(8):
            P.dma('pool', self.b[(k, ci)][:, o:o + sz], src_tile[:, k, :sz], r=rkeys, w=['xnew'])

    def load_local(self, P, dst_tile, t0, sz, key):
        ci, o = self._find(t0, sz)
        for k in range(8):
            P.dma('sp', dst_tile[:, k, :sz], self.b[(k, ci)][:, o:o + sz], r=['xnew'], w=[key])

    def load_gathered(self, P, dst_tile, dcol, rank, t0, sz, key):
        ci, o = self._find(t0, sz)
        for k in range(8):
            P.dma('sp', dst_tile[:, k, dcol:dcol + sz], self.g[(k, ci)][rank * 128:(rank + 1) * 128, o:o + sz], r=['xg'], w=[key])

    def exchange(self, P):
        for key in self.b:
            P.cc(self.b[key], self.g[key], GROUPS, r=['xnew'], w=['xg'])


def build_fused(L=16384, nexp=32, stop_after=99):
    nc = bass.Bass("TRN2", target_bir_lowering=False)
    nc.allow_low_precision("bf16 matmul operands, fp32 accumulation")
    P = Prog(nc)
    A = Arena(nc)
    T = 256 + L
    NL = L // 4
    NT0 = NL + 64
    lam = [0.8 - 0.6 * math.exp(-0.3 * l) for l in range(2)]
    with contextlib.ExitStack() as st:
        bank = [st.enter_context(nc.psum_tensor("bank%d" % i, [128, 512], F32)) for i in range(8)]
        xout = nc.dram_tensor("xout", [128, 8, NL], F32, kind="ExternalOutput").ap()

        def finish():
            stats = P.emit()
            stats['sbuf_peak'] = A.peak
            return nc, stats
        br0 = BrStore(nc, "br0", T, True)
        phase_A(nc, P, A, bank, "A0_", L, True, lam[0], None, br0, pre_rwkv=lambda: br0.exchange(P, (0, 1)))
        P.fence()
        if stop_after == 1:
            return finish()
        br0.exchange(P, (2,))
        P.fence()
        if stop_after == 2:
            return finish()
        xsh = nc.dram_tensor("xsh", [128, 8, NT0], F32, kind="ExternalInput").ap()
        xs = XStore(nc, "xs", NL)

        def x_load0(dst, t0, sz, key):
            P.dma('sp', dst[:, :, :sz], xsh[:, :, t0:t0 + sz], w=[key])
        phase_B(nc, P, A, bank, "B0_", NL, 64, x_load0, br0, 256, lambda src, t0, sz, rk: xs.store(P, src, t0, sz, rk), NEXP=nexp)
        if stop_after == 3:
            return finish()
        xs.exchange(P)
        P.fence()
        if stop_after == 4:
            return finish()

        def x_loader(dst, t0, sz, key):
            if t0 < 256:
                for r in range(4):
                    xs.load_gathered(P, dst, r * 64, r, NL, 64, key)
            else:
                tt = t0 - 256
                xs.load_gathered(P, dst, 0, tt // NL, tt % NL, sz, key)
        br1 = BrStore(nc, "br1", L, False)
        phase_A(nc, P, A, bank, "A1_", L, False, lam[1], x_loader, br1, pre_rwkv=lambda: br1.exchange(P, (0, 1)))
        P.fence()
        if stop_after == 5:
            return finish()
        br1.exchange(P, (2,))
        P.fence()
        if stop_after == 6:
            return finish()

        def x_store1(src, t0, sz, rk):
            P.dma('pool', xout[:, :, t0:t0 + sz], src[:, :, :sz], r=rk, final=True)
        phase_B(nc, P, A, bank, "B1_", NL, 0, lambda dst, t0, sz, key: xs.load_local(P, dst, t0, sz, key), br1, 0, x_store1, NEXP=nexp)
        return finish()


def fused_inputs(inp, L, nexp=32, names=None):
    inp = {k: np.asarray(v) for k, v in inp.items()}
    NL = L // 4
    x = inp['x'][:, :L]
    ctx = inp['ctx']
    lam = [0.8 - 0.6 * math.exp(-0.3 * l) for l in range(2)]
    WB = [weights_B(inp, l) for l in range(2)]
    ident = np.eye(128, dtype=np.float32)
    maps = []
    for i in range(8):
        b, hg = i // 4, i % 4
        d = {}
        for l in range(2):
            a = inputs_A(inp, l, b, hg, L, lam[l], x=x[b], ctx=ctx[b])
            if l == 1:
                a.pop('xT')
            a.update(inputs_rw(inp, l, hg))
            a['identA'] = ident
            for k, v in a.items():
                d["A%d_" % l + k] = v
            for k, v in WB[l].items():
                d["B%d_" % l + k] = v[:nexp] if k in ('wg', 'wu', 'wd') else v
            d["B%d_cT" % l] = fm(np.stack([inp['c'][b], inp['c_ctx']], 1))
            selq = np.zeros((128, 4), np.float32)
            selq[:, hg] = 1.0
            d["B%d_selq" % l] = selq
        xa = np.concatenate([x[b, hg * NL:(hg + 1) * NL], ctx[b, hg * 64:(hg + 1) * 64]], 0)
        d['xsh'] = fm(np.ascontiguousarray(xa.T))
        if names is not None:
            d = {k: v for k, v in d.items() if k in names}
        maps.append(d)
    return maps


def fused_gather(results, L):
    NL = L // 4
    out = np.empty((2, L, 1024), np.float32)
    for i in range(8):
        b, q = i // 4, i % 4
        o = np.asarray(results[i]['xout']).transpose(1, 0, 2).reshape(1024, NL).T
        out[b, q * NL:(q + 1) * NL] = o
    return out


def kernel(**inp):
    inp = {k: np.asarray(v) for k, v in inp.items()}
    L = inp['x'].shape[1]
    nc, _ = build_fused(L)
    maps = fused_inputs(inp, L)
    res = run_bass_kernel_spmd(nc, maps, core_ids=list(range(8)))
    del maps
    return fused_gather(res.results, L)
```
